# Optimizing a Trainium2 kernel written in Bass

```python
import math
import jax, jax.numpy as jnp
from jax import lax
import numpy as np

D_MODEL = 1024
BATCH = 8
SEQ = 2048
DEPTH = 1

GLA_HEADS = 4
GLA_DK = D_MODEL // 8
GLA_DV = D_MODEL // 4
GLA_GATE_RANK = 16
GLA_GATE_TAU = 16.0
GLA_CHUNK = 64
GDN_HEADS = 8
GDN_DK = D_MODEL // 8
GDN_DV = D_MODEL // 8
GDN_CONV = 5
GDN_CHUNK = 64
N_GROUPS = 4
EXPERTS_PER_GROUP = 8
N_EXPERTS = N_GROUPS * EXPERTS_PER_GROUP
TOP_K = 2
D_EXPERT = D_MODEL // 4
MOE_BLOCK = 128
EPS = 1e-6

GLA_QK = GLA_HEADS * GLA_DK
GLA_V = GLA_HEADS * GLA_DV
GDN_QK = GDN_HEADS * GDN_DK
GDN_V = GDN_HEADS * GDN_DV
IN_SPLITS = (
    GLA_QK, GLA_QK, GLA_V, GLA_V,
    GLA_GATE_RANK, GLA_GATE_RANK,
    GDN_QK, GDN_QK, GDN_V, GDN_V,
    4 * GDN_HEADS,
    D_MODEL, D_MODEL,
)
D_IN = sum(IN_SPLITS)

kernel_name = 'hybrid_gla_gdn_hmoe_encoder'


def rms_norm(x, w):
    xf = x.astype(jnp.float32)
    y = xf * lax.rsqrt(jnp.mean(xf * xf, axis=-1, keepdims=True) + EPS)
    return (y * w.astype(jnp.float32)).astype(x.dtype)


def l2_norm(t):
    return t * lax.rsqrt(jnp.sum(t * t, axis=-1, keepdims=True) + EPS)


def to_heads(t, n_heads):
    b, l, _ = t.shape
    return t.reshape(b, l, n_heads, -1).transpose(0, 2, 1, 3)


def from_heads(t):
    b, h, l, d = t.shape
    return t.transpose(0, 2, 1, 3).reshape(b, l, h * d)


def rev(t):
    return jnp.flip(t, axis=2)


def gla_chunked(q, k, v, log_a):
    b_, h_, l_, dk = q.shape
    dv = v.shape[-1]
    c = GLA_CHUNK
    n = l_ // c
    q, k, log_a = (t.reshape(b_, h_, n, c, dk) for t in (q, k, log_a))
    v = v.reshape(b_, h_, n, c, dv)
    cum = jnp.cumsum(log_a, axis=3)
    cum_last = cum[:, :, :, -1:, :]
    q_dec = q * jnp.exp(cum)
    k_inv = k * jnp.exp(-cum)
    causal = jnp.tril(jnp.ones((c, c), dtype=bool))
    scores = jnp.where(causal, jnp.einsum('bhntd,bhnsd->bhnts', q_dec, k_inv), 0.0)
    o_intra = jnp.einsum('bhnts,bhnsv->bhntv', scores, v)
    k_tail = k * jnp.exp(cum_last - cum)
    chunk_kv = jnp.einsum('bhnsd,bhnsv->bhndv', k_tail, v)
    chunk_decay = jnp.exp(cum_last[:, :, :, 0, :])

    def step(state, inp):
        kv, dec = inp
        return dec[..., None] * state + kv, state

    _, states = lax.scan(step, jnp.zeros((b_, h_, dk, dv), q.dtype),
                         (jnp.moveaxis(chunk_kv, 2, 0), jnp.moveaxis(chunk_decay, 2, 0)))
    states = jnp.moveaxis(states, 0, 2)
    o_inter = jnp.einsum('bhntd,bhndv->bhntv', q_dec, states)
    return (o_intra + o_inter).reshape(b_, h_, l_, dv)


def gdn_chunked(q, k, v, g, beta):
    b_, h_, l_, dk = q.shape
    dv = v.shape[-1]
    c = GDN_CHUNK
    n = l_ // c
    q, k = (t.reshape(b_, h_, n, c, dk) for t in (q, k))
    v = v.reshape(b_, h_, n, c, dv)
    g = jnp.cumsum(g.reshape(b_, h_, n, c), axis=-1)
    beta = beta.reshape(b_, h_, n, c)
    tri = jnp.tril(jnp.ones((c, c), dtype=bool))
    strict = jnp.tril(jnp.ones((c, c), dtype=bool), -1)
    diff = g[..., :, None] - g[..., None, :]
    decay = jnp.where(tri, jnp.exp(jnp.where(tri, diff, 0.0)), 0.0)
    k_beta = k * beta[..., None]
    v_beta = v * beta[..., None]
    lower = jnp.where(strict, jnp.einsum('bhntd,bhnsd->bhnts', k_beta, k) * decay, 0.0)
    unit_lower = lower + jnp.eye(c, dtype=q.dtype)
    u = lax.linalg.triangular_solve(unit_lower, v_beta, left_side=True, lower=True,
                                    unit_diagonal=True)
    w = lax.linalg.triangular_solve(unit_lower, k_beta * jnp.exp(g)[..., None],
                                    left_side=True, lower=True, unit_diagonal=True)
    attn = jnp.where(tri, jnp.einsum('bhntd,bhnsd->bhnts', q, k) * decay, 0.0)
    g_last = g[..., -1]
    k_tail = k * jnp.exp(g_last[..., None] - g)[..., None]
    q_dec = q * jnp.exp(g)[..., None]
    chunk_decay = jnp.exp(g_last)

    def step(state, inp):
        q_c, k_c, u_c, w_c, a_c, dec_c = inp
        v_new = u_c - jnp.einsum('bhtd,bhdv->bhtv', w_c, state)
        o = (jnp.einsum('bhtd,bhdv->bhtv', q_c, state)
             + jnp.einsum('bhts,bhsv->bhtv', a_c, v_new))
        state = dec_c[..., None, None] * state + jnp.einsum('bhsd,bhsv->bhdv', k_c, v_new)
        return state, o

    xs = tuple(jnp.moveaxis(t, 2, 0) for t in (q_dec, k_tail, u, w, attn, chunk_decay))
    _, o = lax.scan(step, jnp.zeros((b_, h_, dk, dv), q.dtype), xs)
    return jnp.moveaxis(o, 0, 2).reshape(b_, h_, l_, dv)


def centred_depthwise_conv(x, w):
    kw, ch = w.shape
    pad = kw // 2
    return lax.conv_general_dilated(
        x, w[:, None, :].astype(x.dtype), window_strides=(1,), padding=[(pad, pad)],
        dimension_numbers=('NWC', 'WIO', 'NWC'), feature_group_count=ch)


def token_mixer(h, w_in, gla_w2_f, gla_b_f, gla_w2_b, gla_b_b, gla_norm_w, conv_w,
                a_log_f, dt_bias_f, a_log_b, dt_bias_b, gdn_norm_w, w_out):
    f32 = jnp.float32
    points = [int(p) for p in np.cumsum(IN_SPLITS)[:-1]]
    proj = h @ w_in
    (gq, gk, gv, gr, glr_f, glr_b, dq, dk, dv, dz, dab, m_a, m_b) = jnp.split(proj, points, axis=-1)

    q = to_heads(gq.astype(f32), GLA_HEADS) * (GLA_DK ** -0.5)
    k = to_heads(gk.astype(f32), GLA_HEADS)
    v = to_heads(gv.astype(f32), GLA_HEADS)
    la_f = to_heads(jax.nn.log_sigmoid((glr_f @ gla_w2_f + gla_b_f).astype(f32)) / GLA_GATE_TAU, GLA_HEADS)
    la_b = to_heads(jax.nn.log_sigmoid((glr_b @ gla_w2_b + gla_b_b).astype(f32)) / GLA_GATE_TAU, GLA_HEADS)
    o = gla_chunked(q, k, v, la_f) + rev(gla_chunked(rev(q), rev(k), rev(v), rev(la_b)))
    o_a = from_heads(rms_norm(o, gla_norm_w)) * jax.nn.silu(gr.astype(f32))

    qkv = jax.nn.silu(centred_depthwise_conv(jnp.concatenate([dq, dk, dv], axis=-1), conv_w))
    cq, ck, cv = jnp.split(qkv.astype(f32), [GDN_QK, 2 * GDN_QK], axis=-1)
    q = l2_norm(to_heads(cq, GDN_HEADS)) * (GDN_DK ** -0.5)
    k = l2_norm(to_heads(ck, GDN_HEADS))
    v = to_heads(cv, GDN_HEADS)
    a_f, a_b, b_f, b_b = (t.transpose(0, 2, 1) for t in jnp.split(dab.astype(f32), 4, axis=-1))
    g_f = -jnp.exp(a_log_f.astype(f32))[None, :, None] * jax.nn.softplus(a_f + dt_bias_f.astype(f32)[None, :, None])
    g_b = -jnp.exp(a_log_b.astype(f32))[None, :, None] * jax.nn.softplus(a_b + dt_bias_b.astype(f32)[None, :, None])
    beta_f = jax.nn.sigmoid(b_f)
    beta_b = jax.nn.sigmoid(b_b)
    o = (gdn_chunked(q, k, v, g_f, beta_f)
         + rev(gdn_chunked(rev(q), rev(k), rev(v), rev(g_b), rev(beta_b))))
    o_b = from_heads(rms_norm(o, gdn_norm_w)) * jax.nn.silu(dz.astype(f32))

    mixed = jax.nn.sigmoid(m_a.astype(f32)) * o_a + jax.nn.sigmoid(m_b.astype(f32)) * o_b
    return mixed.astype(h.dtype) @ w_out


def hierarchical_moe(h, w_group, w_router, w_gate, w_up, w_down):
    f32 = jnp.float32
    b_, l_, d = h.shape
    n_tok = b_ * l_
    t = h.reshape(n_tok, d)
    group_logits = (t @ w_group).astype(f32)
    group_prob = jax.nn.softmax(group_logits, axis=-1)
    group_idx = jnp.argmax(group_logits, axis=-1).astype(jnp.int32)
    group_w = jnp.take_along_axis(group_prob, group_idx[:, None], axis=-1)
    exp_logits = (t @ w_router).astype(f32).reshape(n_tok, N_GROUPS, EXPERTS_PER_GROUP)
    exp_logits = jnp.take_along_axis(exp_logits, group_idx[:, None, None], axis=1)[:, 0]
    top_p, top_i = lax.top_k(jax.nn.softmax(exp_logits, axis=-1), TOP_K)
    weights = group_w * top_p / jnp.sum(top_p, axis=-1, keepdims=True)
    expert_id = group_idx[:, None] * EXPERTS_PER_GROUP + top_i.astype(jnp.int32)

    n_assign = n_tok * TOP_K
    flat_e = expert_id.reshape(n_assign)
    flat_tok = jnp.repeat(jnp.arange(n_tok, dtype=jnp.int32), TOP_K)
    flat_w = weights.reshape(n_assign)
    order = jnp.argsort(flat_e)
    sorted_e = flat_e[order]
    counts = jnp.bincount(flat_e, length=N_EXPERTS)
    padded = (counts + MOE_BLOCK - 1) // MOE_BLOCK * MOE_BLOCK
    start = jnp.cumsum(counts) - counts
    ends = jnp.cumsum(padded)
    pstart = ends - padded
    dest = pstart[sorted_e] + jnp.arange(n_assign, dtype=jnp.int32) - start[sorted_e]
    n_blocks = -(-(n_assign + N_EXPERTS * (MOE_BLOCK - 1)) // MOE_BLOCK)
    n_rows = n_blocks * MOE_BLOCK
    row_tok = jnp.full((n_rows,), n_tok, jnp.int32).at[dest].set(flat_tok[order])
    row_w = jnp.zeros((n_rows,), f32).at[dest].set(flat_w[order])
    block_expert = jnp.minimum(
        jnp.searchsorted(ends, jnp.arange(n_blocks, dtype=jnp.int32) * MOE_BLOCK, side='right'),
        N_EXPERTS - 1)
    t_pad = jnp.concatenate([t, jnp.zeros((1, d), t.dtype)], axis=0)
    xb = t_pad[row_tok].reshape(n_blocks, MOE_BLOCK, d)

    def expert_block(args):
        xs, e = args
        hid = jax.nn.silu(xs @ w_gate[e]) * (xs @ w_up[e])
        return hid @ w_down[e]

    yb = lax.map(expert_block, (xb, block_expert)).reshape(n_rows, d)
    out = jnp.zeros((n_tok + 1, d), f32).at[row_tok].add(yb.astype(f32) * row_w[:, None])[:n_tok]
    return out.astype(h.dtype).reshape(b_, l_, d)


def setup_inputs(seed: int = 0) -> dict:
    key = jax.random.key(seed)
    ks = jax.random.split(key, 24)
    f32 = jnp.float32
    nl = DEPTH

    def nrm(k, shape, fan_in):
        return jax.random.normal(k, shape, f32) * (fan_in ** -0.5)

    def gain(k, shape):
        return 1.0 + 0.02 * jax.random.normal(k, shape, f32)

    def dt_bias(k):
        dt = jnp.exp(jax.random.uniform(k, (nl, GDN_HEADS), f32, math.log(1e-3), math.log(1e-1)))
        return dt + jnp.log(-jnp.expm1(-dt))

    def a_log(k):
        return jnp.log(jax.random.uniform(k, (nl, GDN_HEADS), f32, 1.0, 16.0))

    return {
        'x': jax.random.normal(ks[0], (BATCH, SEQ, D_MODEL), f32),
        'norm1_w': gain(ks[1], (nl, D_MODEL)),
        'w_in': nrm(ks[2], (nl, D_MODEL, D_IN), D_MODEL),
        'gla_gate_w2_fwd': nrm(ks[3], (nl, GLA_GATE_RANK, GLA_QK), GLA_GATE_RANK),
        'gla_gate_b_fwd': 0.1 * jax.random.normal(ks[4], (nl, GLA_QK), f32),
        'gla_gate_w2_bwd': nrm(ks[5], (nl, GLA_GATE_RANK, GLA_QK), GLA_GATE_RANK),
        'gla_gate_b_bwd': 0.1 * jax.random.normal(ks[6], (nl, GLA_QK), f32),
        'gla_norm_w': gain(ks[7], (nl, GLA_DV)),
        'gdn_conv_w': nrm(ks[8], (nl, GDN_CONV, 2 * GDN_QK + GDN_V), GDN_CONV),
        'gdn_a_log_fwd': a_log(ks[9]),
        'gdn_dt_bias_fwd': dt_bias(ks[10]),
        'gdn_a_log_bwd': a_log(ks[11]),
        'gdn_dt_bias_bwd': dt_bias(ks[12]),
        'gdn_norm_w': gain(ks[13], (nl, GDN_DV)),
        'w_out': nrm(ks[14], (nl, D_MODEL, D_MODEL), D_MODEL),
        'norm2_w': gain(ks[15], (nl, D_MODEL)),
        'moe_w_group': nrm(ks[16], (nl, D_MODEL, N_GROUPS), D_MODEL),
        'moe_w_router': nrm(ks[17], (nl, D_MODEL, N_EXPERTS), D_MODEL),
        'moe_w_gate': nrm(ks[18], (nl, N_EXPERTS, D_MODEL, D_EXPERT), D_MODEL),
        'moe_w_up': nrm(ks[19], (nl, N_EXPERTS, D_MODEL, D_EXPERT), D_MODEL),
        'moe_w_down': nrm(ks[20], (nl, N_EXPERTS, D_EXPERT, D_MODEL), D_EXPERT),
        'norm_f_w': gain(ks[21], (D_MODEL,)),
    }


def reference(x, norm1_w, w_in, gla_gate_w2_fwd, gla_gate_b_fwd, gla_gate_w2_bwd, gla_gate_b_bwd,
              gla_norm_w, gdn_conv_w, gdn_a_log_fwd, gdn_dt_bias_fwd, gdn_a_log_bwd, gdn_dt_bias_bwd,
              gdn_norm_w, w_out, norm2_w, moe_w_group, moe_w_router, moe_w_gate, moe_w_up,
              moe_w_down, norm_f_w):
    for i in range(DEPTH):
        h = rms_norm(x, norm1_w[i])
        x = x + token_mixer(h, w_in[i], gla_gate_w2_fwd[i], gla_gate_b_fwd[i], gla_gate_w2_bwd[i],
                            gla_gate_b_bwd[i], gla_norm_w[i], gdn_conv_w[i], gdn_a_log_fwd[i],
                            gdn_dt_bias_fwd[i], gdn_a_log_bwd[i], gdn_dt_bias_bwd[i], gdn_norm_w[i],
                            w_out[i])
        h = rms_norm(x, norm2_w[i])
        x = x + hierarchical_moe(h, moe_w_group[i], moe_w_router[i], moe_w_gate[i], moe_w_up[i],
                                 moe_w_down[i])
    return rms_norm(x, norm_f_w)
```

```python
import contextlib
import heapq
import numpy as np
import concourse.bass as bass
import concourse.mybir as mybir
from concourse.bass_utils import run_bass_kernel_spmd

F32 = mybir.dt.float32
BF16 = mybir.dt.bfloat16
I32 = mybir.dt.int32
AF = mybir.ActivationFunctionType
ALU = mybir.AluOpType
AX = mybir.AxisListType

T = 2048
D = 1024
NT = T // 128
KT = D // 128
EPS = 1e-6
SAME_ENGINE_SYNC = True
EPOCH = 20000
SYNC_NS = 120.0
DMA_LAT_NS = 2200.0


class Prog:
    ENGS = ("pe", "act", "dve", "pool", "sp")

    def __init__(self, nc, stack):
        self.nc = nc
        self.stack = stack
        self.streams = {e: [] for e in self.ENGS}
        self.count = {e: 0 for e in self.ENGS}
        self.esems = {e: [] for e in self.ENGS}
        self.known = {e: {} for e in self.ENGS}
        self.last_write = {}
        self.readers = {}
        self.dma_sems = {}
        self.dma_vals = {}
        self.dma_last = {}
        self.enabled = True
        self.seg = []
        self.ticks = {}
        self.nops = 0
        self.seg_base = 0

    def _new_sem(self, name):
        return self.stack.enter_context(self.nc.semaphore(name))

    @staticmethod
    def _psum_fix(reads, writes):
        r2, w2 = [], list(writes)
        for k in reads:
            if isinstance(k, tuple) and k[0] in ("pb", "pbb"):
                if k not in w2:
                    w2.append(k)
            else:
                r2.append(k)
        return r2, w2

    def _record(self, eng, fn, reads, writes, cost, kind, semkey=None):
        reads, writes = self._psum_fix(list(reads), list(writes))
        oid = self.nops
        self.nops += 1
        preds = set()
        for r in reads:
            t = self.last_write.get(r)
            if t is not None:
                preds.add(t)
        for w in writes:
            t = self.last_write.get(w)
            if t is not None:
                preds.add(t)
            preds.update(self.readers.get(w, ()))
        if kind == "dma":
            prev = self.dma_last.get(semkey)
            if prev is not None:
                preds.add(prev)
            self.dma_last[semkey] = oid
        preds = {p for p in preds if p >= self.seg_base}
        self.seg.append(dict(id=oid, eng=eng, fn=fn, preds=preds, cost=float(cost), kind=kind, semkey=semkey))
        for w in writes:
            self.last_write[w] = oid
            self.readers[w] = []
        for r in reads:
            self.readers.setdefault(r, []).append(oid)
        return oid

    def op(self, eng, fn, reads=(), writes=(), cost=300.0):
        if not self.enabled:
            return
        self._record(eng, fn, reads, writes, cost, "op")

    def dma(self, eng, fn, semkey, reads=(), writes=(), nbytes=1 << 20):
        if not self.enabled:
            return
        self._record(eng, fn, reads, writes, DMA_LAT_NS + nbytes / 160.0, "dma", semkey)

    def wait_all(self, eng, keys):
        self._record(eng, None, list(keys), [], 0.0, "op")

    def _schedule_segment(self):
        ops = self.seg
        if not ops:
            return
        byid = {o["id"]: o for o in ops}
        succ = {o["id"]: [] for o in ops}
        indeg = {}
        for o in ops:
            indeg[o["id"]] = len(o["preds"])
            for p in o["preds"]:
                succ[p].append(o["id"])
        ready_t = {o["id"]: 0.0 for o in ops}
        finish = {}
        heaps = {e: [] for e in self.ENGS}
        for o in ops:
            if indeg[o["id"]] == 0:
                heapq.heappush(heaps[o["eng"]], (0.0, o["id"]))
        etime = {e: 0.0 for e in self.ENGS}
        order = {e: [] for e in self.ENGS}
        remaining = len(ops)
        while remaining:
            best = None
            for e in self.ENGS:
                h = heaps[e]
                if not h:
                    continue
                rt, oid = h[0]
                st = max(rt, etime[e])
                if best is None or (st, oid) < (best[0], best[1]):
                    best = (st, oid, e)
            st, oid, e = best
            heapq.heappop(heaps[e])
            o = byid[oid]
            if o["kind"] == "dma":
                etime[e] = st + 150.0
                fin = st + o["cost"]
            else:
                etime[e] = st + o["cost"]
                fin = etime[e]
            finish[oid] = fin
            order[e].append(o)
            remaining -= 1
            for s in succ[oid]:
                so = byid[s]
                lat = SYNC_NS if (so["eng"] != e or o["kind"] == "dma") else (60.0 if e != "pe" else 0.0)
                ready_t[s] = max(ready_t[s], fin + lat)
                indeg[s] -= 1
                if indeg[s] == 0:
                    heapq.heappush(heaps[so["eng"]], (ready_t[s], s))
        self.est_ns = getattr(self, "est_ns", 0.0) + max(list(finish.values()) + [0.0])
        for o in ops:
            if o["kind"] == "dma":
                k = o["semkey"]
                if k not in self.dma_sems:
                    self.dma_sems[k] = self._new_sem(f"d{len(self.dma_sems)}")
                    self.dma_vals[k] = 0
                self.dma_vals[k] += 16
                self.ticks[o["id"]] = (self.dma_sems[k], self.dma_vals[k], "dma")
        for e in self.ENGS:
            for o in order[e]:
                if o["kind"] == "op" and o["fn"] is not None:
                    c = self.count[e]
                    ep, v = divmod(c, EPOCH)
                    while len(self.esems[e]) <= ep:
                        self.esems[e].append(self._new_sem(f"s_{e}_{len(self.esems[e])}"))
                    self.count[e] = c + 1
                    self.ticks[o["id"]] = (self.esems[e][ep], v + 1, e)
        for e in self.ENGS:
            for o in order[e]:
                waits = {}
                for p in o["preds"]:
                    sem, val, src = self.ticks[p]
                    if src == e and (not SAME_ENGINE_SYNC or e == "pe"):
                        continue
                    sid = id(sem)
                    if self.known[e].get(sid, 0) >= val:
                        continue
                    if sid not in waits or waits[sid][1] < val:
                        waits[sid] = (sem, val)
                for sid, (sem, val) in waits.items():
                    self.known[e][sid] = val
                inc = None
                if o["fn"] is not None:
                    sem, val, src = self.ticks[o["id"]]
                    inc = (sem, 16 if o["kind"] == "dma" else 1)
                self.streams[e].append((o["fn"], list(waits.values()), inc))
        self.seg = []
        self.seg_base = self.nops

    def barrier(self):
        if not self.enabled and not self.seg:
            return
        self._schedule_segment()
        ticks = []
        for e2 in self.ENGS:
            c = self.count[e2]
            if c > 0:
                ep, v = divmod(c - 1, EPOCH)
                ticks.append((self.esems[e2][ep], v + 1))
        for k, sem in self.dma_sems.items():
            ticks.append((sem, self.dma_vals[k]))
        for eng in self.ENGS:
            waits = []
            for (sem, val) in ticks:
                if self.known[eng].get(id(sem), 0) >= val:
                    continue
                self.known[eng][id(sem)] = val
                waits.append((sem, val))
            if waits:
                self.streams[eng].append((None, waits, None))

    def emit(self):
        self._schedule_segment()
        nc = self.nc
        with nc.Block() as block:
            def run(e, stream):
                for fn, waits, inc in stream:
                    for sem, val in waits:
                        e.wait_ge(sem, val)
                    if fn is None:
                        continue
                    ins = fn(e)
                    if inc is not None:
                        ins.then_inc(inc[0], inc[1])

            @block.tensor
            def _(e):
                run(e, self.streams["pe"])

            @block.scalar
            def _(e):
                run(e, self.streams["act"])

            @block.vector
            def _(e):
                run(e, self.streams["dve"])

            @block.gpsimd
            def _(e):
                run(e, self.streams["pool"])

            @block.sync
            def _(e):
                run(e, self.streams["sp"])


def _fsz(ap):
    s = ap.shape
    n = 1
    for v in s[1:]:
        n *= int(v)
    return n


C_GQ, C_GK, C_GV, C_GR = 0, 512, 1024, 2048
C_GLF, C_GLB = 3072, 3088
C_DQ, C_DK, C_DV, C_DZ = 3104, 4128, 5152, 6176
C_DAB = 7200
C_MA, C_MB = 7232, 8256
D_IN = 9280


def host_consts():
    r = np.arange(128)[:, None]
    t = np.arange(128)[None, :]
    same = (r // 64) == (t // 64)
    c = {}
    c["ident"] = np.eye(128, dtype=np.float32)
    c["a_le"] = np.where(r <= t, -1.0 / 16, 0.0)
    c["a_ge"] = np.where(r >= t, -1.0 / 16, 0.0)
    c["a_gt"] = np.where(r > t, -1.0 / 16, 0.0)
    c["a_lt"] = np.where(r < t, -1.0 / 16, 0.0)
    c["m_le"] = np.where(r <= t, 1.0, 0.0)
    c["m_ge"] = np.where(r >= t, 1.0, 0.0)
    c["b_le"] = np.where((r <= t) & same, 1.0, 0.0)
    c["b_ge"] = np.where((r >= t) & same, 1.0, 0.0)
    c["b_gt"] = np.where((r > t) & same, 1.0, 0.0)
    c["b_lt"] = np.where((r < t) & same, 1.0, 0.0)
    c["csel0"] = np.where(r < 64, 1.0, 0.0) + 0.0 * t
    c["csel1"] = np.where(r >= 64, 1.0, 0.0) + 0.0 * t
    c["ones"] = np.ones((128, 128))
    names = list(c.keys())
    arr = np.stack([np.asarray(c[n], np.float32) for n in names], axis=1)
    return names, np.ascontiguousarray(arr)


CONST_NAMES, CONST_ARR = host_consts()
NCONST = len(CONST_NAMES)


def build(stage="all", dbg=False):
    nc = bass.Bass("TRN2", target_bir_lowering=False)
    stack = contextlib.ExitStack()
    with stack:
        P = Prog(nc, stack)

        def dram(name, shape, dt=F32, kind="ExternalInput"):
            return nc.dram_tensor(name, list(shape), dt, kind=kind).ap()

        def sb(name, shape, dt=F32):
            return stack.enter_context(nc.sbuf_tensor(name, list(shape), dt))

        def ps(name, shape, dt=F32):
            return stack.enter_context(nc.psum_tensor(name, list(shape), dt))

        def MM(out, lhsT, rhs, start, stop, R, W):
            n = _fsz(rhs)
            c = 70.0 + n * 0.75
            if rhs.dtype == F32:
                c *= 4.0
            P.op("pe", lambda e: e.matmul(out, lhsT, rhs, start=start, stop=stop), R, W, cost=c)

        def TR(out, in_, ident, R, W):
            P.op("pe", lambda e: e.transpose(out=out, in_=in_, identity=ident), R, W, cost=110.0)

        def ACTF(out, in_, func, R, W, **kw):
            c = 120.0 + _fsz(in_) * 0.6 + (90.0 if "accum_out" in kw else 0.0)
            P.op("act", lambda e: e.activation(out=out, in_=in_, func=func, **kw), R, W, cost=c)

        def _vc(eng, n, k=1.5):
            return (100.0 + n * k * 0.6) if eng == "dve" else (150.0 + n * 1.9)

        def TT(eng, out, in0, in1, op, R, W):
            P.op(eng, lambda e: e.tensor_tensor(out=out, in0=in0, in1=in1, op=op), R, W, cost=_vc(eng, _fsz(out)))

        def TS(eng, out, in0, s1, s2, op0, op1, R, W):
            P.op(eng, lambda e: e.tensor_scalar(out=out, in0=in0, scalar1=s1, scalar2=s2, op0=op0, op1=op1), R, W,
                 cost=_vc(eng, _fsz(out), 1.05))

        def STT(out, in0, scalar, in1, op0, op1, R, W):
            P.op("dve", lambda e: e.scalar_tensor_tensor(out=out, in0=in0, scalar=scalar, in1=in1, op0=op0, op1=op1), R, W,
                 cost=_vc("dve", _fsz(out)))

        def CP(eng, out, in_, R, W):
            if eng == "act":
                P.op("act", lambda e: e.activation(out=out, in_=in_, func=AF.Copy), R, W, cost=120.0 + _fsz(in_) * 0.6)
            else:
                P.op(eng, lambda e: e.tensor_copy(out=out, in_=in_), R, W, cost=_vc(eng, _fsz(out), 1.05))

        def MEMSET(eng, ap, val, W):
            P.op(eng, lambda e: e.memset(ap, val), [], W, cost=_vc(eng, _fsz(ap), 0.6))

        def DMA(eng, out, in_, semkey, R, W):
            P.dma(eng, lambda e: e.dma_start(out=out, in_=in_), semkey, R, W, nbytes=_fsz(out) * int(out.shape[0]) * 4)

        def RECIP(out, in_, R, W):
            P.op("dve", lambda e: e.reciprocal(out=out, in_=in_), R, W, cost=_vc("dve", _fsz(out), 1.05))

        def MARK(name):
            if stage == name:
                P.enabled = False

        def rstd_inplace(ap, n, key):
            TS("dve", ap, ap, 1.0 / n, EPS, ALU.mult, ALU.add, [key], [key])
            ACTF(ap, ap, AF.Ln, [key], [key])
            ACTF(ap, ap, AF.Exp, [key], [key], scale=-0.5)

        x_d = dram("x", [T, D])
        n1_d = dram("norm1_w", [1, D])
        n2_d = dram("norm2_w", [1, D])
        nf_d = dram("norm_f_w", [1, D])
        consts_d = dram("consts", [128, NCONST, 128])
        w_in_d = dram("w_in", [D, D_IN])
        w2b_d = [dram("gla_w2b_f", [17, 512]), dram("gla_w2b_b", [17, 512])]
        gnw_d = dram("gla_norm_w", [1, 256])
        out_d = dram("out", [T, D], kind="ExternalOutput")
        dbg_d = dram("dbg", [T, D], kind="ExternalOutput") if dbg else None

        consts = sb("consts_sb", [128, NCONST, 128])
        CI = {n: i for i, n in enumerate(CONST_NAMES)}

        def cst(name):
            return consts[:, CI[name], :]

        ident_b = sb("ident_b", [128, 128], BF16)
        ones_b = sb("ones_b", [128, 128], BF16)
        hT = sb("hT", [128, KT, T], BF16)
        mixed = sb("mixed", [128, NT, D], BF16)
        small = sb("small", [128, 64])
        ARENA_BYTES = 134 * 1024
        arena = sb("arena", [128, ARENA_BYTES // 4])

        def carve(off, shape, dt, base=None, cap=None):
            base = arena if base is None else base
            cap = ARENA_BYTES if cap is None else cap
            nb = int(np.prod(shape)) * (2 if dt == BF16 else 4)
            assert off % 4 == 0 and off + nb <= cap, (off, nb)
            v = base[:, off // 4:(off + nb) // 4]
            if dt != F32:
                v = v.bitcast(dt)
            if len(shape) == 2:
                pat = "p (a b) -> p a b"
                v = v.rearrange(pat, a=shape[0])
            elif len(shape) == 3:
                v = v.rearrange("p (a b c) -> p a b c", a=shape[0], b=shape[1])
            return v, off + nb

        pt = [ps(f"pt{i}", [128, 1024]) for i in range(3)]
        ptb = ps("ptb", [128, 2048], BF16)

        def bank(i):
            return pt[i // 2][:, (i % 2) * 512:(i % 2 + 1) * 512]

        def bkey(i):
            return ("pb", i)

        def bbank(i):
            return ptb[:, i * 1024:(i + 1) * 1024]

        DMA("sp", consts[:], consts_d[:, :, :], "c_consts", [], ["consts"])
        CP("dve", ident_b[:], cst("ident"), ["consts"], ["ident_b"])
        MEMSET("pool", ones_b[:], 1.0, ["ones_b"])

        off = 0
        xt0, off = carve(off, [D], F32)
        xt1, off = carve(off, [D], F32)
        hn0, off = carve(off, [D], BF16)
        hn1, off = carve(off, [D], BF16)
        sq, off = carve(off, [D], F32)
        n1_bc, off = carve(off, [D], F32)
        DMA("sp", n1_bc, n1_d.partition_broadcast(128), "c_n1", [], ["n1_bc"])
        xts = [xt0, xt1]
        hns = [hn0, hn1]
        for tt in range(NT):
            b = tt % 2
            xb, hb = xts[b], hns[b]
            DMA("sp", xb, x_d[tt * 128:(tt + 1) * 128, :], ("xt", b), [], [("xt", b)])
            ACTF(sq, xb, AF.Square, [("xt", b)], ["sq", "ss0"], accum_out=small[:, 0:1])
            rstd_inplace(small[:, 0:1], D, "ss0")
            STT(hb, xb, small[:, 0:1], n1_bc, ALU.mult, ALU.mult, [("xt", b), "ss0", "n1_bc"], [("hn", b)])
            for kt in range(KT):
                TR(bbank(b)[:, kt * 128:(kt + 1) * 128], hb[:, kt * 128:(kt + 1) * 128], ident_b[:],
                   [("hn", b), "ident_b"], [("pbb", b)])
            CP("act", hT[:, :, tt * 128:(tt + 1) * 128], bbank(b).rearrange("p (k t) -> p k t", k=KT),
               [("pbb", b)], [("hT", tt)])
        HT_ALL = [("hT", tt) for tt in range(NT)]
        MARK("p1")

        P.barrier()
        off = 0
        qT, off = carve(off, [T], F32)
        kT, off = carve(off, [T], F32)
        k_tok, off = carve(off, [NT, 128], F32)
        v_tok, off = carve(off, [NT, 256], BF16)
        qdT = [None, None]
        kiT = [None, None]
        ktail = [None, None]
        for d_ in range(2):
            qdT[d_], off = carve(off, [T], BF16)
            kiT[d_], off = carve(off, [T], BF16)
            ktail[d_], off = carve(off, [NT, 128], BF16)
        sb_store, off = carve(off, [NT, 256], BF16)
        dec, off = carve(off, [2, NT], F32)
        S, off = carve(off, [256], F32)
        S_bf, off = carve(off, [256], BF16)
        NTMP = 4
        tmp = []
        for i in range(NTMP):
            d = {}
            for nm in ("e", "lg", "E", "Ei", "Et"):
                d[nm], off = carve(off, [128], F32)
            d["Pf"], off = carve(off, [128], BF16)
            d["Pb"], off = carve(off, [128], BF16)
            d["sig"], off = carve(off, [512], F32)
            d["G"], off = carve(off, [256], F32)
            tmp.append(d)
        gl, off = carve(off, [2, T], BF16)
        w2b, off = carve(off, [2, 512], BF16)
        wqk, off = carve(off, [KT, 256], BF16)
        wkv, off = carve(off, [KT, 384], BF16)
        wgm, off = carve(off, [KT, 512], BF16)
        wgl, off = carve(off, [KT, 32], BF16)
        gnw_bc, off = carve(off, [256], F32)
        GLA_END = off

        DMA("sp", gnw_bc, gnw_d.partition_broadcast(128), "c_gnw", [], ["gnw_bc"])
        MEMSET("pool", gl[0:32, :, :], 1.0, ["gl"])
        MEMSET("pool", w2b[0:32, :, :], 0.0, ["w2b"])
        for d_ in range(2):
            DMA("pool", w2b[0:17, d_, :], w2b_d[d_][:, :], "c_w2b", [], ["w2b"])
        DMA("pool", wgl, w_in_d[:, C_GLF:C_GLF + 32].rearrange("(k p) c -> p k c", p=128), "w_wgl", [], ["wgl"])
        for d_ in range(2):
            for tg in range(4):
                bi = tg % 2
                for kt in range(KT):
                    MM(bank(bi)[0:16, :], wgl[:, kt, d_ * 16:(d_ + 1) * 16], hT[:, kt, tg * 512:(tg + 1) * 512],
                       kt == 0, kt == KT - 1, ["wgl"] + HT_ALL[tg * 4:tg * 4 + 4], [bkey(bi)])
                CP("act", gl[0:16, d_, tg * 512:(tg + 1) * 512], bank(bi)[0:16, :], [bkey(bi)], ["gl"])

        MARK("g0")
        QSCALE = 128.0 ** -0.5
        for h in range(4):
            def wcols(dst, c0, n):
                return (dst, w_in_d[:, c0:c0 + n].rearrange("(k p) c -> p k c", p=128))
            for (dst, src) in (wcols(wqk[:, :, 0:128], C_GQ + h * 128, 128), wcols(wqk[:, :, 128:256], C_GK + h * 128, 128)):
                DMA("pool", dst, src, "w_wqk", [], ["wqk"])
            for (dst, src) in (wcols(wkv[:, :, 0:128], C_GK + h * 128, 128), wcols(wkv[:, :, 128:384], C_GV + h * 256, 256)):
                DMA("pool", dst, src, "w_wkv", [], ["wkv"])
            for (dst, src) in (wcols(wgm[:, :, 0:256], C_GR + h * 256, 256), wcols(wgm[:, :, 256:512], C_MA + h * 256, 256)):
                DMA("pool", dst, src, "w_wgm", [], ["wgm"])
            MARK("g1a")
            for which, dstT in ((0, qT), (1, kT)):
                for tg in range(4):
                    bi = (which * 4 + tg) % 4
                    for kt in range(KT):
                        MM(bank(bi), wqk[:, kt, which * 128:(which + 1) * 128], hT[:, kt, tg * 512:(tg + 1) * 512],
                           kt == 0, kt == KT - 1, ["wqk"] + HT_ALL[tg * 4:tg * 4 + 4], [bkey(bi)])
                    CP("act" if tg % 2 else "dve", dstT[:, tg * 512:(tg + 1) * 512], bank(bi), [bkey(bi)],
                       [("qkT", which, tg)])
            MARK("g1b")
            for n in range(NT):
                bi = 4 + n % 2
                for kt in range(KT):
                    MM(bank(bi)[:, 0:384], hT[:, kt, n * 128:(n + 1) * 128], wkv[:, kt, :],
                       kt == 0, kt == KT - 1, ["wkv", ("hT", n)], [bkey(bi)])
                CP("dve", k_tok[:, n, :], bank(bi)[:, 0:128], [bkey(bi)], [("k_tok", n)])
                CP("act", v_tok[:, n, :], bank(bi)[:, 128:384], [bkey(bi)], [("v_tok", n)])
            MARK("g1")
            for n in range(NT):
                tsl = slice(n * 128, (n + 1) * 128)
                tg = n // 4
                for d_ in range(2):
                    tm = tmp[(n * 2 + d_) % NTMP]
                    tk = ("gtmp", (n * 2 + d_) % NTMP)
                    a_c = cst("a_le") if d_ == 0 else cst("a_ge")
                    a_s = cst("a_gt") if d_ == 0 else cst("a_lt")
                    b0 = (n * 2 + d_) % 2 * 2
                    zb, cb = bank(b0), bank(b0 + 1)
                    MM(zb[:, 0:128], gl[0:32, d_, tsl], w2b[0:32, d_, h * 128:(h + 1) * 128], True, True,
                       ["gl", "w2b"], [bkey(b0)])
                    ACTF(tm["e"], zb[:, 0:128], AF.Exp, [bkey(b0)], [tk], scale=-1.0)
                    ACTF(tm["lg"], tm["e"], AF.Ln, [tk], [tk], bias=1.0)
                    MM(cb[:, 0:128], tm["lg"], a_c, True, True, [tk, "consts"], [bkey(b0 + 1)])
                    MM(cb[:, 128:256], a_s, tm["lg"], True, True, [tk, "consts"], [bkey(b0 + 1)])
                    ACTF(tm["E"], cb[:, 0:128], AF.Exp, [bkey(b0 + 1)], [tk])
                    ACTF(tm["Ei"], cb[:, 0:128], AF.Exp, [bkey(b0 + 1)], [tk], scale=-1.0)
                    ACTF(tm["Et"], cb[:, 128:256], AF.Exp, [bkey(b0 + 1)], [tk])
                    STT(qdT[d_][:, tsl], qT[:, tsl], QSCALE, tm["E"], ALU.mult, ALU.mult,
                        [("qkT", 0, tg), tk], [("qdT", d_, n)])
                    TT("dve", kiT[d_][:, tsl], kT[:, tsl], tm["Ei"], ALU.mult, [("qkT", 1, tg), tk], [("kiT", d_, n)])
                    TT("dve", ktail[d_][:, n, :], k_tok[:, n, :], tm["Et"], ALU.mult, [("k_tok", n), tk], [("ktail", d_, n)])
                    col = 127 if d_ == 0 else 0
                    CP("dve", dec[:, d_, n:n + 1], tm["E"][:, col:col + 1], [tk], [("dec", d_, n)])
            MARK("g2")
            MEMSET("dve", S, 0.0, ["S"])
            for n in range(NT - 1, -1, -1):
                CP("act", sb_store[:, n, :], S, ["S"], [("sb_store", n)])
                bi = 4 + n % 2
                MM(bank(bi)[:, 0:256], ktail[1][:, n, :], v_tok[:, n, :], True, True,
                   [("ktail", 1, n), ("v_tok", n)], [bkey(bi)])
                STT(S, S, dec[:, 1, n:n + 1], bank(bi)[:, 0:256], ALU.mult, ALU.add,
                    ["S", ("dec", 1, n), bkey(bi)], ["S"])
            MARK("g3")
            MEMSET("dve", S, 0.0, ["S"])
            for n in range(NT):
                tsl = slice(n * 128, (n + 1) * 128)
                tm = tmp[n % NTMP]
                tk = ("ftmp", n % NTMP)
                CP("act", S_bf, S, ["S"], ["S_bf"])
                b0 = (n % 2) * 2
                sc = bank(b0)
                MM(sc[:, 0:128], kiT[0][:, tsl], qdT[0][:, tsl], True, True, [("kiT", 0, n), ("qdT", 0, n)], [bkey(b0)])
                MM(sc[:, 128:256], kiT[1][:, tsl], qdT[1][:, tsl], True, True, [("kiT", 1, n), ("qdT", 1, n)], [bkey(b0)])
                TT("dve", tm["Pf"], sc[:, 0:128], cst("m_le"), ALU.mult, [bkey(b0), "consts"], [tk])
                TT("dve", tm["Pb"], sc[:, 128:256], cst("m_ge"), ALU.mult, [bkey(b0), "consts"], [tk])
                ob = bank(b0 + 1)
                ok = bkey(b0 + 1)
                MM(ob[:, 0:256], qdT[0][:, tsl], S_bf, True, False, [("qdT", 0, n), "S_bf"], [ok])
                MM(ob[:, 0:256], qdT[1][:, tsl], sb_store[:, n, :], False, False, [("qdT", 1, n), ("sb_store", n)], [ok])
                MM(ob[:, 0:256], tm["Pf"], v_tok[:, n, :], False, False, [tk, ("v_tok", n)], [ok])
                MM(ob[:, 0:256], tm["Pb"], v_tok[:, n, :], False, True, [tk, ("v_tok", n)], [ok])
                kb = 4 + n % 2
                MM(bank(kb)[:, 0:256], ktail[0][:, n, :], v_tok[:, n, :], True, True,
                   [("ktail", 0, n), ("v_tok", n)], [bkey(kb)])
                STT(S, S, dec[:, 0, n:n + 1], bank(kb)[:, 0:256], ALU.mult, ALU.add,
                    ["S", ("dec", 0, n), bkey(kb)], ["S"])
                gb = 4 + n % 2
                for kt in range(KT):
                    MM(bank(gb), hT[:, kt, tsl], wgm[:, kt, :], kt == 0, kt == KT - 1, ["wgm", ("hT", n)], [bkey(gb)])
                ACTF(tm["sig"], bank(gb), AF.Exp, [bkey(gb)], [("sig", n % NTMP)], scale=-1.0)
                ACTF(tm["sig"], tm["sig"], AF.Ln, [("sig", n % NTMP)], [("sig", n % NTMP)], bias=1.0)
                ACTF(tm["sig"], tm["sig"], AF.Exp, [("sig", n % NTMP)], [("sig", n % NTMP)], scale=-1.0)
                TT("pool", tm["G"], tm["sig"][:, 0:256], tm["sig"][:, 256:512], ALU.mult, [("sig", n % NTMP)], [("G", n % NTMP)])
                TT("dve", tm["G"], tm["G"], bank(gb)[:, 0:256], ALU.mult, [("G", n % NTMP), bkey(gb)], [("G", n % NTMP)])
                TT("pool", tm["G"], tm["G"], gnw_bc, ALU.mult, [("G", n % NTMP), "gnw_bc"], [("G", n % NTMP)])
                ssk = ("ssq", n % 2)
                ssap = small[:, 2 + n % 2:3 + n % 2]
                ACTF(tm["sig"][:, 0:256], ob[:, 0:256], AF.Square, [ok, ("G", n % NTMP)], [("sig", n % NTMP), ssk],
                     accum_out=ssap)
                rstd_inplace(ssap, 256, ssk)
                STT(mixed[:, n, h * 256:(h + 1) * 256], ob[:, 0:256], ssap, tm["G"], ALU.mult, ALU.mult,
                    [ok, ssk, ("G", n % NTMP)], [("mixed", n)])

        P.barrier()
        HG = 4
        off = 0
        gqT, off = carve(off, [HG, T], BF16)
        gkT, off = carve(off, [HG, T], BF16)
        gvT, off = carve(off, [HG, T], BF16)
        o_store, off = carve(off, [NT, HG, 128], BF16)
        dabs, off = carve(off, [NT, 32], F32)
        g_raw, off = carve(off, [NT, 2, 8], F32)
        beta, off = carve(off, [NT, 2, 8], F32)
        gvec, off = carve(off, [64], F32)
        wsl0, off = carve(off, [KT, 512], BF16)
        wsl1, off = carve(off, [KT, 512], BF16)
        wsl = [wsl0, wsl1]
        cwT, off = carve(off, [24, 5], F32)
        gdnw_bc, off = carve(off, [128], F32)
        wdab, off = carve(off, [KT, 32], BF16)
        TMP0 = off
        xc = [None, None]
        xc[0], off = carve(off, [T + 4], BF16)
        xc[1], off = carve(off, [T + 4], BF16)
        diag, off = carve(off, [5, 128], BF16)
        ce = [None, None]
        cy = [None, None]
        for i in range(2):
            ce[i], off = carve(off, [512], F32)
            cy[i], off = carve(off, [512], F32)
        cysq, off = carve(off, [512], BF16)
        crs, off = carve(off, [512], F32)
        CONV_END = off
        off = TMP0
        GMB, off = carve(off, [HG, 128], F32)
        Wd, off = carve(off, [HG, 128], F32)
        decT, off = carve(off, [HG, 128], F32)
        Lm, off = carve(off, [HG, 128], BF16)
        LTm, off = carve(off, [HG, 128], BF16)
        XT, off = carve(off, [HG, 128], BF16)
        Pp = [None, None]
        PTp = [None, None]
        for i in range(2):
            Pp[i], off = carve(off, [HG, 128], BF16)
            PTp[i], off = carve(off, [HG, 128], BF16)
        kbg, off = carve(off, [HG, 128], BF16)
        vbeta, off = carve(off, [HG, 128], BF16)
        qd_tok, off = carve(off, [HG, 128], BF16)
        DB = []
        for d_ in range(2):
            dd = {}
            for nm in ("attnT", "ktl", "qdTg", "wT_sb", "vnew", "Sg_bf"):
                dd[nm], off = carve(off, [HG, 128], BF16)
            dd["u_sb"], off = carve(off, [HG, 128], F32)
            dd["Sg"], off = carve(off, [HG, 128], F32)
            dd["esc"], off = carve(off, [16], F32)
            dd["bg"], off = carve(off, [HG], F32)
            DB.append(dd)
        osum, off = carve(off, [HG, 128], F32)
        fsig, off = carve(off, [1024], F32)
        fG, off = carve(off, [HG, 128], F32)
        frs, off = carve(off, [8], F32)
        SWEEP_END = off

        gdnw_d = dram("gdn_norm_w", [1, 128])
        gvec_d = dram("gdn_vec", [1, 32])
        cw_d = dram("gdn_conv_wT", [128, 24, 5])
        DMA("sp", gdnw_bc, gdnw_d.partition_broadcast(128), "c_gdnw", [], ["gdnw_bc"])
        DMA("sp", gvec[:, 0:32], gvec_d.partition_broadcast(128), "c_gvec", [], ["gvec"])
        DMA("sp", cwT, cw_d[:, :, :], "c_cw", [], ["cwT"])
        DMA("pool", wdab, w_in_d[:, C_DAB:C_DAB + 32].rearrange("(k p) c -> p k c", p=128), "w_wdab", [], ["wdab"])
        ACTF(gvec[:, 16:32], gvec[:, 16:32], AF.Exp, ["gvec"], ["gvec"])
        TS("dve", gvec[:, 16:32], gvec[:, 16:32], -1.0, None, ALU.mult, ALU.bypass, ["gvec"], ["gvec"])
        for n in range(NT):
            bi = n % 2
            for kt in range(KT):
                MM(bank(bi)[:, 0:32], hT[:, kt, n * 128:(n + 1) * 128], wdab[:, kt, :], kt == 0, kt == KT - 1,
                   ["wdab", ("hT", n)], [bkey(bi)])
            CP("act", dabs[:, n, :], bank(bi)[:, 0:32], [bkey(bi)], ["dabs"])
        a_view = dabs[:, :, 0:16]
        b_view = dabs[:, :, 16:32]
        g_flat = g_raw.rearrange("p n d h -> p n (d h)")
        be_flat = beta.rearrange("p n d h -> p n (d h)")
        TT("dve", g_flat, a_view, gvec[:, 0:16].unsqueeze(1).to_broadcast([128, NT, 16]), ALU.add, ["dabs", "gvec"], ["g_raw"])
        ACTF(g_flat, g_flat, AF.Exp, ["g_raw"], ["g_raw"])
        ACTF(g_flat, g_flat, AF.Ln, ["g_raw"], ["g_raw"], bias=1.0)
        TT("dve", g_flat, g_flat, gvec[:, 16:32].unsqueeze(1).to_broadcast([128, NT, 16]), ALU.mult, ["g_raw", "gvec"], ["g_raw"])
        ACTF(be_flat, b_view, AF.Exp, ["dabs"], ["beta"], scale=-1.0)
        TS("dve", be_flat, be_flat, 1.0, None, ALU.add, ALU.bypass, ["beta"], ["beta"])
        RECIP(be_flat, be_flat, ["beta"], ["beta"])
        MARK("d0")

        GSCALE = 128.0 ** -0.5
        ident_bc4 = ident_b[:].unsqueeze(1).to_broadcast([128, HG, 128])

        def bc_h(ap2):
            return ap2.unsqueeze(2).to_broadcast([128, HG, 128])

        def bc_m(ap2):
            return ap2.unsqueeze(1).to_broadcast([128, HG, 128])

        def v4(ap2):
            return ap2.rearrange("p (h d) -> p h d", h=HG)

        for grp in range(2):
            hs0 = grp * HG
            for which, c_base, dstT in ((0, C_DQ, gqT), (1, C_DK, gkT), (2, C_DV, gvT)):
                ws = wsl[which % 2]
                wk = ("wsl", which % 2)
                DMA("pool", ws, w_in_d[:, c_base + hs0 * 128:c_base + (hs0 + HG) * 128].rearrange("(k p) c -> p k c", p=128),
                    ("w_wsl", which % 2), [], [wk])
                for hh in range(HG):
                    ci = which * 8 + hs0 + hh
                    xi = (which * HG + hh) % 2
                    xcb = xc[xi]
                    xk = ("xc", xi)
                    MEMSET("pool", xcb[:, 0:2], 0.0, [xk])
                    MEMSET("pool", xcb[:, T + 2:T + 4], 0.0, [xk])
                    for k in range(5):
                        TS("dve", diag[:, k, :], cst("ident"), cwT[:, ci, k:k + 1], None, ALU.mult, ALU.bypass,
                           ["consts", "cwT"], ["diag"])
                    for tg in range(4):
                        bi = tg % 2
                        for kt in range(KT):
                            MM(bank(bi), ws[:, kt, hh * 128:(hh + 1) * 128], hT[:, kt, tg * 512:(tg + 1) * 512],
                               kt == 0, kt == KT - 1, [wk] + HT_ALL[tg * 4:tg * 4 + 4], [bkey(bi)])
                        CP("act" if tg % 2 else "dve", xcb[:, 2 + tg * 512:2 + (tg + 1) * 512], bank(bi), [bkey(bi)], [xk])
                    for tg in range(4):
                        bi = 2 + tg % 2
                        i2 = tg % 2
                        for k in range(5):
                            MM(bank(bi), diag[:, k, :], xcb[:, tg * 512 + k:tg * 512 + k + 512], k == 0, k == 4,
                               ["diag", xk], [bkey(bi)])
                        ck = ("ctmp", i2)
                        ACTF(ce[i2], bank(bi), AF.Exp, [bkey(bi)], [ck], scale=-1.0)
                        ACTF(ce[i2], ce[i2], AF.Ln, [ck], [ck], bias=1.0)
                        ACTF(ce[i2], ce[i2], AF.Exp, [ck], [ck], scale=-1.0)
                        dst = dstT[:, hh, tg * 512:(tg + 1) * 512]
                        dk = ("gT", which, hh, tg)
                        if which == 2:
                            TT("dve", dst, ce[i2], bank(bi), ALU.mult, [ck, bkey(bi)], [dk])
                        else:
                            TT("dve", cy[i2], ce[i2], bank(bi), ALU.mult, [ck, bkey(bi)], [("cy", i2)])
                            TT("pool", cysq, cy[i2], cy[i2], ALU.mult, [("cy", i2)], ["cysq"])
                            MM(bank(4), ones_b[:], cysq, True, True, ["ones_b", "cysq"], [bkey(4)])
                            ACTF(crs, bank(4), AF.Ln, [bkey(4)], ["crs"], bias=EPS)
                            ACTF(crs, crs, AF.Exp, ["crs"], ["crs"], scale=-0.5)
                            if which == 0:
                                STT(dst, cy[i2], GSCALE, crs, ALU.mult, ALU.mult, [("cy", i2), "crs"], [dk])
                            else:
                                TT("dve", dst, cy[i2], crs, ALU.mult, [("cy", i2), "crs"], [dk])
            MARK("d1")
            P.barrier()
            DMA("pool", wsl[0], w_in_d[:, C_DZ + hs0 * 128:C_DZ + (hs0 + HG) * 128].rearrange("(k p) c -> p k c", p=128),
                ("w_wsl", 0), [], [("wsl", 0)])
            DMA("pool", wsl[1], w_in_d[:, C_MB + hs0 * 128:C_MB + (hs0 + HG) * 128].rearrange("(k p) c -> p k c", p=128),
                ("w_wsl", 1), [], [("wsl", 1)])

            def gT_keys(which, n):
                return [("gT", which, hh, n // 4) for hh in range(HG)]

            stored = set()

            def gdn_tile(d_, n):
                B = DB[d_]
                dk = lambda nm: (nm, d_)
                Mc = cst("b_le") if d_ == 0 else cst("b_ge")
                Ms = cst("b_gt") if d_ == 0 else cst("b_lt")
                esc, bg = B["esc"], B["bg"]
                tsl = slice(n * 128, (n + 1) * 128)
                gv = g_raw[:, n, d_, hs0:hs0 + HG]
                bv = beta[:, n, d_, hs0:hs0 + HG]
                MM(bank(0)[:, 0:4], Mc, gv, True, True, ["consts", "g_raw"], [bkey(0)])
                MM(bank(0)[:, 4:8], Ms, gv, True, True, ["consts", "g_raw"], [bkey(0)])
                MM(bank(0)[:, 8:12], cst("csel0"), gv, True, True, ["consts", "g_raw"], [bkey(0)])
                MM(bank(0)[:, 12:16], cst("csel1"), gv, True, True, ["consts", "g_raw"], [bkey(0)])
                ACTF(esc, bank(0)[:, 0:16], AF.Exp, [bkey(0)], [dk("esc")])
                TT("dve", bg, bv, esc[:, 0:4], ALU.mult, ["beta", dk("esc")], [dk("bg")])
                for hh in range(HG):
                    TR(bbank(0)[:, hh * 128:(hh + 1) * 128], gkT[:, hh, tsl], ident_b[:], gT_keys(1, n) + ["ident_b"], [("pbb", 0)])
                for hh in range(HG):
                    TR(bbank(1)[:, hh * 128:(hh + 1) * 128], gvT[:, hh, tsl], ident_b[:], gT_keys(2, n) + ["ident_b"], [("pbb", 1)])
                TT("dve", kbg, v4(bbank(0)[:, 0:512]), bc_h(bg), ALU.mult, [("pbb", 0), dk("bg")], ["kbg"])
                TT("dve", B["ktl"], v4(bbank(0)[:, 0:512]), bc_h(esc[:, 4:8]), ALU.mult, [("pbb", 0), dk("esc")], [dk("ktl")])
                TT("dve", vbeta, v4(bbank(1)[:, 0:512]), bc_h(bv), ALU.mult, [("pbb", 1), "beta"], ["vbeta"])
                for hh in range(HG):
                    TR(bbank(0)[:, hh * 128:(hh + 1) * 128], gqT[:, hh, tsl], ident_b[:], gT_keys(0, n) + ["ident_b"], [("pbb", 0)])
                TT("dve", qd_tok, v4(bbank(0)[:, 0:512]), bc_h(esc[:, 0:4]), ALU.mult, [("pbb", 0), dk("esc")], ["qd_tok"])
                for hh in range(HG):
                    TR(bbank(1)[:, hh * 128:(hh + 1) * 128], qd_tok[:, hh, :], ident_b[:], ["qd_tok", "ident_b"], [("pbb", 1)])
                CP("act", B["qdTg"], v4(bbank(1)[:, 0:512]), [("pbb", 1)], [dk("qdTg")])
                TT("pool", GMB, bc_h(gv), bc_m(Ms), ALU.mult, ["g_raw", "consts"], ["GMB"])
                MM(bank(1), Mc, GMB.rearrange("p h s -> p (h s)"), True, True, ["consts", "GMB"], [bkey(1)])
                ACTF(Wd.rearrange("p h s -> p (h s)"), bank(1), AF.Exp, [bkey(1)], ["Wd"])
                TT("pool", GMB, bc_h(bv), bc_m(Ms), ALU.mult, ["beta", "consts"], ["GMB"])
                TT("pool", Wd, Wd, GMB, ALU.mult, ["Wd", "GMB"], ["Wd"])
                TT("pool", GMB, bc_h(gv), bc_m(Mc), ALU.mult, ["g_raw", "consts"], ["GMB"])
                MM(bank(0), Ms, GMB.rearrange("p h s -> p (h s)"), True, True, ["consts", "GMB"], [bkey(0)])
                ACTF(decT.rearrange("p h s -> p (h s)"), bank(0), AF.Exp, [bkey(0)], ["decT"])
                TT("pool", decT, decT, bc_m(Mc), ALU.mult, ["decT", "consts"], ["decT"])
                for hh in range(HG):
                    MM(bank(1)[:, hh * 128:(hh + 1) * 128], gkT[:, hh, tsl], gkT[:, hh, tsl], True, True,
                       gT_keys(1, n), [bkey(1)])
                TT("dve", Lm, v4(bank(1)), Wd, ALU.mult, [bkey(1), "Wd"], ["Lm"])
                for hh in range(HG):
                    MM(bank(0)[:, hh * 128:(hh + 1) * 128], gkT[:, hh, tsl], gqT[:, hh, tsl], True, True,
                       gT_keys(1, n) + gT_keys(0, n), [bkey(0)])
                TT("dve", B["attnT"], v4(bank(0)), decT, ALU.mult, [bkey(0), "decT"], [dk("attnT")])
                for hh in range(HG):
                    TR(bbank(0)[:, hh * 128:(hh + 1) * 128], Lm[:, hh, :], ident_b[:], ["Lm", "ident_b"], [("pbb", 0)])
                CP("act", LTm, v4(bbank(0)[:, 0:512]), [("pbb", 0)], ["LTm"])
                TT("dve", XT, ident_bc4, v4(bbank(0)[:, 0:512]), ALU.subtract, ["ident_b", ("pbb", 0)], ["XT"])
                Pc, PTc = Lm, LTm
                pck, ptk_ = "Lm", "LTm"
                for it in range(5):
                    Pn, PTn = Pp[it % 2], PTp[it % 2]
                    pnk, ptnk = ("Pp", it % 2), ("PTp", it % 2)
                    for hh in range(HG):
                        MM(bank(1)[:, hh * 128:(hh + 1) * 128], PTc[:, hh, :], Pc[:, hh, :], True, True, [pck, ptk_], [bkey(1)])
                    CP("act", Pn, v4(bank(1)), [bkey(1)], [pnk])
                    if it < 4:
                        for hh in range(HG):
                            MM(bank(0)[:, hh * 128:(hh + 1) * 128], Pc[:, hh, :], PTc[:, hh, :], True, True, [pck, ptk_], [bkey(0)])
                        CP("dve", PTn, v4(bank(0)), [bkey(0)], [ptnk])
                    for hh in range(HG):
                        MM(bank(1)[:, hh * 128:(hh + 1) * 128], Pn[:, hh, :], XT[:, hh, :], True, True, [pnk, "XT"], [bkey(1)])
                    TT("dve", XT, XT, v4(bank(1)), ALU.add, ["XT", bkey(1)], ["XT"])
                    Pc, PTc, pck, ptk_ = Pn, PTn, pnk, ptnk
                for hh in range(HG):
                    MM(bank(0)[:, hh * 128:(hh + 1) * 128], XT[:, hh, :], vbeta[:, hh, :], True, True, ["XT", "vbeta"], [bkey(0)])
                CP("act", B["u_sb"], v4(bank(0)), [bkey(0)], [dk("u_sb")])
                for hh in range(HG):
                    MM(bank(1)[:, hh * 128:(hh + 1) * 128], kbg[:, hh, :], XT[:, hh, :], True, True, ["kbg", "XT"], [bkey(1)])
                CP("dve", B["wT_sb"], v4(bank(1)), [bkey(1)], [dk("wT_sb")])
                sb0 = 4 if d_ == 0 else 2
                Sg, Sg_bf, vnew = B["Sg"], B["Sg_bf"], B["vnew"]
                chunks = (0, 1) if d_ == 0 else (1, 0)
                for c in chunks:
                    sl = slice(c * 64, c * 64 + 64)
                    for hh in range(HG):
                        MM(bank(sb0)[sl, hh * 128:(hh + 1) * 128], B["wT_sb"][:, hh, sl], Sg_bf[:, hh, :], True, True,
                           [dk("wT_sb"), dk("Sg_bf")], [bkey(sb0)])
                    TT("dve", vnew[sl], B["u_sb"][sl], v4(bank(sb0))[sl], ALU.subtract, [dk("u_sb"), bkey(sb0)], [dk("vnew")])
                    for hh in range(HG):
                        MM(bank(sb0 + 1)[sl, hh * 128:(hh + 1) * 128], B["qdTg"][:, hh, sl], Sg_bf[:, hh, :], True, False,
                           [dk("qdTg"), dk("Sg_bf")], [bkey(sb0 + 1)])
                        MM(bank(sb0 + 1)[sl, hh * 128:(hh + 1) * 128], B["attnT"][sl, hh, sl], vnew[sl, hh, :], False, True,
                           [dk("attnT"), dk("vnew")], [bkey(sb0 + 1)])
                    for hh in range(HG):
                        MM(bank(sb0)[:, hh * 128:(hh + 1) * 128], B["ktl"][sl, hh, :], vnew[sl, hh, :], True, True,
                           [dk("ktl"), dk("vnew")], [bkey(sb0)])
                    TT("pool", Sg, Sg, bc_h(esc[:, 8 + 4 * c:12 + 4 * c]), ALU.mult, [dk("Sg"), dk("esc")], [dk("Sg")])
                    TT("dve", Sg, Sg, v4(bank(sb0)), ALU.add, [dk("Sg"), bkey(sb0)], [dk("Sg")])
                    CP("act", Sg_bf, Sg, [dk("Sg")], [dk("Sg_bf")])
                    if n not in stored:
                        CP("act", o_store[sl, n], v4(bank(sb0 + 1))[sl], [bkey(sb0 + 1)], [("o_store", n)])
                    else:
                        TT("dve", osum[sl], v4(bank(sb0 + 1))[sl], o_store[sl, n], ALU.add, [bkey(sb0 + 1), ("o_store", n)], ["osum"])
                if n not in stored:
                    stored.add(n)
                    return
                osq = fsig[:, 0:512].rearrange("p (h d) -> p h d", h=HG)
                TT("pool", osq, osum, osum, ALU.mult, ["osum"], ["fsig"])
                P.op("dve", lambda e: e.tensor_reduce(out=frs[:, 0:HG], in_=osq, axis=AX.X, op=ALU.add), ["fsig"], ["frs"], cost=600.0)
                rstd_inplace(frs[:, 0:HG], 128, "frs")
                for half, ws in enumerate(wsl):
                    for kt in range(KT):
                        MM(bank(half), hT[:, kt, tsl], ws[:, kt, :], kt == 0, kt == KT - 1,
                           [("wsl", half), ("hT", n)], [bkey(half)])
                zm = pt[0][:, :]
                ACTF(fsig, zm, AF.Exp, [bkey(0), bkey(1)], ["fsig"], scale=-1.0)
                ACTF(fsig, fsig, AF.Ln, ["fsig"], ["fsig"], bias=1.0)
                ACTF(fsig, fsig, AF.Exp, ["fsig"], ["fsig"], scale=-1.0)
                TT("pool", fG.rearrange("p h d -> p (h d)"), fsig[:, 0:512], fsig[:, 512:1024], ALU.mult, ["fsig"], ["fG"])
                TT("dve", fG, fG, v4(bank(0)), ALU.mult, ["fG", bkey(0)], ["fG"])
                TT("pool", fG, fG, bc_m(gdnw_bc), ALU.mult, ["fG", "gdnw_bc"], ["fG"])
                TT("pool", osum, osum, bc_h(frs[:, 0:HG]), ALU.mult, ["osum", "frs"], ["osum"])
                TT("pool", osum, osum, fG, ALU.mult, ["osum", "fG"], ["osum"])
                mslice = mixed[:, n, hs0 * 128:(hs0 + HG) * 128].rearrange("p (h d) -> p h d", h=HG)
                TT("dve", mslice, mslice, osum, ALU.add, ["osum", ("mixed", n)], [("mixed", n)])

            for d_ in range(2):
                MEMSET("dve", DB[d_]["Sg"], 0.0, [("Sg", d_)])
                CP("act", DB[d_]["Sg_bf"], DB[d_]["Sg"], [("Sg", d_)], [("Sg_bf", d_)])
            for i in range(NT):
                gdn_tile(0, i)
                gdn_tile(1, NT - 1 - i)
            MARK("d2")
            P.barrier()

        P.barrier()
        wout_d = dram("w_out", [D, D])
        off = 0
        x1, off = carve(off, [NT, D], F32)
        X1_END = off
        mT, off = carve(off, [KT, T], BF16)
        wout, off = carve(off, [KT, D], BF16)
        hn2 = [None, None]
        hn2[0], off = carve(off, [D], BF16)
        hn2[1], off = carve(off, [D], BF16)
        junk, off = carve(off, [D], BF16)
        n2_bc, off = carve(off, [D], F32)
        DMA("sp", n2_bc, n2_d.partition_broadcast(128), "c_n2", [], ["n2_bc"])
        DMA("pool", wout, wout_d.rearrange("(k p) c -> p k c", p=128), "w_wout", [], ["wout"])
        for n in range(NT):
            b = n % 2
            for kt in range(KT):
                TR(bbank(b)[:, kt * 128:(kt + 1) * 128], mixed[:, n, kt * 128:(kt + 1) * 128], ident_b[:],
                   [("mixed", n), "ident_b"], [("pbb", b)])
            CP("act", mT[:, :, n * 128:(n + 1) * 128], bbank(b).rearrange("p (k t) -> p k t", k=KT), [("pbb", b)], [("mT", n)])
        for n in range(NT):
            tsl = slice(n * 128, (n + 1) * 128)
            DMA("sp", x1[:, n, :], x_d[tsl, :], ("x1ld", n % 4), [], [("x1", n)])
            pp = pt[n % 2]
            for half in range(2):
                for kt in range(KT):
                    MM(pp[:, half * 512:(half + 1) * 512], mT[:, kt, tsl], wout[:, kt, half * 512:(half + 1) * 512],
                       kt == 0, kt == KT - 1, [("mT", n), "wout"], [bkey((n % 2) * 2 + half)])
            TT("dve", x1[:, n, :], x1[:, n, :], pp[:, :], ALU.add, [("x1", n), bkey((n % 2) * 2), bkey((n % 2) * 2 + 1)], [("x1", n)])
            b = n % 2
            ssap = small[:, 8 + b:9 + b]
            ssk = ("ss2", b)
            ACTF(junk, x1[:, n, :], AF.Square, [("x1", n)], ["junk", ssk], accum_out=ssap)
            rstd_inplace(ssap, D, ssk)
            STT(hn2[b], x1[:, n, :], ssap, n2_bc, ALU.mult, ALU.mult, [("x1", n), ssk, "n2_bc"], [("hn2", b)])
            for kt in range(KT):
                TR(bbank(b)[:, kt * 128:(kt + 1) * 128], hn2[b][:, kt * 128:(kt + 1) * 128], ident_b[:],
                   [("hn2", b), "ident_b"], [("pbb", b)])
            CP("act", hT[:, :, tsl], bbank(b).rearrange("p (k t) -> p k t", k=KT), [("pbb", b)], [("hT", n)])
        MARK("e0")

        P.barrier()
        wr_d = dram("moe_wr", [D, 36])
        wg_d = dram("moe_w_gate", [32, D, 256])
        wu_d = dram("moe_w_up", [32, D, 256])
        wd_d = dram("moe_w_down", [32, 256, D])
        off = X1_END
        EG = 2
        mix32 = mixed[:].rearrange("p n d -> p (n d)").bitcast(F32)
        MIXCAP = 32 * 1024
        moff = 0
        hidT, moff = carve(moff, [EG, 2, T], BF16, mix32, MIXCAP)
        wgu = []
        for i in range(2):
            a, off = carve(off, [KT, 512], BF16)
            wgu.append(a)
        wdn = []
        for i in range(4):
            a, off = carve(off, [2, D], BF16)
            wdn.append(a)
        wr, off = carve(off, [KT, 36], BF16)
        lg, off = carve(off, [NT, 36], F32)
        gate, off = carve(off, [NT, 32], F32)
        msk, off = carve(off, [NT, 32], F32)
        oh, off = carve(off, [NT, 32], F32)
        gtmp, off = carve(off, [NT, 4], F32)
        ohg, off = carve(off, [NT, 4], F32)
        rv, off = carve(off, [8, NT], F32)
        GT, moff = carve(moff, [T], F32, mix32, MIXCAP)
        st = [None, None]
        tt_ = [None, None]
        for i in range(2):
            st[i], moff = carve(moff, [512], F32, mix32, MIXCAP)
            tt_[i], moff = carve(moff, [512], F32, mix32, MIXCAP)
        MOE_END = off

        DMA("pool", wr, wr_d.rearrange("(k p) c -> p k c", p=128), "w_wr", [], ["wr"])
        for n in range(NT):
            bi = n % 2
            for kt in range(KT):
                MM(bank(bi)[:, 0:36], hT[:, kt, n * 128:(n + 1) * 128], wr[:, kt, :], kt == 0, kt == KT - 1,
                   ["wr", ("hT", n)], [bkey(bi)])
            CP("act", lg[:, n, :], bank(bi)[:, 0:36], [bkey(bi)], ["lg"])
        BIG = 10000.0
        glv = lg[:, :, 0:4]
        elv = lg[:, :, 4:36]

        def RED(out, in_, op, R, W):
            P.op("dve", lambda e: e.tensor_reduce(out=out, in_=in_, axis=AX.X, op=op), R, W)

        def bcn(ap2, k):
            return ap2.unsqueeze(2).to_broadcast([128, NT, k])

        gmax, gsum, m1, m2, w1, w2 = (rv[:, i, :] for i in range(6))
        RED(gmax, glv, ALU.max, ["lg"], ["rv"])
        TT("dve", ohg, glv, bcn(gmax, 4), ALU.is_equal, ["lg", "rv"], ["ohg"])
        TT("dve", gtmp, glv, bcn(gmax, 4), ALU.subtract, ["lg", "rv"], ["gtmp"])
        ACTF(gtmp, gtmp, AF.Exp, ["gtmp"], ["gtmp"])
        RED(gsum, gtmp, ALU.add, ["gtmp"], ["rv"])
        RECIP(gsum, gsum, ["rv"], ["rv"])
        TS("dve", ohg, ohg, BIG, -BIG, ALU.mult, ALU.add, ["ohg"], ["ohg"])
        TT("dve", msk.rearrange("p n (g e) -> p n g e", g=4), elv.rearrange("p n (g e) -> p n g e", g=4),
           ohg.unsqueeze(3).to_broadcast([128, NT, 4, 8]), ALU.add, ["lg", "ohg"], ["msk"])
        RED(m1, msk, ALU.max, ["msk"], ["rv"])
        TT("dve", oh, msk, bcn(m1, 32), ALU.is_equal, ["msk", "rv"], ["oh"])
        CP("dve", gate, oh, ["oh"], ["gate"])
        STT(msk, oh, -BIG, msk, ALU.mult, ALU.add, ["oh", "msk"], ["msk"])
        RED(m2, msk, ALU.max, ["msk"], ["rv"])
        TT("dve", oh, msk, bcn(m2, 32), ALU.is_equal, ["msk", "rv"], ["oh"])
        TT("dve", w2, m2, m1, ALU.subtract, ["rv"], ["rv"])
        ACTF(w2, w2, AF.Exp, ["rv"], ["rv"])
        TS("dve", w1, w2, 1.0, None, ALU.add, ALU.bypass, ["rv"], ["rv"])
        RECIP(w1, w1, ["rv"], ["rv"])
        TT("dve", w1, w1, gsum, ALU.mult, ["rv"], ["rv"])
        TT("dve", w2, w2, w1, ALU.mult, ["rv"], ["rv"])
        TT("dve", gate, gate, bcn(w1, 32), ALU.mult, ["gate", "rv"], ["gate"])
        TT("dve", oh, oh, bcn(w2, 32), ALU.mult, ["oh", "rv"], ["oh"])
        TT("dve", gate, gate, oh, ALU.add, ["gate", "oh"], ["gate"])
        for n in range(NT):
            bi = n % 2
            P.op("pe", lambda e, n=n, bi=bi: e.transpose(out=bank(bi)[0:32, 0:128], in_=gate[:, n, :], identity=cst("ident")),
                 ["gate", "consts"], [bkey(bi)])
            CP("act", GT[0:32, n * 128:(n + 1) * 128], bank(bi)[0:32, 0:128], [bkey(bi)], ["GT"])
        MARK("e1")

        def load_expert(e):
            s = e % 2
            DMA("pool", wgu[s][:, :, 0:256], wg_d[e].rearrange("(k p) c -> p k c", p=128), ("w_wgu", s), [], [("wgu", s)])
            DMA("pool", wgu[s][:, :, 256:512], wu_d[e].rearrange("(k p) c -> p k c", p=128), ("w_wgu", s), [], [("wgu", s)])
            s4 = e % 4
            DMA("pool", wdn[s4], wd_d[e].rearrange("(k p) c -> p k c", p=128), ("w_wdn", s4), [], [("wdn", s4)])

        load_expert(0)
        for e in range(32):
            if e + 1 < 32:
                load_expert(e + 1)
            s = e % 2
            es = e % EG
            for ft in range(2):
                for tg in range(4):
                    i2 = (ft * 4 + tg) % 2
                    gb, ub = bank(i2 * 2), bank(i2 * 2 + 1)
                    gk, uk = bkey(i2 * 2), bkey(i2 * 2 + 1)
                    hk = HT_ALL[tg * 4:tg * 4 + 4]
                    for kt in range(KT):
                        MM(gb, wgu[s][:, kt, ft * 128:(ft + 1) * 128], hT[:, kt, tg * 512:(tg + 1) * 512],
                           kt == 0, kt == KT - 1, [("wgu", s)] + hk, [gk])
                    for kt in range(KT):
                        MM(ub, wgu[s][:, kt, 256 + ft * 128:256 + (ft + 1) * 128], hT[:, kt, tg * 512:(tg + 1) * 512],
                           kt == 0, kt == KT - 1, [("wgu", s)] + hk, [uk])
                    MM(bank(4 + i2), cst("ident")[0:32, e:e + 1].to_broadcast([32, 128]), GT[0:32, tg * 512:(tg + 1) * 512],
                       True, True, ["consts", "GT"], [bkey(4 + i2)])
                    ACTF(st[i2], gb, AF.Silu, [gk], [("st", i2)])
                    TT("dve", tt_[i2], st[i2], ub, ALU.mult, [("st", i2), uk], [("tt", i2)])
                    TT("dve", hidT[:, es, ft, tg * 512:(tg + 1) * 512], tt_[i2], bank(4 + i2), ALU.mult,
                       [("tt", i2), bkey(4 + i2)], [("hidT", es, ft, tg)])
            if es == EG - 1:
                for n in range(NT):
                    tsl = slice(n * 128, (n + 1) * 128)
                    pp = pt[2] if n % 2 == 0 else pt[1]
                    kb0 = 4 if n % 2 == 0 else 2
                    for half in range(2):
                        cnt = 0
                        for ee in range(EG):
                            eid = e - (EG - 1) + ee
                            for ft in range(2):
                                MM(pp[:, half * 512:(half + 1) * 512], hidT[:, ee, ft, tsl],
                                   wdn[eid % 4][:, ft, half * 512:(half + 1) * 512], cnt == 0, cnt == 2 * EG - 1,
                                   [("hidT", ee, ft, n // 4), ("wdn", eid % 4)], [bkey(kb0 + half)])
                                cnt += 1
                    TT("dve", x1[:, n, :], x1[:, n, :], pp[:, :], ALU.add, [("x1", n), bkey(kb0), bkey(kb0 + 1)], [("x1", n)])
        MARK("e2")

        P.barrier()
        off = X1_END
        nf_bc, off = carve(off, [D], F32)
        ob = [None, None]
        ob[0], off = carve(off, [D], F32)
        ob[1], off = carve(off, [D], F32)
        junk2, off = carve(off, [D], BF16)
        DMA("sp", nf_bc, nf_d.partition_broadcast(128), "c_nf", [], ["nf_bc"])
        for n in range(NT):
            b = n % 2
            ssap = small[:, 12 + b:13 + b]
            ssk = ("ss3", b)
            ACTF(junk2, x1[:, n, :], AF.Square, [("x1", n)], ["junk2", ssk], accum_out=ssap)
            rstd_inplace(ssap, D, ssk)
            STT(ob[b], x1[:, n, :], ssap, nf_bc, ALU.mult, ALU.mult, [("x1", n), ssk, "nf_bc"], [("ob", b)])
            DMA("sp", out_d[n * 128:(n + 1) * 128, :], ob[b], ("out_st", b), [("ob", b)], [("out", n)])
        if not dbg:
            P.wait_all("sp", [("out", n) for n in range(NT)])
        if dbg:
            P.enabled = True
            P.barrier()
            for n in range(NT):
                DMA("sp", dbg_d[n * 128:(n + 1) * 128, :], x1[:, n, :], ("dbg_out", n % 2), [("x1", n)], [("dbg", n)])
            P.wait_all("sp", [("dbg", n) for n in range(NT)] + [("out", n) for n in range(NT)])
        P.emit()
    return nc


def make_in_maps(inputs, n_cores=8):
    f = lambda k: np.asarray(inputs[k], np.float32)
    x = f("x")
    shared = {
        "norm1_w": f("norm1_w").reshape(1, D),
        "norm2_w": f("norm2_w").reshape(1, D),
        "norm_f_w": f("norm_f_w").reshape(1, D),
        "consts": CONST_ARR,
        "w_in": np.ascontiguousarray(f("w_in")[0]),
        "gla_w2b_f": np.ascontiguousarray(np.concatenate([f("gla_gate_w2_fwd")[0], f("gla_gate_b_fwd")], axis=0)),
        "gla_w2b_b": np.ascontiguousarray(np.concatenate([f("gla_gate_w2_bwd")[0], f("gla_gate_b_bwd")], axis=0)),
        "gla_norm_w": f("gla_norm_w").reshape(1, 256),
        "w_out": np.ascontiguousarray(f("w_out")[0]),
        "moe_wr": np.ascontiguousarray(np.concatenate([f("moe_w_group")[0], f("moe_w_router")[0]], axis=1)),
        "moe_w_gate": np.ascontiguousarray(f("moe_w_gate")[0]),
        "moe_w_up": np.ascontiguousarray(f("moe_w_up")[0]),
        "moe_w_down": np.ascontiguousarray(f("moe_w_down")[0]),
        "gdn_norm_w": f("gdn_norm_w").reshape(1, 128),
        "gdn_vec": np.ascontiguousarray(np.concatenate([f("gdn_dt_bias_fwd")[0], f("gdn_dt_bias_bwd")[0],
                                                        f("gdn_a_log_fwd")[0], f("gdn_a_log_bwd")[0]]).reshape(1, 32)),
        "gdn_conv_wT": np.ascontiguousarray(f("gdn_conv_w")[0].T.reshape(24, 128, 5).transpose(1, 0, 2)),
    }
    maps = []
    for c in range(n_cores):
        m = dict(shared)
        m["x"] = np.ascontiguousarray(x[c])
        maps.append(m)
    return maps


def kernel(**inputs):
    nc = build()
    in_maps = make_in_maps(inputs)
    res = run_bass_kernel_spmd(nc, in_maps, core_ids=list(range(8)))
    out = np.stack([np.asarray(r["out"]) for r in res.results], axis=0)
    return out.astype(np.float32)
```

```python
import contextlib
import heapq
import numpy as np
import concourse.bass as bass
import concourse.mybir as mybir
from concourse.bass_utils import run_bass_kernel_spmd

F32 = mybir.dt.float32
BF16 = mybir.dt.bfloat16
I32 = mybir.dt.int32
AF = mybir.ActivationFunctionType
ALU = mybir.AluOpType
AX = mybir.AxisListType

T = 2048
D = 1024
NT = T // 128
KT = D // 128
EPS = 1e-6
SAME_ENGINE_SYNC = True
EPOCH = 20000
SYNC_NS = 120.0
DMA_LAT_NS = 2200.0


class Prog:
    ENGS = ("pe", "act", "dve", "pool", "sp")

    def __init__(self, nc, stack):
        self.nc = nc
        self.stack = stack
        self.streams = {e: [] for e in self.ENGS}
        self.count = {e: 0 for e in self.ENGS}
        self.esems = {e: [] for e in self.ENGS}
        self.known = {e: {} for e in self.ENGS}
        self.last_write = {}
        self.readers = {}
        self.dma_sems = {}
        self.dma_vals = {}
        self.dma_last = {}
        self.enabled = True
        self.seg = []
        self.ticks = {}
        self.nops = 0
        self.seg_base = 0

    def _new_sem(self, name):
        return self.stack.enter_context(self.nc.semaphore(name))

    @staticmethod
    def _psum_fix(reads, writes):
        r2, w2 = [], list(writes)
        for k in reads:
            if isinstance(k, tuple) and k[0] in ("pb", "pbb"):
                if k not in w2:
                    w2.append(k)
            else:
                r2.append(k)
        return r2, w2

    def _record(self, eng, fn, reads, writes, cost, kind, semkey=None):
        reads, writes = self._psum_fix(list(reads), list(writes))
        oid = self.nops
        self.nops += 1
        preds = set()
        for r in reads:
            t = self.last_write.get(r)
            if t is not None:
                preds.add(t)
        for w in writes:
            t = self.last_write.get(w)
            if t is not None:
                preds.add(t)
            preds.update(self.readers.get(w, ()))
        if kind == "dma":
            prev = self.dma_last.get(semkey)
            if prev is not None:
                preds.add(prev)
            self.dma_last[semkey] = oid
        preds = {p for p in preds if p >= self.seg_base}
        self.seg.append(dict(id=oid, eng=eng, fn=fn, preds=preds, cost=float(cost), kind=kind, semkey=semkey))
        for w in writes:
            self.last_write[w] = oid
            self.readers[w] = []
        for r in reads:
            self.readers.setdefault(r, []).append(oid)
        return oid

    def op(self, eng, fn, reads=(), writes=(), cost=300.0):
        if not self.enabled:
            return
        self._record(eng, fn, reads, writes, cost, "op")

    def dma(self, eng, fn, semkey, reads=(), writes=(), nbytes=1 << 20):
        if not self.enabled:
            return
        self._record(eng, fn, reads, writes, DMA_LAT_NS + nbytes / 160.0, "dma", semkey)

    def wait_all(self, eng, keys):
        self._record(eng, None, list(keys), [], 0.0, "op")

    def _schedule_segment(self):
        ops = self.seg
        if not ops:
            return
        byid = {o["id"]: o for o in ops}
        succ = {o["id"]: [] for o in ops}
        indeg = {}
        for o in ops:
            indeg[o["id"]] = len(o["preds"])
            for p in o["preds"]:
                succ[p].append(o["id"])
        ready_t = {o["id"]: 0.0 for o in ops}
        finish = {}
        heaps = {e: [] for e in self.ENGS}
        for o in ops:
            if indeg[o["id"]] == 0:
                heapq.heappush(heaps[o["eng"]], (0.0, o["id"]))
        etime = {e: 0.0 for e in self.ENGS}
        order = {e: [] for e in self.ENGS}
        remaining = len(ops)
        while remaining:
            best = None
            for e in self.ENGS:
                h = heaps[e]
                if not h:
                    continue
                rt, oid = h[0]
                st = max(rt, etime[e])
                if best is None or (st, oid) < (best[0], best[1]):
                    best = (st, oid, e)
            st, oid, e = best
            heapq.heappop(heaps[e])
            o = byid[oid]
            if o["kind"] == "dma":
                etime[e] = st + 150.0
                fin = st + o["cost"]
            else:
                etime[e] = st + o["cost"]
                fin = etime[e]
            finish[oid] = fin
            order[e].append(o)
            remaining -= 1
            for s in succ[oid]:
                so = byid[s]
                lat = SYNC_NS if (so["eng"] != e or o["kind"] == "dma") else (60.0 if e != "pe" else 0.0)
                ready_t[s] = max(ready_t[s], fin + lat)
                indeg[s] -= 1
                if indeg[s] == 0:
                    heapq.heappush(heaps[so["eng"]], (ready_t[s], s))
        self.est_ns = getattr(self, "est_ns", 0.0) + max(list(finish.values()) + [0.0])
        for o in ops:
            if o["kind"] == "dma":
                k = o["semkey"]
                if k not in self.dma_sems:
                    self.dma_sems[k] = self._new_sem(f"d{len(self.dma_sems)}")
                    self.dma_vals[k] = 0
                self.dma_vals[k] += 16
                self.ticks[o["id"]] = (self.dma_sems[k], self.dma_vals[k], "dma")
        def needs_sem(o):
            for s_ in succ[o["id"]]:
                se = byid[s_]["eng"]
                if se != o["eng"] or (SAME_ENGINE_SYNC and se != "pe"):
                    return True
            return False
        for e in self.ENGS:
            real = [o for o in order[e] if o["kind"] == "op" and o["fn"] is not None]
            for i_, o in enumerate(real):
                o["sig"] = needs_sem(o) or i_ == len(real) - 1
        for e in self.ENGS:
            for o in order[e]:
                if o["kind"] == "op" and o["fn"] is not None and o["sig"]:
                    c = self.count[e]
                    ep, v = divmod(c, EPOCH)
                    while len(self.esems[e]) <= ep:
                        self.esems[e].append(self._new_sem(f"s_{e}_{len(self.esems[e])}"))
                    self.count[e] = c + 1
                    self.ticks[o["id"]] = (self.esems[e][ep], v + 1, e)
        for e in self.ENGS:
            for o in order[e]:
                waits = {}
                for p in o["preds"]:
                    if byid[p]["eng"] == e and byid[p]["kind"] == "op" and (not SAME_ENGINE_SYNC or e == "pe"):
                        continue
                    sem, val, src = self.ticks[p]
                    sid = id(sem)
                    if self.known[e].get(sid, 0) >= val:
                        continue
                    if sid not in waits or waits[sid][1] < val:
                        waits[sid] = (sem, val)
                for sid, (sem, val) in waits.items():
                    self.known[e][sid] = val
                inc = None
                if o["fn"] is not None and o["id"] in self.ticks:
                    sem, val, src = self.ticks[o["id"]]
                    inc = (sem, 16 if o["kind"] == "dma" else 1)
                self.streams[e].append((o["fn"], list(waits.values()), inc))
        self.seg = []
        self.seg_base = self.nops

    def barrier(self):
        if not self.enabled and not self.seg:
            return
        self._schedule_segment()
        ticks = []
        for e2 in self.ENGS:
            c = self.count[e2]
            if c > 0:
                ep, v = divmod(c - 1, EPOCH)
                ticks.append((self.esems[e2][ep], v + 1))
        for k, sem in self.dma_sems.items():
            ticks.append((sem, self.dma_vals[k]))
        for eng in self.ENGS:
            waits = []
            for (sem, val) in ticks:
                if self.known[eng].get(id(sem), 0) >= val:
                    continue
                self.known[eng][id(sem)] = val
                waits.append((sem, val))
            if waits:
                self.streams[eng].append((None, waits, None))

    def emit(self):
        self._schedule_segment()
        nc = self.nc
        with nc.Block() as block:
            def run(e, stream):
                for fn, waits, inc in stream:
                    for sem, val in waits:
                        e.wait_ge(sem, val)
                    if fn is None:
                        continue
                    ins = fn(e)
                    if inc is not None:
                        ins.then_inc(inc[0], inc[1])

            @block.tensor
            def _(e):
                run(e, self.streams["pe"])

            @block.scalar
            def _(e):
                run(e, self.streams["act"])

            @block.vector
            def _(e):
                run(e, self.streams["dve"])

            @block.gpsimd
            def _(e):
                run(e, self.streams["pool"])

            @block.sync
            def _(e):
                run(e, self.streams["sp"])


def _fsz(ap):
    s = ap.shape
    n = 1
    for v in s[1:]:
        n *= int(v)
    return n


C_GQ, C_GK, C_GV, C_GR = 0, 512, 1024, 2048
C_GLF, C_GLB = 3072, 3088
C_DQ, C_DK, C_DV, C_DZ = 3104, 4128, 5152, 6176
C_DAB = 7200
C_MA, C_MB = 7232, 8256
D_IN = 9280


def host_consts():
    r = np.arange(128)[:, None]
    t = np.arange(128)[None, :]
    same = (r // 64) == (t // 64)
    c = {}
    c["ident"] = np.eye(128, dtype=np.float32)
    c["a_le"] = np.where(r <= t, -1.0 / 16, 0.0)
    c["a_ge"] = np.where(r >= t, -1.0 / 16, 0.0)
    c["a_gt"] = np.where(r > t, -1.0 / 16, 0.0)
    c["a_lt"] = np.where(r < t, -1.0 / 16, 0.0)
    c["m_le"] = np.where(r <= t, 1.0, 0.0)
    c["m_ge"] = np.where(r >= t, 1.0, 0.0)
    c["b_le"] = np.where((r <= t) & same, 1.0, 0.0)
    c["b_ge"] = np.where((r >= t) & same, 1.0, 0.0)
    c["b_gt"] = np.where((r > t) & same, 1.0, 0.0)
    c["b_lt"] = np.where((r < t) & same, 1.0, 0.0)
    c["csel0"] = np.where(r < 64, 1.0, 0.0) + 0.0 * t
    c["csel1"] = np.where(r >= 64, 1.0, 0.0) + 0.0 * t
    c["ones"] = np.ones((128, 128))
    names = list(c.keys())
    arr = np.stack([np.asarray(c[n], np.float32) for n in names], axis=1)
    return names, np.ascontiguousarray(arr)


CONST_NAMES, CONST_ARR = host_consts()
NCONST = len(CONST_NAMES)


def build(stage="all", dbg=False):
    nc = bass.Bass("TRN2", target_bir_lowering=False)
    stack = contextlib.ExitStack()
    with stack:
        P = Prog(nc, stack)

        def dram(name, shape, dt=F32, kind="ExternalInput"):
            return nc.dram_tensor(name, list(shape), dt, kind=kind).ap()

        def sb(name, shape, dt=F32):
            return stack.enter_context(nc.sbuf_tensor(name, list(shape), dt))

        def ps(name, shape, dt=F32):
            return stack.enter_context(nc.psum_tensor(name, list(shape), dt))

        def MM(out, lhsT, rhs, start, stop, R, W):
            n = _fsz(rhs)
            c = 70.0 + n * 0.75
            if rhs.dtype == F32:
                c *= 4.0
            P.op("pe", lambda e: e.matmul(out, lhsT, rhs, start=start, stop=stop), R, W, cost=c)

        def TR(out, in_, ident, R, W):
            P.op("pe", lambda e: e.transpose(out=out, in_=in_, identity=ident), R, W, cost=110.0)

        def ACTF(out, in_, func, R, W, **kw):
            c = 120.0 + _fsz(in_) * 0.6 + (90.0 if "accum_out" in kw else 0.0)
            P.op("act", lambda e: e.activation(out=out, in_=in_, func=func, **kw), R, W, cost=c)

        def _vc(eng, n, k=1.5):
            return (100.0 + n * k * 0.6) if eng == "dve" else (150.0 + n * 1.9)

        def TT(eng, out, in0, in1, op, R, W):
            P.op(eng, lambda e: e.tensor_tensor(out=out, in0=in0, in1=in1, op=op), R, W, cost=_vc(eng, _fsz(out)))

        def TS(eng, out, in0, s1, s2, op0, op1, R, W):
            P.op(eng, lambda e: e.tensor_scalar(out=out, in0=in0, scalar1=s1, scalar2=s2, op0=op0, op1=op1), R, W,
                 cost=_vc(eng, _fsz(out), 1.05))

        def STT(out, in0, scalar, in1, op0, op1, R, W):
            P.op("dve", lambda e: e.scalar_tensor_tensor(out=out, in0=in0, scalar=scalar, in1=in1, op0=op0, op1=op1), R, W,
                 cost=_vc("dve", _fsz(out)))

        def CP(eng, out, in_, R, W):
            if eng == "act":
                P.op("act", lambda e: e.activation(out=out, in_=in_, func=AF.Copy), R, W, cost=120.0 + _fsz(in_) * 0.6)
            else:
                P.op(eng, lambda e: e.tensor_copy(out=out, in_=in_), R, W, cost=_vc(eng, _fsz(out), 1.05))

        def MEMSET(eng, ap, val, W):
            P.op(eng, lambda e: e.memset(ap, val), [], W, cost=_vc(eng, _fsz(ap), 0.6))

        def DMA(eng, out, in_, semkey, R, W):
            P.dma(eng, lambda e: e.dma_start(out=out, in_=in_), semkey, R, W, nbytes=_fsz(out) * int(out.shape[0]) * 4)

        def RECIP(out, in_, R, W):
            P.op("dve", lambda e: e.reciprocal(out=out, in_=in_), R, W, cost=_vc("dve", _fsz(out), 1.05))

        def MARK(name):
            if stage == name:
                P.enabled = False

        def rstd_inplace(ap, n, key):
            TS("dve", ap, ap, 1.0 / n, EPS, ALU.mult, ALU.add, [key], [key])
            ACTF(ap, ap, AF.Ln, [key], [key])
            ACTF(ap, ap, AF.Exp, [key], [key], scale=-0.5)

        x_d = dram("x", [T, D])
        n1_d = dram("norm1_w", [1, D])
        n2_d = dram("norm2_w", [1, D])
        nf_d = dram("norm_f_w", [1, D])
        consts_d = dram("consts", [128, NCONST, 128])
        w_in_d = dram("w_in", [D, D_IN])
        w2b_d = [dram("gla_w2b_f", [17, 512]), dram("gla_w2b_b", [17, 512])]
        gnw_d = dram("gla_norm_w", [1, 256])
        out_d = dram("out", [T, D], kind="ExternalOutput")
        dbg_d = dram("dbg", [T, D], kind="ExternalOutput") if dbg else None

        consts = sb("consts_sb", [128, NCONST, 128])
        CI = {n: i for i, n in enumerate(CONST_NAMES)}

        def cst(name):
            return consts[:, CI[name], :]

        ident_b = sb("ident_b", [128, 128], BF16)
        ones_b = sb("ones_b", [128, 128], BF16)
        hT = sb("hT", [128, KT, T], BF16)
        mixed = sb("mixed", [128, NT, D], BF16)
        small = sb("small", [128, 64])
        ARENA_BYTES = 134 * 1024
        arena = sb("arena", [128, ARENA_BYTES // 4])

        def carve(off, shape, dt, base=None, cap=None):
            base = arena if base is None else base
            cap = ARENA_BYTES if cap is None else cap
            nb = int(np.prod(shape)) * (2 if dt == BF16 else 4)
            assert off % 4 == 0 and off + nb <= cap, (off, nb)
            v = base[:, off // 4:(off + nb) // 4]
            if dt != F32:
                v = v.bitcast(dt)
            if len(shape) == 2:
                pat = "p (a b) -> p a b"
                v = v.rearrange(pat, a=shape[0])
            elif len(shape) == 3:
                v = v.rearrange("p (a b c) -> p a b c", a=shape[0], b=shape[1])
            return v, off + nb

        pt = [ps(f"pt{i}", [128, 1024]) for i in range(3)]
        ptb = ps("ptb", [128, 2048], BF16)

        def bank(i):
            return pt[i // 2][:, (i % 2) * 512:(i % 2 + 1) * 512]

        def bkey(i):
            return ("pb", i)

        def bbank(i):
            return ptb[:, i * 1024:(i + 1) * 1024]

        DMA("sp", consts[:], consts_d[:, :, :], "c_consts", [], ["consts"])
        CP("dve", ident_b[:], cst("ident"), ["consts"], ["ident_b"])
        MEMSET("pool", ones_b[:], 1.0, ["ones_b"])

        off = 0
        xt0, off = carve(off, [D], F32)
        xt1, off = carve(off, [D], F32)
        hn0, off = carve(off, [D], BF16)
        hn1, off = carve(off, [D], BF16)
        sq, off = carve(off, [D], F32)
        n1_bc, off = carve(off, [D], F32)
        DMA("sp", n1_bc, n1_d.partition_broadcast(128), "c_n1", [], ["n1_bc"])
        xts = [xt0, xt1]
        hns = [hn0, hn1]
        for tt in range(NT):
            b = tt % 2
            xb, hb = xts[b], hns[b]
            DMA("sp", xb, x_d[tt * 128:(tt + 1) * 128, :], ("xt", b), [], [("xt", b)])
            ACTF(sq, xb, AF.Square, [("xt", b)], ["sq", "ss0"], accum_out=small[:, 0:1])
            rstd_inplace(small[:, 0:1], D, "ss0")
            STT(hb, xb, small[:, 0:1], n1_bc, ALU.mult, ALU.mult, [("xt", b), "ss0", "n1_bc"], [("hn", b)])
            for kt in range(KT):
                TR(bbank(b)[:, kt * 128:(kt + 1) * 128], hb[:, kt * 128:(kt + 1) * 128], ident_b[:],
                   [("hn", b), "ident_b"], [("pbb", b)])
            CP("act", hT[:, :, tt * 128:(tt + 1) * 128], bbank(b).rearrange("p (k t) -> p k t", k=KT),
               [("pbb", b)], [("hT", tt)])
        HT_ALL = [("hT", tt) for tt in range(NT)]
        MARK("p1")

        P.barrier()
        off = 0
        qT, off = carve(off, [T], F32)
        kT, off = carve(off, [T], F32)
        k_tok, off = carve(off, [NT, 128], F32)
        v_tok, off = carve(off, [NT, 256], BF16)
        qdT = [None, None]
        kiT = [None, None]
        ktail = [None, None]
        for d_ in range(2):
            qdT[d_], off = carve(off, [T], BF16)
            kiT[d_], off = carve(off, [T], BF16)
            ktail[d_], off = carve(off, [NT, 128], BF16)
        sb_store, off = carve(off, [NT, 256], BF16)
        dec, off = carve(off, [2, NT], F32)
        S, off = carve(off, [256], F32)
        S_bf, off = carve(off, [256], BF16)
        NTMP = 4
        tmp = []
        for i in range(NTMP):
            d = {}
            for nm in ("e", "lg", "E", "Ei", "Et"):
                d[nm], off = carve(off, [128], F32)
            d["Pf"], off = carve(off, [128], BF16)
            d["Pb"], off = carve(off, [128], BF16)
            d["sig"], off = carve(off, [512], F32)
            d["G"], off = carve(off, [256], F32)
            tmp.append(d)
        gl, off = carve(off, [2, T], BF16)
        w2b, off = carve(off, [2, 512], BF16)
        wqk, off = carve(off, [KT, 256], BF16)
        wkv, off = carve(off, [KT, 384], BF16)
        wgm, off = carve(off, [KT, 512], BF16)
        wgl, off = carve(off, [KT, 32], BF16)
        gnw_bc, off = carve(off, [256], F32)
        GLA_END = off

        DMA("sp", gnw_bc, gnw_d.partition_broadcast(128), "c_gnw", [], ["gnw_bc"])
        MEMSET("pool", gl[0:32, :, :], 1.0, ["gl"])
        MEMSET("pool", w2b[0:32, :, :], 0.0, ["w2b"])
        for d_ in range(2):
            DMA("pool", w2b[0:17, d_, :], w2b_d[d_][:, :], "c_w2b", [], ["w2b"])
        DMA("pool", wgl, w_in_d[:, C_GLF:C_GLF + 32].rearrange("(k p) c -> p k c", p=128), "w_wgl", [], ["wgl"])
        for d_ in range(2):
            for tg in range(4):
                bi = tg % 2
                for kt in range(KT):
                    MM(bank(bi)[0:16, :], wgl[:, kt, d_ * 16:(d_ + 1) * 16], hT[:, kt, tg * 512:(tg + 1) * 512],
                       kt == 0, kt == KT - 1, ["wgl"] + HT_ALL[tg * 4:tg * 4 + 4], [bkey(bi)])
                CP("act", gl[0:16, d_, tg * 512:(tg + 1) * 512], bank(bi)[0:16, :], [bkey(bi)], ["gl"])

        MARK("g0")
        QSCALE = 128.0 ** -0.5
        for h in range(4):
            def wcols(dst, c0, n):
                return (dst, w_in_d[:, c0:c0 + n].rearrange("(k p) c -> p k c", p=128))
            for (dst, src) in (wcols(wqk[:, :, 0:128], C_GQ + h * 128, 128), wcols(wqk[:, :, 128:256], C_GK + h * 128, 128)):
                DMA("pool", dst, src, "w_wqk", [], ["wqk"])
            for (dst, src) in (wcols(wkv[:, :, 0:128], C_GK + h * 128, 128), wcols(wkv[:, :, 128:384], C_GV + h * 256, 256)):
                DMA("pool", dst, src, "w_wkv", [], ["wkv"])
            for (dst, src) in (wcols(wgm[:, :, 0:256], C_GR + h * 256, 256), wcols(wgm[:, :, 256:512], C_MA + h * 256, 256)):
                DMA("pool", dst, src, "w_wgm", [], ["wgm"])
            MARK("g1a")
            for which, dstT in ((0, qT), (1, kT)):
                for tg in range(4):
                    bi = (which * 4 + tg) % 4
                    for kt in range(KT):
                        MM(bank(bi), wqk[:, kt, which * 128:(which + 1) * 128], hT[:, kt, tg * 512:(tg + 1) * 512],
                           kt == 0, kt == KT - 1, ["wqk"] + HT_ALL[tg * 4:tg * 4 + 4], [bkey(bi)])
                    CP("act" if tg % 2 else "dve", dstT[:, tg * 512:(tg + 1) * 512], bank(bi), [bkey(bi)],
                       [("qkT", which, tg)])
            MARK("g1b")
            for n in range(NT):
                bi = 4 + n % 2
                for kt in range(KT):
                    MM(bank(bi)[:, 0:384], hT[:, kt, n * 128:(n + 1) * 128], wkv[:, kt, :],
                       kt == 0, kt == KT - 1, ["wkv", ("hT", n)], [bkey(bi)])
                CP("dve", k_tok[:, n, :], bank(bi)[:, 0:128], [bkey(bi)], [("k_tok", n)])
                CP("act", v_tok[:, n, :], bank(bi)[:, 128:384], [bkey(bi)], [("v_tok", n)])
            MARK("g1")
            for n in range(NT):
                tsl = slice(n * 128, (n + 1) * 128)
                tg = n // 4
                for d_ in range(2):
                    tm = tmp[(n * 2 + d_) % NTMP]
                    tk = ("gtmp", (n * 2 + d_) % NTMP)
                    a_c = cst("a_le") if d_ == 0 else cst("a_ge")
                    a_s = cst("a_gt") if d_ == 0 else cst("a_lt")
                    b0 = (n * 2 + d_) % 2 * 2
                    zb, cb = bank(b0), bank(b0 + 1)
                    MM(zb[:, 0:128], gl[0:32, d_, tsl], w2b[0:32, d_, h * 128:(h + 1) * 128], True, True,
                       ["gl", "w2b"], [bkey(b0)])
                    ACTF(tm["e"], zb[:, 0:128], AF.Exp, [bkey(b0)], [tk], scale=-1.0)
                    ACTF(tm["lg"], tm["e"], AF.Ln, [tk], [tk], bias=1.0)
                    MM(cb[:, 0:128], tm["lg"], a_c, True, True, [tk, "consts"], [bkey(b0 + 1)])
                    MM(cb[:, 128:256], a_s, tm["lg"], True, True, [tk, "consts"], [bkey(b0 + 1)])
                    ACTF(tm["E"], cb[:, 0:128], AF.Exp, [bkey(b0 + 1)], [tk])
                    ACTF(tm["Ei"], cb[:, 0:128], AF.Exp, [bkey(b0 + 1)], [tk], scale=-1.0)
                    ACTF(tm["Et"], cb[:, 128:256], AF.Exp, [bkey(b0 + 1)], [tk])
                    STT(qdT[d_][:, tsl], qT[:, tsl], QSCALE, tm["E"], ALU.mult, ALU.mult,
                        [("qkT", 0, tg), tk], [("qdT", d_, n)])
                    TT("dve", kiT[d_][:, tsl], kT[:, tsl], tm["Ei"], ALU.mult, [("qkT", 1, tg), tk], [("kiT", d_, n)])
                    TT("dve", ktail[d_][:, n, :], k_tok[:, n, :], tm["Et"], ALU.mult, [("k_tok", n), tk], [("ktail", d_, n)])
                    col = 127 if d_ == 0 else 0
                    CP("dve", dec[:, d_, n:n + 1], tm["E"][:, col:col + 1], [tk], [("dec", d_, n)])
            MARK("g2")
            MEMSET("dve", S, 0.0, ["S"])
            for n in range(NT - 1, -1, -1):
                CP("act", sb_store[:, n, :], S, ["S"], [("sb_store", n)])
                bi = 4 + n % 2
                MM(bank(bi)[:, 0:256], ktail[1][:, n, :], v_tok[:, n, :], True, True,
                   [("ktail", 1, n), ("v_tok", n)], [bkey(bi)])
                STT(S, S, dec[:, 1, n:n + 1], bank(bi)[:, 0:256], ALU.mult, ALU.add,
                    ["S", ("dec", 1, n), bkey(bi)], ["S"])
            MARK("g3")
            MEMSET("dve", S, 0.0, ["S"])
            for n in range(NT):
                tsl = slice(n * 128, (n + 1) * 128)
                tm = tmp[n % NTMP]
                tk = ("ftmp", n % NTMP)
                CP("act", S_bf, S, ["S"], ["S_bf"])
                b0 = (n % 2) * 2
                sc = bank(b0)
                MM(sc[:, 0:128], kiT[0][:, tsl], qdT[0][:, tsl], True, True, [("kiT", 0, n), ("qdT", 0, n)], [bkey(b0)])
                MM(sc[:, 128:256], kiT[1][:, tsl], qdT[1][:, tsl], True, True, [("kiT", 1, n), ("qdT", 1, n)], [bkey(b0)])
                TT("dve", tm["Pf"], sc[:, 0:128], cst("m_le"), ALU.mult, [bkey(b0), "consts"], [tk])
                TT("dve", tm["Pb"], sc[:, 128:256], cst("m_ge"), ALU.mult, [bkey(b0), "consts"], [tk])
                ob = bank(b0 + 1)
                ok = bkey(b0 + 1)
                MM(ob[:, 0:256], qdT[0][:, tsl], S_bf, True, False, [("qdT", 0, n), "S_bf"], [ok])
                MM(ob[:, 0:256], qdT[1][:, tsl], sb_store[:, n, :], False, False, [("qdT", 1, n), ("sb_store", n)], [ok])
                MM(ob[:, 0:256], tm["Pf"], v_tok[:, n, :], False, False, [tk, ("v_tok", n)], [ok])
                MM(ob[:, 0:256], tm["Pb"], v_tok[:, n, :], False, True, [tk, ("v_tok", n)], [ok])
                kb = 4 + n % 2
                MM(bank(kb)[:, 0:256], ktail[0][:, n, :], v_tok[:, n, :], True, True,
                   [("ktail", 0, n), ("v_tok", n)], [bkey(kb)])
                STT(S, S, dec[:, 0, n:n + 1], bank(kb)[:, 0:256], ALU.mult, ALU.add,
                    ["S", ("dec", 0, n), bkey(kb)], ["S"])
                gb = 4 + n % 2
                for kt in range(KT):
                    MM(bank(gb), hT[:, kt, tsl], wgm[:, kt, :], kt == 0, kt == KT - 1, ["wgm", ("hT", n)], [bkey(gb)])
                ACTF(tm["sig"], bank(gb), AF.Exp, [bkey(gb)], [("sig", n % NTMP)], scale=-1.0)
                ACTF(tm["sig"], tm["sig"], AF.Ln, [("sig", n % NTMP)], [("sig", n % NTMP)], bias=1.0)
                ACTF(tm["sig"], tm["sig"], AF.Exp, [("sig", n % NTMP)], [("sig", n % NTMP)], scale=-1.0)
                TT("pool", tm["G"], tm["sig"][:, 0:256], tm["sig"][:, 256:512], ALU.mult, [("sig", n % NTMP)], [("G", n % NTMP)])
                TT("dve", tm["G"], tm["G"], bank(gb)[:, 0:256], ALU.mult, [("G", n % NTMP), bkey(gb)], [("G", n % NTMP)])
                TT("pool", tm["G"], tm["G"], gnw_bc, ALU.mult, [("G", n % NTMP), "gnw_bc"], [("G", n % NTMP)])
                ssk = ("ssq", n % 2)
                ssap = small[:, 2 + n % 2:3 + n % 2]
                ACTF(tm["sig"][:, 0:256], ob[:, 0:256], AF.Square, [ok, ("G", n % NTMP)], [("sig", n % NTMP), ssk],
                     accum_out=ssap)
                rstd_inplace(ssap, 256, ssk)
                STT(mixed[:, n, h * 256:(h + 1) * 256], ob[:, 0:256], ssap, tm["G"], ALU.mult, ALU.mult,
                    [ok, ssk, ("G", n % NTMP)], [("mixed", n)])

        P.barrier()
        HG = 4
        off = 0
        gqT, off = carve(off, [HG, T], BF16)
        gkT, off = carve(off, [HG, T], BF16)
        gvT, off = carve(off, [HG, T], BF16)
        o_store, off = carve(off, [NT, HG, 128], BF16)
        dabs, off = carve(off, [NT, 32], F32)
        g_raw, off = carve(off, [NT, 2, 8], F32)
        beta, off = carve(off, [NT, 2, 8], F32)
        gvec, off = carve(off, [64], F32)
        wsl0, off = carve(off, [KT, 512], BF16)
        wsl1, off = carve(off, [KT, 512], BF16)
        wsl = [wsl0, wsl1]
        cwT, off = carve(off, [24, 5], F32)
        gdnw_bc, off = carve(off, [128], F32)
        wdab, off = carve(off, [KT, 32], BF16)
        TMP0 = off
        xc = [None, None]
        xc[0], off = carve(off, [T + 4], BF16)
        xc[1], off = carve(off, [T + 4], BF16)
        diag, off = carve(off, [5, 128], BF16)
        ce = [None, None]
        cy = [None, None]
        for i in range(2):
            ce[i], off = carve(off, [512], F32)
            cy[i], off = carve(off, [512], F32)
        cysq, off = carve(off, [512], BF16)
        crs, off = carve(off, [512], F32)
        CONV_END = off
        off = TMP0
        GMB, off = carve(off, [HG, 128], F32)
        Wd, off = carve(off, [HG, 128], F32)
        decT, off = carve(off, [HG, 128], F32)
        Lm, off = carve(off, [HG, 128], BF16)
        LTm, off = carve(off, [HG, 128], BF16)
        XT, off = carve(off, [HG, 128], BF16)
        Pp = [None, None]
        PTp = [None, None]
        for i in range(2):
            Pp[i], off = carve(off, [HG, 128], BF16)
            PTp[i], off = carve(off, [HG, 128], BF16)
        kbg, off = carve(off, [HG, 128], BF16)
        vbeta, off = carve(off, [HG, 128], BF16)
        qd_tok, off = carve(off, [HG, 128], BF16)
        DB = []
        for d_ in range(2):
            dd = {}
            for nm in ("attnT", "ktl", "qdTg", "wT_sb", "vnew", "Sg_bf"):
                dd[nm], off = carve(off, [HG, 128], BF16)
            dd["u_sb"], off = carve(off, [HG, 128], F32)
            dd["Sg"], off = carve(off, [HG, 128], F32)
            dd["esc"], off = carve(off, [16], F32)
            dd["bg"], off = carve(off, [HG], F32)
            DB.append(dd)
        osum, off = carve(off, [HG, 128], F32)
        fsig, off = carve(off, [1024], F32)
        fG, off = carve(off, [HG, 128], F32)
        frs, off = carve(off, [8], F32)
        SWEEP_END = off

        gdnw_d = dram("gdn_norm_w", [1, 128])
        gvec_d = dram("gdn_vec", [1, 32])
        cw_d = dram("gdn_conv_wT", [128, 24, 5])
        DMA("sp", gdnw_bc, gdnw_d.partition_broadcast(128), "c_gdnw", [], ["gdnw_bc"])
        DMA("sp", gvec[:, 0:32], gvec_d.partition_broadcast(128), "c_gvec", [], ["gvec"])
        DMA("sp", cwT, cw_d[:, :, :], "c_cw", [], ["cwT"])
        DMA("pool", wdab, w_in_d[:, C_DAB:C_DAB + 32].rearrange("(k p) c -> p k c", p=128), "w_wdab", [], ["wdab"])
        ACTF(gvec[:, 16:32], gvec[:, 16:32], AF.Exp, ["gvec"], ["gvec"])
        TS("dve", gvec[:, 16:32], gvec[:, 16:32], -1.0, None, ALU.mult, ALU.bypass, ["gvec"], ["gvec"])
        for n in range(NT):
            bi = n % 2
            for kt in range(KT):
                MM(bank(bi)[:, 0:32], hT[:, kt, n * 128:(n + 1) * 128], wdab[:, kt, :], kt == 0, kt == KT - 1,
                   ["wdab", ("hT", n)], [bkey(bi)])
            CP("act", dabs[:, n, :], bank(bi)[:, 0:32], [bkey(bi)], ["dabs"])
        a_view = dabs[:, :, 0:16]
        b_view = dabs[:, :, 16:32]
        g_flat = g_raw.rearrange("p n d h -> p n (d h)")
        be_flat = beta.rearrange("p n d h -> p n (d h)")
        TT("dve", g_flat, a_view, gvec[:, 0:16].unsqueeze(1).to_broadcast([128, NT, 16]), ALU.add, ["dabs", "gvec"], ["g_raw"])
        ACTF(g_flat, g_flat, AF.Exp, ["g_raw"], ["g_raw"])
        ACTF(g_flat, g_flat, AF.Ln, ["g_raw"], ["g_raw"], bias=1.0)
        TT("dve", g_flat, g_flat, gvec[:, 16:32].unsqueeze(1).to_broadcast([128, NT, 16]), ALU.mult, ["g_raw", "gvec"], ["g_raw"])
        ACTF(be_flat, b_view, AF.Exp, ["dabs"], ["beta"], scale=-1.0)
        TS("dve", be_flat, be_flat, 1.0, None, ALU.add, ALU.bypass, ["beta"], ["beta"])
        RECIP(be_flat, be_flat, ["beta"], ["beta"])
        MARK("d0")

        GSCALE = 128.0 ** -0.5
        ident_bc4 = ident_b[:].unsqueeze(1).to_broadcast([128, HG, 128])

        def bc_h(ap2):
            return ap2.unsqueeze(2).to_broadcast([128, HG, 128])

        def bc_m(ap2):
            return ap2.unsqueeze(1).to_broadcast([128, HG, 128])

        def v4(ap2):
            return ap2.rearrange("p (h d) -> p h d", h=HG)

        for grp in range(2):
            hs0 = grp * HG
            for which, c_base, dstT in ((0, C_DQ, gqT), (1, C_DK, gkT), (2, C_DV, gvT)):
                ws = wsl[which % 2]
                wk = ("wsl", which % 2)
                DMA("pool", ws, w_in_d[:, c_base + hs0 * 128:c_base + (hs0 + HG) * 128].rearrange("(k p) c -> p k c", p=128),
                    ("w_wsl", which % 2), [], [wk])
                for hh in range(HG):
                    ci = which * 8 + hs0 + hh
                    xi = (which * HG + hh) % 2
                    xcb = xc[xi]
                    xk = ("xc", xi)
                    MEMSET("pool", xcb[:, 0:2], 0.0, [xk])
                    MEMSET("pool", xcb[:, T + 2:T + 4], 0.0, [xk])
                    for k in range(5):
                        TS("dve", diag[:, k, :], cst("ident"), cwT[:, ci, k:k + 1], None, ALU.mult, ALU.bypass,
                           ["consts", "cwT"], ["diag"])
                    for tg in range(4):
                        bi = tg % 2
                        for kt in range(KT):
                            MM(bank(bi), ws[:, kt, hh * 128:(hh + 1) * 128], hT[:, kt, tg * 512:(tg + 1) * 512],
                               kt == 0, kt == KT - 1, [wk] + HT_ALL[tg * 4:tg * 4 + 4], [bkey(bi)])
                        CP("act" if tg % 2 else "dve", xcb[:, 2 + tg * 512:2 + (tg + 1) * 512], bank(bi), [bkey(bi)], [xk])
                    for tg in range(4):
                        bi = 2 + tg % 2
                        i2 = tg % 2
                        for k in range(5):
                            MM(bank(bi), diag[:, k, :], xcb[:, tg * 512 + k:tg * 512 + k + 512], k == 0, k == 4,
                               ["diag", xk], [bkey(bi)])
                        ck = ("ctmp", i2)
                        ACTF(ce[i2], bank(bi), AF.Exp, [bkey(bi)], [ck], scale=-1.0)
                        ACTF(ce[i2], ce[i2], AF.Ln, [ck], [ck], bias=1.0)
                        ACTF(ce[i2], ce[i2], AF.Exp, [ck], [ck], scale=-1.0)
                        dst = dstT[:, hh, tg * 512:(tg + 1) * 512]
                        dk = ("gT", which, hh, tg)
                        if which == 2:
                            TT("dve", dst, ce[i2], bank(bi), ALU.mult, [ck, bkey(bi)], [dk])
                        else:
                            TT("dve", cy[i2], ce[i2], bank(bi), ALU.mult, [ck, bkey(bi)], [("cy", i2)])
                            TT("pool", cysq, cy[i2], cy[i2], ALU.mult, [("cy", i2)], ["cysq"])
                            MM(bank(4), ones_b[:], cysq, True, True, ["ones_b", "cysq"], [bkey(4)])
                            ACTF(crs, bank(4), AF.Ln, [bkey(4)], ["crs"], bias=EPS)
                            ACTF(crs, crs, AF.Exp, ["crs"], ["crs"], scale=-0.5)
                            if which == 0:
                                STT(dst, cy[i2], GSCALE, crs, ALU.mult, ALU.mult, [("cy", i2), "crs"], [dk])
                            else:
                                TT("dve", dst, cy[i2], crs, ALU.mult, [("cy", i2), "crs"], [dk])
            MARK("d1")
            P.barrier()
            DMA("pool", wsl[0], w_in_d[:, C_DZ + hs0 * 128:C_DZ + (hs0 + HG) * 128].rearrange("(k p) c -> p k c", p=128),
                ("w_wsl", 0), [], [("wsl", 0)])
            DMA("pool", wsl[1], w_in_d[:, C_MB + hs0 * 128:C_MB + (hs0 + HG) * 128].rearrange("(k p) c -> p k c", p=128),
                ("w_wsl", 1), [], [("wsl", 1)])

            def gT_keys(which, n):
                return [("gT", which, hh, n // 4) for hh in range(HG)]

            stored = set()

            def gdn_tile(d_, n):
                B = DB[d_]
                dk = lambda nm: (nm, d_)
                Mc = cst("b_le") if d_ == 0 else cst("b_ge")
                Ms = cst("b_gt") if d_ == 0 else cst("b_lt")
                esc, bg = B["esc"], B["bg"]
                tsl = slice(n * 128, (n + 1) * 128)
                gv = g_raw[:, n, d_, hs0:hs0 + HG]
                bv = beta[:, n, d_, hs0:hs0 + HG]
                MM(bank(0)[:, 0:4], Mc, gv, True, True, ["consts", "g_raw"], [bkey(0)])
                MM(bank(0)[:, 4:8], Ms, gv, True, True, ["consts", "g_raw"], [bkey(0)])
                MM(bank(0)[:, 8:12], cst("csel0"), gv, True, True, ["consts", "g_raw"], [bkey(0)])
                MM(bank(0)[:, 12:16], cst("csel1"), gv, True, True, ["consts", "g_raw"], [bkey(0)])
                ACTF(esc, bank(0)[:, 0:16], AF.Exp, [bkey(0)], [dk("esc")])
                TT("dve", bg, bv, esc[:, 0:4], ALU.mult, ["beta", dk("esc")], [dk("bg")])
                for hh in range(HG):
                    TR(bbank(0)[:, hh * 128:(hh + 1) * 128], gkT[:, hh, tsl], ident_b[:], gT_keys(1, n) + ["ident_b"], [("pbb", 0)])
                for hh in range(HG):
                    TR(bbank(1)[:, hh * 128:(hh + 1) * 128], gvT[:, hh, tsl], ident_b[:], gT_keys(2, n) + ["ident_b"], [("pbb", 1)])
                TT("dve", kbg, v4(bbank(0)[:, 0:512]), bc_h(bg), ALU.mult, [("pbb", 0), dk("bg")], ["kbg"])
                TT("dve", B["ktl"], v4(bbank(0)[:, 0:512]), bc_h(esc[:, 4:8]), ALU.mult, [("pbb", 0), dk("esc")], [dk("ktl")])
                TT("dve", vbeta, v4(bbank(1)[:, 0:512]), bc_h(bv), ALU.mult, [("pbb", 1), "beta"], ["vbeta"])
                for hh in range(HG):
                    TR(bbank(0)[:, hh * 128:(hh + 1) * 128], gqT[:, hh, tsl], ident_b[:], gT_keys(0, n) + ["ident_b"], [("pbb", 0)])
                TT("dve", qd_tok, v4(bbank(0)[:, 0:512]), bc_h(esc[:, 0:4]), ALU.mult, [("pbb", 0), dk("esc")], ["qd_tok"])
                for hh in range(HG):
                    TR(bbank(1)[:, hh * 128:(hh + 1) * 128], qd_tok[:, hh, :], ident_b[:], ["qd_tok", "ident_b"], [("pbb", 1)])
                CP("act", B["qdTg"], v4(bbank(1)[:, 0:512]), [("pbb", 1)], [dk("qdTg")])
                TT("pool", GMB, bc_h(gv), bc_m(Ms), ALU.mult, ["g_raw", "consts"], ["GMB"])
                MM(bank(1), Mc, GMB.rearrange("p h s -> p (h s)"), True, True, ["consts", "GMB"], [bkey(1)])
                ACTF(Wd.rearrange("p h s -> p (h s)"), bank(1), AF.Exp, [bkey(1)], ["Wd"])
                TT("pool", GMB, bc_h(bv), bc_m(Ms), ALU.mult, ["beta", "consts"], ["GMB"])
                TT("pool", Wd, Wd, GMB, ALU.mult, ["Wd", "GMB"], ["Wd"])
                TT("pool", GMB, bc_h(gv), bc_m(Mc), ALU.mult, ["g_raw", "consts"], ["GMB"])
                MM(bank(0), Ms, GMB.rearrange("p h s -> p (h s)"), True, True, ["consts", "GMB"], [bkey(0)])
                ACTF(decT.rearrange("p h s -> p (h s)"), bank(0), AF.Exp, [bkey(0)], ["decT"])
                TT("pool", decT, decT, bc_m(Mc), ALU.mult, ["decT", "consts"], ["decT"])
                for hh in range(HG):
                    MM(bank(1)[:, hh * 128:(hh + 1) * 128], gkT[:, hh, tsl], gkT[:, hh, tsl], True, True,
                       gT_keys(1, n), [bkey(1)])
                TT("dve", Lm, v4(bank(1)), Wd, ALU.mult, [bkey(1), "Wd"], ["Lm"])
                for hh in range(HG):
                    MM(bank(0)[:, hh * 128:(hh + 1) * 128], gkT[:, hh, tsl], gqT[:, hh, tsl], True, True,
                       gT_keys(1, n) + gT_keys(0, n), [bkey(0)])
                TT("dve", B["attnT"], v4(bank(0)), decT, ALU.mult, [bkey(0), "decT"], [dk("attnT")])
                for hh in range(HG):
                    TR(bbank(0)[:, hh * 128:(hh + 1) * 128], Lm[:, hh, :], ident_b[:], ["Lm", "ident_b"], [("pbb", 0)])
                CP("act", LTm, v4(bbank(0)[:, 0:512]), [("pbb", 0)], ["LTm"])
                TT("dve", XT, ident_bc4, v4(bbank(0)[:, 0:512]), ALU.subtract, ["ident_b", ("pbb", 0)], ["XT"])
                Pc, PTc = Lm, LTm
                pck, ptk_ = "Lm", "LTm"
                for it in range(5):
                    Pn, PTn = Pp[it % 2], PTp[it % 2]
                    pnk, ptnk = ("Pp", it % 2), ("PTp", it % 2)
                    for hh in range(HG):
                        MM(bank(1)[:, hh * 128:(hh + 1) * 128], PTc[:, hh, :], Pc[:, hh, :], True, True, [pck, ptk_], [bkey(1)])
                    CP("act", Pn, v4(bank(1)), [bkey(1)], [pnk])
                    if it < 4:
                        for hh in range(HG):
                            MM(bank(0)[:, hh * 128:(hh + 1) * 128], Pc[:, hh, :], PTc[:, hh, :], True, True, [pck, ptk_], [bkey(0)])
                        CP("dve", PTn, v4(bank(0)), [bkey(0)], [ptnk])
                    for hh in range(HG):
                        MM(bank(1)[:, hh * 128:(hh + 1) * 128], Pn[:, hh, :], XT[:, hh, :], True, True, [pnk, "XT"], [bkey(1)])
                    TT("dve", XT, XT, v4(bank(1)), ALU.add, ["XT", bkey(1)], ["XT"])
                    Pc, PTc, pck, ptk_ = Pn, PTn, pnk, ptnk
                for hh in range(HG):
                    MM(bank(0)[:, hh * 128:(hh + 1) * 128], XT[:, hh, :], vbeta[:, hh, :], True, True, ["XT", "vbeta"], [bkey(0)])
                CP("act", B["u_sb"], v4(bank(0)), [bkey(0)], [dk("u_sb")])
                for hh in range(HG):
                    MM(bank(1)[:, hh * 128:(hh + 1) * 128], kbg[:, hh, :], XT[:, hh, :], True, True, ["kbg", "XT"], [bkey(1)])
                CP("dve", B["wT_sb"], v4(bank(1)), [bkey(1)], [dk("wT_sb")])
                sb0 = 4 if d_ == 0 else 2
                Sg, Sg_bf, vnew = B["Sg"], B["Sg_bf"], B["vnew"]
                chunks = (0, 1) if d_ == 0 else (1, 0)
                for c in chunks:
                    sl = slice(c * 64, c * 64 + 64)
                    for hh in range(HG):
                        MM(bank(sb0)[sl, hh * 128:(hh + 1) * 128], B["wT_sb"][:, hh, sl], Sg_bf[:, hh, :], True, True,
                           [dk("wT_sb"), dk("Sg_bf")], [bkey(sb0)])
                    TT("dve", vnew[sl], B["u_sb"][sl], v4(bank(sb0))[sl], ALU.subtract, [dk("u_sb"), bkey(sb0)], [dk("vnew")])
                    for hh in range(HG):
                        MM(bank(sb0 + 1)[sl, hh * 128:(hh + 1) * 128], B["qdTg"][:, hh, sl], Sg_bf[:, hh, :], True, False,
                           [dk("qdTg"), dk("Sg_bf")], [bkey(sb0 + 1)])
                        MM(bank(sb0 + 1)[sl, hh * 128:(hh + 1) * 128], B["attnT"][sl, hh, sl], vnew[sl, hh, :], False, True,
                           [dk("attnT"), dk("vnew")], [bkey(sb0 + 1)])
                    for hh in range(HG):
                        MM(bank(sb0)[:, hh * 128:(hh + 1) * 128], B["ktl"][sl, hh, :], vnew[sl, hh, :], True, True,
                           [dk("ktl"), dk("vnew")], [bkey(sb0)])
                    TT("pool", Sg, Sg, bc_h(esc[:, 8 + 4 * c:12 + 4 * c]), ALU.mult, [dk("Sg"), dk("esc")], [dk("Sg")])
                    TT("dve", Sg, Sg, v4(bank(sb0)), ALU.add, [dk("Sg"), bkey(sb0)], [dk("Sg")])
                    CP("act", Sg_bf, Sg, [dk("Sg")], [dk("Sg_bf")])
                    if n not in stored:
                        CP("act", o_store[sl, n], v4(bank(sb0 + 1))[sl], [bkey(sb0 + 1)], [("o_store", n)])
                    else:
                        TT("dve", osum[sl], v4(bank(sb0 + 1))[sl], o_store[sl, n], ALU.add, [bkey(sb0 + 1), ("o_store", n)], ["osum"])
                if n not in stored:
                    stored.add(n)
                    return
                osq = fsig[:, 0:512].rearrange("p (h d) -> p h d", h=HG)
                TT("pool", osq, osum, osum, ALU.mult, ["osum"], ["fsig"])
                P.op("dve", lambda e: e.tensor_reduce(out=frs[:, 0:HG], in_=osq, axis=AX.X, op=ALU.add), ["fsig"], ["frs"], cost=600.0)
                rstd_inplace(frs[:, 0:HG], 128, "frs")
                for half, ws in enumerate(wsl):
                    for kt in range(KT):
                        MM(bank(half), hT[:, kt, tsl], ws[:, kt, :], kt == 0, kt == KT - 1,
                           [("wsl", half), ("hT", n)], [bkey(half)])
                zm = pt[0][:, :]
                ACTF(fsig, zm, AF.Exp, [bkey(0), bkey(1)], ["fsig"], scale=-1.0)
                ACTF(fsig, fsig, AF.Ln, ["fsig"], ["fsig"], bias=1.0)
                ACTF(fsig, fsig, AF.Exp, ["fsig"], ["fsig"], scale=-1.0)
                TT("pool", fG.rearrange("p h d -> p (h d)"), fsig[:, 0:512], fsig[:, 512:1024], ALU.mult, ["fsig"], ["fG"])
                TT("dve", fG, fG, v4(bank(0)), ALU.mult, ["fG", bkey(0)], ["fG"])
                TT("pool", fG, fG, bc_m(gdnw_bc), ALU.mult, ["fG", "gdnw_bc"], ["fG"])
                TT("pool", osum, osum, bc_h(frs[:, 0:HG]), ALU.mult, ["osum", "frs"], ["osum"])
                TT("pool", osum, osum, fG, ALU.mult, ["osum", "fG"], ["osum"])
                mslice = mixed[:, n, hs0 * 128:(hs0 + HG) * 128].rearrange("p (h d) -> p h d", h=HG)
                TT("dve", mslice, mslice, osum, ALU.add, ["osum", ("mixed", n)], [("mixed", n)])

            for d_ in range(2):
                MEMSET("dve", DB[d_]["Sg"], 0.0, [("Sg", d_)])
                CP("act", DB[d_]["Sg_bf"], DB[d_]["Sg"], [("Sg", d_)], [("Sg_bf", d_)])
            for i in range(NT):
                gdn_tile(0, i)
                gdn_tile(1, NT - 1 - i)
            MARK("d2")
            P.barrier()

        P.barrier()
        wout_d = dram("w_out", [D, D])
        off = 0
        x1, off = carve(off, [NT, D], F32)
        X1_END = off
        mT, off = carve(off, [KT, T], BF16)
        wout, off = carve(off, [KT, D], BF16)
        hn2 = [None, None]
        hn2[0], off = carve(off, [D], BF16)
        hn2[1], off = carve(off, [D], BF16)
        junk, off = carve(off, [D], BF16)
        n2_bc, off = carve(off, [D], F32)
        DMA("sp", n2_bc, n2_d.partition_broadcast(128), "c_n2", [], ["n2_bc"])
        DMA("pool", wout, wout_d.rearrange("(k p) c -> p k c", p=128), "w_wout", [], ["wout"])
        for n in range(NT):
            b = n % 2
            for kt in range(KT):
                TR(bbank(b)[:, kt * 128:(kt + 1) * 128], mixed[:, n, kt * 128:(kt + 1) * 128], ident_b[:],
                   [("mixed", n), "ident_b"], [("pbb", b)])
            CP("act", mT[:, :, n * 128:(n + 1) * 128], bbank(b).rearrange("p (k t) -> p k t", k=KT), [("pbb", b)], [("mT", n)])
        for n in range(NT):
            tsl = slice(n * 128, (n + 1) * 128)
            DMA("sp", x1[:, n, :], x_d[tsl, :], ("x1ld", n % 4), [], [("x1", n)])
            pp = pt[n % 2]
            for half in range(2):
                for kt in range(KT):
                    MM(pp[:, half * 512:(half + 1) * 512], mT[:, kt, tsl], wout[:, kt, half * 512:(half + 1) * 512],
                       kt == 0, kt == KT - 1, [("mT", n), "wout"], [bkey((n % 2) * 2 + half)])
            TT("dve", x1[:, n, :], x1[:, n, :], pp[:, :], ALU.add, [("x1", n), bkey((n % 2) * 2), bkey((n % 2) * 2 + 1)], [("x1", n)])
            b = n % 2
            ssap = small[:, 8 + b:9 + b]
            ssk = ("ss2", b)
            ACTF(junk, x1[:, n, :], AF.Square, [("x1", n)], ["junk", ssk], accum_out=ssap)
            rstd_inplace(ssap, D, ssk)
            STT(hn2[b], x1[:, n, :], ssap, n2_bc, ALU.mult, ALU.mult, [("x1", n), ssk, "n2_bc"], [("hn2", b)])
            for kt in range(KT):
                TR(bbank(b)[:, kt * 128:(kt + 1) * 128], hn2[b][:, kt * 128:(kt + 1) * 128], ident_b[:],
                   [("hn2", b), "ident_b"], [("pbb", b)])
            CP("act", hT[:, :, tsl], bbank(b).rearrange("p (k t) -> p k t", k=KT), [("pbb", b)], [("hT", n)])
        MARK("e0")

        P.barrier()
        wr_d = dram("moe_wr", [D, 36])
        wg_d = dram("moe_w_gate", [32, D, 256])
        wu_d = dram("moe_w_up", [32, D, 256])
        wd_d = dram("moe_w_down", [32, 256, D])
        off = X1_END
        EG = 2
        mix32 = mixed[:].rearrange("p n d -> p (n d)").bitcast(F32)
        MIXCAP = 32 * 1024
        moff = 0
        hidT, moff = carve(moff, [EG, 2, T], BF16, mix32, MIXCAP)
        wgu = []
        for i in range(2):
            a, off = carve(off, [KT, 512], BF16)
            wgu.append(a)
        wdn = []
        for i in range(4):
            a, off = carve(off, [2, D], BF16)
            wdn.append(a)
        wr, off = carve(off, [KT, 36], BF16)
        lg, off = carve(off, [NT, 36], F32)
        gate, off = carve(off, [NT, 32], F32)
        msk, off = carve(off, [NT, 32], F32)
        oh, off = carve(off, [NT, 32], F32)
        gtmp, off = carve(off, [NT, 4], F32)
        ohg, off = carve(off, [NT, 4], F32)
        rv, off = carve(off, [8, NT], F32)
        GT, moff = carve(moff, [T], BF16, mix32, MIXCAP)
        st = [None, None]
        tt_ = [None, None]
        for i in range(2):
            st[i], moff = carve(moff, [512], F32, mix32, MIXCAP)
            tt_[i], moff = carve(moff, [512], F32, mix32, MIXCAP)
        MOE_END = off

        DMA("pool", wr, wr_d.rearrange("(k p) c -> p k c", p=128), "w_wr", [], ["wr"])
        for n in range(NT):
            bi = n % 2
            for kt in range(KT):
                MM(bank(bi)[:, 0:36], hT[:, kt, n * 128:(n + 1) * 128], wr[:, kt, :], kt == 0, kt == KT - 1,
                   ["wr", ("hT", n)], [bkey(bi)])
            CP("act", lg[:, n, :], bank(bi)[:, 0:36], [bkey(bi)], ["lg"])
        BIG = 10000.0
        glv = lg[:, :, 0:4]
        elv = lg[:, :, 4:36]

        def RED(out, in_, op, R, W):
            P.op("dve", lambda e: e.tensor_reduce(out=out, in_=in_, axis=AX.X, op=op), R, W)

        def bcn(ap2, k):
            return ap2.unsqueeze(2).to_broadcast([128, NT, k])

        gmax, gsum, m1, m2, w1, w2 = (rv[:, i, :] for i in range(6))
        RED(gmax, glv, ALU.max, ["lg"], ["rv"])
        TT("dve", ohg, glv, bcn(gmax, 4), ALU.is_equal, ["lg", "rv"], ["ohg"])
        TT("dve", gtmp, glv, bcn(gmax, 4), ALU.subtract, ["lg", "rv"], ["gtmp"])
        ACTF(gtmp, gtmp, AF.Exp, ["gtmp"], ["gtmp"])
        RED(gsum, gtmp, ALU.add, ["gtmp"], ["rv"])
        RECIP(gsum, gsum, ["rv"], ["rv"])
        TS("dve", ohg, ohg, BIG, -BIG, ALU.mult, ALU.add, ["ohg"], ["ohg"])
        TT("dve", msk.rearrange("p n (g e) -> p n g e", g=4), elv.rearrange("p n (g e) -> p n g e", g=4),
           ohg.unsqueeze(3).to_broadcast([128, NT, 4, 8]), ALU.add, ["lg", "ohg"], ["msk"])
        RED(m1, msk, ALU.max, ["msk"], ["rv"])
        TT("dve", oh, msk, bcn(m1, 32), ALU.is_equal, ["msk", "rv"], ["oh"])
        CP("dve", gate, oh, ["oh"], ["gate"])
        STT(msk, oh, -BIG, msk, ALU.mult, ALU.add, ["oh", "msk"], ["msk"])
        RED(m2, msk, ALU.max, ["msk"], ["rv"])
        TT("dve", oh, msk, bcn(m2, 32), ALU.is_equal, ["msk", "rv"], ["oh"])
        TT("dve", w2, m2, m1, ALU.subtract, ["rv"], ["rv"])
        ACTF(w2, w2, AF.Exp, ["rv"], ["rv"])
        TS("dve", w1, w2, 1.0, None, ALU.add, ALU.bypass, ["rv"], ["rv"])
        RECIP(w1, w1, ["rv"], ["rv"])
        TT("dve", w1, w1, gsum, ALU.mult, ["rv"], ["rv"])
        TT("dve", w2, w2, w1, ALU.mult, ["rv"], ["rv"])
        TT("dve", gate, gate, bcn(w1, 32), ALU.mult, ["gate", "rv"], ["gate"])
        TT("dve", oh, oh, bcn(w2, 32), ALU.mult, ["oh", "rv"], ["oh"])
        TT("dve", gate, gate, oh, ALU.add, ["gate", "oh"], ["gate"])
        for n in range(NT):
            bi = n % 2
            P.op("pe", lambda e, n=n, bi=bi: e.transpose(out=bank(bi)[0:32, 0:128], in_=gate[:, n, :], identity=cst("ident")),
                 ["gate", "consts"], [bkey(bi)])
            CP("act", GT[0:32, n * 128:(n + 1) * 128], bank(bi)[0:32, 0:128], [bkey(bi)], ["GT"])
        MARK("e1")

        def load_expert(e):
            s = e % 2
            DMA("pool", wgu[s][:, :, 0:256], wg_d[e].rearrange("(k p) c -> p k c", p=128), ("w_wgu", s), [], [("wgu", s)])
            DMA("pool", wgu[s][:, :, 256:512], wu_d[e].rearrange("(k p) c -> p k c", p=128), ("w_wgu", s), [], [("wgu", s)])
            s4 = e % 4
            DMA("pool", wdn[s4], wd_d[e].rearrange("(k p) c -> p k c", p=128), ("w_wdn", s4), [], [("wdn", s4)])

        load_expert(0)
        for e in range(32):
            if e + 1 < 32:
                load_expert(e + 1)
            s = e % 2
            es = e % EG
            for tg in range(4):
                hk = HT_ALL[tg * 4:tg * 4 + 4]
                gbk = 4 + tg % 2
                MM(bank(gbk), ident_b[0:32, e:e + 1].to_broadcast([32, 128]), GT[0:32, tg * 512:(tg + 1) * 512],
                   True, True, ["ident_b", "GT"], [bkey(gbk)])
                for ft in range(2):
                    i2 = (tg * 2 + ft) % 2
                    gb, ub = bank(i2 * 2), bank(i2 * 2 + 1)
                    gk, uk = bkey(i2 * 2), bkey(i2 * 2 + 1)
                    for kt in range(KT):
                        MM(gb, wgu[s][:, kt, ft * 128:(ft + 1) * 128], hT[:, kt, tg * 512:(tg + 1) * 512],
                           kt == 0, kt == KT - 1, [("wgu", s)] + hk, [gk])
                    for kt in range(KT):
                        MM(ub, wgu[s][:, kt, 256 + ft * 128:256 + (ft + 1) * 128], hT[:, kt, tg * 512:(tg + 1) * 512],
                           kt == 0, kt == KT - 1, [("wgu", s)] + hk, [uk])
                    ACTF(st[i2], gb, AF.Silu, [gk], [("st", i2)])
                    TT("dve", tt_[i2], st[i2], ub, ALU.mult, [("st", i2), uk], [("tt", i2)])
                    TT("dve", hidT[:, es, ft, tg * 512:(tg + 1) * 512], tt_[i2], bank(gbk), ALU.mult,
                       [("tt", i2), bkey(gbk)], [("hidT", es, ft, tg)])
            if es == EG - 1:
                for n in range(NT):
                    tsl = slice(n * 128, (n + 1) * 128)
                    pp = pt[2] if n % 2 == 0 else pt[1]
                    kb0 = 4 if n % 2 == 0 else 2
                    for half in range(2):
                        cnt = 0
                        for ee in range(EG):
                            eid = e - (EG - 1) + ee
                            for ft in range(2):
                                MM(pp[:, half * 512:(half + 1) * 512], hidT[:, ee, ft, tsl],
                                   wdn[eid % 4][:, ft, half * 512:(half + 1) * 512], cnt == 0, cnt == 2 * EG - 1,
                                   [("hidT", ee, ft, n // 4), ("wdn", eid % 4)], [bkey(kb0 + half)])
                                cnt += 1
                    TT("dve", x1[:, n, :], x1[:, n, :], pp[:, :], ALU.add, [("x1", n), bkey(kb0), bkey(kb0 + 1)], [("x1", n)])
        MARK("e2")

        P.barrier()
        off = X1_END
        nf_bc, off = carve(off, [D], F32)
        ob = [None, None]
        ob[0], off = carve(off, [D], F32)
        ob[1], off = carve(off, [D], F32)
        junk2, off = carve(off, [D], BF16)
        DMA("sp", nf_bc, nf_d.partition_broadcast(128), "c_nf", [], ["nf_bc"])
        for n in range(NT):
            b = n % 2
            ssap = small[:, 12 + b:13 + b]
            ssk = ("ss3", b)
            ACTF(junk2, x1[:, n, :], AF.Square, [("x1", n)], ["junk2", ssk], accum_out=ssap)
            rstd_inplace(ssap, D, ssk)
            STT(ob[b], x1[:, n, :], ssap, nf_bc, ALU.mult, ALU.mult, [("x1", n), ssk, "nf_bc"], [("ob", b)])
            DMA("sp", out_d[n * 128:(n + 1) * 128, :], ob[b], ("out_st", b), [("ob", b)], [("out", n)])
        if not dbg:
            P.wait_all("sp", [("out", n) for n in range(NT)])
        if dbg:
            P.enabled = True
            P.barrier()
            for n in range(NT):
                DMA("sp", dbg_d[n * 128:(n + 1) * 128, :], x1[:, n, :], ("dbg_out", n % 2), [("x1", n)], [("dbg", n)])
            P.wait_all("sp", [("dbg", n) for n in range(NT)] + [("out", n) for n in range(NT)])
        P.emit()
    return nc


def make_in_maps(inputs, n_cores=8):
    f = lambda k: np.asarray(inputs[k], np.float32)
    x = f("x")
    shared = {
        "norm1_w": f("norm1_w").reshape(1, D),
        "norm2_w": f("norm2_w").reshape(1, D),
        "norm_f_w": f("norm_f_w").reshape(1, D),
        "consts": CONST_ARR,
        "w_in": np.ascontiguousarray(f("w_in")[0]),
        "gla_w2b_f": np.ascontiguousarray(np.concatenate([f("gla_gate_w2_fwd")[0], f("gla_gate_b_fwd")], axis=0)),
        "gla_w2b_b": np.ascontiguousarray(np.concatenate([f("gla_gate_w2_bwd")[0], f("gla_gate_b_bwd")], axis=0)),
        "gla_norm_w": f("gla_norm_w").reshape(1, 256),
        "w_out": np.ascontiguousarray(f("w_out")[0]),
        "moe_wr": np.ascontiguousarray(np.concatenate([f("moe_w_group")[0], f("moe_w_router")[0]], axis=1)),
        "moe_w_gate": np.ascontiguousarray(f("moe_w_gate")[0]),
        "moe_w_up": np.ascontiguousarray(f("moe_w_up")[0]),
        "moe_w_down": np.ascontiguousarray(f("moe_w_down")[0]),
        "gdn_norm_w": f("gdn_norm_w").reshape(1, 128),
        "gdn_vec": np.ascontiguousarray(np.concatenate([f("gdn_dt_bias_fwd")[0], f("gdn_dt_bias_bwd")[0],
                                                        f("gdn_a_log_fwd")[0], f("gdn_a_log_bwd")[0]]).reshape(1, 32)),
        "gdn_conv_wT": np.ascontiguousarray(f("gdn_conv_w")[0].T.reshape(24, 128, 5).transpose(1, 0, 2)),
    }
    maps = []
    for c in range(n_cores):
        m = dict(shared)
        m["x"] = np.ascontiguousarray(x[c])
        maps.append(m)
    return maps


def kernel(**inputs):
    nc = build()
    in_maps = make_in_maps(inputs)
    res = run_bass_kernel_spmd(nc, in_maps, core_ids=list(range(8)))
    out = np.stack([np.asarray(r["out"]) for r in res.results], axis=0)
    return out.astype(np.float32)
```

```python
import contextlib
import heapq
import numpy as np
import concourse.bass as bass
import concourse.mybir as mybir
from concourse.bass_utils import run_bass_kernel_spmd

F32 = mybir.dt.float32
BF16 = mybir.dt.bfloat16
I32 = mybir.dt.int32
AF = mybir.ActivationFunctionType
ALU = mybir.AluOpType
AX = mybir.AxisListType

T = 2048
D = 1024
NT = T // 128
KT = D // 128
EPS = 1e-6
SAME_ENGINE_SYNC = True
EPOCH = 20000
SYNC_NS = 120.0
DMA_LAT_NS = 2200.0


class Prog:
    ENGS = ("pe", "act", "dve", "pool", "sp")

    def __init__(self, nc, stack):
        self.nc = nc
        self.stack = stack
        self.streams = {e: [] for e in self.ENGS}
        self.count = {e: 0 for e in self.ENGS}
        self.esems = {e: [] for e in self.ENGS}
        self.known = {e: {} for e in self.ENGS}
        self.last_write = {}
        self.readers = {}
        self.dma_sems = {}
        self.dma_vals = {}
        self.dma_last = {}
        self.enabled = True
        self.seg = []
        self.ticks = {}
        self.nops = 0
        self.seg_base = 0

    def _new_sem(self, name):
        return self.stack.enter_context(self.nc.semaphore(name))

    @staticmethod
    def _psum_fix(reads, writes):
        r2, w2 = [], list(writes)
        for k in reads:
            if isinstance(k, tuple) and k[0] in ("pb", "pbb"):
                if k not in w2:
                    w2.append(k)
            else:
                r2.append(k)
        return r2, w2

    def _record(self, eng, fn, reads, writes, cost, kind, semkey=None):
        reads, writes = self._psum_fix(list(reads), list(writes))
        oid = self.nops
        self.nops += 1
        preds = set()
        for r in reads:
            t = self.last_write.get(r)
            if t is not None:
                preds.add(t)
        for w in writes:
            t = self.last_write.get(w)
            if t is not None:
                preds.add(t)
            preds.update(self.readers.get(w, ()))
        if kind == "dma":
            prev = self.dma_last.get(semkey)
            if prev is not None:
                preds.add(prev)
            self.dma_last[semkey] = oid
        preds = {p for p in preds if p >= self.seg_base}
        self.seg.append(dict(id=oid, eng=eng, fn=fn, preds=preds, cost=float(cost), kind=kind, semkey=semkey))
        for w in writes:
            self.last_write[w] = oid
            self.readers[w] = []
        for r in reads:
            self.readers.setdefault(r, []).append(oid)
        return oid

    def op(self, eng, fn, reads=(), writes=(), cost=300.0):
        if not self.enabled:
            return
        self._record(eng, fn, reads, writes, cost, "op")

    def dma(self, eng, fn, semkey, reads=(), writes=(), nbytes=1 << 20):
        if not self.enabled:
            return
        self._record(eng, fn, reads, writes, DMA_LAT_NS + nbytes / 160.0, "dma", semkey)

    def wait_all(self, eng, keys):
        self._record(eng, None, list(keys), [], 0.0, "op")

    def _schedule_segment(self):
        ops = self.seg
        if not ops:
            return
        byid = {o["id"]: o for o in ops}
        succ = {o["id"]: [] for o in ops}
        indeg = {}
        for o in ops:
            indeg[o["id"]] = len(o["preds"])
            for p in o["preds"]:
                succ[p].append(o["id"])
        ready_t = {o["id"]: 0.0 for o in ops}
        finish = {}
        heaps = {e: [] for e in self.ENGS}
        for o in ops:
            if indeg[o["id"]] == 0:
                heapq.heappush(heaps[o["eng"]], (0.0, o["id"]))
        etime = {e: 0.0 for e in self.ENGS}
        order = {e: [] for e in self.ENGS}
        remaining = len(ops)
        while remaining:
            best = None
            for e in self.ENGS:
                h = heaps[e]
                if not h:
                    continue
                rt, oid = h[0]
                st = max(rt, etime[e])
                if best is None or (st, oid) < (best[0], best[1]):
                    best = (st, oid, e)
            st, oid, e = best
            heapq.heappop(heaps[e])
            o = byid[oid]
            if o["kind"] == "dma":
                etime[e] = st + 150.0
                fin = st + o["cost"]
            else:
                etime[e] = st + o["cost"]
                fin = etime[e]
            finish[oid] = fin
            order[e].append(o)
            remaining -= 1
            for s in succ[oid]:
                so = byid[s]
                lat = SYNC_NS if (so["eng"] != e or o["kind"] == "dma") else (60.0 if e != "pe" else 0.0)
                ready_t[s] = max(ready_t[s], fin + lat)
                indeg[s] -= 1
                if indeg[s] == 0:
                    heapq.heappush(heaps[so["eng"]], (ready_t[s], s))
        self.est_ns = getattr(self, "est_ns", 0.0) + max(list(finish.values()) + [0.0])
        for o in ops:
            if o["kind"] == "dma":
                k = o["semkey"]
                if k not in self.dma_sems:
                    self.dma_sems[k] = self._new_sem(f"d{len(self.dma_sems)}")
                    self.dma_vals[k] = 0
                self.dma_vals[k] += 16
                self.ticks[o["id"]] = (self.dma_sems[k], self.dma_vals[k], "dma")
        def needs_sem(o):
            for s_ in succ[o["id"]]:
                se = byid[s_]["eng"]
                if se != o["eng"] or (SAME_ENGINE_SYNC and se != "pe"):
                    return True
            return False
        for e in self.ENGS:
            real = [o for o in order[e] if o["kind"] == "op" and o["fn"] is not None]
            for i_, o in enumerate(real):
                o["sig"] = needs_sem(o) or i_ == len(real) - 1
        for e in self.ENGS:
            for o in order[e]:
                if o["kind"] == "op" and o["fn"] is not None and o["sig"]:
                    c = self.count[e]
                    ep, v = divmod(c, EPOCH)
                    while len(self.esems[e]) <= ep:
                        self.esems[e].append(self._new_sem(f"s_{e}_{len(self.esems[e])}"))
                    self.count[e] = c + 1
                    self.ticks[o["id"]] = (self.esems[e][ep], v + 1, e)
        for e in self.ENGS:
            for o in order[e]:
                waits = {}
                for p in o["preds"]:
                    if byid[p]["eng"] == e and byid[p]["kind"] == "op" and (not SAME_ENGINE_SYNC or e == "pe"):
                        continue
                    sem, val, src = self.ticks[p]
                    sid = id(sem)
                    if self.known[e].get(sid, 0) >= val:
                        continue
                    if sid not in waits or waits[sid][1] < val:
                        waits[sid] = (sem, val)
                for sid, (sem, val) in waits.items():
                    self.known[e][sid] = val
                inc = None
                if o["fn"] is not None and o["id"] in self.ticks:
                    sem, val, src = self.ticks[o["id"]]
                    inc = (sem, 16 if o["kind"] == "dma" else 1)
                self.streams[e].append((o["fn"], list(waits.values()), inc))
        self.seg = []
        self.seg_base = self.nops

    def barrier(self):
        if not self.enabled and not self.seg:
            return
        self._schedule_segment()
        ticks = []
        for e2 in self.ENGS:
            c = self.count[e2]
            if c > 0:
                ep, v = divmod(c - 1, EPOCH)
                ticks.append((self.esems[e2][ep], v + 1))
        for k, sem in self.dma_sems.items():
            ticks.append((sem, self.dma_vals[k]))
        for eng in self.ENGS:
            waits = []
            for (sem, val) in ticks:
                if self.known[eng].get(id(sem), 0) >= val:
                    continue
                self.known[eng][id(sem)] = val
                waits.append((sem, val))
            if waits:
                self.streams[eng].append((None, waits, None))

    def emit(self):
        self._schedule_segment()
        nc = self.nc
        with nc.Block() as block:
            def run(e, stream):
                for fn, waits, inc in stream:
                    for sem, val in waits:
                        e.wait_ge(sem, val)
                    if fn is None:
                        continue
                    ins = fn(e)
                    if inc is not None:
                        ins.then_inc(inc[0], inc[1])

            @block.tensor
            def _(e):
                run(e, self.streams["pe"])

            @block.scalar
            def _(e):
                run(e, self.streams["act"])

            @block.vector
            def _(e):
                run(e, self.streams["dve"])

            @block.gpsimd
            def _(e):
                run(e, self.streams["pool"])

            @block.sync
            def _(e):
                run(e, self.streams["sp"])


def _fsz(ap):
    s = ap.shape
    n = 1
    for v in s[1:]:
        n *= int(v)
    return n


C_GQ, C_GK, C_GV, C_GR = 0, 512, 1024, 2048
C_GLF, C_GLB = 3072, 3088
C_DQ, C_DK, C_DV, C_DZ = 3104, 4128, 5152, 6176
C_DAB = 7200
C_MA, C_MB = 7232, 8256
D_IN = 9280


def host_consts():
    r = np.arange(128)[:, None]
    t = np.arange(128)[None, :]
    same = (r // 64) == (t // 64)
    c = {}
    c["ident"] = np.eye(128, dtype=np.float32)
    c["a_le"] = np.where(r <= t, -1.0 / 16, 0.0)
    c["a_ge"] = np.where(r >= t, -1.0 / 16, 0.0)
    c["a_gt"] = np.where(r > t, -1.0 / 16, 0.0)
    c["a_lt"] = np.where(r < t, -1.0 / 16, 0.0)
    c["m_le"] = np.where(r <= t, 1.0, 0.0)
    c["m_ge"] = np.where(r >= t, 1.0, 0.0)
    c["b_le"] = np.where((r <= t) & same, 1.0, 0.0)
    c["b_ge"] = np.where((r >= t) & same, 1.0, 0.0)
    c["b_gt"] = np.where((r > t) & same, 1.0, 0.0)
    c["b_lt"] = np.where((r < t) & same, 1.0, 0.0)
    c["csel0"] = np.where(r < 64, 1.0, 0.0) + 0.0 * t
    c["csel1"] = np.where(r >= 64, 1.0, 0.0) + 0.0 * t
    c["ones"] = np.ones((128, 128))
    c["m_lt"] = np.where(r < t, 1.0, 0.0)
    c["bvals"] = 128.0 * t + 0.0 * r
    c["pidx"] = 1.0 * r + 0.0 * t
    names = list(c.keys())
    arr = np.stack([np.asarray(c[n], np.float32) for n in names], axis=1)
    return names, np.ascontiguousarray(arr)


CONST_NAMES, CONST_ARR = host_consts()
NCONST = len(CONST_NAMES)


def build(stage="all", dbg=False):
    nc = bass.Bass("TRN2", target_bir_lowering=False)
    stack = contextlib.ExitStack()
    with stack:
        P = Prog(nc, stack)

        def dram(name, shape, dt=F32, kind="ExternalInput"):
            return nc.dram_tensor(name, list(shape), dt, kind=kind).ap()

        def sb(name, shape, dt=F32):
            return stack.enter_context(nc.sbuf_tensor(name, list(shape), dt))

        def ps(name, shape, dt=F32):
            return stack.enter_context(nc.psum_tensor(name, list(shape), dt))

        def MM(out, lhsT, rhs, start, stop, R, W):
            n = _fsz(rhs)
            c = 70.0 + n * 0.75
            if rhs.dtype == F32:
                c *= 4.0
            P.op("pe", lambda e: e.matmul(out, lhsT, rhs, start=start, stop=stop), R, W, cost=c)

        def TR(out, in_, ident, R, W):
            P.op("pe", lambda e: e.transpose(out=out, in_=in_, identity=ident), R, W, cost=110.0)

        def ACTF(out, in_, func, R, W, **kw):
            c = 120.0 + _fsz(in_) * 0.6 + (90.0 if "accum_out" in kw else 0.0)
            P.op("act", lambda e: e.activation(out=out, in_=in_, func=func, **kw), R, W, cost=c)

        def _vc(eng, n, k=1.5):
            return (100.0 + n * k * 0.6) if eng == "dve" else (150.0 + n * 1.9)

        def TT(eng, out, in0, in1, op, R, W):
            P.op(eng, lambda e: e.tensor_tensor(out=out, in0=in0, in1=in1, op=op), R, W, cost=_vc(eng, _fsz(out)))

        def TS(eng, out, in0, s1, s2, op0, op1, R, W):
            P.op(eng, lambda e: e.tensor_scalar(out=out, in0=in0, scalar1=s1, scalar2=s2, op0=op0, op1=op1), R, W,
                 cost=_vc(eng, _fsz(out), 1.05))

        def STT(out, in0, scalar, in1, op0, op1, R, W):
            P.op("dve", lambda e: e.scalar_tensor_tensor(out=out, in0=in0, scalar=scalar, in1=in1, op0=op0, op1=op1), R, W,
                 cost=_vc("dve", _fsz(out)))

        def CP(eng, out, in_, R, W):
            if eng == "act":
                P.op("act", lambda e: e.activation(out=out, in_=in_, func=AF.Copy), R, W, cost=120.0 + _fsz(in_) * 0.6)
            else:
                P.op(eng, lambda e: e.tensor_copy(out=out, in_=in_), R, W, cost=_vc(eng, _fsz(out), 1.05))

        def MEMSET(eng, ap, val, W):
            P.op(eng, lambda e: e.memset(ap, val), [], W, cost=_vc(eng, _fsz(ap), 0.6))

        def DMA(eng, out, in_, semkey, R, W):
            P.dma(eng, lambda e: e.dma_start(out=out, in_=in_), semkey, R, W, nbytes=_fsz(out) * int(out.shape[0]) * 4)

        def RECIP(out, in_, R, W):
            P.op("dve", lambda e: e.reciprocal(out=out, in_=in_), R, W, cost=_vc("dve", _fsz(out), 1.05))

        def MARK(name):
            if stage == name:
                P.enabled = False

        def rstd_inplace(ap, n, key):
            TS("dve", ap, ap, 1.0 / n, EPS, ALU.mult, ALU.add, [key], [key])
            ACTF(ap, ap, AF.Ln, [key], [key])
            ACTF(ap, ap, AF.Exp, [key], [key], scale=-0.5)

        x_d = dram("x", [T, D])
        n1_d = dram("norm1_w", [1, D])
        n2_d = dram("norm2_w", [1, D])
        nf_d = dram("norm_f_w", [1, D])
        consts_d = dram("consts", [128, NCONST, 128])
        w_in_d = dram("w_in", [D, D_IN])
        w2b_d = [dram("gla_w2b_f", [17, 512]), dram("gla_w2b_b", [17, 512])]
        gnw_d = dram("gla_norm_w", [1, 256])
        out_d = dram("out", [T, D], kind="ExternalOutput")
        dbg_d = dram("dbg", [T, D], kind="ExternalOutput") if dbg else None

        consts = sb("consts_sb", [128, NCONST, 128])
        CI = {n: i for i, n in enumerate(CONST_NAMES)}

        def cst(name):
            return consts[:, CI[name], :]

        ident_b = sb("ident_b", [128, 128], BF16)
        ones_b = sb("ones_b", [128, 128], BF16)
        hT = sb("hT", [128, KT, T], BF16)
        mixed = sb("mixed", [128, NT, D], BF16)
        small = sb("small", [128, 64])
        ARENA_BYTES = 134 * 1024
        arena = sb("arena", [128, ARENA_BYTES // 4])

        def carve(off, shape, dt, base=None, cap=None):
            base = arena if base is None else base
            cap = ARENA_BYTES if cap is None else cap
            nb = int(np.prod(shape)) * (2 if dt == BF16 else 4)
            nb = (nb + 3) // 4 * 4
            assert off % 4 == 0 and off + nb <= cap, (off, nb)
            v = base[:, off // 4:(off + nb) // 4]
            if dt != F32:
                v = v.bitcast(dt)
            if len(shape) == 2:
                pat = "p (a b) -> p a b"
                v = v.rearrange(pat, a=shape[0])
            elif len(shape) == 3:
                v = v.rearrange("p (a b c) -> p a b c", a=shape[0], b=shape[1])
            return v, off + nb

        pt = [ps(f"pt{i}", [128, 1024]) for i in range(3)]
        ptb = ps("ptb", [128, 2048], BF16)

        def bank(i):
            return pt[i // 2][:, (i % 2) * 512:(i % 2 + 1) * 512]

        def bkey(i):
            return ("pb", i)

        def bbank(i):
            return ptb[:, i * 1024:(i + 1) * 1024]

        DMA("sp", consts[:], consts_d[:, :, :], "c_consts", [], ["consts"])
        CP("dve", ident_b[:], cst("ident"), ["consts"], ["ident_b"])
        MEMSET("pool", ones_b[:], 1.0, ["ones_b"])

        off = 0
        xt0, off = carve(off, [D], F32)
        xt1, off = carve(off, [D], F32)
        hn0, off = carve(off, [D], BF16)
        hn1, off = carve(off, [D], BF16)
        sq, off = carve(off, [D], F32)
        n1_bc, off = carve(off, [D], F32)
        DMA("sp", n1_bc, n1_d.partition_broadcast(128), "c_n1", [], ["n1_bc"])
        xts = [xt0, xt1]
        hns = [hn0, hn1]
        for tt in range(NT):
            b = tt % 2
            xb, hb = xts[b], hns[b]
            DMA("sp", xb, x_d[tt * 128:(tt + 1) * 128, :], ("xt", b), [], [("xt", b)])
            ACTF(sq, xb, AF.Square, [("xt", b)], ["sq", "ss0"], accum_out=small[:, 0:1])
            rstd_inplace(small[:, 0:1], D, "ss0")
            STT(hb, xb, small[:, 0:1], n1_bc, ALU.mult, ALU.mult, [("xt", b), "ss0", "n1_bc"], [("hn", b)])
            for kt in range(KT):
                TR(bbank(b)[:, kt * 128:(kt + 1) * 128], hb[:, kt * 128:(kt + 1) * 128], ident_b[:],
                   [("hn", b), "ident_b"], [("pbb", b)])
            CP("act", hT[:, :, tt * 128:(tt + 1) * 128], bbank(b).rearrange("p (k t) -> p k t", k=KT),
               [("pbb", b)], [("hT", tt)])
        HT_ALL = [("hT", tt) for tt in range(NT)]
        MARK("p1")

        P.barrier()
        off = 0
        qT, off = carve(off, [T], F32)
        kT, off = carve(off, [T], F32)
        k_tok, off = carve(off, [NT, 128], F32)
        v_tok, off = carve(off, [NT, 256], BF16)
        qdT = [None, None]
        kiT = [None, None]
        ktail = [None, None]
        for d_ in range(2):
            qdT[d_], off = carve(off, [T], BF16)
            kiT[d_], off = carve(off, [T], BF16)
            ktail[d_], off = carve(off, [NT, 128], BF16)
        sb_store, off = carve(off, [NT, 256], BF16)
        dec, off = carve(off, [2, NT], F32)
        S, off = carve(off, [256], F32)
        S_bf, off = carve(off, [256], BF16)
        NTMP = 4
        tmp = []
        for i in range(NTMP):
            d = {}
            for nm in ("e", "lg", "E", "Ei", "Et"):
                d[nm], off = carve(off, [128], F32)
            d["Pf"], off = carve(off, [128], BF16)
            d["Pb"], off = carve(off, [128], BF16)
            d["sig"], off = carve(off, [512], F32)
            d["G"], off = carve(off, [256], F32)
            tmp.append(d)
        gl, off = carve(off, [2, T], BF16)
        w2b, off = carve(off, [2, 512], BF16)
        wqk, off = carve(off, [KT, 256], BF16)
        wkv, off = carve(off, [KT, 384], BF16)
        wgm, off = carve(off, [KT, 512], BF16)
        wgl, off = carve(off, [KT, 32], BF16)
        gnw_bc, off = carve(off, [256], F32)
        GLA_END = off

        DMA("sp", gnw_bc, gnw_d.partition_broadcast(128), "c_gnw", [], ["gnw_bc"])
        MEMSET("pool", gl[0:32, :, :], 1.0, ["gl"])
        MEMSET("pool", w2b[0:32, :, :], 0.0, ["w2b"])
        for d_ in range(2):
            DMA("pool", w2b[0:17, d_, :], w2b_d[d_][:, :], "c_w2b", [], ["w2b"])
        DMA("pool", wgl, w_in_d[:, C_GLF:C_GLF + 32].rearrange("(k p) c -> p k c", p=128), "w_wgl", [], ["wgl"])
        for d_ in range(2):
            for tg in range(4):
                bi = tg % 2
                for kt in range(KT):
                    MM(bank(bi)[0:16, :], wgl[:, kt, d_ * 16:(d_ + 1) * 16], hT[:, kt, tg * 512:(tg + 1) * 512],
                       kt == 0, kt == KT - 1, ["wgl"] + HT_ALL[tg * 4:tg * 4 + 4], [bkey(bi)])
                CP("act", gl[0:16, d_, tg * 512:(tg + 1) * 512], bank(bi)[0:16, :], [bkey(bi)], ["gl"])

        MARK("g0")
        QSCALE = 128.0 ** -0.5
        for h in range(4):
            def wcols(dst, c0, n):
                return (dst, w_in_d[:, c0:c0 + n].rearrange("(k p) c -> p k c", p=128))
            for (dst, src) in (wcols(wqk[:, :, 0:128], C_GQ + h * 128, 128), wcols(wqk[:, :, 128:256], C_GK + h * 128, 128)):
                DMA("pool", dst, src, "w_wqk", [], ["wqk"])
            for (dst, src) in (wcols(wkv[:, :, 0:128], C_GK + h * 128, 128), wcols(wkv[:, :, 128:384], C_GV + h * 256, 256)):
                DMA("pool", dst, src, "w_wkv", [], ["wkv"])
            for (dst, src) in (wcols(wgm[:, :, 0:256], C_GR + h * 256, 256), wcols(wgm[:, :, 256:512], C_MA + h * 256, 256)):
                DMA("pool", dst, src, "w_wgm", [], ["wgm"])
            MARK("g1a")
            for which, dstT in ((0, qT), (1, kT)):
                for tg in range(4):
                    bi = (which * 4 + tg) % 4
                    for kt in range(KT):
                        MM(bank(bi), wqk[:, kt, which * 128:(which + 1) * 128], hT[:, kt, tg * 512:(tg + 1) * 512],
                           kt == 0, kt == KT - 1, ["wqk"] + HT_ALL[tg * 4:tg * 4 + 4], [bkey(bi)])
                    CP("act" if tg % 2 else "dve", dstT[:, tg * 512:(tg + 1) * 512], bank(bi), [bkey(bi)],
                       [("qkT", which, tg)])
            MARK("g1b")
            for n in range(NT):
                bi = 4 + n % 2
                for kt in range(KT):
                    MM(bank(bi)[:, 0:384], hT[:, kt, n * 128:(n + 1) * 128], wkv[:, kt, :],
                       kt == 0, kt == KT - 1, ["wkv", ("hT", n)], [bkey(bi)])
                CP("dve", k_tok[:, n, :], bank(bi)[:, 0:128], [bkey(bi)], [("k_tok", n)])
                CP("act", v_tok[:, n, :], bank(bi)[:, 128:384], [bkey(bi)], [("v_tok", n)])
            MARK("g1")
            for n in range(NT):
                tsl = slice(n * 128, (n + 1) * 128)
                tg = n // 4
                for d_ in range(2):
                    tm = tmp[(n * 2 + d_) % NTMP]
                    tk = ("gtmp", (n * 2 + d_) % NTMP)
                    a_c = cst("a_le") if d_ == 0 else cst("a_ge")
                    a_s = cst("a_gt") if d_ == 0 else cst("a_lt")
                    b0 = (n * 2 + d_) % 2 * 2
                    zb, cb = bank(b0), bank(b0 + 1)
                    MM(zb[:, 0:128], gl[0:32, d_, tsl], w2b[0:32, d_, h * 128:(h + 1) * 128], True, True,
                       ["gl", "w2b"], [bkey(b0)])
                    ACTF(tm["e"], zb[:, 0:128], AF.Exp, [bkey(b0)], [tk], scale=-1.0)
                    ACTF(tm["lg"], tm["e"], AF.Ln, [tk], [tk], bias=1.0)
                    MM(cb[:, 0:128], tm["lg"], a_c, True, True, [tk, "consts"], [bkey(b0 + 1)])
                    MM(cb[:, 128:256], a_s, tm["lg"], True, True, [tk, "consts"], [bkey(b0 + 1)])
                    ACTF(tm["E"], cb[:, 0:128], AF.Exp, [bkey(b0 + 1)], [tk])
                    ACTF(tm["Ei"], cb[:, 0:128], AF.Exp, [bkey(b0 + 1)], [tk], scale=-1.0)
                    ACTF(tm["Et"], cb[:, 128:256], AF.Exp, [bkey(b0 + 1)], [tk])
                    STT(qdT[d_][:, tsl], qT[:, tsl], QSCALE, tm["E"], ALU.mult, ALU.mult,
                        [("qkT", 0, tg), tk], [("qdT", d_, n)])
                    TT("dve", kiT[d_][:, tsl], kT[:, tsl], tm["Ei"], ALU.mult, [("qkT", 1, tg), tk], [("kiT", d_, n)])
                    TT("dve", ktail[d_][:, n, :], k_tok[:, n, :], tm["Et"], ALU.mult, [("k_tok", n), tk], [("ktail", d_, n)])
                    col = 127 if d_ == 0 else 0
                    CP("dve", dec[:, d_, n:n + 1], tm["E"][:, col:col + 1], [tk], [("dec", d_, n)])
            MARK("g2")
            MEMSET("dve", S, 0.0, ["S"])
            for n in range(NT - 1, -1, -1):
                CP("act", sb_store[:, n, :], S, ["S"], [("sb_store", n)])
                bi = 4 + n % 2
                MM(bank(bi)[:, 0:256], ktail[1][:, n, :], v_tok[:, n, :], True, True,
                   [("ktail", 1, n), ("v_tok", n)], [bkey(bi)])
                STT(S, S, dec[:, 1, n:n + 1], bank(bi)[:, 0:256], ALU.mult, ALU.add,
                    ["S", ("dec", 1, n), bkey(bi)], ["S"])
            MARK("g3")
            MEMSET("dve", S, 0.0, ["S"])
            for n in range(NT):
                tsl = slice(n * 128, (n + 1) * 128)
                tm = tmp[n % NTMP]
                tk = ("ftmp", n % NTMP)
                CP("act", S_bf, S, ["S"], ["S_bf"])
                b0 = (n % 2) * 2
                sc = bank(b0)
                MM(sc[:, 0:128], kiT[0][:, tsl], qdT[0][:, tsl], True, True, [("kiT", 0, n), ("qdT", 0, n)], [bkey(b0)])
                MM(sc[:, 128:256], kiT[1][:, tsl], qdT[1][:, tsl], True, True, [("kiT", 1, n), ("qdT", 1, n)], [bkey(b0)])
                TT("dve", tm["Pf"], sc[:, 0:128], cst("m_le"), ALU.mult, [bkey(b0), "consts"], [tk])
                TT("dve", tm["Pb"], sc[:, 128:256], cst("m_ge"), ALU.mult, [bkey(b0), "consts"], [tk])
                ob = bank(b0 + 1)
                ok = bkey(b0 + 1)
                MM(ob[:, 0:256], qdT[0][:, tsl], S_bf, True, False, [("qdT", 0, n), "S_bf"], [ok])
                MM(ob[:, 0:256], qdT[1][:, tsl], sb_store[:, n, :], False, False, [("qdT", 1, n), ("sb_store", n)], [ok])
                MM(ob[:, 0:256], tm["Pf"], v_tok[:, n, :], False, False, [tk, ("v_tok", n)], [ok])
                MM(ob[:, 0:256], tm["Pb"], v_tok[:, n, :], False, True, [tk, ("v_tok", n)], [ok])
                kb = 4 + n % 2
                MM(bank(kb)[:, 0:256], ktail[0][:, n, :], v_tok[:, n, :], True, True,
                   [("ktail", 0, n), ("v_tok", n)], [bkey(kb)])
                STT(S, S, dec[:, 0, n:n + 1], bank(kb)[:, 0:256], ALU.mult, ALU.add,
                    ["S", ("dec", 0, n), bkey(kb)], ["S"])
                gb = 4 + n % 2
                for kt in range(KT):
                    MM(bank(gb), hT[:, kt, tsl], wgm[:, kt, :], kt == 0, kt == KT - 1, ["wgm", ("hT", n)], [bkey(gb)])
                ACTF(tm["sig"], bank(gb), AF.Exp, [bkey(gb)], [("sig", n % NTMP)], scale=-1.0)
                ACTF(tm["sig"], tm["sig"], AF.Ln, [("sig", n % NTMP)], [("sig", n % NTMP)], bias=1.0)
                ACTF(tm["sig"], tm["sig"], AF.Exp, [("sig", n % NTMP)], [("sig", n % NTMP)], scale=-1.0)
                TT("pool", tm["G"], tm["sig"][:, 0:256], tm["sig"][:, 256:512], ALU.mult, [("sig", n % NTMP)], [("G", n % NTMP)])
                TT("dve", tm["G"], tm["G"], bank(gb)[:, 0:256], ALU.mult, [("G", n % NTMP), bkey(gb)], [("G", n % NTMP)])
                TT("pool", tm["G"], tm["G"], gnw_bc, ALU.mult, [("G", n % NTMP), "gnw_bc"], [("G", n % NTMP)])
                ssk = ("ssq", n % 2)
                ssap = small[:, 2 + n % 2:3 + n % 2]
                ACTF(tm["sig"][:, 0:256], ob[:, 0:256], AF.Square, [ok, ("G", n % NTMP)], [("sig", n % NTMP), ssk],
                     accum_out=ssap)
                rstd_inplace(ssap, 256, ssk)
                STT(mixed[:, n, h * 256:(h + 1) * 256], ob[:, 0:256], ssap, tm["G"], ALU.mult, ALU.mult,
                    [ok, ssk, ("G", n % NTMP)], [("mixed", n)])

        P.barrier()
        HG = 4
        off = 0
        gqT, off = carve(off, [HG, T], BF16)
        gkT, off = carve(off, [HG, T], BF16)
        gvT, off = carve(off, [HG, T], BF16)
        o_store, off = carve(off, [NT, HG, 128], BF16)
        dabs, off = carve(off, [NT, 32], F32)
        g_raw, off = carve(off, [NT, 2, 8], F32)
        beta, off = carve(off, [NT, 2, 8], F32)
        gvec, off = carve(off, [64], F32)
        wsl0, off = carve(off, [KT, 512], BF16)
        wsl1, off = carve(off, [KT, 512], BF16)
        wsl = [wsl0, wsl1]
        cwT, off = carve(off, [24, 5], F32)
        gdnw_bc, off = carve(off, [128], F32)
        wdab, off = carve(off, [KT, 32], BF16)
        TMP0 = off
        xc = [None, None]
        xc[0], off = carve(off, [T + 4], BF16)
        xc[1], off = carve(off, [T + 4], BF16)
        diag, off = carve(off, [5, 128], BF16)
        ce = [None, None]
        cy = [None, None]
        for i in range(2):
            ce[i], off = carve(off, [512], F32)
            cy[i], off = carve(off, [512], F32)
        cysq, off = carve(off, [512], BF16)
        crs, off = carve(off, [512], F32)
        CONV_END = off
        off = TMP0
        GMB, off = carve(off, [HG, 128], F32)
        Wd, off = carve(off, [HG, 128], F32)
        decT, off = carve(off, [HG, 128], F32)
        Lm, off = carve(off, [HG, 128], BF16)
        LTm, off = carve(off, [HG, 128], BF16)
        XT, off = carve(off, [HG, 128], BF16)
        Pp = [None, None]
        PTp = [None, None]
        for i in range(2):
            Pp[i], off = carve(off, [HG, 128], BF16)
            PTp[i], off = carve(off, [HG, 128], BF16)
        kbg, off = carve(off, [HG, 128], BF16)
        vbeta, off = carve(off, [HG, 128], BF16)
        qd_tok, off = carve(off, [HG, 128], BF16)
        DB = []
        for d_ in range(2):
            dd = {}
            for nm in ("attnT", "ktl", "qdTg", "wT_sb", "vnew", "Sg_bf"):
                dd[nm], off = carve(off, [HG, 128], BF16)
            dd["u_sb"], off = carve(off, [HG, 128], F32)
            dd["Sg"], off = carve(off, [HG, 128], F32)
            dd["esc"], off = carve(off, [16], F32)
            dd["bg"], off = carve(off, [HG], F32)
            DB.append(dd)
        osum, off = carve(off, [HG, 128], F32)
        fsig, off = carve(off, [1024], F32)
        fG, off = carve(off, [HG, 128], F32)
        frs, off = carve(off, [8], F32)
        SWEEP_END = off

        gdnw_d = dram("gdn_norm_w", [1, 128])
        gvec_d = dram("gdn_vec", [1, 32])
        cw_d = dram("gdn_conv_wT", [128, 24, 5])
        DMA("sp", gdnw_bc, gdnw_d.partition_broadcast(128), "c_gdnw", [], ["gdnw_bc"])
        DMA("sp", gvec[:, 0:32], gvec_d.partition_broadcast(128), "c_gvec", [], ["gvec"])
        DMA("sp", cwT, cw_d[:, :, :], "c_cw", [], ["cwT"])
        DMA("pool", wdab, w_in_d[:, C_DAB:C_DAB + 32].rearrange("(k p) c -> p k c", p=128), "w_wdab", [], ["wdab"])
        ACTF(gvec[:, 16:32], gvec[:, 16:32], AF.Exp, ["gvec"], ["gvec"])
        TS("dve", gvec[:, 16:32], gvec[:, 16:32], -1.0, None, ALU.mult, ALU.bypass, ["gvec"], ["gvec"])
        for n in range(NT):
            bi = n % 2
            for kt in range(KT):
                MM(bank(bi)[:, 0:32], hT[:, kt, n * 128:(n + 1) * 128], wdab[:, kt, :], kt == 0, kt == KT - 1,
                   ["wdab", ("hT", n)], [bkey(bi)])
            CP("act", dabs[:, n, :], bank(bi)[:, 0:32], [bkey(bi)], ["dabs"])
        a_view = dabs[:, :, 0:16]
        b_view = dabs[:, :, 16:32]
        g_flat = g_raw.rearrange("p n d h -> p n (d h)")
        be_flat = beta.rearrange("p n d h -> p n (d h)")
        TT("dve", g_flat, a_view, gvec[:, 0:16].unsqueeze(1).to_broadcast([128, NT, 16]), ALU.add, ["dabs", "gvec"], ["g_raw"])
        ACTF(g_flat, g_flat, AF.Exp, ["g_raw"], ["g_raw"])
        ACTF(g_flat, g_flat, AF.Ln, ["g_raw"], ["g_raw"], bias=1.0)
        TT("dve", g_flat, g_flat, gvec[:, 16:32].unsqueeze(1).to_broadcast([128, NT, 16]), ALU.mult, ["g_raw", "gvec"], ["g_raw"])
        ACTF(be_flat, b_view, AF.Exp, ["dabs"], ["beta"], scale=-1.0)
        TS("dve", be_flat, be_flat, 1.0, None, ALU.add, ALU.bypass, ["beta"], ["beta"])
        RECIP(be_flat, be_flat, ["beta"], ["beta"])
        MARK("d0")

        GSCALE = 128.0 ** -0.5
        ident_bc4 = ident_b[:].unsqueeze(1).to_broadcast([128, HG, 128])

        def bc_h(ap2):
            return ap2.unsqueeze(2).to_broadcast([128, HG, 128])

        def bc_m(ap2):
            return ap2.unsqueeze(1).to_broadcast([128, HG, 128])

        def v4(ap2):
            return ap2.rearrange("p (h d) -> p h d", h=HG)

        for grp in range(2):
            hs0 = grp * HG
            for which, c_base, dstT in ((0, C_DQ, gqT), (1, C_DK, gkT), (2, C_DV, gvT)):
                ws = wsl[which % 2]
                wk = ("wsl", which % 2)
                DMA("pool", ws, w_in_d[:, c_base + hs0 * 128:c_base + (hs0 + HG) * 128].rearrange("(k p) c -> p k c", p=128),
                    ("w_wsl", which % 2), [], [wk])
                for hh in range(HG):
                    ci = which * 8 + hs0 + hh
                    xi = (which * HG + hh) % 2
                    xcb = xc[xi]
                    xk = ("xc", xi)
                    MEMSET("pool", xcb[:, 0:2], 0.0, [xk])
                    MEMSET("pool", xcb[:, T + 2:T + 4], 0.0, [xk])
                    for k in range(5):
                        TS("dve", diag[:, k, :], cst("ident"), cwT[:, ci, k:k + 1], None, ALU.mult, ALU.bypass,
                           ["consts", "cwT"], ["diag"])
                    for tg in range(4):
                        bi = tg % 2
                        for kt in range(KT):
                            MM(bank(bi), ws[:, kt, hh * 128:(hh + 1) * 128], hT[:, kt, tg * 512:(tg + 1) * 512],
                               kt == 0, kt == KT - 1, [wk] + HT_ALL[tg * 4:tg * 4 + 4], [bkey(bi)])
                        CP("act" if tg % 2 else "dve", xcb[:, 2 + tg * 512:2 + (tg + 1) * 512], bank(bi), [bkey(bi)], [xk])
                    for tg in range(4):
                        bi = 2 + tg % 2
                        i2 = tg % 2
                        for k in range(5):
                            MM(bank(bi), diag[:, k, :], xcb[:, tg * 512 + k:tg * 512 + k + 512], k == 0, k == 4,
                               ["diag", xk], [bkey(bi)])
                        ck = ("ctmp", i2)
                        ACTF(ce[i2], bank(bi), AF.Exp, [bkey(bi)], [ck], scale=-1.0)
                        ACTF(ce[i2], ce[i2], AF.Ln, [ck], [ck], bias=1.0)
                        ACTF(ce[i2], ce[i2], AF.Exp, [ck], [ck], scale=-1.0)
                        dst = dstT[:, hh, tg * 512:(tg + 1) * 512]
                        dk = ("gT", which, hh, tg)
                        if which == 2:
                            TT("dve", dst, ce[i2], bank(bi), ALU.mult, [ck, bkey(bi)], [dk])
                        else:
                            TT("dve", cy[i2], ce[i2], bank(bi), ALU.mult, [ck, bkey(bi)], [("cy", i2)])
                            TT("pool", cysq, cy[i2], cy[i2], ALU.mult, [("cy", i2)], ["cysq"])
                            MM(bank(4), ones_b[:], cysq, True, True, ["ones_b", "cysq"], [bkey(4)])
                            ACTF(crs, bank(4), AF.Ln, [bkey(4)], ["crs"], bias=EPS)
                            ACTF(crs, crs, AF.Exp, ["crs"], ["crs"], scale=-0.5)
                            if which == 0:
                                STT(dst, cy[i2], GSCALE, crs, ALU.mult, ALU.mult, [("cy", i2), "crs"], [dk])
                            else:
                                TT("dve", dst, cy[i2], crs, ALU.mult, [("cy", i2), "crs"], [dk])
            MARK("d1")
            P.barrier()
            DMA("pool", wsl[0], w_in_d[:, C_DZ + hs0 * 128:C_DZ + (hs0 + HG) * 128].rearrange("(k p) c -> p k c", p=128),
                ("w_wsl", 0), [], [("wsl", 0)])
            DMA("pool", wsl[1], w_in_d[:, C_MB + hs0 * 128:C_MB + (hs0 + HG) * 128].rearrange("(k p) c -> p k c", p=128),
                ("w_wsl", 1), [], [("wsl", 1)])

            def gT_keys(which, n):
                return [("gT", which, hh, n // 4) for hh in range(HG)]

            stored = set()

            def gdn_tile(d_, n):
                B = DB[d_]
                dk = lambda nm: (nm, d_)
                Mc = cst("b_le") if d_ == 0 else cst("b_ge")
                Ms = cst("b_gt") if d_ == 0 else cst("b_lt")
                esc, bg = B["esc"], B["bg"]
                tsl = slice(n * 128, (n + 1) * 128)
                gv = g_raw[:, n, d_, hs0:hs0 + HG]
                bv = beta[:, n, d_, hs0:hs0 + HG]
                MM(bank(0)[:, 0:4], Mc, gv, True, True, ["consts", "g_raw"], [bkey(0)])
                MM(bank(0)[:, 4:8], Ms, gv, True, True, ["consts", "g_raw"], [bkey(0)])
                MM(bank(0)[:, 8:12], cst("csel0"), gv, True, True, ["consts", "g_raw"], [bkey(0)])
                MM(bank(0)[:, 12:16], cst("csel1"), gv, True, True, ["consts", "g_raw"], [bkey(0)])
                ACTF(esc, bank(0)[:, 0:16], AF.Exp, [bkey(0)], [dk("esc")])
                TT("dve", bg, bv, esc[:, 0:4], ALU.mult, ["beta", dk("esc")], [dk("bg")])
                for hh in range(HG):
                    TR(bbank(0)[:, hh * 128:(hh + 1) * 128], gkT[:, hh, tsl], ident_b[:], gT_keys(1, n) + ["ident_b"], [("pbb", 0)])
                for hh in range(HG):
                    TR(bbank(1)[:, hh * 128:(hh + 1) * 128], gvT[:, hh, tsl], ident_b[:], gT_keys(2, n) + ["ident_b"], [("pbb", 1)])
                TT("dve", kbg, v4(bbank(0)[:, 0:512]), bc_h(bg), ALU.mult, [("pbb", 0), dk("bg")], ["kbg"])
                TT("dve", B["ktl"], v4(bbank(0)[:, 0:512]), bc_h(esc[:, 4:8]), ALU.mult, [("pbb", 0), dk("esc")], [dk("ktl")])
                TT("dve", vbeta, v4(bbank(1)[:, 0:512]), bc_h(bv), ALU.mult, [("pbb", 1), "beta"], ["vbeta"])
                for hh in range(HG):
                    TR(bbank(0)[:, hh * 128:(hh + 1) * 128], gqT[:, hh, tsl], ident_b[:], gT_keys(0, n) + ["ident_b"], [("pbb", 0)])
                TT("dve", qd_tok, v4(bbank(0)[:, 0:512]), bc_h(esc[:, 0:4]), ALU.mult, [("pbb", 0), dk("esc")], ["qd_tok"])
                for hh in range(HG):
                    TR(bbank(1)[:, hh * 128:(hh + 1) * 128], qd_tok[:, hh, :], ident_b[:], ["qd_tok", "ident_b"], [("pbb", 1)])
                CP("act", B["qdTg"], v4(bbank(1)[:, 0:512]), [("pbb", 1)], [dk("qdTg")])
                TT("pool", GMB, bc_h(gv), bc_m(Ms), ALU.mult, ["g_raw", "consts"], ["GMB"])
                MM(bank(1), Mc, GMB.rearrange("p h s -> p (h s)"), True, True, ["consts", "GMB"], [bkey(1)])
                ACTF(Wd.rearrange("p h s -> p (h s)"), bank(1), AF.Exp, [bkey(1)], ["Wd"])
                TT("pool", GMB, bc_h(bv), bc_m(Ms), ALU.mult, ["beta", "consts"], ["GMB"])
                TT("pool", Wd, Wd, GMB, ALU.mult, ["Wd", "GMB"], ["Wd"])
                TT("pool", GMB, bc_h(gv), bc_m(Mc), ALU.mult, ["g_raw", "consts"], ["GMB"])
                MM(bank(0), Ms, GMB.rearrange("p h s -> p (h s)"), True, True, ["consts", "GMB"], [bkey(0)])
                ACTF(decT.rearrange("p h s -> p (h s)"), bank(0), AF.Exp, [bkey(0)], ["decT"])
                TT("pool", decT, decT, bc_m(Mc), ALU.mult, ["decT", "consts"], ["decT"])
                for hh in range(HG):
                    MM(bank(1)[:, hh * 128:(hh + 1) * 128], gkT[:, hh, tsl], gkT[:, hh, tsl], True, True,
                       gT_keys(1, n), [bkey(1)])
                TT("dve", Lm, v4(bank(1)), Wd, ALU.mult, [bkey(1), "Wd"], ["Lm"])
                for hh in range(HG):
                    MM(bank(0)[:, hh * 128:(hh + 1) * 128], gkT[:, hh, tsl], gqT[:, hh, tsl], True, True,
                       gT_keys(1, n) + gT_keys(0, n), [bkey(0)])
                TT("dve", B["attnT"], v4(bank(0)), decT, ALU.mult, [bkey(0), "decT"], [dk("attnT")])
                for hh in range(HG):
                    TR(bbank(0)[:, hh * 128:(hh + 1) * 128], Lm[:, hh, :], ident_b[:], ["Lm", "ident_b"], [("pbb", 0)])
                CP("act", LTm, v4(bbank(0)[:, 0:512]), [("pbb", 0)], ["LTm"])
                TT("dve", XT, ident_bc4, v4(bbank(0)[:, 0:512]), ALU.subtract, ["ident_b", ("pbb", 0)], ["XT"])
                Pc, PTc = Lm, LTm
                pck, ptk_ = "Lm", "LTm"
                for it in range(5):
                    Pn, PTn = Pp[it % 2], PTp[it % 2]
                    pnk, ptnk = ("Pp", it % 2), ("PTp", it % 2)
                    for hh in range(HG):
                        MM(bank(1)[:, hh * 128:(hh + 1) * 128], PTc[:, hh, :], Pc[:, hh, :], True, True, [pck, ptk_], [bkey(1)])
                    CP("act", Pn, v4(bank(1)), [bkey(1)], [pnk])
                    if it < 4:
                        for hh in range(HG):
                            MM(bank(0)[:, hh * 128:(hh + 1) * 128], Pc[:, hh, :], PTc[:, hh, :], True, True, [pck, ptk_], [bkey(0)])
                        CP("dve", PTn, v4(bank(0)), [bkey(0)], [ptnk])
                    for hh in range(HG):
                        MM(bank(1)[:, hh * 128:(hh + 1) * 128], Pn[:, hh, :], XT[:, hh, :], True, True, [pnk, "XT"], [bkey(1)])
                    TT("dve", XT, XT, v4(bank(1)), ALU.add, ["XT", bkey(1)], ["XT"])
                    Pc, PTc, pck, ptk_ = Pn, PTn, pnk, ptnk
                for hh in range(HG):
                    MM(bank(0)[:, hh * 128:(hh + 1) * 128], XT[:, hh, :], vbeta[:, hh, :], True, True, ["XT", "vbeta"], [bkey(0)])
                CP("act", B["u_sb"], v4(bank(0)), [bkey(0)], [dk("u_sb")])
                for hh in range(HG):
                    MM(bank(1)[:, hh * 128:(hh + 1) * 128], kbg[:, hh, :], XT[:, hh, :], True, True, ["kbg", "XT"], [bkey(1)])
                CP("dve", B["wT_sb"], v4(bank(1)), [bkey(1)], [dk("wT_sb")])
                sb0 = 4 if d_ == 0 else 2
                Sg, Sg_bf, vnew = B["Sg"], B["Sg_bf"], B["vnew"]
                chunks = (0, 1) if d_ == 0 else (1, 0)
                for c in chunks:
                    sl = slice(c * 64, c * 64 + 64)
                    for hh in range(HG):
                        MM(bank(sb0)[sl, hh * 128:(hh + 1) * 128], B["wT_sb"][:, hh, sl], Sg_bf[:, hh, :], True, True,
                           [dk("wT_sb"), dk("Sg_bf")], [bkey(sb0)])
                    TT("dve", vnew[sl], B["u_sb"][sl], v4(bank(sb0))[sl], ALU.subtract, [dk("u_sb"), bkey(sb0)], [dk("vnew")])
                    for hh in range(HG):
                        MM(bank(sb0 + 1)[sl, hh * 128:(hh + 1) * 128], B["qdTg"][:, hh, sl], Sg_bf[:, hh, :], True, False,
                           [dk("qdTg"), dk("Sg_bf")], [bkey(sb0 + 1)])
                        MM(bank(sb0 + 1)[sl, hh * 128:(hh + 1) * 128], B["attnT"][sl, hh, sl], vnew[sl, hh, :], False, True,
                           [dk("attnT"), dk("vnew")], [bkey(sb0 + 1)])
                    for hh in range(HG):
                        MM(bank(sb0)[:, hh * 128:(hh + 1) * 128], B["ktl"][sl, hh, :], vnew[sl, hh, :], True, True,
                           [dk("ktl"), dk("vnew")], [bkey(sb0)])
                    TT("pool", Sg, Sg, bc_h(esc[:, 8 + 4 * c:12 + 4 * c]), ALU.mult, [dk("Sg"), dk("esc")], [dk("Sg")])
                    TT("dve", Sg, Sg, v4(bank(sb0)), ALU.add, [dk("Sg"), bkey(sb0)], [dk("Sg")])
                    CP("act", Sg_bf, Sg, [dk("Sg")], [dk("Sg_bf")])
                    if n not in stored:
                        CP("act", o_store[sl, n], v4(bank(sb0 + 1))[sl], [bkey(sb0 + 1)], [("o_store", n)])
                    else:
                        TT("dve", osum[sl], v4(bank(sb0 + 1))[sl], o_store[sl, n], ALU.add, [bkey(sb0 + 1), ("o_store", n)], ["osum"])
                if n not in stored:
                    stored.add(n)
                    return
                osq = fsig[:, 0:512].rearrange("p (h d) -> p h d", h=HG)
                TT("pool", osq, osum, osum, ALU.mult, ["osum"], ["fsig"])
                P.op("dve", lambda e: e.tensor_reduce(out=frs[:, 0:HG], in_=osq, axis=AX.X, op=ALU.add), ["fsig"], ["frs"], cost=600.0)
                rstd_inplace(frs[:, 0:HG], 128, "frs")
                for half, ws in enumerate(wsl):
                    for kt in range(KT):
                        MM(bank(half), hT[:, kt, tsl], ws[:, kt, :], kt == 0, kt == KT - 1,
                           [("wsl", half), ("hT", n)], [bkey(half)])
                zm = pt[0][:, :]
                ACTF(fsig, zm, AF.Exp, [bkey(0), bkey(1)], ["fsig"], scale=-1.0)
                ACTF(fsig, fsig, AF.Ln, ["fsig"], ["fsig"], bias=1.0)
                ACTF(fsig, fsig, AF.Exp, ["fsig"], ["fsig"], scale=-1.0)
                TT("pool", fG.rearrange("p h d -> p (h d)"), fsig[:, 0:512], fsig[:, 512:1024], ALU.mult, ["fsig"], ["fG"])
                TT("dve", fG, fG, v4(bank(0)), ALU.mult, ["fG", bkey(0)], ["fG"])
                TT("pool", fG, fG, bc_m(gdnw_bc), ALU.mult, ["fG", "gdnw_bc"], ["fG"])
                TT("pool", osum, osum, bc_h(frs[:, 0:HG]), ALU.mult, ["osum", "frs"], ["osum"])
                TT("pool", osum, osum, fG, ALU.mult, ["osum", "fG"], ["osum"])
                mslice = mixed[:, n, hs0 * 128:(hs0 + HG) * 128].rearrange("p (h d) -> p h d", h=HG)
                TT("dve", mslice, mslice, osum, ALU.add, ["osum", ("mixed", n)], [("mixed", n)])

            for d_ in range(2):
                MEMSET("dve", DB[d_]["Sg"], 0.0, [("Sg", d_)])
                CP("act", DB[d_]["Sg_bf"], DB[d_]["Sg"], [("Sg", d_)], [("Sg_bf", d_)])
            for i in range(NT):
                gdn_tile(0, i)
                gdn_tile(1, NT - 1 - i)
            MARK("d2")
            P.barrier()

        P.barrier()
        wout_d = dram("w_out", [D, D])
        off = 0
        x1, off = carve(off, [NT, D], F32)
        X1_END = off
        mT, off = carve(off, [KT, T], BF16)
        wout, off = carve(off, [KT, D], BF16)
        hn2 = [None, None]
        hn2[0], off = carve(off, [D], BF16)
        hn2[1], off = carve(off, [D], BF16)
        junk, off = carve(off, [D], BF16)
        n2_bc, off = carve(off, [D], F32)
        DMA("sp", n2_bc, n2_d.partition_broadcast(128), "c_n2", [], ["n2_bc"])
        DMA("pool", wout, wout_d.rearrange("(k p) c -> p k c", p=128), "w_wout", [], ["wout"])
        for n in range(NT):
            b = n % 2
            for kt in range(KT):
                TR(bbank(b)[:, kt * 128:(kt + 1) * 128], mixed[:, n, kt * 128:(kt + 1) * 128], ident_b[:],
                   [("mixed", n), "ident_b"], [("pbb", b)])
            CP("act", mT[:, :, n * 128:(n + 1) * 128], bbank(b).rearrange("p (k t) -> p k t", k=KT), [("pbb", b)], [("mT", n)])
        for n in range(NT):
            tsl = slice(n * 128, (n + 1) * 128)
            DMA("sp", x1[:, n, :], x_d[tsl, :], ("x1ld", n % 4), [], [("x1", n)])
            pp = pt[n % 2]
            for half in range(2):
                for kt in range(KT):
                    MM(pp[:, half * 512:(half + 1) * 512], mT[:, kt, tsl], wout[:, kt, half * 512:(half + 1) * 512],
                       kt == 0, kt == KT - 1, [("mT", n), "wout"], [bkey((n % 2) * 2 + half)])
            TT("dve", x1[:, n, :], x1[:, n, :], pp[:, :], ALU.add, [("x1", n), bkey((n % 2) * 2), bkey((n % 2) * 2 + 1)], [("x1", n)])
            b = n % 2
            ssap = small[:, 8 + b:9 + b]
            ssk = ("ss2", b)
            ACTF(junk, x1[:, n, :], AF.Square, [("x1", n)], ["junk", ssk], accum_out=ssap)
            rstd_inplace(ssap, D, ssk)
            STT(mixed[:, n, :], x1[:, n, :], ssap, n2_bc, ALU.mult, ALU.mult, [("x1", n), ssk, "n2_bc"], [("mixed", n)])
            for kt in range(KT):
                TR(bbank(b)[:, kt * 128:(kt + 1) * 128], mixed[:, n, kt * 128:(kt + 1) * 128], ident_b[:],
                   [("mixed", n), "ident_b"], [("pbb", b)])
            CP("act", hT[:, :, tsl], bbank(b).rearrange("p (k t) -> p k t", k=KT), [("pbb", b)], [("hT", n)])
        MARK("e0")

        P.barrier()
        NB = 64
        wr_d = dram("moe_wr", [D, 36])
        wgu0_d = dram("moe_wgu0", [4096, 2048])
        wgu1_d = dram("moe_wgu1", [4096, 2048])
        wdr_d = dram("moe_wdr", [4096, 2048])
        xb_d = nc.dram_tensor("moe_xb", [NB * 128, D], BF16, kind="Internal").ap()
        yb_d = nc.dram_tensor("moe_yb", [NB * 128, D], F32, kind="Internal").ap()
        off = X1_END
        stg = []
        for i in range(3):
            a, off = carve(off, [2048], F32)
            stg.append(a)
        wgu_bf = []
        wd_bf = []
        for i in range(2):
            a, off = carve(off, [KT, 512], BF16)
            wgu_bf.append(a)
            a, off = carve(off, [2, D], BF16)
            wd_bf.append(a)
        wr, off = carve(off, [KT, 36], BF16)
        lg, off = carve(off, [NT, 36], F32)
        oh1, off = carve(off, [NT, 32], F32)
        oh2, off = carve(off, [NT, 32], F32)
        msk, off = carve(off, [NT, 32], F32)
        rank, off = carve(off, [NT, 32], F32)
        tmp3, off = carve(off, [NT, 32], F32)
        gtmp, off = carve(off, [NT, 4], F32)
        ohg, off = carve(off, [NT, 4], F32)
        rv, off = carve(off, [8, NT], F32)
        mcum, off = carve(off, [32], F32)
        cnt, off = carve(off, [32], F32)
        padded, off = carve(off, [32], F32)
        ends, off = carve(off, [32], F32)
        pstart, off = carve(off, [32], F32)
        ebf, off = carve(off, [NB], F32)
        widx_f, off = carve(off, [NB], F32)
        widx, off = carve(off, [NB], I32)
        dest_f, off = carve(off, [2, NT], F32)
        dest_i, off = carve(off, [2, NT], I32)
        MOE_END = off
        cmpb = stg[0].rearrange("p (b e) -> p b e", b=NB)
        cmpj = stg[1][:, 0:512].rearrange("p (e j) -> p e j", e=32)
        ht32 = hT[:].rearrange("p k t -> p (k t)").bitcast(F32)
        HTCAP = 32 * 1024
        hoff = 0
        xg, xgT, sil, hid_bf, hidT, ysb = [], [], [], [], [], []
        for i in range(2):
            a, hoff = carve(hoff, [D], BF16, ht32, HTCAP); xg.append(a)
            a, hoff = carve(hoff, [KT, 128], BF16, ht32, HTCAP); xgT.append(a)
            a, hoff = carve(hoff, [256], F32, ht32, HTCAP); sil.append(a)
            a, hoff = carve(hoff, [256], BF16, ht32, HTCAP); hid_bf.append(a)
            a, hoff = carve(hoff, [2, 128], BF16, ht32, HTCAP); hidT.append(a)
            a, hoff = carve(hoff, [D], F32, ht32, HTCAP); ysb.append(a)
        mix32 = mixed[:].rearrange("p n d -> p (n d)").bitcast(F32)
        MIXCAP = 32 * 1024
        moff = 0
        yg = []
        for i in range(2):
            a, moff = carve(moff, [D], F32, mix32, MIXCAP); yg.append(a)

        DMA("pool", wr, wr_d.rearrange("(k p) c -> p k c", p=128), "w_wr", [], ["wr"])
        for n in range(NT):
            bi = n % 2
            for kt in range(KT):
                MM(bank(bi)[:, 0:36], hT[:, kt, n * 128:(n + 1) * 128], wr[:, kt, :], kt == 0, kt == KT - 1,
                   ["wr", ("hT", n)], [bkey(bi)])
            CP("act", lg[:, n, :], bank(bi)[:, 0:36], [bkey(bi)], ["lg"])
        BIG = 10000.0
        glv = lg[:, :, 0:4]
        elv = lg[:, :, 4:36]

        def RED(out, in_, op, R, W):
            P.op("dve", lambda e: e.tensor_reduce(out=out, in_=in_, axis=AX.X, op=op), R, W, cost=100.0 + _fsz(in_) * 1.0)

        def bcn(ap2, k):
            return ap2.unsqueeze(2).to_broadcast([128, NT, k])

        gmax, gsum, m1, m2, w1, w2 = (rv[:, i, :] for i in range(6))
        RED(gmax, glv, ALU.max, ["lg"], ["rv"])
        TT("dve", ohg, glv, bcn(gmax, 4), ALU.is_equal, ["lg", "rv"], ["ohg"])
        TT("dve", gtmp, glv, bcn(gmax, 4), ALU.subtract, ["lg", "rv"], ["gtmp"])
        ACTF(gtmp, gtmp, AF.Exp, ["gtmp"], ["gtmp"])
        RED(gsum, gtmp, ALU.add, ["gtmp"], ["rv"])
        RECIP(gsum, gsum, ["rv"], ["rv"])
        TS("dve", ohg, ohg, BIG, -BIG, ALU.mult, ALU.add, ["ohg"], ["ohg"])
        TT("dve", msk.rearrange("p n (g e) -> p n g e", g=4), elv.rearrange("p n (g e) -> p n g e", g=4),
           ohg.unsqueeze(3).to_broadcast([128, NT, 4, 8]), ALU.add, ["lg", "ohg"], ["msk"])
        RED(m1, msk, ALU.max, ["msk"], ["rv"])
        TT("dve", oh1, msk, bcn(m1, 32), ALU.is_equal, ["msk", "rv"], ["oh1"])
        STT(msk, oh1, -BIG, msk, ALU.mult, ALU.add, ["oh1", "msk"], ["msk"])
        RED(m2, msk, ALU.max, ["msk"], ["rv"])
        TT("dve", oh2, msk, bcn(m2, 32), ALU.is_equal, ["msk", "rv"], ["oh2"])
        TT("dve", w2, m2, m1, ALU.subtract, ["rv"], ["rv"])
        ACTF(w2, w2, AF.Exp, ["rv"], ["rv"])
        TS("dve", w1, w2, 1.0, None, ALU.add, ALU.bypass, ["rv"], ["rv"])
        RECIP(w1, w1, ["rv"], ["rv"])
        TT("dve", w1, w1, gsum, ALU.mult, ["rv"], ["rv"])
        TT("dve", w2, w2, w1, ALU.mult, ["rv"], ["rv"])
        TT("dve", msk, oh1, oh2, ALU.add, ["oh1", "oh2", "msk"], ["msk"])
        MEMSET("dve", mcum, 0.0, ["mcum"])
        for n in range(NT):
            bi = n % 2
            MM(bank(bi)[:, 0:32], cst("m_lt"), msk[:, n, :], True, False, ["consts", "msk"], [bkey(bi)])
            MM(bank(bi)[:, 0:32], cst("ones"), mcum, False, True, ["consts", "mcum"], [bkey(bi)])
            CP("act", rank[:, n, :], bank(bi)[:, 0:32], [bkey(bi)], ["rank"])
            TT("dve", mcum, mcum, msk[:, n, :], ALU.add, ["mcum", "msk"], ["mcum"])
        MM(bank(0)[:, 0:32], cst("ones"), mcum, True, True, ["consts", "mcum"], [bkey(0)])
        CP("act", cnt, bank(0)[:, 0:32], [bkey(0)], ["cnt"])
        TT("dve", cmpj, cnt.unsqueeze(2).to_broadcast([128, 32, 16]),
           cst("bvals")[:, 0:16].unsqueeze(1).to_broadcast([128, 32, 16]), ALU.is_gt, ["cnt", "consts"], [("stg", 1)])
        RED(padded, cmpj, ALU.add, [("stg", 1)], ["padded"])
        TS("dve", padded, padded, 128.0, None, ALU.mult, ALU.bypass, ["padded"], ["padded"])
        P.op("dve", lambda e: e.tensor_tensor_scan(out=ends, data0=cst("ones")[:, 0:32], data1=padded, initial=0.0,
                                                  op0=ALU.mult, op1=ALU.add), ["consts", "padded"], ["ends"], cost=300.0)
        TT("dve", pstart, ends, padded, ALU.subtract, ["ends", "padded"], ["pstart"])
        TT("dve", rank, rank, pstart.unsqueeze(1).to_broadcast([128, NT, 32]), ALU.add, ["rank", "pstart"], ["rank"])
        for k, ohk in ((0, oh1), (1, oh2)):
            TT("dve", tmp3, ohk, rank, ALU.mult, ["oh1", "oh2", "rank"], ["tmp3"])
            RED(dest_f[:, k, :], tmp3, ALU.add, ["tmp3"], ["dest_f"])
        CP("dve", dest_i, dest_f, ["dest_f"], ["dest_i"])
        TT("dve", cmpb, ends.unsqueeze(1).to_broadcast([128, NB, 32]),
           cst("bvals")[:, 0:NB].unsqueeze(2).to_broadcast([128, NB, 32]), ALU.is_le, ["ends", "consts"], [("stg", 0)])
        RED(ebf, cmpb, ALU.add, [("stg", 0)], ["ebf"])
        TS("dve", ebf, ebf, 31.0, None, ALU.min, ALU.bypass, ["ebf"], ["ebf"])
        STT(widx_f, ebf, 128.0, cst("pidx")[:, 0:NB], ALU.mult, ALU.add, ["ebf", "consts"], ["widx_f"])
        CP("dve", widx, widx_f, ["widx_f"], ["widx"])
        MARK("e1")

        IOA = bass.IndirectOffsetOnAxis
        XB_KEYS = []
        zt, off = carve(off, [D], BF16)
        MEMSET("pool", zt, 0.0, ["zt"])
        DMA("sp", xb_d.rearrange("(p r) d -> p r d", p=128), zt.unsqueeze(1).to_broadcast([128, NB, D]), "xbz", ["zt"], ["xb0"])
        for n in range(NT):
            for k in range(2):
                idx_ap = dest_i[:, k, n:n + 1]
                src_ap = mixed[:, n, :]
                P.dma("pool", lambda e, idx_ap=idx_ap, src_ap=src_ap: e.indirect_dma_start(
                    out=xb_d[:, :], out_offset=IOA(ap=idx_ap, axis=0), in_=src_ap, in_offset=None),
                    ("sc", (2 * n + k) % 4), [("mixed", n), "dest_i", "xb0"], [("xb", n, k)], nbytes=256 * 1024)
                XB_KEYS.append(("xb", n, k))

        def gather_w(dst, src_d, b, skey):
            idx_ap = widx[:, b:b + 1]
            P.dma("pool", lambda e: e.indirect_dma_start(
                out=dst, out_offset=None, in_=src_d[:, :], in_offset=IOA(ap=idx_ap, axis=0)),
                skey, ["widx"], [skey], nbytes=1 << 20)

        YB_KEYS = []
        for b in range(NB):
            s = b % 2
            gather_w(stg[0], wgu0_d, b, ("stg", 0))
            gather_w(stg[1], wgu1_d, b, ("stg", 1))
            gather_w(stg[2], wdr_d, b, ("stg", 2))
            CP("act", wgu_bf[s][:, 0:4, :], stg[0].rearrange("p (k c) -> p k c", k=4), [("stg", 0)], [("wgu_bf", s, 0)])
            CP("dve", wgu_bf[s][:, 4:8, :], stg[1].rearrange("p (k c) -> p k c", k=4), [("stg", 1)], [("wgu_bf", s, 1)])
            CP("pool", wd_bf[s], stg[2].rearrange("p (k c) -> p k c", k=2), [("stg", 2)], [("wd_bf", s)])
            DMA("sp", xg[s], xb_d[b * 128:(b + 1) * 128, :], ("xg", s), XB_KEYS, [("xg", s)])
            for kt in range(KT):
                TR(bbank(s)[:, kt * 128:(kt + 1) * 128], xg[s][:, kt * 128:(kt + 1) * 128], ident_b[:],
                   [("xg", s), "ident_b"], [("pbb", s)])
            CP("act", xgT[s], bbank(s).rearrange("p (k t) -> p k t", k=KT), [("pbb", s)], [("xgT", s)])
            hb = bank(s)
            for kt in range(KT):
                MM(hb, xgT[s][:, kt, :], wgu_bf[s][:, kt, :], kt == 0, kt == KT - 1,
                   [("xgT", s), ("wgu_bf", s, 0), ("wgu_bf", s, 1)], [bkey(s)])
            ACTF(sil[s], hb[:, 0:256], AF.Silu, [bkey(s)], [("sil", s)])
            TT("dve", hid_bf[s], sil[s], hb[:, 256:512], ALU.mult, [("sil", s), bkey(s)], [("hid_bf", s)])
            for ft in range(2):
                TR(bbank(s)[:, ft * 128:(ft + 1) * 128], hid_bf[s][:, ft * 128:(ft + 1) * 128], ident_b[:],
                   [("hid_bf", s), "ident_b"], [("pbb", s)])
            CP("act", hidT[s], bbank(s)[:, 0:256].rearrange("p (k t) -> p k t", k=2), [("pbb", s)], [("hidT", s)])
            yp = pt[1 + s]
            for half in range(2):
                for ft in range(2):
                    MM(yp[:, half * 512:(half + 1) * 512], hidT[s][:, ft, :], wd_bf[s][:, ft, half * 512:(half + 1) * 512],
                       ft == 0, ft == 1, [("hidT", s), ("wd_bf", s)], [bkey(2 + 2 * s + half)])
            CP("act" if b % 2 else "dve", ysb[s], yp[:, :], [bkey(2 + 2 * s), bkey(3 + 2 * s)], [("ysb", s)])
            DMA("sp", yb_d[b * 128:(b + 1) * 128, :], ysb[s], ("yst", s), [("ysb", s)], [("yb", b)])
            YB_KEYS.append(("yb", b))
        MARK("e2")
        for n in range(NT):
            for k in range(2):
                s = (2 * n + k) % 2
                idx_ap = dest_i[:, k, n:n + 1]
                dst = yg[s]
                P.dma("pool", lambda e, idx_ap=idx_ap, dst=dst: e.indirect_dma_start(
                    out=dst, out_offset=None, in_=yb_d[:, :], in_offset=IOA(ap=idx_ap, axis=0)),
                    ("yg", s), YB_KEYS + ["dest_i"], [("yg", s)], nbytes=512 * 1024)
                wk = rv[:, 4 + k, n:n + 1]
                STT(x1[:, n, :], yg[s], wk, x1[:, n, :], ALU.mult, ALU.add, [("yg", s), "rv", ("x1", n)], [("x1", n)])

        P.barrier()
        off = X1_END
        nf_bc, off = carve(off, [D], F32)
        ob = [None, None]
        ob[0], off = carve(off, [D], F32)
        ob[1], off = carve(off, [D], F32)
        junk2, off = carve(off, [D], BF16)
        DMA("sp", nf_bc, nf_d.partition_broadcast(128), "c_nf", [], ["nf_bc"])
        for n in range(NT):
            b = n % 2
            ssap = small[:, 12 + b:13 + b]
            ssk = ("ss3", b)
            ACTF(junk2, x1[:, n, :], AF.Square, [("x1", n)], ["junk2", ssk], accum_out=ssap)
            rstd_inplace(ssap, D, ssk)
            STT(ob[b], x1[:, n, :], ssap, nf_bc, ALU.mult, ALU.mult, [("x1", n), ssk, "nf_bc"], [("ob", b)])
            DMA("sp", out_d[n * 128:(n + 1) * 128, :], ob[b], ("out_st", b), [("ob", b)], [("out", n)])
        if not dbg:
            P.wait_all("sp", [("out", n) for n in range(NT)])
        if dbg:
            P.enabled = True
            P.barrier()
            for n in range(NT):
                DMA("sp", dbg_d[n * 128:(n + 1) * 128, :], x1[:, n, :], ("dbg_out", n % 2), [("x1", n)], [("dbg", n)])
            P.wait_all("sp", [("dbg", n) for n in range(NT)] + [("out", n) for n in range(NT)])
        P.emit()
    return nc


def make_in_maps(inputs, n_cores=8):
    f = lambda k: np.asarray(inputs[k], np.float32)
    x = f("x")
    _gu = np.concatenate([f("moe_w_gate")[0], f("moe_w_up")[0]], axis=2).reshape(32, 8, 128, 512).transpose(0, 2, 1, 3)
    shared = {
        "norm1_w": f("norm1_w").reshape(1, D),
        "norm2_w": f("norm2_w").reshape(1, D),
        "norm_f_w": f("norm_f_w").reshape(1, D),
        "consts": CONST_ARR,
        "w_in": np.ascontiguousarray(f("w_in")[0]),
        "gla_w2b_f": np.ascontiguousarray(np.concatenate([f("gla_gate_w2_fwd")[0], f("gla_gate_b_fwd")], axis=0)),
        "gla_w2b_b": np.ascontiguousarray(np.concatenate([f("gla_gate_w2_bwd")[0], f("gla_gate_b_bwd")], axis=0)),
        "gla_norm_w": f("gla_norm_w").reshape(1, 256),
        "w_out": np.ascontiguousarray(f("w_out")[0]),
        "moe_wr": np.ascontiguousarray(np.concatenate([f("moe_w_group")[0], f("moe_w_router")[0]], axis=1)),
        "moe_wgu0": _gu[:, :, 0:4, :].reshape(4096, 2048).copy(),
        "moe_wgu1": _gu[:, :, 4:8, :].reshape(4096, 2048).copy(),
        "moe_wdr": np.ascontiguousarray(f("moe_w_down")[0].reshape(32, 2, 128, 1024).transpose(0, 2, 1, 3)).reshape(4096, 2048),
        "gdn_norm_w": f("gdn_norm_w").reshape(1, 128),
        "gdn_vec": np.ascontiguousarray(np.concatenate([f("gdn_dt_bias_fwd")[0], f("gdn_dt_bias_bwd")[0],
                                                        f("gdn_a_log_fwd")[0], f("gdn_a_log_bwd")[0]]).reshape(1, 32)),
        "gdn_conv_wT": np.ascontiguousarray(f("gdn_conv_w")[0].T.reshape(24, 128, 5).transpose(1, 0, 2)),
    }
    maps = []
    for c in range(n_cores):
        m = dict(shared)
        m["x"] = np.ascontiguousarray(x[c])
        maps.append(m)
    return maps


def kernel(**inputs):
    nc = build()
    in_maps = make_in_maps(inputs)
    res = run_bass_kernel_spmd(nc, in_maps, core_ids=list(range(8)))
    out = np.stack([np.asarray(r["out"]) for r in res.results], axis=0)
    return out.astype(np.float32)
```

```python
import contextlib
import heapq
import numpy as np
import concourse.bass as bass
import concourse.mybir as mybir
from concourse.bass_utils import run_bass_kernel_spmd

F32 = mybir.dt.float32
BF16 = mybir.dt.bfloat16
I32 = mybir.dt.int32
AF = mybir.ActivationFunctionType
ALU = mybir.AluOpType
AX = mybir.AxisListType

T = 2048
D = 1024
NT = T // 128
KT = D // 128
EPS = 1e-6
SAME_ENGINE_SYNC = True
EPOCH = 20000
SYNC_NS = 120.0
DMA_LAT_NS = 2200.0


class Prog:
    ENGS = ("pe", "act", "dve", "pool", "sp")

    def __init__(self, nc, stack):
        self.nc = nc
        self.stack = stack
        self.streams = {e: [] for e in self.ENGS}
        self.count = {e: 0 for e in self.ENGS}
        self.esems = {e: [] for e in self.ENGS}
        self.known = {e: {} for e in self.ENGS}
        self.last_write = {}
        self.readers = {}
        self.dma_sems = {}
        self.dma_vals = {}
        self.dma_last = {}
        self.enabled = True
        self.seg = []
        self.ticks = {}
        self.nops = 0
        self.seg_base = 0
        self.pool_init = None

    def _new_sem(self, name):
        return self.stack.enter_context(self.nc.semaphore(name))

    @staticmethod
    def _psum_fix(reads, writes):
        r2, w2 = [], list(writes)
        for k in reads:
            if isinstance(k, tuple) and k[0] in ("pb", "pbb"):
                if k not in w2:
                    w2.append(k)
            else:
                r2.append(k)
        return r2, w2

    def _record(self, eng, fn, reads, writes, cost, kind, semkey=None):
        reads, writes = self._psum_fix(list(reads), list(writes))
        oid = self.nops
        self.nops += 1
        preds = set()
        for r in reads:
            t = self.last_write.get(r)
            if t is not None:
                preds.add(t)
        for w in writes:
            t = self.last_write.get(w)
            if t is not None:
                preds.add(t)
            preds.update(self.readers.get(w, ()))
        if kind == "dma":
            prev = self.dma_last.get(semkey)
            if prev is not None:
                preds.add(prev)
            self.dma_last[semkey] = oid
        preds = {p for p in preds if p >= self.seg_base}
        self.seg.append(dict(id=oid, eng=eng, fn=fn, preds=preds, cost=float(cost), kind=kind, semkey=semkey))
        for w in writes:
            self.last_write[w] = oid
            self.readers[w] = []
        for r in reads:
            self.readers.setdefault(r, []).append(oid)
        return oid

    def op(self, eng, fn, reads=(), writes=(), cost=300.0):
        if not self.enabled:
            return
        self._record(eng, fn, reads, writes, cost, "op")

    def dma(self, eng, fn, semkey, reads=(), writes=(), nbytes=1 << 20):
        if not self.enabled:
            return
        self._record(eng, fn, reads, writes, DMA_LAT_NS + nbytes / 160.0, "dma", semkey)

    def wait_all(self, eng, keys):
        self._record(eng, None, list(keys), [], 0.0, "op")

    def _schedule_segment(self):
        ops = self.seg
        if not ops:
            return
        byid = {o["id"]: o for o in ops}
        succ = {o["id"]: [] for o in ops}
        indeg = {}
        for o in ops:
            indeg[o["id"]] = len(o["preds"])
            for p in o["preds"]:
                succ[p].append(o["id"])
        ready_t = {o["id"]: 0.0 for o in ops}
        finish = {}
        heaps = {e: [] for e in self.ENGS}
        for o in ops:
            if indeg[o["id"]] == 0:
                heapq.heappush(heaps[o["eng"]], (0.0, o["id"]))
        etime = {e: 0.0 for e in self.ENGS}
        order = {e: [] for e in self.ENGS}
        remaining = len(ops)
        while remaining:
            best = None
            for e in self.ENGS:
                h = heaps[e]
                if not h:
                    continue
                rt, oid = h[0]
                st = max(rt, etime[e])
                if best is None or (st, oid) < (best[0], best[1]):
                    best = (st, oid, e)
            st, oid, e = best
            heapq.heappop(heaps[e])
            o = byid[oid]
            if o["kind"] == "dma":
                etime[e] = st + 150.0
                fin = st + o["cost"]
            else:
                etime[e] = st + o["cost"]
                fin = etime[e]
            finish[oid] = fin
            order[e].append(o)
            remaining -= 1
            for s in succ[oid]:
                so = byid[s]
                lat = SYNC_NS if (so["eng"] != e or o["kind"] == "dma") else (60.0 if e != "pe" else 0.0)
                ready_t[s] = max(ready_t[s], fin + lat)
                indeg[s] -= 1
                if indeg[s] == 0:
                    heapq.heappush(heaps[so["eng"]], (ready_t[s], s))
        self.est_ns = getattr(self, "est_ns", 0.0) + max(list(finish.values()) + [0.0])
        for o in ops:
            if o["kind"] == "dma":
                k = o["semkey"]
                if k not in self.dma_sems:
                    self.dma_sems[k] = self._new_sem(f"d{len(self.dma_sems)}")
                    self.dma_vals[k] = 0
                self.dma_vals[k] += 16
                self.ticks[o["id"]] = (self.dma_sems[k], self.dma_vals[k], "dma")
        def needs_sem(o):
            for s_ in succ[o["id"]]:
                se = byid[s_]["eng"]
                if se != o["eng"] or (SAME_ENGINE_SYNC and se != "pe"):
                    return True
            return False
        for e in self.ENGS:
            real = [o for o in order[e] if o["kind"] == "op" and o["fn"] is not None]
            for i_, o in enumerate(real):
                o["sig"] = needs_sem(o) or i_ == len(real) - 1
        for e in self.ENGS:
            for o in order[e]:
                if o["kind"] == "op" and o["fn"] is not None and o["sig"]:
                    c = self.count[e]
                    ep, v = divmod(c, EPOCH)
                    while len(self.esems[e]) <= ep:
                        self.esems[e].append(self._new_sem(f"s_{e}_{len(self.esems[e])}"))
                    self.count[e] = c + 1
                    self.ticks[o["id"]] = (self.esems[e][ep], v + 1, e)
        for e in self.ENGS:
            for o in order[e]:
                waits = {}
                for p in o["preds"]:
                    if byid[p]["eng"] == e and byid[p]["kind"] == "op" and (not SAME_ENGINE_SYNC or e == "pe"):
                        continue
                    sem, val, src = self.ticks[p]
                    sid = id(sem)
                    if self.known[e].get(sid, 0) >= val:
                        continue
                    if sid not in waits or waits[sid][1] < val:
                        waits[sid] = (sem, val)
                for sid, (sem, val) in waits.items():
                    self.known[e][sid] = val
                inc = None
                if o["fn"] is not None and o["id"] in self.ticks:
                    sem, val, src = self.ticks[o["id"]]
                    inc = (sem, 16 if o["kind"] == "dma" else 1)
                self.streams[e].append((o["fn"], list(waits.values()), inc))
        self.seg = []
        self.seg_base = self.nops

    def barrier(self):
        if not self.enabled and not self.seg:
            return
        self._schedule_segment()
        ticks = []
        for e2 in self.ENGS:
            c = self.count[e2]
            if c > 0:
                ep, v = divmod(c - 1, EPOCH)
                ticks.append((self.esems[e2][ep], v + 1))
        for k, sem in self.dma_sems.items():
            ticks.append((sem, self.dma_vals[k]))
        for eng in self.ENGS:
            waits = []
            for (sem, val) in ticks:
                if self.known[eng].get(id(sem), 0) >= val:
                    continue
                self.known[eng][id(sem)] = val
                waits.append((sem, val))
            if waits:
                self.streams[eng].append((None, waits, None))

    def emit(self):
        self._schedule_segment()
        nc = self.nc
        with nc.Block() as block:
            def run(e, stream):
                for fn, waits, inc in stream:
                    for sem, val in waits:
                        e.wait_ge(sem, val)
                    if fn is None:
                        continue
                    ins = fn(e)
                    if inc is not None:
                        ins.then_inc(inc[0], inc[1])

            @block.tensor
            def _(e):
                run(e, self.streams["pe"])

            @block.scalar
            def _(e):
                run(e, self.streams["act"])

            @block.vector
            def _(e):
                run(e, self.streams["dve"])

            @block.gpsimd
            def _(e):
                if self.pool_init is not None:
                    self.pool_init(e)
                run(e, self.streams["pool"])

            @block.sync
            def _(e):
                run(e, self.streams["sp"])


def _fsz(ap):
    s = ap.shape
    n = 1
    for v in s[1:]:
        n *= int(v)
    return n


C_GQ, C_GK, C_GV, C_GR = 0, 512, 1024, 2048
C_GLF, C_GLB = 3072, 3088
C_DQ, C_DK, C_DV, C_DZ = 3104, 4128, 5152, 6176
C_DAB = 7200
C_MA, C_MB = 7232, 8256
D_IN = 9280


def host_consts():
    r = np.arange(128)[:, None]
    t = np.arange(128)[None, :]
    same = (r // 64) == (t // 64)
    c = {}
    c["ident"] = np.eye(128, dtype=np.float32)
    c["a_le"] = np.where(r <= t, -1.0 / 16, 0.0)
    c["a_ge"] = np.where(r >= t, -1.0 / 16, 0.0)
    c["a_gt"] = np.where(r > t, -1.0 / 16, 0.0)
    c["a_lt"] = np.where(r < t, -1.0 / 16, 0.0)
    c["m_le"] = np.where(r <= t, 1.0, 0.0)
    c["m_ge"] = np.where(r >= t, 1.0, 0.0)
    c["b_le"] = np.where((r <= t) & same, 1.0, 0.0)
    c["b_ge"] = np.where((r >= t) & same, 1.0, 0.0)
    c["b_gt"] = np.where((r > t) & same, 1.0, 0.0)
    c["b_lt"] = np.where((r < t) & same, 1.0, 0.0)
    c["csel0"] = np.where(r < 64, 1.0, 0.0) + 0.0 * t
    c["csel1"] = np.where(r >= 64, 1.0, 0.0) + 0.0 * t
    c["ones"] = np.ones((128, 128))
    c["m_lt"] = np.where(r < t, 1.0, 0.0)
    c["bvals"] = 128.0 * t + 0.0 * r
    c["pidx"] = 1.0 * r + 0.0 * t
    names = list(c.keys())
    arr = np.stack([np.asarray(c[n], np.float32) for n in names], axis=1)
    return names, np.ascontiguousarray(arr)


CONST_NAMES, CONST_ARR = host_consts()
NCONST = len(CONST_NAMES)


def build(stage="all", dbg=False):
    nc = bass.Bass("TRN2", target_bir_lowering=False)
    stack = contextlib.ExitStack()
    with stack:
        P = Prog(nc, stack)

        def dram(name, shape, dt=F32, kind="ExternalInput"):
            return nc.dram_tensor(name, list(shape), dt, kind=kind).ap()

        def sb(name, shape, dt=F32):
            return stack.enter_context(nc.sbuf_tensor(name, list(shape), dt))

        def ps(name, shape, dt=F32):
            return stack.enter_context(nc.psum_tensor(name, list(shape), dt))

        def MM(out, lhsT, rhs, start, stop, R, W):
            n = _fsz(rhs)
            c = 70.0 + n * 0.75
            if rhs.dtype == F32:
                c *= 4.0
            P.op("pe", lambda e: e.matmul(out, lhsT, rhs, start=start, stop=stop), R, W, cost=c)

        def TR(out, in_, ident, R, W):
            P.op("pe", lambda e: e.transpose(out=out, in_=in_, identity=ident), R, W, cost=110.0)

        def ACTF(out, in_, func, R, W, **kw):
            c = 120.0 + _fsz(in_) * 0.6 + (90.0 if "accum_out" in kw else 0.0)
            P.op("act", lambda e: e.activation(out=out, in_=in_, func=func, **kw), R, W, cost=c)

        def _vc(eng, n, k=1.5):
            return (100.0 + n * k * 0.6) if eng == "dve" else (150.0 + n * 1.9)

        def TT(eng, out, in0, in1, op, R, W):
            P.op(eng, lambda e: e.tensor_tensor(out=out, in0=in0, in1=in1, op=op), R, W, cost=_vc(eng, _fsz(out)))

        def TS(eng, out, in0, s1, s2, op0, op1, R, W):
            P.op(eng, lambda e: e.tensor_scalar(out=out, in0=in0, scalar1=s1, scalar2=s2, op0=op0, op1=op1), R, W,
                 cost=_vc(eng, _fsz(out), 1.05))

        def STT(out, in0, scalar, in1, op0, op1, R, W):
            P.op("dve", lambda e: e.scalar_tensor_tensor(out=out, in0=in0, scalar=scalar, in1=in1, op0=op0, op1=op1), R, W,
                 cost=_vc("dve", _fsz(out)))

        def CP(eng, out, in_, R, W):
            if eng == "act":
                P.op("act", lambda e: e.activation(out=out, in_=in_, func=AF.Copy), R, W, cost=120.0 + _fsz(in_) * 0.6)
            else:
                P.op(eng, lambda e: e.tensor_copy(out=out, in_=in_), R, W, cost=_vc(eng, _fsz(out), 1.05))

        def MEMSET(eng, ap, val, W):
            P.op(eng, lambda e: e.memset(ap, val), [], W, cost=_vc(eng, _fsz(ap), 0.6))

        def DMA(eng, out, in_, semkey, R, W):
            P.dma(eng, lambda e: e.dma_start(out=out, in_=in_), semkey, R, W, nbytes=_fsz(out) * int(out.shape[0]) * 4)

        def RECIP(out, in_, R, W):
            P.op("dve", lambda e: e.reciprocal(out=out, in_=in_), R, W, cost=_vc("dve", _fsz(out), 1.05))

        def MARK(name):
            if stage == name:
                P.enabled = False

        def rstd_inplace(ap, n, key):
            TS("dve", ap, ap, 1.0 / n, EPS, ALU.mult, ALU.add, [key], [key])
            ACTF(ap, ap, AF.Ln, [key], [key])
            ACTF(ap, ap, AF.Exp, [key], [key], scale=-0.5)

        x_d = dram("x", [T, D])
        n1_d = dram("norm1_w", [1, D])
        n2_d = dram("norm2_w", [1, D])
        nf_d = dram("norm_f_w", [1, D])
        consts_d = dram("consts", [128, NCONST, 128])
        w_in_d = dram("w_in", [D, D_IN])
        w2b_d = [dram("gla_w2b_f", [17, 512]), dram("gla_w2b_b", [17, 512])]
        gnw_d = dram("gla_norm_w", [1, 256])
        out_d = dram("out", [T, D], kind="ExternalOutput")
        dbg_d = dram("dbg", [T, D], kind="ExternalOutput") if dbg else None

        consts = sb("consts_sb", [128, NCONST, 128])
        CI = {n: i for i, n in enumerate(CONST_NAMES)}

        def cst(name):
            return consts[:, CI[name], :]

        ident_b = sb("ident_b", [128, 128], BF16)
        ones_b = sb("ones_b", [128, 128], BF16)
        hT = sb("hT", [128, KT, T], BF16)
        mixed = sb("mixed", [128, NT, D], BF16)
        small = sb("small", [128, 64])
        ARENA_BYTES = 134 * 1024
        arena = sb("arena", [128, ARENA_BYTES // 4])

        def carve(off, shape, dt, base=None, cap=None):
            base = arena if base is None else base
            cap = ARENA_BYTES if cap is None else cap
            nb = int(np.prod(shape)) * (2 if dt == BF16 else 4)
            nb = (nb + 3) // 4 * 4
            assert off % 4 == 0 and off + nb <= cap, (off, nb)
            v = base[:, off // 4:(off + nb) // 4]
            if dt != F32:
                v = v.bitcast(dt)
            if len(shape) == 2:
                pat = "p (a b) -> p a b"
                v = v.rearrange(pat, a=shape[0])
            elif len(shape) == 3:
                v = v.rearrange("p (a b c) -> p a b c", a=shape[0], b=shape[1])
            return v, off + nb

        pt = [ps(f"pt{i}", [128, 1024]) for i in range(3)]
        ptb = ps("ptb", [128, 2048], BF16)

        def bank(i):
            return pt[i // 2][:, (i % 2) * 512:(i % 2 + 1) * 512]

        def bkey(i):
            return ("pb", i)

        def bbank(i):
            return ptb[:, i * 1024:(i + 1) * 1024]

        DMA("sp", consts[:], consts_d[:, :, :], "c_consts", [], ["consts"])
        CP("dve", ident_b[:], cst("ident"), ["consts"], ["ident_b"])
        MEMSET("pool", ones_b[:], 1.0, ["ones_b"])

        off = 0
        xt0, off = carve(off, [D], F32)
        xt1, off = carve(off, [D], F32)
        hn0, off = carve(off, [D], BF16)
        hn1, off = carve(off, [D], BF16)
        sq, off = carve(off, [D], F32)
        n1_bc, off = carve(off, [D], F32)
        DMA("sp", n1_bc, n1_d.partition_broadcast(128), "c_n1", [], ["n1_bc"])
        xts = [xt0, xt1]
        hns = [hn0, hn1]
        for tt in range(NT):
            b = tt % 2
            xb, hb = xts[b], hns[b]
            DMA("sp", xb, x_d[tt * 128:(tt + 1) * 128, :], ("xt", b), [], [("xt", b)])
            ACTF(sq, xb, AF.Square, [("xt", b)], ["sq", "ss0"], accum_out=small[:, 0:1])
            rstd_inplace(small[:, 0:1], D, "ss0")
            STT(hb, xb, small[:, 0:1], n1_bc, ALU.mult, ALU.mult, [("xt", b), "ss0", "n1_bc"], [("hn", b)])
            for kt in range(KT):
                TR(bbank(b)[:, kt * 128:(kt + 1) * 128], hb[:, kt * 128:(kt + 1) * 128], ident_b[:],
                   [("hn", b), "ident_b"], [("pbb", b)])
            CP("act", hT[:, :, tt * 128:(tt + 1) * 128], bbank(b).rearrange("p (k t) -> p k t", k=KT),
               [("pbb", b)], [("hT", tt)])
        HT_ALL = [("hT", tt) for tt in range(NT)]
        MARK("p1")

        P.barrier()
        off = 0
        qT, off = carve(off, [T], F32)
        kT, off = carve(off, [T], F32)
        k_tok, off = carve(off, [NT, 128], F32)
        v_tok, off = carve(off, [NT, 256], BF16)
        qdT = [None, None]
        kiT = [None, None]
        ktail = [None, None]
        for d_ in range(2):
            qdT[d_], off = carve(off, [T], BF16)
            kiT[d_], off = carve(off, [T], BF16)
            ktail[d_], off = carve(off, [NT, 128], BF16)
        sb_store, off = carve(off, [NT, 256], BF16)
        dec, off = carve(off, [2, NT], F32)
        S, off = carve(off, [256], F32)
        S_bf, off = carve(off, [256], BF16)
        NTMP = 4
        tmp = []
        for i in range(NTMP):
            d = {}
            for nm in ("e", "lg", "E", "Ei", "Et"):
                d[nm], off = carve(off, [128], F32)
            d["Pf"], off = carve(off, [128], BF16)
            d["Pb"], off = carve(off, [128], BF16)
            d["sig"], off = carve(off, [512], F32)
            d["G"], off = carve(off, [256], F32)
            tmp.append(d)
        gl, off = carve(off, [2, T], BF16)
        w2b, off = carve(off, [2, 512], BF16)
        wqk, off = carve(off, [KT, 256], BF16)
        wkv, off = carve(off, [KT, 384], BF16)
        wgm, off = carve(off, [KT, 512], BF16)
        wgl, off = carve(off, [KT, 32], BF16)
        gnw_bc, off = carve(off, [256], F32)
        GLA_END = off

        DMA("sp", gnw_bc, gnw_d.partition_broadcast(128), "c_gnw", [], ["gnw_bc"])
        MEMSET("pool", gl[0:32, :, :], 1.0, ["gl"])
        MEMSET("pool", w2b[0:32, :, :], 0.0, ["w2b"])
        for d_ in range(2):
            DMA("pool", w2b[0:17, d_, :], w2b_d[d_][:, :], "c_w2b", [], ["w2b"])
        DMA("pool", wgl, w_in_d[:, C_GLF:C_GLF + 32].rearrange("(k p) c -> p k c", p=128), "w_wgl", [], ["wgl"])
        for d_ in range(2):
            for tg in range(4):
                bi = tg % 2
                for kt in range(KT):
                    MM(bank(bi)[0:16, :], wgl[:, kt, d_ * 16:(d_ + 1) * 16], hT[:, kt, tg * 512:(tg + 1) * 512],
                       kt == 0, kt == KT - 1, ["wgl"] + HT_ALL[tg * 4:tg * 4 + 4], [bkey(bi)])
                CP("act", gl[0:16, d_, tg * 512:(tg + 1) * 512], bank(bi)[0:16, :], [bkey(bi)], ["gl"])

        MARK("g0")
        QSCALE = 128.0 ** -0.5
        for h in range(4):
            def wcols(dst, c0, n):
                return (dst, w_in_d[:, c0:c0 + n].rearrange("(k p) c -> p k c", p=128))
            for (dst, src) in (wcols(wqk[:, :, 0:128], C_GQ + h * 128, 128), wcols(wqk[:, :, 128:256], C_GK + h * 128, 128)):
                DMA("pool", dst, src, "w_wqk", [], ["wqk"])
            for (dst, src) in (wcols(wkv[:, :, 0:128], C_GK + h * 128, 128), wcols(wkv[:, :, 128:384], C_GV + h * 256, 256)):
                DMA("pool", dst, src, "w_wkv", [], ["wkv"])
            for (dst, src) in (wcols(wgm[:, :, 0:256], C_GR + h * 256, 256), wcols(wgm[:, :, 256:512], C_MA + h * 256, 256)):
                DMA("pool", dst, src, "w_wgm", [], ["wgm"])
            MARK("g1a")
            for which, dstT in ((0, qT), (1, kT)):
                for tg in range(4):
                    bi = (which * 4 + tg) % 4
                    for kt in range(KT):
                        MM(bank(bi), wqk[:, kt, which * 128:(which + 1) * 128], hT[:, kt, tg * 512:(tg + 1) * 512],
                           kt == 0, kt == KT - 1, ["wqk"] + HT_ALL[tg * 4:tg * 4 + 4], [bkey(bi)])
                    CP("act" if tg % 2 else "dve", dstT[:, tg * 512:(tg + 1) * 512], bank(bi), [bkey(bi)],
                       [("qkT", which, tg)])
            MARK("g1b")
            for n in range(NT):
                bi = 4 + n % 2
                for kt in range(KT):
                    MM(bank(bi)[:, 0:384], hT[:, kt, n * 128:(n + 1) * 128], wkv[:, kt, :],
                       kt == 0, kt == KT - 1, ["wkv", ("hT", n)], [bkey(bi)])
                CP("dve", k_tok[:, n, :], bank(bi)[:, 0:128], [bkey(bi)], [("k_tok", n)])
                CP("act", v_tok[:, n, :], bank(bi)[:, 128:384], [bkey(bi)], [("v_tok", n)])
            MARK("g1")
            for n in range(NT):
                tsl = slice(n * 128, (n + 1) * 128)
                tg = n // 4
                for d_ in range(2):
                    tm = tmp[(n * 2 + d_) % NTMP]
                    tk = ("gtmp", (n * 2 + d_) % NTMP)
                    a_c = cst("a_le") if d_ == 0 else cst("a_ge")
                    a_s = cst("a_gt") if d_ == 0 else cst("a_lt")
                    b0 = (n * 2 + d_) % 2 * 2
                    zb, cb = bank(b0), bank(b0 + 1)
                    MM(zb[:, 0:128], gl[0:32, d_, tsl], w2b[0:32, d_, h * 128:(h + 1) * 128], True, True,
                       ["gl", "w2b"], [bkey(b0)])
                    ACTF(tm["e"], zb[:, 0:128], AF.Exp, [bkey(b0)], [tk], scale=-1.0)
                    ACTF(tm["lg"], tm["e"], AF.Ln, [tk], [tk], bias=1.0)
                    MM(cb[:, 0:128], tm["lg"], a_c, True, True, [tk, "consts"], [bkey(b0 + 1)])
                    MM(cb[:, 128:256], a_s, tm["lg"], True, True, [tk, "consts"], [bkey(b0 + 1)])
                    ACTF(tm["E"], cb[:, 0:128], AF.Exp, [bkey(b0 + 1)], [tk])
                    ACTF(tm["Ei"], cb[:, 0:128], AF.Exp, [bkey(b0 + 1)], [tk], scale=-1.0)
                    ACTF(tm["Et"], cb[:, 128:256], AF.Exp, [bkey(b0 + 1)], [tk])
                    STT(qdT[d_][:, tsl], qT[:, tsl], QSCALE, tm["E"], ALU.mult, ALU.mult,
                        [("qkT", 0, tg), tk], [("qdT", d_, n)])
                    TT("dve", kiT[d_][:, tsl], kT[:, tsl], tm["Ei"], ALU.mult, [("qkT", 1, tg), tk], [("kiT", d_, n)])
                    TT("dve", ktail[d_][:, n, :], k_tok[:, n, :], tm["Et"], ALU.mult, [("k_tok", n), tk], [("ktail", d_, n)])
                    col = 127 if d_ == 0 else 0
                    CP("dve", dec[:, d_, n:n + 1], tm["E"][:, col:col + 1], [tk], [("dec", d_, n)])
            MARK("g2")
            MEMSET("dve", S, 0.0, ["S"])
            for n in range(NT - 1, -1, -1):
                CP("act", sb_store[:, n, :], S, ["S"], [("sb_store", n)])
                bi = 4 + n % 2
                MM(bank(bi)[:, 0:256], ktail[1][:, n, :], v_tok[:, n, :], True, True,
                   [("ktail", 1, n), ("v_tok", n)], [bkey(bi)])
                STT(S, S, dec[:, 1, n:n + 1], bank(bi)[:, 0:256], ALU.mult, ALU.add,
                    ["S", ("dec", 1, n), bkey(bi)], ["S"])
            MARK("g3")
            MEMSET("dve", S, 0.0, ["S"])
            for n in range(NT):
                tsl = slice(n * 128, (n + 1) * 128)
                tm = tmp[n % NTMP]
                tk = ("ftmp", n % NTMP)
                CP("act", S_bf, S, ["S"], ["S_bf"])
                b0 = (n % 2) * 2
                sc = bank(b0)
                MM(sc[:, 0:128], kiT[0][:, tsl], qdT[0][:, tsl], True, True, [("kiT", 0, n), ("qdT", 0, n)], [bkey(b0)])
                MM(sc[:, 128:256], kiT[1][:, tsl], qdT[1][:, tsl], True, True, [("kiT", 1, n), ("qdT", 1, n)], [bkey(b0)])
                TT("dve", tm["Pf"], sc[:, 0:128], cst("m_le"), ALU.mult, [bkey(b0), "consts"], [tk])
                TT("dve", tm["Pb"], sc[:, 128:256], cst("m_ge"), ALU.mult, [bkey(b0), "consts"], [tk])
                ob = bank(b0 + 1)
                ok = bkey(b0 + 1)
                MM(ob[:, 0:256], qdT[0][:, tsl], S_bf, True, False, [("qdT", 0, n), "S_bf"], [ok])
                MM(ob[:, 0:256], qdT[1][:, tsl], sb_store[:, n, :], False, False, [("qdT", 1, n), ("sb_store", n)], [ok])
                MM(ob[:, 0:256], tm["Pf"], v_tok[:, n, :], False, False, [tk, ("v_tok", n)], [ok])
                MM(ob[:, 0:256], tm["Pb"], v_tok[:, n, :], False, True, [tk, ("v_tok", n)], [ok])
                kb = 4 + n % 2
                MM(bank(kb)[:, 0:256], ktail[0][:, n, :], v_tok[:, n, :], True, True,
                   [("ktail", 0, n), ("v_tok", n)], [bkey(kb)])
                STT(S, S, dec[:, 0, n:n + 1], bank(kb)[:, 0:256], ALU.mult, ALU.add,
                    ["S", ("dec", 0, n), bkey(kb)], ["S"])
                gb = 4 + n % 2
                for kt in range(KT):
                    MM(bank(gb), hT[:, kt, tsl], wgm[:, kt, :], kt == 0, kt == KT - 1, ["wgm", ("hT", n)], [bkey(gb)])
                ACTF(tm["sig"], bank(gb), AF.Exp, [bkey(gb)], [("sig", n % NTMP)], scale=-1.0)
                ACTF(tm["sig"], tm["sig"], AF.Ln, [("sig", n % NTMP)], [("sig", n % NTMP)], bias=1.0)
                ACTF(tm["sig"], tm["sig"], AF.Exp, [("sig", n % NTMP)], [("sig", n % NTMP)], scale=-1.0)
                TT("pool", tm["G"], tm["sig"][:, 0:256], tm["sig"][:, 256:512], ALU.mult, [("sig", n % NTMP)], [("G", n % NTMP)])
                TT("dve", tm["G"], tm["G"], bank(gb)[:, 0:256], ALU.mult, [("G", n % NTMP), bkey(gb)], [("G", n % NTMP)])
                TT("pool", tm["G"], tm["G"], gnw_bc, ALU.mult, [("G", n % NTMP), "gnw_bc"], [("G", n % NTMP)])
                ssk = ("ssq", n % 2)
                ssap = small[:, 2 + n % 2:3 + n % 2]
                ACTF(tm["sig"][:, 0:256], ob[:, 0:256], AF.Square, [ok, ("G", n % NTMP)], [("sig", n % NTMP), ssk],
                     accum_out=ssap)
                rstd_inplace(ssap, 256, ssk)
                STT(mixed[:, n, h * 256:(h + 1) * 256], ob[:, 0:256], ssap, tm["G"], ALU.mult, ALU.mult,
                    [ok, ssk, ("G", n % NTMP)], [("mixed", n)])

        P.barrier()
        HG = 4
        off = 0
        gqT, off = carve(off, [HG, T], BF16)
        gkT, off = carve(off, [HG, T], BF16)
        gvT, off = carve(off, [HG, T], BF16)
        o_store, off = carve(off, [NT, HG, 128], BF16)
        dabs, off = carve(off, [NT, 32], F32)
        g_raw, off = carve(off, [NT, 2, 8], F32)
        beta, off = carve(off, [NT, 2, 8], F32)
        gvec, off = carve(off, [64], F32)
        wsl0, off = carve(off, [KT, 512], BF16)
        wsl1, off = carve(off, [KT, 512], BF16)
        wsl = [wsl0, wsl1]
        cwT, off = carve(off, [24, 5], F32)
        gdnw_bc, off = carve(off, [128], F32)
        wdab, off = carve(off, [KT, 32], BF16)
        TMP0 = off
        xc = [None, None]
        xc[0], off = carve(off, [T + 4], BF16)
        xc[1], off = carve(off, [T + 4], BF16)
        diag, off = carve(off, [5, 128], BF16)
        ce = [None, None]
        cy = [None, None]
        for i in range(2):
            ce[i], off = carve(off, [512], F32)
            cy[i], off = carve(off, [512], F32)
        cysq, off = carve(off, [512], BF16)
        crs, off = carve(off, [512], F32)
        CONV_END = off
        off = TMP0
        GMB, off = carve(off, [HG, 128], F32)
        Wd, off = carve(off, [HG, 128], F32)
        decT, off = carve(off, [HG, 128], F32)
        Lm, off = carve(off, [HG, 128], BF16)
        LTm, off = carve(off, [HG, 128], BF16)
        XT, off = carve(off, [HG, 128], BF16)
        Pp = [None, None]
        PTp = [None, None]
        for i in range(2):
            Pp[i], off = carve(off, [HG, 128], BF16)
            PTp[i], off = carve(off, [HG, 128], BF16)
        kbg, off = carve(off, [HG, 128], BF16)
        vbeta, off = carve(off, [HG, 128], BF16)
        qd_tok, off = carve(off, [HG, 128], BF16)
        DB = []
        for d_ in range(2):
            dd = {}
            for nm in ("attnT", "ktl", "qdTg", "wT_sb", "vnew", "Sg_bf"):
                dd[nm], off = carve(off, [HG, 128], BF16)
            dd["u_sb"], off = carve(off, [HG, 128], F32)
            dd["Sg"], off = carve(off, [HG, 128], F32)
            dd["esc"], off = carve(off, [16], F32)
            dd["bg"], off = carve(off, [HG], F32)
            DB.append(dd)
        osum, off = carve(off, [HG, 128], F32)
        fsig, off = carve(off, [1024], F32)
        fG, off = carve(off, [HG, 128], F32)
        frs, off = carve(off, [8], F32)
        SWEEP_END = off

        gdnw_d = dram("gdn_norm_w", [1, 128])
        gvec_d = dram("gdn_vec", [1, 32])
        cw_d = dram("gdn_conv_wT", [128, 24, 5])
        DMA("sp", gdnw_bc, gdnw_d.partition_broadcast(128), "c_gdnw", [], ["gdnw_bc"])
        DMA("sp", gvec[:, 0:32], gvec_d.partition_broadcast(128), "c_gvec", [], ["gvec"])
        DMA("sp", cwT, cw_d[:, :, :], "c_cw", [], ["cwT"])
        DMA("pool", wdab, w_in_d[:, C_DAB:C_DAB + 32].rearrange("(k p) c -> p k c", p=128), "w_wdab", [], ["wdab"])
        ACTF(gvec[:, 16:32], gvec[:, 16:32], AF.Exp, ["gvec"], ["gvec"])
        TS("dve", gvec[:, 16:32], gvec[:, 16:32], -1.0, None, ALU.mult, ALU.bypass, ["gvec"], ["gvec"])
        for n in range(NT):
            bi = n % 2
            for kt in range(KT):
                MM(bank(bi)[:, 0:32], hT[:, kt, n * 128:(n + 1) * 128], wdab[:, kt, :], kt == 0, kt == KT - 1,
                   ["wdab", ("hT", n)], [bkey(bi)])
            CP("act", dabs[:, n, :], bank(bi)[:, 0:32], [bkey(bi)], ["dabs"])
        a_view = dabs[:, :, 0:16]
        b_view = dabs[:, :, 16:32]
        g_flat = g_raw.rearrange("p n d h -> p n (d h)")
        be_flat = beta.rearrange("p n d h -> p n (d h)")
        TT("dve", g_flat, a_view, gvec[:, 0:16].unsqueeze(1).to_broadcast([128, NT, 16]), ALU.add, ["dabs", "gvec"], ["g_raw"])
        ACTF(g_flat, g_flat, AF.Exp, ["g_raw"], ["g_raw"])
        ACTF(g_flat, g_flat, AF.Ln, ["g_raw"], ["g_raw"], bias=1.0)
        TT("dve", g_flat, g_flat, gvec[:, 16:32].unsqueeze(1).to_broadcast([128, NT, 16]), ALU.mult, ["g_raw", "gvec"], ["g_raw"])
        ACTF(be_flat, b_view, AF.Exp, ["dabs"], ["beta"], scale=-1.0)
        TS("dve", be_flat, be_flat, 1.0, None, ALU.add, ALU.bypass, ["beta"], ["beta"])
        RECIP(be_flat, be_flat, ["beta"], ["beta"])
        MARK("d0")

        GSCALE = 128.0 ** -0.5
        ident_bc4 = ident_b[:].unsqueeze(1).to_broadcast([128, HG, 128])

        def bc_h(ap2):
            return ap2.unsqueeze(2).to_broadcast([128, HG, 128])

        def bc_m(ap2):
            return ap2.unsqueeze(1).to_broadcast([128, HG, 128])

        def v4(ap2):
            return ap2.rearrange("p (h d) -> p h d", h=HG)

        for grp in range(2):
            hs0 = grp * HG
            for which, c_base, dstT in ((0, C_DQ, gqT), (1, C_DK, gkT), (2, C_DV, gvT)):
                ws = wsl[which % 2]
                wk = ("wsl", which % 2)
                DMA("pool", ws, w_in_d[:, c_base + hs0 * 128:c_base + (hs0 + HG) * 128].rearrange("(k p) c -> p k c", p=128),
                    ("w_wsl", which % 2), [], [wk])
                for hh in range(HG):
                    ci = which * 8 + hs0 + hh
                    xi = (which * HG + hh) % 2
                    xcb = xc[xi]
                    xk = ("xc", xi)
                    MEMSET("pool", xcb[:, 0:2], 0.0, [xk])
                    MEMSET("pool", xcb[:, T + 2:T + 4], 0.0, [xk])
                    for k in range(5):
                        TS("dve", diag[:, k, :], cst("ident"), cwT[:, ci, k:k + 1], None, ALU.mult, ALU.bypass,
                           ["consts", "cwT"], ["diag"])
                    for tg in range(4):
                        bi = tg % 2
                        for kt in range(KT):
                            MM(bank(bi), ws[:, kt, hh * 128:(hh + 1) * 128], hT[:, kt, tg * 512:(tg + 1) * 512],
                               kt == 0, kt == KT - 1, [wk] + HT_ALL[tg * 4:tg * 4 + 4], [bkey(bi)])
                        CP("act" if tg % 2 else "dve", xcb[:, 2 + tg * 512:2 + (tg + 1) * 512], bank(bi), [bkey(bi)], [xk])
                    for tg in range(4):
                        bi = 2 + tg % 2
                        i2 = tg % 2
                        for k in range(5):
                            MM(bank(bi), diag[:, k, :], xcb[:, tg * 512 + k:tg * 512 + k + 512], k == 0, k == 4,
                               ["diag", xk], [bkey(bi)])
                        ck = ("ctmp", i2)
                        ACTF(ce[i2], bank(bi), AF.Exp, [bkey(bi)], [ck], scale=-1.0)
                        ACTF(ce[i2], ce[i2], AF.Ln, [ck], [ck], bias=1.0)
                        ACTF(ce[i2], ce[i2], AF.Exp, [ck], [ck], scale=-1.0)
                        dst = dstT[:, hh, tg * 512:(tg + 1) * 512]
                        dk = ("gT", which, hh, tg)
                        if which == 2:
                            TT("dve", dst, ce[i2], bank(bi), ALU.mult, [ck, bkey(bi)], [dk])
                        else:
                            TT("dve", cy[i2], ce[i2], bank(bi), ALU.mult, [ck, bkey(bi)], [("cy", i2)])
                            TT("pool", cysq, cy[i2], cy[i2], ALU.mult, [("cy", i2)], ["cysq"])
                            MM(bank(4), ones_b[:], cysq, True, True, ["ones_b", "cysq"], [bkey(4)])
                            ACTF(crs, bank(4), AF.Ln, [bkey(4)], ["crs"], bias=EPS)
                            ACTF(crs, crs, AF.Exp, ["crs"], ["crs"], scale=-0.5)
                            if which == 0:
                                STT(dst, cy[i2], GSCALE, crs, ALU.mult, ALU.mult, [("cy", i2), "crs"], [dk])
                            else:
                                TT("dve", dst, cy[i2], crs, ALU.mult, [("cy", i2), "crs"], [dk])
            MARK("d1")
            P.barrier()
            DMA("pool", wsl[0], w_in_d[:, C_DZ + hs0 * 128:C_DZ + (hs0 + HG) * 128].rearrange("(k p) c -> p k c", p=128),
                ("w_wsl", 0), [], [("wsl", 0)])
            DMA("pool", wsl[1], w_in_d[:, C_MB + hs0 * 128:C_MB + (hs0 + HG) * 128].rearrange("(k p) c -> p k c", p=128),
                ("w_wsl", 1), [], [("wsl", 1)])

            def gT_keys(which, n):
                return [("gT", which, hh, n // 4) for hh in range(HG)]

            stored = set()

            def gdn_tile(d_, n):
                B = DB[d_]
                dk = lambda nm: (nm, d_)
                Mc = cst("b_le") if d_ == 0 else cst("b_ge")
                Ms = cst("b_gt") if d_ == 0 else cst("b_lt")
                esc, bg = B["esc"], B["bg"]
                tsl = slice(n * 128, (n + 1) * 128)
                gv = g_raw[:, n, d_, hs0:hs0 + HG]
                bv = beta[:, n, d_, hs0:hs0 + HG]
                MM(bank(0)[:, 0:4], Mc, gv, True, True, ["consts", "g_raw"], [bkey(0)])
                MM(bank(0)[:, 4:8], Ms, gv, True, True, ["consts", "g_raw"], [bkey(0)])
                MM(bank(0)[:, 8:12], cst("csel0"), gv, True, True, ["consts", "g_raw"], [bkey(0)])
                MM(bank(0)[:, 12:16], cst("csel1"), gv, True, True, ["consts", "g_raw"], [bkey(0)])
                ACTF(esc, bank(0)[:, 0:16], AF.Exp, [bkey(0)], [dk("esc")])
                TT("dve", bg, bv, esc[:, 0:4], ALU.mult, ["beta", dk("esc")], [dk("bg")])
                for hh in range(HG):
                    TR(bbank(0)[:, hh * 128:(hh + 1) * 128], gkT[:, hh, tsl], ident_b[:], gT_keys(1, n) + ["ident_b"], [("pbb", 0)])
                for hh in range(HG):
                    TR(bbank(1)[:, hh * 128:(hh + 1) * 128], gvT[:, hh, tsl], ident_b[:], gT_keys(2, n) + ["ident_b"], [("pbb", 1)])
                TT("dve", kbg, v4(bbank(0)[:, 0:512]), bc_h(bg), ALU.mult, [("pbb", 0), dk("bg")], ["kbg"])
                TT("dve", B["ktl"], v4(bbank(0)[:, 0:512]), bc_h(esc[:, 4:8]), ALU.mult, [("pbb", 0), dk("esc")], [dk("ktl")])
                TT("dve", vbeta, v4(bbank(1)[:, 0:512]), bc_h(bv), ALU.mult, [("pbb", 1), "beta"], ["vbeta"])
                for hh in range(HG):
                    TR(bbank(0)[:, hh * 128:(hh + 1) * 128], gqT[:, hh, tsl], ident_b[:], gT_keys(0, n) + ["ident_b"], [("pbb", 0)])
                TT("dve", qd_tok, v4(bbank(0)[:, 0:512]), bc_h(esc[:, 0:4]), ALU.mult, [("pbb", 0), dk("esc")], ["qd_tok"])
                for hh in range(HG):
                    TR(bbank(1)[:, hh * 128:(hh + 1) * 128], qd_tok[:, hh, :], ident_b[:], ["qd_tok", "ident_b"], [("pbb", 1)])
                CP("act", B["qdTg"], v4(bbank(1)[:, 0:512]), [("pbb", 1)], [dk("qdTg")])
                TT("pool", GMB, bc_h(gv), bc_m(Ms), ALU.mult, ["g_raw", "consts"], ["GMB"])
                MM(bank(1), Mc, GMB.rearrange("p h s -> p (h s)"), True, True, ["consts", "GMB"], [bkey(1)])
                ACTF(Wd.rearrange("p h s -> p (h s)"), bank(1), AF.Exp, [bkey(1)], ["Wd"])
                TT("pool", GMB, bc_h(bv), bc_m(Ms), ALU.mult, ["beta", "consts"], ["GMB"])
                TT("pool", Wd, Wd, GMB, ALU.mult, ["Wd", "GMB"], ["Wd"])
                TT("pool", GMB, bc_h(gv), bc_m(Mc), ALU.mult, ["g_raw", "consts"], ["GMB"])
                MM(bank(0), Ms, GMB.rearrange("p h s -> p (h s)"), True, True, ["consts", "GMB"], [bkey(0)])
                ACTF(decT.rearrange("p h s -> p (h s)"), bank(0), AF.Exp, [bkey(0)], ["decT"])
                TT("pool", decT, decT, bc_m(Mc), ALU.mult, ["decT", "consts"], ["decT"])
                for hh in range(HG):
                    MM(bank(1)[:, hh * 128:(hh + 1) * 128], gkT[:, hh, tsl], gkT[:, hh, tsl], True, True,
                       gT_keys(1, n), [bkey(1)])
                TT("dve", Lm, v4(bank(1)), Wd, ALU.mult, [bkey(1), "Wd"], ["Lm"])
                for hh in range(HG):
                    MM(bank(0)[:, hh * 128:(hh + 1) * 128], gkT[:, hh, tsl], gqT[:, hh, tsl], True, True,
                       gT_keys(1, n) + gT_keys(0, n), [bkey(0)])
                TT("dve", B["attnT"], v4(bank(0)), decT, ALU.mult, [bkey(0), "decT"], [dk("attnT")])
                for hh in range(HG):
                    TR(bbank(0)[:, hh * 128:(hh + 1) * 128], Lm[:, hh, :], ident_b[:], ["Lm", "ident_b"], [("pbb", 0)])
                CP("act", LTm, v4(bbank(0)[:, 0:512]), [("pbb", 0)], ["LTm"])
                TT("dve", XT, ident_bc4, v4(bbank(0)[:, 0:512]), ALU.subtract, ["ident_b", ("pbb", 0)], ["XT"])
                Pc, PTc = Lm, LTm
                pck, ptk_ = "Lm", "LTm"
                for it in range(5):
                    Pn, PTn = Pp[it % 2], PTp[it % 2]
                    pnk, ptnk = ("Pp", it % 2), ("PTp", it % 2)
                    for hh in range(HG):
                        MM(bank(1)[:, hh * 128:(hh + 1) * 128], PTc[:, hh, :], Pc[:, hh, :], True, True, [pck, ptk_], [bkey(1)])
                    CP("act", Pn, v4(bank(1)), [bkey(1)], [pnk])
                    if it < 4:
                        for hh in range(HG):
                            MM(bank(0)[:, hh * 128:(hh + 1) * 128], Pc[:, hh, :], PTc[:, hh, :], True, True, [pck, ptk_], [bkey(0)])
                        CP("dve", PTn, v4(bank(0)), [bkey(0)], [ptnk])
                    for hh in range(HG):
                        MM(bank(1)[:, hh * 128:(hh + 1) * 128], Pn[:, hh, :], XT[:, hh, :], True, True, [pnk, "XT"], [bkey(1)])
                    TT("dve", XT, XT, v4(bank(1)), ALU.add, ["XT", bkey(1)], ["XT"])
                    Pc, PTc, pck, ptk_ = Pn, PTn, pnk, ptnk
                for hh in range(HG):
                    MM(bank(0)[:, hh * 128:(hh + 1) * 128], XT[:, hh, :], vbeta[:, hh, :], True, True, ["XT", "vbeta"], [bkey(0)])
                CP("act", B["u_sb"], v4(bank(0)), [bkey(0)], [dk("u_sb")])
                for hh in range(HG):
                    MM(bank(1)[:, hh * 128:(hh + 1) * 128], kbg[:, hh, :], XT[:, hh, :], True, True, ["kbg", "XT"], [bkey(1)])
                CP("dve", B["wT_sb"], v4(bank(1)), [bkey(1)], [dk("wT_sb")])
                sb0 = 4 if d_ == 0 else 2
                Sg, Sg_bf, vnew = B["Sg"], B["Sg_bf"], B["vnew"]
                chunks = (0, 1) if d_ == 0 else (1, 0)
                for c in chunks:
                    sl = slice(c * 64, c * 64 + 64)
                    for hh in range(HG):
                        MM(bank(sb0)[sl, hh * 128:(hh + 1) * 128], B["wT_sb"][:, hh, sl], Sg_bf[:, hh, :], True, True,
                           [dk("wT_sb"), dk("Sg_bf")], [bkey(sb0)])
                    TT("dve", vnew[sl], B["u_sb"][sl], v4(bank(sb0))[sl], ALU.subtract, [dk("u_sb"), bkey(sb0)], [dk("vnew")])
                    for hh in range(HG):
                        MM(bank(sb0 + 1)[sl, hh * 128:(hh + 1) * 128], B["qdTg"][:, hh, sl], Sg_bf[:, hh, :], True, False,
                           [dk("qdTg"), dk("Sg_bf")], [bkey(sb0 + 1)])
                        MM(bank(sb0 + 1)[sl, hh * 128:(hh + 1) * 128], B["attnT"][sl, hh, sl], vnew[sl, hh, :], False, True,
                           [dk("attnT"), dk("vnew")], [bkey(sb0 + 1)])
                    for hh in range(HG):
                        MM(bank(sb0)[:, hh * 128:(hh + 1) * 128], B["ktl"][sl, hh, :], vnew[sl, hh, :], True, True,
                           [dk("ktl"), dk("vnew")], [bkey(sb0)])
                    TT("pool", Sg, Sg, bc_h(esc[:, 8 + 4 * c:12 + 4 * c]), ALU.mult, [dk("Sg"), dk("esc")], [dk("Sg")])
                    TT("dve", Sg, Sg, v4(bank(sb0)), ALU.add, [dk("Sg"), bkey(sb0)], [dk("Sg")])
                    CP("act", Sg_bf, Sg, [dk("Sg")], [dk("Sg_bf")])
                    if n not in stored:
                        CP("act", o_store[sl, n], v4(bank(sb0 + 1))[sl], [bkey(sb0 + 1)], [("o_store", n)])
                    else:
                        TT("dve", osum[sl], v4(bank(sb0 + 1))[sl], o_store[sl, n], ALU.add, [bkey(sb0 + 1), ("o_store", n)], ["osum"])
                if n not in stored:
                    stored.add(n)
                    return
                osq = fsig[:, 0:512].rearrange("p (h d) -> p h d", h=HG)
                TT("pool", osq, osum, osum, ALU.mult, ["osum"], ["fsig"])
                P.op("dve", lambda e: e.tensor_reduce(out=frs[:, 0:HG], in_=osq, axis=AX.X, op=ALU.add), ["fsig"], ["frs"], cost=600.0)
                rstd_inplace(frs[:, 0:HG], 128, "frs")
                for half, ws in enumerate(wsl):
                    for kt in range(KT):
                        MM(bank(half), hT[:, kt, tsl], ws[:, kt, :], kt == 0, kt == KT - 1,
                           [("wsl", half), ("hT", n)], [bkey(half)])
                zm = pt[0][:, :]
                ACTF(fsig, zm, AF.Exp, [bkey(0), bkey(1)], ["fsig"], scale=-1.0)
                ACTF(fsig, fsig, AF.Ln, ["fsig"], ["fsig"], bias=1.0)
                ACTF(fsig, fsig, AF.Exp, ["fsig"], ["fsig"], scale=-1.0)
                TT("pool", fG.rearrange("p h d -> p (h d)"), fsig[:, 0:512], fsig[:, 512:1024], ALU.mult, ["fsig"], ["fG"])
                TT("dve", fG, fG, v4(bank(0)), ALU.mult, ["fG", bkey(0)], ["fG"])
                TT("pool", fG, fG, bc_m(gdnw_bc), ALU.mult, ["fG", "gdnw_bc"], ["fG"])
                TT("pool", osum, osum, bc_h(frs[:, 0:HG]), ALU.mult, ["osum", "frs"], ["osum"])
                TT("pool", osum, osum, fG, ALU.mult, ["osum", "fG"], ["osum"])
                mslice = mixed[:, n, hs0 * 128:(hs0 + HG) * 128].rearrange("p (h d) -> p h d", h=HG)
                TT("dve", mslice, mslice, osum, ALU.add, ["osum", ("mixed", n)], [("mixed", n)])

            for d_ in range(2):
                MEMSET("dve", DB[d_]["Sg"], 0.0, [("Sg", d_)])
                CP("act", DB[d_]["Sg_bf"], DB[d_]["Sg"], [("Sg", d_)], [("Sg_bf", d_)])
            for i in range(NT):
                gdn_tile(0, i)
                gdn_tile(1, NT - 1 - i)
            MARK("d2")
            P.barrier()

        P.barrier()
        wout_d = dram("w_out", [D, D])
        off = 0
        x1, off = carve(off, [NT, D], F32)
        X1_END = off
        mT, off = carve(off, [KT, T], BF16)
        wout, off = carve(off, [KT, D], BF16)
        hn2 = [None, None]
        hn2[0], off = carve(off, [D], BF16)
        hn2[1], off = carve(off, [D], BF16)
        junk, off = carve(off, [D], BF16)
        n2_bc, off = carve(off, [D], F32)
        DMA("sp", n2_bc, n2_d.partition_broadcast(128), "c_n2", [], ["n2_bc"])
        DMA("pool", wout, wout_d.rearrange("(k p) c -> p k c", p=128), "w_wout", [], ["wout"])
        for n in range(NT):
            b = n % 2
            for kt in range(KT):
                TR(bbank(b)[:, kt * 128:(kt + 1) * 128], mixed[:, n, kt * 128:(kt + 1) * 128], ident_b[:],
                   [("mixed", n), "ident_b"], [("pbb", b)])
            CP("act", mT[:, :, n * 128:(n + 1) * 128], bbank(b).rearrange("p (k t) -> p k t", k=KT), [("pbb", b)], [("mT", n)])
        for n in range(NT):
            tsl = slice(n * 128, (n + 1) * 128)
            DMA("sp", x1[:, n, :], x_d[tsl, :], ("x1ld", n % 4), [], [("x1", n)])
            pp = pt[n % 2]
            for half in range(2):
                for kt in range(KT):
                    MM(pp[:, half * 512:(half + 1) * 512], mT[:, kt, tsl], wout[:, kt, half * 512:(half + 1) * 512],
                       kt == 0, kt == KT - 1, [("mT", n), "wout"], [bkey((n % 2) * 2 + half)])
            TT("dve", x1[:, n, :], x1[:, n, :], pp[:, :], ALU.add, [("x1", n), bkey((n % 2) * 2), bkey((n % 2) * 2 + 1)], [("x1", n)])
            b = n % 2
            ssap = small[:, 8 + b:9 + b]
            ssk = ("ss2", b)
            ACTF(junk, x1[:, n, :], AF.Square, [("x1", n)], ["junk", ssk], accum_out=ssap)
            rstd_inplace(ssap, D, ssk)
            STT(mixed[:, n, :], x1[:, n, :], ssap, n2_bc, ALU.mult, ALU.mult, [("x1", n), ssk, "n2_bc"], [("mixed", n)])
            for kt in range(KT):
                TR(bbank(b)[:, kt * 128:(kt + 1) * 128], mixed[:, n, kt * 128:(kt + 1) * 128], ident_b[:],
                   [("mixed", n), "ident_b"], [("pbb", b)])
            CP("act", hT[:, :, tsl], bbank(b).rearrange("p (k t) -> p k t", k=KT), [("pbb", b)], [("hT", n)])
        MARK("e0")

        P.barrier()
        NB = 64
        wr_d = dram("moe_wr", [D, 36])
        wgu0_d = dram("moe_wgu0", [4096, 2048])
        wgu1_d = dram("moe_wgu1", [4096, 2048])
        wdr_d = dram("moe_wdr", [4096, 2048])
        xb_d = nc.dram_tensor("moe_xb", [NB * 128, D], BF16, kind="Internal").ap()
        yb_d = nc.dram_tensor("moe_yb", [NB * 128, D], F32, kind="Internal").ap()
        off = X1_END
        stg = []
        for i in range(3):
            a, off = carve(off, [2048], F32)
            stg.append(a)
        wgu_bf = []
        wd_bf = []
        for i in range(2):
            a, off = carve(off, [KT, 512], BF16)
            wgu_bf.append(a)
            a, off = carve(off, [2, D], BF16)
            wd_bf.append(a)
        wr, off = carve(off, [KT, 36], BF16)
        lg, off = carve(off, [NT, 36], F32)
        oh1, off = carve(off, [NT, 32], F32)
        oh2, off = carve(off, [NT, 32], F32)
        msk, off = carve(off, [NT, 32], F32)
        rank, off = carve(off, [NT, 32], F32)
        tmp3, off = carve(off, [NT, 32], F32)
        gtmp, off = carve(off, [NT, 4], F32)
        ohg, off = carve(off, [NT, 4], F32)
        rv, off = carve(off, [8, NT], F32)
        mcum, off = carve(off, [32], F32)
        cnt, off = carve(off, [32], F32)
        padded, off = carve(off, [32], F32)
        ends, off = carve(off, [32], F32)
        pstart, off = carve(off, [32], F32)
        ebf, off = carve(off, [NB], F32)
        widx_f, off = carve(off, [NB], F32)
        widx, off = carve(off, [NB], I32)
        dest_f, off = carve(off, [2, NT], F32)
        dest_i, off = carve(off, [2, NT], I32)
        MOE_END = off
        cmpb = stg[0].rearrange("p (b e) -> p b e", b=NB)
        cmpj = stg[1][:, 0:512].rearrange("p (e j) -> p e j", e=32)
        ht32 = hT[:].rearrange("p k t -> p (k t)").bitcast(F32)
        HTCAP = 32 * 1024
        hoff = 0
        xg, xgT, sil, hid_bf, hidT, ysb = [], [], [], [], [], []
        for i in range(2):
            a, hoff = carve(hoff, [D], BF16, ht32, HTCAP); xg.append(a)
            a, hoff = carve(hoff, [KT, 128], BF16, ht32, HTCAP); xgT.append(a)
            a, hoff = carve(hoff, [256], F32, ht32, HTCAP); sil.append(a)
            a, hoff = carve(hoff, [256], BF16, ht32, HTCAP); hid_bf.append(a)
            a, hoff = carve(hoff, [2, 128], BF16, ht32, HTCAP); hidT.append(a)
            a, hoff = carve(hoff, [D], F32, ht32, HTCAP); ysb.append(a)
        mix32 = mixed[:].rearrange("p n d -> p (n d)").bitcast(F32)
        MIXCAP = 32 * 1024
        moff = 0
        yg = []
        for i in range(2):
            a, moff = carve(moff, [D], F32, mix32, MIXCAP); yg.append(a)
        stgB = []
        for i in range(3):
            a, moff = carve(moff, [2048], F32, mix32, MIXCAP); stgB.append(a)

        DMA("pool", wr, wr_d.rearrange("(k p) c -> p k c", p=128), "w_wr", [], ["wr"])
        for n in range(NT):
            bi = n % 2
            for kt in range(KT):
                MM(bank(bi)[:, 0:36], hT[:, kt, n * 128:(n + 1) * 128], wr[:, kt, :], kt == 0, kt == KT - 1,
                   ["wr", ("hT", n)], [bkey(bi)])
            CP("act", lg[:, n, :], bank(bi)[:, 0:36], [bkey(bi)], ["lg"])
        BIG = 10000.0
        glv = lg[:, :, 0:4]
        elv = lg[:, :, 4:36]

        def RED(out, in_, op, R, W):
            P.op("dve", lambda e: e.tensor_reduce(out=out, in_=in_, axis=AX.X, op=op), R, W, cost=100.0 + _fsz(in_) * 1.0)

        def bcn(ap2, k):
            return ap2.unsqueeze(2).to_broadcast([128, NT, k])

        gmax, gsum, m1, m2, w1, w2 = (rv[:, i, :] for i in range(6))
        RED(gmax, glv, ALU.max, ["lg"], ["rv"])
        TT("dve", ohg, glv, bcn(gmax, 4), ALU.is_equal, ["lg", "rv"], ["ohg"])
        TT("dve", gtmp, glv, bcn(gmax, 4), ALU.subtract, ["lg", "rv"], ["gtmp"])
        ACTF(gtmp, gtmp, AF.Exp, ["gtmp"], ["gtmp"])
        RED(gsum, gtmp, ALU.add, ["gtmp"], ["rv"])
        RECIP(gsum, gsum, ["rv"], ["rv"])
        TS("dve", ohg, ohg, BIG, -BIG, ALU.mult, ALU.add, ["ohg"], ["ohg"])
        TT("dve", msk.rearrange("p n (g e) -> p n g e", g=4), elv.rearrange("p n (g e) -> p n g e", g=4),
           ohg.unsqueeze(3).to_broadcast([128, NT, 4, 8]), ALU.add, ["lg", "ohg"], ["msk"])
        RED(m1, msk, ALU.max, ["msk"], ["rv"])
        TT("dve", oh1, msk, bcn(m1, 32), ALU.is_equal, ["msk", "rv"], ["oh1"])
        STT(msk, oh1, -BIG, msk, ALU.mult, ALU.add, ["oh1", "msk"], ["msk"])
        RED(m2, msk, ALU.max, ["msk"], ["rv"])
        TT("dve", oh2, msk, bcn(m2, 32), ALU.is_equal, ["msk", "rv"], ["oh2"])
        TT("dve", w2, m2, m1, ALU.subtract, ["rv"], ["rv"])
        ACTF(w2, w2, AF.Exp, ["rv"], ["rv"])
        TS("dve", w1, w2, 1.0, None, ALU.add, ALU.bypass, ["rv"], ["rv"])
        RECIP(w1, w1, ["rv"], ["rv"])
        TT("dve", w1, w1, gsum, ALU.mult, ["rv"], ["rv"])
        TT("dve", w2, w2, w1, ALU.mult, ["rv"], ["rv"])
        TT("dve", msk, oh1, oh2, ALU.add, ["oh1", "oh2", "msk"], ["msk"])
        MEMSET("dve", mcum, 0.0, ["mcum"])
        for n in range(NT):
            bi = n % 2
            MM(bank(bi)[:, 0:32], cst("m_lt"), msk[:, n, :], True, False, ["consts", "msk"], [bkey(bi)])
            MM(bank(bi)[:, 0:32], cst("ones"), mcum, False, True, ["consts", "mcum"], [bkey(bi)])
            CP("act", rank[:, n, :], bank(bi)[:, 0:32], [bkey(bi)], ["rank"])
            TT("dve", mcum, mcum, msk[:, n, :], ALU.add, ["mcum", "msk"], ["mcum"])
        MM(bank(0)[:, 0:32], cst("ones"), mcum, True, True, ["consts", "mcum"], [bkey(0)])
        CP("act", cnt, bank(0)[:, 0:32], [bkey(0)], ["cnt"])
        TT("dve", cmpj, cnt.unsqueeze(2).to_broadcast([128, 32, 16]),
           cst("bvals")[:, 0:16].unsqueeze(1).to_broadcast([128, 32, 16]), ALU.is_gt, ["cnt", "consts"], [("stg", 1)])
        RED(padded, cmpj, ALU.add, [("stg", 1)], ["padded"])
        TS("dve", padded, padded, 128.0, None, ALU.mult, ALU.bypass, ["padded"], ["padded"])
        P.op("dve", lambda e: e.tensor_tensor_scan(out=ends, data0=cst("ones")[:, 0:32], data1=padded, initial=0.0,
                                                  op0=ALU.mult, op1=ALU.add), ["consts", "padded"], ["ends"], cost=300.0)
        TT("dve", pstart, ends, padded, ALU.subtract, ["ends", "padded"], ["pstart"])
        TT("dve", rank, rank, pstart.unsqueeze(1).to_broadcast([128, NT, 32]), ALU.add, ["rank", "pstart"], ["rank"])
        for k, ohk in ((0, oh1), (1, oh2)):
            TT("dve", tmp3, ohk, rank, ALU.mult, ["oh1", "oh2", "rank"], ["tmp3"])
            RED(dest_f[:, k, :], tmp3, ALU.add, ["tmp3"], ["dest_f"])
        CP("dve", dest_i, dest_f, ["dest_f"], ["dest_i"])
        TT("dve", cmpb, ends.unsqueeze(1).to_broadcast([128, NB, 32]),
           cst("bvals")[:, 0:NB].unsqueeze(2).to_broadcast([128, NB, 32]), ALU.is_le, ["ends", "consts"], [("stg", 0)])
        RED(ebf, cmpb, ALU.add, [("stg", 0)], ["ebf"])
        STT(widx_f, ebf, 128.0, cst("pidx")[:, 0:NB], ALU.mult, ALU.add, ["ebf", "consts"], ["widx_f"])
        CP("dve", widx, widx_f, ["widx_f"], ["widx"])
        MARK("e1")

        IOA = bass.IndirectOffsetOnAxis
        regs = {}

        def _pool_init(e):
            regs["bc"] = e.alloc_register("moe_bc")
            e.reg_mov(regs["bc"], 4095)
        P.pool_init = _pool_init
        XB_KEYS = []
        zt, off = carve(off, [D], BF16)
        MEMSET("pool", zt, 0.0, ["zt"])
        DMA("sp", xb_d.rearrange("(p r) d -> p r d", p=128), zt.unsqueeze(1).to_broadcast([128, NB, D]), "xbz", ["zt"], ["xb0"])
        for n in range(NT):
            for k in range(2):
                idx_ap = dest_i[:, k, n:n + 1]
                src_ap = mixed[:, n, :]
                P.dma("pool", lambda e, idx_ap=idx_ap, src_ap=src_ap: e.indirect_dma_start(
                    out=xb_d[:, :], out_offset=IOA(ap=idx_ap, axis=0), in_=src_ap, in_offset=None),
                    ("sc", (2 * n + k) % 4), [("mixed", n), "dest_i", "xb0"], [("xb", n, k)], nbytes=256 * 1024)
                XB_KEYS.append(("xb", n, k))

        def gather_w(dst, src_d, b, skey, extra):
            idx_ap = widx[:, b:b + 1]
            P.dma("pool", lambda e: e.indirect_dma_start(
                out=dst, out_offset=None, in_=src_d[:, :], in_offset=IOA(ap=idx_ap, axis=0),
                bounds_check=regs["bc"], oob_is_err=False),
                skey, ["widx"] + extra, [skey], nbytes=1 << 20)

        YB_KEYS = []
        for b in range(NB):
            s = b % 2
            sset = stg if b % 2 == 0 else stgB
            so = 0 if b % 2 == 0 else 3
            extra = [] if b % 2 == 0 else XB_KEYS
            gather_w(sset[0], wgu0_d, b, ("stg", so + 0), extra)
            gather_w(sset[1], wgu1_d, b, ("stg", so + 1), extra)
            gather_w(sset[2], wdr_d, b, ("stg", so + 2), extra)
            CP("act", wgu_bf[s][:, 0:4, :], sset[0].rearrange("p (k c) -> p k c", k=4), [("stg", so + 0)], [("wgu_bf", s, 0)])
            CP("dve", wgu_bf[s][:, 4:8, :], sset[1].rearrange("p (k c) -> p k c", k=4), [("stg", so + 1)], [("wgu_bf", s, 1)])
            CP("act" if b % 4 < 2 else "dve", wd_bf[s], sset[2].rearrange("p (k c) -> p k c", k=2), [("stg", so + 2)], [("wd_bf", s)])
            DMA("sp", xg[s], xb_d[b * 128:(b + 1) * 128, :], ("xg", s), XB_KEYS, [("xg", s)])
            for kt in range(KT):
                TR(bbank(s)[:, kt * 128:(kt + 1) * 128], xg[s][:, kt * 128:(kt + 1) * 128], ident_b[:],
                   [("xg", s), "ident_b"], [("pbb", s)])
            CP("act", xgT[s], bbank(s).rearrange("p (k t) -> p k t", k=KT), [("pbb", s)], [("xgT", s)])
            hb = bank(s)
            for kt in range(KT):
                MM(hb, xgT[s][:, kt, :], wgu_bf[s][:, kt, :], kt == 0, kt == KT - 1,
                   [("xgT", s), ("wgu_bf", s, 0), ("wgu_bf", s, 1)], [bkey(s)])
            ACTF(sil[s], hb[:, 0:256], AF.Silu, [bkey(s)], [("sil", s)])
            TT("dve", hid_bf[s], sil[s], hb[:, 256:512], ALU.mult, [("sil", s), bkey(s)], [("hid_bf", s)])
            for ft in range(2):
                TR(bbank(s)[:, ft * 128:(ft + 1) * 128], hid_bf[s][:, ft * 128:(ft + 1) * 128], ident_b[:],
                   [("hid_bf", s), "ident_b"], [("pbb", s)])
            CP("act", hidT[s], bbank(s)[:, 0:256].rearrange("p (k t) -> p k t", k=2), [("pbb", s)], [("hidT", s)])
            yp = pt[1 + s]
            for half in range(2):
                for ft in range(2):
                    MM(yp[:, half * 512:(half + 1) * 512], hidT[s][:, ft, :], wd_bf[s][:, ft, half * 512:(half + 1) * 512],
                       ft == 0, ft == 1, [("hidT", s), ("wd_bf", s)], [bkey(2 + 2 * s + half)])
            CP("act" if b % 2 else "dve", ysb[s], yp[:, :], [bkey(2 + 2 * s), bkey(3 + 2 * s)], [("ysb", s)])
            DMA("sp", yb_d[b * 128:(b + 1) * 128, :], ysb[s], ("yst", s), [("ysb", s)], [("yb", b)])
            YB_KEYS.append(("yb", b))
        MARK("e2")
        for n in range(NT):
            for k in range(2):
                s = (2 * n + k) % 2
                idx_ap = dest_i[:, k, n:n + 1]
                dst = yg[s]
                P.dma("pool", lambda e, idx_ap=idx_ap, dst=dst: e.indirect_dma_start(
                    out=dst, out_offset=None, in_=yb_d[:, :], in_offset=IOA(ap=idx_ap, axis=0)),
                    ("yg", s), YB_KEYS + ["dest_i"], [("yg", s)], nbytes=512 * 1024)
                wk = rv[:, 4 + k, n:n + 1]
                STT(x1[:, n, :], yg[s], wk, x1[:, n, :], ALU.mult, ALU.add, [("yg", s), "rv", ("x1", n)], [("x1", n)])

        P.barrier()
        off = X1_END
        nf_bc, off = carve(off, [D], F32)
        ob = [None, None]
        ob[0], off = carve(off, [D], F32)
        ob[1], off = carve(off, [D], F32)
        junk2, off = carve(off, [D], BF16)
        DMA("sp", nf_bc, nf_d.partition_broadcast(128), "c_nf", [], ["nf_bc"])
        for n in range(NT):
            b = n % 2
            ssap = small[:, 12 + b:13 + b]
            ssk = ("ss3", b)
            ACTF(junk2, x1[:, n, :], AF.Square, [("x1", n)], ["junk2", ssk], accum_out=ssap)
            rstd_inplace(ssap, D, ssk)
            STT(ob[b], x1[:, n, :], ssap, nf_bc, ALU.mult, ALU.mult, [("x1", n), ssk, "nf_bc"], [("ob", b)])
            DMA("sp", out_d[n * 128:(n + 1) * 128, :], ob[b], ("out_st", b), [("ob", b)], [("out", n)])
        if not dbg:
            P.wait_all("sp", [("out", n) for n in range(NT)])
        if dbg:
            P.enabled = True
            P.barrier()
            for n in range(NT):
                DMA("sp", dbg_d[n * 128:(n + 1) * 128, :], x1[:, n, :], ("dbg_out", n % 2), [("x1", n)], [("dbg", n)])
            P.wait_all("sp", [("dbg", n) for n in range(NT)] + [("out", n) for n in range(NT)])
        P.emit()
    return nc


def make_in_maps(inputs, n_cores=8):
    f = lambda k: np.asarray(inputs[k], np.float32)
    x = f("x")
    _gu = np.concatenate([f("moe_w_gate")[0], f("moe_w_up")[0]], axis=2).reshape(32, 8, 128, 512).transpose(0, 2, 1, 3)
    shared = {
        "norm1_w": f("norm1_w").reshape(1, D),
        "norm2_w": f("norm2_w").reshape(1, D),
        "norm_f_w": f("norm_f_w").reshape(1, D),
        "consts": CONST_ARR,
        "w_in": np.ascontiguousarray(f("w_in")[0]),
        "gla_w2b_f": np.ascontiguousarray(np.concatenate([f("gla_gate_w2_fwd")[0], f("gla_gate_b_fwd")], axis=0)),
        "gla_w2b_b": np.ascontiguousarray(np.concatenate([f("gla_gate_w2_bwd")[0], f("gla_gate_b_bwd")], axis=0)),
        "gla_norm_w": f("gla_norm_w").reshape(1, 256),
        "w_out": np.ascontiguousarray(f("w_out")[0]),
        "moe_wr": np.ascontiguousarray(np.concatenate([f("moe_w_group")[0], f("moe_w_router")[0]], axis=1)),
        "moe_wgu0": _gu[:, :, 0:4, :].reshape(4096, 2048).copy(),
        "moe_wgu1": _gu[:, :, 4:8, :].reshape(4096, 2048).copy(),
        "moe_wdr": np.ascontiguousarray(f("moe_w_down")[0].reshape(32, 2, 128, 1024).transpose(0, 2, 1, 3)).reshape(4096, 2048),
        "gdn_norm_w": f("gdn_norm_w").reshape(1, 128),
        "gdn_vec": np.ascontiguousarray(np.concatenate([f("gdn_dt_bias_fwd")[0], f("gdn_dt_bias_bwd")[0],
                                                        f("gdn_a_log_fwd")[0], f("gdn_a_log_bwd")[0]]).reshape(1, 32)),
        "gdn_conv_wT": np.ascontiguousarray(f("gdn_conv_w")[0].T.reshape(24, 128, 5).transpose(1, 0, 2)),
    }
    maps = []
    for c in range(n_cores):
        m = dict(shared)
        m["x"] = np.ascontiguousarray(x[c])
        maps.append(m)
    return maps


def kernel(**inputs):
    nc = build()
    in_maps = make_in_maps(inputs)
    res = run_bass_kernel_spmd(nc, in_maps, core_ids=list(range(8)))
    out = np.stack([np.asarray(r["out"]) for r in res.results], axis=0)
    return out.astype(np.float32)
```

```python
import contextlib
import heapq
import numpy as np
import concourse.bass as bass
import concourse.mybir as mybir
from concourse.bass_utils import run_bass_kernel_spmd

F32 = mybir.dt.float32
BF16 = mybir.dt.bfloat16
I32 = mybir.dt.int32
AF = mybir.ActivationFunctionType
ALU = mybir.AluOpType
AX = mybir.AxisListType

T = 2048
D = 1024
NT = T // 128
KT = D // 128
EPS = 1e-6
SAME_ENGINE_SYNC = True
EPOCH = 20000
SYNC_NS = 120.0
DMA_LAT_NS = 2200.0


class Prog:
    ENGS = ("pe", "act", "dve", "pool", "sp")

    def __init__(self, nc, stack):
        self.nc = nc
        self.stack = stack
        self.streams = {e: [] for e in self.ENGS}
        self.count = {e: 0 for e in self.ENGS}
        self.esems = {e: [] for e in self.ENGS}
        self.known = {e: {} for e in self.ENGS}
        self.last_write = {}
        self.readers = {}
        self.dma_sems = {}
        self.dma_vals = {}
        self.dma_last = {}
        self.enabled = True
        self.seg = []
        self.ticks = {}
        self.nops = 0
        self.seg_base = 0
        self.pool_init = None

    def _new_sem(self, name):
        return self.stack.enter_context(self.nc.semaphore(name))

    @staticmethod
    def _psum_fix(reads, writes):
        r2, w2 = [], list(writes)
        for k in reads:
            if isinstance(k, tuple) and k[0] in ("pb", "pbb"):
                if k not in w2:
                    w2.append(k)
            else:
                r2.append(k)
        return r2, w2

    def _record(self, eng, fn, reads, writes, cost, kind, semkey=None):
        reads, writes = self._psum_fix(list(reads), list(writes))
        oid = self.nops
        self.nops += 1
        preds = set()
        for r in reads:
            t = self.last_write.get(r)
            if t is not None:
                preds.add(t)
        for w in writes:
            t = self.last_write.get(w)
            if t is not None:
                preds.add(t)
            preds.update(self.readers.get(w, ()))
        if kind == "dma":
            prev = self.dma_last.get(semkey)
            if prev is not None:
                preds.add(prev)
            self.dma_last[semkey] = oid
        preds = {p for p in preds if p >= self.seg_base}
        self.seg.append(dict(id=oid, eng=eng, fn=fn, preds=preds, cost=float(cost), kind=kind, semkey=semkey))
        for w in writes:
            self.last_write[w] = oid
            self.readers[w] = []
        for r in reads:
            self.readers.setdefault(r, []).append(oid)
        return oid

    def op(self, eng, fn, reads=(), writes=(), cost=300.0):
        if not self.enabled:
            return
        self._record(eng, fn, reads, writes, cost, "op")

    def dma(self, eng, fn, semkey, reads=(), writes=(), nbytes=1 << 20):
        if not self.enabled:
            return
        self._record(eng, fn, reads, writes, DMA_LAT_NS + nbytes / 160.0, "dma", semkey)

    def wait_all(self, eng, keys):
        self._record(eng, None, list(keys), [], 0.0, "op")

    def _schedule_segment(self):
        ops = self.seg
        if not ops:
            return
        byid = {o["id"]: o for o in ops}
        succ = {o["id"]: [] for o in ops}
        indeg = {}
        for o in ops:
            indeg[o["id"]] = len(o["preds"])
            for p in o["preds"]:
                succ[p].append(o["id"])
        ready_t = {o["id"]: 0.0 for o in ops}
        finish = {}
        heaps = {e: [] for e in self.ENGS}
        for o in ops:
            if indeg[o["id"]] == 0:
                heapq.heappush(heaps[o["eng"]], (0.0, o["id"]))
        etime = {e: 0.0 for e in self.ENGS}
        order = {e: [] for e in self.ENGS}
        remaining = len(ops)
        while remaining:
            best = None
            for e in self.ENGS:
                h = heaps[e]
                if not h:
                    continue
                rt, oid = h[0]
                st = max(rt, etime[e])
                if best is None or (st, oid) < (best[0], best[1]):
                    best = (st, oid, e)
            st, oid, e = best
            heapq.heappop(heaps[e])
            o = byid[oid]
            if o["kind"] == "dma":
                etime[e] = st + 150.0
                fin = st + o["cost"]
            else:
                etime[e] = st + o["cost"]
                fin = etime[e]
            finish[oid] = fin
            order[e].append(o)
            remaining -= 1
            for s in succ[oid]:
                so = byid[s]
                lat = SYNC_NS if (so["eng"] != e or o["kind"] == "dma") else (60.0 if e != "pe" else 0.0)
                ready_t[s] = max(ready_t[s], fin + lat)
                indeg[s] -= 1
                if indeg[s] == 0:
                    heapq.heappush(heaps[so["eng"]], (ready_t[s], s))
        self.est_ns = getattr(self, "est_ns", 0.0) + max(list(finish.values()) + [0.0])
        for o in ops:
            if o["kind"] == "dma":
                k = o["semkey"]
                if k not in self.dma_sems:
                    self.dma_sems[k] = self._new_sem(f"d{len(self.dma_sems)}")
                    self.dma_vals[k] = 0
                self.dma_vals[k] += 16
                self.ticks[o["id"]] = (self.dma_sems[k], self.dma_vals[k], "dma")
        def needs_sem(o):
            for s_ in succ[o["id"]]:
                se = byid[s_]["eng"]
                if se != o["eng"] or (SAME_ENGINE_SYNC and se != "pe"):
                    return True
            return False
        for e in self.ENGS:
            real = [o for o in order[e] if o["kind"] == "op" and o["fn"] is not None]
            for i_, o in enumerate(real):
                o["sig"] = needs_sem(o) or i_ == len(real) - 1
        for e in self.ENGS:
            for o in order[e]:
                if o["kind"] == "op" and o["fn"] is not None and o["sig"]:
                    c = self.count[e]
                    ep, v = divmod(c, EPOCH)
                    while len(self.esems[e]) <= ep:
                        self.esems[e].append(self._new_sem(f"s_{e}_{len(self.esems[e])}"))
                    self.count[e] = c + 1
                    self.ticks[o["id"]] = (self.esems[e][ep], v + 1, e)
        for e in self.ENGS:
            for o in order[e]:
                waits = {}
                for p in o["preds"]:
                    if byid[p]["eng"] == e and byid[p]["kind"] == "op" and (not SAME_ENGINE_SYNC or e == "pe"):
                        continue
                    sem, val, src = self.ticks[p]
                    sid = id(sem)
                    if self.known[e].get(sid, 0) >= val:
                        continue
                    if sid not in waits or waits[sid][1] < val:
                        waits[sid] = (sem, val)
                for sid, (sem, val) in waits.items():
                    self.known[e][sid] = val
                inc = None
                if o["fn"] is not None and o["id"] in self.ticks:
                    sem, val, src = self.ticks[o["id"]]
                    inc = (sem, 16 if o["kind"] == "dma" else 1)
                self.streams[e].append((o["fn"], list(waits.values()), inc))
        self.seg = []
        self.seg_base = self.nops

    def barrier(self):
        if not self.enabled and not self.seg:
            return
        self._schedule_segment()
        ticks = []
        for e2 in self.ENGS:
            c = self.count[e2]
            if c > 0:
                ep, v = divmod(c - 1, EPOCH)
                ticks.append((self.esems[e2][ep], v + 1))
        for k, sem in self.dma_sems.items():
            ticks.append((sem, self.dma_vals[k]))
        for eng in self.ENGS:
            waits = []
            for (sem, val) in ticks:
                if self.known[eng].get(id(sem), 0) >= val:
                    continue
                self.known[eng][id(sem)] = val
                waits.append((sem, val))
            if waits:
                self.streams[eng].append((None, waits, None))

    def emit(self):
        self._schedule_segment()
        nc = self.nc
        with nc.Block() as block:
            def run(e, stream):
                for fn, waits, inc in stream:
                    for sem, val in waits:
                        e.wait_ge(sem, val)
                    if fn is None:
                        continue
                    ins = fn(e)
                    if inc is not None:
                        ins.then_inc(inc[0], inc[1])

            @block.tensor
            def _(e):
                run(e, self.streams["pe"])

            @block.scalar
            def _(e):
                run(e, self.streams["act"])

            @block.vector
            def _(e):
                run(e, self.streams["dve"])

            @block.gpsimd
            def _(e):
                if self.pool_init is not None:
                    self.pool_init(e)
                run(e, self.streams["pool"])

            @block.sync
            def _(e):
                run(e, self.streams["sp"])


def _fsz(ap):
    s = ap.shape
    n = 1
    for v in s[1:]:
        n *= int(v)
    return n


C_GQ, C_GK, C_GV, C_GR = 0, 512, 1024, 2048
C_GLF, C_GLB = 3072, 3088
C_DQ, C_DK, C_DV, C_DZ = 3104, 4128, 5152, 6176
C_DAB = 7200
C_MA, C_MB = 7232, 8256
D_IN = 9280


def host_consts():
    r = np.arange(128)[:, None]
    t = np.arange(128)[None, :]
    same = (r // 64) == (t // 64)
    c = {}
    c["ident"] = np.eye(128, dtype=np.float32)
    c["a_le"] = np.where(r <= t, -1.0 / 16, 0.0)
    c["a_ge"] = np.where(r >= t, -1.0 / 16, 0.0)
    c["a_gt"] = np.where(r > t, -1.0 / 16, 0.0)
    c["a_lt"] = np.where(r < t, -1.0 / 16, 0.0)
    c["m_le"] = np.where(r <= t, 1.0, 0.0)
    c["m_ge"] = np.where(r >= t, 1.0, 0.0)
    c["b_le"] = np.where((r <= t) & same, 1.0, 0.0)
    c["b_ge"] = np.where((r >= t) & same, 1.0, 0.0)
    c["b_gt"] = np.where((r > t) & same, 1.0, 0.0)
    c["b_lt"] = np.where((r < t) & same, 1.0, 0.0)
    c["csel0"] = np.where(r < 64, 1.0, 0.0) + 0.0 * t
    c["csel1"] = np.where(r >= 64, 1.0, 0.0) + 0.0 * t
    c["ones"] = np.ones((128, 128))
    c["m_lt"] = np.where(r < t, 1.0, 0.0)
    c["bvals"] = 128.0 * t + 0.0 * r
    c["pidx"] = 1.0 * r + 0.0 * t
    names = list(c.keys())
    arr = np.stack([np.asarray(c[n], np.float32) for n in names], axis=1)
    return names, np.ascontiguousarray(arr)


CONST_NAMES, CONST_ARR = host_consts()
NCONST = len(CONST_NAMES)


def build(stage="all", dbg=False):
    nc = bass.Bass("TRN2", target_bir_lowering=False)
    stack = contextlib.ExitStack()
    with stack:
        P = Prog(nc, stack)

        def dram(name, shape, dt=F32, kind="ExternalInput"):
            return nc.dram_tensor(name, list(shape), dt, kind=kind).ap()

        def sb(name, shape, dt=F32):
            return stack.enter_context(nc.sbuf_tensor(name, list(shape), dt))

        def ps(name, shape, dt=F32):
            return stack.enter_context(nc.psum_tensor(name, list(shape), dt))

        def MM(out, lhsT, rhs, start, stop, R, W):
            n = _fsz(rhs)
            c = 70.0 + n * 0.75
            if rhs.dtype == F32:
                c *= 4.0
            P.op("pe", lambda e: e.matmul(out, lhsT, rhs, start=start, stop=stop), R, W, cost=c)

        def TR(out, in_, ident, R, W):
            P.op("pe", lambda e: e.transpose(out=out, in_=in_, identity=ident), R, W, cost=110.0)

        def ACTF(out, in_, func, R, W, **kw):
            c = 120.0 + _fsz(in_) * 0.6 + (90.0 if "accum_out" in kw else 0.0)
            P.op("act", lambda e: e.activation(out=out, in_=in_, func=func, **kw), R, W, cost=c)

        def _vc(eng, n, k=1.5):
            return (100.0 + n * k * 0.6) if eng == "dve" else (150.0 + n * 1.9)

        def TT(eng, out, in0, in1, op, R, W):
            P.op(eng, lambda e: e.tensor_tensor(out=out, in0=in0, in1=in1, op=op), R, W, cost=_vc(eng, _fsz(out)))

        def TS(eng, out, in0, s1, s2, op0, op1, R, W):
            P.op(eng, lambda e: e.tensor_scalar(out=out, in0=in0, scalar1=s1, scalar2=s2, op0=op0, op1=op1), R, W,
                 cost=_vc(eng, _fsz(out), 1.05))

        def STT(out, in0, scalar, in1, op0, op1, R, W):
            P.op("dve", lambda e: e.scalar_tensor_tensor(out=out, in0=in0, scalar=scalar, in1=in1, op0=op0, op1=op1), R, W,
                 cost=_vc("dve", _fsz(out)))

        def CP(eng, out, in_, R, W):
            if eng == "act":
                P.op("act", lambda e: e.activation(out=out, in_=in_, func=AF.Copy), R, W, cost=120.0 + _fsz(in_) * 0.6)
            else:
                P.op(eng, lambda e: e.tensor_copy(out=out, in_=in_), R, W, cost=_vc(eng, _fsz(out), 1.05))

        def MEMSET(eng, ap, val, W):
            P.op(eng, lambda e: e.memset(ap, val), [], W, cost=_vc(eng, _fsz(ap), 0.6))

        def DMA(eng, out, in_, semkey, R, W):
            P.dma(eng, lambda e: e.dma_start(out=out, in_=in_), semkey, R, W, nbytes=_fsz(out) * int(out.shape[0]) * 4)

        def RECIP(out, in_, R, W):
            P.op("dve", lambda e: e.reciprocal(out=out, in_=in_), R, W, cost=_vc("dve", _fsz(out), 1.05))

        def MARK(name):
            if stage == name:
                P.enabled = False

        def rstd_inplace(ap, n, key):
            TS("dve", ap, ap, 1.0 / n, EPS, ALU.mult, ALU.add, [key], [key])
            ACTF(ap, ap, AF.Ln, [key], [key])
            ACTF(ap, ap, AF.Exp, [key], [key], scale=-0.5)

        x_d = dram("x", [T, D])
        n1_d = dram("norm1_w", [1, D])
        n2_d = dram("norm2_w", [1, D])
        nf_d = dram("norm_f_w", [1, D])
        consts_d = dram("consts", [128, NCONST, 128])
        w_in_d = dram("w_in", [D, D_IN])
        w2b_d = [dram("gla_w2b_f", [17, 512]), dram("gla_w2b_b", [17, 512])]
        gnw_d = dram("gla_norm_w", [1, 256])
        out_d = dram("out", [T, D], kind="ExternalOutput")
        dbg_d = dram("dbg", [T, D], kind="ExternalOutput") if dbg else None

        consts = sb("consts_sb", [128, NCONST, 128])
        CI = {n: i for i, n in enumerate(CONST_NAMES)}

        def cst(name):
            return consts[:, CI[name], :]

        ident_b = sb("ident_b", [128, 128], BF16)
        ones_b = sb("ones_b", [128, 128], BF16)
        hT = sb("hT", [128, KT, T], BF16)
        mixed = sb("mixed", [128, NT, D], BF16)
        small = sb("small", [128, 64])
        ARENA_BYTES = 134 * 1024
        arena = sb("arena", [128, ARENA_BYTES // 4])

        def carve(off, shape, dt, base=None, cap=None):
            base = arena if base is None else base
            cap = ARENA_BYTES if cap is None else cap
            nb = int(np.prod(shape)) * (2 if dt == BF16 else 4)
            nb = (nb + 3) // 4 * 4
            assert off % 4 == 0 and off + nb <= cap, (off, nb)
            v = base[:, off // 4:(off + nb) // 4]
            if dt != F32:
                v = v.bitcast(dt)
            if len(shape) == 2:
                pat = "p (a b) -> p a b"
                v = v.rearrange(pat, a=shape[0])
            elif len(shape) == 3:
                v = v.rearrange("p (a b c) -> p a b c", a=shape[0], b=shape[1])
            return v, off + nb

        pt = [ps(f"pt{i}", [128, 1024]) for i in range(3)]
        ptb = ps("ptb", [128, 2048], BF16)

        def bank(i):
            return pt[i // 2][:, (i % 2) * 512:(i % 2 + 1) * 512]

        def bkey(i):
            return ("pb", i)

        def bbank(i):
            return ptb[:, i * 1024:(i + 1) * 1024]

        DMA("sp", consts[:], consts_d[:, :, :], "c_consts", [], ["consts"])
        CP("dve", ident_b[:], cst("ident"), ["consts"], ["ident_b"])
        MEMSET("pool", ones_b[:], 1.0, ["ones_b"])

        off = 0
        xt0, off = carve(off, [D], F32)
        xt1, off = carve(off, [D], F32)
        hn0, off = carve(off, [D], BF16)
        hn1, off = carve(off, [D], BF16)
        sq, off = carve(off, [D], F32)
        n1_bc, off = carve(off, [D], F32)
        DMA("sp", n1_bc, n1_d.partition_broadcast(128), "c_n1", [], ["n1_bc"])
        xts = [xt0, xt1]
        hns = [hn0, hn1]
        for tt in range(NT):
            b = tt % 2
            xb, hb = xts[b], hns[b]
            DMA("sp", xb, x_d[tt * 128:(tt + 1) * 128, :], ("xt", b), [], [("xt", b)])
            ACTF(sq, xb, AF.Square, [("xt", b)], ["sq", "ss0"], accum_out=small[:, 0:1])
            rstd_inplace(small[:, 0:1], D, "ss0")
            STT(hb, xb, small[:, 0:1], n1_bc, ALU.mult, ALU.mult, [("xt", b), "ss0", "n1_bc"], [("hn", b)])
            for kt in range(KT):
                TR(bbank(b)[:, kt * 128:(kt + 1) * 128], hb[:, kt * 128:(kt + 1) * 128], ident_b[:],
                   [("hn", b), "ident_b"], [("pbb", b)])
            CP("act", hT[:, :, tt * 128:(tt + 1) * 128], bbank(b).rearrange("p (k t) -> p k t", k=KT),
               [("pbb", b)], [("hT", tt)])
        HT_ALL = [("hT", tt) for tt in range(NT)]
        MARK("p1")

        P.barrier()
        off = 0
        qT, off = carve(off, [T], F32)
        kT, off = carve(off, [T], F32)
        k_tok, off = carve(off, [NT, 128], F32)
        v_tok, off = carve(off, [NT, 256], BF16)
        qdT = [None, None]
        kiT = [None, None]
        ktail = [None, None]
        for d_ in range(2):
            qdT[d_], off = carve(off, [T], BF16)
            kiT[d_], off = carve(off, [T], BF16)
            ktail[d_], off = carve(off, [NT, 128], BF16)
        sb_store, off = carve(off, [NT, 256], BF16)
        dec, off = carve(off, [2, NT], F32)
        S, off = carve(off, [256], F32)
        S_bf, off = carve(off, [256], BF16)
        NTMP = 4
        tmp = []
        for i in range(NTMP):
            d = {}
            for nm in ("e", "lg", "E", "Ei", "Et"):
                d[nm], off = carve(off, [128], F32)
            d["Pf"], off = carve(off, [128], BF16)
            d["Pb"], off = carve(off, [128], BF16)
            d["sig"], off = carve(off, [512], F32)
            d["G"], off = carve(off, [256], F32)
            tmp.append(d)
        gl, off = carve(off, [2, T], BF16)
        w2b, off = carve(off, [2, 512], BF16)
        wqk, off = carve(off, [KT, 256], BF16)
        wkv, off = carve(off, [KT, 384], BF16)
        wgm, off = carve(off, [KT, 512], BF16)
        wgl, off = carve(off, [KT, 32], BF16)
        gnw_bc, off = carve(off, [256], F32)
        GLA_END = off

        DMA("sp", gnw_bc, gnw_d.partition_broadcast(128), "c_gnw", [], ["gnw_bc"])
        MEMSET("pool", gl[0:32, :, :], 1.0, ["gl"])
        MEMSET("pool", w2b[0:32, :, :], 0.0, ["w2b"])
        for d_ in range(2):
            DMA("pool", w2b[0:17, d_, :], w2b_d[d_][:, :], "c_w2b", [], ["w2b"])
        DMA("pool", wgl, w_in_d[:, C_GLF:C_GLF + 32].rearrange("(k p) c -> p k c", p=128), "w_wgl", [], ["wgl"])
        for d_ in range(2):
            for tg in range(4):
                bi = tg % 2
                for kt in range(KT):
                    MM(bank(bi)[0:16, :], wgl[:, kt, d_ * 16:(d_ + 1) * 16], hT[:, kt, tg * 512:(tg + 1) * 512],
                       kt == 0, kt == KT - 1, ["wgl"] + HT_ALL[tg * 4:tg * 4 + 4], [bkey(bi)])
                CP("act", gl[0:16, d_, tg * 512:(tg + 1) * 512], bank(bi)[0:16, :], [bkey(bi)], ["gl"])

        MARK("g0")
        QSCALE = 128.0 ** -0.5
        for h in range(4):
            def wcols(dst, c0, n):
                return (dst, w_in_d[:, c0:c0 + n].rearrange("(k p) c -> p k c", p=128))
            for (dst, src) in (wcols(wqk[:, :, 0:128], C_GQ + h * 128, 128), wcols(wqk[:, :, 128:256], C_GK + h * 128, 128)):
                DMA("pool", dst, src, "w_wqk", [], ["wqk"])
            for (dst, src) in (wcols(wkv[:, :, 0:128], C_GK + h * 128, 128), wcols(wkv[:, :, 128:384], C_GV + h * 256, 256)):
                DMA("pool", dst, src, "w_wkv", [], ["wkv"])
            for (dst, src) in (wcols(wgm[:, :, 0:256], C_GR + h * 256, 256), wcols(wgm[:, :, 256:512], C_MA + h * 256, 256)):
                DMA("pool", dst, src, "w_wgm", [], ["wgm"])
            MARK("g1a")
            for which, dstT in ((0, qT), (1, kT)):
                for tg in range(4):
                    bi = (which * 4 + tg) % 4
                    for kt in range(KT):
                        MM(bank(bi), wqk[:, kt, which * 128:(which + 1) * 128], hT[:, kt, tg * 512:(tg + 1) * 512],
                           kt == 0, kt == KT - 1, ["wqk"] + HT_ALL[tg * 4:tg * 4 + 4], [bkey(bi)])
                    CP("act" if tg % 2 else "dve", dstT[:, tg * 512:(tg + 1) * 512], bank(bi), [bkey(bi)],
                       [("qkT", which, tg)])
            MARK("g1b")
            for n in range(NT):
                bi = 4 + n % 2
                for kt in range(KT):
                    MM(bank(bi)[:, 0:384], hT[:, kt, n * 128:(n + 1) * 128], wkv[:, kt, :],
                       kt == 0, kt == KT - 1, ["wkv", ("hT", n)], [bkey(bi)])
                CP("dve", k_tok[:, n, :], bank(bi)[:, 0:128], [bkey(bi)], [("k_tok", n)])
                CP("act", v_tok[:, n, :], bank(bi)[:, 128:384], [bkey(bi)], [("v_tok", n)])
            MARK("g1")
            for n in range(NT):
                tsl = slice(n * 128, (n + 1) * 128)
                tg = n // 4
                for d_ in range(2):
                    tm = tmp[(n * 2 + d_) % NTMP]
                    tk = ("gtmp", (n * 2 + d_) % NTMP)
                    a_c = cst("a_le") if d_ == 0 else cst("a_ge")
                    a_s = cst("a_gt") if d_ == 0 else cst("a_lt")
                    b0 = (n * 2 + d_) % 2 * 2
                    zb, cb = bank(b0), bank(b0 + 1)
                    MM(zb[:, 0:128], gl[0:32, d_, tsl], w2b[0:32, d_, h * 128:(h + 1) * 128], True, True,
                       ["gl", "w2b"], [bkey(b0)])
                    ACTF(tm["e"], zb[:, 0:128], AF.Exp, [bkey(b0)], [tk], scale=-1.0)
                    ACTF(tm["lg"], tm["e"], AF.Ln, [tk], [tk], bias=1.0)
                    MM(cb[:, 0:128], tm["lg"], a_c, True, True, [tk, "consts"], [bkey(b0 + 1)])
                    MM(cb[:, 128:256], a_s, tm["lg"], True, True, [tk, "consts"], [bkey(b0 + 1)])
                    ACTF(tm["E"], cb[:, 0:128], AF.Exp, [bkey(b0 + 1)], [tk])
                    ACTF(tm["Ei"], cb[:, 0:128], AF.Exp, [bkey(b0 + 1)], [tk], scale=-1.0)
                    ACTF(tm["Et"], cb[:, 128:256], AF.Exp, [bkey(b0 + 1)], [tk])
                    STT(qdT[d_][:, tsl], qT[:, tsl], QSCALE, tm["E"], ALU.mult, ALU.mult,
                        [("qkT", 0, tg), tk], [("qdT", d_, n)])
                    TT("dve", kiT[d_][:, tsl], kT[:, tsl], tm["Ei"], ALU.mult, [("qkT", 1, tg), tk], [("kiT", d_, n)])
                    TT("dve", ktail[d_][:, n, :], k_tok[:, n, :], tm["Et"], ALU.mult, [("k_tok", n), tk], [("ktail", d_, n)])
                    col = 127 if d_ == 0 else 0
                    CP("dve", dec[:, d_, n:n + 1], tm["E"][:, col:col + 1], [tk], [("dec", d_, n)])
            MARK("g2")
            MEMSET("dve", S, 0.0, ["S"])
            for n in range(NT - 1, -1, -1):
                CP("act", sb_store[:, n, :], S, ["S"], [("sb_store", n)])
                bi = 4 + n % 2
                MM(bank(bi)[:, 0:256], ktail[1][:, n, :], v_tok[:, n, :], True, True,
                   [("ktail", 1, n), ("v_tok", n)], [bkey(bi)])
                STT(S, S, dec[:, 1, n:n + 1], bank(bi)[:, 0:256], ALU.mult, ALU.add,
                    ["S", ("dec", 1, n), bkey(bi)], ["S"])
            MARK("g3")
            MEMSET("dve", S, 0.0, ["S"])
            for n in range(NT):
                tsl = slice(n * 128, (n + 1) * 128)
                tm = tmp[n % NTMP]
                tk = ("ftmp", n % NTMP)
                CP("act", S_bf, S, ["S"], ["S_bf"])
                b0 = (n % 2) * 2
                sc = bank(b0)
                MM(sc[:, 0:128], kiT[0][:, tsl], qdT[0][:, tsl], True, True, [("kiT", 0, n), ("qdT", 0, n)], [bkey(b0)])
                MM(sc[:, 128:256], kiT[1][:, tsl], qdT[1][:, tsl], True, True, [("kiT", 1, n), ("qdT", 1, n)], [bkey(b0)])
                TT("dve", tm["Pf"], sc[:, 0:128], cst("m_le"), ALU.mult, [bkey(b0), "consts"], [tk])
                TT("dve", tm["Pb"], sc[:, 128:256], cst("m_ge"), ALU.mult, [bkey(b0), "consts"], [tk])
                ob = bank(b0 + 1)
                ok = bkey(b0 + 1)
                MM(ob[:, 0:256], qdT[0][:, tsl], S_bf, True, False, [("qdT", 0, n), "S_bf"], [ok])
                MM(ob[:, 0:256], qdT[1][:, tsl], sb_store[:, n, :], False, False, [("qdT", 1, n), ("sb_store", n)], [ok])
                MM(ob[:, 0:256], tm["Pf"], v_tok[:, n, :], False, False, [tk, ("v_tok", n)], [ok])
                MM(ob[:, 0:256], tm["Pb"], v_tok[:, n, :], False, True, [tk, ("v_tok", n)], [ok])
                kb = 4 + n % 2
                MM(bank(kb)[:, 0:256], ktail[0][:, n, :], v_tok[:, n, :], True, True,
                   [("ktail", 0, n), ("v_tok", n)], [bkey(kb)])
                STT(S, S, dec[:, 0, n:n + 1], bank(kb)[:, 0:256], ALU.mult, ALU.add,
                    ["S", ("dec", 0, n), bkey(kb)], ["S"])
                gb = 4 + n % 2
                for kt in range(KT):
                    MM(bank(gb), hT[:, kt, tsl], wgm[:, kt, :], kt == 0, kt == KT - 1, ["wgm", ("hT", n)], [bkey(gb)])
                ACTF(tm["sig"], bank(gb), AF.Exp, [bkey(gb)], [("sig", n % NTMP)], scale=-1.0)
                ACTF(tm["sig"], tm["sig"], AF.Ln, [("sig", n % NTMP)], [("sig", n % NTMP)], bias=1.0)
                ACTF(tm["sig"], tm["sig"], AF.Exp, [("sig", n % NTMP)], [("sig", n % NTMP)], scale=-1.0)
                TT("pool", tm["G"], tm["sig"][:, 0:256], tm["sig"][:, 256:512], ALU.mult, [("sig", n % NTMP)], [("G", n % NTMP)])
                TT("dve", tm["G"], tm["G"], bank(gb)[:, 0:256], ALU.mult, [("G", n % NTMP), bkey(gb)], [("G", n % NTMP)])
                TT("pool", tm["G"], tm["G"], gnw_bc, ALU.mult, [("G", n % NTMP), "gnw_bc"], [("G", n % NTMP)])
                ssk = ("ssq", n % 2)
                ssap = small[:, 2 + n % 2:3 + n % 2]
                ACTF(tm["sig"][:, 0:256], ob[:, 0:256], AF.Square, [ok, ("G", n % NTMP)], [("sig", n % NTMP), ssk],
                     accum_out=ssap)
                rstd_inplace(ssap, 256, ssk)
                STT(mixed[:, n, h * 256:(h + 1) * 256], ob[:, 0:256], ssap, tm["G"], ALU.mult, ALU.mult,
                    [ok, ssk, ("G", n % NTMP)], [("mixed", n)])

        P.barrier()
        HG = 4
        off = 0
        gqT, off = carve(off, [HG, T], BF16)
        gkT, off = carve(off, [HG, T], BF16)
        gvT, off = carve(off, [HG, T], BF16)
        o_store, off = carve(off, [NT, HG, 128], BF16)
        dabs, off = carve(off, [NT, 32], F32)
        g_raw, off = carve(off, [NT, 2, 8], F32)
        beta, off = carve(off, [NT, 2, 8], F32)
        gvec, off = carve(off, [64], F32)
        wsl0, off = carve(off, [KT, 512], BF16)
        wsl1, off = carve(off, [KT, 512], BF16)
        wsl = [wsl0, wsl1]
        cwT, off = carve(off, [24, 5], F32)
        gdnw_bc, off = carve(off, [128], F32)
        wdab, off = carve(off, [KT, 32], BF16)
        TMP0 = off
        xc = [None, None]
        xc[0], off = carve(off, [T + 4], BF16)
        xc[1], off = carve(off, [T + 4], BF16)
        diag, off = carve(off, [5, 128], BF16)
        ce = [None, None]
        cy = [None, None]
        for i in range(2):
            ce[i], off = carve(off, [512], F32)
            cy[i], off = carve(off, [512], F32)
        cysq, off = carve(off, [512], BF16)
        crs, off = carve(off, [512], F32)
        CONV_END = off
        off = TMP0
        GMB, off = carve(off, [HG, 128], F32)
        Wd, off = carve(off, [HG, 128], F32)
        decT, off = carve(off, [HG, 128], F32)
        Lm, off = carve(off, [HG, 128], BF16)
        LTm, off = carve(off, [HG, 128], BF16)
        XT, off = carve(off, [HG, 128], BF16)
        Pp = [None, None]
        PTp = [None, None]
        for i in range(2):
            Pp[i], off = carve(off, [HG, 128], BF16)
            PTp[i], off = carve(off, [HG, 128], BF16)
        kbg, off = carve(off, [HG, 128], BF16)
        vbeta, off = carve(off, [HG, 128], BF16)
        qd_tok, off = carve(off, [HG, 128], BF16)
        DB = []
        for d_ in range(2):
            dd = {}
            for nm in ("attnT", "ktl", "qdTg", "wT_sb", "vnew", "Sg_bf"):
                dd[nm], off = carve(off, [HG, 128], BF16)
            dd["u_sb"], off = carve(off, [HG, 128], F32)
            dd["Sg"], off = carve(off, [HG, 128], F32)
            dd["esc"], off = carve(off, [16], F32)
            dd["bg"], off = carve(off, [HG], F32)
            DB.append(dd)
        osum, off = carve(off, [HG, 128], F32)
        fsig, off = carve(off, [1024], F32)
        fG, off = carve(off, [HG, 128], F32)
        frs, off = carve(off, [8], F32)
        SWEEP_END = off

        gdnw_d = dram("gdn_norm_w", [1, 128])
        gvec_d = dram("gdn_vec", [1, 32])
        cw_d = dram("gdn_conv_wT", [128, 24, 5])
        DMA("sp", gdnw_bc, gdnw_d.partition_broadcast(128), "c_gdnw", [], ["gdnw_bc"])
        DMA("sp", gvec[:, 0:32], gvec_d.partition_broadcast(128), "c_gvec", [], ["gvec"])
        DMA("sp", cwT, cw_d[:, :, :], "c_cw", [], ["cwT"])
        DMA("pool", wdab, w_in_d[:, C_DAB:C_DAB + 32].rearrange("(k p) c -> p k c", p=128), "w_wdab", [], ["wdab"])
        ACTF(gvec[:, 16:32], gvec[:, 16:32], AF.Exp, ["gvec"], ["gvec"])
        TS("dve", gvec[:, 16:32], gvec[:, 16:32], -1.0, None, ALU.mult, ALU.bypass, ["gvec"], ["gvec"])
        for n in range(NT):
            bi = n % 2
            for kt in range(KT):
                MM(bank(bi)[:, 0:32], hT[:, kt, n * 128:(n + 1) * 128], wdab[:, kt, :], kt == 0, kt == KT - 1,
                   ["wdab", ("hT", n)], [bkey(bi)])
            CP("act", dabs[:, n, :], bank(bi)[:, 0:32], [bkey(bi)], ["dabs"])
        a_view = dabs[:, :, 0:16]
        b_view = dabs[:, :, 16:32]
        g_flat = g_raw.rearrange("p n d h -> p n (d h)")
        be_flat = beta.rearrange("p n d h -> p n (d h)")
        TT("dve", g_flat, a_view, gvec[:, 0:16].unsqueeze(1).to_broadcast([128, NT, 16]), ALU.add, ["dabs", "gvec"], ["g_raw"])
        ACTF(g_flat, g_flat, AF.Exp, ["g_raw"], ["g_raw"])
        ACTF(g_flat, g_flat, AF.Ln, ["g_raw"], ["g_raw"], bias=1.0)
        TT("dve", g_flat, g_flat, gvec[:, 16:32].unsqueeze(1).to_broadcast([128, NT, 16]), ALU.mult, ["g_raw", "gvec"], ["g_raw"])
        ACTF(be_flat, b_view, AF.Exp, ["dabs"], ["beta"], scale=-1.0)
        TS("dve", be_flat, be_flat, 1.0, None, ALU.add, ALU.bypass, ["beta"], ["beta"])
        RECIP(be_flat, be_flat, ["beta"], ["beta"])
        MARK("d0")

        GSCALE = 128.0 ** -0.5
        ident_bc4 = ident_b[:].unsqueeze(1).to_broadcast([128, HG, 128])

        def bc_h(ap2):
            return ap2.unsqueeze(2).to_broadcast([128, HG, 128])

        def bc_m(ap2):
            return ap2.unsqueeze(1).to_broadcast([128, HG, 128])

        def v4(ap2):
            return ap2.rearrange("p (h d) -> p h d", h=HG)

        for grp in range(2):
            hs0 = grp * HG
            for which, c_base, dstT in ((0, C_DQ, gqT), (1, C_DK, gkT), (2, C_DV, gvT)):
                ws = wsl[which % 2]
                wk = ("wsl", which % 2)
                DMA("pool", ws, w_in_d[:, c_base + hs0 * 128:c_base + (hs0 + HG) * 128].rearrange("(k p) c -> p k c", p=128),
                    ("w_wsl", which % 2), [], [wk])
                for hh in range(HG):
                    ci = which * 8 + hs0 + hh
                    xi = (which * HG + hh) % 2
                    xcb = xc[xi]
                    xk = ("xc", xi)
                    MEMSET("pool", xcb[:, 0:2], 0.0, [xk])
                    MEMSET("pool", xcb[:, T + 2:T + 4], 0.0, [xk])
                    for k in range(5):
                        TS("dve", diag[:, k, :], cst("ident"), cwT[:, ci, k:k + 1], None, ALU.mult, ALU.bypass,
                           ["consts", "cwT"], ["diag"])
                    for tg in range(4):
                        bi = tg % 2
                        for kt in range(KT):
                            MM(bank(bi), ws[:, kt, hh * 128:(hh + 1) * 128], hT[:, kt, tg * 512:(tg + 1) * 512],
                               kt == 0, kt == KT - 1, [wk] + HT_ALL[tg * 4:tg * 4 + 4], [bkey(bi)])
                        CP("act" if tg % 2 else "dve", xcb[:, 2 + tg * 512:2 + (tg + 1) * 512], bank(bi), [bkey(bi)], [xk])
                    for tg in range(4):
                        bi = 2 + tg % 2
                        i2 = tg % 2
                        for k in range(5):
                            MM(bank(bi), diag[:, k, :], xcb[:, tg * 512 + k:tg * 512 + k + 512], k == 0, k == 4,
                               ["diag", xk], [bkey(bi)])
                        ck = ("ctmp", i2)
                        ACTF(ce[i2], bank(bi), AF.Exp, [bkey(bi)], [ck], scale=-1.0)
                        ACTF(ce[i2], ce[i2], AF.Ln, [ck], [ck], bias=1.0)
                        ACTF(ce[i2], ce[i2], AF.Exp, [ck], [ck], scale=-1.0)
                        dst = dstT[:, hh, tg * 512:(tg + 1) * 512]
                        dk = ("gT", which, hh, tg)
                        if which == 2:
                            TT("dve", dst, ce[i2], bank(bi), ALU.mult, [ck, bkey(bi)], [dk])
                        else:
                            TT("dve", cy[i2], ce[i2], bank(bi), ALU.mult, [ck, bkey(bi)], [("cy", i2)])
                            TT("pool", cysq, cy[i2], cy[i2], ALU.mult, [("cy", i2)], ["cysq"])
                            MM(bank(4), ones_b[:], cysq, True, True, ["ones_b", "cysq"], [bkey(4)])
                            ACTF(crs, bank(4), AF.Ln, [bkey(4)], ["crs"], bias=EPS)
                            ACTF(crs, crs, AF.Exp, ["crs"], ["crs"], scale=-0.5)
                            if which == 0:
                                STT(dst, cy[i2], GSCALE, crs, ALU.mult, ALU.mult, [("cy", i2), "crs"], [dk])
                            else:
                                TT("dve", dst, cy[i2], crs, ALU.mult, [("cy", i2), "crs"], [dk])
            MARK("d1")
            P.barrier()
            DMA("pool", wsl[0], w_in_d[:, C_DZ + hs0 * 128:C_DZ + (hs0 + HG) * 128].rearrange("(k p) c -> p k c", p=128),
                ("w_wsl", 0), [], [("wsl", 0)])
            DMA("pool", wsl[1], w_in_d[:, C_MB + hs0 * 128:C_MB + (hs0 + HG) * 128].rearrange("(k p) c -> p k c", p=128),
                ("w_wsl", 1), [], [("wsl", 1)])

            def gT_keys(which, n):
                return [("gT", which, hh, n // 4) for hh in range(HG)]

            stored = set()
            esc_all = [dabs[:, 0:8, :].rearrange("p a b -> p (a b)").rearrange("p (k n h) -> p k n h", k=4, n=NT),
                       dabs[:, 8:16, :].rearrange("p a b -> p (a b)").rearrange("p (k n h) -> p k n h", k=4, n=NT)]
            for d_ in range(2):
                Mc_ = cst("b_le") if d_ == 0 else cst("b_ge")
                Ms_ = cst("b_gt") if d_ == 0 else cst("b_lt")
                for ki, mk in enumerate((Mc_, Ms_, cst("csel0"), cst("csel1"))):
                    MM(bank(d_)[:, ki * 64:(ki + 1) * 64].rearrange("p (n h) -> p n h", n=NT), mk,
                       g_raw[:, :, d_, hs0:hs0 + HG], True, True, ["consts", "g_raw"], [bkey(d_)])
                ACTF(esc_all[d_].rearrange("p k n h -> p (k n h)"), bank(d_)[:, 0:256], AF.Exp, [bkey(d_)], [("esc_all", d_), "dabs"])

            gb0 = ptb[:, 0:512]
            gb1 = ptb[:, 512:1024]
            xbank = ptb[:, 1024:2048].bitcast(F32)
            XK = ("pb", 7)

            def gdn_tile(d_, n):
                B = DB[d_]
                dk = lambda nm: (nm, d_)
                Mc = cst("b_le") if d_ == 0 else cst("b_ge")
                Ms = cst("b_gt") if d_ == 0 else cst("b_lt")
                esc, bg = B["esc"], B["bg"]
                tsl = slice(n * 128, (n + 1) * 128)
                gv = g_raw[:, n, d_, hs0:hs0 + HG]
                bv = beta[:, n, d_, hs0:hs0 + HG]
                EA = esc_all[d_]
                e_cum, e_tail = EA[:, 0, n, :], EA[:, 1, n, :]
                TT("dve", bg, bv, e_cum, ALU.mult, ["beta", ("esc_all", d_)], [dk("bg")])
                for hh in range(HG):
                    TR(gb0[:, hh * 128:(hh + 1) * 128], gkT[:, hh, tsl], ident_b[:], gT_keys(1, n) + ["ident_b"], [("pbb", 0)])
                for hh in range(HG):
                    TR(gb1[:, hh * 128:(hh + 1) * 128], gvT[:, hh, tsl], ident_b[:], gT_keys(2, n) + ["ident_b"], [("pbb", 0)])
                TT("dve", kbg, v4(gb0), bc_h(bg), ALU.mult, [("pbb", 0), dk("bg")], ["kbg"])
                TT("dve", B["ktl"], v4(gb0), bc_h(e_tail), ALU.mult, [("pbb", 0), ("esc_all", d_)], [dk("ktl")])
                TT("dve", vbeta, v4(gb1), bc_h(bv), ALU.mult, [("pbb", 0), "beta"], ["vbeta"])
                for hh in range(HG):
                    TR(gb0[:, hh * 128:(hh + 1) * 128], gqT[:, hh, tsl], ident_b[:], gT_keys(0, n) + ["ident_b"], [("pbb", 0)])
                TT("dve", qd_tok, v4(gb0), bc_h(e_cum), ALU.mult, [("pbb", 0), ("esc_all", d_)], ["qd_tok"])
                for hh in range(HG):
                    TR(gb1[:, hh * 128:(hh + 1) * 128], qd_tok[:, hh, :], ident_b[:], ["qd_tok", "ident_b"], [("pbb", 0)])
                CP("act", B["qdTg"], v4(gb1), [("pbb", 0)], [dk("qdTg")])
                TT("pool", GMB, bc_h(gv), bc_m(Ms), ALU.mult, ["g_raw", "consts"], ["GMB"])
                MM(bank(1), Mc, GMB.rearrange("p h s -> p (h s)"), True, True, ["consts", "GMB"], [bkey(1)])
                ACTF(Wd.rearrange("p h s -> p (h s)"), bank(1), AF.Exp, [bkey(1)], ["Wd"])
                TT("pool", GMB, bc_h(bv), bc_m(Ms), ALU.mult, ["beta", "consts"], ["GMB"])
                TT("pool", Wd, Wd, GMB, ALU.mult, ["Wd", "GMB"], ["Wd"])
                TT("pool", GMB, bc_h(gv), bc_m(Mc), ALU.mult, ["g_raw", "consts"], ["GMB"])
                MM(bank(0), Ms, GMB.rearrange("p h s -> p (h s)"), True, True, ["consts", "GMB"], [bkey(0)])
                ACTF(decT.rearrange("p h s -> p (h s)"), bank(0), AF.Exp, [bkey(0)], ["decT"])
                TT("pool", decT, decT, bc_m(Mc), ALU.mult, ["decT", "consts"], ["decT"])
                for hh in range(HG):
                    MM(bank(1)[:, hh * 128:(hh + 1) * 128], gkT[:, hh, tsl], gkT[:, hh, tsl], True, True,
                       gT_keys(1, n), [bkey(1)])
                TT("dve", Lm, v4(bank(1)), Wd, ALU.mult, [bkey(1), "Wd"], ["Lm"])
                for hh in range(HG):
                    MM(bank(0)[:, hh * 128:(hh + 1) * 128], gkT[:, hh, tsl], gqT[:, hh, tsl], True, True,
                       gT_keys(1, n) + gT_keys(0, n), [bkey(0)])
                TT("dve", B["attnT"], v4(bank(0)), decT, ALU.mult, [bkey(0), "decT"], [dk("attnT")])
                for hh in range(HG):
                    TR(gb0[:, hh * 128:(hh + 1) * 128], Lm[:, hh, :], ident_b[:], ["Lm", "ident_b"], [("pbb", 0)])
                CP("act", LTm, v4(gb0), [("pbb", 0)], ["LTm"])
                TT("dve", XT, ident_bc4, v4(gb0), ALU.subtract, ["ident_b", ("pbb", 0)], ["XT"])
                Pc, PTc = Lm, LTm
                pck, ptk_ = "Lm", "LTm"
                for it in range(5):
                    Pn, PTn = Pp[it % 2], PTp[it % 2]
                    pnk, ptnk = ("Pp", it % 2), ("PTp", it % 2)
                    for hh in range(HG):
                        MM(bank(1)[:, hh * 128:(hh + 1) * 128], PTc[:, hh, :], Pc[:, hh, :], True, True, [pck, ptk_], [bkey(1)])
                    CP("act", Pn, v4(bank(1)), [bkey(1)], [pnk])
                    if it < 4:
                        for hh in range(HG):
                            MM(bank(0)[:, hh * 128:(hh + 1) * 128], Pc[:, hh, :], PTc[:, hh, :], True, True, [pck, ptk_], [bkey(0)])
                        CP("dve", PTn, v4(bank(0)), [bkey(0)], [ptnk])
                    for hh in range(HG):
                        MM(xbank[:, hh * 128:(hh + 1) * 128], Pn[:, hh, :], XT[:, hh, :], True, True, [pnk, "XT"], [XK])
                    TT("dve", XT, XT, v4(xbank), ALU.add, ["XT", XK], ["XT"])
                    Pc, PTc, pck, ptk_ = Pn, PTn, pnk, ptnk
                for hh in range(HG):
                    MM(bank(0)[:, hh * 128:(hh + 1) * 128], XT[:, hh, :], vbeta[:, hh, :], True, True, ["XT", "vbeta"], [bkey(0)])
                CP("act", B["u_sb"], v4(bank(0)), [bkey(0)], [dk("u_sb")])
                for hh in range(HG):
                    MM(bank(1)[:, hh * 128:(hh + 1) * 128], kbg[:, hh, :], XT[:, hh, :], True, True, ["kbg", "XT"], [bkey(1)])
                CP("dve", B["wT_sb"], v4(bank(1)), [bkey(1)], [dk("wT_sb")])
                sb0 = 4 if d_ == 0 else 2
                Sg, Sg_bf, vnew = B["Sg"], B["Sg_bf"], B["vnew"]
                chunks = (0, 1) if d_ == 0 else (1, 0)
                for c in chunks:
                    sl = slice(c * 64, c * 64 + 64)
                    for hh in range(HG):
                        MM(bank(sb0)[sl, hh * 128:(hh + 1) * 128], B["wT_sb"][:, hh, sl], Sg_bf[:, hh, :], True, True,
                           [dk("wT_sb"), dk("Sg_bf")], [bkey(sb0)])
                    TT("dve", vnew[sl], B["u_sb"][sl], v4(bank(sb0))[sl], ALU.subtract, [dk("u_sb"), bkey(sb0)], [dk("vnew")])
                    for hh in range(HG):
                        MM(bank(sb0 + 1)[sl, hh * 128:(hh + 1) * 128], B["qdTg"][:, hh, sl], Sg_bf[:, hh, :], True, False,
                           [dk("qdTg"), dk("Sg_bf")], [bkey(sb0 + 1)])
                        MM(bank(sb0 + 1)[sl, hh * 128:(hh + 1) * 128], B["attnT"][sl, hh, sl], vnew[sl, hh, :], False, True,
                           [dk("attnT"), dk("vnew")], [bkey(sb0 + 1)])
                    for hh in range(HG):
                        MM(bank(sb0)[:, hh * 128:(hh + 1) * 128], B["ktl"][sl, hh, :], vnew[sl, hh, :], True, True,
                           [dk("ktl"), dk("vnew")], [bkey(sb0)])
                    TT("pool", Sg, Sg, bc_h(EA[:, 2 + c, n, :]), ALU.mult, [dk("Sg"), ("esc_all", d_)], [dk("Sg")])
                    TT("dve", Sg, Sg, v4(bank(sb0)), ALU.add, [dk("Sg"), bkey(sb0)], [dk("Sg")])
                    CP("act", Sg_bf, Sg, [dk("Sg")], [dk("Sg_bf")])
                    if n not in stored:
                        CP("act", o_store[sl, n], v4(bank(sb0 + 1))[sl], [bkey(sb0 + 1)], [("o_store", n)])
                    else:
                        TT("dve", osum[sl], v4(bank(sb0 + 1))[sl], o_store[sl, n], ALU.add, [bkey(sb0 + 1), ("o_store", n)], ["osum"])
                if n not in stored:
                    stored.add(n)
                    return
                osq = fsig[:, 0:512].rearrange("p (h d) -> p h d", h=HG)
                TT("pool", osq, osum, osum, ALU.mult, ["osum"], ["fsig"])
                P.op("dve", lambda e: e.tensor_reduce(out=frs[:, 0:HG], in_=osq, axis=AX.X, op=ALU.add), ["fsig"], ["frs"], cost=600.0)
                rstd_inplace(frs[:, 0:HG], 128, "frs")
                for half, ws in enumerate(wsl):
                    for kt in range(KT):
                        MM(bank(half), hT[:, kt, tsl], ws[:, kt, :], kt == 0, kt == KT - 1,
                           [("wsl", half), ("hT", n)], [bkey(half)])
                zm = pt[0][:, :]
                ACTF(fsig, zm, AF.Exp, [bkey(0), bkey(1)], ["fsig"], scale=-1.0)
                ACTF(fsig, fsig, AF.Ln, ["fsig"], ["fsig"], bias=1.0)
                ACTF(fsig, fsig, AF.Exp, ["fsig"], ["fsig"], scale=-1.0)
                TT("pool", fG.rearrange("p h d -> p (h d)"), fsig[:, 0:512], fsig[:, 512:1024], ALU.mult, ["fsig"], ["fG"])
                TT("dve", fG, fG, v4(bank(0)), ALU.mult, ["fG", bkey(0)], ["fG"])
                TT("pool", fG, fG, bc_m(gdnw_bc), ALU.mult, ["fG", "gdnw_bc"], ["fG"])
                TT("pool", osum, osum, bc_h(frs[:, 0:HG]), ALU.mult, ["osum", "frs"], ["osum"])
                TT("pool", osum, osum, fG, ALU.mult, ["osum", "fG"], ["osum"])
                mslice = mixed[:, n, hs0 * 128:(hs0 + HG) * 128].rearrange("p (h d) -> p h d", h=HG)
                TT("dve", mslice, mslice, osum, ALU.add, ["osum", ("mixed", n)], [("mixed", n)])

            for d_ in range(2):
                MEMSET("dve", DB[d_]["Sg"], 0.0, [("Sg", d_)])
                CP("act", DB[d_]["Sg_bf"], DB[d_]["Sg"], [("Sg", d_)], [("Sg_bf", d_)])
            for i in range(NT):
                gdn_tile(0, i)
                gdn_tile(1, NT - 1 - i)
            MARK("d2")
            P.barrier()

        P.barrier()
        wout_d = dram("w_out", [D, D])
        off = 0
        x1, off = carve(off, [NT, D], F32)
        X1_END = off
        mT, off = carve(off, [KT, T], BF16)
        wout, off = carve(off, [KT, D], BF16)
        hn2 = [None, None]
        hn2[0], off = carve(off, [D], BF16)
        hn2[1], off = carve(off, [D], BF16)
        junk, off = carve(off, [D], BF16)
        n2_bc, off = carve(off, [D], F32)
        DMA("sp", n2_bc, n2_d.partition_broadcast(128), "c_n2", [], ["n2_bc"])
        DMA("pool", wout, wout_d.rearrange("(k p) c -> p k c", p=128), "w_wout", [], ["wout"])
        for n in range(NT):
            b = n % 2
            for kt in range(KT):
                TR(bbank(b)[:, kt * 128:(kt + 1) * 128], mixed[:, n, kt * 128:(kt + 1) * 128], ident_b[:],
                   [("mixed", n), "ident_b"], [("pbb", b)])
            CP("act", mT[:, :, n * 128:(n + 1) * 128], bbank(b).rearrange("p (k t) -> p k t", k=KT), [("pbb", b)], [("mT", n)])
        for n in range(NT):
            tsl = slice(n * 128, (n + 1) * 128)
            DMA("sp", x1[:, n, :], x_d[tsl, :], ("x1ld", n % 4), [], [("x1", n)])
            pp = pt[n % 2]
            for half in range(2):
                for kt in range(KT):
                    MM(pp[:, half * 512:(half + 1) * 512], mT[:, kt, tsl], wout[:, kt, half * 512:(half + 1) * 512],
                       kt == 0, kt == KT - 1, [("mT", n), "wout"], [bkey((n % 2) * 2 + half)])
            TT("dve", x1[:, n, :], x1[:, n, :], pp[:, :], ALU.add, [("x1", n), bkey((n % 2) * 2), bkey((n % 2) * 2 + 1)], [("x1", n)])
            b = n % 2
            ssap = small[:, 8 + b:9 + b]
            ssk = ("ss2", b)
            ACTF(junk, x1[:, n, :], AF.Square, [("x1", n)], ["junk", ssk], accum_out=ssap)
            rstd_inplace(ssap, D, ssk)
            STT(mixed[:, n, :], x1[:, n, :], ssap, n2_bc, ALU.mult, ALU.mult, [("x1", n), ssk, "n2_bc"], [("mixed", n)])
            for kt in range(KT):
                TR(bbank(b)[:, kt * 128:(kt + 1) * 128], mixed[:, n, kt * 128:(kt + 1) * 128], ident_b[:],
                   [("mixed", n), "ident_b"], [("pbb", b)])
            CP("act", hT[:, :, tsl], bbank(b).rearrange("p (k t) -> p k t", k=KT), [("pbb", b)], [("hT", n)])
        MARK("e0")

        P.barrier()
        NB = 64
        wr_d = dram("moe_wr", [D, 36])
        wgu0_d = dram("moe_wgu0", [4096, 2048])
        wgu1_d = dram("moe_wgu1", [4096, 2048])
        wdr_d = dram("moe_wdr", [4096, 2048])
        xb_d = nc.dram_tensor("moe_xb", [NB * 128, D], BF16, kind="Internal").ap()
        yb_d = nc.dram_tensor("moe_yb", [NB * 128, D], F32, kind="Internal").ap()
        off = X1_END
        stg = []
        for i in range(3):
            a, off = carve(off, [2048], F32)
            stg.append(a)
        wgu_bf = []
        wd_bf = []
        for i in range(2):
            a, off = carve(off, [KT, 512], BF16)
            wgu_bf.append(a)
            a, off = carve(off, [2, D], BF16)
            wd_bf.append(a)
        wr, off = carve(off, [KT, 36], BF16)
        lg, off = carve(off, [NT, 36], F32)
        oh1, off = carve(off, [NT, 32], F32)
        oh2, off = carve(off, [NT, 32], F32)
        msk, off = carve(off, [NT, 32], F32)
        rank, off = carve(off, [NT, 32], F32)
        tmp3, off = carve(off, [NT, 32], F32)
        gtmp, off = carve(off, [NT, 4], F32)
        ohg, off = carve(off, [NT, 4], F32)
        rv, off = carve(off, [8, NT], F32)
        mcum, off = carve(off, [32], F32)
        cnt, off = carve(off, [32], F32)
        padded, off = carve(off, [32], F32)
        ends, off = carve(off, [32], F32)
        pstart, off = carve(off, [32], F32)
        ebf, off = carve(off, [NB], F32)
        widx_f, off = carve(off, [NB], F32)
        widx, off = carve(off, [NB], I32)
        dest_f, off = carve(off, [2, NT], F32)
        dest_i, off = carve(off, [2, NT], I32)
        MOE_END = off
        cmpb = stg[0].rearrange("p (b e) -> p b e", b=NB)
        cmpj = stg[1][:, 0:512].rearrange("p (e j) -> p e j", e=32)
        ht32 = hT[:].rearrange("p k t -> p (k t)").bitcast(F32)
        HTCAP = 32 * 1024
        hoff = 0
        xg, xgT, sil, hid_bf, hidT, ysb = [], [], [], [], [], []
        for i in range(2):
            a, hoff = carve(hoff, [D], BF16, ht32, HTCAP); xg.append(a)
            a, hoff = carve(hoff, [KT, 128], BF16, ht32, HTCAP); xgT.append(a)
            a, hoff = carve(hoff, [256], F32, ht32, HTCAP); sil.append(a)
            a, hoff = carve(hoff, [256], BF16, ht32, HTCAP); hid_bf.append(a)
            a, hoff = carve(hoff, [2, 128], BF16, ht32, HTCAP); hidT.append(a)
            a, hoff = carve(hoff, [D], F32, ht32, HTCAP); ysb.append(a)
        mix32 = mixed[:].rearrange("p n d -> p (n d)").bitcast(F32)
        MIXCAP = 32 * 1024
        moff = 0
        yg = []
        for i in range(2):
            a, moff = carve(moff, [D], F32, mix32, MIXCAP); yg.append(a)
        stgB = []
        for i in range(3):
            a, moff = carve(moff, [2048], F32, mix32, MIXCAP); stgB.append(a)

        DMA("pool", wr, wr_d.rearrange("(k p) c -> p k c", p=128), "w_wr", [], ["wr"])
        for n in range(NT):
            bi = n % 2
            for kt in range(KT):
                MM(bank(bi)[:, 0:36], hT[:, kt, n * 128:(n + 1) * 128], wr[:, kt, :], kt == 0, kt == KT - 1,
                   ["wr", ("hT", n)], [bkey(bi)])
            CP("act", lg[:, n, :], bank(bi)[:, 0:36], [bkey(bi)], ["lg"])
        BIG = 10000.0
        glv = lg[:, :, 0:4]
        elv = lg[:, :, 4:36]

        def RED(out, in_, op, R, W):
            P.op("dve", lambda e: e.tensor_reduce(out=out, in_=in_, axis=AX.X, op=op), R, W, cost=100.0 + _fsz(in_) * 1.0)

        def bcn(ap2, k):
            return ap2.unsqueeze(2).to_broadcast([128, NT, k])

        gmax, gsum, m1, m2, w1, w2 = (rv[:, i, :] for i in range(6))
        RED(gmax, glv, ALU.max, ["lg"], ["rv"])
        TT("dve", ohg, glv, bcn(gmax, 4), ALU.is_equal, ["lg", "rv"], ["ohg"])
        TT("dve", gtmp, glv, bcn(gmax, 4), ALU.subtract, ["lg", "rv"], ["gtmp"])
        ACTF(gtmp, gtmp, AF.Exp, ["gtmp"], ["gtmp"])
        RED(gsum, gtmp, ALU.add, ["gtmp"], ["rv"])
        RECIP(gsum, gsum, ["rv"], ["rv"])
        TS("dve", ohg, ohg, BIG, -BIG, ALU.mult, ALU.add, ["ohg"], ["ohg"])
        TT("dve", msk.rearrange("p n (g e) -> p n g e", g=4), elv.rearrange("p n (g e) -> p n g e", g=4),
           ohg.unsqueeze(3).to_broadcast([128, NT, 4, 8]), ALU.add, ["lg", "ohg"], ["msk"])
        RED(m1, msk, ALU.max, ["msk"], ["rv"])
        TT("dve", oh1, msk, bcn(m1, 32), ALU.is_equal, ["msk", "rv"], ["oh1"])
        STT(msk, oh1, -BIG, msk, ALU.mult, ALU.add, ["oh1", "msk"], ["msk"])
        RED(m2, msk, ALU.max, ["msk"], ["rv"])
        TT("dve", oh2, msk, bcn(m2, 32), ALU.is_equal, ["msk", "rv"], ["oh2"])
        TT("dve", w2, m2, m1, ALU.subtract, ["rv"], ["rv"])
        ACTF(w2, w2, AF.Exp, ["rv"], ["rv"])
        TS("dve", w1, w2, 1.0, None, ALU.add, ALU.bypass, ["rv"], ["rv"])
        RECIP(w1, w1, ["rv"], ["rv"])
        TT("dve", w1, w1, gsum, ALU.mult, ["rv"], ["rv"])
        TT("dve", w2, w2, w1, ALU.mult, ["rv"], ["rv"])
        TT("dve", msk, oh1, oh2, ALU.add, ["oh1", "oh2", "msk"], ["msk"])
        MEMSET("dve", mcum, 0.0, ["mcum"])
        for n in range(NT):
            bi = n % 2
            MM(bank(bi)[:, 0:32], cst("m_lt"), msk[:, n, :], True, False, ["consts", "msk"], [bkey(bi)])
            MM(bank(bi)[:, 0:32], cst("ones"), mcum, False, True, ["consts", "mcum"], [bkey(bi)])
            CP("act", rank[:, n, :], bank(bi)[:, 0:32], [bkey(bi)], ["rank"])
            TT("dve", mcum, mcum, msk[:, n, :], ALU.add, ["mcum", "msk"], ["mcum"])
        MM(bank(0)[:, 0:32], cst("ones"), mcum, True, True, ["consts", "mcum"], [bkey(0)])
        CP("act", cnt, bank(0)[:, 0:32], [bkey(0)], ["cnt"])
        TT("dve", cmpj, cnt.unsqueeze(2).to_broadcast([128, 32, 16]),
           cst("bvals")[:, 0:16].unsqueeze(1).to_broadcast([128, 32, 16]), ALU.is_gt, ["cnt", "consts"], [("stg", 1)])
        RED(padded, cmpj, ALU.add, [("stg", 1)], ["padded"])
        TS("dve", padded, padded, 128.0, None, ALU.mult, ALU.bypass, ["padded"], ["padded"])
        P.op("dve", lambda e: e.tensor_tensor_scan(out=ends, data0=cst("ones")[:, 0:32], data1=padded, initial=0.0,
                                                  op0=ALU.mult, op1=ALU.add), ["consts", "padded"], ["ends"], cost=300.0)
        TT("dve", pstart, ends, padded, ALU.subtract, ["ends", "padded"], ["pstart"])
        TT("dve", rank, rank, pstart.unsqueeze(1).to_broadcast([128, NT, 32]), ALU.add, ["rank", "pstart"], ["rank"])
        for k, ohk in ((0, oh1), (1, oh2)):
            TT("dve", tmp3, ohk, rank, ALU.mult, ["oh1", "oh2", "rank"], ["tmp3"])
            RED(dest_f[:, k, :], tmp3, ALU.add, ["tmp3"], ["dest_f"])
        CP("dve", dest_i, dest_f, ["dest_f"], ["dest_i"])
        TT("dve", cmpb, ends.unsqueeze(1).to_broadcast([128, NB, 32]),
           cst("bvals")[:, 0:NB].unsqueeze(2).to_broadcast([128, NB, 32]), ALU.is_le, ["ends", "consts"], [("stg", 0)])
        RED(ebf, cmpb, ALU.add, [("stg", 0)], ["ebf"])
        STT(widx_f, ebf, 128.0, cst("pidx")[:, 0:NB], ALU.mult, ALU.add, ["ebf", "consts"], ["widx_f"])
        CP("dve", widx, widx_f, ["widx_f"], ["widx"])
        MARK("e1")

        IOA = bass.IndirectOffsetOnAxis
        regs = {}

        def _pool_init(e):
            regs["bc"] = e.alloc_register("moe_bc")
            e.reg_mov(regs["bc"], 4095)
        P.pool_init = _pool_init
        XB_KEYS = []
        zt, off = carve(off, [D], BF16)
        MEMSET("pool", zt, 0.0, ["zt"])
        DMA("sp", xb_d.rearrange("(p r) d -> p r d", p=128), zt.unsqueeze(1).to_broadcast([128, NB, D]), "xbz", ["zt"], ["xb0"])
        for n in range(NT):
            for k in range(2):
                idx_ap = dest_i[:, k, n:n + 1]
                src_ap = mixed[:, n, :]
                P.dma("pool", lambda e, idx_ap=idx_ap, src_ap=src_ap: e.indirect_dma_start(
                    out=xb_d[:, :], out_offset=IOA(ap=idx_ap, axis=0), in_=src_ap, in_offset=None),
                    ("sc", (2 * n + k) % 4), [("mixed", n), "dest_i", "xb0"], [("xb", n, k)], nbytes=256 * 1024)
                XB_KEYS.append(("xb", n, k))

        def gather_w(dst, src_d, b, skey, extra):
            idx_ap = widx[:, b:b + 1]
            P.dma("pool", lambda e: e.indirect_dma_start(
                out=dst, out_offset=None, in_=src_d[:, :], in_offset=IOA(ap=idx_ap, axis=0),
                bounds_check=regs["bc"], oob_is_err=False),
                skey, ["widx"] + extra, [skey], nbytes=1 << 20)

        YB_KEYS = []
        for b in range(NB):
            s = b % 2
            sset = stg if b % 2 == 0 else stgB
            so = 0 if b % 2 == 0 else 3
            extra = [] if b % 2 == 0 else XB_KEYS
            gather_w(sset[0], wgu0_d, b, ("stg", so + 0), extra)
            gather_w(sset[1], wgu1_d, b, ("stg", so + 1), extra)
            gather_w(sset[2], wdr_d, b, ("stg", so + 2), extra)
            CP("act", wgu_bf[s][:, 0:4, :], sset[0].rearrange("p (k c) -> p k c", k=4), [("stg", so + 0)], [("wgu_bf", s, 0)])
            CP("dve", wgu_bf[s][:, 4:8, :], sset[1].rearrange("p (k c) -> p k c", k=4), [("stg", so + 1)], [("wgu_bf", s, 1)])
            CP("act" if b % 4 < 2 else "dve", wd_bf[s], sset[2].rearrange("p (k c) -> p k c", k=2), [("stg", so + 2)], [("wd_bf", s)])
            DMA("sp", xg[s], xb_d[b * 128:(b + 1) * 128, :], ("xg", s), XB_KEYS, [("xg", s)])
            for kt in range(KT):
                TR(bbank(s)[:, kt * 128:(kt + 1) * 128], xg[s][:, kt * 128:(kt + 1) * 128], ident_b[:],
                   [("xg", s), "ident_b"], [("pbb", s)])
            CP("act", xgT[s], bbank(s).rearrange("p (k t) -> p k t", k=KT), [("pbb", s)], [("xgT", s)])
            hb = bank(s)
            for kt in range(KT):
                MM(hb, xgT[s][:, kt, :], wgu_bf[s][:, kt, :], kt == 0, kt == KT - 1,
                   [("xgT", s), ("wgu_bf", s, 0), ("wgu_bf", s, 1)], [bkey(s)])
            ACTF(sil[s], hb[:, 0:256], AF.Silu, [bkey(s)], [("sil", s)])
            TT("dve", hid_bf[s], sil[s], hb[:, 256:512], ALU.mult, [("sil", s), bkey(s)], [("hid_bf", s)])
            for ft in range(2):
                TR(bbank(s)[:, ft * 128:(ft + 1) * 128], hid_bf[s][:, ft * 128:(ft + 1) * 128], ident_b[:],
                   [("hid_bf", s), "ident_b"], [("pbb", s)])
            CP("act", hidT[s], bbank(s)[:, 0:256].rearrange("p (k t) -> p k t", k=2), [("pbb", s)], [("hidT", s)])
            yp = pt[1 + s]
            for half in range(2):
                for ft in range(2):
                    MM(yp[:, half * 512:(half + 1) * 512], hidT[s][:, ft, :], wd_bf[s][:, ft, half * 512:(half + 1) * 512],
                       ft == 0, ft == 1, [("hidT", s), ("wd_bf", s)], [bkey(2 + 2 * s + half)])
            CP("act" if b % 2 else "dve", ysb[s], yp[:, :], [bkey(2 + 2 * s), bkey(3 + 2 * s)], [("ysb", s)])
            DMA("sp", yb_d[b * 128:(b + 1) * 128, :], ysb[s], ("yst", s), [("ysb", s)], [("yb", b)])
            YB_KEYS.append(("yb", b))
        MARK("e2")
        for n in range(NT):
            for k in range(2):
                s = (2 * n + k) % 2
                idx_ap = dest_i[:, k, n:n + 1]
                dst = yg[s]
                P.dma("pool", lambda e, idx_ap=idx_ap, dst=dst: e.indirect_dma_start(
                    out=dst, out_offset=None, in_=yb_d[:, :], in_offset=IOA(ap=idx_ap, axis=0)),
                    ("yg", s), YB_KEYS + ["dest_i"], [("yg", s)], nbytes=512 * 1024)
                wk = rv[:, 4 + k, n:n + 1]
                STT(x1[:, n, :], yg[s], wk, x1[:, n, :], ALU.mult, ALU.add, [("yg", s), "rv", ("x1", n)], [("x1", n)])

        P.barrier()
        off = X1_END
        nf_bc, off = carve(off, [D], F32)
        ob = [None, None]
        ob[0], off = carve(off, [D], F32)
        ob[1], off = carve(off, [D], F32)
        junk2, off = carve(off, [D], BF16)
        DMA("sp", nf_bc, nf_d.partition_broadcast(128), "c_nf", [], ["nf_bc"])
        for n in range(NT):
            b = n % 2
            ssap = small[:, 12 + b:13 + b]
            ssk = ("ss3", b)
            ACTF(junk2, x1[:, n, :], AF.Square, [("x1", n)], ["junk2", ssk], accum_out=ssap)
            rstd_inplace(ssap, D, ssk)
            STT(ob[b], x1[:, n, :], ssap, nf_bc, ALU.mult, ALU.mult, [("x1", n), ssk, "nf_bc"], [("ob", b)])
            DMA("sp", out_d[n * 128:(n + 1) * 128, :], ob[b], ("out_st", b), [("ob", b)], [("out", n)])
        if not dbg:
            P.wait_all("sp", [("out", n) for n in range(NT)])
        if dbg:
            P.enabled = True
            P.barrier()
            for n in range(NT):
                DMA("sp", dbg_d[n * 128:(n + 1) * 128, :], x1[:, n, :], ("dbg_out", n % 2), [("x1", n)], [("dbg", n)])
            P.wait_all("sp", [("dbg", n) for n in range(NT)] + [("out", n) for n in range(NT)])
        P.emit()
    return nc


def make_in_maps(inputs, n_cores=8):
    f = lambda k: np.asarray(inputs[k], np.float32)
    x = f("x")
    _gu = np.concatenate([f("moe_w_gate")[0], f("moe_w_up")[0]], axis=2).reshape(32, 8, 128, 512).transpose(0, 2, 1, 3)
    shared = {
        "norm1_w": f("norm1_w").reshape(1, D),
        "norm2_w": f("norm2_w").reshape(1, D),
        "norm_f_w": f("norm_f_w").reshape(1, D),
        "consts": CONST_ARR,
        "w_in": np.ascontiguousarray(f("w_in")[0]),
        "gla_w2b_f": np.ascontiguousarray(np.concatenate([f("gla_gate_w2_fwd")[0], f("gla_gate_b_fwd")], axis=0)),
        "gla_w2b_b": np.ascontiguousarray(np.concatenate([f("gla_gate_w2_bwd")[0], f("gla_gate_b_bwd")], axis=0)),
        "gla_norm_w": f("gla_norm_w").reshape(1, 256),
        "w_out": np.ascontiguousarray(f("w_out")[0]),
        "moe_wr": np.ascontiguousarray(np.concatenate([f("moe_w_group")[0], f("moe_w_router")[0]], axis=1)),
        "moe_wgu0": _gu[:, :, 0:4, :].reshape(4096, 2048).copy(),
        "moe_wgu1": _gu[:, :, 4:8, :].reshape(4096, 2048).copy(),
        "moe_wdr": np.ascontiguousarray(f("moe_w_down")[0].reshape(32, 2, 128, 1024).transpose(0, 2, 1, 3)).reshape(4096, 2048),
        "gdn_norm_w": f("gdn_norm_w").reshape(1, 128),
        "gdn_vec": np.ascontiguousarray(np.concatenate([f("gdn_dt_bias_fwd")[0], f("gdn_dt_bias_bwd")[0],
                                                        f("gdn_a_log_fwd")[0], f("gdn_a_log_bwd")[0]]).reshape(1, 32)),
        "gdn_conv_wT": np.ascontiguousarray(f("gdn_conv_w")[0].T.reshape(24, 128, 5).transpose(1, 0, 2)),
    }
    maps = []
    for c in range(n_cores):
        m = dict(shared)
        m["x"] = np.ascontiguousarray(x[c])
        maps.append(m)
    return maps


def kernel(**inputs):
    nc = build()
    in_maps = make_in_maps(inputs)
    res = run_bass_kernel_spmd(nc, in_maps, core_ids=list(range(8)))
    out = np.stack([np.asarray(r["out"]) for r in res.results], axis=0)
    return out.astype(np.float32)
```

```python
import contextlib
import heapq
import numpy as np
import concourse.bass as bass
import concourse.mybir as mybir
from concourse.bass_utils import run_bass_kernel_spmd

F32 = mybir.dt.float32
BF16 = mybir.dt.bfloat16
I32 = mybir.dt.int32
AF = mybir.ActivationFunctionType
ALU = mybir.AluOpType
AX = mybir.AxisListType

T = 2048
D = 1024
NT = T // 128
KT = D // 128
EPS = 1e-6
SAME_ENGINE_SYNC = True
EPOCH = 20000
SYNC_NS = 120.0
DMA_LAT_NS = 2200.0


class Prog:
    ENGS = ("pe", "act", "dve", "pool", "sp")

    def __init__(self, nc, stack):
        self.nc = nc
        self.stack = stack
        self.streams = {e: [] for e in self.ENGS}
        self.count = {e: 0 for e in self.ENGS}
        self.esems = {e: [] for e in self.ENGS}
        self.known = {e: {} for e in self.ENGS}
        self.last_write = {}
        self.readers = {}
        self.dma_sems = {}
        self.dma_vals = {}
        self.dma_last = {}
        self.enabled = True
        self.seg = []
        self.ticks = {}
        self.nops = 0
        self.seg_base = 0
        self.pool_init = None

    def _new_sem(self, name):
        return self.stack.enter_context(self.nc.semaphore(name))

    @staticmethod
    def _psum_fix(reads, writes):
        r2, w2 = [], list(writes)
        for k in reads:
            if isinstance(k, tuple) and k[0] in ("pb", "pbb"):
                if k not in w2:
                    w2.append(k)
            else:
                r2.append(k)
        return r2, w2

    def _record(self, eng, fn, reads, writes, cost, kind, semkey=None):
        reads, writes = self._psum_fix(list(reads), list(writes))
        oid = self.nops
        self.nops += 1
        preds = set()
        for r in reads:
            t = self.last_write.get(r)
            if t is not None:
                preds.add(t)
        for w in writes:
            t = self.last_write.get(w)
            if t is not None:
                preds.add(t)
            preds.update(self.readers.get(w, ()))
        if kind == "dma":
            prev = self.dma_last.get(semkey)
            if prev is not None:
                preds.add(prev)
            self.dma_last[semkey] = oid
        preds = {p for p in preds if p >= self.seg_base}
        self.seg.append(dict(id=oid, eng=eng, fn=fn, preds=preds, cost=float(cost), kind=kind, semkey=semkey))
        for w in writes:
            self.last_write[w] = oid
            self.readers[w] = []
        for r in reads:
            self.readers.setdefault(r, []).append(oid)
        return oid

    def op(self, eng, fn, reads=(), writes=(), cost=300.0):
        if not self.enabled:
            return
        self._record(eng, fn, reads, writes, cost, "op")

    def dma(self, eng, fn, semkey, reads=(), writes=(), nbytes=1 << 20):
        if not self.enabled:
            return
        self._record(eng, fn, reads, writes, DMA_LAT_NS + nbytes / 160.0, "dma", semkey)

    def wait_all(self, eng, keys):
        self._record(eng, None, list(keys), [], 0.0, "op")

    def _schedule_segment(self):
        ops = self.seg
        if not ops:
            return
        byid = {o["id"]: o for o in ops}
        succ = {o["id"]: [] for o in ops}
        indeg = {}
        for o in ops:
            indeg[o["id"]] = len(o["preds"])
            for p in o["preds"]:
                succ[p].append(o["id"])
        ready_t = {o["id"]: 0.0 for o in ops}
        finish = {}
        heaps = {e: [] for e in self.ENGS}
        for o in ops:
            if indeg[o["id"]] == 0:
                heapq.heappush(heaps[o["eng"]], (0.0, o["id"]))
        etime = {e: 0.0 for e in self.ENGS}
        order = {e: [] for e in self.ENGS}
        remaining = len(ops)
        while remaining:
            best = None
            for e in self.ENGS:
                h = heaps[e]
                if not h:
                    continue
                rt, oid = h[0]
                st = max(rt, etime[e])
                if best is None or (st, oid) < (best[0], best[1]):
                    best = (st, oid, e)
            st, oid, e = best
            heapq.heappop(heaps[e])
            o = byid[oid]
            if o["kind"] == "dma":
                etime[e] = st + 150.0
                fin = st + o["cost"]
            else:
                etime[e] = st + o["cost"]
                fin = etime[e]
            finish[oid] = fin
            order[e].append(o)
            remaining -= 1
            for s in succ[oid]:
                so = byid[s]
                lat = SYNC_NS if (so["eng"] != e or o["kind"] == "dma") else (60.0 if e != "pe" else 0.0)
                ready_t[s] = max(ready_t[s], fin + lat)
                indeg[s] -= 1
                if indeg[s] == 0:
                    heapq.heappush(heaps[so["eng"]], (ready_t[s], s))
        self.est_ns = getattr(self, "est_ns", 0.0) + max(list(finish.values()) + [0.0])
        for o in ops:
            if o["kind"] == "dma":
                k = o["semkey"]
                if k not in self.dma_sems:
                    self.dma_sems[k] = self._new_sem(f"d{len(self.dma_sems)}")
                    self.dma_vals[k] = 0
                self.dma_vals[k] += 16
                self.ticks[o["id"]] = (self.dma_sems[k], self.dma_vals[k], "dma")
        def needs_sem(o):
            for s_ in succ[o["id"]]:
                se = byid[s_]["eng"]
                if se != o["eng"] or (SAME_ENGINE_SYNC and se != "pe"):
                    return True
            return False
        for e in self.ENGS:
            real = [o for o in order[e] if o["kind"] == "op" and o["fn"] is not None]
            for i_, o in enumerate(real):
                o["sig"] = needs_sem(o) or i_ == len(real) - 1
        for e in self.ENGS:
            for o in order[e]:
                if o["kind"] == "op" and o["fn"] is not None and o["sig"]:
                    c = self.count[e]
                    ep, v = divmod(c, EPOCH)
                    while len(self.esems[e]) <= ep:
                        self.esems[e].append(self._new_sem(f"s_{e}_{len(self.esems[e])}"))
                    self.count[e] = c + 1
                    self.ticks[o["id"]] = (self.esems[e][ep], v + 1, e)
        for e in self.ENGS:
            for o in order[e]:
                waits = {}
                for p in o["preds"]:
                    if byid[p]["eng"] == e and byid[p]["kind"] == "op" and (not SAME_ENGINE_SYNC or e == "pe"):
                        continue
                    sem, val, src = self.ticks[p]
                    sid = id(sem)
                    if self.known[e].get(sid, 0) >= val:
                        continue
                    if sid not in waits or waits[sid][1] < val:
                        waits[sid] = (sem, val)
                for sid, (sem, val) in waits.items():
                    self.known[e][sid] = val
                inc = None
                if o["fn"] is not None and o["id"] in self.ticks:
                    sem, val, src = self.ticks[o["id"]]
                    inc = (sem, 16 if o["kind"] == "dma" else 1)
                self.streams[e].append((o["fn"], list(waits.values()), inc))
        self.seg = []
        self.seg_base = self.nops

    def barrier(self):
        if not self.enabled and not self.seg:
            return
        self._schedule_segment()
        ticks = []
        for e2 in self.ENGS:
            c = self.count[e2]
            if c > 0:
                ep, v = divmod(c - 1, EPOCH)
                ticks.append((self.esems[e2][ep], v + 1))
        for k, sem in self.dma_sems.items():
            ticks.append((sem, self.dma_vals[k]))
        for eng in self.ENGS:
            waits = []
            for (sem, val) in ticks:
                if self.known[eng].get(id(sem), 0) >= val:
                    continue
                self.known[eng][id(sem)] = val
                waits.append((sem, val))
            if waits:
                self.streams[eng].append((None, waits, None))

    def emit(self):
        self._schedule_segment()
        nc = self.nc
        with nc.Block() as block:
            def run(e, stream):
                for fn, waits, inc in stream:
                    for sem, val in waits:
                        e.wait_ge(sem, val)
                    if fn is None:
                        continue
                    ins = fn(e)
                    if inc is not None:
                        ins.then_inc(inc[0], inc[1])

            @block.tensor
            def _(e):
                run(e, self.streams["pe"])

            @block.scalar
            def _(e):
                run(e, self.streams["act"])

            @block.vector
            def _(e):
                run(e, self.streams["dve"])

            @block.gpsimd
            def _(e):
                if self.pool_init is not None:
                    self.pool_init(e)
                run(e, self.streams["pool"])

            @block.sync
            def _(e):
                run(e, self.streams["sp"])


def _fsz(ap):
    s = ap.shape
    n = 1
    for v in s[1:]:
        n *= int(v)
    return n


C_GQ, C_GK, C_GV, C_GR = 0, 512, 1024, 2048
C_GLF, C_GLB = 3072, 3088
C_DQ, C_DK, C_DV, C_DZ = 3104, 4128, 5152, 6176
C_DAB = 7200
C_MA, C_MB = 7232, 8256
D_IN = 9280


def host_consts():
    r = np.arange(128)[:, None]
    t = np.arange(128)[None, :]
    same = (r // 64) == (t // 64)
    c = {}
    c["ident"] = np.eye(128, dtype=np.float32)
    c["a_le"] = np.where(r <= t, -1.0 / 16, 0.0)
    c["a_ge"] = np.where(r >= t, -1.0 / 16, 0.0)
    c["a_gt"] = np.where(r > t, -1.0 / 16, 0.0)
    c["a_lt"] = np.where(r < t, -1.0 / 16, 0.0)
    c["m_le"] = np.where(r <= t, 1.0, 0.0)
    c["m_ge"] = np.where(r >= t, 1.0, 0.0)
    c["b_le"] = np.where((r <= t) & same, 1.0, 0.0)
    c["b_ge"] = np.where((r >= t) & same, 1.0, 0.0)
    c["b_gt"] = np.where((r > t) & same, 1.0, 0.0)
    c["b_lt"] = np.where((r < t) & same, 1.0, 0.0)
    c["csel0"] = np.where(r < 64, 1.0, 0.0) + 0.0 * t
    c["csel1"] = np.where(r >= 64, 1.0, 0.0) + 0.0 * t
    c["ones"] = np.ones((128, 128))
    c["m_lt"] = np.where(r < t, 1.0, 0.0)
    c["bvals"] = 128.0 * t + 0.0 * r
    c["pidx"] = 1.0 * r + 0.0 * t
    names = list(c.keys())
    arr = np.stack([np.asarray(c[n], np.float32) for n in names], axis=1)
    return names, np.ascontiguousarray(arr)


CONST_NAMES, CONST_ARR = host_consts()
NCONST = len(CONST_NAMES)


def build(stage="all", dbg=False):
    nc = bass.Bass("TRN2", target_bir_lowering=False)
    stack = contextlib.ExitStack()
    with stack:
        P = Prog(nc, stack)

        def dram(name, shape, dt=F32, kind="ExternalInput"):
            return nc.dram_tensor(name, list(shape), dt, kind=kind).ap()

        def sb(name, shape, dt=F32):
            return stack.enter_context(nc.sbuf_tensor(name, list(shape), dt))

        def ps(name, shape, dt=F32):
            return stack.enter_context(nc.psum_tensor(name, list(shape), dt))

        def MM(out, lhsT, rhs, start, stop, R, W):
            n = _fsz(rhs)
            c = 70.0 + n * 0.75
            if rhs.dtype == F32:
                c *= 4.0
            P.op("pe", lambda e: e.matmul(out, lhsT, rhs, start=start, stop=stop), R, W, cost=c)

        def TR(out, in_, ident, R, W):
            P.op("pe", lambda e: e.transpose(out=out, in_=in_, identity=ident), R, W, cost=110.0)

        def ACTF(out, in_, func, R, W, **kw):
            c = 120.0 + _fsz(in_) * 0.6 + (90.0 if "accum_out" in kw else 0.0)
            P.op("act", lambda e: e.activation(out=out, in_=in_, func=func, **kw), R, W, cost=c)

        def _vc(eng, n, k=1.5):
            return (100.0 + n * k * 0.6) if eng == "dve" else (150.0 + n * 1.9)

        def TT(eng, out, in0, in1, op, R, W):
            P.op(eng, lambda e: e.tensor_tensor(out=out, in0=in0, in1=in1, op=op), R, W, cost=_vc(eng, _fsz(out)))

        def TS(eng, out, in0, s1, s2, op0, op1, R, W):
            P.op(eng, lambda e: e.tensor_scalar(out=out, in0=in0, scalar1=s1, scalar2=s2, op0=op0, op1=op1), R, W,
                 cost=_vc(eng, _fsz(out), 1.05))

        def STT(out, in0, scalar, in1, op0, op1, R, W):
            P.op("dve", lambda e: e.scalar_tensor_tensor(out=out, in0=in0, scalar=scalar, in1=in1, op0=op0, op1=op1), R, W,
                 cost=_vc("dve", _fsz(out)))

        def CP(eng, out, in_, R, W):
            if eng == "act":
                P.op("act", lambda e: e.activation(out=out, in_=in_, func=AF.Copy), R, W, cost=120.0 + _fsz(in_) * 0.6)
            else:
                P.op(eng, lambda e: e.tensor_copy(out=out, in_=in_), R, W, cost=_vc(eng, _fsz(out), 1.05))

        def MEMSET(eng, ap, val, W):
            P.op(eng, lambda e: e.memset(ap, val), [], W, cost=_vc(eng, _fsz(ap), 0.6))

        def DMA(eng, out, in_, semkey, R, W):
            P.dma(eng, lambda e: e.dma_start(out=out, in_=in_), semkey, R, W, nbytes=_fsz(out) * int(out.shape[0]) * 4)

        def RECIP(out, in_, R, W):
            P.op("dve", lambda e: e.reciprocal(out=out, in_=in_), R, W, cost=_vc("dve", _fsz(out), 1.05))

        def MARK(name):
            if stage == name:
                P.enabled = False

        def rstd_inplace(ap, n, key):
            TS("dve", ap, ap, 1.0 / n, EPS, ALU.mult, ALU.add, [key], [key])
            ACTF(ap, ap, AF.Ln, [key], [key])
            ACTF(ap, ap, AF.Exp, [key], [key], scale=-0.5)

        x_d = dram("x", [T, D])
        n1_d = dram("norm1_w", [1, D])
        n2_d = dram("norm2_w", [1, D])
        nf_d = dram("norm_f_w", [1, D])
        consts_d = dram("consts", [128, NCONST, 128])
        w_in_d = dram("w_in", [D, D_IN])
        w2b_d = [dram("gla_w2b_f", [17, 512]), dram("gla_w2b_b", [17, 512])]
        gnw_d = dram("gla_norm_w", [1, 256])
        out_d = dram("out", [T, D], kind="ExternalOutput")
        dbg_d = dram("dbg", [T, D], kind="ExternalOutput") if dbg else None

        consts = sb("consts_sb", [128, NCONST, 128])
        CI = {n: i for i, n in enumerate(CONST_NAMES)}

        def cst(name):
            return consts[:, CI[name], :]

        ident_b = sb("ident_b", [128, 128], BF16)
        ones_b = sb("ones_b", [128, 128], BF16)
        hT = sb("hT", [128, KT, T], BF16)
        mixed = sb("mixed", [128, NT, D], BF16)
        small = sb("small", [128, 64])
        ARENA_BYTES = 134 * 1024
        arena = sb("arena", [128, ARENA_BYTES // 4])

        def carve(off, shape, dt, base=None, cap=None):
            base = arena if base is None else base
            cap = ARENA_BYTES if cap is None else cap
            nb = int(np.prod(shape)) * (2 if dt == BF16 else 4)
            nb = (nb + 3) // 4 * 4
            assert off % 4 == 0 and off + nb <= cap, (off, nb)
            v = base[:, off // 4:(off + nb) // 4]
            if dt != F32:
                v = v.bitcast(dt)
            if len(shape) == 2:
                pat = "p (a b) -> p a b"
                v = v.rearrange(pat, a=shape[0])
            elif len(shape) == 3:
                v = v.rearrange("p (a b c) -> p a b c", a=shape[0], b=shape[1])
            return v, off + nb

        pt = [ps(f"pt{i}", [128, 1024]) for i in range(3)]
        ptb = ps("ptb", [128, 2048], BF16)

        def bank(i):
            return pt[i // 2][:, (i % 2) * 512:(i % 2 + 1) * 512]

        def bkey(i):
            return ("pb", i)

        def bbank(i):
            return ptb[:, i * 1024:(i + 1) * 1024]

        DMA("sp", consts[:], consts_d[:, :, :], "c_consts", [], ["consts"])
        CP("dve", ident_b[:], cst("ident"), ["consts"], ["ident_b"])
        MEMSET("pool", ones_b[:], 1.0, ["ones_b"])

        off = 0
        xt0, off = carve(off, [D], F32)
        xt1, off = carve(off, [D], F32)
        hn0, off = carve(off, [D], BF16)
        hn1, off = carve(off, [D], BF16)
        sq, off = carve(off, [D], F32)
        n1_bc, off = carve(off, [D], F32)
        DMA("sp", n1_bc, n1_d.partition_broadcast(128), "c_n1", [], ["n1_bc"])
        xts = [xt0, xt1]
        hns = [hn0, hn1]
        for tt in range(NT):
            b = tt % 2
            xb, hb = xts[b], hns[b]
            DMA("sp", xb, x_d[tt * 128:(tt + 1) * 128, :], ("xt", b), [], [("xt", b)])
            ACTF(sq, xb, AF.Square, [("xt", b)], ["sq", "ss0"], accum_out=small[:, 0:1])
            rstd_inplace(small[:, 0:1], D, "ss0")
            STT(hb, xb, small[:, 0:1], n1_bc, ALU.mult, ALU.mult, [("xt", b), "ss0", "n1_bc"], [("hn", b)])
            for kt in range(KT):
                TR(bbank(b)[:, kt * 128:(kt + 1) * 128], hb[:, kt * 128:(kt + 1) * 128], ident_b[:],
                   [("hn", b), "ident_b"], [("pbb", b)])
            CP("act", hT[:, :, tt * 128:(tt + 1) * 128], bbank(b).rearrange("p (k t) -> p k t", k=KT),
               [("pbb", b)], [("hT", tt)])
        HT_ALL = [("hT", tt) for tt in range(NT)]
        MARK("p1")

        P.barrier()
        off = 0
        qT, off = carve(off, [T], F32)
        kT, off = carve(off, [T], F32)
        k_tok, off = carve(off, [NT, 128], F32)
        v_tok, off = carve(off, [NT, 256], BF16)
        qdT = [None, None]
        kiT = [None, None]
        ktail = [None, None]
        for d_ in range(2):
            qdT[d_], off = carve(off, [T], BF16)
            kiT[d_], off = carve(off, [T], BF16)
            ktail[d_], off = carve(off, [NT, 128], BF16)
        sb_store, off = carve(off, [NT, 256], BF16)
        dec, off = carve(off, [2, NT], F32)
        S, off = carve(off, [256], F32)
        S_bf, off = carve(off, [256], BF16)
        NTMP = 4
        tmp = []
        for i in range(NTMP):
            d = {}
            for nm in ("e", "lg", "E", "Ei", "Et"):
                d[nm], off = carve(off, [128], F32)
            d["Pf"], off = carve(off, [128], BF16)
            d["Pb"], off = carve(off, [128], BF16)
            d["sig"], off = carve(off, [512], F32)
            d["G"], off = carve(off, [256], F32)
            tmp.append(d)
        gl, off = carve(off, [2, T], BF16)
        w2b, off = carve(off, [2, 512], BF16)
        wqk, off = carve(off, [KT, 256], BF16)
        wkv, off = carve(off, [KT, 384], BF16)
        wgm, off = carve(off, [KT, 512], BF16)
        wgl, off = carve(off, [KT, 32], BF16)
        gnw_bc, off = carve(off, [256], F32)
        GLA_END = off

        DMA("sp", gnw_bc, gnw_d.partition_broadcast(128), "c_gnw", [], ["gnw_bc"])
        MEMSET("pool", gl[0:32, :, :], 1.0, ["gl"])
        MEMSET("pool", w2b[0:32, :, :], 0.0, ["w2b"])
        for d_ in range(2):
            DMA("pool", w2b[0:17, d_, :], w2b_d[d_][:, :], "c_w2b", [], ["w2b"])
        DMA("pool", wgl, w_in_d[:, C_GLF:C_GLF + 32].rearrange("(k p) c -> p k c", p=128), "w_wgl", [], ["wgl"])
        for d_ in range(2):
            for tg in range(4):
                bi = tg % 2
                for kt in range(KT):
                    MM(bank(bi)[0:16, :], wgl[:, kt, d_ * 16:(d_ + 1) * 16], hT[:, kt, tg * 512:(tg + 1) * 512],
                       kt == 0, kt == KT - 1, ["wgl"] + HT_ALL[tg * 4:tg * 4 + 4], [bkey(bi)])
                CP("act", gl[0:16, d_, tg * 512:(tg + 1) * 512], bank(bi)[0:16, :], [bkey(bi)], ["gl"])

        MARK("g0")
        QSCALE = 128.0 ** -0.5
        for h in range(4):
            def wcols(dst, c0, n):
                return (dst, w_in_d[:, c0:c0 + n].rearrange("(k p) c -> p k c", p=128))
            for (dst, src) in (wcols(wqk[:, :, 0:128], C_GQ + h * 128, 128), wcols(wqk[:, :, 128:256], C_GK + h * 128, 128)):
                DMA("pool", dst, src, "w_wqk", [], ["wqk"])
            for (dst, src) in (wcols(wkv[:, :, 0:128], C_GK + h * 128, 128), wcols(wkv[:, :, 128:384], C_GV + h * 256, 256)):
                DMA("pool", dst, src, "w_wkv", [], ["wkv"])
            for (dst, src) in (wcols(wgm[:, :, 0:256], C_GR + h * 256, 256), wcols(wgm[:, :, 256:512], C_MA + h * 256, 256)):
                DMA("pool", dst, src, "w_wgm", [], ["wgm"])
            MARK("g1a")
            for which, dstT in ((0, qT), (1, kT)):
                for tg in range(4):
                    bi = (which * 4 + tg) % 4
                    for kt in range(KT):
                        MM(bank(bi), wqk[:, kt, which * 128:(which + 1) * 128], hT[:, kt, tg * 512:(tg + 1) * 512],
                           kt == 0, kt == KT - 1, ["wqk"] + HT_ALL[tg * 4:tg * 4 + 4], [bkey(bi)])
                    CP("act" if tg % 2 else "dve", dstT[:, tg * 512:(tg + 1) * 512], bank(bi), [bkey(bi)],
                       [("qkT", which, tg)])
            MARK("g1b")
            for n in range(NT):
                bi = 4 + n % 2
                for kt in range(KT):
                    MM(bank(bi)[:, 0:384], hT[:, kt, n * 128:(n + 1) * 128], wkv[:, kt, :],
                       kt == 0, kt == KT - 1, ["wkv", ("hT", n)], [bkey(bi)])
                CP("dve", k_tok[:, n, :], bank(bi)[:, 0:128], [bkey(bi)], [("k_tok", n)])
                CP("act", v_tok[:, n, :], bank(bi)[:, 128:384], [bkey(bi)], [("v_tok", n)])
            MARK("g1")
            for n in range(NT):
                tsl = slice(n * 128, (n + 1) * 128)
                tg = n // 4
                for d_ in range(2):
                    tm = tmp[(n * 2 + d_) % NTMP]
                    tk = ("gtmp", (n * 2 + d_) % NTMP)
                    a_c = cst("a_le") if d_ == 0 else cst("a_ge")
                    a_s = cst("a_gt") if d_ == 0 else cst("a_lt")
                    b0 = (n * 2 + d_) % 2 * 2
                    zb, cb = bank(b0), bank(b0 + 1)
                    MM(zb[:, 0:128], gl[0:32, d_, tsl], w2b[0:32, d_, h * 128:(h + 1) * 128], True, True,
                       ["gl", "w2b"], [bkey(b0)])
                    ACTF(tm["e"], zb[:, 0:128], AF.Exp, [bkey(b0)], [tk], scale=-1.0)
                    ACTF(tm["lg"], tm["e"], AF.Ln, [tk], [tk], bias=1.0)
                    MM(cb[:, 0:128], tm["lg"], a_c, True, True, [tk, "consts"], [bkey(b0 + 1)])
                    MM(cb[:, 128:256], a_s, tm["lg"], True, True, [tk, "consts"], [bkey(b0 + 1)])
                    ACTF(tm["E"], cb[:, 0:128], AF.Exp, [bkey(b0 + 1)], [tk])
                    ACTF(tm["Ei"], cb[:, 0:128], AF.Exp, [bkey(b0 + 1)], [tk], scale=-1.0)
                    ACTF(tm["Et"], cb[:, 128:256], AF.Exp, [bkey(b0 + 1)], [tk])
                    STT(qdT[d_][:, tsl], qT[:, tsl], QSCALE, tm["E"], ALU.mult, ALU.mult,
                        [("qkT", 0, tg), tk], [("qdT", d_, n)])
                    TT("dve", kiT[d_][:, tsl], kT[:, tsl], tm["Ei"], ALU.mult, [("qkT", 1, tg), tk], [("kiT", d_, n)])
                    TT("dve", ktail[d_][:, n, :], k_tok[:, n, :], tm["Et"], ALU.mult, [("k_tok", n), tk], [("ktail", d_, n)])
                    col = 127 if d_ == 0 else 0
                    CP("dve", dec[:, d_, n:n + 1], tm["E"][:, col:col + 1], [tk], [("dec", d_, n)])
            MARK("g2")
            MEMSET("dve", S, 0.0, ["S"])
            for n in range(NT - 1, -1, -1):
                CP("act", sb_store[:, n, :], S, ["S"], [("sb_store", n)])
                bi = 4 + n % 2
                MM(bank(bi)[:, 0:256], ktail[1][:, n, :], v_tok[:, n, :], True, True,
                   [("ktail", 1, n), ("v_tok", n)], [bkey(bi)])
                STT(S, S, dec[:, 1, n:n + 1], bank(bi)[:, 0:256], ALU.mult, ALU.add,
                    ["S", ("dec", 1, n), bkey(bi)], ["S"])
            MARK("g3")
            MEMSET("dve", S, 0.0, ["S"])
            for n in range(NT):
                tsl = slice(n * 128, (n + 1) * 128)
                tm = tmp[n % NTMP]
                tk = ("ftmp", n % NTMP)
                CP("act", S_bf, S, ["S"], ["S_bf"])
                b0 = (n % 2) * 2
                sc = bank(b0)
                MM(sc[:, 0:128], kiT[0][:, tsl], qdT[0][:, tsl], True, True, [("kiT", 0, n), ("qdT", 0, n)], [bkey(b0)])
                MM(sc[:, 128:256], kiT[1][:, tsl], qdT[1][:, tsl], True, True, [("kiT", 1, n), ("qdT", 1, n)], [bkey(b0)])
                TT("dve", tm["Pf"], sc[:, 0:128], cst("m_le"), ALU.mult, [bkey(b0), "consts"], [tk])
                TT("dve", tm["Pb"], sc[:, 128:256], cst("m_ge"), ALU.mult, [bkey(b0), "consts"], [tk])
                ob = bank(b0 + 1)
                ok = bkey(b0 + 1)
                MM(ob[:, 0:256], qdT[0][:, tsl], S_bf, True, False, [("qdT", 0, n), "S_bf"], [ok])
                MM(ob[:, 0:256], qdT[1][:, tsl], sb_store[:, n, :], False, False, [("qdT", 1, n), ("sb_store", n)], [ok])
                MM(ob[:, 0:256], tm["Pf"], v_tok[:, n, :], False, False, [tk, ("v_tok", n)], [ok])
                MM(ob[:, 0:256], tm["Pb"], v_tok[:, n, :], False, True, [tk, ("v_tok", n)], [ok])
                kb = 4 + n % 2
                MM(bank(kb)[:, 0:256], ktail[0][:, n, :], v_tok[:, n, :], True, True,
                   [("ktail", 0, n), ("v_tok", n)], [bkey(kb)])
                STT(S, S, dec[:, 0, n:n + 1], bank(kb)[:, 0:256], ALU.mult, ALU.add,
                    ["S", ("dec", 0, n), bkey(kb)], ["S"])
                gb = 4 + n % 2
                for kt in range(KT):
                    MM(bank(gb), hT[:, kt, tsl], wgm[:, kt, :], kt == 0, kt == KT - 1, ["wgm", ("hT", n)], [bkey(gb)])
                ACTF(tm["sig"], bank(gb), AF.Exp, [bkey(gb)], [("sig", n % NTMP)], scale=-1.0)
                ACTF(tm["sig"], tm["sig"], AF.Ln, [("sig", n % NTMP)], [("sig", n % NTMP)], bias=1.0)
                ACTF(tm["sig"], tm["sig"], AF.Exp, [("sig", n % NTMP)], [("sig", n % NTMP)], scale=-1.0)
                TT("pool", tm["G"], tm["sig"][:, 0:256], tm["sig"][:, 256:512], ALU.mult, [("sig", n % NTMP)], [("G", n % NTMP)])
                TT("dve", tm["G"], tm["G"], bank(gb)[:, 0:256], ALU.mult, [("G", n % NTMP), bkey(gb)], [("G", n % NTMP)])
                TT("pool", tm["G"], tm["G"], gnw_bc, ALU.mult, [("G", n % NTMP), "gnw_bc"], [("G", n % NTMP)])
                ssk = ("ssq", n % 2)
                ssap = small[:, 2 + n % 2:3 + n % 2]
                ACTF(tm["sig"][:, 0:256], ob[:, 0:256], AF.Square, [ok, ("G", n % NTMP)], [("sig", n % NTMP), ssk],
                     accum_out=ssap)
                rstd_inplace(ssap, 256, ssk)
                STT(mixed[:, n, h * 256:(h + 1) * 256], ob[:, 0:256], ssap, tm["G"], ALU.mult, ALU.mult,
                    [ok, ssk, ("G", n % NTMP)], [("mixed", n)])

        P.barrier()
        HG = 4
        off = 0
        gqT, off = carve(off, [HG, T], BF16)
        gkT, off = carve(off, [HG, T], BF16)
        gvT, off = carve(off, [HG, T], BF16)
        dabs, off = carve(off, [NT, 32], F32)
        g_raw, off = carve(off, [NT, 2, 8], F32)
        beta, off = carve(off, [NT, 2, 8], F32)
        gvec, off = carve(off, [64], F32)
        wsl0, off = carve(off, [KT, 512], BF16)
        wsl1, off = carve(off, [KT, 512], BF16)
        wsl = [wsl0, wsl1]
        cwT, off = carve(off, [24, 5], F32)
        gdnw_bc, off = carve(off, [128], F32)
        wdab, off = carve(off, [KT, 32], BF16)
        TMP0 = off
        xc = [None, None]
        xc[0], off = carve(off, [T + 4], BF16)
        xc[1], off = carve(off, [T + 4], BF16)
        diag, off = carve(off, [5, 128], BF16)
        ce = [None, None]
        cy = [None, None]
        for i in range(2):
            ce[i], off = carve(off, [512], F32)
            cy[i], off = carve(off, [512], F32)
        cysq, off = carve(off, [512], BF16)
        crs, off = carve(off, [512], F32)
        CONV_END = off
        off = TMP0
        DB = []
        for d_ in range(2):
            dd = {}
            for nm in ("GMB", "Wd", "decT", "u_sb", "Sg"):
                dd[nm], off = carve(off, [HG, 128], F32)
            for nm in ("Lm", "LTm", "XT", "Pp0", "Pp1", "PTp0", "PTp1", "kbg", "vbeta", "qd_tok",
                       "attnT", "ktl", "qdTg", "wT_sb", "vnew", "Sg_bf", "ostage"):
                dd[nm], off = carve(off, [HG, 128], BF16)
            dd["bg"], off = carve(off, [HG], F32)
            DB.append(dd)
        oland = []
        for i in range(2):
            a, off = carve(off, [HG, 128], BF16)
            oland.append(a)
        osum, off = carve(off, [HG, 128], F32)
        fsig, off = carve(off, [512], F32)
        fG, off = carve(off, [HG, 128], F32)
        frs, off = carve(off, [8], F32)
        SWEEP_END = off
        gdn_o = nc.dram_tensor("gdn_o_spill", [NT, 128, HG * 128], BF16, kind="Internal").ap()

        gdnw_d = dram("gdn_norm_w", [1, 128])
        gvec_d = dram("gdn_vec", [1, 32])
        cw_d = dram("gdn_conv_wT", [128, 24, 5])
        DMA("sp", gdnw_bc, gdnw_d.partition_broadcast(128), "c_gdnw", [], ["gdnw_bc"])
        DMA("sp", gvec[:, 0:32], gvec_d.partition_broadcast(128), "c_gvec", [], ["gvec"])
        DMA("sp", cwT, cw_d[:, :, :], "c_cw", [], ["cwT"])
        DMA("pool", wdab, w_in_d[:, C_DAB:C_DAB + 32].rearrange("(k p) c -> p k c", p=128), "w_wdab", [], ["wdab"])
        ACTF(gvec[:, 16:32], gvec[:, 16:32], AF.Exp, ["gvec"], ["gvec"])
        TS("dve", gvec[:, 16:32], gvec[:, 16:32], -1.0, None, ALU.mult, ALU.bypass, ["gvec"], ["gvec"])
        for n in range(NT):
            bi = n % 2
            for kt in range(KT):
                MM(bank(bi)[:, 0:32], hT[:, kt, n * 128:(n + 1) * 128], wdab[:, kt, :], kt == 0, kt == KT - 1,
                   ["wdab", ("hT", n)], [bkey(bi)])
            CP("act", dabs[:, n, :], bank(bi)[:, 0:32], [bkey(bi)], ["dabs"])
        a_view = dabs[:, :, 0:16]
        b_view = dabs[:, :, 16:32]
        g_flat = g_raw.rearrange("p n d h -> p n (d h)")
        be_flat = beta.rearrange("p n d h -> p n (d h)")
        TT("dve", g_flat, a_view, gvec[:, 0:16].unsqueeze(1).to_broadcast([128, NT, 16]), ALU.add, ["dabs", "gvec"], ["g_raw"])
        ACTF(g_flat, g_flat, AF.Exp, ["g_raw"], ["g_raw"])
        ACTF(g_flat, g_flat, AF.Ln, ["g_raw"], ["g_raw"], bias=1.0)
        TT("dve", g_flat, g_flat, gvec[:, 16:32].unsqueeze(1).to_broadcast([128, NT, 16]), ALU.mult, ["g_raw", "gvec"], ["g_raw"])
        ACTF(be_flat, b_view, AF.Exp, ["dabs"], ["beta"], scale=-1.0)
        TS("dve", be_flat, be_flat, 1.0, None, ALU.add, ALU.bypass, ["beta"], ["beta"])
        RECIP(be_flat, be_flat, ["beta"], ["beta"])
        MARK("d0")

        GSCALE = 128.0 ** -0.5
        ident_bc4 = ident_b[:].unsqueeze(1).to_broadcast([128, HG, 128])

        def bc_h(ap2):
            return ap2.unsqueeze(2).to_broadcast([128, HG, 128])

        def bc_m(ap2):
            return ap2.unsqueeze(1).to_broadcast([128, HG, 128])

        def v4(ap2):
            return ap2.rearrange("p (h d) -> p h d", h=HG)

        for grp in range(2):
            hs0 = grp * HG
            for which, c_base, dstT in ((0, C_DQ, gqT), (1, C_DK, gkT), (2, C_DV, gvT)):
                ws = wsl[which % 2]
                wk = ("wsl", which % 2)
                DMA("pool", ws, w_in_d[:, c_base + hs0 * 128:c_base + (hs0 + HG) * 128].rearrange("(k p) c -> p k c", p=128),
                    ("w_wsl", which % 2), [], [wk])
                for hh in range(HG):
                    ci = which * 8 + hs0 + hh
                    xi = (which * HG + hh) % 2
                    xcb = xc[xi]
                    xk = ("xc", xi)
                    MEMSET("pool", xcb[:, 0:2], 0.0, [xk])
                    MEMSET("pool", xcb[:, T + 2:T + 4], 0.0, [xk])
                    for k in range(5):
                        TS("dve", diag[:, k, :], cst("ident"), cwT[:, ci, k:k + 1], None, ALU.mult, ALU.bypass,
                           ["consts", "cwT"], ["diag"])
                    for tg in range(4):
                        bi = tg % 2
                        for kt in range(KT):
                            MM(bank(bi), ws[:, kt, hh * 128:(hh + 1) * 128], hT[:, kt, tg * 512:(tg + 1) * 512],
                               kt == 0, kt == KT - 1, [wk] + HT_ALL[tg * 4:tg * 4 + 4], [bkey(bi)])
                        CP("act" if tg % 2 else "dve", xcb[:, 2 + tg * 512:2 + (tg + 1) * 512], bank(bi), [bkey(bi)], [xk])
                    for tg in range(4):
                        bi = 2 + tg % 2
                        i2 = tg % 2
                        for k in range(5):
                            MM(bank(bi), diag[:, k, :], xcb[:, tg * 512 + k:tg * 512 + k + 512], k == 0, k == 4,
                               ["diag", xk], [bkey(bi)])
                        ck = ("ctmp", i2)
                        ACTF(ce[i2], bank(bi), AF.Exp, [bkey(bi)], [ck], scale=-1.0)
                        ACTF(ce[i2], ce[i2], AF.Ln, [ck], [ck], bias=1.0)
                        ACTF(ce[i2], ce[i2], AF.Exp, [ck], [ck], scale=-1.0)
                        dst = dstT[:, hh, tg * 512:(tg + 1) * 512]
                        dk = ("gT", which, hh, tg)
                        if which == 2:
                            TT("dve", dst, ce[i2], bank(bi), ALU.mult, [ck, bkey(bi)], [dk])
                        else:
                            TT("dve", cy[i2], ce[i2], bank(bi), ALU.mult, [ck, bkey(bi)], [("cy", i2)])
                            TT("pool", cysq, cy[i2], cy[i2], ALU.mult, [("cy", i2)], ["cysq"])
                            MM(bank(4), ones_b[:], cysq, True, True, ["ones_b", "cysq"], [bkey(4)])
                            ACTF(crs, bank(4), AF.Ln, [bkey(4)], ["crs"], bias=EPS)
                            ACTF(crs, crs, AF.Exp, ["crs"], ["crs"], scale=-0.5)
                            if which == 0:
                                STT(dst, cy[i2], GSCALE, crs, ALU.mult, ALU.mult, [("cy", i2), "crs"], [dk])
                            else:
                                TT("dve", dst, cy[i2], crs, ALU.mult, [("cy", i2), "crs"], [dk])
            MARK("d1")
            P.barrier()
            DMA("pool", wsl[0], w_in_d[:, C_DZ + hs0 * 128:C_DZ + (hs0 + HG) * 128].rearrange("(k p) c -> p k c", p=128),
                ("w_wsl", 0), [], [("wsl", 0)])
            DMA("pool", wsl[1], w_in_d[:, C_MB + hs0 * 128:C_MB + (hs0 + HG) * 128].rearrange("(k p) c -> p k c", p=128),
                ("w_wsl", 1), [], [("wsl", 1)])

            def gT_keys(which, n):
                return [("gT", which, hh, n // 4) for hh in range(HG)]

            stored = set()
            esc_all = [dabs[:, 0:8, :].rearrange("p a b -> p (a b)").rearrange("p (k n h) -> p k n h", k=4, n=NT),
                       dabs[:, 8:16, :].rearrange("p a b -> p (a b)").rearrange("p (k n h) -> p k n h", k=4, n=NT)]
            for d_ in range(2):
                Mc_ = cst("b_le") if d_ == 0 else cst("b_ge")
                Ms_ = cst("b_gt") if d_ == 0 else cst("b_lt")
                for ki, mk in enumerate((Mc_, Ms_, cst("csel0"), cst("csel1"))):
                    MM(bank(d_)[:, ki * 64:(ki + 1) * 64].rearrange("p (n h) -> p n h", n=NT), mk,
                       g_raw[:, :, d_, hs0:hs0 + HG], True, True, ["consts", "g_raw"], [bkey(d_)])
                ACTF(esc_all[d_].rearrange("p k n h -> p (k n h)"), bank(d_)[:, 0:256], AF.Exp, [bkey(d_)], [("esc_all", d_), "dabs"])

            gb0 = ptb[:, 0:512]
            gb1 = ptb[:, 512:1024]
            xbank = ptb[:, 1024:2048].bitcast(F32)
            XK = ("pb", 7)

            def gdn_tile(d_, n):
                B = DB[d_]
                dk = lambda nm: (nm, d_)
                Mc = cst("b_le") if d_ == 0 else cst("b_ge")
                Ms = cst("b_gt") if d_ == 0 else cst("b_lt")
                bg = B["bg"]
                GMB, Wd, decT, Lm, LTm, XT = B["GMB"], B["Wd"], B["decT"], B["Lm"], B["LTm"], B["XT"]
                kbg, vbeta, qd_tok = B["kbg"], B["vbeta"], B["qd_tok"]
                Ppd = [B["Pp0"], B["Pp1"]]
                PTpd = [B["PTp0"], B["PTp1"]]
                pa, pb_ = (0, 1) if d_ == 0 else (2, 3)
                tsl = slice(n * 128, (n + 1) * 128)
                gv = g_raw[:, n, d_, hs0:hs0 + HG]
                bv = beta[:, n, d_, hs0:hs0 + HG]
                EA = esc_all[d_]
                e_cum, e_tail = EA[:, 0, n, :], EA[:, 1, n, :]
                second = n in stored
                if second:
                    par = n % 2
                    DMA("sp", oland[par].rearrange("p h d -> p (h d)"), gdn_o[n], ("oland", par), [("o_dram", n)], [("oland", par)])
                TT("dve", bg, bv, e_cum, ALU.mult, ["beta", ("esc_all", d_)], [dk("bg")])
                for hh in range(HG):
                    TR(gb0[:, hh * 128:(hh + 1) * 128], gkT[:, hh, tsl], ident_b[:], gT_keys(1, n) + ["ident_b"], [("pbb", 0)])
                for hh in range(HG):
                    TR(gb1[:, hh * 128:(hh + 1) * 128], gvT[:, hh, tsl], ident_b[:], gT_keys(2, n) + ["ident_b"], [("pbb", 0)])
                TT("dve", kbg, v4(gb0), bc_h(bg), ALU.mult, [("pbb", 0), dk("bg")], [dk("kbg")])
                TT("dve", B["ktl"], v4(gb0), bc_h(e_tail), ALU.mult, [("pbb", 0), ("esc_all", d_)], [dk("ktl")])
                TT("dve", vbeta, v4(gb1), bc_h(bv), ALU.mult, [("pbb", 0), "beta"], [dk("vbeta")])
                for hh in range(HG):
                    TR(gb0[:, hh * 128:(hh + 1) * 128], gqT[:, hh, tsl], ident_b[:], gT_keys(0, n) + ["ident_b"], [("pbb", 0)])
                TT("dve", qd_tok, v4(gb0), bc_h(e_cum), ALU.mult, [("pbb", 0), ("esc_all", d_)], [dk("qd_tok")])
                for hh in range(HG):
                    TR(gb1[:, hh * 128:(hh + 1) * 128], qd_tok[:, hh, :], ident_b[:], [dk("qd_tok"), "ident_b"], [("pbb", 0)])
                CP("act", B["qdTg"], v4(gb1), [("pbb", 0)], [dk("qdTg")])
                TT("pool", GMB, bc_h(gv), bc_m(Ms), ALU.mult, ["g_raw", "consts"], [dk("GMB")])
                MM(bank(pb_), Mc, GMB.rearrange("p h s -> p (h s)"), True, True, ["consts", dk("GMB")], [bkey(pb_)])
                ACTF(Wd.rearrange("p h s -> p (h s)"), bank(pb_), AF.Exp, [bkey(pb_)], [dk("Wd")])
                TT("pool", GMB, bc_h(bv), bc_m(Ms), ALU.mult, ["beta", "consts"], [dk("GMB")])
                TT("pool", Wd, Wd, GMB, ALU.mult, [dk("Wd"), dk("GMB")], [dk("Wd")])
                TT("pool", GMB, bc_h(gv), bc_m(Mc), ALU.mult, ["g_raw", "consts"], [dk("GMB")])
                MM(bank(pa), Ms, GMB.rearrange("p h s -> p (h s)"), True, True, ["consts", dk("GMB")], [bkey(pa)])
                ACTF(decT.rearrange("p h s -> p (h s)"), bank(pa), AF.Exp, [bkey(pa)], [dk("decT")])
                TT("pool", decT, decT, bc_m(Mc), ALU.mult, [dk("decT"), "consts"], [dk("decT")])
                for hh in range(HG):
                    MM(bank(pb_)[:, hh * 128:(hh + 1) * 128], gkT[:, hh, tsl], gkT[:, hh, tsl], True, True,
                       gT_keys(1, n), [bkey(pb_)])
                TT("dve", Lm, v4(bank(pb_)), Wd, ALU.mult, [bkey(pb_), dk("Wd")], [dk("Lm")])
                for hh in range(HG):
                    MM(bank(pa)[:, hh * 128:(hh + 1) * 128], gkT[:, hh, tsl], gqT[:, hh, tsl], True, True,
                       gT_keys(1, n) + gT_keys(0, n), [bkey(pa)])
                TT("dve", B["attnT"], v4(bank(pa)), decT, ALU.mult, [bkey(pa), dk("decT")], [dk("attnT")])
                for hh in range(HG):
                    TR(gb0[:, hh * 128:(hh + 1) * 128], Lm[:, hh, :], ident_b[:], [dk("Lm"), "ident_b"], [("pbb", 0)])
                CP("act", LTm, v4(gb0), [("pbb", 0)], [dk("LTm")])
                TT("dve", XT, ident_bc4, v4(gb0), ALU.subtract, ["ident_b", ("pbb", 0)], [dk("XT")])
                Pc, PTc = Lm, LTm
                pck, ptk_ = dk("Lm"), dk("LTm")
                for it in range(5):
                    Pn, PTn = Ppd[it % 2], PTpd[it % 2]
                    pnk, ptnk = ("Pp", it % 2, d_), ("PTp", it % 2, d_)
                    for hh in range(HG):
                        MM(bank(pb_)[:, hh * 128:(hh + 1) * 128], PTc[:, hh, :], Pc[:, hh, :], True, True, [pck, ptk_], [bkey(pb_)])
                    CP("act", Pn, v4(bank(pb_)), [bkey(pb_)], [pnk])
                    if it < 4:
                        for hh in range(HG):
                            MM(bank(pa)[:, hh * 128:(hh + 1) * 128], Pc[:, hh, :], PTc[:, hh, :], True, True, [pck, ptk_], [bkey(pa)])
                        CP("dve", PTn, v4(bank(pa)), [bkey(pa)], [ptnk])
                    for hh in range(HG):
                        MM(xbank[:, hh * 128:(hh + 1) * 128], Pn[:, hh, :], XT[:, hh, :], True, True, [pnk, dk("XT")], [XK])
                    TT("dve", XT, XT, v4(xbank), ALU.add, [dk("XT"), XK], [dk("XT")])
                    Pc, PTc, pck, ptk_ = Pn, PTn, pnk, ptnk
                for hh in range(HG):
                    MM(bank(pa)[:, hh * 128:(hh + 1) * 128], XT[:, hh, :], vbeta[:, hh, :], True, True, [dk("XT"), dk("vbeta")], [bkey(pa)])
                CP("act", B["u_sb"], v4(bank(pa)), [bkey(pa)], [dk("u_sb")])
                for hh in range(HG):
                    MM(bank(pb_)[:, hh * 128:(hh + 1) * 128], kbg[:, hh, :], XT[:, hh, :], True, True, [dk("kbg"), dk("XT")], [bkey(pb_)])
                CP("dve", B["wT_sb"], v4(bank(pb_)), [bkey(pb_)], [dk("wT_sb")])
                sb0 = 4
                Sg, Sg_bf, vnew = B["Sg"], B["Sg_bf"], B["vnew"]
                chunks = (0, 1) if d_ == 0 else (1, 0)
                for c in chunks:
                    sl = slice(c * 64, c * 64 + 64)
                    for hh in range(HG):
                        MM(bank(sb0)[sl, hh * 128:(hh + 1) * 128], B["wT_sb"][:, hh, sl], Sg_bf[:, hh, :], True, True,
                           [dk("wT_sb"), dk("Sg_bf")], [bkey(sb0)])
                    TT("dve", vnew[sl], B["u_sb"][sl], v4(bank(sb0))[sl], ALU.subtract, [dk("u_sb"), bkey(sb0)], [dk("vnew")])
                    for hh in range(HG):
                        MM(bank(sb0 + 1)[sl, hh * 128:(hh + 1) * 128], B["qdTg"][:, hh, sl], Sg_bf[:, hh, :], True, False,
                           [dk("qdTg"), dk("Sg_bf")], [bkey(sb0 + 1)])
                        MM(bank(sb0 + 1)[sl, hh * 128:(hh + 1) * 128], B["attnT"][sl, hh, sl], vnew[sl, hh, :], False, True,
                           [dk("attnT"), dk("vnew")], [bkey(sb0 + 1)])
                    for hh in range(HG):
                        MM(bank(sb0)[:, hh * 128:(hh + 1) * 128], B["ktl"][sl, hh, :], vnew[sl, hh, :], True, True,
                           [dk("ktl"), dk("vnew")], [bkey(sb0)])
                    TT("pool", Sg, Sg, bc_h(EA[:, 2 + c, n, :]), ALU.mult, [dk("Sg"), ("esc_all", d_)], [dk("Sg")])
                    TT("dve", Sg, Sg, v4(bank(sb0)), ALU.add, [dk("Sg"), bkey(sb0)], [dk("Sg")])
                    CP("act", Sg_bf, Sg, [dk("Sg")], [dk("Sg_bf")])
                    if not second:
                        CP("act", B["ostage"][sl], v4(bank(sb0 + 1))[sl], [bkey(sb0 + 1)], [dk("ostage")])
                    else:
                        TT("dve", osum[sl], v4(bank(sb0 + 1))[sl], oland[n % 2][sl], ALU.add,
                           [bkey(sb0 + 1), ("oland", n % 2)], ["osum"])
                if not second:
                    stored.add(n)
                    DMA("sp", gdn_o[n], B["ostage"].rearrange("p h d -> p (h d)"), ("ost", d_), [dk("ostage")], [("o_dram", n)])
                    return
                osq = fsig.rearrange("p (h d) -> p h d", h=HG)
                TT("pool", osq, osum, osum, ALU.mult, ["osum"], ["fsig"])
                P.op("dve", lambda e: e.tensor_reduce(out=frs[:, 0:HG], in_=osq, axis=AX.X, op=ALU.add), ["fsig"], ["frs"], cost=600.0)
                rstd_inplace(frs[:, 0:HG], 128, "frs")
                for half, ws in enumerate(wsl):
                    for kt in range(KT):
                        MM(bank(half), hT[:, kt, tsl], ws[:, kt, :], kt == 0, kt == KT - 1,
                           [("wsl", half), ("hT", n)], [bkey(half)])
                fGf = fG.rearrange("p h d -> p (h d)")
                for half in range(2):
                    ACTF(fsig, bank(half), AF.Exp, [bkey(half)], ["fsig"], scale=-1.0)
                    ACTF(fsig, fsig, AF.Ln, ["fsig"], ["fsig"], bias=1.0)
                    ACTF(fsig, fsig, AF.Exp, ["fsig"], ["fsig"], scale=-1.0)
                    if half == 0:
                        TT("dve", fGf, fsig, bank(0), ALU.mult, ["fsig", bkey(0)], ["fG"])
                    else:
                        TT("pool", fGf, fGf, fsig, ALU.mult, ["fsig", "fG"], ["fG"])
                TT("pool", fG, fG, bc_m(gdnw_bc), ALU.mult, ["fG", "gdnw_bc"], ["fG"])
                TT("pool", osum, osum, bc_h(frs[:, 0:HG]), ALU.mult, ["osum", "frs"], ["osum"])
                TT("pool", osum, osum, fG, ALU.mult, ["osum", "fG"], ["osum"])
                mslice = mixed[:, n, hs0 * 128:(hs0 + HG) * 128].rearrange("p (h d) -> p h d", h=HG)
                TT("dve", mslice, mslice, osum, ALU.add, ["osum", ("mixed", n)], [("mixed", n)])

            for d_ in range(2):
                MEMSET("dve", DB[d_]["Sg"], 0.0, [("Sg", d_)])
                CP("act", DB[d_]["Sg_bf"], DB[d_]["Sg"], [("Sg", d_)], [("Sg_bf", d_)])
            for i in range(NT):
                gdn_tile(0, i)
                gdn_tile(1, NT - 1 - i)
            MARK("d2")
            P.barrier()

        P.barrier()
        wout_d = dram("w_out", [D, D])
        off = 0
        x1, off = carve(off, [NT, D], F32)
        X1_END = off
        mT, off = carve(off, [KT, T], BF16)
        wout, off = carve(off, [KT, D], BF16)
        hn2 = [None, None]
        hn2[0], off = carve(off, [D], BF16)
        hn2[1], off = carve(off, [D], BF16)
        junk, off = carve(off, [D], BF16)
        n2_bc, off = carve(off, [D], F32)
        DMA("sp", n2_bc, n2_d.partition_broadcast(128), "c_n2", [], ["n2_bc"])
        DMA("pool", wout, wout_d.rearrange("(k p) c -> p k c", p=128), "w_wout", [], ["wout"])
        for n in range(NT):
            b = n % 2
            for kt in range(KT):
                TR(bbank(b)[:, kt * 128:(kt + 1) * 128], mixed[:, n, kt * 128:(kt + 1) * 128], ident_b[:],
                   [("mixed", n), "ident_b"], [("pbb", b)])
            CP("act", mT[:, :, n * 128:(n + 1) * 128], bbank(b).rearrange("p (k t) -> p k t", k=KT), [("pbb", b)], [("mT", n)])
        for n in range(NT):
            tsl = slice(n * 128, (n + 1) * 128)
            DMA("sp", x1[:, n, :], x_d[tsl, :], ("x1ld", n % 4), [], [("x1", n)])
            pp = pt[n % 2]
            for half in range(2):
                for kt in range(KT):
                    MM(pp[:, half * 512:(half + 1) * 512], mT[:, kt, tsl], wout[:, kt, half * 512:(half + 1) * 512],
                       kt == 0, kt == KT - 1, [("mT", n), "wout"], [bkey((n % 2) * 2 + half)])
            TT("dve", x1[:, n, :], x1[:, n, :], pp[:, :], ALU.add, [("x1", n), bkey((n % 2) * 2), bkey((n % 2) * 2 + 1)], [("x1", n)])
            b = n % 2
            ssap = small[:, 8 + b:9 + b]
            ssk = ("ss2", b)
            ACTF(junk, x1[:, n, :], AF.Square, [("x1", n)], ["junk", ssk], accum_out=ssap)
            rstd_inplace(ssap, D, ssk)
            STT(mixed[:, n, :], x1[:, n, :], ssap, n2_bc, ALU.mult, ALU.mult, [("x1", n), ssk, "n2_bc"], [("mixed", n)])
            for kt in range(KT):
                TR(bbank(b)[:, kt * 128:(kt + 1) * 128], mixed[:, n, kt * 128:(kt + 1) * 128], ident_b[:],
                   [("mixed", n), "ident_b"], [("pbb", b)])
            CP("act", hT[:, :, tsl], bbank(b).rearrange("p (k t) -> p k t", k=KT), [("pbb", b)], [("hT", n)])
        MARK("e0")

        P.barrier()
        NB = 64
        wr_d = dram("moe_wr", [D, 36])
        wgu0_d = dram("moe_wgu0", [4096, 2048])
        wgu1_d = dram("moe_wgu1", [4096, 2048])
        wdr_d = dram("moe_wdr", [4096, 2048])
        xb_d = nc.dram_tensor("moe_xb", [NB * 128, D], BF16, kind="Internal").ap()
        yb_d = nc.dram_tensor("moe_yb", [NB * 128, D], F32, kind="Internal").ap()
        off = X1_END
        stg = []
        for i in range(3):
            a, off = carve(off, [2048], F32)
            stg.append(a)
        wgu_bf = []
        wd_bf = []
        for i in range(2):
            a, off = carve(off, [KT, 512], BF16)
            wgu_bf.append(a)
            a, off = carve(off, [2, D], BF16)
            wd_bf.append(a)
        wr, off = carve(off, [KT, 36], BF16)
        lg, off = carve(off, [NT, 36], F32)
        oh1, off = carve(off, [NT, 32], F32)
        oh2, off = carve(off, [NT, 32], F32)
        msk, off = carve(off, [NT, 32], F32)
        rank, off = carve(off, [NT, 32], F32)
        tmp3, off = carve(off, [NT, 32], F32)
        gtmp, off = carve(off, [NT, 4], F32)
        ohg, off = carve(off, [NT, 4], F32)
        rv, off = carve(off, [8, NT], F32)
        mcum, off = carve(off, [32], F32)
        cnt, off = carve(off, [32], F32)
        padded, off = carve(off, [32], F32)
        ends, off = carve(off, [32], F32)
        pstart, off = carve(off, [32], F32)
        ebf, off = carve(off, [NB], F32)
        widx_f, off = carve(off, [NB], F32)
        widx, off = carve(off, [NB], I32)
        dest_f, off = carve(off, [2, NT], F32)
        dest_i, off = carve(off, [2, NT], I32)
        MOE_END = off
        cmpb = stg[0].rearrange("p (b e) -> p b e", b=NB)
        cmpj = stg[1][:, 0:512].rearrange("p (e j) -> p e j", e=32)
        ht32 = hT[:].rearrange("p k t -> p (k t)").bitcast(F32)
        HTCAP = 32 * 1024
        hoff = 0
        xg, xgT, sil, hid_bf, hidT, ysb = [], [], [], [], [], []
        for i in range(2):
            a, hoff = carve(hoff, [D], BF16, ht32, HTCAP); xg.append(a)
            a, hoff = carve(hoff, [KT, 128], BF16, ht32, HTCAP); xgT.append(a)
            a, hoff = carve(hoff, [256], F32, ht32, HTCAP); sil.append(a)
            a, hoff = carve(hoff, [256], BF16, ht32, HTCAP); hid_bf.append(a)
            a, hoff = carve(hoff, [2, 128], BF16, ht32, HTCAP); hidT.append(a)
            a, hoff = carve(hoff, [D], F32, ht32, HTCAP); ysb.append(a)
        mix32 = mixed[:].rearrange("p n d -> p (n d)").bitcast(F32)
        MIXCAP = 32 * 1024
        moff = 0
        yg = []
        for i in range(2):
            a, moff = carve(moff, [D], F32, mix32, MIXCAP); yg.append(a)
        stgB = []
        for i in range(3):
            a, moff = carve(moff, [2048], F32, mix32, MIXCAP); stgB.append(a)

        DMA("pool", wr, wr_d.rearrange("(k p) c -> p k c", p=128), "w_wr", [], ["wr"])
        for n in range(NT):
            bi = n % 2
            for kt in range(KT):
                MM(bank(bi)[:, 0:36], hT[:, kt, n * 128:(n + 1) * 128], wr[:, kt, :], kt == 0, kt == KT - 1,
                   ["wr", ("hT", n)], [bkey(bi)])
            CP("act", lg[:, n, :], bank(bi)[:, 0:36], [bkey(bi)], ["lg"])
        BIG = 10000.0
        glv = lg[:, :, 0:4]
        elv = lg[:, :, 4:36]

        def RED(out, in_, op, R, W):
            P.op("dve", lambda e: e.tensor_reduce(out=out, in_=in_, axis=AX.X, op=op), R, W, cost=100.0 + _fsz(in_) * 1.0)

        def bcn(ap2, k):
            return ap2.unsqueeze(2).to_broadcast([128, NT, k])

        gmax, gsum, m1, m2, w1, w2 = (rv[:, i, :] for i in range(6))
        RED(gmax, glv, ALU.max, ["lg"], ["rv"])
        TT("dve", ohg, glv, bcn(gmax, 4), ALU.is_equal, ["lg", "rv"], ["ohg"])
        TT("dve", gtmp, glv, bcn(gmax, 4), ALU.subtract, ["lg", "rv"], ["gtmp"])
        ACTF(gtmp, gtmp, AF.Exp, ["gtmp"], ["gtmp"])
        RED(gsum, gtmp, ALU.add, ["gtmp"], ["rv"])
        RECIP(gsum, gsum, ["rv"], ["rv"])
        TS("dve", ohg, ohg, BIG, -BIG, ALU.mult, ALU.add, ["ohg"], ["ohg"])
        TT("dve", msk.rearrange("p n (g e) -> p n g e", g=4), elv.rearrange("p n (g e) -> p n g e", g=4),
           ohg.unsqueeze(3).to_broadcast([128, NT, 4, 8]), ALU.add, ["lg", "ohg"], ["msk"])
        RED(m1, msk, ALU.max, ["msk"], ["rv"])
        TT("dve", oh1, msk, bcn(m1, 32), ALU.is_equal, ["msk", "rv"], ["oh1"])
        STT(msk, oh1, -BIG, msk, ALU.mult, ALU.add, ["oh1", "msk"], ["msk"])
        RED(m2, msk, ALU.max, ["msk"], ["rv"])
        TT("dve", oh2, msk, bcn(m2, 32), ALU.is_equal, ["msk", "rv"], ["oh2"])
        TT("dve", w2, m2, m1, ALU.subtract, ["rv"], ["rv"])
        ACTF(w2, w2, AF.Exp, ["rv"], ["rv"])
        TS("dve", w1, w2, 1.0, None, ALU.add, ALU.bypass, ["rv"], ["rv"])
        RECIP(w1, w1, ["rv"], ["rv"])
        TT("dve", w1, w1, gsum, ALU.mult, ["rv"], ["rv"])
        TT("dve", w2, w2, w1, ALU.mult, ["rv"], ["rv"])
        TT("dve", msk, oh1, oh2, ALU.add, ["oh1", "oh2", "msk"], ["msk"])
        MEMSET("dve", mcum, 0.0, ["mcum"])
        for n in range(NT):
            bi = n % 2
            MM(bank(bi)[:, 0:32], cst("m_lt"), msk[:, n, :], True, False, ["consts", "msk"], [bkey(bi)])
            MM(bank(bi)[:, 0:32], cst("ones"), mcum, False, True, ["consts", "mcum"], [bkey(bi)])
            CP("act", rank[:, n, :], bank(bi)[:, 0:32], [bkey(bi)], ["rank"])
            TT("dve", mcum, mcum, msk[:, n, :], ALU.add, ["mcum", "msk"], ["mcum"])
        MM(bank(0)[:, 0:32], cst("ones"), mcum, True, True, ["consts", "mcum"], [bkey(0)])
        CP("act", cnt, bank(0)[:, 0:32], [bkey(0)], ["cnt"])
        TT("dve", cmpj, cnt.unsqueeze(2).to_broadcast([128, 32, 16]),
           cst("bvals")[:, 0:16].unsqueeze(1).to_broadcast([128, 32, 16]), ALU.is_gt, ["cnt", "consts"], [("stg", 1)])
        RED(padded, cmpj, ALU.add, [("stg", 1)], ["padded"])
        TS("dve", padded, padded, 128.0, None, ALU.mult, ALU.bypass, ["padded"], ["padded"])
        P.op("dve", lambda e: e.tensor_tensor_scan(out=ends, data0=cst("ones")[:, 0:32], data1=padded, initial=0.0,
                                                  op0=ALU.mult, op1=ALU.add), ["consts", "padded"], ["ends"], cost=300.0)
        TT("dve", pstart, ends, padded, ALU.subtract, ["ends", "padded"], ["pstart"])
        TT("dve", rank, rank, pstart.unsqueeze(1).to_broadcast([128, NT, 32]), ALU.add, ["rank", "pstart"], ["rank"])
        for k, ohk in ((0, oh1), (1, oh2)):
            TT("dve", tmp3, ohk, rank, ALU.mult, ["oh1", "oh2", "rank"], ["tmp3"])
            RED(dest_f[:, k, :], tmp3, ALU.add, ["tmp3"], ["dest_f"])
        CP("dve", dest_i, dest_f, ["dest_f"], ["dest_i"])
        TT("dve", cmpb, ends.unsqueeze(1).to_broadcast([128, NB, 32]),
           cst("bvals")[:, 0:NB].unsqueeze(2).to_broadcast([128, NB, 32]), ALU.is_le, ["ends", "consts"], [("stg", 0)])
        RED(ebf, cmpb, ALU.add, [("stg", 0)], ["ebf"])
        STT(widx_f, ebf, 128.0, cst("pidx")[:, 0:NB], ALU.mult, ALU.add, ["ebf", "consts"], ["widx_f"])
        CP("dve", widx, widx_f, ["widx_f"], ["widx"])
        MARK("e1")

        IOA = bass.IndirectOffsetOnAxis
        regs = {}

        def _pool_init(e):
            regs["bc"] = e.alloc_register("moe_bc")
            e.reg_mov(regs["bc"], 4095)
        P.pool_init = _pool_init
        XB_KEYS = []
        zt, off = carve(off, [D], BF16)
        MEMSET("pool", zt, 0.0, ["zt"])
        DMA("sp", xb_d.rearrange("(p r) d -> p r d", p=128), zt.unsqueeze(1).to_broadcast([128, NB, D]), "xbz", ["zt"], ["xb0"])
        for n in range(NT):
            for k in range(2):
                idx_ap = dest_i[:, k, n:n + 1]
                src_ap = mixed[:, n, :]
                P.dma("pool", lambda e, idx_ap=idx_ap, src_ap=src_ap: e.indirect_dma_start(
                    out=xb_d[:, :], out_offset=IOA(ap=idx_ap, axis=0), in_=src_ap, in_offset=None),
                    ("sc", (2 * n + k) % 4), [("mixed", n), "dest_i", "xb0"], [("xb", n, k)], nbytes=256 * 1024)
                XB_KEYS.append(("xb", n, k))

        def gather_w(dst, src_d, b, skey, extra):
            idx_ap = widx[:, b:b + 1]
            P.dma("pool", lambda e: e.indirect_dma_start(
                out=dst, out_offset=None, in_=src_d[:, :], in_offset=IOA(ap=idx_ap, axis=0),
                bounds_check=regs["bc"], oob_is_err=False),
                skey, ["widx"] + extra, [skey], nbytes=1 << 20)

        YB_KEYS = []
        for b in range(NB):
            s = b % 2
            sset = stg if b % 2 == 0 else stgB
            so = 0 if b % 2 == 0 else 3
            extra = [] if b % 2 == 0 else XB_KEYS
            gather_w(sset[0], wgu0_d, b, ("stg", so + 0), extra)
            gather_w(sset[1], wgu1_d, b, ("stg", so + 1), extra)
            gather_w(sset[2], wdr_d, b, ("stg", so + 2), extra)
            CP("act", wgu_bf[s][:, 0:4, :], sset[0].rearrange("p (k c) -> p k c", k=4), [("stg", so + 0)], [("wgu_bf", s, 0)])
            CP("dve", wgu_bf[s][:, 4:8, :], sset[1].rearrange("p (k c) -> p k c", k=4), [("stg", so + 1)], [("wgu_bf", s, 1)])
            CP("act" if b % 4 < 2 else "dve", wd_bf[s], sset[2].rearrange("p (k c) -> p k c", k=2), [("stg", so + 2)], [("wd_bf", s)])
            DMA("sp", xg[s], xb_d[b * 128:(b + 1) * 128, :], ("xg", s), XB_KEYS, [("xg", s)])
            for kt in range(KT):
                TR(bbank(s)[:, kt * 128:(kt + 1) * 128], xg[s][:, kt * 128:(kt + 1) * 128], ident_b[:],
                   [("xg", s), "ident_b"], [("pbb", s)])
            CP("act", xgT[s], bbank(s).rearrange("p (k t) -> p k t", k=KT), [("pbb", s)], [("xgT", s)])
            hb = bank(s)
            for kt in range(KT):
                MM(hb, xgT[s][:, kt, :], wgu_bf[s][:, kt, :], kt == 0, kt == KT - 1,
                   [("xgT", s), ("wgu_bf", s, 0), ("wgu_bf", s, 1)], [bkey(s)])
            ACTF(sil[s], hb[:, 0:256], AF.Silu, [bkey(s)], [("sil", s)])
            TT("dve", hid_bf[s], sil[s], hb[:, 256:512], ALU.mult, [("sil", s), bkey(s)], [("hid_bf", s)])
            for ft in range(2):
                TR(bbank(s)[:, ft * 128:(ft + 1) * 128], hid_bf[s][:, ft * 128:(ft + 1) * 128], ident_b[:],
                   [("hid_bf", s), "ident_b"], [("pbb", s)])
            CP("act", hidT[s], bbank(s)[:, 0:256].rearrange("p (k t) -> p k t", k=2), [("pbb", s)], [("hidT", s)])
            yp = pt[1 + s]
            for half in range(2):
                for ft in range(2):
                    MM(yp[:, half * 512:(half + 1) * 512], hidT[s][:, ft, :], wd_bf[s][:, ft, half * 512:(half + 1) * 512],
                       ft == 0, ft == 1, [("hidT", s), ("wd_bf", s)], [bkey(2 + 2 * s + half)])
            CP("act" if b % 2 else "dve", ysb[s], yp[:, :], [bkey(2 + 2 * s), bkey(3 + 2 * s)], [("ysb", s)])
            DMA("sp", yb_d[b * 128:(b + 1) * 128, :], ysb[s], ("yst", s), [("ysb", s)], [("yb", b)])
            YB_KEYS.append(("yb", b))
        MARK("e2")
        for n in range(NT):
            for k in range(2):
                s = (2 * n + k) % 2
                idx_ap = dest_i[:, k, n:n + 1]
                dst = yg[s]
                P.dma("pool", lambda e, idx_ap=idx_ap, dst=dst: e.indirect_dma_start(
                    out=dst, out_offset=None, in_=yb_d[:, :], in_offset=IOA(ap=idx_ap, axis=0)),
                    ("yg", s), YB_KEYS + ["dest_i"], [("yg", s)], nbytes=512 * 1024)
                wk = rv[:, 4 + k, n:n + 1]
                STT(x1[:, n, :], yg[s], wk, x1[:, n, :], ALU.mult, ALU.add, [("yg", s), "rv", ("x1", n)], [("x1", n)])

        P.barrier()
        off = X1_END
        nf_bc, off = carve(off, [D], F32)
        ob = [None, None]
        ob[0], off = carve(off, [D], F32)
        ob[1], off = carve(off, [D], F32)
        junk2, off = carve(off, [D], BF16)
        DMA("sp", nf_bc, nf_d.partition_broadcast(128), "c_nf", [], ["nf_bc"])
        for n in range(NT):
            b = n % 2
            ssap = small[:, 12 + b:13 + b]
            ssk = ("ss3", b)
            ACTF(junk2, x1[:, n, :], AF.Square, [("x1", n)], ["junk2", ssk], accum_out=ssap)
            rstd_inplace(ssap, D, ssk)
            STT(ob[b], x1[:, n, :], ssap, nf_bc, ALU.mult, ALU.mult, [("x1", n), ssk, "nf_bc"], [("ob", b)])
            DMA("sp", out_d[n * 128:(n + 1) * 128, :], ob[b], ("out_st", b), [("ob", b)], [("out", n)])
        if not dbg:
            P.wait_all("sp", [("out", n) for n in range(NT)])
        if dbg:
            P.enabled = True
            P.barrier()
            for n in range(NT):
                DMA("sp", dbg_d[n * 128:(n + 1) * 128, :], x1[:, n, :], ("dbg_out", n % 2), [("x1", n)], [("dbg", n)])
            P.wait_all("sp", [("dbg", n) for n in range(NT)] + [("out", n) for n in range(NT)])
        P.emit()
    return nc


def make_in_maps(inputs, n_cores=8):
    f = lambda k: np.asarray(inputs[k], np.float32)
    x = f("x")
    _gu = np.concatenate([f("moe_w_gate")[0], f("moe_w_up")[0]], axis=2).reshape(32, 8, 128, 512).transpose(0, 2, 1, 3)
    shared = {
        "norm1_w": f("norm1_w").reshape(1, D),
        "norm2_w": f("norm2_w").reshape(1, D),
        "norm_f_w": f("norm_f_w").reshape(1, D),
        "consts": CONST_ARR,
        "w_in": np.ascontiguousarray(f("w_in")[0]),
        "gla_w2b_f": np.ascontiguousarray(np.concatenate([f("gla_gate_w2_fwd")[0], f("gla_gate_b_fwd")], axis=0)),
        "gla_w2b_b": np.ascontiguousarray(np.concatenate([f("gla_gate_w2_bwd")[0], f("gla_gate_b_bwd")], axis=0)),
        "gla_norm_w": f("gla_norm_w").reshape(1, 256),
        "w_out": np.ascontiguousarray(f("w_out")[0]),
        "moe_wr": np.ascontiguousarray(np.concatenate([f("moe_w_group")[0], f("moe_w_router")[0]], axis=1)),
        "moe_wgu0": _gu[:, :, 0:4, :].reshape(4096, 2048).copy(),
        "moe_wgu1": _gu[:, :, 4:8, :].reshape(4096, 2048).copy(),
        "moe_wdr": np.ascontiguousarray(f("moe_w_down")[0].reshape(32, 2, 128, 1024).transpose(0, 2, 1, 3)).reshape(4096, 2048),
        "gdn_norm_w": f("gdn_norm_w").reshape(1, 128),
        "gdn_vec": np.ascontiguousarray(np.concatenate([f("gdn_dt_bias_fwd")[0], f("gdn_dt_bias_bwd")[0],
                                                        f("gdn_a_log_fwd")[0], f("gdn_a_log_bwd")[0]]).reshape(1, 32)),
        "gdn_conv_wT": np.ascontiguousarray(f("gdn_conv_w")[0].T.reshape(24, 128, 5).transpose(1, 0, 2)),
    }
    maps = []
    for c in range(n_cores):
        m = dict(shared)
        m["x"] = np.ascontiguousarray(x[c])
        maps.append(m)
    return maps


def kernel(**inputs):
    nc = build()
    in_maps = make_in_maps(inputs)
    res = run_bass_kernel_spmd(nc, in_maps, core_ids=list(range(8)))
    out = np.stack([np.asarray(r["out"]) for r in res.results], axis=0)
    return out.astype(np.float32)
```

```python
import contextlib
import heapq
import numpy as np
import concourse.bass as bass
import concourse.mybir as mybir
from concourse.bass_utils import run_bass_kernel_spmd

F32 = mybir.dt.float32
BF16 = mybir.dt.bfloat16
I32 = mybir.dt.int32
AF = mybir.ActivationFunctionType
ALU = mybir.AluOpType
AX = mybir.AxisListType

T = 2048
D = 1024
NT = T // 128
KT = D // 128
EPS = 1e-6
SAME_ENGINE_SYNC = True
EPOCH = 20000
SYNC_NS = 120.0
DMA_LAT_NS = 2200.0


class Prog:
    ENGS = ("pe", "act", "dve", "pool", "sp")

    def __init__(self, nc, stack):
        self.nc = nc
        self.stack = stack
        self.streams = {e: [] for e in self.ENGS}
        self.count = {e: 0 for e in self.ENGS}
        self.esems = {e: [] for e in self.ENGS}
        self.known = {e: {} for e in self.ENGS}
        self.last_write = {}
        self.readers = {}
        self.dma_sems = {}
        self.dma_vals = {}
        self.dma_last = {}
        self.enabled = True
        self.seg = []
        self.ticks = {}
        self.nops = 0
        self.seg_base = 0
        self.pool_init = None

    def _new_sem(self, name):
        return self.stack.enter_context(self.nc.semaphore(name))

    @staticmethod
    def _psum_fix(reads, writes):
        r2, w2 = [], list(writes)
        for k in reads:
            if isinstance(k, tuple) and k[0] in ("pb", "pbb"):
                if k not in w2:
                    w2.append(k)
            else:
                r2.append(k)
        return r2, w2

    def _record(self, eng, fn, reads, writes, cost, kind, semkey=None):
        reads, writes = self._psum_fix(list(reads), list(writes))
        oid = self.nops
        self.nops += 1
        preds = set()
        for r in reads:
            t = self.last_write.get(r)
            if t is not None:
                preds.add(t)
        for w in writes:
            t = self.last_write.get(w)
            if t is not None:
                preds.add(t)
            preds.update(self.readers.get(w, ()))
        if kind == "dma":
            prev = self.dma_last.get(semkey)
            if prev is not None:
                preds.add(prev)
            self.dma_last[semkey] = oid
        preds = {p for p in preds if p >= self.seg_base}
        self.seg.append(dict(id=oid, eng=eng, fn=fn, preds=preds, cost=float(cost), kind=kind, semkey=semkey))
        for w in writes:
            self.last_write[w] = oid
            self.readers[w] = []
        for r in reads:
            self.readers.setdefault(r, []).append(oid)
        return oid

    def op(self, eng, fn, reads=(), writes=(), cost=300.0):
        if not self.enabled:
            return
        self._record(eng, fn, reads, writes, cost, "op")

    def dma(self, eng, fn, semkey, reads=(), writes=(), nbytes=1 << 20):
        if not self.enabled:
            return
        self._record(eng, fn, reads, writes, DMA_LAT_NS + nbytes / 160.0, "dma", semkey)

    def wait_all(self, eng, keys):
        self._record(eng, None, list(keys), [], 0.0, "op")

    def _schedule_segment(self):
        ops = self.seg
        if not ops:
            return
        byid = {o["id"]: o for o in ops}
        succ = {o["id"]: [] for o in ops}
        indeg = {}
        for o in ops:
            indeg[o["id"]] = len(o["preds"])
            for p in o["preds"]:
                succ[p].append(o["id"])
        ready_t = {o["id"]: 0.0 for o in ops}
        finish = {}
        heaps = {e: [] for e in self.ENGS}
        for o in ops:
            if indeg[o["id"]] == 0:
                heapq.heappush(heaps[o["eng"]], (0.0, o["id"]))
        etime = {e: 0.0 for e in self.ENGS}
        order = {e: [] for e in self.ENGS}
        remaining = len(ops)
        while remaining:
            best = None
            for e in self.ENGS:
                h = heaps[e]
                if not h:
                    continue
                rt, oid = h[0]
                st = max(rt, etime[e])
                if best is None or (st, oid) < (best[0], best[1]):
                    best = (st, oid, e)
            st, oid, e = best
            heapq.heappop(heaps[e])
            o = byid[oid]
            if o["kind"] == "dma":
                etime[e] = st + 150.0
                fin = st + o["cost"]
            else:
                etime[e] = st + o["cost"]
                fin = etime[e]
            finish[oid] = fin
            order[e].append(o)
            remaining -= 1
            for s in succ[oid]:
                so = byid[s]
                lat = SYNC_NS if (so["eng"] != e or o["kind"] == "dma") else (60.0 if e != "pe" else 0.0)
                ready_t[s] = max(ready_t[s], fin + lat)
                indeg[s] -= 1
                if indeg[s] == 0:
                    heapq.heappush(heaps[so["eng"]], (ready_t[s], s))
        self.est_ns = getattr(self, "est_ns", 0.0) + max(list(finish.values()) + [0.0])
        for o in ops:
            if o["kind"] == "dma":
                k = o["semkey"]
                if k not in self.dma_sems:
                    self.dma_sems[k] = self._new_sem(f"d{len(self.dma_sems)}")
                    self.dma_vals[k] = 0
                self.dma_vals[k] += 16
                self.ticks[o["id"]] = (self.dma_sems[k], self.dma_vals[k], "dma")
        def needs_sem(o):
            for s_ in succ[o["id"]]:
                se = byid[s_]["eng"]
                if se != o["eng"] or (SAME_ENGINE_SYNC and se != "pe"):
                    return True
            return False
        for e in self.ENGS:
            real = [o for o in order[e] if o["kind"] == "op" and o["fn"] is not None]
            for i_, o in enumerate(real):
                o["sig"] = needs_sem(o) or i_ == len(real) - 1
        for e in self.ENGS:
            for o in order[e]:
                if o["kind"] == "op" and o["fn"] is not None and o["sig"]:
                    c = self.count[e]
                    ep, v = divmod(c, EPOCH)
                    while len(self.esems[e]) <= ep:
                        self.esems[e].append(self._new_sem(f"s_{e}_{len(self.esems[e])}"))
                    self.count[e] = c + 1
                    self.ticks[o["id"]] = (self.esems[e][ep], v + 1, e)
        for e in self.ENGS:
            for o in order[e]:
                waits = {}
                for p in o["preds"]:
                    if byid[p]["eng"] == e and byid[p]["kind"] == "op" and (not SAME_ENGINE_SYNC or e == "pe"):
                        continue
                    sem, val, src = self.ticks[p]
                    sid = id(sem)
                    if self.known[e].get(sid, 0) >= val:
                        continue
                    if sid not in waits or waits[sid][1] < val:
                        waits[sid] = (sem, val)
                for sid, (sem, val) in waits.items():
                    self.known[e][sid] = val
                inc = None
                if o["fn"] is not None and o["id"] in self.ticks:
                    sem, val, src = self.ticks[o["id"]]
                    inc = (sem, 16 if o["kind"] == "dma" else 1)
                self.streams[e].append((o["fn"], list(waits.values()), inc))
        self.seg = []
        self.seg_base = self.nops

    def barrier(self):
        if not self.enabled and not self.seg:
            return
        self._schedule_segment()
        ticks = []
        for e2 in self.ENGS:
            c = self.count[e2]
            if c > 0:
                ep, v = divmod(c - 1, EPOCH)
                ticks.append((self.esems[e2][ep], v + 1))
        for k, sem in self.dma_sems.items():
            ticks.append((sem, self.dma_vals[k]))
        for eng in self.ENGS:
            waits = []
            for (sem, val) in ticks:
                if self.known[eng].get(id(sem), 0) >= val:
                    continue
                self.known[eng][id(sem)] = val
                waits.append((sem, val))
            if waits:
                self.streams[eng].append((None, waits, None))

    def emit(self):
        self._schedule_segment()
        nc = self.nc
        with nc.Block() as block:
            def run(e, stream):
                for fn, waits, inc in stream:
                    for sem, val in waits:
                        e.wait_ge(sem, val)
                    if fn is None:
                        continue
                    ins = fn(e)
                    if inc is not None:
                        ins.then_inc(inc[0], inc[1])

            @block.tensor
            def _(e):
                run(e, self.streams["pe"])

            @block.scalar
            def _(e):
                run(e, self.streams["act"])

            @block.vector
            def _(e):
                run(e, self.streams["dve"])

            @block.gpsimd
            def _(e):
                if self.pool_init is not None:
                    self.pool_init(e)
                run(e, self.streams["pool"])

            @block.sync
            def _(e):
                run(e, self.streams["sp"])


def _fsz(ap):
    s = ap.shape
    n = 1
    for v in s[1:]:
        n *= int(v)
    return n


C_GQ, C_GK, C_GV, C_GR = 0, 512, 1024, 2048
C_GLF, C_GLB = 3072, 3088
C_DQ, C_DK, C_DV, C_DZ = 3104, 4128, 5152, 6176
C_DAB = 7200
C_MA, C_MB = 7232, 8256
D_IN = 9280


def host_consts():
    r = np.arange(128)[:, None]
    t = np.arange(128)[None, :]
    same = (r // 64) == (t // 64)
    c = {}
    c["ident"] = np.eye(128, dtype=np.float32)
    c["a_le"] = np.where(r <= t, -1.0 / 16, 0.0)
    c["a_ge"] = np.where(r >= t, -1.0 / 16, 0.0)
    c["a_gt"] = np.where(r > t, -1.0 / 16, 0.0)
    c["a_lt"] = np.where(r < t, -1.0 / 16, 0.0)
    c["m_le"] = np.where(r <= t, 1.0, 0.0)
    c["m_ge"] = np.where(r >= t, 1.0, 0.0)
    c["b_le"] = np.where((r <= t) & same, 1.0, 0.0)
    c["b_ge"] = np.where((r >= t) & same, 1.0, 0.0)
    c["b_gt"] = np.where((r > t) & same, 1.0, 0.0)
    c["b_lt"] = np.where((r < t) & same, 1.0, 0.0)
    c["csel0"] = np.where(r < 64, 1.0, 0.0) + 0.0 * t
    c["csel1"] = np.where(r >= 64, 1.0, 0.0) + 0.0 * t
    c["ones"] = np.ones((128, 128))
    c["m_lt"] = np.where(r < t, 1.0, 0.0)
    c["bvals"] = 128.0 * t + 0.0 * r
    c["pidx"] = 1.0 * r + 0.0 * t
    names = list(c.keys())
    arr = np.stack([np.asarray(c[n], np.float32) for n in names], axis=1)
    return names, np.ascontiguousarray(arr)


CONST_NAMES, CONST_ARR = host_consts()
NCONST = len(CONST_NAMES)


def build(stage="all", dbg=False):
    nc = bass.Bass("TRN2", target_bir_lowering=False)
    stack = contextlib.ExitStack()
    with stack:
        P = Prog(nc, stack)

        def dram(name, shape, dt=F32, kind="ExternalInput"):
            return nc.dram_tensor(name, list(shape), dt, kind=kind).ap()

        def sb(name, shape, dt=F32):
            return stack.enter_context(nc.sbuf_tensor(name, list(shape), dt))

        def ps(name, shape, dt=F32):
            return stack.enter_context(nc.psum_tensor(name, list(shape), dt))

        def MM(out, lhsT, rhs, start, stop, R, W):
            n = _fsz(rhs)
            c = 70.0 + n * 0.75
            if rhs.dtype == F32:
                c *= 4.0
            P.op("pe", lambda e: e.matmul(out, lhsT, rhs, start=start, stop=stop), R, W, cost=c)

        def TR(out, in_, ident, R, W):
            P.op("pe", lambda e: e.transpose(out=out, in_=in_, identity=ident), R, W, cost=110.0)

        def ACTF(out, in_, func, R, W, **kw):
            c = 120.0 + _fsz(in_) * 0.6 + (90.0 if "accum_out" in kw else 0.0)
            P.op("act", lambda e: e.activation(out=out, in_=in_, func=func, **kw), R, W, cost=c)

        def _vc(eng, n, k=1.5):
            return (100.0 + n * k * 0.6) if eng == "dve" else (150.0 + n * 1.9)

        def TT(eng, out, in0, in1, op, R, W):
            P.op(eng, lambda e: e.tensor_tensor(out=out, in0=in0, in1=in1, op=op), R, W, cost=_vc(eng, _fsz(out)))

        def TS(eng, out, in0, s1, s2, op0, op1, R, W):
            P.op(eng, lambda e: e.tensor_scalar(out=out, in0=in0, scalar1=s1, scalar2=s2, op0=op0, op1=op1), R, W,
                 cost=_vc(eng, _fsz(out), 1.05))

        def STT(out, in0, scalar, in1, op0, op1, R, W):
            P.op("dve", lambda e: e.scalar_tensor_tensor(out=out, in0=in0, scalar=scalar, in1=in1, op0=op0, op1=op1), R, W,
                 cost=_vc("dve", _fsz(out)))

        def CP(eng, out, in_, R, W):
            if eng == "act":
                P.op("act", lambda e: e.activation(out=out, in_=in_, func=AF.Copy), R, W, cost=120.0 + _fsz(in_) * 0.6)
            else:
                P.op(eng, lambda e: e.tensor_copy(out=out, in_=in_), R, W, cost=_vc(eng, _fsz(out), 1.05))

        def MEMSET(eng, ap, val, W):
            P.op(eng, lambda e: e.memset(ap, val), [], W, cost=_vc(eng, _fsz(ap), 0.6))

        def DMA(eng, out, in_, semkey, R, W):
            P.dma(eng, lambda e: e.dma_start(out=out, in_=in_), semkey, R, W, nbytes=_fsz(out) * int(out.shape[0]) * 4)

        def RECIP(out, in_, R, W):
            P.op("dve", lambda e: e.reciprocal(out=out, in_=in_), R, W, cost=_vc("dve", _fsz(out), 1.05))

        def MARK(name):
            if stage == name:
                P.enabled = False

        def rstd_inplace(ap, n, key):
            TS("dve", ap, ap, 1.0 / n, EPS, ALU.mult, ALU.add, [key], [key])
            ACTF(ap, ap, AF.Ln, [key], [key])
            ACTF(ap, ap, AF.Exp, [key], [key], scale=-0.5)

        x_d = dram("x", [T, D])
        n1_d = dram("norm1_w", [1, D])
        n2_d = dram("norm2_w", [1, D])
        nf_d = dram("norm_f_w", [1, D])
        consts_d = dram("consts", [128, NCONST, 128])
        w_in_d = dram("w_in", [D, D_IN])
        w2b_d = [dram("gla_w2b_f", [17, 512]), dram("gla_w2b_b", [17, 512])]
        gnw_d = dram("gla_norm_w", [1, 256])
        out_d = dram("out", [T, D], kind="ExternalOutput")
        dbg_d = dram("dbg", [T, D], kind="ExternalOutput") if dbg else None

        consts = sb("consts_sb", [128, NCONST, 128])
        CI = {n: i for i, n in enumerate(CONST_NAMES)}

        def cst(name):
            return consts[:, CI[name], :]

        ident_b = sb("ident_b", [128, 128], BF16)
        ones_b = sb("ones_b", [128, 128], BF16)
        hT = sb("hT", [128, KT, T], BF16)
        mixed = sb("mixed", [128, NT, D], BF16)
        small = sb("small", [128, 64])
        ARENA_BYTES = 134 * 1024
        arena = sb("arena", [128, ARENA_BYTES // 4])

        def carve(off, shape, dt, base=None, cap=None):
            base = arena if base is None else base
            cap = ARENA_BYTES if cap is None else cap
            nb = int(np.prod(shape)) * (2 if dt == BF16 else 4)
            nb = (nb + 3) // 4 * 4
            assert off % 4 == 0 and off + nb <= cap, (off, nb)
            v = base[:, off // 4:(off + nb) // 4]
            if dt != F32:
                v = v.bitcast(dt)
            if len(shape) == 2:
                pat = "p (a b) -> p a b"
                v = v.rearrange(pat, a=shape[0])
            elif len(shape) == 3:
                v = v.rearrange("p (a b c) -> p a b c", a=shape[0], b=shape[1])
            return v, off + nb

        pt = [ps(f"pt{i}", [128, 1024]) for i in range(3)]
        ptb = ps("ptb", [128, 2048], BF16)

        def bank(i):
            return pt[i // 2][:, (i % 2) * 512:(i % 2 + 1) * 512]

        def bkey(i):
            return ("pb", i)

        def bbank(i):
            return ptb[:, i * 1024:(i + 1) * 1024]

        DMA("sp", consts[:], consts_d[:, :, :], "c_consts", [], ["consts"])
        CP("dve", ident_b[:], cst("ident"), ["consts"], ["ident_b"])
        MEMSET("pool", ones_b[:], 1.0, ["ones_b"])

        off = 0
        xt0, off = carve(off, [D], F32)
        xt1, off = carve(off, [D], F32)
        hn0, off = carve(off, [D], BF16)
        hn1, off = carve(off, [D], BF16)
        sq, off = carve(off, [D], F32)
        n1_bc, off = carve(off, [D], F32)
        DMA("sp", n1_bc, n1_d.partition_broadcast(128), "c_n1", [], ["n1_bc"])
        xts = [xt0, xt1]
        hns = [hn0, hn1]
        for tt in range(NT):
            b = tt % 2
            xb, hb = xts[b], hns[b]
            DMA("sp", xb, x_d[tt * 128:(tt + 1) * 128, :], ("xt", b), [], [("xt", b)])
            ACTF(sq, xb, AF.Square, [("xt", b)], ["sq", "ss0"], accum_out=small[:, 0:1])
            rstd_inplace(small[:, 0:1], D, "ss0")
            STT(hb, xb, small[:, 0:1], n1_bc, ALU.mult, ALU.mult, [("xt", b), "ss0", "n1_bc"], [("hn", b)])
            for kt in range(KT):
                TR(bbank(b)[:, kt * 128:(kt + 1) * 128], hb[:, kt * 128:(kt + 1) * 128], ident_b[:],
                   [("hn", b), "ident_b"], [("pbb", b)])
            CP("act", hT[:, :, tt * 128:(tt + 1) * 128], bbank(b).rearrange("p (k t) -> p k t", k=KT),
               [("pbb", b)], [("hT", tt)])
        HT_ALL = [("hT", tt) for tt in range(NT)]
        MARK("p1")

        P.barrier()
        off = 0
        qT, off = carve(off, [T], F32)
        kT, off = carve(off, [T], F32)
        k_tok, off = carve(off, [NT, 128], F32)
        v_tok, off = carve(off, [NT, 256], BF16)
        qdT = [None, None]
        kiT = [None, None]
        ktail = [None, None]
        for d_ in range(2):
            qdT[d_], off = carve(off, [T], BF16)
            kiT[d_], off = carve(off, [T], BF16)
            ktail[d_], off = carve(off, [NT, 128], BF16)
        sb_store, off = carve(off, [NT, 256], BF16)
        dec, off = carve(off, [2, NT], F32)
        S, off = carve(off, [256], F32)
        S_bf, off = carve(off, [256], BF16)
        NTMP = 4
        tmp = []
        for i in range(NTMP):
            d = {}
            for nm in ("e", "lg", "E", "Ei", "Et"):
                d[nm], off = carve(off, [128], F32)
            d["Pf"], off = carve(off, [128], BF16)
            d["Pb"], off = carve(off, [128], BF16)
            d["sig"], off = carve(off, [512], F32)
            d["G"], off = carve(off, [256], F32)
            tmp.append(d)
        gl, off = carve(off, [2, T], BF16)
        w2b, off = carve(off, [2, 512], BF16)
        wqk, off = carve(off, [KT, 256], BF16)
        wkv, off = carve(off, [KT, 384], BF16)
        wgm, off = carve(off, [KT, 512], BF16)
        wgl, off = carve(off, [KT, 32], BF16)
        gnw_bc, off = carve(off, [256], F32)
        GLA_END = off

        DMA("sp", gnw_bc, gnw_d.partition_broadcast(128), "c_gnw", [], ["gnw_bc"])
        MEMSET("pool", gl[:, :, :], 1.0, ["gl"])
        MEMSET("pool", w2b[:, :, :], 0.0, ["w2b"])
        for d_ in range(2):
            DMA("pool", w2b[0:17, d_, :], w2b_d[d_][:, :], "c_w2b", [], ["w2b"])
        DMA("pool", wgl, w_in_d[:, C_GLF:C_GLF + 32].rearrange("(k p) c -> p k c", p=128), "w_wgl", [], ["wgl"])
        for d_ in range(2):
            for tg in range(4):
                bi = tg % 2
                for kt in range(KT):
                    MM(bank(bi)[0:16, :], wgl[:, kt, d_ * 16:(d_ + 1) * 16], hT[:, kt, tg * 512:(tg + 1) * 512],
                       kt == 0, kt == KT - 1, ["wgl"] + HT_ALL[tg * 4:tg * 4 + 4], [bkey(bi)])
                CP("act", gl[0:16, d_, tg * 512:(tg + 1) * 512], bank(bi)[0:16, :], [bkey(bi)], ["gl"])

        MARK("g0")
        QSCALE = 128.0 ** -0.5
        for h in range(4):
            def wcols(dst, c0, n):
                return (dst, w_in_d[:, c0:c0 + n].rearrange("(k p) c -> p k c", p=128))
            for (dst, src) in (wcols(wqk[:, :, 0:128], C_GQ + h * 128, 128), wcols(wqk[:, :, 128:256], C_GK + h * 128, 128)):
                DMA("pool", dst, src, "w_wqk", [], ["wqk"])
            for (dst, src) in (wcols(wkv[:, :, 0:128], C_GK + h * 128, 128), wcols(wkv[:, :, 128:384], C_GV + h * 256, 256)):
                DMA("pool", dst, src, "w_wkv", [], ["wkv"])
            for (dst, src) in (wcols(wgm[:, :, 0:256], C_GR + h * 256, 256), wcols(wgm[:, :, 256:512], C_MA + h * 256, 256)):
                DMA("pool", dst, src, "w_wgm", [], ["wgm"])
            MARK("g1a")
            for which, dstT in ((0, qT), (1, kT)):
                for tg in range(4):
                    bi = (which * 4 + tg) % 4
                    for kt in range(KT):
                        MM(bank(bi), wqk[:, kt, which * 128:(which + 1) * 128], hT[:, kt, tg * 512:(tg + 1) * 512],
                           kt == 0, kt == KT - 1, ["wqk"] + HT_ALL[tg * 4:tg * 4 + 4], [bkey(bi)])
                    CP("act" if tg % 2 else "dve", dstT[:, tg * 512:(tg + 1) * 512], bank(bi), [bkey(bi)],
                       [("qkT", which, tg)])
            MARK("g1b")
            for n in range(NT):
                bi = 4 + n % 2
                for kt in range(KT):
                    MM(bank(bi)[:, 0:384], hT[:, kt, n * 128:(n + 1) * 128], wkv[:, kt, :],
                       kt == 0, kt == KT - 1, ["wkv", ("hT", n)], [bkey(bi)])
                CP("dve", k_tok[:, n, :], bank(bi)[:, 0:128], [bkey(bi)], [("k_tok", n)])
                CP("act", v_tok[:, n, :], bank(bi)[:, 128:384], [bkey(bi)], [("v_tok", n)])
            MARK("g1")
            for n in range(NT):
                tsl = slice(n * 128, (n + 1) * 128)
                tg = n // 4
                for d_ in range(2):
                    tm = tmp[(n * 2 + d_) % NTMP]
                    tk = ("gtmp", (n * 2 + d_) % NTMP)
                    a_c = cst("a_le") if d_ == 0 else cst("a_ge")
                    a_s = cst("a_gt") if d_ == 0 else cst("a_lt")
                    b0 = (n * 2 + d_) % 2 * 2
                    zb, cb = bank(b0), bank(b0 + 1)
                    MM(zb[:, 0:128], gl[:, d_, tsl], w2b[:, d_, h * 128:(h + 1) * 128], True, True,
                       ["gl", "w2b"], [bkey(b0)])
                    ACTF(tm["e"], zb[:, 0:128], AF.Exp, [bkey(b0)], [tk], scale=-1.0)
                    ACTF(tm["lg"], tm["e"], AF.Ln, [tk], [tk], bias=1.0)
                    MM(cb[:, 0:128], tm["lg"], a_c, True, True, [tk, "consts"], [bkey(b0 + 1)])
                    MM(cb[:, 128:256], a_s, tm["lg"], True, True, [tk, "consts"], [bkey(b0 + 1)])
                    ACTF(tm["E"], cb[:, 0:128], AF.Exp, [bkey(b0 + 1)], [tk])
                    ACTF(tm["Ei"], cb[:, 0:128], AF.Exp, [bkey(b0 + 1)], [tk], scale=-1.0)
                    ACTF(tm["Et"], cb[:, 128:256], AF.Exp, [bkey(b0 + 1)], [tk])
                    STT(qdT[d_][:, tsl], qT[:, tsl], QSCALE, tm["E"], ALU.mult, ALU.mult,
                        [("qkT", 0, tg), tk], [("qdT", d_, n)])
                    TT("dve", kiT[d_][:, tsl], kT[:, tsl], tm["Ei"], ALU.mult, [("qkT", 1, tg), tk], [("kiT", d_, n)])
                    TT("dve", ktail[d_][:, n, :], k_tok[:, n, :], tm["Et"], ALU.mult, [("k_tok", n), tk], [("ktail", d_, n)])
                    col = 127 if d_ == 0 else 0
                    CP("dve", dec[:, d_, n:n + 1], tm["E"][:, col:col + 1], [tk], [("dec", d_, n)])
            MARK("g2")
            MEMSET("dve", S, 0.0, ["S"])
            for n in range(NT - 1, -1, -1):
                CP("act", sb_store[:, n, :], S, ["S"], [("sb_store", n)])
                bi = 4 + n % 2
                MM(bank(bi)[:, 0:256], ktail[1][:, n, :], v_tok[:, n, :], True, True,
                   [("ktail", 1, n), ("v_tok", n)], [bkey(bi)])
                STT(S, S, dec[:, 1, n:n + 1], bank(bi)[:, 0:256], ALU.mult, ALU.add,
                    ["S", ("dec", 1, n), bkey(bi)], ["S"])
            MARK("g3")
            MEMSET("dve", S, 0.0, ["S"])
            for n in range(NT):
                tsl = slice(n * 128, (n + 1) * 128)
                tm = tmp[n % NTMP]
                tk = ("ftmp", n % NTMP)
                CP("act", S_bf, S, ["S"], ["S_bf"])
                b0 = (n % 2) * 2
                sc = bank(b0)
                MM(sc[:, 0:128], kiT[0][:, tsl], qdT[0][:, tsl], True, True, [("kiT", 0, n), ("qdT", 0, n)], [bkey(b0)])
                MM(sc[:, 128:256], kiT[1][:, tsl], qdT[1][:, tsl], True, True, [("kiT", 1, n), ("qdT", 1, n)], [bkey(b0)])
                TT("dve", tm["Pf"], sc[:, 0:128], cst("m_le"), ALU.mult, [bkey(b0), "consts"], [tk])
                TT("dve", tm["Pb"], sc[:, 128:256], cst("m_ge"), ALU.mult, [bkey(b0), "consts"], [tk])
                ob = bank(b0 + 1)
                ok = bkey(b0 + 1)
                MM(ob[:, 0:256], qdT[0][:, tsl], S_bf, True, False, [("qdT", 0, n), "S_bf"], [ok])
                MM(ob[:, 0:256], qdT[1][:, tsl], sb_store[:, n, :], False, False, [("qdT", 1, n), ("sb_store", n)], [ok])
                MM(ob[:, 0:256], tm["Pf"], v_tok[:, n, :], False, False, [tk, ("v_tok", n)], [ok])
                MM(ob[:, 0:256], tm["Pb"], v_tok[:, n, :], False, True, [tk, ("v_tok", n)], [ok])
                kb = 4 + n % 2
                MM(bank(kb)[:, 0:256], ktail[0][:, n, :], v_tok[:, n, :], True, True,
                   [("ktail", 0, n), ("v_tok", n)], [bkey(kb)])
                STT(S, S, dec[:, 0, n:n + 1], bank(kb)[:, 0:256], ALU.mult, ALU.add,
                    ["S", ("dec", 0, n), bkey(kb)], ["S"])
                gb = 4 + n % 2
                for kt in range(KT):
                    MM(bank(gb), hT[:, kt, tsl], wgm[:, kt, :], kt == 0, kt == KT - 1, ["wgm", ("hT", n)], [bkey(gb)])
                ACTF(tm["sig"], bank(gb), AF.Exp, [bkey(gb)], [("sig", n % NTMP)], scale=-1.0)
                ACTF(tm["sig"], tm["sig"], AF.Ln, [("sig", n % NTMP)], [("sig", n % NTMP)], bias=1.0)
                ACTF(tm["sig"], tm["sig"], AF.Exp, [("sig", n % NTMP)], [("sig", n % NTMP)], scale=-1.0)
                TT("pool", tm["G"], tm["sig"][:, 0:256], tm["sig"][:, 256:512], ALU.mult, [("sig", n % NTMP)], [("G", n % NTMP)])
                TT("dve", tm["G"], tm["G"], bank(gb)[:, 0:256], ALU.mult, [("G", n % NTMP), bkey(gb)], [("G", n % NTMP)])
                TT("pool", tm["G"], tm["G"], gnw_bc, ALU.mult, [("G", n % NTMP), "gnw_bc"], [("G", n % NTMP)])
                ssk = ("ssq", n % 2)
                ssap = small[:, 2 + n % 2:3 + n % 2]
                ACTF(tm["sig"][:, 0:256], ob[:, 0:256], AF.Square, [ok, ("G", n % NTMP)], [("sig", n % NTMP), ssk],
                     accum_out=ssap)
                rstd_inplace(ssap, 256, ssk)
                STT(mixed[:, n, h * 256:(h + 1) * 256], ob[:, 0:256], ssap, tm["G"], ALU.mult, ALU.mult,
                    [ok, ssk, ("G", n % NTMP)], [("mixed", n)])

        P.barrier()
        HG = 4
        off = 0
        gqT, off = carve(off, [HG, T], BF16)
        gkT, off = carve(off, [HG, T], BF16)
        gvT, off = carve(off, [HG, T], BF16)
        dabs, off = carve(off, [NT, 32], F32)
        g_raw, off = carve(off, [NT, 2, 8], F32)
        beta, off = carve(off, [NT, 2, 8], F32)
        gvec, off = carve(off, [64], F32)
        wsl0, off = carve(off, [KT, 512], BF16)
        wsl1, off = carve(off, [KT, 512], BF16)
        wsl = [wsl0, wsl1]
        cwT, off = carve(off, [24, 5], F32)
        gdnw_bc, off = carve(off, [128], F32)
        wdab, off = carve(off, [KT, 32], BF16)
        TMP0 = off
        xc = [None, None]
        xc[0], off = carve(off, [T + 4], BF16)
        xc[1], off = carve(off, [T + 4], BF16)
        diag, off = carve(off, [5, 128], BF16)
        ce = [None, None]
        cy = [None, None]
        for i in range(2):
            ce[i], off = carve(off, [512], F32)
            cy[i], off = carve(off, [512], F32)
        cysq, off = carve(off, [512], BF16)
        crs, off = carve(off, [512], F32)
        CONV_END = off
        off = TMP0
        DB = []
        for d_ in range(2):
            dd = {}
            for nm in ("GMB", "Wd", "decT", "u_sb", "Sg"):
                dd[nm], off = carve(off, [HG, 128], F32)
            for nm in ("Lm", "LTm", "XT", "Pp0", "Pp1", "PTp0", "PTp1", "kbg", "vbeta", "qd_tok",
                       "attnT", "ktl0", "ktl1", "qdTg", "wT_sb", "vnew", "Sg_bf", "ostage"):
                dd[nm], off = carve(off, [HG, 128], BF16)
            dd["bg"], off = carve(off, [HG], F32)
            dd["et2"], off = carve(off, [2, HG], F32)
            DB.append(dd)
        oland = []
        for i in range(2):
            a, off = carve(off, [HG, 128], BF16)
            oland.append(a)
        osum, off = carve(off, [HG, 128], F32)
        fsig, off = carve(off, [512], F32)
        fG, off = carve(off, [HG, 128], F32)
        frs, off = carve(off, [8], F32)
        SWEEP_END = off
        gdn_o = nc.dram_tensor("gdn_o_spill", [NT, 128, HG * 128], BF16, kind="Internal").ap()

        gdnw_d = dram("gdn_norm_w", [1, 128])
        gvec_d = dram("gdn_vec", [1, 32])
        cw_d = dram("gdn_conv_wT", [128, 24, 5])
        DMA("sp", gdnw_bc, gdnw_d.partition_broadcast(128), "c_gdnw", [], ["gdnw_bc"])
        DMA("sp", gvec[:, 0:32], gvec_d.partition_broadcast(128), "c_gvec", [], ["gvec"])
        DMA("sp", cwT, cw_d[:, :, :], "c_cw", [], ["cwT"])
        DMA("pool", wdab, w_in_d[:, C_DAB:C_DAB + 32].rearrange("(k p) c -> p k c", p=128), "w_wdab", [], ["wdab"])
        ACTF(gvec[:, 16:32], gvec[:, 16:32], AF.Exp, ["gvec"], ["gvec"])
        TS("dve", gvec[:, 16:32], gvec[:, 16:32], -1.0, None, ALU.mult, ALU.bypass, ["gvec"], ["gvec"])
        for n in range(NT):
            bi = n % 2
            for kt in range(KT):
                MM(bank(bi)[:, 0:32], hT[:, kt, n * 128:(n + 1) * 128], wdab[:, kt, :], kt == 0, kt == KT - 1,
                   ["wdab", ("hT", n)], [bkey(bi)])
            CP("act", dabs[:, n, :], bank(bi)[:, 0:32], [bkey(bi)], ["dabs"])
        a_view = dabs[:, :, 0:16]
        b_view = dabs[:, :, 16:32]
        g_flat = g_raw.rearrange("p n d h -> p n (d h)")
        be_flat = beta.rearrange("p n d h -> p n (d h)")
        TT("dve", g_flat, a_view, gvec[:, 0:16].unsqueeze(1).to_broadcast([128, NT, 16]), ALU.add, ["dabs", "gvec"], ["g_raw"])
        ACTF(g_flat, g_flat, AF.Exp, ["g_raw"], ["g_raw"])
        ACTF(g_flat, g_flat, AF.Ln, ["g_raw"], ["g_raw"], bias=1.0)
        TT("dve", g_flat, g_flat, gvec[:, 16:32].unsqueeze(1).to_broadcast([128, NT, 16]), ALU.mult, ["g_raw", "gvec"], ["g_raw"])
        ACTF(be_flat, b_view, AF.Exp, ["dabs"], ["beta"], scale=-1.0)
        TS("dve", be_flat, be_flat, 1.0, None, ALU.add, ALU.bypass, ["beta"], ["beta"])
        RECIP(be_flat, be_flat, ["beta"], ["beta"])
        MARK("d0")

        GSCALE = 128.0 ** -0.5
        ident_bc4 = ident_b[:].unsqueeze(1).to_broadcast([128, HG, 128])

        def bc_h(ap2):
            return ap2.unsqueeze(2).to_broadcast([128, HG, 128])

        def bc_m(ap2):
            return ap2.unsqueeze(1).to_broadcast([128, HG, 128])

        def v4(ap2):
            return ap2.rearrange("p (h d) -> p h d", h=HG)

        for grp in range(2):
            hs0 = grp * HG
            for which, c_base, dstT in ((0, C_DQ, gqT), (1, C_DK, gkT), (2, C_DV, gvT)):
                ws = wsl[which % 2]
                wk = ("wsl", which % 2)
                DMA("pool", ws, w_in_d[:, c_base + hs0 * 128:c_base + (hs0 + HG) * 128].rearrange("(k p) c -> p k c", p=128),
                    ("w_wsl", which % 2), [], [wk])
                for hh in range(HG):
                    ci = which * 8 + hs0 + hh
                    xi = (which * HG + hh) % 2
                    xcb = xc[xi]
                    xk = ("xc", xi)
                    MEMSET("pool", xcb[:, 0:2], 0.0, [xk])
                    MEMSET("pool", xcb[:, T + 2:T + 4], 0.0, [xk])
                    for k in range(5):
                        TS("dve", diag[:, k, :], cst("ident"), cwT[:, ci, k:k + 1], None, ALU.mult, ALU.bypass,
                           ["consts", "cwT"], ["diag"])
                    for tg in range(4):
                        bi = tg % 2
                        for kt in range(KT):
                            MM(bank(bi), ws[:, kt, hh * 128:(hh + 1) * 128], hT[:, kt, tg * 512:(tg + 1) * 512],
                               kt == 0, kt == KT - 1, [wk] + HT_ALL[tg * 4:tg * 4 + 4], [bkey(bi)])
                        CP("act" if tg % 2 else "dve", xcb[:, 2 + tg * 512:2 + (tg + 1) * 512], bank(bi), [bkey(bi)], [xk])
                    for tg in range(4):
                        bi = 2 + tg % 2
                        i2 = tg % 2
                        for k in range(5):
                            MM(bank(bi), diag[:, k, :], xcb[:, tg * 512 + k:tg * 512 + k + 512], k == 0, k == 4,
                               ["diag", xk], [bkey(bi)])
                        ck = ("ctmp", i2)
                        ACTF(ce[i2], bank(bi), AF.Exp, [bkey(bi)], [ck], scale=-1.0)
                        ACTF(ce[i2], ce[i2], AF.Ln, [ck], [ck], bias=1.0)
                        ACTF(ce[i2], ce[i2], AF.Exp, [ck], [ck], scale=-1.0)
                        dst = dstT[:, hh, tg * 512:(tg + 1) * 512]
                        dk = ("gT", which, hh, tg)
                        if which == 2:
                            TT("dve", dst, ce[i2], bank(bi), ALU.mult, [ck, bkey(bi)], [dk])
                        else:
                            TT("dve", cy[i2], ce[i2], bank(bi), ALU.mult, [ck, bkey(bi)], [("cy", i2)])
                            TT("pool", cysq, cy[i2], cy[i2], ALU.mult, [("cy", i2)], ["cysq"])
                            MM(bank(4), ones_b[:], cysq, True, True, ["ones_b", "cysq"], [bkey(4)])
                            ACTF(crs, bank(4), AF.Ln, [bkey(4)], ["crs"], bias=EPS)
                            ACTF(crs, crs, AF.Exp, ["crs"], ["crs"], scale=-0.5)
                            if which == 0:
                                STT(dst, cy[i2], GSCALE, crs, ALU.mult, ALU.mult, [("cy", i2), "crs"], [dk])
                            else:
                                TT("dve", dst, cy[i2], crs, ALU.mult, [("cy", i2), "crs"], [dk])
            MARK("d1")
            P.barrier()
            DMA("pool", wsl[0], w_in_d[:, C_DZ + hs0 * 128:C_DZ + (hs0 + HG) * 128].rearrange("(k p) c -> p k c", p=128),
                ("w_wsl", 0), [], [("wsl", 0)])
            DMA("pool", wsl[1], w_in_d[:, C_MB + hs0 * 128:C_MB + (hs0 + HG) * 128].rearrange("(k p) c -> p k c", p=128),
                ("w_wsl", 1), [], [("wsl", 1)])

            def gT_keys(which, n):
                return [("gT", which, hh, n // 4) for hh in range(HG)]

            stored = set()
            esc_all = [dabs[:, 0:8, :].rearrange("p a b -> p (a b)").rearrange("p (k n h) -> p k n h", k=4, n=NT),
                       dabs[:, 8:16, :].rearrange("p a b -> p (a b)").rearrange("p (k n h) -> p k n h", k=4, n=NT)]
            for d_ in range(2):
                Mc_ = cst("b_le") if d_ == 0 else cst("b_ge")
                Ms_ = cst("b_gt") if d_ == 0 else cst("b_lt")
                for ki, mk in enumerate((Mc_, Ms_, cst("csel0"), cst("csel1"))):
                    MM(bank(d_)[:, ki * 64:(ki + 1) * 64].rearrange("p (n h) -> p n h", n=NT), mk,
                       g_raw[:, :, d_, hs0:hs0 + HG], True, True, ["consts", "g_raw"], [bkey(d_)])
                ACTF(esc_all[d_].rearrange("p k n h -> p (k n h)"), bank(d_)[:, 0:256], AF.Exp, [bkey(d_)], [("esc_all", d_), "dabs"])

            gb0 = ptb[:, 0:512]
            gb1 = ptb[:, 512:1024]
            xbank = ptb[:, 1024:2048].bitcast(F32)
            XK = ("pb", 7)

            def gdn_tile(d_, n):
                B = DB[d_]
                dk = lambda nm: (nm, d_)
                Mc = cst("b_le") if d_ == 0 else cst("b_ge")
                Ms = cst("b_gt") if d_ == 0 else cst("b_lt")
                bg = B["bg"]
                GMB, Wd, decT, Lm, LTm, XT = B["GMB"], B["Wd"], B["decT"], B["Lm"], B["LTm"], B["XT"]
                kbg, vbeta, qd_tok = B["kbg"], B["vbeta"], B["qd_tok"]
                Ppd = [B["Pp0"], B["Pp1"]]
                PTpd = [B["PTp0"], B["PTp1"]]
                pa, pb_ = (0, 1) if d_ == 0 else (2, 3)
                tsl = slice(n * 128, (n + 1) * 128)
                gv = g_raw[:, n, d_, hs0:hs0 + HG]
                bv = beta[:, n, d_, hs0:hs0 + HG]
                EA = esc_all[d_]
                e_cum, e_tail = EA[:, 0, n, :], EA[:, 1, n, :]
                second = n in stored
                if second:
                    par = n % 2
                    DMA("sp", oland[par].rearrange("p h d -> p (h d)"), gdn_o[n], ("oland", par), [("o_dram", n)], [("oland", par)])
                TT("dve", bg, bv, e_cum, ALU.mult, ["beta", ("esc_all", d_)], [dk("bg")])
                for hh in range(HG):
                    TR(gb0[:, hh * 128:(hh + 1) * 128], gkT[:, hh, tsl], ident_b[:], gT_keys(1, n) + ["ident_b"], [("pbb", 0)])
                for hh in range(HG):
                    TR(gb1[:, hh * 128:(hh + 1) * 128], gvT[:, hh, tsl], ident_b[:], gT_keys(2, n) + ["ident_b"], [("pbb", 0)])
                TT("dve", kbg, v4(gb0), bc_h(bg), ALU.mult, [("pbb", 0), dk("bg")], [dk("kbg")])
                for c_ in range(2):
                    TT("dve", B["et2"][:, c_, :], e_tail, cst("csel%d" % c_)[:, 0:HG], ALU.mult, [("esc_all", d_), "consts"], [dk("et2")])
                    TT("dve", B["ktl%d" % c_], v4(gb0), bc_h(B["et2"][:, c_, :]), ALU.mult, [("pbb", 0), dk("et2")], [dk("ktl%d" % c_)])
                TT("dve", vbeta, v4(gb1), bc_h(bv), ALU.mult, [("pbb", 0), "beta"], [dk("vbeta")])
                for hh in range(HG):
                    TR(gb0[:, hh * 128:(hh + 1) * 128], gqT[:, hh, tsl], ident_b[:], gT_keys(0, n) + ["ident_b"], [("pbb", 0)])
                TT("dve", qd_tok, v4(gb0), bc_h(e_cum), ALU.mult, [("pbb", 0), ("esc_all", d_)], [dk("qd_tok")])
                for hh in range(HG):
                    TR(gb1[:, hh * 128:(hh + 1) * 128], qd_tok[:, hh, :], ident_b[:], [dk("qd_tok"), "ident_b"], [("pbb", 0)])
                CP("act", B["qdTg"], v4(gb1), [("pbb", 0)], [dk("qdTg")])
                TT("pool", GMB, bc_h(gv), bc_m(Ms), ALU.mult, ["g_raw", "consts"], [dk("GMB")])
                MM(bank(pb_), Mc, GMB.rearrange("p h s -> p (h s)"), True, True, ["consts", dk("GMB")], [bkey(pb_)])
                ACTF(Wd.rearrange("p h s -> p (h s)"), bank(pb_), AF.Exp, [bkey(pb_)], [dk("Wd")])
                TT("pool", GMB, bc_h(bv), bc_m(Ms), ALU.mult, ["beta", "consts"], [dk("GMB")])
                TT("pool", Wd, Wd, GMB, ALU.mult, [dk("Wd"), dk("GMB")], [dk("Wd")])
                TT("pool", GMB, bc_h(gv), bc_m(Mc), ALU.mult, ["g_raw", "consts"], [dk("GMB")])
                MM(bank(pa), Ms, GMB.rearrange("p h s -> p (h s)"), True, True, ["consts", dk("GMB")], [bkey(pa)])
                ACTF(decT.rearrange("p h s -> p (h s)"), bank(pa), AF.Exp, [bkey(pa)], [dk("decT")])
                TT("pool", decT, decT, bc_m(Mc), ALU.mult, [dk("decT"), "consts"], [dk("decT")])
                for hh in range(HG):
                    MM(bank(pb_)[:, hh * 128:(hh + 1) * 128], gkT[:, hh, tsl], gkT[:, hh, tsl], True, True,
                       gT_keys(1, n), [bkey(pb_)])
                TT("dve", Lm, v4(bank(pb_)), Wd, ALU.mult, [bkey(pb_), dk("Wd")], [dk("Lm")])
                for hh in range(HG):
                    MM(bank(pa)[:, hh * 128:(hh + 1) * 128], gkT[:, hh, tsl], gqT[:, hh, tsl], True, True,
                       gT_keys(1, n) + gT_keys(0, n), [bkey(pa)])
                TT("dve", B["attnT"], v4(bank(pa)), decT, ALU.mult, [bkey(pa), dk("decT")], [dk("attnT")])
                for hh in range(HG):
                    TR(gb0[:, hh * 128:(hh + 1) * 128], Lm[:, hh, :], ident_b[:], [dk("Lm"), "ident_b"], [("pbb", 0)])
                CP("act", LTm, v4(gb0), [("pbb", 0)], [dk("LTm")])
                TT("dve", XT, ident_bc4, v4(gb0), ALU.subtract, ["ident_b", ("pbb", 0)], [dk("XT")])
                Pc, PTc = Lm, LTm
                pck, ptk_ = dk("Lm"), dk("LTm")
                for it in range(5):
                    Pn, PTn = Ppd[it % 2], PTpd[it % 2]
                    pnk, ptnk = ("Pp", it % 2, d_), ("PTp", it % 2, d_)
                    for hh in range(HG):
                        MM(bank(pb_)[:, hh * 128:(hh + 1) * 128], PTc[:, hh, :], Pc[:, hh, :], True, True, [pck, ptk_], [bkey(pb_)])
                    CP("act", Pn, v4(bank(pb_)), [bkey(pb_)], [pnk])
                    if it < 4:
                        for hh in range(HG):
                            MM(bank(pa)[:, hh * 128:(hh + 1) * 128], Pc[:, hh, :], PTc[:, hh, :], True, True, [pck, ptk_], [bkey(pa)])
                        CP("dve", PTn, v4(bank(pa)), [bkey(pa)], [ptnk])
                    for hh in range(HG):
                        MM(xbank[:, hh * 128:(hh + 1) * 128], Pn[:, hh, :], XT[:, hh, :], True, True, [pnk, dk("XT")], [XK])
                    TT("dve", XT, XT, v4(xbank), ALU.add, [dk("XT"), XK], [dk("XT")])
                    Pc, PTc, pck, ptk_ = Pn, PTn, pnk, ptnk
                for hh in range(HG):
                    MM(bank(pa)[:, hh * 128:(hh + 1) * 128], XT[:, hh, :], vbeta[:, hh, :], True, True, [dk("XT"), dk("vbeta")], [bkey(pa)])
                CP("act", B["u_sb"], v4(bank(pa)), [bkey(pa)], [dk("u_sb")])
                for hh in range(HG):
                    MM(bank(pb_)[:, hh * 128:(hh + 1) * 128], kbg[:, hh, :], XT[:, hh, :], True, True, [dk("kbg"), dk("XT")], [bkey(pb_)])
                CP("dve", B["wT_sb"], v4(bank(pb_)), [bkey(pb_)], [dk("wT_sb")])
                sb0 = 4
                Sg, Sg_bf, vnew = B["Sg"], B["Sg_bf"], B["vnew"]
                chunks = (0, 1) if d_ == 0 else (1, 0)
                for c in chunks:
                    sl = slice(c * 64, c * 64 + 64)
                    for hh in range(HG):
                        MM(bank(sb0)[:, hh * 128:(hh + 1) * 128], B["wT_sb"][:, hh, :], Sg_bf[:, hh, :], True, True,
                           [dk("wT_sb"), dk("Sg_bf")], [bkey(sb0)])
                    TT("dve", vnew[sl], B["u_sb"][sl], v4(bank(sb0))[sl], ALU.subtract, [dk("u_sb"), bkey(sb0)], [dk("vnew")])
                    for hh in range(HG):
                        MM(bank(sb0 + 1)[:, hh * 128:(hh + 1) * 128], B["qdTg"][:, hh, :], Sg_bf[:, hh, :], True, False,
                           [dk("qdTg"), dk("Sg_bf")], [bkey(sb0 + 1)])
                        MM(bank(sb0 + 1)[:, hh * 128:(hh + 1) * 128], B["attnT"][:, hh, :], vnew[:, hh, :], False, True,
                           [dk("attnT"), dk("vnew")], [bkey(sb0 + 1)])
                    for hh in range(HG):
                        MM(bank(sb0)[:, hh * 128:(hh + 1) * 128], B["ktl%d" % c][:, hh, :], vnew[:, hh, :], True, True,
                           [dk("ktl%d" % c), dk("vnew")], [bkey(sb0)])
                    TT("pool", Sg, Sg, bc_h(EA[:, 2 + c, n, :]), ALU.mult, [dk("Sg"), ("esc_all", d_)], [dk("Sg")])
                    TT("dve", Sg, Sg, v4(bank(sb0)), ALU.add, [dk("Sg"), bkey(sb0)], [dk("Sg")])
                    CP("act", Sg_bf, Sg, [dk("Sg")], [dk("Sg_bf")])
                    if not second:
                        CP("act", B["ostage"][sl], v4(bank(sb0 + 1))[sl], [bkey(sb0 + 1)], [dk("ostage")])
                    else:
                        TT("dve", osum[sl], v4(bank(sb0 + 1))[sl], oland[n % 2][sl], ALU.add,
                           [bkey(sb0 + 1), ("oland", n % 2)], ["osum"])
                if not second:
                    stored.add(n)
                    DMA("sp", gdn_o[n], B["ostage"].rearrange("p h d -> p (h d)"), ("ost", d_), [dk("ostage")], [("o_dram", n)])
                    return
                osq = fsig.rearrange("p (h d) -> p h d", h=HG)
                TT("pool", osq, osum, osum, ALU.mult, ["osum"], ["fsig"])
                P.op("dve", lambda e: e.tensor_reduce(out=frs[:, 0:HG], in_=osq, axis=AX.X, op=ALU.add), ["fsig"], ["frs"], cost=600.0)
                rstd_inplace(frs[:, 0:HG], 128, "frs")
                for half, ws in enumerate(wsl):
                    for kt in range(KT):
                        MM(bank(half), hT[:, kt, tsl], ws[:, kt, :], kt == 0, kt == KT - 1,
                           [("wsl", half), ("hT", n)], [bkey(half)])
                fGf = fG.rearrange("p h d -> p (h d)")
                for half in range(2):
                    ACTF(fsig, bank(half), AF.Exp, [bkey(half)], ["fsig"], scale=-1.0)
                    ACTF(fsig, fsig, AF.Ln, ["fsig"], ["fsig"], bias=1.0)
                    ACTF(fsig, fsig, AF.Exp, ["fsig"], ["fsig"], scale=-1.0)
                    if half == 0:
                        TT("dve", fGf, fsig, bank(0), ALU.mult, ["fsig", bkey(0)], ["fG"])
                    else:
                        TT("pool", fGf, fGf, fsig, ALU.mult, ["fsig", "fG"], ["fG"])
                TT("pool", fG, fG, bc_m(gdnw_bc), ALU.mult, ["fG", "gdnw_bc"], ["fG"])
                TT("pool", osum, osum, bc_h(frs[:, 0:HG]), ALU.mult, ["osum", "frs"], ["osum"])
                TT("pool", osum, osum, fG, ALU.mult, ["osum", "fG"], ["osum"])
                mslice = mixed[:, n, hs0 * 128:(hs0 + HG) * 128].rearrange("p (h d) -> p h d", h=HG)
                TT("dve", mslice, mslice, osum, ALU.add, ["osum", ("mixed", n)], [("mixed", n)])

            for d_ in range(2):
                MEMSET("pool", DB[d_]["vnew"], 0.0, [("vnew", d_)])
                MEMSET("dve", DB[d_]["Sg"], 0.0, [("Sg", d_)])
                CP("act", DB[d_]["Sg_bf"], DB[d_]["Sg"], [("Sg", d_)], [("Sg_bf", d_)])
            for i in range(NT):
                gdn_tile(0, i)
                gdn_tile(1, NT - 1 - i)
            MARK("d2")
            P.barrier()

        P.barrier()
        wout_d = dram("w_out", [D, D])
        off = 0
        x1, off = carve(off, [NT, D], F32)
        X1_END = off
        mT, off = carve(off, [KT, T], BF16)
        wout, off = carve(off, [KT, D], BF16)
        hn2 = [None, None]
        hn2[0], off = carve(off, [D], BF16)
        hn2[1], off = carve(off, [D], BF16)
        junk, off = carve(off, [D], BF16)
        n2_bc, off = carve(off, [D], F32)
        DMA("sp", n2_bc, n2_d.partition_broadcast(128), "c_n2", [], ["n2_bc"])
        DMA("pool", wout, wout_d.rearrange("(k p) c -> p k c", p=128), "w_wout", [], ["wout"])
        for n in range(NT):
            b = n % 2
            for kt in range(KT):
                TR(bbank(b)[:, kt * 128:(kt + 1) * 128], mixed[:, n, kt * 128:(kt + 1) * 128], ident_b[:],
                   [("mixed", n), "ident_b"], [("pbb", b)])
            CP("act", mT[:, :, n * 128:(n + 1) * 128], bbank(b).rearrange("p (k t) -> p k t", k=KT), [("pbb", b)], [("mT", n)])
        for n in range(NT):
            tsl = slice(n * 128, (n + 1) * 128)
            DMA("sp", x1[:, n, :], x_d[tsl, :], ("x1ld", n % 4), [], [("x1", n)])
            pp = pt[n % 2]
            for half in range(2):
                for kt in range(KT):
                    MM(pp[:, half * 512:(half + 1) * 512], mT[:, kt, tsl], wout[:, kt, half * 512:(half + 1) * 512],
                       kt == 0, kt == KT - 1, [("mT", n), "wout"], [bkey((n % 2) * 2 + half)])
            TT("dve", x1[:, n, :], x1[:, n, :], pp[:, :], ALU.add, [("x1", n), bkey((n % 2) * 2), bkey((n % 2) * 2 + 1)], [("x1", n)])
            b = n % 2
            ssap = small[:, 8 + b:9 + b]
            ssk = ("ss2", b)
            ACTF(junk, x1[:, n, :], AF.Square, [("x1", n)], ["junk", ssk], accum_out=ssap)
            rstd_inplace(ssap, D, ssk)
            STT(mixed[:, n, :], x1[:, n, :], ssap, n2_bc, ALU.mult, ALU.mult, [("x1", n), ssk, "n2_bc"], [("mixed", n)])
            for kt in range(KT):
                TR(bbank(b)[:, kt * 128:(kt + 1) * 128], mixed[:, n, kt * 128:(kt + 1) * 128], ident_b[:],
                   [("mixed", n), "ident_b"], [("pbb", b)])
            CP("act", hT[:, :, tsl], bbank(b).rearrange("p (k t) -> p k t", k=KT), [("pbb", b)], [("hT", n)])
        MARK("e0")

        P.barrier()
        NB = 64
        wr_d = dram("moe_wr", [D, 36])
        wgu0_d = dram("moe_wgu0", [4096, 2048])
        wgu1_d = dram("moe_wgu1", [4096, 2048])
        wdr_d = dram("moe_wdr", [4096, 2048])
        xb_d = nc.dram_tensor("moe_xb", [NB * 128, D], BF16, kind="Internal").ap()
        yb_d = nc.dram_tensor("moe_yb", [NB * 128, D], F32, kind="Internal").ap()
        off = X1_END
        stg = []
        for i in range(3):
            a, off = carve(off, [2048], F32)
            stg.append(a)
        wgu_bf = []
        wd_bf = []
        for i in range(2):
            a, off = carve(off, [KT, 512], BF16)
            wgu_bf.append(a)
            a, off = carve(off, [2, D], BF16)
            wd_bf.append(a)
        wr, off = carve(off, [KT, 36], BF16)
        lg, off = carve(off, [NT, 36], F32)
        oh1, off = carve(off, [NT, 32], F32)
        oh2, off = carve(off, [NT, 32], F32)
        msk, off = carve(off, [NT, 32], F32)
        rank, off = carve(off, [NT, 32], F32)
        tmp3, off = carve(off, [NT, 32], F32)
        gtmp, off = carve(off, [NT, 4], F32)
        ohg, off = carve(off, [NT, 4], F32)
        rv, off = carve(off, [8, NT], F32)
        mcum, off = carve(off, [32], F32)
        cnt, off = carve(off, [32], F32)
        padded, off = carve(off, [32], F32)
        ends, off = carve(off, [32], F32)
        pstart, off = carve(off, [32], F32)
        ebf, off = carve(off, [NB], F32)
        widx_f, off = carve(off, [NB], F32)
        widx, off = carve(off, [NB], I32)
        dest_f, off = carve(off, [2, NT], F32)
        dest_i, off = carve(off, [2, NT], I32)
        MOE_END = off
        cmpb = stg[0].rearrange("p (b e) -> p b e", b=NB)
        cmpj = stg[1][:, 0:512].rearrange("p (e j) -> p e j", e=32)
        ht32 = hT[:].rearrange("p k t -> p (k t)").bitcast(F32)
        HTCAP = 32 * 1024
        hoff = 0
        xg, xgT, sil, hid_bf, hidT, ysb = [], [], [], [], [], []
        for i in range(2):
            a, hoff = carve(hoff, [D], BF16, ht32, HTCAP); xg.append(a)
            a, hoff = carve(hoff, [KT, 128], BF16, ht32, HTCAP); xgT.append(a)
            a, hoff = carve(hoff, [256], F32, ht32, HTCAP); sil.append(a)
            a, hoff = carve(hoff, [256], BF16, ht32, HTCAP); hid_bf.append(a)
            a, hoff = carve(hoff, [2, 128], BF16, ht32, HTCAP); hidT.append(a)
            a, hoff = carve(hoff, [D], F32, ht32, HTCAP); ysb.append(a)
        mix32 = mixed[:].rearrange("p n d -> p (n d)").bitcast(F32)
        MIXCAP = 32 * 1024
        moff = 0
        yg = []
        for i in range(2):
            a, moff = carve(moff, [D], F32, mix32, MIXCAP); yg.append(a)
        stgB = []
        for i in range(3):
            a, moff = carve(moff, [2048], F32, mix32, MIXCAP); stgB.append(a)

        DMA("pool", wr, wr_d.rearrange("(k p) c -> p k c", p=128), "w_wr", [], ["wr"])
        for n in range(NT):
            bi = n % 2
            for kt in range(KT):
                MM(bank(bi)[:, 0:36], hT[:, kt, n * 128:(n + 1) * 128], wr[:, kt, :], kt == 0, kt == KT - 1,
                   ["wr", ("hT", n)], [bkey(bi)])
            CP("act", lg[:, n, :], bank(bi)[:, 0:36], [bkey(bi)], ["lg"])
        BIG = 10000.0
        glv = lg[:, :, 0:4]
        elv = lg[:, :, 4:36]

        def RED(out, in_, op, R, W):
            P.op("dve", lambda e: e.tensor_reduce(out=out, in_=in_, axis=AX.X, op=op), R, W, cost=100.0 + _fsz(in_) * 1.0)

        def bcn(ap2, k):
            return ap2.unsqueeze(2).to_broadcast([128, NT, k])

        gmax, gsum, m1, m2, w1, w2 = (rv[:, i, :] for i in range(6))
        RED(gmax, glv, ALU.max, ["lg"], ["rv"])
        TT("dve", ohg, glv, bcn(gmax, 4), ALU.is_equal, ["lg", "rv"], ["ohg"])
        TT("dve", gtmp, glv, bcn(gmax, 4), ALU.subtract, ["lg", "rv"], ["gtmp"])
        ACTF(gtmp, gtmp, AF.Exp, ["gtmp"], ["gtmp"])
        RED(gsum, gtmp, ALU.add, ["gtmp"], ["rv"])
        RECIP(gsum, gsum, ["rv"], ["rv"])
        TS("dve", ohg, ohg, BIG, -BIG, ALU.mult, ALU.add, ["ohg"], ["ohg"])
        TT("dve", msk.rearrange("p n (g e) -> p n g e", g=4), elv.rearrange("p n (g e) -> p n g e", g=4),
           ohg.unsqueeze(3).to_broadcast([128, NT, 4, 8]), ALU.add, ["lg", "ohg"], ["msk"])
        RED(m1, msk, ALU.max, ["msk"], ["rv"])
        TT("dve", oh1, msk, bcn(m1, 32), ALU.is_equal, ["msk", "rv"], ["oh1"])
        STT(msk, oh1, -BIG, msk, ALU.mult, ALU.add, ["oh1", "msk"], ["msk"])
        RED(m2, msk, ALU.max, ["msk"], ["rv"])
        TT("dve", oh2, msk, bcn(m2, 32), ALU.is_equal, ["msk", "rv"], ["oh2"])
        TT("dve", w2, m2, m1, ALU.subtract, ["rv"], ["rv"])
        ACTF(w2, w2, AF.Exp, ["rv"], ["rv"])
        TS("dve", w1, w2, 1.0, None, ALU.add, ALU.bypass, ["rv"], ["rv"])
        RECIP(w1, w1, ["rv"], ["rv"])
        TT("dve", w1, w1, gsum, ALU.mult, ["rv"], ["rv"])
        TT("dve", w2, w2, w1, ALU.mult, ["rv"], ["rv"])
        TT("dve", msk, oh1, oh2, ALU.add, ["oh1", "oh2", "msk"], ["msk"])
        MEMSET("dve", mcum, 0.0, ["mcum"])
        for n in range(NT):
            bi = n % 2
            MM(bank(bi)[:, 0:32], cst("m_lt"), msk[:, n, :], True, False, ["consts", "msk"], [bkey(bi)])
            MM(bank(bi)[:, 0:32], cst("ones"), mcum, False, True, ["consts", "mcum"], [bkey(bi)])
            CP("act", rank[:, n, :], bank(bi)[:, 0:32], [bkey(bi)], ["rank"])
            TT("dve", mcum, mcum, msk[:, n, :], ALU.add, ["mcum", "msk"], ["mcum"])
        MM(bank(0)[:, 0:32], cst("ones"), mcum, True, True, ["consts", "mcum"], [bkey(0)])
        CP("act", cnt, bank(0)[:, 0:32], [bkey(0)], ["cnt"])
        TT("dve", cmpj, cnt.unsqueeze(2).to_broadcast([128, 32, 16]),
           cst("bvals")[:, 0:16].unsqueeze(1).to_broadcast([128, 32, 16]), ALU.is_gt, ["cnt", "consts"], [("stg", 1)])
        RED(padded, cmpj, ALU.add, [("stg", 1)], ["padded"])
        TS("dve", padded, padded, 128.0, None, ALU.mult, ALU.bypass, ["padded"], ["padded"])
        P.op("dve", lambda e: e.tensor_tensor_scan(out=ends, data0=cst("ones")[:, 0:32], data1=padded, initial=0.0,
                                                  op0=ALU.mult, op1=ALU.add), ["consts", "padded"], ["ends"], cost=300.0)
        TT("dve", pstart, ends, padded, ALU.subtract, ["ends", "padded"], ["pstart"])
        TT("dve", rank, rank, pstart.unsqueeze(1).to_broadcast([128, NT, 32]), ALU.add, ["rank", "pstart"], ["rank"])
        for k, ohk in ((0, oh1), (1, oh2)):
            TT("dve", tmp3, ohk, rank, ALU.mult, ["oh1", "oh2", "rank"], ["tmp3"])
            RED(dest_f[:, k, :], tmp3, ALU.add, ["tmp3"], ["dest_f"])
        CP("dve", dest_i, dest_f, ["dest_f"], ["dest_i"])
        TT("dve", cmpb, ends.unsqueeze(1).to_broadcast([128, NB, 32]),
           cst("bvals")[:, 0:NB].unsqueeze(2).to_broadcast([128, NB, 32]), ALU.is_le, ["ends", "consts"], [("stg", 0)])
        RED(ebf, cmpb, ALU.add, [("stg", 0)], ["ebf"])
        STT(widx_f, ebf, 128.0, cst("pidx")[:, 0:NB], ALU.mult, ALU.add, ["ebf", "consts"], ["widx_f"])
        CP("dve", widx, widx_f, ["widx_f"], ["widx"])
        MARK("e1")

        IOA = bass.IndirectOffsetOnAxis
        regs = {}

        def _pool_init(e):
            regs["bc"] = e.alloc_register("moe_bc")
            e.reg_mov(regs["bc"], 4095)
        P.pool_init = _pool_init
        XB_KEYS = []
        zt, off = carve(off, [D], BF16)
        MEMSET("pool", zt, 0.0, ["zt"])
        DMA("sp", xb_d.rearrange("(p r) d -> p r d", p=128), zt.unsqueeze(1).to_broadcast([128, NB, D]), "xbz", ["zt"], ["xb0"])
        for n in range(NT):
            for k in range(2):
                idx_ap = dest_i[:, k, n:n + 1]
                src_ap = mixed[:, n, :]
                P.dma("pool", lambda e, idx_ap=idx_ap, src_ap=src_ap: e.indirect_dma_start(
                    out=xb_d[:, :], out_offset=IOA(ap=idx_ap, axis=0), in_=src_ap, in_offset=None),
                    ("sc", (2 * n + k) % 4), [("mixed", n), "dest_i", "xb0"], [("xb", n, k)], nbytes=256 * 1024)
                XB_KEYS.append(("xb", n, k))

        def gather_w(dst, src_d, b, skey, extra):
            idx_ap = widx[:, b:b + 1]
            P.dma("pool", lambda e: e.indirect_dma_start(
                out=dst, out_offset=None, in_=src_d[:, :], in_offset=IOA(ap=idx_ap, axis=0),
                bounds_check=regs["bc"], oob_is_err=False),
                skey, ["widx"] + extra, [skey], nbytes=1 << 20)

        YB_KEYS = []
        for b in range(NB):
            s = b % 2
            sset = stg if b % 2 == 0 else stgB
            so = 0 if b % 2 == 0 else 3
            extra = [] if b % 2 == 0 else XB_KEYS
            gather_w(sset[0], wgu0_d, b, ("stg", so + 0), extra)
            gather_w(sset[1], wgu1_d, b, ("stg", so + 1), extra)
            gather_w(sset[2], wdr_d, b, ("stg", so + 2), extra)
            CP("act", wgu_bf[s][:, 0:4, :], sset[0].rearrange("p (k c) -> p k c", k=4), [("stg", so + 0)], [("wgu_bf", s, 0)])
            CP("dve", wgu_bf[s][:, 4:8, :], sset[1].rearrange("p (k c) -> p k c", k=4), [("stg", so + 1)], [("wgu_bf", s, 1)])
            CP("act" if b % 4 < 2 else "dve", wd_bf[s], sset[2].rearrange("p (k c) -> p k c", k=2), [("stg", so + 2)], [("wd_bf", s)])
            DMA("sp", xg[s], xb_d[b * 128:(b + 1) * 128, :], ("xg", s), XB_KEYS, [("xg", s)])
            for kt in range(KT):
                TR(bbank(s)[:, kt * 128:(kt + 1) * 128], xg[s][:, kt * 128:(kt + 1) * 128], ident_b[:],
                   [("xg", s), "ident_b"], [("pbb", s)])
            CP("act", xgT[s], bbank(s).rearrange("p (k t) -> p k t", k=KT), [("pbb", s)], [("xgT", s)])
            hb = bank(s)
            for kt in range(KT):
                MM(hb, xgT[s][:, kt, :], wgu_bf[s][:, kt, :], kt == 0, kt == KT - 1,
                   [("xgT", s), ("wgu_bf", s, 0), ("wgu_bf", s, 1)], [bkey(s)])
            ACTF(sil[s], hb[:, 0:256], AF.Silu, [bkey(s)], [("sil", s)])
            TT("dve", hid_bf[s], sil[s], hb[:, 256:512], ALU.mult, [("sil", s), bkey(s)], [("hid_bf", s)])
            for ft in range(2):
                TR(bbank(s)[:, ft * 128:(ft + 1) * 128], hid_bf[s][:, ft * 128:(ft + 1) * 128], ident_b[:],
                   [("hid_bf", s), "ident_b"], [("pbb", s)])
            CP("act", hidT[s], bbank(s)[:, 0:256].rearrange("p (k t) -> p k t", k=2), [("pbb", s)], [("hidT", s)])
            yp = pt[1 + s]
            for half in range(2):
                for ft in range(2):
                    MM(yp[:, half * 512:(half + 1) * 512], hidT[s][:, ft, :], wd_bf[s][:, ft, half * 512:(half + 1) * 512],
                       ft == 0, ft == 1, [("hidT", s), ("wd_bf", s)], [bkey(2 + 2 * s + half)])
            CP("act" if b % 2 else "dve", ysb[s], yp[:, :], [bkey(2 + 2 * s), bkey(3 + 2 * s)], [("ysb", s)])
            DMA("sp", yb_d[b * 128:(b + 1) * 128, :], ysb[s], ("yst", s), [("ysb", s)], [("yb", b)])
            YB_KEYS.append(("yb", b))
        MARK("e2")
        for n in range(NT):
            for k in range(2):
                s = (2 * n + k) % 2
                idx_ap = dest_i[:, k, n:n + 1]
                dst = yg[s]
                P.dma("pool", lambda e, idx_ap=idx_ap, dst=dst: e.indirect_dma_start(
                    out=dst, out_offset=None, in_=yb_d[:, :], in_offset=IOA(ap=idx_ap, axis=0)),
                    ("yg", s), YB_KEYS + ["dest_i"], [("yg", s)], nbytes=512 * 1024)
                wk = rv[:, 4 + k, n:n + 1]
                STT(x1[:, n, :], yg[s], wk, x1[:, n, :], ALU.mult, ALU.add, [("yg", s), "rv", ("x1", n)], [("x1", n)])

        P.barrier()
        off = X1_END
        nf_bc, off = carve(off, [D], F32)
        ob = [None, None]
        ob[0], off = carve(off, [D], F32)
        ob[1], off = carve(off, [D], F32)
        junk2, off = carve(off, [D], BF16)
        DMA("sp", nf_bc, nf_d.partition_broadcast(128), "c_nf", [], ["nf_bc"])
        for n in range(NT):
            b = n % 2
            ssap = small[:, 12 + b:13 + b]
            ssk = ("ss3", b)
            ACTF(junk2, x1[:, n, :], AF.Square, [("x1", n)], ["junk2", ssk], accum_out=ssap)
            rstd_inplace(ssap, D, ssk)
            STT(ob[b], x1[:, n, :], ssap, nf_bc, ALU.mult, ALU.mult, [("x1", n), ssk, "nf_bc"], [("ob", b)])
            DMA("sp", out_d[n * 128:(n + 1) * 128, :], ob[b], ("out_st", b), [("ob", b)], [("out", n)])
        if not dbg:
            P.wait_all("sp", [("out", n) for n in range(NT)])
        if dbg:
            P.enabled = True
            P.barrier()
            for n in range(NT):
                DMA("sp", dbg_d[n * 128:(n + 1) * 128, :], x1[:, n, :], ("dbg_out", n % 2), [("x1", n)], [("dbg", n)])
            P.wait_all("sp", [("dbg", n) for n in range(NT)] + [("out", n) for n in range(NT)])
        P.emit()
    return nc


def make_in_maps(inputs, n_cores=8):
    f = lambda k: np.asarray(inputs[k], np.float32)
    x = f("x")
    _gu = np.concatenate([f("moe_w_gate")[0], f("moe_w_up")[0]], axis=2).reshape(32, 8, 128, 512).transpose(0, 2, 1, 3)
    shared = {
        "norm1_w": f("norm1_w").reshape(1, D),
        "norm2_w": f("norm2_w").reshape(1, D),
        "norm_f_w": f("norm_f_w").reshape(1, D),
        "consts": CONST_ARR,
        "w_in": np.ascontiguousarray(f("w_in")[0]),
        "gla_w2b_f": np.ascontiguousarray(np.concatenate([f("gla_gate_w2_fwd")[0], f("gla_gate_b_fwd")], axis=0)),
        "gla_w2b_b": np.ascontiguousarray(np.concatenate([f("gla_gate_w2_bwd")[0], f("gla_gate_b_bwd")], axis=0)),
        "gla_norm_w": f("gla_norm_w").reshape(1, 256),
        "w_out": np.ascontiguousarray(f("w_out")[0]),
        "moe_wr": np.ascontiguousarray(np.concatenate([f("moe_w_group")[0], f("moe_w_router")[0]], axis=1)),
        "moe_wgu0": _gu[:, :, 0:4, :].reshape(4096, 2048).copy(),
        "moe_wgu1": _gu[:, :, 4:8, :].reshape(4096, 2048).copy(),
        "moe_wdr": np.ascontiguousarray(f("moe_w_down")[0].reshape(32, 2, 128, 1024).transpose(0, 2, 1, 3)).reshape(4096, 2048),
        "gdn_norm_w": f("gdn_norm_w").reshape(1, 128),
        "gdn_vec": np.ascontiguousarray(np.concatenate([f("gdn_dt_bias_fwd")[0], f("gdn_dt_bias_bwd")[0],
                                                        f("gdn_a_log_fwd")[0], f("gdn_a_log_bwd")[0]]).reshape(1, 32)),
        "gdn_conv_wT": np.ascontiguousarray(f("gdn_conv_w")[0].T.reshape(24, 128, 5).transpose(1, 0, 2)),
    }
    maps = []
    for c in range(n_cores):
        m = dict(shared)
        m["x"] = np.ascontiguousarray(x[c])
        maps.append(m)
    return maps


def kernel(**inputs):
    nc = build()
    in_maps = make_in_maps(inputs)
    res = run_bass_kernel_spmd(nc, in_maps, core_ids=list(range(8)))
    out = np.stack([np.asarray(r["out"]) for r in res.results], axis=0)
    return out.astype(np.float32)
```

```python
import contextlib
import heapq
import numpy as np
import concourse.bass as bass
import concourse.mybir as mybir
from concourse.bass_utils import run_bass_kernel_spmd

F32 = mybir.dt.float32
BF16 = mybir.dt.bfloat16
I32 = mybir.dt.int32
AF = mybir.ActivationFunctionType
ALU = mybir.AluOpType
AX = mybir.AxisListType

T = 2048
D = 1024
NT = T // 128
KT = D // 128
EPS = 1e-6
SAME_ENGINE_SYNC = True
EPOCH = 20000
SYNC_NS = 120.0
DMA_LAT_NS = 2200.0


class Prog:
    ENGS = ("pe", "act", "dve", "pool", "sp")

    def __init__(self, nc, stack):
        self.nc = nc
        self.stack = stack
        self.streams = {e: [] for e in self.ENGS}
        self.count = {e: 0 for e in self.ENGS}
        self.esems = {e: [] for e in self.ENGS}
        self.known = {e: {} for e in self.ENGS}
        self.last_write = {}
        self.readers = {}
        self.dma_sems = {}
        self.dma_vals = {}
        self.dma_last = {}
        self.enabled = True
        self.seg = []
        self.ticks = {}
        self.nops = 0
        self.seg_base = 0
        self.pool_init = None

    def _new_sem(self, name):
        return self.stack.enter_context(self.nc.semaphore(name))

    @staticmethod
    def _psum_fix(reads, writes):
        r2, w2 = [], list(writes)
        for k in reads:
            if isinstance(k, tuple) and k[0] in ("pb", "pbb"):
                if k not in w2:
                    w2.append(k)
            else:
                r2.append(k)
        return r2, w2

    def _record(self, eng, fn, reads, writes, cost, kind, semkey=None):
        reads, writes = self._psum_fix(list(reads), list(writes))
        oid = self.nops
        self.nops += 1
        preds = set()
        for r in reads:
            t = self.last_write.get(r)
            if t is not None:
                preds.add(t)
        for w in writes:
            t = self.last_write.get(w)
            if t is not None:
                preds.add(t)
            preds.update(self.readers.get(w, ()))
        if kind == "dma":
            prev = self.dma_last.get(semkey)
            if prev is not None:
                preds.add(prev)
            self.dma_last[semkey] = oid
        preds = {p for p in preds if p >= self.seg_base}
        self.seg.append(dict(id=oid, eng=eng, fn=fn, preds=preds, cost=float(cost), kind=kind, semkey=semkey))
        for w in writes:
            self.last_write[w] = oid
            self.readers[w] = []
        for r in reads:
            self.readers.setdefault(r, []).append(oid)
        return oid

    def op(self, eng, fn, reads=(), writes=(), cost=300.0):
        if not self.enabled:
            return
        self._record(eng, fn, reads, writes, cost, "op")

    def dma(self, eng, fn, semkey, reads=(), writes=(), nbytes=1 << 20):
        if not self.enabled:
            return
        self._record(eng, fn, reads, writes, DMA_LAT_NS + nbytes / 160.0, "dma", semkey)

    def wait_all(self, eng, keys):
        self._record(eng, None, list(keys), [], 0.0, "op")

    def _schedule_segment(self):
        ops = self.seg
        if not ops:
            return
        byid = {o["id"]: o for o in ops}
        succ = {o["id"]: [] for o in ops}
        indeg = {}
        for o in ops:
            indeg[o["id"]] = len(o["preds"])
            for p in o["preds"]:
                succ[p].append(o["id"])
        ready_t = {o["id"]: 0.0 for o in ops}
        finish = {}
        heaps = {e: [] for e in self.ENGS}
        for o in ops:
            if indeg[o["id"]] == 0:
                heapq.heappush(heaps[o["eng"]], (0.0, o["id"]))
        etime = {e: 0.0 for e in self.ENGS}
        order = {e: [] for e in self.ENGS}
        remaining = len(ops)
        while remaining:
            best = None
            for e in self.ENGS:
                h = heaps[e]
                if not h:
                    continue
                rt, oid = h[0]
                st = max(rt, etime[e])
                if best is None or (st, oid) < (best[0], best[1]):
                    best = (st, oid, e)
            st, oid, e = best
            heapq.heappop(heaps[e])
            o = byid[oid]
            if o["kind"] == "dma":
                etime[e] = st + 150.0
                fin = st + o["cost"]
            else:
                etime[e] = st + o["cost"]
                fin = etime[e]
            finish[oid] = fin
            order[e].append(o)
            remaining -= 1
            for s in succ[oid]:
                so = byid[s]
                lat = SYNC_NS if (so["eng"] != e or o["kind"] == "dma") else (60.0 if e != "pe" else 0.0)
                ready_t[s] = max(ready_t[s], fin + lat)
                indeg[s] -= 1
                if indeg[s] == 0:
                    heapq.heappush(heaps[so["eng"]], (ready_t[s], s))
        self.est_ns = getattr(self, "est_ns", 0.0) + max(list(finish.values()) + [0.0])
        for o in ops:
            if o["kind"] == "dma":
                k = o["semkey"]
                if k not in self.dma_sems:
                    self.dma_sems[k] = self._new_sem(f"d{len(self.dma_sems)}")
                    self.dma_vals[k] = 0
                self.dma_vals[k] += 16
                self.ticks[o["id"]] = (self.dma_sems[k], self.dma_vals[k], "dma")
        def needs_sem(o):
            for s_ in succ[o["id"]]:
                se = byid[s_]["eng"]
                if se != o["eng"] or (SAME_ENGINE_SYNC and se != "pe"):
                    return True
            return False
        for e in self.ENGS:
            real = [o for o in order[e] if o["kind"] == "op" and o["fn"] is not None]
            for i_, o in enumerate(real):
                o["sig"] = needs_sem(o) or i_ == len(real) - 1
        for e in self.ENGS:
            for o in order[e]:
                if o["kind"] == "op" and o["fn"] is not None and o["sig"]:
                    c = self.count[e]
                    ep, v = divmod(c, EPOCH)
                    while len(self.esems[e]) <= ep:
                        self.esems[e].append(self._new_sem(f"s_{e}_{len(self.esems[e])}"))
                    self.count[e] = c + 1
                    self.ticks[o["id"]] = (self.esems[e][ep], v + 1, e)
        for e in self.ENGS:
            for o in order[e]:
                waits = {}
                for p in o["preds"]:
                    if byid[p]["eng"] == e and byid[p]["kind"] == "op" and (not SAME_ENGINE_SYNC or e == "pe"):
                        continue
                    sem, val, src = self.ticks[p]
                    sid = id(sem)
                    if self.known[e].get(sid, 0) >= val:
                        continue
                    if sid not in waits or waits[sid][1] < val:
                        waits[sid] = (sem, val)
                for sid, (sem, val) in waits.items():
                    self.known[e][sid] = val
                inc = None
                if o["fn"] is not None and o["id"] in self.ticks:
                    sem, val, src = self.ticks[o["id"]]
                    inc = (sem, 16 if o["kind"] == "dma" else 1)
                self.streams[e].append((o["fn"], list(waits.values()), inc))
        self.seg = []
        self.seg_base = self.nops

    def barrier(self):
        if not self.enabled and not self.seg:
            return
        self._schedule_segment()
        ticks = []
        for e2 in self.ENGS:
            c = self.count[e2]
            if c > 0:
                ep, v = divmod(c - 1, EPOCH)
                ticks.append((self.esems[e2][ep], v + 1))
        for k, sem in self.dma_sems.items():
            ticks.append((sem, self.dma_vals[k]))
        for eng in self.ENGS:
            waits = []
            for (sem, val) in ticks:
                if self.known[eng].get(id(sem), 0) >= val:
                    continue
                self.known[eng][id(sem)] = val
                waits.append((sem, val))
            if waits:
                self.streams[eng].append((None, waits, None))

    def emit(self):
        self._schedule_segment()
        nc = self.nc
        with nc.Block() as block:
            def run(e, stream):
                for fn, waits, inc in stream:
                    for sem, val in waits:
                        e.wait_ge(sem, val)
                    if fn is None:
                        continue
                    ins = fn(e)
                    if inc is not None:
                        ins.then_inc(inc[0], inc[1])

            @block.tensor
            def _(e):
                run(e, self.streams["pe"])

            @block.scalar
            def _(e):
                run(e, self.streams["act"])

            @block.vector
            def _(e):
                run(e, self.streams["dve"])

            @block.gpsimd
            def _(e):
                if self.pool_init is not None:
                    self.pool_init(e)
                run(e, self.streams["pool"])

            @block.sync
            def _(e):
                run(e, self.streams["sp"])


def _fsz(ap):
    s = ap.shape
    n = 1
    for v in s[1:]:
        n *= int(v)
    return n


C_GQ, C_GK, C_GV, C_GR = 0, 512, 1024, 2048
C_GLF, C_GLB = 3072, 3088
C_DQ, C_DK, C_DV, C_DZ = 3104, 4128, 5152, 6176
C_DAB = 7200
C_MA, C_MB = 7232, 8256
D_IN = 9280


def host_consts():
    r = np.arange(128)[:, None]
    t = np.arange(128)[None, :]
    same = (r // 64) == (t // 64)
    c = {}
    c["ident"] = np.eye(128, dtype=np.float32)
    c["a_le"] = np.where(r <= t, -1.0 / 16, 0.0)
    c["a_ge"] = np.where(r >= t, -1.0 / 16, 0.0)
    c["a_gt"] = np.where(r > t, -1.0 / 16, 0.0)
    c["a_lt"] = np.where(r < t, -1.0 / 16, 0.0)
    c["m_le"] = np.where(r <= t, 1.0, 0.0)
    c["m_ge"] = np.where(r >= t, 1.0, 0.0)
    c["b_le"] = np.where((r <= t) & same, 1.0, 0.0)
    c["b_ge"] = np.where((r >= t) & same, 1.0, 0.0)
    c["b_gt"] = np.where((r > t) & same, 1.0, 0.0)
    c["b_lt"] = np.where((r < t) & same, 1.0, 0.0)
    c["csel0"] = np.where(r < 64, 1.0, 0.0) + 0.0 * t
    c["csel1"] = np.where(r >= 64, 1.0, 0.0) + 0.0 * t
    c["ones"] = np.ones((128, 128))
    c["m_lt"] = np.where(r < t, 1.0, 0.0)
    c["bvals"] = 128.0 * t + 0.0 * r
    c["pidx"] = 1.0 * r + 0.0 * t
    names = list(c.keys())
    arr = np.stack([np.asarray(c[n], np.float32) for n in names], axis=1)
    return names, np.ascontiguousarray(arr)


CONST_NAMES, CONST_ARR = host_consts()
NCONST = len(CONST_NAMES)


def build(stage="all", dbg=False):
    nc = bass.Bass("TRN2", target_bir_lowering=False)
    stack = contextlib.ExitStack()
    with stack:
        P = Prog(nc, stack)

        def dram(name, shape, dt=F32, kind="ExternalInput"):
            return nc.dram_tensor(name, list(shape), dt, kind=kind).ap()

        def sb(name, shape, dt=F32):
            return stack.enter_context(nc.sbuf_tensor(name, list(shape), dt))

        def ps(name, shape, dt=F32):
            return stack.enter_context(nc.psum_tensor(name, list(shape), dt))

        def MM(out, lhsT, rhs, start, stop, R, W):
            n = _fsz(rhs)
            c = 70.0 + n * 0.75
            if rhs.dtype == F32:
                c *= 4.0
            P.op("pe", lambda e: e.matmul(out, lhsT, rhs, start=start, stop=stop), R, W, cost=c)

        def TR(out, in_, ident, R, W):
            P.op("pe", lambda e: e.transpose(out=out, in_=in_, identity=ident), R, W, cost=110.0)

        def ACTF(out, in_, func, R, W, **kw):
            c = 120.0 + _fsz(in_) * 0.6 + (90.0 if "accum_out" in kw else 0.0)
            P.op("act", lambda e: e.activation(out=out, in_=in_, func=func, **kw), R, W, cost=c)

        def _vc(eng, n, k=1.5):
            return (100.0 + n * k * 0.6) if eng == "dve" else (150.0 + n * 1.9)

        def TT(eng, out, in0, in1, op, R, W):
            P.op(eng, lambda e: e.tensor_tensor(out=out, in0=in0, in1=in1, op=op), R, W, cost=_vc(eng, _fsz(out)))

        def TS(eng, out, in0, s1, s2, op0, op1, R, W):
            P.op(eng, lambda e: e.tensor_scalar(out=out, in0=in0, scalar1=s1, scalar2=s2, op0=op0, op1=op1), R, W,
                 cost=_vc(eng, _fsz(out), 1.05))

        def STT(out, in0, scalar, in1, op0, op1, R, W):
            P.op("dve", lambda e: e.scalar_tensor_tensor(out=out, in0=in0, scalar=scalar, in1=in1, op0=op0, op1=op1), R, W,
                 cost=_vc("dve", _fsz(out)))

        def CP(eng, out, in_, R, W):
            if eng == "act":
                P.op("act", lambda e: e.activation(out=out, in_=in_, func=AF.Copy), R, W, cost=120.0 + _fsz(in_) * 0.6)
            else:
                P.op(eng, lambda e: e.tensor_copy(out=out, in_=in_), R, W, cost=_vc(eng, _fsz(out), 1.05))

        def MEMSET(eng, ap, val, W):
            P.op(eng, lambda e: e.memset(ap, val), [], W, cost=_vc(eng, _fsz(ap), 0.6))

        def DMA(eng, out, in_, semkey, R, W):
            P.dma(eng, lambda e: e.dma_start(out=out, in_=in_), semkey, R, W, nbytes=_fsz(out) * int(out.shape[0]) * 4)

        def RECIP(out, in_, R, W):
            P.op("dve", lambda e: e.reciprocal(out=out, in_=in_), R, W, cost=_vc("dve", _fsz(out), 1.05))

        def MARK(name):
            if stage == name:
                P.enabled = False

        def rstd_inplace(ap, n, key):
            TS("dve", ap, ap, 1.0 / n, EPS, ALU.mult, ALU.add, [key], [key])
            ACTF(ap, ap, AF.Ln, [key], [key])
            ACTF(ap, ap, AF.Exp, [key], [key], scale=-0.5)

        x_d = dram("x", [T, D])
        n1_d = dram("norm1_w", [1, D])
        n2_d = dram("norm2_w", [1, D])
        nf_d = dram("norm_f_w", [1, D])
        consts_d = dram("consts", [128, NCONST, 128])
        w_in_d = dram("w_in", [D, D_IN])
        w2b_d = [dram("gla_w2b_f", [17, 512]), dram("gla_w2b_b", [17, 512])]
        gnw_d = dram("gla_norm_w", [1, 256])
        out_d = dram("out", [T, D], kind="ExternalOutput")
        dbg_d = dram("dbg", [T, D], kind="ExternalOutput") if dbg else None

        consts = sb("consts_sb", [128, NCONST, 128])
        CI = {n: i for i, n in enumerate(CONST_NAMES)}

        def cst(name):
            return consts[:, CI[name], :]

        ident_b = sb("ident_b", [128, 128], BF16)
        ones_b = sb("ones_b", [128, 128], BF16)
        hT = sb("hT", [128, KT, T], BF16)
        mixed = sb("mixed", [128, NT, D], BF16)
        small = sb("small", [128, 64])
        ARENA_BYTES = 134 * 1024
        arena = sb("arena", [128, ARENA_BYTES // 4])

        def carve(off, shape, dt, base=None, cap=None):
            base = arena if base is None else base
            cap = ARENA_BYTES if cap is None else cap
            nb = int(np.prod(shape)) * (2 if dt == BF16 else 4)
            nb = (nb + 3) // 4 * 4
            assert off % 4 == 0 and off + nb <= cap, (off, nb)
            v = base[:, off // 4:(off + nb) // 4]
            if dt != F32:
                v = v.bitcast(dt)
            if len(shape) == 2:
                pat = "p (a b) -> p a b"
                v = v.rearrange(pat, a=shape[0])
            elif len(shape) == 3:
                v = v.rearrange("p (a b c) -> p a b c", a=shape[0], b=shape[1])
            return v, off + nb

        pt = [ps(f"pt{i}", [128, 1024]) for i in range(3)]
        ptb = ps("ptb", [128, 2048], BF16)

        def bank(i):
            return pt[i // 2][:, (i % 2) * 512:(i % 2 + 1) * 512]

        def bkey(i):
            return ("pb", i)

        def bbank(i):
            return ptb[:, i * 1024:(i + 1) * 1024]

        DMA("sp", consts[:], consts_d[:, :, :], "c_consts", [], ["consts"])
        CP("dve", ident_b[:], cst("ident"), ["consts"], ["ident_b"])
        MEMSET("pool", ones_b[:], 1.0, ["ones_b"])

        off = 116 * 1024
        xt0, off = carve(off, [D], F32)
        xt1, off = carve(off, [D], F32)
        hn0, off = carve(off, [D], BF16)
        hn1, off = carve(off, [D], BF16)
        sq, off = carve(off, [D], BF16)
        n1_bc, off = carve(off, [D], F32)
        DMA("sp", n1_bc, n1_d.partition_broadcast(128), "c_n1", [], ["n1_bc"])
        xts = [xt0, xt1]
        hns = [hn0, hn1]
        for tt in range(NT):
            b = tt % 2
            xb, hb = xts[b], hns[b]
            DMA("sp", xb, x_d[tt * 128:(tt + 1) * 128, :], ("xt", b), [], [("xt", b)])
            ACTF(sq, xb, AF.Square, [("xt", b)], ["sq", "ss0"], accum_out=small[:, 0:1])
            rstd_inplace(small[:, 0:1], D, "ss0")
            STT(hb, xb, small[:, 0:1], n1_bc, ALU.mult, ALU.mult, [("xt", b), "ss0", "n1_bc"], [("hn", b)])
            for kt in range(KT):
                TR(bbank(b)[:, kt * 128:(kt + 1) * 128], hb[:, kt * 128:(kt + 1) * 128], ident_b[:],
                   [("hn", b), "ident_b"], [("pbb", b)])
            CP("act", hT[:, :, tt * 128:(tt + 1) * 128], bbank(b).rearrange("p (k t) -> p k t", k=KT),
               [("pbb", b)], [("hT", tt)])
        HT_ALL = [("hT", tt) for tt in range(NT)]
        MARK("p1")

        off = 0
        qT, off = carve(off, [T], F32)
        kT, off = carve(off, [T], F32)
        k_tok, off = carve(off, [NT, 128], F32)
        v_tok, off = carve(off, [NT, 256], BF16)
        qdT = [None, None]
        kiT = [None, None]
        ktail = [None, None]
        for d_ in range(2):
            qdT[d_], off = carve(off, [T], BF16)
            kiT[d_], off = carve(off, [T], BF16)
            ktail[d_], off = carve(off, [NT, 128], BF16)
        sb_store, off = carve(off, [NT, 256], BF16)
        dec, off = carve(off, [2, NT], F32)
        S, off = carve(off, [256], F32)
        S_bf, off = carve(off, [256], BF16)
        NTMP = 3
        tmp = []
        for i in range(NTMP):
            d = {}
            for nm in ("e", "lg", "E", "Ei", "Et"):
                d[nm], off = carve(off, [128], F32)
            d["Pf"], off = carve(off, [128], BF16)
            d["Pb"], off = carve(off, [128], BF16)
            d["sig"], off = carve(off, [512], F32)
            d["G"], off = carve(off, [256], F32)
            tmp.append(d)
        gl, off = carve(off, [2, T], BF16)
        w2b, off = carve(off, [2, 512], BF16)
        wqk, off = carve(off, [KT, 256], BF16)
        wkv, off = carve(off, [KT, 384], BF16)
        wgm, off = carve(off, [KT, 512], BF16)
        wgl, off = carve(off, [KT, 32], BF16)
        gnw_bc, off = carve(off, [256], F32)
        GLA_END = off
        assert GLA_END <= 116 * 1024, GLA_END

        DMA("sp", gnw_bc, gnw_d.partition_broadcast(128), "c_gnw", [], ["gnw_bc"])
        MEMSET("pool", gl[:, :, :], 1.0, ["gl"])
        MEMSET("pool", w2b[:, :, :], 0.0, ["w2b"])
        for d_ in range(2):
            DMA("pool", w2b[0:17, d_, :], w2b_d[d_][:, :], "c_w2b", [], ["w2b"])
        DMA("pool", wgl, w_in_d[:, C_GLF:C_GLF + 32].rearrange("(k p) c -> p k c", p=128), "w_wgl", [], ["wgl"])
        for d_ in range(2):
            for tg in range(4):
                bi = tg % 2
                for kt in range(KT):
                    MM(bank(bi)[0:16, :], wgl[:, kt, d_ * 16:(d_ + 1) * 16], hT[:, kt, tg * 512:(tg + 1) * 512],
                       kt == 0, kt == KT - 1, ["wgl"] + HT_ALL[tg * 4:tg * 4 + 4], [bkey(bi)])
                CP("act", gl[0:16, d_, tg * 512:(tg + 1) * 512], bank(bi)[0:16, :], [bkey(bi)], ["gl"])

        MARK("g0")
        QSCALE = 128.0 ** -0.5
        for h in range(4):
            def wcols(dst, c0, n):
                return (dst, w_in_d[:, c0:c0 + n].rearrange("(k p) c -> p k c", p=128))
            for (dst, src) in (wcols(wqk[:, :, 0:128], C_GQ + h * 128, 128), wcols(wqk[:, :, 128:256], C_GK + h * 128, 128)):
                DMA("pool", dst, src, "w_wqk", [], ["wqk"])
            for (dst, src) in (wcols(wkv[:, :, 0:128], C_GK + h * 128, 128), wcols(wkv[:, :, 128:384], C_GV + h * 256, 256)):
                DMA("pool", dst, src, "w_wkv", [], ["wkv"])
            for (dst, src) in (wcols(wgm[:, :, 0:256], C_GR + h * 256, 256), wcols(wgm[:, :, 256:512], C_MA + h * 256, 256)):
                DMA("pool", dst, src, "w_wgm", [], ["wgm"])
            MARK("g1a")
            for which, dstT in ((0, qT), (1, kT)):
                for tg in range(4):
                    bi = (which * 4 + tg) % 4
                    for kt in range(KT):
                        MM(bank(bi), wqk[:, kt, which * 128:(which + 1) * 128], hT[:, kt, tg * 512:(tg + 1) * 512],
                           kt == 0, kt == KT - 1, ["wqk"] + HT_ALL[tg * 4:tg * 4 + 4], [bkey(bi)])
                    CP("act" if tg % 2 else "dve", dstT[:, tg * 512:(tg + 1) * 512], bank(bi), [bkey(bi)],
                       [("qkT", which, tg)])
            MARK("g1b")
            for n in range(NT):
                bi = 4 + n % 2
                for kt in range(KT):
                    MM(bank(bi)[:, 0:384], hT[:, kt, n * 128:(n + 1) * 128], wkv[:, kt, :],
                       kt == 0, kt == KT - 1, ["wkv", ("hT", n)], [bkey(bi)])
                CP("dve", k_tok[:, n, :], bank(bi)[:, 0:128], [bkey(bi)], [("k_tok", n)])
                CP("act", v_tok[:, n, :], bank(bi)[:, 128:384], [bkey(bi)], [("v_tok", n)])
            MARK("g1")
            for n in range(NT):
                tsl = slice(n * 128, (n + 1) * 128)
                tg = n // 4
                for d_ in range(2):
                    tm = tmp[(n * 2 + d_) % NTMP]
                    tk = ("gtmp", (n * 2 + d_) % NTMP)
                    a_c = cst("a_le") if d_ == 0 else cst("a_ge")
                    a_s = cst("a_gt") if d_ == 0 else cst("a_lt")
                    b0 = (n * 2 + d_) % 2 * 2
                    zb, cb = bank(b0), bank(b0 + 1)
                    MM(zb[:, 0:128], gl[:, d_, tsl], w2b[:, d_, h * 128:(h + 1) * 128], True, True,
                       ["gl", "w2b"], [bkey(b0)])
                    ACTF(tm["e"], zb[:, 0:128], AF.Exp, [bkey(b0)], [tk], scale=-1.0)
                    ACTF(tm["lg"], tm["e"], AF.Ln, [tk], [tk], bias=1.0)
                    MM(cb[:, 0:128], tm["lg"], a_c, True, True, [tk, "consts"], [bkey(b0 + 1)])
                    MM(cb[:, 128:256], a_s, tm["lg"], True, True, [tk, "consts"], [bkey(b0 + 1)])
                    ACTF(tm["E"], cb[:, 0:128], AF.Exp, [bkey(b0 + 1)], [tk])
                    ACTF(tm["Ei"], cb[:, 0:128], AF.Exp, [bkey(b0 + 1)], [tk], scale=-1.0)
                    ACTF(tm["Et"], cb[:, 128:256], AF.Exp, [bkey(b0 + 1)], [tk])
                    STT(qdT[d_][:, tsl], qT[:, tsl], QSCALE, tm["E"], ALU.mult, ALU.mult,
                        [("qkT", 0, tg), tk], [("qdT", d_, n)])
                    TT("dve", kiT[d_][:, tsl], kT[:, tsl], tm["Ei"], ALU.mult, [("qkT", 1, tg), tk], [("kiT", d_, n)])
                    TT("dve", ktail[d_][:, n, :], k_tok[:, n, :], tm["Et"], ALU.mult, [("k_tok", n), tk], [("ktail", d_, n)])
                    col = 127 if d_ == 0 else 0
                    CP("dve", dec[:, d_, n:n + 1], tm["E"][:, col:col + 1], [tk], [("dec", d_, n)])
            MARK("g2")
            MEMSET("dve", S, 0.0, ["S"])
            for n in range(NT - 1, -1, -1):
                CP("act", sb_store[:, n, :], S, ["S"], [("sb_store", n)])
                bi = 4 + n % 2
                MM(bank(bi)[:, 0:256], ktail[1][:, n, :], v_tok[:, n, :], True, True,
                   [("ktail", 1, n), ("v_tok", n)], [bkey(bi)])
                STT(S, S, dec[:, 1, n:n + 1], bank(bi)[:, 0:256], ALU.mult, ALU.add,
                    ["S", ("dec", 1, n), bkey(bi)], ["S"])
            MARK("g3")
            MEMSET("dve", S, 0.0, ["S"])
            for n in range(NT):
                tsl = slice(n * 128, (n + 1) * 128)
                tm = tmp[n % NTMP]
                tk = ("ftmp", n % NTMP)
                CP("act", S_bf, S, ["S"], ["S_bf"])
                b0 = (n % 2) * 2
                sc = bank(b0)
                MM(sc[:, 0:128], kiT[0][:, tsl], qdT[0][:, tsl], True, True, [("kiT", 0, n), ("qdT", 0, n)], [bkey(b0)])
                MM(sc[:, 128:256], kiT[1][:, tsl], qdT[1][:, tsl], True, True, [("kiT", 1, n), ("qdT", 1, n)], [bkey(b0)])
                TT("dve", tm["Pf"], sc[:, 0:128], cst("m_le"), ALU.mult, [bkey(b0), "consts"], [tk])
                TT("dve", tm["Pb"], sc[:, 128:256], cst("m_ge"), ALU.mult, [bkey(b0), "consts"], [tk])
                ob = bank(b0 + 1)
                ok = bkey(b0 + 1)
                MM(ob[:, 0:256], qdT[0][:, tsl], S_bf, True, False, [("qdT", 0, n), "S_bf"], [ok])
                MM(ob[:, 0:256], qdT[1][:, tsl], sb_store[:, n, :], False, False, [("qdT", 1, n), ("sb_store", n)], [ok])
                MM(ob[:, 0:256], tm["Pf"], v_tok[:, n, :], False, False, [tk, ("v_tok", n)], [ok])
                MM(ob[:, 0:256], tm["Pb"], v_tok[:, n, :], False, True, [tk, ("v_tok", n)], [ok])
                kb = 4 + n % 2
                MM(bank(kb)[:, 0:256], ktail[0][:, n, :], v_tok[:, n, :], True, True,
                   [("ktail", 0, n), ("v_tok", n)], [bkey(kb)])
                STT(S, S, dec[:, 0, n:n + 1], bank(kb)[:, 0:256], ALU.mult, ALU.add,
                    ["S", ("dec", 0, n), bkey(kb)], ["S"])
                gb = 4 + n % 2
                for kt in range(KT):
                    MM(bank(gb), hT[:, kt, tsl], wgm[:, kt, :], kt == 0, kt == KT - 1, ["wgm", ("hT", n)], [bkey(gb)])
                ACTF(tm["sig"], bank(gb), AF.Exp, [bkey(gb)], [("sig", n % NTMP)], scale=-1.0)
                ACTF(tm["sig"], tm["sig"], AF.Ln, [("sig", n % NTMP)], [("sig", n % NTMP)], bias=1.0)
                ACTF(tm["sig"], tm["sig"], AF.Exp, [("sig", n % NTMP)], [("sig", n % NTMP)], scale=-1.0)
                TT("pool", tm["G"], tm["sig"][:, 0:256], tm["sig"][:, 256:512], ALU.mult, [("sig", n % NTMP)], [("G", n % NTMP)])
                TT("dve", tm["G"], tm["G"], bank(gb)[:, 0:256], ALU.mult, [("G", n % NTMP), bkey(gb)], [("G", n % NTMP)])
                TT("pool", tm["G"], tm["G"], gnw_bc, ALU.mult, [("G", n % NTMP), "gnw_bc"], [("G", n % NTMP)])
                ssk = ("ssq", n % 2)
                ssap = small[:, 2 + n % 2:3 + n % 2]
                ACTF(tm["sig"][:, 0:256], ob[:, 0:256], AF.Square, [ok, ("G", n % NTMP)], [("sig", n % NTMP), ssk],
                     accum_out=ssap)
                rstd_inplace(ssap, 256, ssk)
                STT(mixed[:, n, h * 256:(h + 1) * 256], ob[:, 0:256], ssap, tm["G"], ALU.mult, ALU.mult,
                    [ok, ssk, ("G", n % NTMP)], [("mixed", n)])

        P.barrier()
        HG = 4
        off = 0
        gqT, off = carve(off, [HG, T], BF16)
        gkT, off = carve(off, [HG, T], BF16)
        gvT, off = carve(off, [HG, T], BF16)
        dabs, off = carve(off, [NT, 32], F32)
        g_raw, off = carve(off, [NT, 2, 8], F32)
        beta, off = carve(off, [NT, 2, 8], F32)
        gvec, off = carve(off, [64], F32)
        wsl0, off = carve(off, [KT, 512], BF16)
        wsl1, off = carve(off, [KT, 512], BF16)
        wsl = [wsl0, wsl1]
        cwT, off = carve(off, [24, 5], F32)
        gdnw_bc, off = carve(off, [128], F32)
        wdab, off = carve(off, [KT, 32], BF16)
        TMP0 = off
        xc = [None, None]
        xc[0], off = carve(off, [T + 4], BF16)
        xc[1], off = carve(off, [T + 4], BF16)
        diag, off = carve(off, [5, 128], BF16)
        ce = [None, None]
        cy = [None, None]
        for i in range(2):
            ce[i], off = carve(off, [512], F32)
            cy[i], off = carve(off, [512], F32)
        cysq, off = carve(off, [512], BF16)
        crs, off = carve(off, [512], F32)
        CONV_END = off
        off = TMP0
        DB = []
        for d_ in range(2):
            dd = {}
            for nm in ("GMB", "Wd", "decT", "u_sb", "Sg"):
                dd[nm], off = carve(off, [HG, 128], F32)
            for nm in ("Lm", "LTm", "XT", "Pp0", "Pp1", "PTp0", "PTp1", "kbg", "vbeta", "qd_tok",
                       "attnT", "ktl0", "ktl1", "qdTg", "wT_sb", "vnew", "Sg_bf", "ostage"):
                dd[nm], off = carve(off, [HG, 128], BF16)
            dd["bg"], off = carve(off, [HG], F32)
            dd["et2"], off = carve(off, [2, HG], F32)
            DB.append(dd)
        oland = []
        for i in range(2):
            a, off = carve(off, [HG, 128], BF16)
            oland.append(a)
        osum, off = carve(off, [HG, 128], F32)
        fsig, off = carve(off, [512], F32)
        fG, off = carve(off, [HG, 128], F32)
        frs, off = carve(off, [8], F32)
        SWEEP_END = off
        gdn_o = nc.dram_tensor("gdn_o_spill", [NT, 128, HG * 128], BF16, kind="Internal").ap()

        gdnw_d = dram("gdn_norm_w", [1, 128])
        gvec_d = dram("gdn_vec", [1, 32])
        cw_d = dram("gdn_conv_wT", [128, 24, 5])
        DMA("sp", gdnw_bc, gdnw_d.partition_broadcast(128), "c_gdnw", [], ["gdnw_bc"])
        DMA("sp", gvec[:, 0:32], gvec_d.partition_broadcast(128), "c_gvec", [], ["gvec"])
        DMA("sp", cwT, cw_d[:, :, :], "c_cw", [], ["cwT"])
        DMA("pool", wdab, w_in_d[:, C_DAB:C_DAB + 32].rearrange("(k p) c -> p k c", p=128), "w_wdab", [], ["wdab"])
        ACTF(gvec[:, 16:32], gvec[:, 16:32], AF.Exp, ["gvec"], ["gvec"])
        TS("dve", gvec[:, 16:32], gvec[:, 16:32], -1.0, None, ALU.mult, ALU.bypass, ["gvec"], ["gvec"])
        for n in range(NT):
            bi = n % 2
            for kt in range(KT):
                MM(bank(bi)[:, 0:32], hT[:, kt, n * 128:(n + 1) * 128], wdab[:, kt, :], kt == 0, kt == KT - 1,
                   ["wdab", ("hT", n)], [bkey(bi)])
            CP("act", dabs[:, n, :], bank(bi)[:, 0:32], [bkey(bi)], ["dabs"])
        a_view = dabs[:, :, 0:16]
        b_view = dabs[:, :, 16:32]
        g_flat = g_raw.rearrange("p n d h -> p n (d h)")
        be_flat = beta.rearrange("p n d h -> p n (d h)")
        TT("dve", g_flat, a_view, gvec[:, 0:16].unsqueeze(1).to_broadcast([128, NT, 16]), ALU.add, ["dabs", "gvec"], ["g_raw"])
        ACTF(g_flat, g_flat, AF.Exp, ["g_raw"], ["g_raw"])
        ACTF(g_flat, g_flat, AF.Ln, ["g_raw"], ["g_raw"], bias=1.0)
        TT("dve", g_flat, g_flat, gvec[:, 16:32].unsqueeze(1).to_broadcast([128, NT, 16]), ALU.mult, ["g_raw", "gvec"], ["g_raw"])
        ACTF(be_flat, b_view, AF.Exp, ["dabs"], ["beta"], scale=-1.0)
        TS("dve", be_flat, be_flat, 1.0, None, ALU.add, ALU.bypass, ["beta"], ["beta"])
        RECIP(be_flat, be_flat, ["beta"], ["beta"])
        MARK("d0")

        GSCALE = 128.0 ** -0.5
        ident_bc4 = ident_b[:].unsqueeze(1).to_broadcast([128, HG, 128])

        def bc_h(ap2):
            return ap2.unsqueeze(2).to_broadcast([128, HG, 128])

        def bc_m(ap2):
            return ap2.unsqueeze(1).to_broadcast([128, HG, 128])

        def v4(ap2):
            return ap2.rearrange("p (h d) -> p h d", h=HG)

        for grp in range(2):
            hs0 = grp * HG
            for which, c_base, dstT in ((0, C_DQ, gqT), (1, C_DK, gkT), (2, C_DV, gvT)):
                ws = wsl[which % 2]
                wk = ("wsl", which % 2)
                DMA("pool", ws, w_in_d[:, c_base + hs0 * 128:c_base + (hs0 + HG) * 128].rearrange("(k p) c -> p k c", p=128),
                    ("w_wsl", which % 2), [], [wk])
                for hh in range(HG):
                    ci = which * 8 + hs0 + hh
                    xi = (which * HG + hh) % 2
                    xcb = xc[xi]
                    xk = ("xc", xi)
                    MEMSET("pool", xcb[:, 0:2], 0.0, [xk])
                    MEMSET("pool", xcb[:, T + 2:T + 4], 0.0, [xk])
                    for k in range(5):
                        TS("dve", diag[:, k, :], cst("ident"), cwT[:, ci, k:k + 1], None, ALU.mult, ALU.bypass,
                           ["consts", "cwT"], ["diag"])
                    for tg in range(4):
                        bi = tg % 2
                        for kt in range(KT):
                            MM(bank(bi), ws[:, kt, hh * 128:(hh + 1) * 128], hT[:, kt, tg * 512:(tg + 1) * 512],
                               kt == 0, kt == KT - 1, [wk] + HT_ALL[tg * 4:tg * 4 + 4], [bkey(bi)])
                        CP("act" if tg % 2 else "dve", xcb[:, 2 + tg * 512:2 + (tg + 1) * 512], bank(bi), [bkey(bi)], [xk])
                    for tg in range(4):
                        bi = 2 + tg % 2
                        i2 = tg % 2
                        for k in range(5):
                            MM(bank(bi), diag[:, k, :], xcb[:, tg * 512 + k:tg * 512 + k + 512], k == 0, k == 4,
                               ["diag", xk], [bkey(bi)])
                        ck = ("ctmp", i2)
                        ACTF(ce[i2], bank(bi), AF.Exp, [bkey(bi)], [ck], scale=-1.0)
                        ACTF(ce[i2], ce[i2], AF.Ln, [ck], [ck], bias=1.0)
                        ACTF(ce[i2], ce[i2], AF.Exp, [ck], [ck], scale=-1.0)
                        dst = dstT[:, hh, tg * 512:(tg + 1) * 512]
                        dk = ("gT", which, hh, tg)
                        if which == 2:
                            TT("dve", dst, ce[i2], bank(bi), ALU.mult, [ck, bkey(bi)], [dk])
                        else:
                            TT("dve", cy[i2], ce[i2], bank(bi), ALU.mult, [ck, bkey(bi)], [("cy", i2)])
                            TT("pool", cysq, cy[i2], cy[i2], ALU.mult, [("cy", i2)], ["cysq"])
                            MM(bank(4), ones_b[:], cysq, True, True, ["ones_b", "cysq"], [bkey(4)])
                            ACTF(crs, bank(4), AF.Ln, [bkey(4)], ["crs"], bias=EPS)
                            ACTF(crs, crs, AF.Exp, ["crs"], ["crs"], scale=-0.5)
                            if which == 0:
                                STT(dst, cy[i2], GSCALE, crs, ALU.mult, ALU.mult, [("cy", i2), "crs"], [dk])
                            else:
                                TT("dve", dst, cy[i2], crs, ALU.mult, [("cy", i2), "crs"], [dk])
            MARK("d1")
            P.barrier()
            DMA("pool", wsl[0], w_in_d[:, C_DZ + hs0 * 128:C_DZ + (hs0 + HG) * 128].rearrange("(k p) c -> p k c", p=128),
                ("w_wsl", 0), [], [("wsl", 0)])
            DMA("pool", wsl[1], w_in_d[:, C_MB + hs0 * 128:C_MB + (hs0 + HG) * 128].rearrange("(k p) c -> p k c", p=128),
                ("w_wsl", 1), [], [("wsl", 1)])

            def gT_keys(which, n):
                return [("gT", which, hh, n // 4) for hh in range(HG)]

            stored = set()
            esc_all = [dabs[:, 0:8, :].rearrange("p a b -> p (a b)").rearrange("p (k n h) -> p k n h", k=4, n=NT),
                       dabs[:, 8:16, :].rearrange("p a b -> p (a b)").rearrange("p (k n h) -> p k n h", k=4, n=NT)]
            for d_ in range(2):
                Mc_ = cst("b_le") if d_ == 0 else cst("b_ge")
                Ms_ = cst("b_gt") if d_ == 0 else cst("b_lt")
                for ki, mk in enumerate((Mc_, Ms_, cst("csel0"), cst("csel1"))):
                    MM(bank(d_)[:, ki * 64:(ki + 1) * 64].rearrange("p (n h) -> p n h", n=NT), mk,
                       g_raw[:, :, d_, hs0:hs0 + HG], True, True, ["consts", "g_raw"], [bkey(d_)])
                ACTF(esc_all[d_].rearrange("p k n h -> p (k n h)"), bank(d_)[:, 0:256], AF.Exp, [bkey(d_)], [("esc_all", d_), "dabs"])

            gb0 = ptb[:, 0:512]
            gb1 = ptb[:, 512:1024]
            xbank = ptb[:, 1024:2048].bitcast(F32)
            XK = ("pb", 7)

            def gdn_tile(d_, n):
                B = DB[d_]
                dk = lambda nm: (nm, d_)
                Mc = cst("b_le") if d_ == 0 else cst("b_ge")
                Ms = cst("b_gt") if d_ == 0 else cst("b_lt")
                bg = B["bg"]
                GMB, Wd, decT, Lm, LTm, XT = B["GMB"], B["Wd"], B["decT"], B["Lm"], B["LTm"], B["XT"]
                kbg, vbeta, qd_tok = B["kbg"], B["vbeta"], B["qd_tok"]
                Ppd = [B["Pp0"], B["Pp1"]]
                PTpd = [B["PTp0"], B["PTp1"]]
                pa, pb_ = (0, 1) if d_ == 0 else (2, 3)
                tsl = slice(n * 128, (n + 1) * 128)
                gv = g_raw[:, n, d_, hs0:hs0 + HG]
                bv = beta[:, n, d_, hs0:hs0 + HG]
                EA = esc_all[d_]
                e_cum, e_tail = EA[:, 0, n, :], EA[:, 1, n, :]
                second = n in stored
                if second:
                    par = n % 2
                    DMA("sp", oland[par].rearrange("p h d -> p (h d)"), gdn_o[n], ("oland", par), [("o_dram", n)], [("oland", par)])
                TT("dve", bg, bv, e_cum, ALU.mult, ["beta", ("esc_all", d_)], [dk("bg")])
                for hh in range(HG):
                    TR(gb0[:, hh * 128:(hh + 1) * 128], gkT[:, hh, tsl], ident_b[:], gT_keys(1, n) + ["ident_b"], [("pbb", 0)])
                for hh in range(HG):
                    TR(gb1[:, hh * 128:(hh + 1) * 128], gvT[:, hh, tsl], ident_b[:], gT_keys(2, n) + ["ident_b"], [("pbb", 0)])
                TT("dve", kbg, v4(gb0), bc_h(bg), ALU.mult, [("pbb", 0), dk("bg")], [dk("kbg")])
                for c_ in range(2):
                    TT("dve", B["et2"][:, c_, :], e_tail, cst("csel%d" % c_)[:, 0:HG], ALU.mult, [("esc_all", d_), "consts"], [dk("et2")])
                    TT("dve", B["ktl%d" % c_], v4(gb0), bc_h(B["et2"][:, c_, :]), ALU.mult, [("pbb", 0), dk("et2")], [dk("ktl%d" % c_)])
                TT("dve", vbeta, v4(gb1), bc_h(bv), ALU.mult, [("pbb", 0), "beta"], [dk("vbeta")])
                for hh in range(HG):
                    TR(gb0[:, hh * 128:(hh + 1) * 128], gqT[:, hh, tsl], ident_b[:], gT_keys(0, n) + ["ident_b"], [("pbb", 0)])
                TT("dve", qd_tok, v4(gb0), bc_h(e_cum), ALU.mult, [("pbb", 0), ("esc_all", d_)], [dk("qd_tok")])
                for hh in range(HG):
                    TR(gb1[:, hh * 128:(hh + 1) * 128], qd_tok[:, hh, :], ident_b[:], [dk("qd_tok"), "ident_b"], [("pbb", 0)])
                CP("act", B["qdTg"], v4(gb1), [("pbb", 0)], [dk("qdTg")])
                TT("pool", GMB, bc_h(gv), bc_m(Ms), ALU.mult, ["g_raw", "consts"], [dk("GMB")])
                MM(bank(pb_), Mc, GMB.rearrange("p h s -> p (h s)"), True, True, ["consts", dk("GMB")], [bkey(pb_)])
                ACTF(Wd.rearrange("p h s -> p (h s)"), bank(pb_), AF.Exp, [bkey(pb_)], [dk("Wd")])
                TT("pool", GMB, bc_h(bv), bc_m(Ms), ALU.mult, ["beta", "consts"], [dk("GMB")])
                TT("pool", Wd, Wd, GMB, ALU.mult, [dk("Wd"), dk("GMB")], [dk("Wd")])
                TT("pool", GMB, bc_h(gv), bc_m(Mc), ALU.mult, ["g_raw", "consts"], [dk("GMB")])
                MM(bank(pa), Ms, GMB.rearrange("p h s -> p (h s)"), True, True, ["consts", dk("GMB")], [bkey(pa)])
                ACTF(decT.rearrange("p h s -> p (h s)"), bank(pa), AF.Exp, [bkey(pa)], [dk("decT")])
                TT("pool", decT, decT, bc_m(Mc), ALU.mult, [dk("decT"), "consts"], [dk("decT")])
                for hh in range(HG):
                    MM(bank(pb_)[:, hh * 128:(hh + 1) * 128], gkT[:, hh, tsl], gkT[:, hh, tsl], True, True,
                       gT_keys(1, n), [bkey(pb_)])
                TT("dve", Lm, v4(bank(pb_)), Wd, ALU.mult, [bkey(pb_), dk("Wd")], [dk("Lm")])
                for hh in range(HG):
                    MM(bank(pa)[:, hh * 128:(hh + 1) * 128], gkT[:, hh, tsl], gqT[:, hh, tsl], True, True,
                       gT_keys(1, n) + gT_keys(0, n), [bkey(pa)])
                TT("dve", B["attnT"], v4(bank(pa)), decT, ALU.mult, [bkey(pa), dk("decT")], [dk("attnT")])
                for hh in range(HG):
                    TR(gb0[:, hh * 128:(hh + 1) * 128], Lm[:, hh, :], ident_b[:], [dk("Lm"), "ident_b"], [("pbb", 0)])
                CP("act", LTm, v4(gb0), [("pbb", 0)], [dk("LTm")])
                TT("dve", XT, ident_bc4, v4(gb0), ALU.subtract, ["ident_b", ("pbb", 0)], [dk("XT")])
                Pc, PTc = Lm, LTm
                pck, ptk_ = dk("Lm"), dk("LTm")
                for it in range(5):
                    Pn, PTn = Ppd[it % 2], PTpd[it % 2]
                    pnk, ptnk = ("Pp", it % 2, d_), ("PTp", it % 2, d_)
                    for hh in range(HG):
                        MM(bank(pb_)[:, hh * 128:(hh + 1) * 128], PTc[:, hh, :], Pc[:, hh, :], True, True, [pck, ptk_], [bkey(pb_)])
                    CP("act", Pn, v4(bank(pb_)), [bkey(pb_)], [pnk])
                    if it < 4:
                        for hh in range(HG):
                            MM(bank(pa)[:, hh * 128:(hh + 1) * 128], Pc[:, hh, :], PTc[:, hh, :], True, True, [pck, ptk_], [bkey(pa)])
                        CP("dve", PTn, v4(bank(pa)), [bkey(pa)], [ptnk])
                    for hh in range(HG):
                        MM(xbank[:, hh * 128:(hh + 1) * 128], Pn[:, hh, :], XT[:, hh, :], True, True, [pnk, dk("XT")], [XK])
                    TT("dve", XT, XT, v4(xbank), ALU.add, [dk("XT"), XK], [dk("XT")])
                    Pc, PTc, pck, ptk_ = Pn, PTn, pnk, ptnk
                for hh in range(HG):
                    MM(bank(pa)[:, hh * 128:(hh + 1) * 128], XT[:, hh, :], vbeta[:, hh, :], True, True, [dk("XT"), dk("vbeta")], [bkey(pa)])
                CP("act", B["u_sb"], v4(bank(pa)), [bkey(pa)], [dk("u_sb")])
                for hh in range(HG):
                    MM(bank(pb_)[:, hh * 128:(hh + 1) * 128], kbg[:, hh, :], XT[:, hh, :], True, True, [dk("kbg"), dk("XT")], [bkey(pb_)])
                CP("dve", B["wT_sb"], v4(bank(pb_)), [bkey(pb_)], [dk("wT_sb")])
                sb0 = 4
                Sg, Sg_bf, vnew = B["Sg"], B["Sg_bf"], B["vnew"]
                chunks = (0, 1) if d_ == 0 else (1, 0)
                for c in chunks:
                    sl = slice(c * 64, c * 64 + 64)
                    for hh in range(HG):
                        MM(bank(sb0)[:, hh * 128:(hh + 1) * 128], B["wT_sb"][:, hh, :], Sg_bf[:, hh, :], True, True,
                           [dk("wT_sb"), dk("Sg_bf")], [bkey(sb0)])
                    TT("dve", vnew[sl], B["u_sb"][sl], v4(bank(sb0))[sl], ALU.subtract, [dk("u_sb"), bkey(sb0)], [dk("vnew")])
                    for hh in range(HG):
                        MM(bank(sb0 + 1)[:, hh * 128:(hh + 1) * 128], B["qdTg"][:, hh, :], Sg_bf[:, hh, :], True, False,
                           [dk("qdTg"), dk("Sg_bf")], [bkey(sb0 + 1)])
                        MM(bank(sb0 + 1)[:, hh * 128:(hh + 1) * 128], B["attnT"][:, hh, :], vnew[:, hh, :], False, True,
                           [dk("attnT"), dk("vnew")], [bkey(sb0 + 1)])
                    for hh in range(HG):
                        MM(bank(sb0)[:, hh * 128:(hh + 1) * 128], B["ktl%d" % c][:, hh, :], vnew[:, hh, :], True, True,
                           [dk("ktl%d" % c), dk("vnew")], [bkey(sb0)])
                    TT("pool", Sg, Sg, bc_h(EA[:, 2 + c, n, :]), ALU.mult, [dk("Sg"), ("esc_all", d_)], [dk("Sg")])
                    TT("dve", Sg, Sg, v4(bank(sb0)), ALU.add, [dk("Sg"), bkey(sb0)], [dk("Sg")])
                    CP("act", Sg_bf, Sg, [dk("Sg")], [dk("Sg_bf")])
                    if not second:
                        CP("act", B["ostage"][sl], v4(bank(sb0 + 1))[sl], [bkey(sb0 + 1)], [dk("ostage")])
                    else:
                        TT("dve", osum[sl], v4(bank(sb0 + 1))[sl], oland[n % 2][sl], ALU.add,
                           [bkey(sb0 + 1), ("oland", n % 2)], ["osum"])
                if not second:
                    stored.add(n)
                    DMA("sp", gdn_o[n], B["ostage"].rearrange("p h d -> p (h d)"), ("ost", d_), [dk("ostage")], [("o_dram", n)])
                    return
                osq = fsig.rearrange("p (h d) -> p h d", h=HG)
                TT("pool", osq, osum, osum, ALU.mult, ["osum"], ["fsig"])
                P.op("dve", lambda e: e.tensor_reduce(out=frs[:, 0:HG], in_=osq, axis=AX.X, op=ALU.add), ["fsig"], ["frs"], cost=600.0)
                rstd_inplace(frs[:, 0:HG], 128, "frs")
                for half, ws in enumerate(wsl):
                    for kt in range(KT):
                        MM(bank(half), hT[:, kt, tsl], ws[:, kt, :], kt == 0, kt == KT - 1,
                           [("wsl", half), ("hT", n)], [bkey(half)])
                fGf = fG.rearrange("p h d -> p (h d)")
                for half in range(2):
                    ACTF(fsig, bank(half), AF.Exp, [bkey(half)], ["fsig"], scale=-1.0)
                    ACTF(fsig, fsig, AF.Ln, ["fsig"], ["fsig"], bias=1.0)
                    ACTF(fsig, fsig, AF.Exp, ["fsig"], ["fsig"], scale=-1.0)
                    if half == 0:
                        TT("dve", fGf, fsig, bank(0), ALU.mult, ["fsig", bkey(0)], ["fG"])
                    else:
                        TT("pool", fGf, fGf, fsig, ALU.mult, ["fsig", "fG"], ["fG"])
                TT("pool", fG, fG, bc_m(gdnw_bc), ALU.mult, ["fG", "gdnw_bc"], ["fG"])
                TT("pool", osum, osum, bc_h(frs[:, 0:HG]), ALU.mult, ["osum", "frs"], ["osum"])
                TT("pool", osum, osum, fG, ALU.mult, ["osum", "fG"], ["osum"])
                mslice = mixed[:, n, hs0 * 128:(hs0 + HG) * 128].rearrange("p (h d) -> p h d", h=HG)
                TT("dve", mslice, mslice, osum, ALU.add, ["osum", ("mixed", n)], [("mixed", n)])

            for d_ in range(2):
                MEMSET("pool", DB[d_]["vnew"], 0.0, [("vnew", d_)])
                MEMSET("dve", DB[d_]["Sg"], 0.0, [("Sg", d_)])
                CP("act", DB[d_]["Sg_bf"], DB[d_]["Sg"], [("Sg", d_)], [("Sg_bf", d_)])
            for i in range(NT):
                gdn_tile(0, i)
                gdn_tile(1, NT - 1 - i)
            MARK("d2")
            P.barrier()

        P.barrier()
        wout_d = dram("w_out", [D, D])
        off = 0
        x1, off = carve(off, [NT, D], F32)
        X1_END = off
        mT, off = carve(off, [KT, T], BF16)
        wout, off = carve(off, [KT, D], BF16)
        hn2 = [None, None]
        hn2[0], off = carve(off, [D], BF16)
        hn2[1], off = carve(off, [D], BF16)
        junk, off = carve(off, [D], BF16)
        n2_bc, off = carve(off, [D], F32)
        DMA("sp", n2_bc, n2_d.partition_broadcast(128), "c_n2", [], ["n2_bc"])
        DMA("pool", wout, wout_d.rearrange("(k p) c -> p k c", p=128), "w_wout", [], ["wout"])
        for n in range(NT):
            b = n % 2
            for kt in range(KT):
                TR(bbank(b)[:, kt * 128:(kt + 1) * 128], mixed[:, n, kt * 128:(kt + 1) * 128], ident_b[:],
                   [("mixed", n), "ident_b"], [("pbb", b)])
            CP("act", mT[:, :, n * 128:(n + 1) * 128], bbank(b).rearrange("p (k t) -> p k t", k=KT), [("pbb", b)], [("mT", n)])
        for n in range(NT):
            tsl = slice(n * 128, (n + 1) * 128)
            DMA("sp", x1[:, n, :], x_d[tsl, :], ("x1ld", n % 4), [], [("x1", n)])
            pp = pt[n % 2]
            for half in range(2):
                for kt in range(KT):
                    MM(pp[:, half * 512:(half + 1) * 512], mT[:, kt, tsl], wout[:, kt, half * 512:(half + 1) * 512],
                       kt == 0, kt == KT - 1, [("mT", n), "wout"], [bkey((n % 2) * 2 + half)])
            TT("dve", x1[:, n, :], x1[:, n, :], pp[:, :], ALU.add, [("x1", n), bkey((n % 2) * 2), bkey((n % 2) * 2 + 1)], [("x1", n)])
            b = n % 2
            ssap = small[:, 8 + b:9 + b]
            ssk = ("ss2", b)
            ACTF(junk, x1[:, n, :], AF.Square, [("x1", n)], ["junk", ssk], accum_out=ssap)
            rstd_inplace(ssap, D, ssk)
            STT(mixed[:, n, :], x1[:, n, :], ssap, n2_bc, ALU.mult, ALU.mult, [("x1", n), ssk, "n2_bc"], [("mixed", n)])
            for kt in range(KT):
                TR(bbank(b)[:, kt * 128:(kt + 1) * 128], mixed[:, n, kt * 128:(kt + 1) * 128], ident_b[:],
                   [("mixed", n), "ident_b"], [("pbb", b)])
            CP("act", hT[:, :, tsl], bbank(b).rearrange("p (k t) -> p k t", k=KT), [("pbb", b)], [("hT", n)])
        MARK("e0")

        P.barrier()
        NB = 64
        wr_d = dram("moe_wr", [D, 36])
        wgu0_d = dram("moe_wgu0", [4096, 2048])
        wgu1_d = dram("moe_wgu1", [4096, 2048])
        wdr_d = dram("moe_wdr", [4096, 2048])
        xb_d = nc.dram_tensor("moe_xb", [NB * 128, D], BF16, kind="Internal").ap()
        yb_d = nc.dram_tensor("moe_yb", [NB * 128, D], F32, kind="Internal").ap()
        off = X1_END
        stg = []
        for i in range(3):
            a, off = carve(off, [2048], F32)
            stg.append(a)
        wgu_bf = []
        wd_bf = []
        for i in range(2):
            a, off = carve(off, [KT, 512], BF16)
            wgu_bf.append(a)
            a, off = carve(off, [2, D], BF16)
            wd_bf.append(a)
        wr, off = carve(off, [KT, 36], BF16)
        lg, off = carve(off, [NT, 36], F32)
        oh1, off = carve(off, [NT, 32], F32)
        oh2, off = carve(off, [NT, 32], F32)
        msk, off = carve(off, [NT, 32], F32)
        rank, off = carve(off, [NT, 32], F32)
        tmp3, off = carve(off, [NT, 32], F32)
        gtmp, off = carve(off, [NT, 4], F32)
        ohg, off = carve(off, [NT, 4], F32)
        rv, off = carve(off, [8, NT], F32)
        mcum, off = carve(off, [32], F32)
        cnt, off = carve(off, [32], F32)
        padded, off = carve(off, [32], F32)
        ends, off = carve(off, [32], F32)
        pstart, off = carve(off, [32], F32)
        ebf, off = carve(off, [NB], F32)
        widx_f, off = carve(off, [NB], F32)
        widx, off = carve(off, [NB], I32)
        dest_f, off = carve(off, [2, NT], F32)
        dest_i, off = carve(off, [2, NT], I32)
        MOE_END = off
        cmpb = stg[0].rearrange("p (b e) -> p b e", b=NB)
        cmpj = stg[1][:, 0:512].rearrange("p (e j) -> p e j", e=32)
        ht32 = hT[:].rearrange("p k t -> p (k t)").bitcast(F32)
        HTCAP = 32 * 1024
        hoff = 0
        xg, xgT, sil, hid_bf, hidT, ysb = [], [], [], [], [], []
        for i in range(2):
            a, hoff = carve(hoff, [D], BF16, ht32, HTCAP); xg.append(a)
            a, hoff = carve(hoff, [KT, 128], BF16, ht32, HTCAP); xgT.append(a)
            a, hoff = carve(hoff, [256], F32, ht32, HTCAP); sil.append(a)
            a, hoff = carve(hoff, [256], BF16, ht32, HTCAP); hid_bf.append(a)
            a, hoff = carve(hoff, [2, 128], BF16, ht32, HTCAP); hidT.append(a)
            a, hoff = carve(hoff, [D], F32, ht32, HTCAP); ysb.append(a)
        mix32 = mixed[:].rearrange("p n d -> p (n d)").bitcast(F32)
        MIXCAP = 32 * 1024
        moff = 0
        yg = []
        for i in range(2):
            a, moff = carve(moff, [D], F32, mix32, MIXCAP); yg.append(a)
        stgB = []
        for i in range(3):
            a, moff = carve(moff, [2048], F32, mix32, MIXCAP); stgB.append(a)

        DMA("pool", wr, wr_d.rearrange("(k p) c -> p k c", p=128), "w_wr", [], ["wr"])
        for n in range(NT):
            bi = n % 2
            for kt in range(KT):
                MM(bank(bi)[:, 0:36], hT[:, kt, n * 128:(n + 1) * 128], wr[:, kt, :], kt == 0, kt == KT - 1,
                   ["wr", ("hT", n)], [bkey(bi)])
            CP("act", lg[:, n, :], bank(bi)[:, 0:36], [bkey(bi)], ["lg"])
        BIG = 10000.0
        glv = lg[:, :, 0:4]
        elv = lg[:, :, 4:36]

        def RED(out, in_, op, R, W):
            P.op("dve", lambda e: e.tensor_reduce(out=out, in_=in_, axis=AX.X, op=op), R, W, cost=100.0 + _fsz(in_) * 1.0)

        def bcn(ap2, k):
            return ap2.unsqueeze(2).to_broadcast([128, NT, k])

        gmax, gsum, m1, m2, w1, w2 = (rv[:, i, :] for i in range(6))
        RED(gmax, glv, ALU.max, ["lg"], ["rv"])
        TT("dve", ohg, glv, bcn(gmax, 4), ALU.is_equal, ["lg", "rv"], ["ohg"])
        TT("dve", gtmp, glv, bcn(gmax, 4), ALU.subtract, ["lg", "rv"], ["gtmp"])
        ACTF(gtmp, gtmp, AF.Exp, ["gtmp"], ["gtmp"])
        RED(gsum, gtmp, ALU.add, ["gtmp"], ["rv"])
        RECIP(gsum, gsum, ["rv"], ["rv"])
        TS("dve", ohg, ohg, BIG, -BIG, ALU.mult, ALU.add, ["ohg"], ["ohg"])
        TT("dve", msk.rearrange("p n (g e) -> p n g e", g=4), elv.rearrange("p n (g e) -> p n g e", g=4),
           ohg.unsqueeze(3).to_broadcast([128, NT, 4, 8]), ALU.add, ["lg", "ohg"], ["msk"])
        RED(m1, msk, ALU.max, ["msk"], ["rv"])
        TT("dve", oh1, msk, bcn(m1, 32), ALU.is_equal, ["msk", "rv"], ["oh1"])
        STT(msk, oh1, -BIG, msk, ALU.mult, ALU.add, ["oh1", "msk"], ["msk"])
        RED(m2, msk, ALU.max, ["msk"], ["rv"])
        TT("dve", oh2, msk, bcn(m2, 32), ALU.is_equal, ["msk", "rv"], ["oh2"])
        TT("dve", w2, m2, m1, ALU.subtract, ["rv"], ["rv"])
        ACTF(w2, w2, AF.Exp, ["rv"], ["rv"])
        TS("dve", w1, w2, 1.0, None, ALU.add, ALU.bypass, ["rv"], ["rv"])
        RECIP(w1, w1, ["rv"], ["rv"])
        TT("dve", w1, w1, gsum, ALU.mult, ["rv"], ["rv"])
        TT("dve", w2, w2, w1, ALU.mult, ["rv"], ["rv"])
        TT("dve", msk, oh1, oh2, ALU.add, ["oh1", "oh2", "msk"], ["msk"])
        MEMSET("dve", mcum, 0.0, ["mcum"])
        for n in range(NT):
            bi = n % 2
            MM(bank(bi)[:, 0:32], cst("m_lt"), msk[:, n, :], True, False, ["consts", "msk"], [bkey(bi)])
            MM(bank(bi)[:, 0:32], cst("ones"), mcum, False, True, ["consts", "mcum"], [bkey(bi)])
            CP("act", rank[:, n, :], bank(bi)[:, 0:32], [bkey(bi)], ["rank"])
            TT("dve", mcum, mcum, msk[:, n, :], ALU.add, ["mcum", "msk"], ["mcum"])
        MM(bank(0)[:, 0:32], cst("ones"), mcum, True, True, ["consts", "mcum"], [bkey(0)])
        CP("act", cnt, bank(0)[:, 0:32], [bkey(0)], ["cnt"])
        TT("dve", cmpj, cnt.unsqueeze(2).to_broadcast([128, 32, 16]),
           cst("bvals")[:, 0:16].unsqueeze(1).to_broadcast([128, 32, 16]), ALU.is_gt, ["cnt", "consts"], [("stg", 1)])
        RED(padded, cmpj, ALU.add, [("stg", 1)], ["padded"])
        TS("dve", padded, padded, 128.0, None, ALU.mult, ALU.bypass, ["padded"], ["padded"])
        P.op("dve", lambda e: e.tensor_tensor_scan(out=ends, data0=cst("ones")[:, 0:32], data1=padded, initial=0.0,
                                                  op0=ALU.mult, op1=ALU.add), ["consts", "padded"], ["ends"], cost=300.0)
        TT("dve", pstart, ends, padded, ALU.subtract, ["ends", "padded"], ["pstart"])
        TT("dve", rank, rank, pstart.unsqueeze(1).to_broadcast([128, NT, 32]), ALU.add, ["rank", "pstart"], ["rank"])
        for k, ohk in ((0, oh1), (1, oh2)):
            TT("dve", tmp3, ohk, rank, ALU.mult, ["oh1", "oh2", "rank"], ["tmp3"])
            RED(dest_f[:, k, :], tmp3, ALU.add, ["tmp3"], ["dest_f"])
        CP("dve", dest_i, dest_f, ["dest_f"], ["dest_i"])
        TT("dve", cmpb, ends.unsqueeze(1).to_broadcast([128, NB, 32]),
           cst("bvals")[:, 0:NB].unsqueeze(2).to_broadcast([128, NB, 32]), ALU.is_le, ["ends", "consts"], [("stg", 0)])
        RED(ebf, cmpb, ALU.add, [("stg", 0)], ["ebf"])
        STT(widx_f, ebf, 128.0, cst("pidx")[:, 0:NB], ALU.mult, ALU.add, ["ebf", "consts"], ["widx_f"])
        CP("dve", widx, widx_f, ["widx_f"], ["widx"])
        MARK("e1")

        IOA = bass.IndirectOffsetOnAxis
        regs = {}

        def _pool_init(e):
            regs["bc"] = e.alloc_register("moe_bc")
            e.reg_mov(regs["bc"], 4095)
        P.pool_init = _pool_init
        XB_KEYS = []
        zt, off = carve(off, [D], BF16)
        MEMSET("pool", zt, 0.0, ["zt"])
        DMA("sp", xb_d.rearrange("(p r) d -> p r d", p=128), zt.unsqueeze(1).to_broadcast([128, NB, D]), "xbz", ["zt"], ["xb0"])
        for n in range(NT):
            for k in range(2):
                idx_ap = dest_i[:, k, n:n + 1]
                src_ap = mixed[:, n, :]
                P.dma("pool", lambda e, idx_ap=idx_ap, src_ap=src_ap: e.indirect_dma_start(
                    out=xb_d[:, :], out_offset=IOA(ap=idx_ap, axis=0), in_=src_ap, in_offset=None),
                    ("sc", (2 * n + k) % 4), [("mixed", n), "dest_i", "xb0"], [("xb", n, k)], nbytes=256 * 1024)
                XB_KEYS.append(("xb", n, k))

        def gather_w(dst, src_d, b, skey, extra):
            idx_ap = widx[:, b:b + 1]
            P.dma("pool", lambda e: e.indirect_dma_start(
                out=dst, out_offset=None, in_=src_d[:, :], in_offset=IOA(ap=idx_ap, axis=0),
                bounds_check=regs["bc"], oob_is_err=False),
                skey, ["widx"] + extra, [skey], nbytes=1 << 20)

        YB_KEYS = []
        for b in range(NB):
            s = b % 2
            sset = stg if b % 2 == 0 else stgB
            so = 0 if b % 2 == 0 else 3
            extra = [] if b % 2 == 0 else XB_KEYS
            gather_w(sset[0], wgu0_d, b, ("stg", so + 0), extra)
            gather_w(sset[1], wgu1_d, b, ("stg", so + 1), extra)
            gather_w(sset[2], wdr_d, b, ("stg", so + 2), extra)
            CP("act", wgu_bf[s][:, 0:4, :], sset[0].rearrange("p (k c) -> p k c", k=4), [("stg", so + 0)], [("wgu_bf", s, 0)])
            CP("dve", wgu_bf[s][:, 4:8, :], sset[1].rearrange("p (k c) -> p k c", k=4), [("stg", so + 1)], [("wgu_bf", s, 1)])
            CP("act" if b % 4 < 2 else "dve", wd_bf[s], sset[2].rearrange("p (k c) -> p k c", k=2), [("stg", so + 2)], [("wd_bf", s)])
            DMA("sp", xg[s], xb_d[b * 128:(b + 1) * 128, :], ("xg", s), XB_KEYS, [("xg", s)])
            for kt in range(KT):
                TR(bbank(s)[:, kt * 128:(kt + 1) * 128], xg[s][:, kt * 128:(kt + 1) * 128], ident_b[:],
                   [("xg", s), "ident_b"], [("pbb", s)])
            CP("act", xgT[s], bbank(s).rearrange("p (k t) -> p k t", k=KT), [("pbb", s)], [("xgT", s)])
            hb = bank(s)
            for kt in range(KT):
                MM(hb, xgT[s][:, kt, :], wgu_bf[s][:, kt, :], kt == 0, kt == KT - 1,
                   [("xgT", s), ("wgu_bf", s, 0), ("wgu_bf", s, 1)], [bkey(s)])
            ACTF(sil[s], hb[:, 0:256], AF.Silu, [bkey(s)], [("sil", s)])
            TT("dve", hid_bf[s], sil[s], hb[:, 256:512], ALU.mult, [("sil", s), bkey(s)], [("hid_bf", s)])
            for ft in range(2):
                TR(bbank(s)[:, ft * 128:(ft + 1) * 128], hid_bf[s][:, ft * 128:(ft + 1) * 128], ident_b[:],
                   [("hid_bf", s), "ident_b"], [("pbb", s)])
            CP("act", hidT[s], bbank(s)[:, 0:256].rearrange("p (k t) -> p k t", k=2), [("pbb", s)], [("hidT", s)])
            yp = pt[1 + s]
            for half in range(2):
                for ft in range(2):
                    MM(yp[:, half * 512:(half + 1) * 512], hidT[s][:, ft, :], wd_bf[s][:, ft, half * 512:(half + 1) * 512],
                       ft == 0, ft == 1, [("hidT", s), ("wd_bf", s)], [bkey(2 + 2 * s + half)])
            CP("act" if b % 2 else "dve", ysb[s], yp[:, :], [bkey(2 + 2 * s), bkey(3 + 2 * s)], [("ysb", s)])
            DMA("sp", yb_d[b * 128:(b + 1) * 128, :], ysb[s], ("yst", s), [("ysb", s)], [("yb", b)])
            YB_KEYS.append(("yb", b))
        MARK("e2")
        ygs = list(yg)
        for sb_ in stgB:
            ygs.append(sb_[:, 0:1024])
            ygs.append(sb_[:, 1024:2048])
        for n in range(NT):
            for k in range(2):
                s = (2 * n + k) % len(ygs)
                idx_ap = dest_i[:, k, n:n + 1]
                dst = ygs[s]
                P.dma("pool", lambda e, idx_ap=idx_ap, dst=dst: e.indirect_dma_start(
                    out=dst, out_offset=None, in_=yb_d[:, :], in_offset=IOA(ap=idx_ap, axis=0)),
                    ("yg", s), YB_KEYS + ["dest_i"], [("yg", s)], nbytes=512 * 1024)
                wk = rv[:, 4 + k, n:n + 1]
                STT(x1[:, n, :], ygs[s], wk, x1[:, n, :], ALU.mult, ALU.add, [("yg", s), "rv", ("x1", n)], [("x1", n)])

        P.barrier()
        off = X1_END
        nf_bc, off = carve(off, [D], F32)
        ob = [None, None]
        ob[0], off = carve(off, [D], F32)
        ob[1], off = carve(off, [D], F32)
        junk2, off = carve(off, [D], BF16)
        DMA("sp", nf_bc, nf_d.partition_broadcast(128), "c_nf", [], ["nf_bc"])
        for n in range(NT):
            b = n % 2
            ssap = small[:, 12 + b:13 + b]
            ssk = ("ss3", b)
            ACTF(junk2, x1[:, n, :], AF.Square, [("x1", n)], ["junk2", ssk], accum_out=ssap)
            rstd_inplace(ssap, D, ssk)
            STT(ob[b], x1[:, n, :], ssap, nf_bc, ALU.mult, ALU.mult, [("x1", n), ssk, "nf_bc"], [("ob", b)])
            DMA("sp", out_d[n * 128:(n + 1) * 128, :], ob[b], ("out_st", b), [("ob", b)], [("out", n)])
        if not dbg:
            P.wait_all("sp", [("out", n) for n in range(NT)])
        if dbg:
            P.enabled = True
            P.barrier()
            for n in range(NT):
                DMA("sp", dbg_d[n * 128:(n + 1) * 128, :], x1[:, n, :], ("dbg_out", n % 2), [("x1", n)], [("dbg", n)])
            P.wait_all("sp", [("dbg", n) for n in range(NT)] + [("out", n) for n in range(NT)])
        P.emit()
    return nc


def make_in_maps(inputs, n_cores=8):
    f = lambda k: np.asarray(inputs[k], np.float32)
    x = f("x")
    _gu = np.concatenate([f("moe_w_gate")[0], f("moe_w_up")[0]], axis=2).reshape(32, 8, 128, 512).transpose(0, 2, 1, 3)
    shared = {
        "norm1_w": f("norm1_w").reshape(1, D),
        "norm2_w": f("norm2_w").reshape(1, D),
        "norm_f_w": f("norm_f_w").reshape(1, D),
        "consts": CONST_ARR,
        "w_in": np.ascontiguousarray(f("w_in")[0]),
        "gla_w2b_f": np.ascontiguousarray(np.concatenate([f("gla_gate_w2_fwd")[0], f("gla_gate_b_fwd")], axis=0)),
        "gla_w2b_b": np.ascontiguousarray(np.concatenate([f("gla_gate_w2_bwd")[0], f("gla_gate_b_bwd")], axis=0)),
        "gla_norm_w": f("gla_norm_w").reshape(1, 256),
        "w_out": np.ascontiguousarray(f("w_out")[0]),
        "moe_wr": np.ascontiguousarray(np.concatenate([f("moe_w_group")[0], f("moe_w_router")[0]], axis=1)),
        "moe_wgu0": _gu[:, :, 0:4, :].reshape(4096, 2048).copy(),
        "moe_wgu1": _gu[:, :, 4:8, :].reshape(4096, 2048).copy(),
        "moe_wdr": np.ascontiguousarray(f("moe_w_down")[0].reshape(32, 2, 128, 1024).transpose(0, 2, 1, 3)).reshape(4096, 2048),
        "gdn_norm_w": f("gdn_norm_w").reshape(1, 128),
        "gdn_vec": np.ascontiguousarray(np.concatenate([f("gdn_dt_bias_fwd")[0], f("gdn_dt_bias_bwd")[0],
                                                        f("gdn_a_log_fwd")[0], f("gdn_a_log_bwd")[0]]).reshape(1, 32)),
        "gdn_conv_wT": np.ascontiguousarray(f("gdn_conv_w")[0].T.reshape(24, 128, 5).transpose(1, 0, 2)),
    }
    maps = []
    for c in range(n_cores):
        m = dict(shared)
        m["x"] = np.ascontiguousarray(x[c])
        maps.append(m)
    return maps


def kernel(**inputs):
    nc = build()
    in_maps = make_in_maps(inputs)
    res = run_bass_kernel_spmd(nc, in_maps, core_ids=list(range(8)))
    out = np.stack([np.asarray(r["out"]) for r in res.results], axis=0)
    return out.astype(np.float32)
```

```python
import contextlib
import heapq
import numpy as np
import concourse.bass as bass
import concourse.mybir as mybir
from concourse.bass_utils import run_bass_kernel_spmd

F32 = mybir.dt.float32
BF16 = mybir.dt.bfloat16
I32 = mybir.dt.int32
AF = mybir.ActivationFunctionType
ALU = mybir.AluOpType
AX = mybir.AxisListType

T = 2048
D = 1024
NT = T // 128
KT = D // 128
EPS = 1e-6
SAME_ENGINE_SYNC = True
EPOCH = 20000
SYNC_NS = 120.0
DMA_LAT_NS = 2200.0


class Prog:
    ENGS = ("pe", "act", "dve", "pool", "sp")

    def __init__(self, nc, stack):
        self.nc = nc
        self.stack = stack
        self.streams = {e: [] for e in self.ENGS}
        self.count = {e: 0 for e in self.ENGS}
        self.esems = {e: [] for e in self.ENGS}
        self.known = {e: {} for e in self.ENGS}
        self.last_write = {}
        self.readers = {}
        self.dma_sems = {}
        self.dma_vals = {}
        self.dma_last = {}
        self.enabled = True
        self.seg = []
        self.ticks = {}
        self.nops = 0
        self.seg_base = 0
        self.pool_init = None

    def _new_sem(self, name):
        return self.stack.enter_context(self.nc.semaphore(name))

    @staticmethod
    def _psum_fix(reads, writes):
        r2, w2 = [], list(writes)
        for k in reads:
            if isinstance(k, tuple) and k[0] in ("pb", "pbb"):
                if k not in w2:
                    w2.append(k)
            else:
                r2.append(k)
        return r2, w2

    def _record(self, eng, fn, reads, writes, cost, kind, semkey=None):
        reads, writes = self._psum_fix(list(reads), list(writes))
        oid = self.nops
        self.nops += 1
        preds = set()
        for r in reads:
            t = self.last_write.get(r)
            if t is not None:
                preds.add(t)
        for w in writes:
            t = self.last_write.get(w)
            if t is not None:
                preds.add(t)
            preds.update(self.readers.get(w, ()))
        if kind == "dma":
            prev = self.dma_last.get(semkey)
            if prev is not None:
                preds.add(prev)
            self.dma_last[semkey] = oid
        preds = {p for p in preds if p >= self.seg_base}
        self.seg.append(dict(id=oid, eng=eng, fn=fn, preds=preds, cost=float(cost), kind=kind, semkey=semkey))
        for w in writes:
            self.last_write[w] = oid
            self.readers[w] = []
        for r in reads:
            self.readers.setdefault(r, []).append(oid)
        return oid

    def op(self, eng, fn, reads=(), writes=(), cost=300.0):
        if not self.enabled:
            return
        self._record(eng, fn, reads, writes, cost, "op")

    def dma(self, eng, fn, semkey, reads=(), writes=(), nbytes=1 << 20):
        if not self.enabled:
            return
        self._record(eng, fn, reads, writes, DMA_LAT_NS + nbytes / 160.0, "dma", semkey)

    def wait_all(self, eng, keys):
        self._record(eng, None, list(keys), [], 0.0, "op")

    def _schedule_segment(self):
        ops = self.seg
        if not ops:
            return
        byid = {o["id"]: o for o in ops}
        succ = {o["id"]: [] for o in ops}
        indeg = {}
        for o in ops:
            indeg[o["id"]] = len(o["preds"])
            for p in o["preds"]:
                succ[p].append(o["id"])
        ready_t = {o["id"]: 0.0 for o in ops}
        finish = {}
        heaps = {e: [] for e in self.ENGS}
        for o in ops:
            if indeg[o["id"]] == 0:
                heapq.heappush(heaps[o["eng"]], (0.0, o["id"]))
        etime = {e: 0.0 for e in self.ENGS}
        order = {e: [] for e in self.ENGS}
        remaining = len(ops)
        while remaining:
            best = None
            for e in self.ENGS:
                h = heaps[e]
                if not h:
                    continue
                rt, oid = h[0]
                st = max(rt, etime[e])
                if best is None or (st, oid) < (best[0], best[1]):
                    best = (st, oid, e)
            st, oid, e = best
            heapq.heappop(heaps[e])
            o = byid[oid]
            if o["kind"] == "dma":
                etime[e] = st + 150.0
                fin = st + o["cost"]
            else:
                etime[e] = st + o["cost"]
                fin = etime[e]
            finish[oid] = fin
            order[e].append(o)
            remaining -= 1
            for s in succ[oid]:
                so = byid[s]
                lat = SYNC_NS if (so["eng"] != e or o["kind"] == "dma") else (60.0 if e != "pe" else 0.0)
                ready_t[s] = max(ready_t[s], fin + lat)
                indeg[s] -= 1
                if indeg[s] == 0:
                    heapq.heappush(heaps[so["eng"]], (ready_t[s], s))
        self.est_ns = getattr(self, "est_ns", 0.0) + max(list(finish.values()) + [0.0])
        for o in ops:
            if o["kind"] == "dma":
                k = o["semkey"]
                if k not in self.dma_sems:
                    self.dma_sems[k] = self._new_sem(f"d{len(self.dma_sems)}")
                    self.dma_vals[k] = 0
                self.dma_vals[k] += 16
                self.ticks[o["id"]] = (self.dma_sems[k], self.dma_vals[k], "dma")
        def needs_sem(o):
            for s_ in succ[o["id"]]:
                se = byid[s_]["eng"]
                if se != o["eng"] or (SAME_ENGINE_SYNC and se != "pe"):
                    return True
            return False
        for e in self.ENGS:
            real = [o for o in order[e] if o["kind"] == "op" and o["fn"] is not None]
            for i_, o in enumerate(real):
                o["sig"] = needs_sem(o) or i_ == len(real) - 1
        for e in self.ENGS:
            for o in order[e]:
                if o["kind"] == "op" and o["fn"] is not None and o["sig"]:
                    c = self.count[e]
                    ep, v = divmod(c, EPOCH)
                    while len(self.esems[e]) <= ep:
                        self.esems[e].append(self._new_sem(f"s_{e}_{len(self.esems[e])}"))
                    self.count[e] = c + 1
                    self.ticks[o["id"]] = (self.esems[e][ep], v + 1, e)
        for e in self.ENGS:
            for o in order[e]:
                waits = {}
                for p in o["preds"]:
                    if byid[p]["eng"] == e and byid[p]["kind"] == "op" and (not SAME_ENGINE_SYNC or e == "pe"):
                        continue
                    sem, val, src = self.ticks[p]
                    sid = id(sem)
                    if self.known[e].get(sid, 0) >= val:
                        continue
                    if sid not in waits or waits[sid][1] < val:
                        waits[sid] = (sem, val)
                for sid, (sem, val) in waits.items():
                    self.known[e][sid] = val
                inc = None
                if o["fn"] is not None and o["id"] in self.ticks:
                    sem, val, src = self.ticks[o["id"]]
                    inc = (sem, 16 if o["kind"] == "dma" else 1)
                self.streams[e].append((o["fn"], list(waits.values()), inc))
        self.seg = []
        self.seg_base = self.nops

    def barrier(self):
        if not self.enabled and not self.seg:
            return
        self._schedule_segment()
        ticks = []
        for e2 in self.ENGS:
            c = self.count[e2]
            if c > 0:
                ep, v = divmod(c - 1, EPOCH)
                ticks.append((self.esems[e2][ep], v + 1))
        for k, sem in self.dma_sems.items():
            ticks.append((sem, self.dma_vals[k]))
        for eng in self.ENGS:
            waits = []
            for (sem, val) in ticks:
                if self.known[eng].get(id(sem), 0) >= val:
                    continue
                self.known[eng][id(sem)] = val
                waits.append((sem, val))
            if waits:
                self.streams[eng].append((None, waits, None))

    def emit(self):
        self._schedule_segment()
        nc = self.nc
        with nc.Block() as block:
            def run(e, stream):
                for fn, waits, inc in stream:
                    for sem, val in waits:
                        e.wait_ge(sem, val)
                    if fn is None:
                        continue
                    ins = fn(e)
                    if inc is not None:
                        ins.then_inc(inc[0], inc[1])

            @block.tensor
            def _(e):
                run(e, self.streams["pe"])

            @block.scalar
            def _(e):
                run(e, self.streams["act"])

            @block.vector
            def _(e):
                run(e, self.streams["dve"])

            @block.gpsimd
            def _(e):
                if self.pool_init is not None:
                    self.pool_init(e)
                run(e, self.streams["pool"])

            @block.sync
            def _(e):
                run(e, self.streams["sp"])


def _fsz(ap):
    s = ap.shape
    n = 1
    for v in s[1:]:
        n *= int(v)
    return n


C_GQ, C_GK, C_GV, C_GR = 0, 512, 1024, 2048
C_GLF, C_GLB = 3072, 3088
C_DQ, C_DK, C_DV, C_DZ = 3104, 4128, 5152, 6176
C_DAB = 7200
C_MA, C_MB = 7232, 8256
D_IN = 9280


def host_consts():
    r = np.arange(128)[:, None]
    t = np.arange(128)[None, :]
    same = (r // 64) == (t // 64)
    c = {}
    c["ident"] = np.eye(128, dtype=np.float32)
    c["a_le"] = np.where(r <= t, -1.0 / 16, 0.0)
    c["a_ge"] = np.where(r >= t, -1.0 / 16, 0.0)
    c["a_gt"] = np.where(r > t, -1.0 / 16, 0.0)
    c["a_lt"] = np.where(r < t, -1.0 / 16, 0.0)
    c["m_le"] = np.where(r <= t, 1.0, 0.0)
    c["m_ge"] = np.where(r >= t, 1.0, 0.0)
    c["b_le"] = np.where((r <= t) & same, 1.0, 0.0)
    c["b_ge"] = np.where((r >= t) & same, 1.0, 0.0)
    c["b_gt"] = np.where((r > t) & same, 1.0, 0.0)
    c["b_lt"] = np.where((r < t) & same, 1.0, 0.0)
    c["csel0"] = np.where(r < 64, 1.0, 0.0) + 0.0 * t
    c["csel1"] = np.where(r >= 64, 1.0, 0.0) + 0.0 * t
    c["ones"] = np.ones((128, 128))
    c["m_lt"] = np.where(r < t, 1.0, 0.0)
    c["bvals"] = 128.0 * t + 0.0 * r
    c["pidx"] = 1.0 * r + 0.0 * t
    names = list(c.keys())
    arr = np.stack([np.asarray(c[n], np.float32) for n in names], axis=1)
    return names, np.ascontiguousarray(arr)


CONST_NAMES, CONST_ARR = host_consts()
NCONST = len(CONST_NAMES)


def build(stage="all", dbg=False):
    nc = bass.Bass("TRN2", target_bir_lowering=False)
    stack = contextlib.ExitStack()
    with stack:
        P = Prog(nc, stack)

        def dram(name, shape, dt=F32, kind="ExternalInput"):
            return nc.dram_tensor(name, list(shape), dt, kind=kind).ap()

        def sb(name, shape, dt=F32):
            return stack.enter_context(nc.sbuf_tensor(name, list(shape), dt))

        def ps(name, shape, dt=F32):
            return stack.enter_context(nc.psum_tensor(name, list(shape), dt))

        def MM(out, lhsT, rhs, start, stop, R, W):
            n = _fsz(rhs)
            c = 55.0 + n * 0.45
            if rhs.dtype == F32:
                c *= 3.0
            P.op("pe", lambda e: e.matmul(out, lhsT, rhs, start=start, stop=stop), R, W, cost=c)

        def TR(out, in_, ident, R, W):
            P.op("pe", lambda e: e.transpose(out=out, in_=in_, identity=ident), R, W, cost=110.0)

        def ACTF(out, in_, func, R, W, **kw):
            c = 150.0 + _fsz(in_) * 1.0 + (90.0 if "accum_out" in kw else 0.0)
            P.op("act", lambda e: e.activation(out=out, in_=in_, func=func, **kw), R, W, cost=c)

        def _vc(eng, n, k=1.5):
            return (100.0 + n * k * 0.6) if eng == "dve" else (150.0 + n * 1.9)

        def TT(eng, out, in0, in1, op, R, W):
            P.op(eng, lambda e: e.tensor_tensor(out=out, in0=in0, in1=in1, op=op), R, W, cost=_vc(eng, _fsz(out)))

        def TS(eng, out, in0, s1, s2, op0, op1, R, W):
            P.op(eng, lambda e: e.tensor_scalar(out=out, in0=in0, scalar1=s1, scalar2=s2, op0=op0, op1=op1), R, W,
                 cost=_vc(eng, _fsz(out), 1.05))

        def STT(out, in0, scalar, in1, op0, op1, R, W):
            P.op("dve", lambda e: e.scalar_tensor_tensor(out=out, in0=in0, scalar=scalar, in1=in1, op0=op0, op1=op1), R, W,
                 cost=_vc("dve", _fsz(out)))

        def CP(eng, out, in_, R, W):
            if eng == "act":
                P.op("act", lambda e: e.activation(out=out, in_=in_, func=AF.Copy), R, W, cost=150.0 + _fsz(in_) * 1.0)
            else:
                P.op(eng, lambda e: e.tensor_copy(out=out, in_=in_), R, W, cost=_vc(eng, _fsz(out), 1.05))

        def MEMSET(eng, ap, val, W):
            P.op(eng, lambda e: e.memset(ap, val), [], W, cost=_vc(eng, _fsz(ap), 0.6))

        def DMA(eng, out, in_, semkey, R, W):
            P.dma(eng, lambda e: e.dma_start(out=out, in_=in_), semkey, R, W, nbytes=_fsz(out) * int(out.shape[0]) * 4)

        def RECIP(out, in_, R, W):
            P.op("dve", lambda e: e.reciprocal(out=out, in_=in_), R, W, cost=_vc("dve", _fsz(out), 1.05))

        def MARK(name):
            if stage == name:
                P.enabled = False

        def rstd_inplace(ap, n, key):
            TS("dve", ap, ap, 1.0 / n, EPS, ALU.mult, ALU.add, [key], [key])
            ACTF(ap, ap, AF.Ln, [key], [key])
            ACTF(ap, ap, AF.Exp, [key], [key], scale=-0.5)

        x_d = dram("x", [T, D])
        n1_d = dram("norm1_w", [1, D])
        n2_d = dram("norm2_w", [1, D])
        nf_d = dram("norm_f_w", [1, D])
        consts_d = dram("consts", [128, NCONST, 128])
        w_in_d = dram("w_in", [D, D_IN])
        w2b_d = [dram("gla_w2b_f", [17, 512]), dram("gla_w2b_b", [17, 512])]
        gnw_d = dram("gla_norm_w", [1, 256])
        out_d = dram("out", [T, D], kind="ExternalOutput")
        dbg_d = dram("dbg", [T, D], kind="ExternalOutput") if dbg else None

        consts = sb("consts_sb", [128, NCONST, 128])
        CI = {n: i for i, n in enumerate(CONST_NAMES)}

        def cst(name):
            return consts[:, CI[name], :]

        ident_b = sb("ident_b", [128, 128], BF16)
        ones_b = sb("ones_b", [128, 128], BF16)
        hT = sb("hT", [128, KT, T], BF16)
        mixed = sb("mixed", [128, NT, D], BF16)
        small = sb("small", [128, 64])
        ARENA_BYTES = 134 * 1024
        arena = sb("arena", [128, ARENA_BYTES // 4])

        def carve(off, shape, dt, base=None, cap=None):
            base = arena if base is None else base
            cap = ARENA_BYTES if cap is None else cap
            nb = int(np.prod(shape)) * (2 if dt == BF16 else 4)
            nb = (nb + 3) // 4 * 4
            assert off % 4 == 0 and off + nb <= cap, (off, nb)
            v = base[:, off // 4:(off + nb) // 4]
            if dt != F32:
                v = v.bitcast(dt)
            if len(shape) == 2:
                pat = "p (a b) -> p a b"
                v = v.rearrange(pat, a=shape[0])
            elif len(shape) == 3:
                v = v.rearrange("p (a b c) -> p a b c", a=shape[0], b=shape[1])
            return v, off + nb

        pt = [ps(f"pt{i}", [128, 1024]) for i in range(3)]
        ptb = ps("ptb", [128, 2048], BF16)

        def bank(i):
            return pt[i // 2][:, (i % 2) * 512:(i % 2 + 1) * 512]

        def bkey(i):
            return ("pb", i)

        def bbank(i):
            return ptb[:, i * 1024:(i + 1) * 1024]

        DMA("sp", consts[:], consts_d[:, :, :], "c_consts", [], ["consts"])
        CP("dve", ident_b[:], cst("ident"), ["consts"], ["ident_b"])
        MEMSET("pool", ones_b[:], 1.0, ["ones_b"])

        off = 116 * 1024
        xt0, off = carve(off, [D], F32)
        xt1, off = carve(off, [D], F32)
        hn0, off = carve(off, [D], BF16)
        hn1, off = carve(off, [D], BF16)
        sq, off = carve(off, [D], BF16)
        n1_bc, off = carve(off, [D], F32)
        DMA("sp", n1_bc, n1_d.partition_broadcast(128), "c_n1", [], ["n1_bc"])
        xts = [xt0, xt1]
        hns = [hn0, hn1]
        for tt in range(NT):
            b = tt % 2
            xb, hb = xts[b], hns[b]
            DMA("sp", xb, x_d[tt * 128:(tt + 1) * 128, :], ("xt", b), [], [("xt", b)])
            ACTF(sq, xb, AF.Square, [("xt", b)], ["sq", "ss0"], accum_out=small[:, 0:1])
            rstd_inplace(small[:, 0:1], D, "ss0")
            STT(hb, xb, small[:, 0:1], n1_bc, ALU.mult, ALU.mult, [("xt", b), "ss0", "n1_bc"], [("hn", b)])
            for kt in range(KT):
                TR(bbank(b)[:, kt * 128:(kt + 1) * 128], hb[:, kt * 128:(kt + 1) * 128], ident_b[:],
                   [("hn", b), "ident_b"], [("pbb", b)])
            CP("act", hT[:, :, tt * 128:(tt + 1) * 128], bbank(b).rearrange("p (k t) -> p k t", k=KT),
               [("pbb", b)], [("hT", tt)])
        HT_ALL = [("hT", tt) for tt in range(NT)]
        MARK("p1")

        off = 0
        qT, off = carve(off, [T], F32)
        kT, off = carve(off, [T], F32)
        k_tok, off = carve(off, [NT, 128], F32)
        v_tok, off = carve(off, [NT, 256], BF16)
        qdT = [None, None]
        kiT = [None, None]
        ktail = [None, None]
        for d_ in range(2):
            qdT[d_], off = carve(off, [T], BF16)
            kiT[d_], off = carve(off, [T], BF16)
            ktail[d_], off = carve(off, [NT, 128], BF16)
        sb_store, off = carve(off, [NT, 256], BF16)
        dec, off = carve(off, [2, NT], F32)
        S, off = carve(off, [256], F32)
        S_bf, off = carve(off, [256], BF16)
        NTMP = 3
        tmp = []
        for i in range(NTMP):
            d = {}
            for nm in ("e", "lg", "E", "Ei", "Et"):
                d[nm], off = carve(off, [128], F32)
            d["Pf"], off = carve(off, [128], BF16)
            d["Pb"], off = carve(off, [128], BF16)
            d["sig"], off = carve(off, [512], F32)
            d["G"], off = carve(off, [256], F32)
            tmp.append(d)
        gl, off = carve(off, [2, T], BF16)
        w2b, off = carve(off, [2, 512], BF16)
        wqk, off = carve(off, [KT, 256], BF16)
        wkv, off = carve(off, [KT, 384], BF16)
        wgm, off = carve(off, [KT, 512], BF16)
        wgl, off = carve(off, [KT, 32], BF16)
        gnw_bc, off = carve(off, [256], F32)
        GLA_END = off
        assert GLA_END <= 116 * 1024, GLA_END

        DMA("sp", gnw_bc, gnw_d.partition_broadcast(128), "c_gnw", [], ["gnw_bc"])
        MEMSET("pool", gl[:, :, :], 1.0, ["gl"])
        MEMSET("pool", w2b[:, :, :], 0.0, ["w2b"])
        for d_ in range(2):
            DMA("pool", w2b[0:17, d_, :], w2b_d[d_][:, :], "c_w2b", [], ["w2b"])
        DMA("pool", wgl, w_in_d[:, C_GLF:C_GLF + 32].rearrange("(k p) c -> p k c", p=128), "w_wgl", [], ["wgl"])
        for d_ in range(2):
            for tg in range(4):
                bi = tg % 2
                for kt in range(KT):
                    MM(bank(bi)[0:16, :], wgl[:, kt, d_ * 16:(d_ + 1) * 16], hT[:, kt, tg * 512:(tg + 1) * 512],
                       kt == 0, kt == KT - 1, ["wgl"] + HT_ALL[tg * 4:tg * 4 + 4], [bkey(bi)])
                CP("act", gl[0:16, d_, tg * 512:(tg + 1) * 512], bank(bi)[0:16, :], [bkey(bi)], ["gl"])

        MARK("g0")
        QSCALE = 128.0 ** -0.5
        for h in range(4):
            def wcols(dst, c0, n):
                return (dst, w_in_d[:, c0:c0 + n].rearrange("(k p) c -> p k c", p=128))
            for (dst, src) in (wcols(wqk[:, :, 0:128], C_GQ + h * 128, 128), wcols(wqk[:, :, 128:256], C_GK + h * 128, 128)):
                DMA("pool", dst, src, "w_wqk", [], ["wqk"])
            for (dst, src) in (wcols(wkv[:, :, 0:128], C_GK + h * 128, 128), wcols(wkv[:, :, 128:384], C_GV + h * 256, 256)):
                DMA("pool", dst, src, "w_wkv", [], ["wkv"])
            for (dst, src) in (wcols(wgm[:, :, 0:256], C_GR + h * 256, 256), wcols(wgm[:, :, 256:512], C_MA + h * 256, 256)):
                DMA("pool", dst, src, "w_wgm", [], ["wgm"])
            MARK("g1a")
            for which, dstT in ((0, qT), (1, kT)):
                for tg in range(4):
                    bi = (which * 4 + tg) % 4
                    for kt in range(KT):
                        MM(bank(bi), wqk[:, kt, which * 128:(which + 1) * 128], hT[:, kt, tg * 512:(tg + 1) * 512],
                           kt == 0, kt == KT - 1, ["wqk"] + HT_ALL[tg * 4:tg * 4 + 4], [bkey(bi)])
                    CP("act" if tg % 2 else "dve", dstT[:, tg * 512:(tg + 1) * 512], bank(bi), [bkey(bi)],
                       [("qkT", which, tg)])
            MARK("g1b")
            for n in range(NT):
                bi = 4 + n % 2
                for kt in range(KT):
                    MM(bank(bi)[:, 0:384], hT[:, kt, n * 128:(n + 1) * 128], wkv[:, kt, :],
                       kt == 0, kt == KT - 1, ["wkv", ("hT", n)], [bkey(bi)])
                CP("dve", k_tok[:, n, :], bank(bi)[:, 0:128], [bkey(bi)], [("k_tok", n)])
                CP("act", v_tok[:, n, :], bank(bi)[:, 128:384], [bkey(bi)], [("v_tok", n)])
            MARK("g1")
            for n in range(NT):
                tsl = slice(n * 128, (n + 1) * 128)
                tg = n // 4
                for d_ in range(2):
                    tm = tmp[(n * 2 + d_) % NTMP]
                    tk = ("gtmp", (n * 2 + d_) % NTMP)
                    a_c = cst("a_le") if d_ == 0 else cst("a_ge")
                    a_s = cst("a_gt") if d_ == 0 else cst("a_lt")
                    b0 = (n * 2 + d_) % 2 * 2
                    zb, cb = bank(b0), bank(b0 + 1)
                    MM(zb[:, 0:128], gl[:, d_, tsl], w2b[:, d_, h * 128:(h + 1) * 128], True, True,
                       ["gl", "w2b"], [bkey(b0)])
                    ACTF(tm["e"], zb[:, 0:128], AF.Exp, [bkey(b0)], [tk], scale=-1.0)
                    ACTF(tm["lg"], tm["e"], AF.Ln, [tk], [tk], bias=1.0)
                    MM(cb[:, 0:128], tm["lg"], a_c, True, True, [tk, "consts"], [bkey(b0 + 1)])
                    MM(cb[:, 128:256], a_s, tm["lg"], True, True, [tk, "consts"], [bkey(b0 + 1)])
                    ACTF(tm["E"], cb[:, 0:128], AF.Exp, [bkey(b0 + 1)], [tk])
                    ACTF(tm["Ei"], cb[:, 0:128], AF.Exp, [bkey(b0 + 1)], [tk], scale=-1.0)
                    ACTF(tm["Et"], cb[:, 128:256], AF.Exp, [bkey(b0 + 1)], [tk])
                    STT(qdT[d_][:, tsl], qT[:, tsl], QSCALE, tm["E"], ALU.mult, ALU.mult,
                        [("qkT", 0, tg), tk], [("qdT", d_, n)])
                    TT("dve", kiT[d_][:, tsl], kT[:, tsl], tm["Ei"], ALU.mult, [("qkT", 1, tg), tk], [("kiT", d_, n)])
                    TT("dve", ktail[d_][:, n, :], k_tok[:, n, :], tm["Et"], ALU.mult, [("k_tok", n), tk], [("ktail", d_, n)])
                    col = 127 if d_ == 0 else 0
                    CP("dve", dec[:, d_, n:n + 1], tm["E"][:, col:col + 1], [tk], [("dec", d_, n)])
            MARK("g2")
            MEMSET("dve", S, 0.0, ["S"])
            for n in range(NT - 1, -1, -1):
                CP("act", sb_store[:, n, :], S, ["S"], [("sb_store", n)])
                bi = 4 + n % 2
                MM(bank(bi)[:, 0:256], ktail[1][:, n, :], v_tok[:, n, :], True, True,
                   [("ktail", 1, n), ("v_tok", n)], [bkey(bi)])
                STT(S, S, dec[:, 1, n:n + 1], bank(bi)[:, 0:256], ALU.mult, ALU.add,
                    ["S", ("dec", 1, n), bkey(bi)], ["S"])
            MARK("g3")
            MEMSET("dve", S, 0.0, ["S"])
            for n in range(NT):
                tsl = slice(n * 128, (n + 1) * 128)
                tm = tmp[n % NTMP]
                tk = ("ftmp", n % NTMP)
                CP("act", S_bf, S, ["S"], ["S_bf"])
                b0 = (n % 2) * 2
                sc = bank(b0)
                MM(sc[:, 0:128], kiT[0][:, tsl], qdT[0][:, tsl], True, True, [("kiT", 0, n), ("qdT", 0, n)], [bkey(b0)])
                MM(sc[:, 128:256], kiT[1][:, tsl], qdT[1][:, tsl], True, True, [("kiT", 1, n), ("qdT", 1, n)], [bkey(b0)])
                TT("dve", tm["Pf"], sc[:, 0:128], cst("m_le"), ALU.mult, [bkey(b0), "consts"], [tk])
                TT("dve", tm["Pb"], sc[:, 128:256], cst("m_ge"), ALU.mult, [bkey(b0), "consts"], [tk])
                ob = bank(b0 + 1)
                ok = bkey(b0 + 1)
                MM(ob[:, 0:256], qdT[0][:, tsl], S_bf, True, False, [("qdT", 0, n), "S_bf"], [ok])
                MM(ob[:, 0:256], qdT[1][:, tsl], sb_store[:, n, :], False, False, [("qdT", 1, n), ("sb_store", n)], [ok])
                MM(ob[:, 0:256], tm["Pf"], v_tok[:, n, :], False, False, [tk, ("v_tok", n)], [ok])
                MM(ob[:, 0:256], tm["Pb"], v_tok[:, n, :], False, True, [tk, ("v_tok", n)], [ok])
                kb = 4 + n % 2
                MM(bank(kb)[:, 0:256], ktail[0][:, n, :], v_tok[:, n, :], True, True,
                   [("ktail", 0, n), ("v_tok", n)], [bkey(kb)])
                STT(S, S, dec[:, 0, n:n + 1], bank(kb)[:, 0:256], ALU.mult, ALU.add,
                    ["S", ("dec", 0, n), bkey(kb)], ["S"])
                gb = 4 + n % 2
                for kt in range(KT):
                    MM(bank(gb), hT[:, kt, tsl], wgm[:, kt, :], kt == 0, kt == KT - 1, ["wgm", ("hT", n)], [bkey(gb)])
                ACTF(tm["sig"], bank(gb), AF.Exp, [bkey(gb)], [("sig", n % NTMP)], scale=-1.0)
                ACTF(tm["sig"], tm["sig"], AF.Ln, [("sig", n % NTMP)], [("sig", n % NTMP)], bias=1.0)
                ACTF(tm["sig"], tm["sig"], AF.Exp, [("sig", n % NTMP)], [("sig", n % NTMP)], scale=-1.0)
                TT("pool", tm["G"], tm["sig"][:, 0:256], tm["sig"][:, 256:512], ALU.mult, [("sig", n % NTMP)], [("G", n % NTMP)])
                TT("dve", tm["G"], tm["G"], bank(gb)[:, 0:256], ALU.mult, [("G", n % NTMP), bkey(gb)], [("G", n % NTMP)])
                TT("pool", tm["G"], tm["G"], gnw_bc, ALU.mult, [("G", n % NTMP), "gnw_bc"], [("G", n % NTMP)])
                ssk = ("ssq", n % 2)
                ssap = small[:, 2 + n % 2:3 + n % 2]
                ACTF(tm["sig"][:, 0:256], ob[:, 0:256], AF.Square, [ok, ("G", n % NTMP)], [("sig", n % NTMP), ssk],
                     accum_out=ssap)
                rstd_inplace(ssap, 256, ssk)
                STT(mixed[:, n, h * 256:(h + 1) * 256], ob[:, 0:256], ssap, tm["G"], ALU.mult, ALU.mult,
                    [ok, ssk, ("G", n % NTMP)], [("mixed", n)])

        P.barrier()
        HG = 4
        off = 0
        gqT, off = carve(off, [HG, T], BF16)
        gkT, off = carve(off, [HG, T], BF16)
        gvT, off = carve(off, [HG, T], BF16)
        dabs, off = carve(off, [NT, 32], F32)
        g_raw, off = carve(off, [NT, 2, 8], F32)
        beta, off = carve(off, [NT, 2, 8], F32)
        gvec, off = carve(off, [64], F32)
        wsl0, off = carve(off, [KT, 512], BF16)
        wsl1, off = carve(off, [KT, 512], BF16)
        wsl = [wsl0, wsl1]
        cwT, off = carve(off, [24, 5], F32)
        gdnw_bc, off = carve(off, [128], F32)
        wdab, off = carve(off, [KT, 32], BF16)
        TMP0 = off
        xc = [None, None]
        xc[0], off = carve(off, [T + 4], BF16)
        xc[1], off = carve(off, [T + 4], BF16)
        diag, off = carve(off, [5, 128], BF16)
        ce = [None, None]
        cy = [None, None]
        for i in range(2):
            ce[i], off = carve(off, [512], F32)
            cy[i], off = carve(off, [512], F32)
        cysq, off = carve(off, [512], BF16)
        crs, off = carve(off, [512], F32)
        CONV_END = off
        off = TMP0
        DB = []
        for d_ in range(2):
            dd = {}
            for nm in ("GMB", "Wd", "decT", "u_sb", "Sg"):
                dd[nm], off = carve(off, [HG, 128], F32)
            for nm in ("Lm", "LTm", "XT", "Pp0", "Pp1", "PTp0", "PTp1", "kbg", "vbeta", "qd_tok",
                       "attnT", "ktl0", "ktl1", "qdTg", "wT_sb", "vnew", "Sg_bf", "ostage"):
                dd[nm], off = carve(off, [HG, 128], BF16)
            dd["bg"], off = carve(off, [HG], F32)
            dd["et2"], off = carve(off, [2, HG], F32)
            DB.append(dd)
        oland = []
        for i in range(2):
            a, off = carve(off, [HG, 128], BF16)
            oland.append(a)
        osum, off = carve(off, [HG, 128], F32)
        fsig, off = carve(off, [512], F32)
        fG, off = carve(off, [HG, 128], F32)
        frs, off = carve(off, [8], F32)
        SWEEP_END = off
        gdn_o = nc.dram_tensor("gdn_o_spill", [NT, 128, HG * 128], BF16, kind="Internal").ap()

        gdnw_d = dram("gdn_norm_w", [1, 128])
        gvec_d = dram("gdn_vec", [1, 32])
        cw_d = dram("gdn_conv_wT", [128, 24, 5])
        DMA("sp", gdnw_bc, gdnw_d.partition_broadcast(128), "c_gdnw", [], ["gdnw_bc"])
        DMA("sp", gvec[:, 0:32], gvec_d.partition_broadcast(128), "c_gvec", [], ["gvec"])
        DMA("sp", cwT, cw_d[:, :, :], "c_cw", [], ["cwT"])
        DMA("pool", wdab, w_in_d[:, C_DAB:C_DAB + 32].rearrange("(k p) c -> p k c", p=128), "w_wdab", [], ["wdab"])
        ACTF(gvec[:, 16:32], gvec[:, 16:32], AF.Exp, ["gvec"], ["gvec"])
        TS("dve", gvec[:, 16:32], gvec[:, 16:32], -1.0, None, ALU.mult, ALU.bypass, ["gvec"], ["gvec"])
        for n in range(NT):
            bi = n % 2
            for kt in range(KT):
                MM(bank(bi)[:, 0:32], hT[:, kt, n * 128:(n + 1) * 128], wdab[:, kt, :], kt == 0, kt == KT - 1,
                   ["wdab", ("hT", n)], [bkey(bi)])
            CP("act", dabs[:, n, :], bank(bi)[:, 0:32], [bkey(bi)], ["dabs"])
        a_view = dabs[:, :, 0:16]
        b_view = dabs[:, :, 16:32]
        g_flat = g_raw.rearrange("p n d h -> p n (d h)")
        be_flat = beta.rearrange("p n d h -> p n (d h)")
        TT("dve", g_flat, a_view, gvec[:, 0:16].unsqueeze(1).to_broadcast([128, NT, 16]), ALU.add, ["dabs", "gvec"], ["g_raw"])
        ACTF(g_flat, g_flat, AF.Exp, ["g_raw"], ["g_raw"])
        ACTF(g_flat, g_flat, AF.Ln, ["g_raw"], ["g_raw"], bias=1.0)
        TT("dve", g_flat, g_flat, gvec[:, 16:32].unsqueeze(1).to_broadcast([128, NT, 16]), ALU.mult, ["g_raw", "gvec"], ["g_raw"])
        ACTF(be_flat, b_view, AF.Exp, ["dabs"], ["beta"], scale=-1.0)
        TS("dve", be_flat, be_flat, 1.0, None, ALU.add, ALU.bypass, ["beta"], ["beta"])
        RECIP(be_flat, be_flat, ["beta"], ["beta"])
        MARK("d0")

        GSCALE = 128.0 ** -0.5
        ident_bc4 = ident_b[:].unsqueeze(1).to_broadcast([128, HG, 128])

        def bc_h(ap2):
            return ap2.unsqueeze(2).to_broadcast([128, HG, 128])

        def bc_m(ap2):
            return ap2.unsqueeze(1).to_broadcast([128, HG, 128])

        def v4(ap2):
            return ap2.rearrange("p (h d) -> p h d", h=HG)

        for grp in range(2):
            hs0 = grp * HG
            for which, c_base, dstT in ((0, C_DQ, gqT), (1, C_DK, gkT), (2, C_DV, gvT)):
                ws = wsl[which % 2]
                wk = ("wsl", which % 2)
                DMA("pool", ws, w_in_d[:, c_base + hs0 * 128:c_base + (hs0 + HG) * 128].rearrange("(k p) c -> p k c", p=128),
                    ("w_wsl", which % 2), [], [wk])
                for hh in range(HG):
                    ci = which * 8 + hs0 + hh
                    xi = (which * HG + hh) % 2
                    xcb = xc[xi]
                    xk = ("xc", xi)
                    MEMSET("pool", xcb[:, 0:2], 0.0, [xk])
                    MEMSET("pool", xcb[:, T + 2:T + 4], 0.0, [xk])
                    for k in range(5):
                        TS("dve", diag[:, k, :], cst("ident"), cwT[:, ci, k:k + 1], None, ALU.mult, ALU.bypass,
                           ["consts", "cwT"], ["diag"])
                    for tg in range(4):
                        bi = tg % 2
                        for kt in range(KT):
                            MM(bank(bi), ws[:, kt, hh * 128:(hh + 1) * 128], hT[:, kt, tg * 512:(tg + 1) * 512],
                               kt == 0, kt == KT - 1, [wk] + HT_ALL[tg * 4:tg * 4 + 4], [bkey(bi)])
                        CP("act" if tg % 2 else "dve", xcb[:, 2 + tg * 512:2 + (tg + 1) * 512], bank(bi), [bkey(bi)], [xk])
                    for tg in range(4):
                        bi = 2 + tg % 2
                        i2 = tg % 2
                        for k in range(5):
                            MM(bank(bi), diag[:, k, :], xcb[:, tg * 512 + k:tg * 512 + k + 512], k == 0, k == 4,
                               ["diag", xk], [bkey(bi)])
                        ck = ("ctmp", i2)
                        ACTF(ce[i2], bank(bi), AF.Exp, [bkey(bi)], [ck], scale=-1.0)
                        ACTF(ce[i2], ce[i2], AF.Ln, [ck], [ck], bias=1.0)
                        ACTF(ce[i2], ce[i2], AF.Exp, [ck], [ck], scale=-1.0)
                        dst = dstT[:, hh, tg * 512:(tg + 1) * 512]
                        dk = ("gT", which, hh, tg)
                        if which == 2:
                            TT("dve", dst, ce[i2], bank(bi), ALU.mult, [ck, bkey(bi)], [dk])
                        else:
                            TT("dve", cy[i2], ce[i2], bank(bi), ALU.mult, [ck, bkey(bi)], [("cy", i2)])
                            TT("pool", cysq, cy[i2], cy[i2], ALU.mult, [("cy", i2)], ["cysq"])
                            MM(bank(4), ones_b[:], cysq, True, True, ["ones_b", "cysq"], [bkey(4)])
                            ACTF(crs, bank(4), AF.Ln, [bkey(4)], ["crs"], bias=EPS)
                            ACTF(crs, crs, AF.Exp, ["crs"], ["crs"], scale=-0.5)
                            if which == 0:
                                STT(dst, cy[i2], GSCALE, crs, ALU.mult, ALU.mult, [("cy", i2), "crs"], [dk])
                            else:
                                TT("dve", dst, cy[i2], crs, ALU.mult, [("cy", i2), "crs"], [dk])
            MARK("d1")
            P.barrier()
            DMA("pool", wsl[0], w_in_d[:, C_DZ + hs0 * 128:C_DZ + (hs0 + HG) * 128].rearrange("(k p) c -> p k c", p=128),
                ("w_wsl", 0), [], [("wsl", 0)])
            DMA("pool", wsl[1], w_in_d[:, C_MB + hs0 * 128:C_MB + (hs0 + HG) * 128].rearrange("(k p) c -> p k c", p=128),
                ("w_wsl", 1), [], [("wsl", 1)])

            def gT_keys(which, n):
                return [("gT", which, hh, n // 4) for hh in range(HG)]

            stored = set()
            esc_all = [dabs[:, 0:8, :].rearrange("p a b -> p (a b)").rearrange("p (k n h) -> p k n h", k=4, n=NT),
                       dabs[:, 8:16, :].rearrange("p a b -> p (a b)").rearrange("p (k n h) -> p k n h", k=4, n=NT)]
            for d_ in range(2):
                Mc_ = cst("b_le") if d_ == 0 else cst("b_ge")
                Ms_ = cst("b_gt") if d_ == 0 else cst("b_lt")
                for ki, mk in enumerate((Mc_, Ms_, cst("csel0"), cst("csel1"))):
                    MM(bank(d_)[:, ki * 64:(ki + 1) * 64].rearrange("p (n h) -> p n h", n=NT), mk,
                       g_raw[:, :, d_, hs0:hs0 + HG], True, True, ["consts", "g_raw"], [bkey(d_)])
                ACTF(esc_all[d_].rearrange("p k n h -> p (k n h)"), bank(d_)[:, 0:256], AF.Exp, [bkey(d_)], [("esc_all", d_), "dabs"])

            gb0 = ptb[:, 0:512]
            gb1 = ptb[:, 512:1024]
            xbank = ptb[:, 1024:2048].bitcast(F32)
            XK = ("pb", 7)

            def gdn_tile(d_, n):
                B = DB[d_]
                dk = lambda nm: (nm, d_)
                Mc = cst("b_le") if d_ == 0 else cst("b_ge")
                Ms = cst("b_gt") if d_ == 0 else cst("b_lt")
                bg = B["bg"]
                GMB, Wd, decT, Lm, LTm, XT = B["GMB"], B["Wd"], B["decT"], B["Lm"], B["LTm"], B["XT"]
                kbg, vbeta, qd_tok = B["kbg"], B["vbeta"], B["qd_tok"]
                Ppd = [B["Pp0"], B["Pp1"]]
                PTpd = [B["PTp0"], B["PTp1"]]
                pa, pb_ = (0, 1) if d_ == 0 else (2, 3)
                tsl = slice(n * 128, (n + 1) * 128)
                gv = g_raw[:, n, d_, hs0:hs0 + HG]
                bv = beta[:, n, d_, hs0:hs0 + HG]
                EA = esc_all[d_]
                e_cum, e_tail = EA[:, 0, n, :], EA[:, 1, n, :]
                second = n in stored
                if second:
                    par = n % 2
                    DMA("sp", oland[par].rearrange("p h d -> p (h d)"), gdn_o[n], ("oland", par), [("o_dram", n)], [("oland", par)])
                TT("dve", bg, bv, e_cum, ALU.mult, ["beta", ("esc_all", d_)], [dk("bg")])
                for hh in range(HG):
                    TR(gb0[:, hh * 128:(hh + 1) * 128], gkT[:, hh, tsl], ident_b[:], gT_keys(1, n) + ["ident_b"], [("pbb", 0)])
                for hh in range(HG):
                    TR(gb1[:, hh * 128:(hh + 1) * 128], gvT[:, hh, tsl], ident_b[:], gT_keys(2, n) + ["ident_b"], [("pbb", 0)])
                TT("dve", kbg, v4(gb0), bc_h(bg), ALU.mult, [("pbb", 0), dk("bg")], [dk("kbg")])
                for c_ in range(2):
                    TT("dve", B["et2"][:, c_, :], e_tail, cst("csel%d" % c_)[:, 0:HG], ALU.mult, [("esc_all", d_), "consts"], [dk("et2")])
                    TT("dve", B["ktl%d" % c_], v4(gb0), bc_h(B["et2"][:, c_, :]), ALU.mult, [("pbb", 0), dk("et2")], [dk("ktl%d" % c_)])
                TT("dve", vbeta, v4(gb1), bc_h(bv), ALU.mult, [("pbb", 0), "beta"], [dk("vbeta")])
                for hh in range(HG):
                    TR(gb0[:, hh * 128:(hh + 1) * 128], gqT[:, hh, tsl], ident_b[:], gT_keys(0, n) + ["ident_b"], [("pbb", 0)])
                TT("dve", qd_tok, v4(gb0), bc_h(e_cum), ALU.mult, [("pbb", 0), ("esc_all", d_)], [dk("qd_tok")])
                for hh in range(HG):
                    TR(gb1[:, hh * 128:(hh + 1) * 128], qd_tok[:, hh, :], ident_b[:], [dk("qd_tok"), "ident_b"], [("pbb", 0)])
                CP("act", B["qdTg"], v4(gb1), [("pbb", 0)], [dk("qdTg")])
                TT("pool", GMB, bc_h(gv), bc_m(Ms), ALU.mult, ["g_raw", "consts"], [dk("GMB")])
                MM(bank(pb_), Mc, GMB.rearrange("p h s -> p (h s)"), True, True, ["consts", dk("GMB")], [bkey(pb_)])
                ACTF(Wd.rearrange("p h s -> p (h s)"), bank(pb_), AF.Exp, [bkey(pb_)], [dk("Wd")])
                TT("pool", GMB, bc_h(bv), bc_m(Ms), ALU.mult, ["beta", "consts"], [dk("GMB")])
                TT("pool", Wd, Wd, GMB, ALU.mult, [dk("Wd"), dk("GMB")], [dk("Wd")])
                TT("pool", GMB, bc_h(gv), bc_m(Mc), ALU.mult, ["g_raw", "consts"], [dk("GMB")])
                MM(bank(pa), Ms, GMB.rearrange("p h s -> p (h s)"), True, True, ["consts", dk("GMB")], [bkey(pa)])
                ACTF(decT.rearrange("p h s -> p (h s)"), bank(pa), AF.Exp, [bkey(pa)], [dk("decT")])
                TT("pool", decT, decT, bc_m(Mc), ALU.mult, [dk("decT"), "consts"], [dk("decT")])
                for hh in range(HG):
                    MM(bank(pb_)[:, hh * 128:(hh + 1) * 128], gkT[:, hh, tsl], gkT[:, hh, tsl], True, True,
                       gT_keys(1, n), [bkey(pb_)])
                TT("dve", Lm, v4(bank(pb_)), Wd, ALU.mult, [bkey(pb_), dk("Wd")], [dk("Lm")])
                for hh in range(HG):
                    MM(bank(pa)[:, hh * 128:(hh + 1) * 128], gkT[:, hh, tsl], gqT[:, hh, tsl], True, True,
                       gT_keys(1, n) + gT_keys(0, n), [bkey(pa)])
                TT("dve", B["attnT"], v4(bank(pa)), decT, ALU.mult, [bkey(pa), dk("decT")], [dk("attnT")])
                for hh in range(HG):
                    TR(gb0[:, hh * 128:(hh + 1) * 128], Lm[:, hh, :], ident_b[:], [dk("Lm"), "ident_b"], [("pbb", 0)])
                CP("act", LTm, v4(gb0), [("pbb", 0)], [dk("LTm")])
                TT("dve", XT, ident_bc4, v4(gb0), ALU.subtract, ["ident_b", ("pbb", 0)], [dk("XT")])
                Pc, PTc = Lm, LTm
                pck, ptk_ = dk("Lm"), dk("LTm")
                for it in range(5):
                    Pn, PTn = Ppd[it % 2], PTpd[it % 2]
                    pnk, ptnk = ("Pp", it % 2, d_), ("PTp", it % 2, d_)
                    for hh in range(HG):
                        MM(bank(pb_)[:, hh * 128:(hh + 1) * 128], PTc[:, hh, :], Pc[:, hh, :], True, True, [pck, ptk_], [bkey(pb_)])
                    CP("act", Pn, v4(bank(pb_)), [bkey(pb_)], [pnk])
                    if it < 4:
                        for hh in range(HG):
                            MM(bank(pa)[:, hh * 128:(hh + 1) * 128], Pc[:, hh, :], PTc[:, hh, :], True, True, [pck, ptk_], [bkey(pa)])
                        CP("dve", PTn, v4(bank(pa)), [bkey(pa)], [ptnk])
                    for hh in range(HG):
                        MM(xbank[:, hh * 128:(hh + 1) * 128], Pn[:, hh, :], XT[:, hh, :], True, True, [pnk, dk("XT")], [XK])
                    TT("dve", XT, XT, v4(xbank), ALU.add, [dk("XT"), XK], [dk("XT")])
                    Pc, PTc, pck, ptk_ = Pn, PTn, pnk, ptnk
                for hh in range(HG):
                    MM(bank(pa)[:, hh * 128:(hh + 1) * 128], XT[:, hh, :], vbeta[:, hh, :], True, True, [dk("XT"), dk("vbeta")], [bkey(pa)])
                CP("act", B["u_sb"], v4(bank(pa)), [bkey(pa)], [dk("u_sb")])
                for hh in range(HG):
                    MM(bank(pb_)[:, hh * 128:(hh + 1) * 128], kbg[:, hh, :], XT[:, hh, :], True, True, [dk("kbg"), dk("XT")], [bkey(pb_)])
                CP("dve", B["wT_sb"], v4(bank(pb_)), [bkey(pb_)], [dk("wT_sb")])
                sb0 = 4
                Sg, Sg_bf, vnew = B["Sg"], B["Sg_bf"], B["vnew"]
                chunks = (0, 1) if d_ == 0 else (1, 0)
                for c in chunks:
                    sl = slice(c * 64, c * 64 + 64)
                    for hh in range(HG):
                        MM(bank(sb0)[:, hh * 128:(hh + 1) * 128], B["wT_sb"][:, hh, :], Sg_bf[:, hh, :], True, True,
                           [dk("wT_sb"), dk("Sg_bf")], [bkey(sb0)])
                    TT("dve", vnew[sl], B["u_sb"][sl], v4(bank(sb0))[sl], ALU.subtract, [dk("u_sb"), bkey(sb0)], [dk("vnew")])
                    for hh in range(HG):
                        MM(bank(sb0 + 1)[:, hh * 128:(hh + 1) * 128], B["qdTg"][:, hh, :], Sg_bf[:, hh, :], True, False,
                           [dk("qdTg"), dk("Sg_bf")], [bkey(sb0 + 1)])
                        MM(bank(sb0 + 1)[:, hh * 128:(hh + 1) * 128], B["attnT"][:, hh, :], vnew[:, hh, :], False, True,
                           [dk("attnT"), dk("vnew")], [bkey(sb0 + 1)])
                    for hh in range(HG):
                        MM(bank(sb0)[:, hh * 128:(hh + 1) * 128], B["ktl%d" % c][:, hh, :], vnew[:, hh, :], True, True,
                           [dk("ktl%d" % c), dk("vnew")], [bkey(sb0)])
                    TT("pool", Sg, Sg, bc_h(EA[:, 2 + c, n, :]), ALU.mult, [dk("Sg"), ("esc_all", d_)], [dk("Sg")])
                    TT("dve", Sg, Sg, v4(bank(sb0)), ALU.add, [dk("Sg"), bkey(sb0)], [dk("Sg")])
                    CP("act", Sg_bf, Sg, [dk("Sg")], [dk("Sg_bf")])
                    if not second:
                        CP("act", B["ostage"][sl], v4(bank(sb0 + 1))[sl], [bkey(sb0 + 1)], [dk("ostage")])
                    else:
                        TT("dve", osum[sl], v4(bank(sb0 + 1))[sl], oland[n % 2][sl], ALU.add,
                           [bkey(sb0 + 1), ("oland", n % 2)], ["osum"])
                if not second:
                    stored.add(n)
                    DMA("sp", gdn_o[n], B["ostage"].rearrange("p h d -> p (h d)"), ("ost", d_), [dk("ostage")], [("o_dram", n)])
                    return
                osq = fsig.rearrange("p (h d) -> p h d", h=HG)
                TT("pool", osq, osum, osum, ALU.mult, ["osum"], ["fsig"])
                P.op("dve", lambda e: e.tensor_reduce(out=frs[:, 0:HG], in_=osq, axis=AX.X, op=ALU.add), ["fsig"], ["frs"], cost=600.0)
                rstd_inplace(frs[:, 0:HG], 128, "frs")
                for half, ws in enumerate(wsl):
                    for kt in range(KT):
                        MM(bank(half), hT[:, kt, tsl], ws[:, kt, :], kt == 0, kt == KT - 1,
                           [("wsl", half), ("hT", n)], [bkey(half)])
                fGf = fG.rearrange("p h d -> p (h d)")
                for half in range(2):
                    ACTF(fsig, bank(half), AF.Exp, [bkey(half)], ["fsig"], scale=-1.0)
                    ACTF(fsig, fsig, AF.Ln, ["fsig"], ["fsig"], bias=1.0)
                    ACTF(fsig, fsig, AF.Exp, ["fsig"], ["fsig"], scale=-1.0)
                    if half == 0:
                        TT("dve", fGf, fsig, bank(0), ALU.mult, ["fsig", bkey(0)], ["fG"])
                    else:
                        TT("pool", fGf, fGf, fsig, ALU.mult, ["fsig", "fG"], ["fG"])
                TT("pool", fG, fG, bc_m(gdnw_bc), ALU.mult, ["fG", "gdnw_bc"], ["fG"])
                TT("pool", osum, osum, bc_h(frs[:, 0:HG]), ALU.mult, ["osum", "frs"], ["osum"])
                TT("pool", osum, osum, fG, ALU.mult, ["osum", "fG"], ["osum"])
                mslice = mixed[:, n, hs0 * 128:(hs0 + HG) * 128].rearrange("p (h d) -> p h d", h=HG)
                TT("dve", mslice, mslice, osum, ALU.add, ["osum", ("mixed", n)], [("mixed", n)])

            for d_ in range(2):
                MEMSET("pool", DB[d_]["vnew"], 0.0, [("vnew", d_)])
                MEMSET("dve", DB[d_]["Sg"], 0.0, [("Sg", d_)])
                CP("act", DB[d_]["Sg_bf"], DB[d_]["Sg"], [("Sg", d_)], [("Sg_bf", d_)])
            for i in range(NT):
                gdn_tile(0, i)
                gdn_tile(1, NT - 1 - i)
            MARK("d2")
            P.barrier()

        P.barrier()
        wout_d = dram("w_out", [D, D])
        off = 0
        x1, off = carve(off, [NT, D], F32)
        X1_END = off
        mT, off = carve(off, [KT, T], BF16)
        wout, off = carve(off, [KT, D], BF16)
        hn2 = [None, None]
        hn2[0], off = carve(off, [D], BF16)
        hn2[1], off = carve(off, [D], BF16)
        junk, off = carve(off, [D], BF16)
        n2_bc, off = carve(off, [D], F32)
        DMA("sp", n2_bc, n2_d.partition_broadcast(128), "c_n2", [], ["n2_bc"])
        DMA("pool", wout, wout_d.rearrange("(k p) c -> p k c", p=128), "w_wout", [], ["wout"])
        for n in range(NT):
            b = n % 2
            for kt in range(KT):
                TR(bbank(b)[:, kt * 128:(kt + 1) * 128], mixed[:, n, kt * 128:(kt + 1) * 128], ident_b[:],
                   [("mixed", n), "ident_b"], [("pbb", b)])
            CP("act", mT[:, :, n * 128:(n + 1) * 128], bbank(b).rearrange("p (k t) -> p k t", k=KT), [("pbb", b)], [("mT", n)])
        for n in range(NT):
            tsl = slice(n * 128, (n + 1) * 128)
            DMA("sp", x1[:, n, :], x_d[tsl, :], ("x1ld", n % 4), [], [("x1", n)])
            pp = pt[n % 2]
            for half in range(2):
                for kt in range(KT):
                    MM(pp[:, half * 512:(half + 1) * 512], mT[:, kt, tsl], wout[:, kt, half * 512:(half + 1) * 512],
                       kt == 0, kt == KT - 1, [("mT", n), "wout"], [bkey((n % 2) * 2 + half)])
            TT("dve", x1[:, n, :], x1[:, n, :], pp[:, :], ALU.add, [("x1", n), bkey((n % 2) * 2), bkey((n % 2) * 2 + 1)], [("x1", n)])
            b = n % 2
            ssap = small[:, 8 + b:9 + b]
            ssk = ("ss2", b)
            ACTF(junk, x1[:, n, :], AF.Square, [("x1", n)], ["junk", ssk], accum_out=ssap)
            rstd_inplace(ssap, D, ssk)
            STT(mixed[:, n, :], x1[:, n, :], ssap, n2_bc, ALU.mult, ALU.mult, [("x1", n), ssk, "n2_bc"], [("mixed", n)])
            for kt in range(KT):
                TR(bbank(b)[:, kt * 128:(kt + 1) * 128], mixed[:, n, kt * 128:(kt + 1) * 128], ident_b[:],
                   [("mixed", n), "ident_b"], [("pbb", b)])
            CP("act", hT[:, :, tsl], bbank(b).rearrange("p (k t) -> p k t", k=KT), [("pbb", b)], [("hT", n)])
        MARK("e0")

        P.barrier()
        NB = 64
        wr_d = dram("moe_wr", [D, 36])
        wgu0_d = dram("moe_wgu0", [4096, 2048])
        wgu1_d = dram("moe_wgu1", [4096, 2048])
        wdr_d = dram("moe_wdr", [4096, 2048])
        xb_d = nc.dram_tensor("moe_xb", [NB * 128, D], BF16, kind="Internal").ap()
        yb_d = nc.dram_tensor("moe_yb", [NB * 128, D], F32, kind="Internal").ap()
        off = X1_END
        stg = []
        for i in range(3):
            a, off = carve(off, [2048], F32)
            stg.append(a)
        wgu_bf = []
        wd_bf = []
        for i in range(2):
            a, off = carve(off, [KT, 512], BF16)
            wgu_bf.append(a)
            a, off = carve(off, [2, D], BF16)
            wd_bf.append(a)
        wr, off = carve(off, [KT, 36], BF16)
        lg, off = carve(off, [NT, 36], F32)
        oh1, off = carve(off, [NT, 32], F32)
        oh2, off = carve(off, [NT, 32], F32)
        msk, off = carve(off, [NT, 32], F32)
        rank, off = carve(off, [NT, 32], F32)
        tmp3, off = carve(off, [NT, 32], F32)
        gtmp, off = carve(off, [NT, 4], F32)
        ohg, off = carve(off, [NT, 4], F32)
        rv, off = carve(off, [8, NT], F32)
        mcum, off = carve(off, [32], F32)
        cnt, off = carve(off, [32], F32)
        padded, off = carve(off, [32], F32)
        ends, off = carve(off, [32], F32)
        pstart, off = carve(off, [32], F32)
        ebf, off = carve(off, [NB], F32)
        widx_f, off = carve(off, [NB], F32)
        widx, off = carve(off, [NB], I32)
        dest_f, off = carve(off, [2, NT], F32)
        dest_i, off = carve(off, [2, NT], I32)
        MOE_END = off
        cmpb = stg[0].rearrange("p (b e) -> p b e", b=NB)
        cmpj = stg[1][:, 0:512].rearrange("p (e j) -> p e j", e=32)
        ht32 = hT[:].rearrange("p k t -> p (k t)").bitcast(F32)
        HTCAP = 32 * 1024
        hoff = 0
        xg, xgT, sil, hid_bf, hidT, ysb = [], [], [], [], [], []
        for i in range(2):
            a, hoff = carve(hoff, [D], BF16, ht32, HTCAP); xg.append(a)
            a, hoff = carve(hoff, [KT, 128], BF16, ht32, HTCAP); xgT.append(a)
            a, hoff = carve(hoff, [256], F32, ht32, HTCAP); sil.append(a)
            a, hoff = carve(hoff, [256], BF16, ht32, HTCAP); hid_bf.append(a)
            a, hoff = carve(hoff, [2, 128], BF16, ht32, HTCAP); hidT.append(a)
            a, hoff = carve(hoff, [D], F32, ht32, HTCAP); ysb.append(a)
        mix32 = mixed[:].rearrange("p n d -> p (n d)").bitcast(F32)
        MIXCAP = 32 * 1024
        moff = 0
        yg = []
        for i in range(2):
            a, moff = carve(moff, [D], F32, mix32, MIXCAP); yg.append(a)
        stgB = []
        for i in range(3):
            a, moff = carve(moff, [2048], F32, mix32, MIXCAP); stgB.append(a)

        DMA("pool", wr, wr_d.rearrange("(k p) c -> p k c", p=128), "w_wr", [], ["wr"])
        for n in range(NT):
            bi = n % 2
            for kt in range(KT):
                MM(bank(bi)[:, 0:36], hT[:, kt, n * 128:(n + 1) * 128], wr[:, kt, :], kt == 0, kt == KT - 1,
                   ["wr", ("hT", n)], [bkey(bi)])
            CP("act", lg[:, n, :], bank(bi)[:, 0:36], [bkey(bi)], ["lg"])
        BIG = 10000.0
        glv = lg[:, :, 0:4]
        elv = lg[:, :, 4:36]

        def RED(out, in_, op, R, W):
            P.op("dve", lambda e: e.tensor_reduce(out=out, in_=in_, axis=AX.X, op=op), R, W, cost=100.0 + _fsz(in_) * 1.0)

        def bcn(ap2, k):
            return ap2.unsqueeze(2).to_broadcast([128, NT, k])

        gmax, gsum, m1, m2, w1, w2 = (rv[:, i, :] for i in range(6))
        RED(gmax, glv, ALU.max, ["lg"], ["rv"])
        TT("dve", ohg, glv, bcn(gmax, 4), ALU.is_equal, ["lg", "rv"], ["ohg"])
        TT("dve", gtmp, glv, bcn(gmax, 4), ALU.subtract, ["lg", "rv"], ["gtmp"])
        ACTF(gtmp, gtmp, AF.Exp, ["gtmp"], ["gtmp"])
        RED(gsum, gtmp, ALU.add, ["gtmp"], ["rv"])
        RECIP(gsum, gsum, ["rv"], ["rv"])
        TS("dve", ohg, ohg, BIG, -BIG, ALU.mult, ALU.add, ["ohg"], ["ohg"])
        TT("dve", msk.rearrange("p n (g e) -> p n g e", g=4), elv.rearrange("p n (g e) -> p n g e", g=4),
           ohg.unsqueeze(3).to_broadcast([128, NT, 4, 8]), ALU.add, ["lg", "ohg"], ["msk"])
        RED(m1, msk, ALU.max, ["msk"], ["rv"])
        TT("dve", oh1, msk, bcn(m1, 32), ALU.is_equal, ["msk", "rv"], ["oh1"])
        STT(msk, oh1, -BIG, msk, ALU.mult, ALU.add, ["oh1", "msk"], ["msk"])
        RED(m2, msk, ALU.max, ["msk"], ["rv"])
        TT("dve", oh2, msk, bcn(m2, 32), ALU.is_equal, ["msk", "rv"], ["oh2"])
        TT("dve", w2, m2, m1, ALU.subtract, ["rv"], ["rv"])
        ACTF(w2, w2, AF.Exp, ["rv"], ["rv"])
        TS("dve", w1, w2, 1.0, None, ALU.add, ALU.bypass, ["rv"], ["rv"])
        RECIP(w1, w1, ["rv"], ["rv"])
        TT("dve", w1, w1, gsum, ALU.mult, ["rv"], ["rv"])
        TT("dve", w2, w2, w1, ALU.mult, ["rv"], ["rv"])
        TT("dve", msk, oh1, oh2, ALU.add, ["oh1", "oh2", "msk"], ["msk"])
        MEMSET("dve", mcum, 0.0, ["mcum"])
        for n in range(NT):
            bi = n % 2
            MM(bank(bi)[:, 0:32], cst("m_lt"), msk[:, n, :], True, False, ["consts", "msk"], [bkey(bi)])
            MM(bank(bi)[:, 0:32], cst("ones"), mcum, False, True, ["consts", "mcum"], [bkey(bi)])
            CP("act", rank[:, n, :], bank(bi)[:, 0:32], [bkey(bi)], ["rank"])
            TT("dve", mcum, mcum, msk[:, n, :], ALU.add, ["mcum", "msk"], ["mcum"])
        MM(bank(0)[:, 0:32], cst("ones"), mcum, True, True, ["consts", "mcum"], [bkey(0)])
        CP("act", cnt, bank(0)[:, 0:32], [bkey(0)], ["cnt"])
        TT("dve", cmpj, cnt.unsqueeze(2).to_broadcast([128, 32, 16]),
           cst("bvals")[:, 0:16].unsqueeze(1).to_broadcast([128, 32, 16]), ALU.is_gt, ["cnt", "consts"], [("stg", 1)])
        RED(padded, cmpj, ALU.add, [("stg", 1)], ["padded"])
        TS("dve", padded, padded, 128.0, None, ALU.mult, ALU.bypass, ["padded"], ["padded"])
        P.op("dve", lambda e: e.tensor_tensor_scan(out=ends, data0=cst("ones")[:, 0:32], data1=padded, initial=0.0,
                                                  op0=ALU.mult, op1=ALU.add), ["consts", "padded"], ["ends"], cost=300.0)
        TT("dve", pstart, ends, padded, ALU.subtract, ["ends", "padded"], ["pstart"])
        TT("dve", rank, rank, pstart.unsqueeze(1).to_broadcast([128, NT, 32]), ALU.add, ["rank", "pstart"], ["rank"])
        for k, ohk in ((0, oh1), (1, oh2)):
            TT("dve", tmp3, ohk, rank, ALU.mult, ["oh1", "oh2", "rank"], ["tmp3"])
            RED(dest_f[:, k, :], tmp3, ALU.add, ["tmp3"], ["dest_f"])
        CP("dve", dest_i, dest_f, ["dest_f"], ["dest_i"])
        TT("dve", cmpb, ends.unsqueeze(1).to_broadcast([128, NB, 32]),
           cst("bvals")[:, 0:NB].unsqueeze(2).to_broadcast([128, NB, 32]), ALU.is_le, ["ends", "consts"], [("stg", 0)])
        RED(ebf, cmpb, ALU.add, [("stg", 0)], ["ebf"])
        STT(widx_f, ebf, 128.0, cst("pidx")[:, 0:NB], ALU.mult, ALU.add, ["ebf", "consts"], ["widx_f"])
        CP("dve", widx, widx_f, ["widx_f"], ["widx"])
        MARK("e1")

        IOA = bass.IndirectOffsetOnAxis
        regs = {}

        def _pool_init(e):
            regs["bc"] = e.alloc_register("moe_bc")
            e.reg_mov(regs["bc"], 4095)
        P.pool_init = _pool_init
        XB_KEYS = []
        zt, off = carve(off, [D], BF16)
        MEMSET("pool", zt, 0.0, ["zt"])
        DMA("sp", xb_d.rearrange("(p r) d -> p r d", p=128), zt.unsqueeze(1).to_broadcast([128, NB, D]), "xbz", ["zt"], ["xb0"])
        for n in range(NT):
            for k in range(2):
                idx_ap = dest_i[:, k, n:n + 1]
                src_ap = mixed[:, n, :]
                P.dma("pool", lambda e, idx_ap=idx_ap, src_ap=src_ap: e.indirect_dma_start(
                    out=xb_d[:, :], out_offset=IOA(ap=idx_ap, axis=0), in_=src_ap, in_offset=None),
                    ("sc", (2 * n + k) % 4), [("mixed", n), "dest_i", "xb0"], [("xb", n, k)], nbytes=256 * 1024)
                XB_KEYS.append(("xb", n, k))

        def gather_w(dst, src_d, b, skey, extra):
            idx_ap = widx[:, b:b + 1]
            P.dma("pool", lambda e: e.indirect_dma_start(
                out=dst, out_offset=None, in_=src_d[:, :], in_offset=IOA(ap=idx_ap, axis=0),
                bounds_check=regs["bc"], oob_is_err=False),
                skey, ["widx"] + extra, [skey], nbytes=1 << 20)

        YB_KEYS = []
        for b in range(NB):
            s = b % 2
            sset = stg if b % 2 == 0 else stgB
            so = 0 if b % 2 == 0 else 3
            extra = [] if b % 2 == 0 else XB_KEYS
            gather_w(sset[0], wgu0_d, b, ("stg", so + 0), extra)
            gather_w(sset[1], wgu1_d, b, ("stg", so + 1), extra)
            gather_w(sset[2], wdr_d, b, ("stg", so + 2), extra)
            CP("act", wgu_bf[s][:, 0:4, :], sset[0].rearrange("p (k c) -> p k c", k=4), [("stg", so + 0)], [("wgu_bf", s, 0)])
            CP("dve", wgu_bf[s][:, 4:8, :], sset[1].rearrange("p (k c) -> p k c", k=4), [("stg", so + 1)], [("wgu_bf", s, 1)])
            CP("act" if b % 4 < 2 else "dve", wd_bf[s], sset[2].rearrange("p (k c) -> p k c", k=2), [("stg", so + 2)], [("wd_bf", s)])
            DMA("sp", xg[s], xb_d[b * 128:(b + 1) * 128, :], ("xg", s), XB_KEYS, [("xg", s)])
            for kt in range(KT):
                TR(bbank(s)[:, kt * 128:(kt + 1) * 128], xg[s][:, kt * 128:(kt + 1) * 128], ident_b[:],
                   [("xg", s), "ident_b"], [("pbb", s)])
            CP("act", xgT[s], bbank(s).rearrange("p (k t) -> p k t", k=KT), [("pbb", s)], [("xgT", s)])
            hb = bank(s)
            for kt in range(KT):
                MM(hb, xgT[s][:, kt, :], wgu_bf[s][:, kt, :], kt == 0, kt == KT - 1,
                   [("xgT", s), ("wgu_bf", s, 0), ("wgu_bf", s, 1)], [bkey(s)])
            ACTF(sil[s], hb[:, 0:256], AF.Silu, [bkey(s)], [("sil", s)])
            TT("dve", hid_bf[s], sil[s], hb[:, 256:512], ALU.mult, [("sil", s), bkey(s)], [("hid_bf", s)])
            for ft in range(2):
                TR(bbank(s)[:, ft * 128:(ft + 1) * 128], hid_bf[s][:, ft * 128:(ft + 1) * 128], ident_b[:],
                   [("hid_bf", s), "ident_b"], [("pbb", s)])
            CP("act", hidT[s], bbank(s)[:, 0:256].rearrange("p (k t) -> p k t", k=2), [("pbb", s)], [("hidT", s)])
            yp = pt[1 + s]
            for half in range(2):
                for ft in range(2):
                    MM(yp[:, half * 512:(half + 1) * 512], hidT[s][:, ft, :], wd_bf[s][:, ft, half * 512:(half + 1) * 512],
                       ft == 0, ft == 1, [("hidT", s), ("wd_bf", s)], [bkey(2 + 2 * s + half)])
            CP("act" if b % 2 else "dve", ysb[s], yp[:, :], [bkey(2 + 2 * s), bkey(3 + 2 * s)], [("ysb", s)])
            DMA("sp", yb_d[b * 128:(b + 1) * 128, :], ysb[s], ("yst", s), [("ysb", s)], [("yb", b)])
            YB_KEYS.append(("yb", b))
        MARK("e2")
        ygs = list(yg)
        for sb_ in stgB:
            ygs.append(sb_[:, 0:1024])
            ygs.append(sb_[:, 1024:2048])
        for n in range(NT):
            for k in range(2):
                s = (2 * n + k) % len(ygs)
                idx_ap = dest_i[:, k, n:n + 1]
                dst = ygs[s]
                P.dma("pool", lambda e, idx_ap=idx_ap, dst=dst: e.indirect_dma_start(
                    out=dst, out_offset=None, in_=yb_d[:, :], in_offset=IOA(ap=idx_ap, axis=0)),
                    ("yg", s), YB_KEYS + ["dest_i"], [("yg", s)], nbytes=512 * 1024)
                wk = rv[:, 4 + k, n:n + 1]
                STT(x1[:, n, :], ygs[s], wk, x1[:, n, :], ALU.mult, ALU.add, [("yg", s), "rv", ("x1", n)], [("x1", n)])

        P.barrier()
        off = X1_END
        nf_bc, off = carve(off, [D], F32)
        ob = [None, None]
        ob[0], off = carve(off, [D], F32)
        ob[1], off = carve(off, [D], F32)
        junk2, off = carve(off, [D], BF16)
        DMA("sp", nf_bc, nf_d.partition_broadcast(128), "c_nf", [], ["nf_bc"])
        for n in range(NT):
            b = n % 2
            ssap = small[:, 12 + b:13 + b]
            ssk = ("ss3", b)
            ACTF(junk2, x1[:, n, :], AF.Square, [("x1", n)], ["junk2", ssk], accum_out=ssap)
            rstd_inplace(ssap, D, ssk)
            STT(ob[b], x1[:, n, :], ssap, nf_bc, ALU.mult, ALU.mult, [("x1", n), ssk, "nf_bc"], [("ob", b)])
            DMA("sp", out_d[n * 128:(n + 1) * 128, :], ob[b], ("out_st", b), [("ob", b)], [("out", n)])
        if not dbg:
            P.wait_all("sp", [("out", n) for n in range(NT)])
        if dbg:
            P.enabled = True
            P.barrier()
            for n in range(NT):
                DMA("sp", dbg_d[n * 128:(n + 1) * 128, :], x1[:, n, :], ("dbg_out", n % 2), [("x1", n)], [("dbg", n)])
            P.wait_all("sp", [("dbg", n) for n in range(NT)] + [("out", n) for n in range(NT)])
        P.emit()
    return nc


def make_in_maps(inputs, n_cores=8):
    f = lambda k: np.asarray(inputs[k], np.float32)
    x = f("x")
    _gu = np.concatenate([f("moe_w_gate")[0], f("moe_w_up")[0]], axis=2).reshape(32, 8, 128, 512).transpose(0, 2, 1, 3)
    shared = {
        "norm1_w": f("norm1_w").reshape(1, D),
        "norm2_w": f("norm2_w").reshape(1, D),
        "norm_f_w": f("norm_f_w").reshape(1, D),
        "consts": CONST_ARR,
        "w_in": np.ascontiguousarray(f("w_in")[0]),
        "gla_w2b_f": np.ascontiguousarray(np.concatenate([f("gla_gate_w2_fwd")[0], f("gla_gate_b_fwd")], axis=0)),
        "gla_w2b_b": np.ascontiguousarray(np.concatenate([f("gla_gate_w2_bwd")[0], f("gla_gate_b_bwd")], axis=0)),
        "gla_norm_w": f("gla_norm_w").reshape(1, 256),
        "w_out": np.ascontiguousarray(f("w_out")[0]),
        "moe_wr": np.ascontiguousarray(np.concatenate([f("moe_w_group")[0], f("moe_w_router")[0]], axis=1)),
        "moe_wgu0": _gu[:, :, 0:4, :].reshape(4096, 2048).copy(),
        "moe_wgu1": _gu[:, :, 4:8, :].reshape(4096, 2048).copy(),
        "moe_wdr": np.ascontiguousarray(f("moe_w_down")[0].reshape(32, 2, 128, 1024).transpose(0, 2, 1, 3)).reshape(4096, 2048),
        "gdn_norm_w": f("gdn_norm_w").reshape(1, 128),
        "gdn_vec": np.ascontiguousarray(np.concatenate([f("gdn_dt_bias_fwd")[0], f("gdn_dt_bias_bwd")[0],
                                                        f("gdn_a_log_fwd")[0], f("gdn_a_log_bwd")[0]]).reshape(1, 32)),
        "gdn_conv_wT": np.ascontiguousarray(f("gdn_conv_w")[0].T.reshape(24, 128, 5).transpose(1, 0, 2)),
    }
    maps = []
    for c in range(n_cores):
        m = dict(shared)
        m["x"] = np.ascontiguousarray(x[c])
        maps.append(m)
    return maps


def kernel(**inputs):
    nc = build()
    in_maps = make_in_maps(inputs)
    res = run_bass_kernel_spmd(nc, in_maps, core_ids=list(range(8)))
    out = np.stack([np.asarray(r["out"]) for r in res.results], axis=0)
    return out.astype(np.float32)
```

```python
import contextlib
import heapq
import numpy as np
import concourse.bass as bass
import concourse.mybir as mybir
from concourse.bass_utils import run_bass_kernel_spmd

F32 = mybir.dt.float32
BF16 = mybir.dt.bfloat16
I32 = mybir.dt.int32
AF = mybir.ActivationFunctionType
ALU = mybir.AluOpType
AX = mybir.AxisListType

T = 2048
D = 1024
NT = T // 128
KT = D // 128
EPS = 1e-6
SAME_ENGINE_SYNC = True
EPOCH = 20000
SYNC_NS = 250.0
DMA_LAT_NS = 2200.0


class Prog:
    ENGS = ("pe", "act", "dve", "pool", "sp")

    def __init__(self, nc, stack):
        self.nc = nc
        self.stack = stack
        self.streams = {e: [] for e in self.ENGS}
        self.count = {e: 0 for e in self.ENGS}
        self.esems = {e: [] for e in self.ENGS}
        self.known = {e: {} for e in self.ENGS}
        self.last_write = {}
        self.readers = {}
        self.dma_sems = {}
        self.dma_vals = {}
        self.dma_last = {}
        self.enabled = True
        self.seg = []
        self.ticks = {}
        self.nops = 0
        self.seg_base = 0
        self.pool_init = None

    def _new_sem(self, name):
        return self.stack.enter_context(self.nc.semaphore(name))

    @staticmethod
    def _psum_fix(reads, writes):
        r2, w2 = [], list(writes)
        for k in reads:
            if isinstance(k, tuple) and k[0] in ("pb", "pbb"):
                if k not in w2:
                    w2.append(k)
            else:
                r2.append(k)
        return r2, w2

    def _record(self, eng, fn, reads, writes, cost, kind, semkey=None):
        reads, writes = self._psum_fix(list(reads), list(writes))
        oid = self.nops
        self.nops += 1
        preds = set()
        for r in reads:
            t = self.last_write.get(r)
            if t is not None:
                preds.add(t)
        for w in writes:
            t = self.last_write.get(w)
            if t is not None:
                preds.add(t)
            preds.update(self.readers.get(w, ()))
        if kind == "dma":
            prev = self.dma_last.get(semkey)
            if prev is not None:
                preds.add(prev)
            self.dma_last[semkey] = oid
        preds = {p for p in preds if p >= self.seg_base}
        self.seg.append(dict(id=oid, eng=eng, fn=fn, preds=preds, cost=float(cost), kind=kind, semkey=semkey))
        for w in writes:
            self.last_write[w] = oid
            self.readers[w] = []
        for r in reads:
            self.readers.setdefault(r, []).append(oid)
        return oid

    def op(self, eng, fn, reads=(), writes=(), cost=300.0):
        if not self.enabled:
            return
        self._record(eng, fn, reads, writes, cost, "op")

    def dma(self, eng, fn, semkey, reads=(), writes=(), nbytes=1 << 20):
        if not self.enabled:
            return
        self._record(eng, fn, reads, writes, DMA_LAT_NS + nbytes / 160.0, "dma", semkey)

    def wait_all(self, eng, keys):
        self._record(eng, None, list(keys), [], 0.0, "op")

    def _schedule_segment(self):
        ops = self.seg
        if not ops:
            return
        byid = {o["id"]: o for o in ops}
        succ = {o["id"]: [] for o in ops}
        indeg = {}
        for o in ops:
            indeg[o["id"]] = len(o["preds"])
            for p in o["preds"]:
                succ[p].append(o["id"])
        ready_t = {o["id"]: 0.0 for o in ops}
        finish = {}
        heaps = {e: [] for e in self.ENGS}
        for o in ops:
            if indeg[o["id"]] == 0:
                heapq.heappush(heaps[o["eng"]], (0.0, o["id"]))
        etime = {e: 0.0 for e in self.ENGS}
        order = {e: [] for e in self.ENGS}
        remaining = len(ops)
        while remaining:
            best = None
            for e in self.ENGS:
                h = heaps[e]
                if not h:
                    continue
                rt, oid = h[0]
                st = max(rt, etime[e])
                if best is None or (st, oid) < (best[0], best[1]):
                    best = (st, oid, e)
            st, oid, e = best
            heapq.heappop(heaps[e])
            o = byid[oid]
            if o["kind"] == "dma":
                etime[e] = st + 150.0
                fin = st + o["cost"]
            else:
                etime[e] = st + o["cost"]
                fin = etime[e]
            finish[oid] = fin
            order[e].append(o)
            remaining -= 1
            for s in succ[oid]:
                so = byid[s]
                lat = SYNC_NS if (so["eng"] != e or o["kind"] == "dma") else (60.0 if e != "pe" else 0.0)
                ready_t[s] = max(ready_t[s], fin + lat)
                indeg[s] -= 1
                if indeg[s] == 0:
                    heapq.heappush(heaps[so["eng"]], (ready_t[s], s))
        self.est_ns = getattr(self, "est_ns", 0.0) + max(list(finish.values()) + [0.0])
        for o in ops:
            if o["kind"] == "dma":
                k = o["semkey"]
                if k not in self.dma_sems:
                    self.dma_sems[k] = self._new_sem(f"d{len(self.dma_sems)}")
                    self.dma_vals[k] = 0
                self.dma_vals[k] += 16
                self.ticks[o["id"]] = (self.dma_sems[k], self.dma_vals[k], "dma")
        def needs_sem(o):
            for s_ in succ[o["id"]]:
                se = byid[s_]["eng"]
                if se != o["eng"] or (SAME_ENGINE_SYNC and se != "pe"):
                    return True
            return False
        for e in self.ENGS:
            real = [o for o in order[e] if o["kind"] == "op" and o["fn"] is not None]
            for i_, o in enumerate(real):
                o["sig"] = needs_sem(o) or i_ == len(real) - 1
        for e in self.ENGS:
            for o in order[e]:
                if o["kind"] == "op" and o["fn"] is not None and o["sig"]:
                    c = self.count[e]
                    ep, v = divmod(c, EPOCH)
                    while len(self.esems[e]) <= ep:
                        self.esems[e].append(self._new_sem(f"s_{e}_{len(self.esems[e])}"))
                    self.count[e] = c + 1
                    self.ticks[o["id"]] = (self.esems[e][ep], v + 1, e)
        for e in self.ENGS:
            for o in order[e]:
                waits = {}
                for p in o["preds"]:
                    if byid[p]["eng"] == e and byid[p]["kind"] == "op" and (not SAME_ENGINE_SYNC or e == "pe"):
                        continue
                    sem, val, src = self.ticks[p]
                    sid = id(sem)
                    if self.known[e].get(sid, 0) >= val:
                        continue
                    if sid not in waits or waits[sid][1] < val:
                        waits[sid] = (sem, val)
                for sid, (sem, val) in waits.items():
                    self.known[e][sid] = val
                inc = None
                if o["fn"] is not None and o["id"] in self.ticks:
                    sem, val, src = self.ticks[o["id"]]
                    inc = (sem, 16 if o["kind"] == "dma" else 1)
                self.streams[e].append((o["fn"], list(waits.values()), inc))
        self.seg = []
        self.seg_base = self.nops

    def barrier(self):
        if not self.enabled and not self.seg:
            return
        self._schedule_segment()
        ticks = []
        for e2 in self.ENGS:
            c = self.count[e2]
            if c > 0:
                ep, v = divmod(c - 1, EPOCH)
                ticks.append((self.esems[e2][ep], v + 1))
        for k, sem in self.dma_sems.items():
            ticks.append((sem, self.dma_vals[k]))
        for eng in self.ENGS:
            waits = []
            for (sem, val) in ticks:
                if self.known[eng].get(id(sem), 0) >= val:
                    continue
                self.known[eng][id(sem)] = val
                waits.append((sem, val))
            if waits:
                self.streams[eng].append((None, waits, None))

    def emit(self):
        self._schedule_segment()
        nc = self.nc
        with nc.Block() as block:
            def run(e, stream):
                for fn, waits, inc in stream:
                    for sem, val in waits:
                        e.wait_ge(sem, val)
                    if fn is None:
                        continue
                    ins = fn(e)
                    if inc is not None:
                        ins.then_inc(inc[0], inc[1])

            @block.tensor
            def _(e):
                run(e, self.streams["pe"])

            @block.scalar
            def _(e):
                run(e, self.streams["act"])

            @block.vector
            def _(e):
                run(e, self.streams["dve"])

            @block.gpsimd
            def _(e):
                if self.pool_init is not None:
                    self.pool_init(e)
                run(e, self.streams["pool"])

            @block.sync
            def _(e):
                run(e, self.streams["sp"])


def _fsz(ap):
    s = ap.shape
    n = 1
    for v in s[1:]:
        n *= int(v)
    return n


C_GQ, C_GK, C_GV, C_GR = 0, 512, 1024, 2048
C_GLF, C_GLB = 3072, 3088
C_DQ, C_DK, C_DV, C_DZ = 3104, 4128, 5152, 6176
C_DAB = 7200
C_MA, C_MB = 7232, 8256
D_IN = 9280


def host_consts():
    r = np.arange(128)[:, None]
    t = np.arange(128)[None, :]
    same = (r // 64) == (t // 64)
    c = {}
    c["ident"] = np.eye(128, dtype=np.float32)
    c["a_le"] = np.where(r <= t, -1.0 / 16, 0.0)
    c["a_ge"] = np.where(r >= t, -1.0 / 16, 0.0)
    c["a_gt"] = np.where(r > t, -1.0 / 16, 0.0)
    c["a_lt"] = np.where(r < t, -1.0 / 16, 0.0)
    c["m_le"] = np.where(r <= t, 1.0, 0.0)
    c["m_ge"] = np.where(r >= t, 1.0, 0.0)
    c["b_le"] = np.where((r <= t) & same, 1.0, 0.0)
    c["b_ge"] = np.where((r >= t) & same, 1.0, 0.0)
    c["b_gt"] = np.where((r > t) & same, 1.0, 0.0)
    c["b_lt"] = np.where((r < t) & same, 1.0, 0.0)
    c["csel0"] = np.where(r < 64, 1.0, 0.0) + 0.0 * t
    c["csel1"] = np.where(r >= 64, 1.0, 0.0) + 0.0 * t
    c["ones"] = np.ones((128, 128))
    c["m_lt"] = np.where(r < t, 1.0, 0.0)
    c["bvals"] = 128.0 * t + 0.0 * r
    c["pidx"] = 1.0 * r + 0.0 * t
    names = list(c.keys())
    arr = np.stack([np.asarray(c[n], np.float32) for n in names], axis=1)
    return names, np.ascontiguousarray(arr)


CONST_NAMES, CONST_ARR = host_consts()
NCONST = len(CONST_NAMES)


COST = dict(pe_a=40.0, pe_b=0.35, pe_f32=3.0, tr=110.0, act_a=200.0, act_b=1.2, dve_a=100.0, dve_b=0.8,
            pool_a=150.0, pool_b=2.4)


def build(stage="all", dbg=False):
    nc = bass.Bass("TRN2", target_bir_lowering=False)
    stack = contextlib.ExitStack()
    with stack:
        P = Prog(nc, stack)

        def dram(name, shape, dt=F32, kind="ExternalInput"):
            return nc.dram_tensor(name, list(shape), dt, kind=kind).ap()

        def sb(name, shape, dt=F32):
            return stack.enter_context(nc.sbuf_tensor(name, list(shape), dt))

        def ps(name, shape, dt=F32):
            return stack.enter_context(nc.psum_tensor(name, list(shape), dt))

        def MM(out, lhsT, rhs, start, stop, R, W):
            n = _fsz(rhs)
            c = COST["pe_a"] + n * COST["pe_b"]
            if rhs.dtype == F32:
                c *= COST["pe_f32"]
            P.op("pe", lambda e: e.matmul(out, lhsT, rhs, start=start, stop=stop), R, W, cost=c)

        def TR(out, in_, ident, R, W):
            P.op("pe", lambda e: e.transpose(out=out, in_=in_, identity=ident), R, W, cost=COST["tr"])

        def ACTF(out, in_, func, R, W, **kw):
            c = COST["act_a"] + _fsz(in_) * COST["act_b"] + (90.0 if "accum_out" in kw else 0.0)
            P.op("act", lambda e: e.activation(out=out, in_=in_, func=func, **kw), R, W, cost=c)

        def _vc(eng, n, k=1.5):
            return (COST["dve_a"] + n * k * COST["dve_b"]) if eng == "dve" else (COST["pool_a"] + n * COST["pool_b"])

        def TT(eng, out, in0, in1, op, R, W):
            P.op(eng, lambda e: e.tensor_tensor(out=out, in0=in0, in1=in1, op=op), R, W, cost=_vc(eng, _fsz(out)))

        def TS(eng, out, in0, s1, s2, op0, op1, R, W):
            P.op(eng, lambda e: e.tensor_scalar(out=out, in0=in0, scalar1=s1, scalar2=s2, op0=op0, op1=op1), R, W,
                 cost=_vc(eng, _fsz(out), 1.05))

        def STT(out, in0, scalar, in1, op0, op1, R, W):
            P.op("dve", lambda e: e.scalar_tensor_tensor(out=out, in0=in0, scalar=scalar, in1=in1, op0=op0, op1=op1), R, W,
                 cost=_vc("dve", _fsz(out)))

        def CP(eng, out, in_, R, W):
            if eng == "act":
                P.op("act", lambda e: e.activation(out=out, in_=in_, func=AF.Copy), R, W, cost=COST["act_a"] + _fsz(in_) * COST["act_b"])
            else:
                P.op(eng, lambda e: e.tensor_copy(out=out, in_=in_), R, W, cost=_vc(eng, _fsz(out), 1.05))

        def MEMSET(eng, ap, val, W):
            P.op(eng, lambda e: e.memset(ap, val), [], W, cost=_vc(eng, _fsz(ap), 0.6))

        def DMA(eng, out, in_, semkey, R, W):
            P.dma(eng, lambda e: e.dma_start(out=out, in_=in_), semkey, R, W, nbytes=_fsz(out) * int(out.shape[0]) * 4)

        def RECIP(out, in_, R, W):
            P.op("dve", lambda e: e.reciprocal(out=out, in_=in_), R, W, cost=_vc("dve", _fsz(out), 1.05))

        def MARK(name):
            if stage == name:
                P.enabled = False

        def rstd_inplace(ap, n, key):
            TS("dve", ap, ap, 1.0 / n, EPS, ALU.mult, ALU.add, [key], [key])
            ACTF(ap, ap, AF.Ln, [key], [key])
            ACTF(ap, ap, AF.Exp, [key], [key], scale=-0.5)

        x_d = dram("x", [T, D])
        n1_d = dram("norm1_w", [1, D])
        n2_d = dram("norm2_w", [1, D])
        nf_d = dram("norm_f_w", [1, D])
        consts_d = dram("consts", [128, NCONST, 128])
        w_in_d = dram("w_in", [D, D_IN])
        w2b_d = [dram("gla_w2b_f", [17, 512]), dram("gla_w2b_b", [17, 512])]
        gnw_d = dram("gla_norm_w", [1, 256])
        out_d = dram("out", [T, D], kind="ExternalOutput")
        dbg_d = dram("dbg", [T, D], kind="ExternalOutput") if dbg else None

        consts = sb("consts_sb", [128, NCONST, 128])
        CI = {n: i for i, n in enumerate(CONST_NAMES)}

        def cst(name):
            return consts[:, CI[name], :]

        ident_b = sb("ident_b", [128, 128], BF16)
        ones_b = sb("ones_b", [128, 128], BF16)
        hT = sb("hT", [128, KT, T], BF16)
        mixed = sb("mixed", [128, NT, D], BF16)
        small = sb("small", [128, 64])
        ARENA_BYTES = 134 * 1024
        arena = sb("arena", [128, ARENA_BYTES // 4])

        def carve(off, shape, dt, base=None, cap=None):
            base = arena if base is None else base
            cap = ARENA_BYTES if cap is None else cap
            nb = int(np.prod(shape)) * (2 if dt == BF16 else 4)
            nb = (nb + 3) // 4 * 4
            assert off % 4 == 0 and off + nb <= cap, (off, nb)
            v = base[:, off // 4:(off + nb) // 4]
            if dt != F32:
                v = v.bitcast(dt)
            if len(shape) == 2:
                pat = "p (a b) -> p a b"
                v = v.rearrange(pat, a=shape[0])
            elif len(shape) == 3:
                v = v.rearrange("p (a b c) -> p a b c", a=shape[0], b=shape[1])
            return v, off + nb

        pt = [ps(f"pt{i}", [128, 1024]) for i in range(3)]
        ptb = ps("ptb", [128, 2048], BF16)

        def bank(i):
            return pt[i // 2][:, (i % 2) * 512:(i % 2 + 1) * 512]

        def bkey(i):
            return ("pb", i)

        def bbank(i):
            return ptb[:, i * 1024:(i + 1) * 1024]

        DMA("sp", consts[:], consts_d[:, :, :], "c_consts", [], ["consts"])
        CP("dve", ident_b[:], cst("ident"), ["consts"], ["ident_b"])
        MEMSET("pool", ones_b[:], 1.0, ["ones_b"])

        off = 116 * 1024
        xt0, off = carve(off, [D], F32)
        xt1, off = carve(off, [D], F32)
        hn0, off = carve(off, [D], BF16)
        hn1, off = carve(off, [D], BF16)
        sq, off = carve(off, [D], BF16)
        n1_bc, off = carve(off, [D], F32)
        DMA("sp", n1_bc, n1_d.partition_broadcast(128), "c_n1", [], ["n1_bc"])
        xts = [xt0, xt1]
        hns = [hn0, hn1]
        for tt in range(NT):
            b = tt % 2
            xb, hb = xts[b], hns[b]
            DMA("sp", xb, x_d[tt * 128:(tt + 1) * 128, :], ("xt", b), [], [("xt", b)])
            ACTF(sq, xb, AF.Square, [("xt", b)], ["sq", "ss0"], accum_out=small[:, 0:1])
            rstd_inplace(small[:, 0:1], D, "ss0")
            STT(hb, xb, small[:, 0:1], n1_bc, ALU.mult, ALU.mult, [("xt", b), "ss0", "n1_bc"], [("hn", b)])
            for kt in range(KT):
                TR(bbank(b)[:, kt * 128:(kt + 1) * 128], hb[:, kt * 128:(kt + 1) * 128], ident_b[:],
                   [("hn", b), "ident_b"], [("pbb", b)])
            CP("act", hT[:, :, tt * 128:(tt + 1) * 128], bbank(b).rearrange("p (k t) -> p k t", k=KT),
               [("pbb", b)], [("hT", tt)])
        HT_ALL = [("hT", tt) for tt in range(NT)]
        MARK("p1")

        off = 0
        qT, off = carve(off, [T], F32)
        kT, off = carve(off, [T], F32)
        k_tok, off = carve(off, [NT, 128], F32)
        v_tok, off = carve(off, [NT, 256], BF16)
        qdT = [None, None]
        kiT = [None, None]
        ktail = [None, None]
        for d_ in range(2):
            qdT[d_], off = carve(off, [T], BF16)
            kiT[d_], off = carve(off, [T], BF16)
            ktail[d_], off = carve(off, [NT, 128], BF16)
        sb_store, off = carve(off, [NT, 256], BF16)
        dec, off = carve(off, [2, NT], F32)
        S, off = carve(off, [256], F32)
        S_bf, off = carve(off, [256], BF16)
        NTMP = 3
        tmp = []
        for i in range(NTMP):
            d = {}
            for nm in ("e", "lg", "E", "Ei", "Et"):
                d[nm], off = carve(off, [128], F32)
            d["Pf"], off = carve(off, [128], BF16)
            d["Pb"], off = carve(off, [128], BF16)
            d["sig"], off = carve(off, [512], F32)
            d["G"], off = carve(off, [256], F32)
            tmp.append(d)
        gl, off = carve(off, [2, T], BF16)
        w2b, off = carve(off, [2, 512], BF16)
        wqk, off = carve(off, [KT, 256], BF16)
        wkv, off = carve(off, [KT, 384], BF16)
        wgm, off = carve(off, [KT, 512], BF16)
        wgl, off = carve(off, [KT, 32], BF16)
        gnw_bc, off = carve(off, [256], F32)
        GLA_END = off
        assert GLA_END <= 116 * 1024, GLA_END

        DMA("sp", gnw_bc, gnw_d.partition_broadcast(128), "c_gnw", [], ["gnw_bc"])
        MEMSET("pool", gl[:, :, :], 1.0, ["gl"])
        MEMSET("pool", w2b[:, :, :], 0.0, ["w2b"])
        for d_ in range(2):
            DMA("pool", w2b[0:17, d_, :], w2b_d[d_][:, :], "c_w2b", [], ["w2b"])
        DMA("pool", wgl, w_in_d[:, C_GLF:C_GLF + 32].rearrange("(k p) c -> p k c", p=128), "w_wgl", [], ["wgl"])
        for d_ in range(2):
            for tg in range(4):
                bi = tg % 2
                for kt in range(KT):
                    MM(bank(bi)[0:16, :], wgl[:, kt, d_ * 16:(d_ + 1) * 16], hT[:, kt, tg * 512:(tg + 1) * 512],
                       kt == 0, kt == KT - 1, ["wgl"] + HT_ALL[tg * 4:tg * 4 + 4], [bkey(bi)])
                CP("act", gl[0:16, d_, tg * 512:(tg + 1) * 512], bank(bi)[0:16, :], [bkey(bi)], ["gl"])

        MARK("g0")
        QSCALE = 128.0 ** -0.5
        for h in range(4):
            def wcols(dst, c0, n):
                return (dst, w_in_d[:, c0:c0 + n].rearrange("(k p) c -> p k c", p=128))
            for (dst, src) in (wcols(wqk[:, :, 0:128], C_GQ + h * 128, 128), wcols(wqk[:, :, 128:256], C_GK + h * 128, 128)):
                DMA("pool", dst, src, "w_wqk", [], ["wqk"])
            for (dst, src) in (wcols(wkv[:, :, 0:128], C_GK + h * 128, 128), wcols(wkv[:, :, 128:384], C_GV + h * 256, 256)):
                DMA("pool", dst, src, "w_wkv", [], ["wkv"])
            for (dst, src) in (wcols(wgm[:, :, 0:256], C_GR + h * 256, 256), wcols(wgm[:, :, 256:512], C_MA + h * 256, 256)):
                DMA("pool", dst, src, "w_wgm", [], ["wgm"])
            MARK("g1a")
            for which, dstT in ((0, qT), (1, kT)):
                for tg in range(4):
                    bi = (which * 4 + tg) % 4
                    for kt in range(KT):
                        MM(bank(bi), wqk[:, kt, which * 128:(which + 1) * 128], hT[:, kt, tg * 512:(tg + 1) * 512],
                           kt == 0, kt == KT - 1, ["wqk"] + HT_ALL[tg * 4:tg * 4 + 4], [bkey(bi)])
                    CP("act" if tg % 2 else "dve", dstT[:, tg * 512:(tg + 1) * 512], bank(bi), [bkey(bi)],
                       [("qkT", which, tg)])
            MARK("g1b")
            for n in range(NT):
                bi = 4 + n % 2
                for kt in range(KT):
                    MM(bank(bi)[:, 0:384], hT[:, kt, n * 128:(n + 1) * 128], wkv[:, kt, :],
                       kt == 0, kt == KT - 1, ["wkv", ("hT", n)], [bkey(bi)])
                CP("dve", k_tok[:, n, :], bank(bi)[:, 0:128], [bkey(bi)], [("k_tok", n)])
                CP("act", v_tok[:, n, :], bank(bi)[:, 128:384], [bkey(bi)], [("v_tok", n)])
            MARK("g1")
            for n in range(NT):
                tsl = slice(n * 128, (n + 1) * 128)
                tg = n // 4
                for d_ in range(2):
                    tm = tmp[(n * 2 + d_) % NTMP]
                    tk = ("gtmp", (n * 2 + d_) % NTMP)
                    a_c = cst("a_le") if d_ == 0 else cst("a_ge")
                    a_s = cst("a_gt") if d_ == 0 else cst("a_lt")
                    b0 = (n * 2 + d_) % 2 * 2
                    zb, cb = bank(b0), bank(b0 + 1)
                    MM(zb[:, 0:128], gl[:, d_, tsl], w2b[:, d_, h * 128:(h + 1) * 128], True, True,
                       ["gl", "w2b"], [bkey(b0)])
                    ACTF(tm["e"], zb[:, 0:128], AF.Exp, [bkey(b0)], [tk], scale=-1.0)
                    ACTF(tm["lg"], tm["e"], AF.Ln, [tk], [tk], bias=1.0)
                    MM(cb[:, 0:128], tm["lg"], a_c, True, True, [tk, "consts"], [bkey(b0 + 1)])
                    MM(cb[:, 128:256], a_s, tm["lg"], True, True, [tk, "consts"], [bkey(b0 + 1)])
                    ACTF(tm["E"], cb[:, 0:128], AF.Exp, [bkey(b0 + 1)], [tk])
                    ACTF(tm["Ei"], cb[:, 0:128], AF.Exp, [bkey(b0 + 1)], [tk], scale=-1.0)
                    ACTF(tm["Et"], cb[:, 128:256], AF.Exp, [bkey(b0 + 1)], [tk])
                    STT(qdT[d_][:, tsl], qT[:, tsl], QSCALE, tm["E"], ALU.mult, ALU.mult,
                        [("qkT", 0, tg), tk], [("qdT", d_, n)])
                    TT("dve", kiT[d_][:, tsl], kT[:, tsl], tm["Ei"], ALU.mult, [("qkT", 1, tg), tk], [("kiT", d_, n)])
                    TT("dve", ktail[d_][:, n, :], k_tok[:, n, :], tm["Et"], ALU.mult, [("k_tok", n), tk], [("ktail", d_, n)])
                    col = 127 if d_ == 0 else 0
                    CP("dve", dec[:, d_, n:n + 1], tm["E"][:, col:col + 1], [tk], [("dec", d_, n)])
            MARK("g2")
            MEMSET("dve", S, 0.0, ["S"])
            for n in range(NT - 1, -1, -1):
                CP("act", sb_store[:, n, :], S, ["S"], [("sb_store", n)])
                bi = 4 + n % 2
                MM(bank(bi)[:, 0:256], ktail[1][:, n, :], v_tok[:, n, :], True, True,
                   [("ktail", 1, n), ("v_tok", n)], [bkey(bi)])
                STT(S, S, dec[:, 1, n:n + 1], bank(bi)[:, 0:256], ALU.mult, ALU.add,
                    ["S", ("dec", 1, n), bkey(bi)], ["S"])
            MARK("g3")
            MEMSET("dve", S, 0.0, ["S"])
            for n in range(NT):
                tsl = slice(n * 128, (n + 1) * 128)
                tm = tmp[n % NTMP]
                tk = ("ftmp", n % NTMP)
                CP("act", S_bf, S, ["S"], ["S_bf"])
                b0 = (n % 2) * 2
                sc = bank(b0)
                MM(sc[:, 0:128], kiT[0][:, tsl], qdT[0][:, tsl], True, True, [("kiT", 0, n), ("qdT", 0, n)], [bkey(b0)])
                MM(sc[:, 128:256], kiT[1][:, tsl], qdT[1][:, tsl], True, True, [("kiT", 1, n), ("qdT", 1, n)], [bkey(b0)])
                TT("dve", tm["Pf"], sc[:, 0:128], cst("m_le"), ALU.mult, [bkey(b0), "consts"], [tk])
                TT("dve", tm["Pb"], sc[:, 128:256], cst("m_ge"), ALU.mult, [bkey(b0), "consts"], [tk])
                ob = bank(b0 + 1)
                ok = bkey(b0 + 1)
                MM(ob[:, 0:256], qdT[0][:, tsl], S_bf, True, False, [("qdT", 0, n), "S_bf"], [ok])
                MM(ob[:, 0:256], qdT[1][:, tsl], sb_store[:, n, :], False, False, [("qdT", 1, n), ("sb_store", n)], [ok])
                MM(ob[:, 0:256], tm["Pf"], v_tok[:, n, :], False, False, [tk, ("v_tok", n)], [ok])
                MM(ob[:, 0:256], tm["Pb"], v_tok[:, n, :], False, True, [tk, ("v_tok", n)], [ok])
                kb = 4 + n % 2
                MM(bank(kb)[:, 0:256], ktail[0][:, n, :], v_tok[:, n, :], True, True,
                   [("ktail", 0, n), ("v_tok", n)], [bkey(kb)])
                STT(S, S, dec[:, 0, n:n + 1], bank(kb)[:, 0:256], ALU.mult, ALU.add,
                    ["S", ("dec", 0, n), bkey(kb)], ["S"])
                gb = 4 + n % 2
                for kt in range(KT):
                    MM(bank(gb), hT[:, kt, tsl], wgm[:, kt, :], kt == 0, kt == KT - 1, ["wgm", ("hT", n)], [bkey(gb)])
                ACTF(tm["sig"], bank(gb), AF.Exp, [bkey(gb)], [("sig", n % NTMP)], scale=-1.0)
                ACTF(tm["sig"], tm["sig"], AF.Ln, [("sig", n % NTMP)], [("sig", n % NTMP)], bias=1.0)
                ACTF(tm["sig"], tm["sig"], AF.Exp, [("sig", n % NTMP)], [("sig", n % NTMP)], scale=-1.0)
                TT("pool", tm["G"], tm["sig"][:, 0:256], tm["sig"][:, 256:512], ALU.mult, [("sig", n % NTMP)], [("G", n % NTMP)])
                TT("dve", tm["G"], tm["G"], bank(gb)[:, 0:256], ALU.mult, [("G", n % NTMP), bkey(gb)], [("G", n % NTMP)])
                TT("pool", tm["G"], tm["G"], gnw_bc, ALU.mult, [("G", n % NTMP), "gnw_bc"], [("G", n % NTMP)])
                ssk = ("ssq", n % 2)
                ssap = small[:, 2 + n % 2:3 + n % 2]
                ACTF(tm["sig"][:, 0:256], ob[:, 0:256], AF.Square, [ok, ("G", n % NTMP)], [("sig", n % NTMP), ssk],
                     accum_out=ssap)
                rstd_inplace(ssap, 256, ssk)
                STT(mixed[:, n, h * 256:(h + 1) * 256], ob[:, 0:256], ssap, tm["G"], ALU.mult, ALU.mult,
                    [ok, ssk, ("G", n % NTMP)], [("mixed", n)])

        P.barrier()
        HG = 4
        off = 0
        gqT, off = carve(off, [HG, T], BF16)
        gkT, off = carve(off, [HG, T], BF16)
        gvT, off = carve(off, [HG, T], BF16)
        dabs, off = carve(off, [NT, 32], F32)
        g_raw, off = carve(off, [NT, 2, 8], F32)
        beta, off = carve(off, [NT, 2, 8], F32)
        gvec, off = carve(off, [64], F32)
        wsl0, off = carve(off, [KT, 512], BF16)
        wsl1, off = carve(off, [KT, 512], BF16)
        wsl = [wsl0, wsl1]
        cwT, off = carve(off, [24, 5], F32)
        gdnw_bc, off = carve(off, [128], F32)
        wdab, off = carve(off, [KT, 32], BF16)
        TMP0 = off
        xc = [None, None]
        xc[0], off = carve(off, [T + 4], BF16)
        xc[1], off = carve(off, [T + 4], BF16)
        diag, off = carve(off, [5, 128], BF16)
        ce = [None, None]
        cy = [None, None]
        for i in range(2):
            ce[i], off = carve(off, [512], F32)
            cy[i], off = carve(off, [512], F32)
        cysq, off = carve(off, [512], BF16)
        crs, off = carve(off, [512], F32)
        CONV_END = off
        off = TMP0
        DB = []
        for d_ in range(2):
            dd = {}
            for nm in ("GMB", "Wd", "decT", "u_sb", "Sg"):
                dd[nm], off = carve(off, [HG, 128], F32)
            for nm in ("Lm", "LTm", "XT", "Pp0", "Pp1", "PTp0", "PTp1", "kbg", "vbeta", "qd_tok",
                       "attnT", "ktl0", "ktl1", "qdTg", "wT_sb", "vnew", "Sg_bf", "ostage"):
                dd[nm], off = carve(off, [HG, 128], BF16)
            dd["bg"], off = carve(off, [HG], F32)
            dd["et2"], off = carve(off, [2, HG], F32)
            DB.append(dd)
        oland = []
        for i in range(2):
            a, off = carve(off, [HG, 128], BF16)
            oland.append(a)
        osum, off = carve(off, [HG, 128], F32)
        fsig, off = carve(off, [512], F32)
        fG, off = carve(off, [HG, 128], F32)
        frs, off = carve(off, [8], F32)
        SWEEP_END = off
        gdn_o = nc.dram_tensor("gdn_o_spill", [NT, 128, HG * 128], BF16, kind="Internal").ap()

        gdnw_d = dram("gdn_norm_w", [1, 128])
        gvec_d = dram("gdn_vec", [1, 32])
        cw_d = dram("gdn_conv_wT", [128, 24, 5])
        DMA("sp", gdnw_bc, gdnw_d.partition_broadcast(128), "c_gdnw", [], ["gdnw_bc"])
        DMA("sp", gvec[:, 0:32], gvec_d.partition_broadcast(128), "c_gvec", [], ["gvec"])
        DMA("sp", cwT, cw_d[:, :, :], "c_cw", [], ["cwT"])
        DMA("pool", wdab, w_in_d[:, C_DAB:C_DAB + 32].rearrange("(k p) c -> p k c", p=128), "w_wdab", [], ["wdab"])
        ACTF(gvec[:, 16:32], gvec[:, 16:32], AF.Exp, ["gvec"], ["gvec"])
        TS("dve", gvec[:, 16:32], gvec[:, 16:32], -1.0, None, ALU.mult, ALU.bypass, ["gvec"], ["gvec"])
        for n in range(NT):
            bi = n % 2
            for kt in range(KT):
                MM(bank(bi)[:, 0:32], hT[:, kt, n * 128:(n + 1) * 128], wdab[:, kt, :], kt == 0, kt == KT - 1,
                   ["wdab", ("hT", n)], [bkey(bi)])
            CP("act", dabs[:, n, :], bank(bi)[:, 0:32], [bkey(bi)], ["dabs"])
        a_view = dabs[:, :, 0:16]
        b_view = dabs[:, :, 16:32]
        g_flat = g_raw.rearrange("p n d h -> p n (d h)")
        be_flat = beta.rearrange("p n d h -> p n (d h)")
        TT("dve", g_flat, a_view, gvec[:, 0:16].unsqueeze(1).to_broadcast([128, NT, 16]), ALU.add, ["dabs", "gvec"], ["g_raw"])
        ACTF(g_flat, g_flat, AF.Exp, ["g_raw"], ["g_raw"])
        ACTF(g_flat, g_flat, AF.Ln, ["g_raw"], ["g_raw"], bias=1.0)
        TT("dve", g_flat, g_flat, gvec[:, 16:32].unsqueeze(1).to_broadcast([128, NT, 16]), ALU.mult, ["g_raw", "gvec"], ["g_raw"])
        ACTF(be_flat, b_view, AF.Exp, ["dabs"], ["beta"], scale=-1.0)
        TS("dve", be_flat, be_flat, 1.0, None, ALU.add, ALU.bypass, ["beta"], ["beta"])
        RECIP(be_flat, be_flat, ["beta"], ["beta"])
        MARK("d0")

        GSCALE = 128.0 ** -0.5
        ident_bc4 = ident_b[:].unsqueeze(1).to_broadcast([128, HG, 128])

        def bc_h(ap2):
            return ap2.unsqueeze(2).to_broadcast([128, HG, 128])

        def bc_m(ap2):
            return ap2.unsqueeze(1).to_broadcast([128, HG, 128])

        def v4(ap2):
            return ap2.rearrange("p (h d) -> p h d", h=HG)

        for grp in range(2):
            hs0 = grp * HG
            for which, c_base, dstT in ((0, C_DQ, gqT), (1, C_DK, gkT), (2, C_DV, gvT)):
                ws = wsl[which % 2]
                wk = ("wsl", which % 2)
                DMA("pool", ws, w_in_d[:, c_base + hs0 * 128:c_base + (hs0 + HG) * 128].rearrange("(k p) c -> p k c", p=128),
                    ("w_wsl", which % 2), [], [wk])
                for hh in range(HG):
                    ci = which * 8 + hs0 + hh
                    xi = (which * HG + hh) % 2
                    xcb = xc[xi]
                    xk = ("xc", xi)
                    MEMSET("pool", xcb[:, 0:2], 0.0, [xk])
                    MEMSET("pool", xcb[:, T + 2:T + 4], 0.0, [xk])
                    for k in range(5):
                        TS("dve", diag[:, k, :], cst("ident"), cwT[:, ci, k:k + 1], None, ALU.mult, ALU.bypass,
                           ["consts", "cwT"], ["diag"])
                    for tg in range(4):
                        bi = tg % 2
                        for kt in range(KT):
                            MM(bank(bi), ws[:, kt, hh * 128:(hh + 1) * 128], hT[:, kt, tg * 512:(tg + 1) * 512],
                               kt == 0, kt == KT - 1, [wk] + HT_ALL[tg * 4:tg * 4 + 4], [bkey(bi)])
                        CP("act" if tg % 2 else "dve", xcb[:, 2 + tg * 512:2 + (tg + 1) * 512], bank(bi), [bkey(bi)], [xk])
                    for tg in range(4):
                        bi = 2 + tg % 2
                        i2 = tg % 2
                        for k in range(5):
                            MM(bank(bi), diag[:, k, :], xcb[:, tg * 512 + k:tg * 512 + k + 512], k == 0, k == 4,
                               ["diag", xk], [bkey(bi)])
                        ck = ("ctmp", i2)
                        ACTF(ce[i2], bank(bi), AF.Exp, [bkey(bi)], [ck], scale=-1.0)
                        ACTF(ce[i2], ce[i2], AF.Ln, [ck], [ck], bias=1.0)
                        ACTF(ce[i2], ce[i2], AF.Exp, [ck], [ck], scale=-1.0)
                        dst = dstT[:, hh, tg * 512:(tg + 1) * 512]
                        dk = ("gT", which, hh, tg)
                        if which == 2:
                            TT("dve", dst, ce[i2], bank(bi), ALU.mult, [ck, bkey(bi)], [dk])
                        else:
                            TT("dve", cy[i2], ce[i2], bank(bi), ALU.mult, [ck, bkey(bi)], [("cy", i2)])
                            TT("pool", cysq, cy[i2], cy[i2], ALU.mult, [("cy", i2)], ["cysq"])
                            MM(bank(4), ones_b[:], cysq, True, True, ["ones_b", "cysq"], [bkey(4)])
                            ACTF(crs, bank(4), AF.Ln, [bkey(4)], ["crs"], bias=EPS)
                            ACTF(crs, crs, AF.Exp, ["crs"], ["crs"], scale=-0.5)
                            if which == 0:
                                STT(dst, cy[i2], GSCALE, crs, ALU.mult, ALU.mult, [("cy", i2), "crs"], [dk])
                            else:
                                TT("dve", dst, cy[i2], crs, ALU.mult, [("cy", i2), "crs"], [dk])
            MARK("d1")
            P.barrier()
            DMA("pool", wsl[0], w_in_d[:, C_DZ + hs0 * 128:C_DZ + (hs0 + HG) * 128].rearrange("(k p) c -> p k c", p=128),
                ("w_wsl", 0), [], [("wsl", 0)])
            DMA("pool", wsl[1], w_in_d[:, C_MB + hs0 * 128:C_MB + (hs0 + HG) * 128].rearrange("(k p) c -> p k c", p=128),
                ("w_wsl", 1), [], [("wsl", 1)])

            def gT_keys(which, n):
                return [("gT", which, hh, n // 4) for hh in range(HG)]

            stored = set()
            esc_all = [dabs[:, 0:8, :].rearrange("p a b -> p (a b)").rearrange("p (k n h) -> p k n h", k=4, n=NT),
                       dabs[:, 8:16, :].rearrange("p a b -> p (a b)").rearrange("p (k n h) -> p k n h", k=4, n=NT)]
            for d_ in range(2):
                Mc_ = cst("b_le") if d_ == 0 else cst("b_ge")
                Ms_ = cst("b_gt") if d_ == 0 else cst("b_lt")
                for ki, mk in enumerate((Mc_, Ms_, cst("csel0"), cst("csel1"))):
                    MM(bank(d_)[:, ki * 64:(ki + 1) * 64].rearrange("p (n h) -> p n h", n=NT), mk,
                       g_raw[:, :, d_, hs0:hs0 + HG], True, True, ["consts", "g_raw"], [bkey(d_)])
                ACTF(esc_all[d_].rearrange("p k n h -> p (k n h)"), bank(d_)[:, 0:256], AF.Exp, [bkey(d_)], [("esc_all", d_), "dabs"])

            gb0 = ptb[:, 0:512]
            gb1 = ptb[:, 512:1024]
            xbank = ptb[:, 1024:2048].bitcast(F32)
            XK = ("pb", 7)

            def gdn_tile(d_, n):
                B = DB[d_]
                dk = lambda nm: (nm, d_)
                Mc = cst("b_le") if d_ == 0 else cst("b_ge")
                Ms = cst("b_gt") if d_ == 0 else cst("b_lt")
                bg = B["bg"]
                GMB, Wd, decT, Lm, LTm, XT = B["GMB"], B["Wd"], B["decT"], B["Lm"], B["LTm"], B["XT"]
                kbg, vbeta, qd_tok = B["kbg"], B["vbeta"], B["qd_tok"]
                Ppd = [B["Pp0"], B["Pp1"]]
                PTpd = [B["PTp0"], B["PTp1"]]
                pa, pb_ = (0, 1) if d_ == 0 else (2, 3)
                tsl = slice(n * 128, (n + 1) * 128)
                gv = g_raw[:, n, d_, hs0:hs0 + HG]
                bv = beta[:, n, d_, hs0:hs0 + HG]
                EA = esc_all[d_]
                e_cum, e_tail = EA[:, 0, n, :], EA[:, 1, n, :]
                second = n in stored
                if second:
                    par = n % 2
                    DMA("sp", oland[par].rearrange("p h d -> p (h d)"), gdn_o[n], ("oland", par), [("o_dram", n)], [("oland", par)])
                TT("dve", bg, bv, e_cum, ALU.mult, ["beta", ("esc_all", d_)], [dk("bg")])
                for hh in range(HG):
                    TR(gb0[:, hh * 128:(hh + 1) * 128], gkT[:, hh, tsl], ident_b[:], gT_keys(1, n) + ["ident_b"], [("pbb", 0)])
                for hh in range(HG):
                    TR(gb1[:, hh * 128:(hh + 1) * 128], gvT[:, hh, tsl], ident_b[:], gT_keys(2, n) + ["ident_b"], [("pbb", 0)])
                TT("dve", kbg, v4(gb0), bc_h(bg), ALU.mult, [("pbb", 0), dk("bg")], [dk("kbg")])
                for c_ in range(2):
                    TT("dve", B["et2"][:, c_, :], e_tail, cst("csel%d" % c_)[:, 0:HG], ALU.mult, [("esc_all", d_), "consts"], [dk("et2")])
                    TT("dve", B["ktl%d" % c_], v4(gb0), bc_h(B["et2"][:, c_, :]), ALU.mult, [("pbb", 0), dk("et2")], [dk("ktl%d" % c_)])
                TT("dve", vbeta, v4(gb1), bc_h(bv), ALU.mult, [("pbb", 0), "beta"], [dk("vbeta")])
                for hh in range(HG):
                    TR(gb0[:, hh * 128:(hh + 1) * 128], gqT[:, hh, tsl], ident_b[:], gT_keys(0, n) + ["ident_b"], [("pbb", 0)])
                TT("dve", qd_tok, v4(gb0), bc_h(e_cum), ALU.mult, [("pbb", 0), ("esc_all", d_)], [dk("qd_tok")])
                for hh in range(HG):
                    TR(gb1[:, hh * 128:(hh + 1) * 128], qd_tok[:, hh, :], ident_b[:], [dk("qd_tok"), "ident_b"], [("pbb", 0)])
                CP("act", B["qdTg"], v4(gb1), [("pbb", 0)], [dk("qdTg")])
                TT("pool", GMB, bc_h(gv), bc_m(Ms), ALU.mult, ["g_raw", "consts"], [dk("GMB")])
                MM(bank(pb_), Mc, GMB.rearrange("p h s -> p (h s)"), True, True, ["consts", dk("GMB")], [bkey(pb_)])
                ACTF(Wd.rearrange("p h s -> p (h s)"), bank(pb_), AF.Exp, [bkey(pb_)], [dk("Wd")])
                TT("pool", GMB, bc_h(bv), bc_m(Ms), ALU.mult, ["beta", "consts"], [dk("GMB")])
                TT("pool", Wd, Wd, GMB, ALU.mult, [dk("Wd"), dk("GMB")], [dk("Wd")])
                TT("pool", GMB, bc_h(gv), bc_m(Mc), ALU.mult, ["g_raw", "consts"], [dk("GMB")])
                MM(bank(pa), Ms, GMB.rearrange("p h s -> p (h s)"), True, True, ["consts", dk("GMB")], [bkey(pa)])
                ACTF(decT.rearrange("p h s -> p (h s)"), bank(pa), AF.Exp, [bkey(pa)], [dk("decT")])
                TT("pool", decT, decT, bc_m(Mc), ALU.mult, [dk("decT"), "consts"], [dk("decT")])
                for hh in range(HG):
                    MM(bank(pb_)[:, hh * 128:(hh + 1) * 128], gkT[:, hh, tsl], gkT[:, hh, tsl], True, True,
                       gT_keys(1, n), [bkey(pb_)])
                TT("dve", Lm, v4(bank(pb_)), Wd, ALU.mult, [bkey(pb_), dk("Wd")], [dk("Lm")])
                for hh in range(HG):
                    MM(bank(pa)[:, hh * 128:(hh + 1) * 128], gkT[:, hh, tsl], gqT[:, hh, tsl], True, True,
                       gT_keys(1, n) + gT_keys(0, n), [bkey(pa)])
                TT("dve", B["attnT"], v4(bank(pa)), decT, ALU.mult, [bkey(pa), dk("decT")], [dk("attnT")])
                for hh in range(HG):
                    TR(gb0[:, hh * 128:(hh + 1) * 128], Lm[:, hh, :], ident_b[:], [dk("Lm"), "ident_b"], [("pbb", 0)])
                CP("act", LTm, v4(gb0), [("pbb", 0)], [dk("LTm")])
                TT("dve", XT, ident_bc4, v4(gb0), ALU.subtract, ["ident_b", ("pbb", 0)], [dk("XT")])
                Pc, PTc = Lm, LTm
                pck, ptk_ = dk("Lm"), dk("LTm")
                for it in range(5):
                    Pn, PTn = Ppd[it % 2], PTpd[it % 2]
                    pnk, ptnk = ("Pp", it % 2, d_), ("PTp", it % 2, d_)
                    for hh in range(HG):
                        MM(bank(pb_)[:, hh * 128:(hh + 1) * 128], PTc[:, hh, :], Pc[:, hh, :], True, True, [pck, ptk_], [bkey(pb_)])
                    CP("act", Pn, v4(bank(pb_)), [bkey(pb_)], [pnk])
                    if it < 4:
                        for hh in range(HG):
                            MM(bank(pa)[:, hh * 128:(hh + 1) * 128], Pc[:, hh, :], PTc[:, hh, :], True, True, [pck, ptk_], [bkey(pa)])
                        CP("dve", PTn, v4(bank(pa)), [bkey(pa)], [ptnk])
                    for hh in range(HG):
                        MM(xbank[:, hh * 128:(hh + 1) * 128], Pn[:, hh, :], XT[:, hh, :], True, True, [pnk, dk("XT")], [XK])
                    TT("dve", XT, XT, v4(xbank), ALU.add, [dk("XT"), XK], [dk("XT")])
                    Pc, PTc, pck, ptk_ = Pn, PTn, pnk, ptnk
                for hh in range(HG):
                    MM(bank(pa)[:, hh * 128:(hh + 1) * 128], XT[:, hh, :], vbeta[:, hh, :], True, True, [dk("XT"), dk("vbeta")], [bkey(pa)])
                CP("act", B["u_sb"], v4(bank(pa)), [bkey(pa)], [dk("u_sb")])
                for hh in range(HG):
                    MM(bank(pb_)[:, hh * 128:(hh + 1) * 128], kbg[:, hh, :], XT[:, hh, :], True, True, [dk("kbg"), dk("XT")], [bkey(pb_)])
                CP("dve", B["wT_sb"], v4(bank(pb_)), [bkey(pb_)], [dk("wT_sb")])
                sb0 = 4
                Sg, Sg_bf, vnew = B["Sg"], B["Sg_bf"], B["vnew"]
                chunks = (0, 1) if d_ == 0 else (1, 0)
                for c in chunks:
                    sl = slice(c * 64, c * 64 + 64)
                    for hh in range(HG):
                        MM(bank(sb0)[:, hh * 128:(hh + 1) * 128], B["wT_sb"][:, hh, :], Sg_bf[:, hh, :], True, True,
                           [dk("wT_sb"), dk("Sg_bf")], [bkey(sb0)])
                    TT("dve", vnew[sl], B["u_sb"][sl], v4(bank(sb0))[sl], ALU.subtract, [dk("u_sb"), bkey(sb0)], [dk("vnew")])
                    for hh in range(HG):
                        MM(bank(sb0 + 1)[:, hh * 128:(hh + 1) * 128], B["qdTg"][:, hh, :], Sg_bf[:, hh, :], True, False,
                           [dk("qdTg"), dk("Sg_bf")], [bkey(sb0 + 1)])
                        MM(bank(sb0 + 1)[:, hh * 128:(hh + 1) * 128], B["attnT"][:, hh, :], vnew[:, hh, :], False, True,
                           [dk("attnT"), dk("vnew")], [bkey(sb0 + 1)])
                    for hh in range(HG):
                        MM(bank(sb0)[:, hh * 128:(hh + 1) * 128], B["ktl%d" % c][:, hh, :], vnew[:, hh, :], True, True,
                           [dk("ktl%d" % c), dk("vnew")], [bkey(sb0)])
                    TT("pool", Sg, Sg, bc_h(EA[:, 2 + c, n, :]), ALU.mult, [dk("Sg"), ("esc_all", d_)], [dk("Sg")])
                    TT("dve", Sg, Sg, v4(bank(sb0)), ALU.add, [dk("Sg"), bkey(sb0)], [dk("Sg")])
                    CP("act", Sg_bf, Sg, [dk("Sg")], [dk("Sg_bf")])
                    if not second:
                        CP("act", B["ostage"][sl], v4(bank(sb0 + 1))[sl], [bkey(sb0 + 1)], [dk("ostage")])
                    else:
                        TT("dve", osum[sl], v4(bank(sb0 + 1))[sl], oland[n % 2][sl], ALU.add,
                           [bkey(sb0 + 1), ("oland", n % 2)], ["osum"])
                if not second:
                    stored.add(n)
                    DMA("sp", gdn_o[n], B["ostage"].rearrange("p h d -> p (h d)"), ("ost", d_), [dk("ostage")], [("o_dram", n)])
                    return
                osq = fsig.rearrange("p (h d) -> p h d", h=HG)
                TT("pool", osq, osum, osum, ALU.mult, ["osum"], ["fsig"])
                P.op("dve", lambda e: e.tensor_reduce(out=frs[:, 0:HG], in_=osq, axis=AX.X, op=ALU.add), ["fsig"], ["frs"], cost=600.0)
                rstd_inplace(frs[:, 0:HG], 128, "frs")
                for half, ws in enumerate(wsl):
                    for kt in range(KT):
                        MM(bank(half), hT[:, kt, tsl], ws[:, kt, :], kt == 0, kt == KT - 1,
                           [("wsl", half), ("hT", n)], [bkey(half)])
                fGf = fG.rearrange("p h d -> p (h d)")
                for half in range(2):
                    ACTF(fsig, bank(half), AF.Exp, [bkey(half)], ["fsig"], scale=-1.0)
                    ACTF(fsig, fsig, AF.Ln, ["fsig"], ["fsig"], bias=1.0)
                    ACTF(fsig, fsig, AF.Exp, ["fsig"], ["fsig"], scale=-1.0)
                    if half == 0:
                        TT("dve", fGf, fsig, bank(0), ALU.mult, ["fsig", bkey(0)], ["fG"])
                    else:
                        TT("pool", fGf, fGf, fsig, ALU.mult, ["fsig", "fG"], ["fG"])
                TT("pool", fG, fG, bc_m(gdnw_bc), ALU.mult, ["fG", "gdnw_bc"], ["fG"])
                TT("pool", osum, osum, bc_h(frs[:, 0:HG]), ALU.mult, ["osum", "frs"], ["osum"])
                TT("pool", osum, osum, fG, ALU.mult, ["osum", "fG"], ["osum"])
                mslice = mixed[:, n, hs0 * 128:(hs0 + HG) * 128].rearrange("p (h d) -> p h d", h=HG)
                TT("dve", mslice, mslice, osum, ALU.add, ["osum", ("mixed", n)], [("mixed", n)])

            for d_ in range(2):
                MEMSET("pool", DB[d_]["vnew"], 0.0, [("vnew", d_)])
                MEMSET("dve", DB[d_]["Sg"], 0.0, [("Sg", d_)])
                CP("act", DB[d_]["Sg_bf"], DB[d_]["Sg"], [("Sg", d_)], [("Sg_bf", d_)])
            for i in range(NT):
                gdn_tile(0, i)
                gdn_tile(1, NT - 1 - i)
            MARK("d2")
            P.barrier()

        P.barrier()
        wout_d = dram("w_out", [D, D])
        off = 0
        x1, off = carve(off, [NT, D], F32)
        X1_END = off
        mT, off = carve(off, [KT, T], BF16)
        wout, off = carve(off, [KT, D], BF16)
        hn2 = [None, None]
        hn2[0], off = carve(off, [D], BF16)
        hn2[1], off = carve(off, [D], BF16)
        junk, off = carve(off, [D], BF16)
        n2_bc, off = carve(off, [D], F32)
        DMA("sp", n2_bc, n2_d.partition_broadcast(128), "c_n2", [], ["n2_bc"])
        DMA("pool", wout, wout_d.rearrange("(k p) c -> p k c", p=128), "w_wout", [], ["wout"])
        for n in range(NT):
            b = n % 2
            for kt in range(KT):
                TR(bbank(b)[:, kt * 128:(kt + 1) * 128], mixed[:, n, kt * 128:(kt + 1) * 128], ident_b[:],
                   [("mixed", n), "ident_b"], [("pbb", b)])
            CP("act", mT[:, :, n * 128:(n + 1) * 128], bbank(b).rearrange("p (k t) -> p k t", k=KT), [("pbb", b)], [("mT", n)])
        for n in range(NT):
            tsl = slice(n * 128, (n + 1) * 128)
            DMA("sp", x1[:, n, :], x_d[tsl, :], ("x1ld", n % 4), [], [("x1", n)])
            pp = pt[n % 2]
            for half in range(2):
                for kt in range(KT):
                    MM(pp[:, half * 512:(half + 1) * 512], mT[:, kt, tsl], wout[:, kt, half * 512:(half + 1) * 512],
                       kt == 0, kt == KT - 1, [("mT", n), "wout"], [bkey((n % 2) * 2 + half)])
            TT("dve", x1[:, n, :], x1[:, n, :], pp[:, :], ALU.add, [("x1", n), bkey((n % 2) * 2), bkey((n % 2) * 2 + 1)], [("x1", n)])
            b = n % 2
            ssap = small[:, 8 + b:9 + b]
            ssk = ("ss2", b)
            ACTF(junk, x1[:, n, :], AF.Square, [("x1", n)], ["junk", ssk], accum_out=ssap)
            rstd_inplace(ssap, D, ssk)
            STT(mixed[:, n, :], x1[:, n, :], ssap, n2_bc, ALU.mult, ALU.mult, [("x1", n), ssk, "n2_bc"], [("mixed", n)])
            for kt in range(KT):
                TR(bbank(b)[:, kt * 128:(kt + 1) * 128], mixed[:, n, kt * 128:(kt + 1) * 128], ident_b[:],
                   [("mixed", n), "ident_b"], [("pbb", b)])
            CP("act", hT[:, :, tsl], bbank(b).rearrange("p (k t) -> p k t", k=KT), [("pbb", b)], [("hT", n)])
        MARK("e0")

        P.barrier()
        NB = 64
        wr_d = dram("moe_wr", [D, 36])
        wgu0_d = dram("moe_wgu0", [4096, 2048])
        wgu1_d = dram("moe_wgu1", [4096, 2048])
        wdr_d = dram("moe_wdr", [4096, 2048])
        xb_d = nc.dram_tensor("moe_xb", [NB * 128, D], BF16, kind="Internal").ap()
        yb_d = nc.dram_tensor("moe_yb", [NB * 128, D], F32, kind="Internal").ap()
        off = X1_END
        stg = []
        for i in range(3):
            a, off = carve(off, [2048], F32)
            stg.append(a)
        wgu_bf = []
        wd_bf = []
        for i in range(2):
            a, off = carve(off, [KT, 512], BF16)
            wgu_bf.append(a)
            a, off = carve(off, [2, D], BF16)
            wd_bf.append(a)
        wr, off = carve(off, [KT, 36], BF16)
        lg, off = carve(off, [NT, 36], F32)
        oh1, off = carve(off, [NT, 32], F32)
        oh2, off = carve(off, [NT, 32], F32)
        msk, off = carve(off, [NT, 32], F32)
        rank, off = carve(off, [NT, 32], F32)
        tmp3, off = carve(off, [NT, 32], F32)
        gtmp, off = carve(off, [NT, 4], F32)
        ohg, off = carve(off, [NT, 4], F32)
        rv, off = carve(off, [8, NT], F32)
        mcum, off = carve(off, [32], F32)
        cnt, off = carve(off, [32], F32)
        padded, off = carve(off, [32], F32)
        ends, off = carve(off, [32], F32)
        pstart, off = carve(off, [32], F32)
        ebf, off = carve(off, [NB], F32)
        widx_f, off = carve(off, [NB], F32)
        widx, off = carve(off, [NB], I32)
        dest_f, off = carve(off, [2, NT], F32)
        dest_i, off = carve(off, [2, NT], I32)
        MOE_END = off
        cmpb = stg[0].rearrange("p (b e) -> p b e", b=NB)
        cmpj = stg[1][:, 0:512].rearrange("p (e j) -> p e j", e=32)
        ht32 = hT[:].rearrange("p k t -> p (k t)").bitcast(F32)
        HTCAP = 32 * 1024
        hoff = 0
        xg, xgT, sil, hid_bf, hidT, ysb = [], [], [], [], [], []
        for i in range(2):
            a, hoff = carve(hoff, [D], BF16, ht32, HTCAP); xg.append(a)
            a, hoff = carve(hoff, [KT, 128], BF16, ht32, HTCAP); xgT.append(a)
            a, hoff = carve(hoff, [256], F32, ht32, HTCAP); sil.append(a)
            a, hoff = carve(hoff, [256], BF16, ht32, HTCAP); hid_bf.append(a)
            a, hoff = carve(hoff, [2, 128], BF16, ht32, HTCAP); hidT.append(a)
            a, hoff = carve(hoff, [D], F32, ht32, HTCAP); ysb.append(a)
        mix32 = mixed[:].rearrange("p n d -> p (n d)").bitcast(F32)
        MIXCAP = 32 * 1024
        moff = 0
        yg = []
        for i in range(2):
            a, moff = carve(moff, [D], F32, mix32, MIXCAP); yg.append(a)
        stgB = []
        for i in range(3):
            a, moff = carve(moff, [2048], F32, mix32, MIXCAP); stgB.append(a)

        DMA("pool", wr, wr_d.rearrange("(k p) c -> p k c", p=128), "w_wr", [], ["wr"])
        for n in range(NT):
            bi = n % 2
            for kt in range(KT):
                MM(bank(bi)[:, 0:36], hT[:, kt, n * 128:(n + 1) * 128], wr[:, kt, :], kt == 0, kt == KT - 1,
                   ["wr", ("hT", n)], [bkey(bi)])
            CP("act", lg[:, n, :], bank(bi)[:, 0:36], [bkey(bi)], ["lg"])
        BIG = 10000.0
        glv = lg[:, :, 0:4]
        elv = lg[:, :, 4:36]

        def RED(out, in_, op, R, W):
            P.op("dve", lambda e: e.tensor_reduce(out=out, in_=in_, axis=AX.X, op=op), R, W, cost=100.0 + _fsz(in_) * 1.0)

        def bcn(ap2, k):
            return ap2.unsqueeze(2).to_broadcast([128, NT, k])

        gmax, gsum, m1, m2, w1, w2 = (rv[:, i, :] for i in range(6))
        RED(gmax, glv, ALU.max, ["lg"], ["rv"])
        TT("dve", ohg, glv, bcn(gmax, 4), ALU.is_equal, ["lg", "rv"], ["ohg"])
        TT("dve", gtmp, glv, bcn(gmax, 4), ALU.subtract, ["lg", "rv"], ["gtmp"])
        ACTF(gtmp, gtmp, AF.Exp, ["gtmp"], ["gtmp"])
        RED(gsum, gtmp, ALU.add, ["gtmp"], ["rv"])
        RECIP(gsum, gsum, ["rv"], ["rv"])
        TS("dve", ohg, ohg, BIG, -BIG, ALU.mult, ALU.add, ["ohg"], ["ohg"])
        TT("dve", msk.rearrange("p n (g e) -> p n g e", g=4), elv.rearrange("p n (g e) -> p n g e", g=4),
           ohg.unsqueeze(3).to_broadcast([128, NT, 4, 8]), ALU.add, ["lg", "ohg"], ["msk"])
        RED(m1, msk, ALU.max, ["msk"], ["rv"])
        TT("dve", oh1, msk, bcn(m1, 32), ALU.is_equal, ["msk", "rv"], ["oh1"])
        STT(msk, oh1, -BIG, msk, ALU.mult, ALU.add, ["oh1", "msk"], ["msk"])
        RED(m2, msk, ALU.max, ["msk"], ["rv"])
        TT("dve", oh2, msk, bcn(m2, 32), ALU.is_equal, ["msk", "rv"], ["oh2"])
        TT("dve", w2, m2, m1, ALU.subtract, ["rv"], ["rv"])
        ACTF(w2, w2, AF.Exp, ["rv"], ["rv"])
        TS("dve", w1, w2, 1.0, None, ALU.add, ALU.bypass, ["rv"], ["rv"])
        RECIP(w1, w1, ["rv"], ["rv"])
        TT("dve", w1, w1, gsum, ALU.mult, ["rv"], ["rv"])
        TT("dve", w2, w2, w1, ALU.mult, ["rv"], ["rv"])
        TT("dve", msk, oh1, oh2, ALU.add, ["oh1", "oh2", "msk"], ["msk"])
        MEMSET("dve", mcum, 0.0, ["mcum"])
        for n in range(NT):
            bi = n % 2
            MM(bank(bi)[:, 0:32], cst("m_lt"), msk[:, n, :], True, False, ["consts", "msk"], [bkey(bi)])
            MM(bank(bi)[:, 0:32], cst("ones"), mcum, False, True, ["consts", "mcum"], [bkey(bi)])
            CP("act", rank[:, n, :], bank(bi)[:, 0:32], [bkey(bi)], ["rank"])
            TT("dve", mcum, mcum, msk[:, n, :], ALU.add, ["mcum", "msk"], ["mcum"])
        MM(bank(0)[:, 0:32], cst("ones"), mcum, True, True, ["consts", "mcum"], [bkey(0)])
        CP("act", cnt, bank(0)[:, 0:32], [bkey(0)], ["cnt"])
        TT("dve", cmpj, cnt.unsqueeze(2).to_broadcast([128, 32, 16]),
           cst("bvals")[:, 0:16].unsqueeze(1).to_broadcast([128, 32, 16]), ALU.is_gt, ["cnt", "consts"], [("stg", 1)])
        RED(padded, cmpj, ALU.add, [("stg", 1)], ["padded"])
        TS("dve", padded, padded, 128.0, None, ALU.mult, ALU.bypass, ["padded"], ["padded"])
        P.op("dve", lambda e: e.tensor_tensor_scan(out=ends, data0=cst("ones")[:, 0:32], data1=padded, initial=0.0,
                                                  op0=ALU.mult, op1=ALU.add), ["consts", "padded"], ["ends"], cost=300.0)
        TT("dve", pstart, ends, padded, ALU.subtract, ["ends", "padded"], ["pstart"])
        TT("dve", rank, rank, pstart.unsqueeze(1).to_broadcast([128, NT, 32]), ALU.add, ["rank", "pstart"], ["rank"])
        for k, ohk in ((0, oh1), (1, oh2)):
            TT("dve", tmp3, ohk, rank, ALU.mult, ["oh1", "oh2", "rank"], ["tmp3"])
            RED(dest_f[:, k, :], tmp3, ALU.add, ["tmp3"], ["dest_f"])
        CP("dve", dest_i, dest_f, ["dest_f"], ["dest_i"])
        TT("dve", cmpb, ends.unsqueeze(1).to_broadcast([128, NB, 32]),
           cst("bvals")[:, 0:NB].unsqueeze(2).to_broadcast([128, NB, 32]), ALU.is_le, ["ends", "consts"], [("stg", 0)])
        RED(ebf, cmpb, ALU.add, [("stg", 0)], ["ebf"])
        STT(widx_f, ebf, 128.0, cst("pidx")[:, 0:NB], ALU.mult, ALU.add, ["ebf", "consts"], ["widx_f"])
        CP("dve", widx, widx_f, ["widx_f"], ["widx"])
        MARK("e1")

        IOA = bass.IndirectOffsetOnAxis
        regs = {}

        def _pool_init(e):
            regs["bc"] = e.alloc_register("moe_bc")
            e.reg_mov(regs["bc"], 4095)
        P.pool_init = _pool_init
        XB_KEYS = []
        zt, off = carve(off, [D], BF16)
        MEMSET("pool", zt, 0.0, ["zt"])
        DMA("sp", xb_d.rearrange("(p r) d -> p r d", p=128), zt.unsqueeze(1).to_broadcast([128, NB, D]), "xbz", ["zt"], ["xb0"])
        for n in range(NT):
            for k in range(2):
                idx_ap = dest_i[:, k, n:n + 1]
                src_ap = mixed[:, n, :]
                P.dma("pool", lambda e, idx_ap=idx_ap, src_ap=src_ap: e.indirect_dma_start(
                    out=xb_d[:, :], out_offset=IOA(ap=idx_ap, axis=0), in_=src_ap, in_offset=None),
                    ("sc", (2 * n + k) % 4), [("mixed", n), "dest_i", "xb0"], [("xb", n, k)], nbytes=256 * 1024)
                XB_KEYS.append(("xb", n, k))

        def gather_w(dst, src_d, b, skey, extra):
            idx_ap = widx[:, b:b + 1]
            P.dma("pool", lambda e: e.indirect_dma_start(
                out=dst, out_offset=None, in_=src_d[:, :], in_offset=IOA(ap=idx_ap, axis=0),
                bounds_check=regs["bc"], oob_is_err=False),
                skey, ["widx"] + extra, [skey], nbytes=1 << 20)

        YB_KEYS = []
        for b in range(NB):
            s = b % 2
            sset = stg if b % 2 == 0 else stgB
            so = 0 if b % 2 == 0 else 3
            extra = [] if b % 2 == 0 else XB_KEYS
            gather_w(sset[0], wgu0_d, b, ("stg", so + 0), extra)
            gather_w(sset[1], wgu1_d, b, ("stg", so + 1), extra)
            gather_w(sset[2], wdr_d, b, ("stg", so + 2), extra)
            CP("act", wgu_bf[s][:, 0:4, :], sset[0].rearrange("p (k c) -> p k c", k=4), [("stg", so + 0)], [("wgu_bf", s, 0)])
            CP("dve", wgu_bf[s][:, 4:8, :], sset[1].rearrange("p (k c) -> p k c", k=4), [("stg", so + 1)], [("wgu_bf", s, 1)])
            CP("act" if b % 4 < 2 else "dve", wd_bf[s], sset[2].rearrange("p (k c) -> p k c", k=2), [("stg", so + 2)], [("wd_bf", s)])
            DMA("sp", xg[s], xb_d[b * 128:(b + 1) * 128, :], ("xg", s), XB_KEYS, [("xg", s)])
            for kt in range(KT):
                TR(bbank(s)[:, kt * 128:(kt + 1) * 128], xg[s][:, kt * 128:(kt + 1) * 128], ident_b[:],
                   [("xg", s), "ident_b"], [("pbb", s)])
            CP("act", xgT[s], bbank(s).rearrange("p (k t) -> p k t", k=KT), [("pbb", s)], [("xgT", s)])
            hb = bank(s)
            for kt in range(KT):
                MM(hb, xgT[s][:, kt, :], wgu_bf[s][:, kt, :], kt == 0, kt == KT - 1,
                   [("xgT", s), ("wgu_bf", s, 0), ("wgu_bf", s, 1)], [bkey(s)])
            ACTF(sil[s], hb[:, 0:256], AF.Silu, [bkey(s)], [("sil", s)])
            TT("dve", hid_bf[s], sil[s], hb[:, 256:512], ALU.mult, [("sil", s), bkey(s)], [("hid_bf", s)])
            for ft in range(2):
                TR(bbank(s)[:, ft * 128:(ft + 1) * 128], hid_bf[s][:, ft * 128:(ft + 1) * 128], ident_b[:],
                   [("hid_bf", s), "ident_b"], [("pbb", s)])
            CP("act", hidT[s], bbank(s)[:, 0:256].rearrange("p (k t) -> p k t", k=2), [("pbb", s)], [("hidT", s)])
            yp = pt[1 + s]
            for half in range(2):
                for ft in range(2):
                    MM(yp[:, half * 512:(half + 1) * 512], hidT[s][:, ft, :], wd_bf[s][:, ft, half * 512:(half + 1) * 512],
                       ft == 0, ft == 1, [("hidT", s), ("wd_bf", s)], [bkey(2 + 2 * s + half)])
            CP("act" if b % 2 else "dve", ysb[s], yp[:, :], [bkey(2 + 2 * s), bkey(3 + 2 * s)], [("ysb", s)])
            DMA("sp", yb_d[b * 128:(b + 1) * 128, :], ysb[s], ("yst", s), [("ysb", s)], [("yb", b)])
            YB_KEYS.append(("yb", b))
        MARK("e2")
        ygs = list(yg)
        for sb_ in stgB:
            ygs.append(sb_[:, 0:1024])
            ygs.append(sb_[:, 1024:2048])
        for n in range(NT):
            for k in range(2):
                s = (2 * n + k) % len(ygs)
                idx_ap = dest_i[:, k, n:n + 1]
                dst = ygs[s]
                P.dma("pool", lambda e, idx_ap=idx_ap, dst=dst: e.indirect_dma_start(
                    out=dst, out_offset=None, in_=yb_d[:, :], in_offset=IOA(ap=idx_ap, axis=0)),
                    ("yg", s), YB_KEYS + ["dest_i"], [("yg", s)], nbytes=512 * 1024)
                wk = rv[:, 4 + k, n:n + 1]
                STT(x1[:, n, :], ygs[s], wk, x1[:, n, :], ALU.mult, ALU.add, [("yg", s), "rv", ("x1", n)], [("x1", n)])

        P.barrier()
        off = X1_END
        nf_bc, off = carve(off, [D], F32)
        ob = [None, None]
        ob[0], off = carve(off, [D], F32)
        ob[1], off = carve(off, [D], F32)
        junk2, off = carve(off, [D], BF16)
        DMA("sp", nf_bc, nf_d.partition_broadcast(128), "c_nf", [], ["nf_bc"])
        for n in range(NT):
            b = n % 2
            ssap = small[:, 12 + b:13 + b]
            ssk = ("ss3", b)
            ACTF(junk2, x1[:, n, :], AF.Square, [("x1", n)], ["junk2", ssk], accum_out=ssap)
            rstd_inplace(ssap, D, ssk)
            STT(ob[b], x1[:, n, :], ssap, nf_bc, ALU.mult, ALU.mult, [("x1", n), ssk, "nf_bc"], [("ob", b)])
            DMA("sp", out_d[n * 128:(n + 1) * 128, :], ob[b], ("out_st", b), [("ob", b)], [("out", n)])
        if not dbg:
            P.wait_all("sp", [("out", n) for n in range(NT)])
        if dbg:
            P.enabled = True
            P.barrier()
            for n in range(NT):
                DMA("sp", dbg_d[n * 128:(n + 1) * 128, :], x1[:, n, :], ("dbg_out", n % 2), [("x1", n)], [("dbg", n)])
            P.wait_all("sp", [("dbg", n) for n in range(NT)] + [("out", n) for n in range(NT)])
        P.emit()
    return nc


def make_in_maps(inputs, n_cores=8):
    f = lambda k: np.asarray(inputs[k], np.float32)
    x = f("x")
    _gu = np.concatenate([f("moe_w_gate")[0], f("moe_w_up")[0]], axis=2).reshape(32, 8, 128, 512).transpose(0, 2, 1, 3)
    shared = {
        "norm1_w": f("norm1_w").reshape(1, D),
        "norm2_w": f("norm2_w").reshape(1, D),
        "norm_f_w": f("norm_f_w").reshape(1, D),
        "consts": CONST_ARR,
        "w_in": np.ascontiguousarray(f("w_in")[0]),
        "gla_w2b_f": np.ascontiguousarray(np.concatenate([f("gla_gate_w2_fwd")[0], f("gla_gate_b_fwd")], axis=0)),
        "gla_w2b_b": np.ascontiguousarray(np.concatenate([f("gla_gate_w2_bwd")[0], f("gla_gate_b_bwd")], axis=0)),
        "gla_norm_w": f("gla_norm_w").reshape(1, 256),
        "w_out": np.ascontiguousarray(f("w_out")[0]),
        "moe_wr": np.ascontiguousarray(np.concatenate([f("moe_w_group")[0], f("moe_w_router")[0]], axis=1)),
        "moe_wgu0": _gu[:, :, 0:4, :].reshape(4096, 2048).copy(),
        "moe_wgu1": _gu[:, :, 4:8, :].reshape(4096, 2048).copy(),
        "moe_wdr": np.ascontiguousarray(f("moe_w_down")[0].reshape(32, 2, 128, 1024).transpose(0, 2, 1, 3)).reshape(4096, 2048),
        "gdn_norm_w": f("gdn_norm_w").reshape(1, 128),
        "gdn_vec": np.ascontiguousarray(np.concatenate([f("gdn_dt_bias_fwd")[0], f("gdn_dt_bias_bwd")[0],
                                                        f("gdn_a_log_fwd")[0], f("gdn_a_log_bwd")[0]]).reshape(1, 32)),
        "gdn_conv_wT": np.ascontiguousarray(f("gdn_conv_w")[0].T.reshape(24, 128, 5).transpose(1, 0, 2)),
    }
    maps = []
    for c in range(n_cores):
        m = dict(shared)
        m["x"] = np.ascontiguousarray(x[c])
        maps.append(m)
    return maps


def kernel(**inputs):
    nc = build()
    in_maps = make_in_maps(inputs)
    res = run_bass_kernel_spmd(nc, in_maps, core_ids=list(range(8)))
    out = np.stack([np.asarray(r["out"]) for r in res.results], axis=0)
    return out.astype(np.float32)
```

```python
import contextlib
import heapq
import numpy as np
import concourse.bass as bass
import concourse.mybir as mybir
from concourse.bass_utils import run_bass_kernel_spmd

F32 = mybir.dt.float32
BF16 = mybir.dt.bfloat16
I32 = mybir.dt.int32
AF = mybir.ActivationFunctionType
ALU = mybir.AluOpType
AX = mybir.AxisListType

T = 2048
D = 1024
NT = T // 128
KT = D // 128
EPS = 1e-6
SAME_ENGINE_SYNC = True
EPOCH = 20000
SYNC_NS = 250.0
DMA_LAT_NS = 2200.0
PRIO = True


class Prog:
    ENGS = ("pe", "act", "dve", "pool", "sp")

    def __init__(self, nc, stack):
        self.nc = nc
        self.stack = stack
        self.streams = {e: [] for e in self.ENGS}
        self.count = {e: 0 for e in self.ENGS}
        self.esems = {e: [] for e in self.ENGS}
        self.known = {e: {} for e in self.ENGS}
        self.last_write = {}
        self.readers = {}
        self.dma_sems = {}
        self.dma_vals = {}
        self.dma_last = {}
        self.enabled = True
        self.seg = []
        self.ticks = {}
        self.nops = 0
        self.seg_base = 0
        self.pool_init = None

    def _new_sem(self, name):
        return self.stack.enter_context(self.nc.semaphore(name))

    @staticmethod
    def _psum_fix(reads, writes):
        r2, w2 = [], list(writes)
        for k in reads:
            if isinstance(k, tuple) and k[0] in ("pb", "pbb"):
                if k not in w2:
                    w2.append(k)
            else:
                r2.append(k)
        return r2, w2

    def _record(self, eng, fn, reads, writes, cost, kind, semkey=None):
        reads, writes = self._psum_fix(list(reads), list(writes))
        oid = self.nops
        self.nops += 1
        preds = set()
        for r in reads:
            t = self.last_write.get(r)
            if t is not None:
                preds.add(t)
        for w in writes:
            t = self.last_write.get(w)
            if t is not None:
                preds.add(t)
            preds.update(self.readers.get(w, ()))
        if kind == "dma":
            prev = self.dma_last.get(semkey)
            if prev is not None:
                preds.add(prev)
            self.dma_last[semkey] = oid
        preds = {p for p in preds if p >= self.seg_base}
        self.seg.append(dict(id=oid, eng=eng, fn=fn, preds=preds, cost=float(cost), kind=kind, semkey=semkey))
        for w in writes:
            self.last_write[w] = oid
            self.readers[w] = []
        for r in reads:
            self.readers.setdefault(r, []).append(oid)
        return oid

    def op(self, eng, fn, reads=(), writes=(), cost=300.0):
        if not self.enabled:
            return
        self._record(eng, fn, reads, writes, cost, "op")

    def dma(self, eng, fn, semkey, reads=(), writes=(), nbytes=1 << 20):
        if not self.enabled:
            return
        self._record(eng, fn, reads, writes, DMA_LAT_NS + nbytes / 160.0, "dma", semkey)

    def wait_all(self, eng, keys):
        self._record(eng, None, list(keys), [], 0.0, "op")

    def _schedule_segment(self):
        ops = self.seg
        if not ops:
            return
        byid = {o["id"]: o for o in ops}
        succ = {o["id"]: [] for o in ops}
        indeg = {}
        for o in ops:
            indeg[o["id"]] = len(o["preds"])
            for p in o["preds"]:
                succ[p].append(o["id"])
        ready_t = {o["id"]: 0.0 for o in ops}
        finish = {}
        bl = {}
        for o in reversed(ops):
            m = 0.0
            for s_ in succ[o["id"]]:
                if bl[s_] > m:
                    m = bl[s_]
            bl[o["id"]] = o["cost"] + m
        future = {e: [] for e in self.ENGS}
        avail = {e: [] for e in self.ENGS}
        for o in ops:
            if indeg[o["id"]] == 0:
                heapq.heappush(future[o["eng"]], (0.0, o["id"]))
        etime = {e: 0.0 for e in self.ENGS}
        order = {e: [] for e in self.ENGS}
        remaining = len(ops)
        while remaining:
            best = None
            for e in self.ENGS:
                fu, av = future[e], avail[e]
                while fu and fu[0][0] <= etime[e]:
                    rt, oid = heapq.heappop(fu)
                    heapq.heappush(av, (-bl[oid] if PRIO else rt, oid))
                if av:
                    cand = (etime[e], av[0][0], av[0][1], e, True)
                elif fu:
                    cand = (fu[0][0], 0.0, fu[0][1], e, False)
                else:
                    continue
                if best is None or cand[:3] < best[:3]:
                    best = cand
            st, _, oid, e, from_av = best
            if from_av:
                heapq.heappop(avail[e])
            else:
                heapq.heappop(future[e])
            o = byid[oid]
            if o["kind"] == "dma":
                etime[e] = st + 150.0
                fin = st + o["cost"]
            else:
                etime[e] = st + o["cost"]
                fin = etime[e]
            finish[oid] = fin
            order[e].append(o)
            remaining -= 1
            for s in succ[oid]:
                so = byid[s]
                lat = SYNC_NS if (so["eng"] != e or o["kind"] == "dma") else (60.0 if e != "pe" else 0.0)
                ready_t[s] = max(ready_t[s], fin + lat)
                indeg[s] -= 1
                if indeg[s] == 0:
                    heapq.heappush(future[so["eng"]], (ready_t[s], s))
        self.est_ns = getattr(self, "est_ns", 0.0) + max(list(finish.values()) + [0.0])
        for o in ops:
            if o["kind"] == "dma":
                k = o["semkey"]
                if k not in self.dma_sems:
                    self.dma_sems[k] = self._new_sem(f"d{len(self.dma_sems)}")
                    self.dma_vals[k] = 0
                self.dma_vals[k] += 16
                self.ticks[o["id"]] = (self.dma_sems[k], self.dma_vals[k], "dma")
        def needs_sem(o):
            for s_ in succ[o["id"]]:
                se = byid[s_]["eng"]
                if se != o["eng"] or (SAME_ENGINE_SYNC and se != "pe"):
                    return True
            return False
        for e in self.ENGS:
            real = [o for o in order[e] if o["kind"] == "op" and o["fn"] is not None]
            for i_, o in enumerate(real):
                o["sig"] = needs_sem(o) or i_ == len(real) - 1
        for e in self.ENGS:
            for o in order[e]:
                if o["kind"] == "op" and o["fn"] is not None and o["sig"]:
                    c = self.count[e]
                    ep, v = divmod(c, EPOCH)
                    while len(self.esems[e]) <= ep:
                        self.esems[e].append(self._new_sem(f"s_{e}_{len(self.esems[e])}"))
                    self.count[e] = c + 1
                    self.ticks[o["id"]] = (self.esems[e][ep], v + 1, e)
        for e in self.ENGS:
            for o in order[e]:
                waits = {}
                for p in o["preds"]:
                    if byid[p]["eng"] == e and byid[p]["kind"] == "op" and (not SAME_ENGINE_SYNC or e == "pe"):
                        continue
                    sem, val, src = self.ticks[p]
                    sid = id(sem)
                    if self.known[e].get(sid, 0) >= val:
                        continue
                    if sid not in waits or waits[sid][1] < val:
                        waits[sid] = (sem, val)
                for sid, (sem, val) in waits.items():
                    self.known[e][sid] = val
                inc = None
                if o["fn"] is not None and o["id"] in self.ticks:
                    sem, val, src = self.ticks[o["id"]]
                    inc = (sem, 16 if o["kind"] == "dma" else 1)
                self.streams[e].append((o["fn"], list(waits.values()), inc))
        self.seg = []
        self.seg_base = self.nops

    def barrier(self):
        if not self.enabled and not self.seg:
            return
        self._schedule_segment()
        ticks = []
        for e2 in self.ENGS:
            c = self.count[e2]
            if c > 0:
                ep, v = divmod(c - 1, EPOCH)
                ticks.append((self.esems[e2][ep], v + 1))
        for k, sem in self.dma_sems.items():
            ticks.append((sem, self.dma_vals[k]))
        for eng in self.ENGS:
            waits = []
            for (sem, val) in ticks:
                if self.known[eng].get(id(sem), 0) >= val:
                    continue
                self.known[eng][id(sem)] = val
                waits.append((sem, val))
            if waits:
                self.streams[eng].append((None, waits, None))

    def emit(self):
        self._schedule_segment()
        nc = self.nc
        with nc.Block() as block:
            def run(e, stream):
                for fn, waits, inc in stream:
                    for sem, val in waits:
                        e.wait_ge(sem, val)
                    if fn is None:
                        continue
                    ins = fn(e)
                    if inc is not None:
                        ins.then_inc(inc[0], inc[1])

            @block.tensor
            def _(e):
                run(e, self.streams["pe"])

            @block.scalar
            def _(e):
                run(e, self.streams["act"])

            @block.vector
            def _(e):
                run(e, self.streams["dve"])

            @block.gpsimd
            def _(e):
                if self.pool_init is not None:
                    self.pool_init(e)
                run(e, self.streams["pool"])

            @block.sync
            def _(e):
                run(e, self.streams["sp"])


def _fsz(ap):
    s = ap.shape
    n = 1
    for v in s[1:]:
        n *= int(v)
    return n


C_GQ, C_GK, C_GV, C_GR = 0, 512, 1024, 2048
C_GLF, C_GLB = 3072, 3088
C_DQ, C_DK, C_DV, C_DZ = 3104, 4128, 5152, 6176
C_DAB = 7200
C_MA, C_MB = 7232, 8256
D_IN = 9280


def host_consts():
    r = np.arange(128)[:, None]
    t = np.arange(128)[None, :]
    same = (r // 64) == (t // 64)
    c = {}
    c["ident"] = np.eye(128, dtype=np.float32)
    c["a_le"] = np.where(r <= t, -1.0 / 16, 0.0)
    c["a_ge"] = np.where(r >= t, -1.0 / 16, 0.0)
    c["a_gt"] = np.where(r > t, -1.0 / 16, 0.0)
    c["a_lt"] = np.where(r < t, -1.0 / 16, 0.0)
    c["m_le"] = np.where(r <= t, 1.0, 0.0)
    c["m_ge"] = np.where(r >= t, 1.0, 0.0)
    c["b_le"] = np.where((r <= t) & same, 1.0, 0.0)
    c["b_ge"] = np.where((r >= t) & same, 1.0, 0.0)
    c["b_gt"] = np.where((r > t) & same, 1.0, 0.0)
    c["b_lt"] = np.where((r < t) & same, 1.0, 0.0)
    c["csel0"] = np.where(r < 64, 1.0, 0.0) + 0.0 * t
    c["csel1"] = np.where(r >= 64, 1.0, 0.0) + 0.0 * t
    c["ones"] = np.ones((128, 128))
    c["m_lt"] = np.where(r < t, 1.0, 0.0)
    c["bvals"] = 128.0 * t + 0.0 * r
    c["pidx"] = 1.0 * r + 0.0 * t
    names = list(c.keys())
    arr = np.stack([np.asarray(c[n], np.float32) for n in names], axis=1)
    return names, np.ascontiguousarray(arr)


CONST_NAMES, CONST_ARR = host_consts()
NCONST = len(CONST_NAMES)


COST = dict(pe_a=40.0, pe_b=0.35, pe_f32=3.0, tr=110.0, act_a=200.0, act_b=1.2, dve_a=100.0, dve_b=0.8,
            pool_a=150.0, pool_b=2.4)


def build(stage="all", dbg=False):
    nc = bass.Bass("TRN2", target_bir_lowering=False)
    stack = contextlib.ExitStack()
    with stack:
        P = Prog(nc, stack)

        def dram(name, shape, dt=F32, kind="ExternalInput"):
            return nc.dram_tensor(name, list(shape), dt, kind=kind).ap()

        def sb(name, shape, dt=F32):
            return stack.enter_context(nc.sbuf_tensor(name, list(shape), dt))

        def ps(name, shape, dt=F32):
            return stack.enter_context(nc.psum_tensor(name, list(shape), dt))

        def MM(out, lhsT, rhs, start, stop, R, W):
            n = _fsz(rhs)
            c = COST["pe_a"] + n * COST["pe_b"]
            if rhs.dtype == F32:
                c *= COST["pe_f32"]
            P.op("pe", lambda e: e.matmul(out, lhsT, rhs, start=start, stop=stop), R, W, cost=c)

        def TR(out, in_, ident, R, W):
            P.op("pe", lambda e: e.transpose(out=out, in_=in_, identity=ident), R, W, cost=COST["tr"])

        def ACTF(out, in_, func, R, W, **kw):
            c = COST["act_a"] + _fsz(in_) * COST["act_b"] + (90.0 if "accum_out" in kw else 0.0)
            P.op("act", lambda e: e.activation(out=out, in_=in_, func=func, **kw), R, W, cost=c)

        def _vc(eng, n, k=1.5):
            return (COST["dve_a"] + n * k * COST["dve_b"]) if eng == "dve" else (COST["pool_a"] + n * COST["pool_b"])

        def TT(eng, out, in0, in1, op, R, W):
            P.op(eng, lambda e: e.tensor_tensor(out=out, in0=in0, in1=in1, op=op), R, W, cost=_vc(eng, _fsz(out)))

        def TS(eng, out, in0, s1, s2, op0, op1, R, W):
            P.op(eng, lambda e: e.tensor_scalar(out=out, in0=in0, scalar1=s1, scalar2=s2, op0=op0, op1=op1), R, W,
                 cost=_vc(eng, _fsz(out), 1.05))

        def STT(out, in0, scalar, in1, op0, op1, R, W):
            P.op("dve", lambda e: e.scalar_tensor_tensor(out=out, in0=in0, scalar=scalar, in1=in1, op0=op0, op1=op1), R, W,
                 cost=_vc("dve", _fsz(out)))

        def CP(eng, out, in_, R, W):
            if eng == "act":
                P.op("act", lambda e: e.activation(out=out, in_=in_, func=AF.Copy), R, W, cost=COST["act_a"] + _fsz(in_) * COST["act_b"])
            else:
                P.op(eng, lambda e: e.tensor_copy(out=out, in_=in_), R, W, cost=_vc(eng, _fsz(out), 1.05))

        def MEMSET(eng, ap, val, W):
            P.op(eng, lambda e: e.memset(ap, val), [], W, cost=_vc(eng, _fsz(ap), 0.6))

        def DMA(eng, out, in_, semkey, R, W):
            P.dma(eng, lambda e: e.dma_start(out=out, in_=in_), semkey, R, W, nbytes=_fsz(out) * int(out.shape[0]) * 4)

        def RECIP(out, in_, R, W):
            P.op("dve", lambda e: e.reciprocal(out=out, in_=in_), R, W, cost=_vc("dve", _fsz(out), 1.05))

        def MARK(name):
            if stage == name:
                P.enabled = False

        def rstd_inplace(ap, n, key):
            TS("dve", ap, ap, 1.0 / n, EPS, ALU.mult, ALU.add, [key], [key])
            ACTF(ap, ap, AF.Ln, [key], [key])
            ACTF(ap, ap, AF.Exp, [key], [key], scale=-0.5)

        x_d = dram("x", [T, D])
        n1_d = dram("norm1_w", [1, D])
        n2_d = dram("norm2_w", [1, D])
        nf_d = dram("norm_f_w", [1, D])
        consts_d = dram("consts", [128, NCONST, 128])
        w_in_d = dram("w_in", [D, D_IN])
        w2b_d = [dram("gla_w2b_f", [17, 512]), dram("gla_w2b_b", [17, 512])]
        gnw_d = dram("gla_norm_w", [1, 256])
        out_d = dram("out", [T, D], kind="ExternalOutput")
        dbg_d = dram("dbg", [T, D], kind="ExternalOutput") if dbg else None

        consts = sb("consts_sb", [128, NCONST, 128])
        CI = {n: i for i, n in enumerate(CONST_NAMES)}

        def cst(name):
            return consts[:, CI[name], :]

        ident_b = sb("ident_b", [128, 128], BF16)
        ones_b = sb("ones_b", [128, 128], BF16)
        hT = sb("hT", [128, KT, T], BF16)
        mixed = sb("mixed", [128, NT, D], BF16)
        small = sb("small", [128, 64])
        ARENA_BYTES = 134 * 1024
        arena = sb("arena", [128, ARENA_BYTES // 4])

        def carve(off, shape, dt, base=None, cap=None):
            base = arena if base is None else base
            cap = ARENA_BYTES if cap is None else cap
            nb = int(np.prod(shape)) * (2 if dt == BF16 else 4)
            nb = (nb + 3) // 4 * 4
            assert off % 4 == 0 and off + nb <= cap, (off, nb)
            v = base[:, off // 4:(off + nb) // 4]
            if dt != F32:
                v = v.bitcast(dt)
            if len(shape) == 2:
                pat = "p (a b) -> p a b"
                v = v.rearrange(pat, a=shape[0])
            elif len(shape) == 3:
                v = v.rearrange("p (a b c) -> p a b c", a=shape[0], b=shape[1])
            return v, off + nb

        pt = [ps(f"pt{i}", [128, 1024]) for i in range(3)]
        ptb = ps("ptb", [128, 2048], BF16)

        def bank(i):
            return pt[i // 2][:, (i % 2) * 512:(i % 2 + 1) * 512]

        def bkey(i):
            return ("pb", i)

        def bbank(i):
            return ptb[:, i * 1024:(i + 1) * 1024]

        DMA("sp", consts[:], consts_d[:, :, :], "c_consts", [], ["consts"])
        CP("dve", ident_b[:], cst("ident"), ["consts"], ["ident_b"])
        MEMSET("pool", ones_b[:], 1.0, ["ones_b"])

        off = 116 * 1024
        xt0, off = carve(off, [D], F32)
        xt1, off = carve(off, [D], F32)
        hn0, off = carve(off, [D], BF16)
        hn1, off = carve(off, [D], BF16)
        sq, off = carve(off, [D], BF16)
        n1_bc, off = carve(off, [D], F32)
        DMA("sp", n1_bc, n1_d.partition_broadcast(128), "c_n1", [], ["n1_bc"])
        xts = [xt0, xt1]
        hns = [hn0, hn1]
        for tt in range(NT):
            b = tt % 2
            xb, hb = xts[b], hns[b]
            DMA("sp", xb, x_d[tt * 128:(tt + 1) * 128, :], ("xt", b), [], [("xt", b)])
            ACTF(sq, xb, AF.Square, [("xt", b)], ["sq", "ss0"], accum_out=small[:, 0:1])
            rstd_inplace(small[:, 0:1], D, "ss0")
            STT(hb, xb, small[:, 0:1], n1_bc, ALU.mult, ALU.mult, [("xt", b), "ss0", "n1_bc"], [("hn", b)])
            for kt in range(KT):
                TR(bbank(b)[:, kt * 128:(kt + 1) * 128], hb[:, kt * 128:(kt + 1) * 128], ident_b[:],
                   [("hn", b), "ident_b"], [("pbb", b)])
            CP("act", hT[:, :, tt * 128:(tt + 1) * 128], bbank(b).rearrange("p (k t) -> p k t", k=KT),
               [("pbb", b)], [("hT", tt)])
        HT_ALL = [("hT", tt) for tt in range(NT)]
        MARK("p1")

        off = 0
        qT, off = carve(off, [T], F32)
        kT, off = carve(off, [T], F32)
        k_tok, off = carve(off, [NT, 128], F32)
        v_tok, off = carve(off, [NT, 256], BF16)
        qdT = [None, None]
        kiT = [None, None]
        ktail = [None, None]
        for d_ in range(2):
            qdT[d_], off = carve(off, [T], BF16)
            kiT[d_], off = carve(off, [T], BF16)
            ktail[d_], off = carve(off, [NT, 128], BF16)
        sb_store, off = carve(off, [NT, 256], BF16)
        dec, off = carve(off, [2, NT], F32)
        S, off = carve(off, [256], F32)
        S_bf, off = carve(off, [256], BF16)
        NTMP = 3
        tmp = []
        for i in range(NTMP):
            d = {}
            for nm in ("e", "lg", "E", "Ei", "Et"):
                d[nm], off = carve(off, [128], F32)
            d["Pf"], off = carve(off, [128], BF16)
            d["Pb"], off = carve(off, [128], BF16)
            d["sig"], off = carve(off, [512], F32)
            d["G"], off = carve(off, [256], F32)
            tmp.append(d)
        gl, off = carve(off, [2, T], BF16)
        w2b, off = carve(off, [2, 512], BF16)
        wqk, off = carve(off, [KT, 256], BF16)
        wkv, off = carve(off, [KT, 384], BF16)
        wgm, off = carve(off, [KT, 512], BF16)
        wgl, off = carve(off, [KT, 32], BF16)
        gnw_bc, off = carve(off, [256], F32)
        GLA_END = off
        assert GLA_END <= 116 * 1024, GLA_END

        DMA("sp", gnw_bc, gnw_d.partition_broadcast(128), "c_gnw", [], ["gnw_bc"])
        MEMSET("pool", gl[:, :, :], 1.0, ["gl"])
        MEMSET("pool", w2b[:, :, :], 0.0, ["w2b"])
        for d_ in range(2):
            DMA("pool", w2b[0:17, d_, :], w2b_d[d_][:, :], "c_w2b", [], ["w2b"])
        DMA("pool", wgl, w_in_d[:, C_GLF:C_GLF + 32].rearrange("(k p) c -> p k c", p=128), "w_wgl", [], ["wgl"])
        for d_ in range(2):
            for tg in range(4):
                bi = tg % 2
                for kt in range(KT):
                    MM(bank(bi)[0:16, :], wgl[:, kt, d_ * 16:(d_ + 1) * 16], hT[:, kt, tg * 512:(tg + 1) * 512],
                       kt == 0, kt == KT - 1, ["wgl"] + HT_ALL[tg * 4:tg * 4 + 4], [bkey(bi)])
                CP("act", gl[0:16, d_, tg * 512:(tg + 1) * 512], bank(bi)[0:16, :], [bkey(bi)], ["gl"])

        MARK("g0")
        QSCALE = 128.0 ** -0.5
        for h in range(4):
            def wcols(dst, c0, n):
                return (dst, w_in_d[:, c0:c0 + n].rearrange("(k p) c -> p k c", p=128))
            for (dst, src) in (wcols(wqk[:, :, 0:128], C_GQ + h * 128, 128), wcols(wqk[:, :, 128:256], C_GK + h * 128, 128)):
                DMA("pool", dst, src, "w_wqk", [], ["wqk"])
            for (dst, src) in (wcols(wkv[:, :, 0:128], C_GK + h * 128, 128), wcols(wkv[:, :, 128:384], C_GV + h * 256, 256)):
                DMA("pool", dst, src, "w_wkv", [], ["wkv"])
            for (dst, src) in (wcols(wgm[:, :, 0:256], C_GR + h * 256, 256), wcols(wgm[:, :, 256:512], C_MA + h * 256, 256)):
                DMA("pool", dst, src, "w_wgm", [], ["wgm"])
            MARK("g1a")
            for which, dstT in ((0, qT), (1, kT)):
                for tg in range(4):
                    bi = (which * 4 + tg) % 4
                    for kt in range(KT):
                        MM(bank(bi), wqk[:, kt, which * 128:(which + 1) * 128], hT[:, kt, tg * 512:(tg + 1) * 512],
                           kt == 0, kt == KT - 1, ["wqk"] + HT_ALL[tg * 4:tg * 4 + 4], [bkey(bi)])
                    CP("act" if tg % 2 else "dve", dstT[:, tg * 512:(tg + 1) * 512], bank(bi), [bkey(bi)],
                       [("qkT", which, tg)])
            MARK("g1b")
            for n in range(NT):
                bi = 4 + n % 2
                for kt in range(KT):
                    MM(bank(bi)[:, 0:384], hT[:, kt, n * 128:(n + 1) * 128], wkv[:, kt, :],
                       kt == 0, kt == KT - 1, ["wkv", ("hT", n)], [bkey(bi)])
                CP("dve", k_tok[:, n, :], bank(bi)[:, 0:128], [bkey(bi)], [("k_tok", n)])
                CP("act", v_tok[:, n, :], bank(bi)[:, 128:384], [bkey(bi)], [("v_tok", n)])
            MARK("g1")
            for n in range(NT):
                tsl = slice(n * 128, (n + 1) * 128)
                tg = n // 4
                for d_ in range(2):
                    tm = tmp[(n * 2 + d_) % NTMP]
                    tk = ("gtmp", (n * 2 + d_) % NTMP)
                    a_c = cst("a_le") if d_ == 0 else cst("a_ge")
                    a_s = cst("a_gt") if d_ == 0 else cst("a_lt")
                    b0 = (n * 2 + d_) % 2 * 2
                    zb, cb = bank(b0), bank(b0 + 1)
                    MM(zb[:, 0:128], gl[:, d_, tsl], w2b[:, d_, h * 128:(h + 1) * 128], True, True,
                       ["gl", "w2b"], [bkey(b0)])
                    ACTF(tm["e"], zb[:, 0:128], AF.Exp, [bkey(b0)], [tk], scale=-1.0)
                    ACTF(tm["lg"], tm["e"], AF.Ln, [tk], [tk], bias=1.0)
                    MM(cb[:, 0:128], tm["lg"], a_c, True, True, [tk, "consts"], [bkey(b0 + 1)])
                    MM(cb[:, 128:256], a_s, tm["lg"], True, True, [tk, "consts"], [bkey(b0 + 1)])
                    ACTF(tm["E"], cb[:, 0:128], AF.Exp, [bkey(b0 + 1)], [tk])
                    ACTF(tm["Ei"], cb[:, 0:128], AF.Exp, [bkey(b0 + 1)], [tk], scale=-1.0)
                    ACTF(tm["Et"], cb[:, 128:256], AF.Exp, [bkey(b0 + 1)], [tk])
                    STT(qdT[d_][:, tsl], qT[:, tsl], QSCALE, tm["E"], ALU.mult, ALU.mult,
                        [("qkT", 0, tg), tk], [("qdT", d_, n)])
                    TT("dve", kiT[d_][:, tsl], kT[:, tsl], tm["Ei"], ALU.mult, [("qkT", 1, tg), tk], [("kiT", d_, n)])
                    TT("dve", ktail[d_][:, n, :], k_tok[:, n, :], tm["Et"], ALU.mult, [("k_tok", n), tk], [("ktail", d_, n)])
                    col = 127 if d_ == 0 else 0
                    CP("dve", dec[:, d_, n:n + 1], tm["E"][:, col:col + 1], [tk], [("dec", d_, n)])
            MARK("g2")
            MEMSET("dve", S, 0.0, ["S"])
            for n in range(NT - 1, -1, -1):
                CP("act", sb_store[:, n, :], S, ["S"], [("sb_store", n)])
                bi = 4 + n % 2
                MM(bank(bi)[:, 0:256], ktail[1][:, n, :], v_tok[:, n, :], True, True,
                   [("ktail", 1, n), ("v_tok", n)], [bkey(bi)])
                STT(S, S, dec[:, 1, n:n + 1], bank(bi)[:, 0:256], ALU.mult, ALU.add,
                    ["S", ("dec", 1, n), bkey(bi)], ["S"])
            MARK("g3")
            MEMSET("dve", S, 0.0, ["S"])
            for n in range(NT):
                tsl = slice(n * 128, (n + 1) * 128)
                tm = tmp[n % NTMP]
                tk = ("ftmp", n % NTMP)
                CP("act", S_bf, S, ["S"], ["S_bf"])
                b0 = (n % 2) * 2
                sc = bank(b0)
                MM(sc[:, 0:128], kiT[0][:, tsl], qdT[0][:, tsl], True, True, [("kiT", 0, n), ("qdT", 0, n)], [bkey(b0)])
                MM(sc[:, 128:256], kiT[1][:, tsl], qdT[1][:, tsl], True, True, [("kiT", 1, n), ("qdT", 1, n)], [bkey(b0)])
                TT("dve", tm["Pf"], sc[:, 0:128], cst("m_le"), ALU.mult, [bkey(b0), "consts"], [tk])
                TT("dve", tm["Pb"], sc[:, 128:256], cst("m_ge"), ALU.mult, [bkey(b0), "consts"], [tk])
                ob = bank(b0 + 1)
                ok = bkey(b0 + 1)
                MM(ob[:, 0:256], qdT[0][:, tsl], S_bf, True, False, [("qdT", 0, n), "S_bf"], [ok])
                MM(ob[:, 0:256], qdT[1][:, tsl], sb_store[:, n, :], False, False, [("qdT", 1, n), ("sb_store", n)], [ok])
                MM(ob[:, 0:256], tm["Pf"], v_tok[:, n, :], False, False, [tk, ("v_tok", n)], [ok])
                MM(ob[:, 0:256], tm["Pb"], v_tok[:, n, :], False, True, [tk, ("v_tok", n)], [ok])
                kb = 4 + n % 2
                MM(bank(kb)[:, 0:256], ktail[0][:, n, :], v_tok[:, n, :], True, True,
                   [("ktail", 0, n), ("v_tok", n)], [bkey(kb)])
                STT(S, S, dec[:, 0, n:n + 1], bank(kb)[:, 0:256], ALU.mult, ALU.add,
                    ["S", ("dec", 0, n), bkey(kb)], ["S"])
                gb = 4 + n % 2
                for kt in range(KT):
                    MM(bank(gb), hT[:, kt, tsl], wgm[:, kt, :], kt == 0, kt == KT - 1, ["wgm", ("hT", n)], [bkey(gb)])
                ACTF(tm["sig"], bank(gb), AF.Exp, [bkey(gb)], [("sig", n % NTMP)], scale=-1.0)
                ACTF(tm["sig"], tm["sig"], AF.Ln, [("sig", n % NTMP)], [("sig", n % NTMP)], bias=1.0)
                ACTF(tm["sig"], tm["sig"], AF.Exp, [("sig", n % NTMP)], [("sig", n % NTMP)], scale=-1.0)
                TT("pool", tm["G"], tm["sig"][:, 0:256], tm["sig"][:, 256:512], ALU.mult, [("sig", n % NTMP)], [("G", n % NTMP)])
                TT("dve", tm["G"], tm["G"], bank(gb)[:, 0:256], ALU.mult, [("G", n % NTMP), bkey(gb)], [("G", n % NTMP)])
                TT("pool", tm["G"], tm["G"], gnw_bc, ALU.mult, [("G", n % NTMP), "gnw_bc"], [("G", n % NTMP)])
                ssk = ("ssq", n % 2)
                ssap = small[:, 2 + n % 2:3 + n % 2]
                ACTF(tm["sig"][:, 0:256], ob[:, 0:256], AF.Square, [ok, ("G", n % NTMP)], [("sig", n % NTMP), ssk],
                     accum_out=ssap)
                rstd_inplace(ssap, 256, ssk)
                STT(mixed[:, n, h * 256:(h + 1) * 256], ob[:, 0:256], ssap, tm["G"], ALU.mult, ALU.mult,
                    [ok, ssk, ("G", n % NTMP)], [("mixed", n)])

        P.barrier()
        HG = 4
        off = 0
        gqT, off = carve(off, [HG, T], BF16)
        gkT, off = carve(off, [HG, T], BF16)
        gvT, off = carve(off, [HG, T], BF16)
        dabs, off = carve(off, [NT, 32], F32)
        g_raw, off = carve(off, [NT, 2, 8], F32)
        beta, off = carve(off, [NT, 2, 8], F32)
        gvec, off = carve(off, [64], F32)
        wsl0, off = carve(off, [KT, 512], BF16)
        wsl1, off = carve(off, [KT, 512], BF16)
        wsl = [wsl0, wsl1]
        cwT, off = carve(off, [24, 5], F32)
        gdnw_bc, off = carve(off, [128], F32)
        wdab, off = carve(off, [KT, 32], BF16)
        TMP0 = off
        xc = [None, None]
        xc[0], off = carve(off, [T + 4], BF16)
        xc[1], off = carve(off, [T + 4], BF16)
        diag, off = carve(off, [5, 128], BF16)
        ce = [None, None]
        cy = [None, None]
        for i in range(2):
            ce[i], off = carve(off, [512], F32)
            cy[i], off = carve(off, [512], F32)
        cysq, off = carve(off, [512], BF16)
        crs, off = carve(off, [512], F32)
        CONV_END = off
        off = TMP0
        DB = []
        for d_ in range(2):
            dd = {}
            for nm in ("GMB", "Wd", "decT", "u_sb", "Sg"):
                dd[nm], off = carve(off, [HG, 128], F32)
            for nm in ("Lm", "LTm", "XT", "Pp0", "Pp1", "PTp0", "PTp1", "kbg", "vbeta", "qd_tok",
                       "attnT", "ktl0", "ktl1", "qdTg", "wT_sb", "vnew", "Sg_bf", "ostage"):
                dd[nm], off = carve(off, [HG, 128], BF16)
            dd["bg"], off = carve(off, [HG], F32)
            dd["et2"], off = carve(off, [2, HG], F32)
            DB.append(dd)
        oland = []
        for i in range(2):
            a, off = carve(off, [HG, 128], BF16)
            oland.append(a)
        osum, off = carve(off, [HG, 128], F32)
        fsig, off = carve(off, [512], F32)
        fG, off = carve(off, [HG, 128], F32)
        frs, off = carve(off, [8], F32)
        SWEEP_END = off
        gdn_o = nc.dram_tensor("gdn_o_spill", [NT, 128, HG * 128], BF16, kind="Internal").ap()

        gdnw_d = dram("gdn_norm_w", [1, 128])
        gvec_d = dram("gdn_vec", [1, 32])
        cw_d = dram("gdn_conv_wT", [128, 24, 5])
        DMA("sp", gdnw_bc, gdnw_d.partition_broadcast(128), "c_gdnw", [], ["gdnw_bc"])
        DMA("sp", gvec[:, 0:32], gvec_d.partition_broadcast(128), "c_gvec", [], ["gvec"])
        DMA("sp", cwT, cw_d[:, :, :], "c_cw", [], ["cwT"])
        DMA("pool", wdab, w_in_d[:, C_DAB:C_DAB + 32].rearrange("(k p) c -> p k c", p=128), "w_wdab", [], ["wdab"])
        ACTF(gvec[:, 16:32], gvec[:, 16:32], AF.Exp, ["gvec"], ["gvec"])
        TS("dve", gvec[:, 16:32], gvec[:, 16:32], -1.0, None, ALU.mult, ALU.bypass, ["gvec"], ["gvec"])
        for n in range(NT):
            bi = n % 2
            for kt in range(KT):
                MM(bank(bi)[:, 0:32], hT[:, kt, n * 128:(n + 1) * 128], wdab[:, kt, :], kt == 0, kt == KT - 1,
                   ["wdab", ("hT", n)], [bkey(bi)])
            CP("act", dabs[:, n, :], bank(bi)[:, 0:32], [bkey(bi)], ["dabs"])
        a_view = dabs[:, :, 0:16]
        b_view = dabs[:, :, 16:32]
        g_flat = g_raw.rearrange("p n d h -> p n (d h)")
        be_flat = beta.rearrange("p n d h -> p n (d h)")
        TT("dve", g_flat, a_view, gvec[:, 0:16].unsqueeze(1).to_broadcast([128, NT, 16]), ALU.add, ["dabs", "gvec"], ["g_raw"])
        ACTF(g_flat, g_flat, AF.Exp, ["g_raw"], ["g_raw"])
        ACTF(g_flat, g_flat, AF.Ln, ["g_raw"], ["g_raw"], bias=1.0)
        TT("dve", g_flat, g_flat, gvec[:, 16:32].unsqueeze(1).to_broadcast([128, NT, 16]), ALU.mult, ["g_raw", "gvec"], ["g_raw"])
        ACTF(be_flat, b_view, AF.Exp, ["dabs"], ["beta"], scale=-1.0)
        TS("dve", be_flat, be_flat, 1.0, None, ALU.add, ALU.bypass, ["beta"], ["beta"])
        RECIP(be_flat, be_flat, ["beta"], ["beta"])
        MARK("d0")

        GSCALE = 128.0 ** -0.5
        ident_bc4 = ident_b[:].unsqueeze(1).to_broadcast([128, HG, 128])

        def bc_h(ap2):
            return ap2.unsqueeze(2).to_broadcast([128, HG, 128])

        def bc_m(ap2):
            return ap2.unsqueeze(1).to_broadcast([128, HG, 128])

        def v4(ap2):
            return ap2.rearrange("p (h d) -> p h d", h=HG)

        for grp in range(2):
            hs0 = grp * HG
            for which, c_base, dstT in ((0, C_DQ, gqT), (1, C_DK, gkT), (2, C_DV, gvT)):
                ws = wsl[which % 2]
                wk = ("wsl", which % 2)
                DMA("pool", ws, w_in_d[:, c_base + hs0 * 128:c_base + (hs0 + HG) * 128].rearrange("(k p) c -> p k c", p=128),
                    ("w_wsl", which % 2), [], [wk])
                for hh in range(HG):
                    ci = which * 8 + hs0 + hh
                    xi = (which * HG + hh) % 2
                    xcb = xc[xi]
                    xk = ("xc", xi)
                    MEMSET("pool", xcb[:, 0:2], 0.0, [xk])
                    MEMSET("pool", xcb[:, T + 2:T + 4], 0.0, [xk])
                    for k in range(5):
                        TS("dve", diag[:, k, :], cst("ident"), cwT[:, ci, k:k + 1], None, ALU.mult, ALU.bypass,
                           ["consts", "cwT"], ["diag"])
                    for tg in range(4):
                        bi = tg % 2
                        for kt in range(KT):
                            MM(bank(bi), ws[:, kt, hh * 128:(hh + 1) * 128], hT[:, kt, tg * 512:(tg + 1) * 512],
                               kt == 0, kt == KT - 1, [wk] + HT_ALL[tg * 4:tg * 4 + 4], [bkey(bi)])
                        CP("act" if tg % 2 else "dve", xcb[:, 2 + tg * 512:2 + (tg + 1) * 512], bank(bi), [bkey(bi)], [xk])
                    for tg in range(4):
                        bi = 2 + tg % 2
                        i2 = tg % 2
                        for k in range(5):
                            MM(bank(bi), diag[:, k, :], xcb[:, tg * 512 + k:tg * 512 + k + 512], k == 0, k == 4,
                               ["diag", xk], [bkey(bi)])
                        ck = ("ctmp", i2)
                        ACTF(ce[i2], bank(bi), AF.Exp, [bkey(bi)], [ck], scale=-1.0)
                        ACTF(ce[i2], ce[i2], AF.Ln, [ck], [ck], bias=1.0)
                        ACTF(ce[i2], ce[i2], AF.Exp, [ck], [ck], scale=-1.0)
                        dst = dstT[:, hh, tg * 512:(tg + 1) * 512]
                        dk = ("gT", which, hh, tg)
                        if which == 2:
                            TT("dve", dst, ce[i2], bank(bi), ALU.mult, [ck, bkey(bi)], [dk])
                        else:
                            TT("dve", cy[i2], ce[i2], bank(bi), ALU.mult, [ck, bkey(bi)], [("cy", i2)])
                            TT("pool", cysq, cy[i2], cy[i2], ALU.mult, [("cy", i2)], ["cysq"])
                            MM(bank(4), ones_b[:], cysq, True, True, ["ones_b", "cysq"], [bkey(4)])
                            ACTF(crs, bank(4), AF.Ln, [bkey(4)], ["crs"], bias=EPS)
                            ACTF(crs, crs, AF.Exp, ["crs"], ["crs"], scale=-0.5)
                            if which == 0:
                                STT(dst, cy[i2], GSCALE, crs, ALU.mult, ALU.mult, [("cy", i2), "crs"], [dk])
                            else:
                                TT("dve", dst, cy[i2], crs, ALU.mult, [("cy", i2), "crs"], [dk])
            MARK("d1")
            P.barrier()
            DMA("pool", wsl[0], w_in_d[:, C_DZ + hs0 * 128:C_DZ + (hs0 + HG) * 128].rearrange("(k p) c -> p k c", p=128),
                ("w_wsl", 0), [], [("wsl", 0)])
            DMA("pool", wsl[1], w_in_d[:, C_MB + hs0 * 128:C_MB + (hs0 + HG) * 128].rearrange("(k p) c -> p k c", p=128),
                ("w_wsl", 1), [], [("wsl", 1)])

            def gT_keys(which, n):
                return [("gT", which, hh, n // 4) for hh in range(HG)]

            stored = set()
            esc_all = [dabs[:, 0:8, :].rearrange("p a b -> p (a b)").rearrange("p (k n h) -> p k n h", k=4, n=NT),
                       dabs[:, 8:16, :].rearrange("p a b -> p (a b)").rearrange("p (k n h) -> p k n h", k=4, n=NT)]
            for d_ in range(2):
                Mc_ = cst("b_le") if d_ == 0 else cst("b_ge")
                Ms_ = cst("b_gt") if d_ == 0 else cst("b_lt")
                for ki, mk in enumerate((Mc_, Ms_, cst("csel0"), cst("csel1"))):
                    MM(bank(d_)[:, ki * 64:(ki + 1) * 64].rearrange("p (n h) -> p n h", n=NT), mk,
                       g_raw[:, :, d_, hs0:hs0 + HG], True, True, ["consts", "g_raw"], [bkey(d_)])
                ACTF(esc_all[d_].rearrange("p k n h -> p (k n h)"), bank(d_)[:, 0:256], AF.Exp, [bkey(d_)], [("esc_all", d_), "dabs"])

            gb0 = ptb[:, 0:512]
            gb1 = ptb[:, 512:1024]
            xbank = ptb[:, 1024:2048].bitcast(F32)
            XK = ("pb", 7)

            def gdn_tile(d_, n):
                B = DB[d_]
                dk = lambda nm: (nm, d_)
                Mc = cst("b_le") if d_ == 0 else cst("b_ge")
                Ms = cst("b_gt") if d_ == 0 else cst("b_lt")
                bg = B["bg"]
                GMB, Wd, decT, Lm, LTm, XT = B["GMB"], B["Wd"], B["decT"], B["Lm"], B["LTm"], B["XT"]
                kbg, vbeta, qd_tok = B["kbg"], B["vbeta"], B["qd_tok"]
                Ppd = [B["Pp0"], B["Pp1"]]
                PTpd = [B["PTp0"], B["PTp1"]]
                pa, pb_ = (0, 1) if d_ == 0 else (2, 3)
                tsl = slice(n * 128, (n + 1) * 128)
                gv = g_raw[:, n, d_, hs0:hs0 + HG]
                bv = beta[:, n, d_, hs0:hs0 + HG]
                EA = esc_all[d_]
                e_cum, e_tail = EA[:, 0, n, :], EA[:, 1, n, :]
                second = n in stored
                if second:
                    par = n % 2
                    DMA("sp", oland[par].rearrange("p h d -> p (h d)"), gdn_o[n], ("oland", par), [("o_dram", n)], [("oland", par)])
                TT("dve", bg, bv, e_cum, ALU.mult, ["beta", ("esc_all", d_)], [dk("bg")])
                for hh in range(HG):
                    TR(gb0[:, hh * 128:(hh + 1) * 128], gkT[:, hh, tsl], ident_b[:], gT_keys(1, n) + ["ident_b"], [("pbb", 0)])
                for hh in range(HG):
                    TR(gb1[:, hh * 128:(hh + 1) * 128], gvT[:, hh, tsl], ident_b[:], gT_keys(2, n) + ["ident_b"], [("pbb", 0)])
                TT("dve", kbg, v4(gb0), bc_h(bg), ALU.mult, [("pbb", 0), dk("bg")], [dk("kbg")])
                for c_ in range(2):
                    TT("dve", B["et2"][:, c_, :], e_tail, cst("csel%d" % c_)[:, 0:HG], ALU.mult, [("esc_all", d_), "consts"], [dk("et2")])
                    TT("dve", B["ktl%d" % c_], v4(gb0), bc_h(B["et2"][:, c_, :]), ALU.mult, [("pbb", 0), dk("et2")], [dk("ktl%d" % c_)])
                TT("dve", vbeta, v4(gb1), bc_h(bv), ALU.mult, [("pbb", 0), "beta"], [dk("vbeta")])
                for hh in range(HG):
                    TR(gb0[:, hh * 128:(hh + 1) * 128], gqT[:, hh, tsl], ident_b[:], gT_keys(0, n) + ["ident_b"], [("pbb", 0)])
                TT("dve", qd_tok, v4(gb0), bc_h(e_cum), ALU.mult, [("pbb", 0), ("esc_all", d_)], [dk("qd_tok")])
                for hh in range(HG):
                    TR(gb1[:, hh * 128:(hh + 1) * 128], qd_tok[:, hh, :], ident_b[:], [dk("qd_tok"), "ident_b"], [("pbb", 0)])
                CP("act", B["qdTg"], v4(gb1), [("pbb", 0)], [dk("qdTg")])
                TT("pool", GMB, bc_h(gv), bc_m(Ms), ALU.mult, ["g_raw", "consts"], [dk("GMB")])
                MM(bank(pb_), Mc, GMB.rearrange("p h s -> p (h s)"), True, True, ["consts", dk("GMB")], [bkey(pb_)])
                ACTF(Wd.rearrange("p h s -> p (h s)"), bank(pb_), AF.Exp, [bkey(pb_)], [dk("Wd")])
                TT("pool", GMB, bc_h(bv), bc_m(Ms), ALU.mult, ["beta", "consts"], [dk("GMB")])
                TT("pool", Wd, Wd, GMB, ALU.mult, [dk("Wd"), dk("GMB")], [dk("Wd")])
                TT("pool", GMB, bc_h(gv), bc_m(Mc), ALU.mult, ["g_raw", "consts"], [dk("GMB")])
                MM(bank(pa), Ms, GMB.rearrange("p h s -> p (h s)"), True, True, ["consts", dk("GMB")], [bkey(pa)])
                ACTF(decT.rearrange("p h s -> p (h s)"), bank(pa), AF.Exp, [bkey(pa)], [dk("decT")])
                TT("pool", decT, decT, bc_m(Mc), ALU.mult, [dk("decT"), "consts"], [dk("decT")])
                for hh in range(HG):
                    MM(bank(pb_)[:, hh * 128:(hh + 1) * 128], gkT[:, hh, tsl], gkT[:, hh, tsl], True, True,
                       gT_keys(1, n), [bkey(pb_)])
                TT("dve", Lm, v4(bank(pb_)), Wd, ALU.mult, [bkey(pb_), dk("Wd")], [dk("Lm")])
                for hh in range(HG):
                    MM(bank(pa)[:, hh * 128:(hh + 1) * 128], gkT[:, hh, tsl], gqT[:, hh, tsl], True, True,
                       gT_keys(1, n) + gT_keys(0, n), [bkey(pa)])
                TT("dve", B["attnT"], v4(bank(pa)), decT, ALU.mult, [bkey(pa), dk("decT")], [dk("attnT")])
                for hh in range(HG):
                    TR(gb0[:, hh * 128:(hh + 1) * 128], Lm[:, hh, :], ident_b[:], [dk("Lm"), "ident_b"], [("pbb", 0)])
                CP("act", LTm, v4(gb0), [("pbb", 0)], [dk("LTm")])
                TT("dve", XT, ident_bc4, v4(gb0), ALU.subtract, ["ident_b", ("pbb", 0)], [dk("XT")])
                Pc, PTc = Lm, LTm
                pck, ptk_ = dk("Lm"), dk("LTm")
                for it in range(5):
                    Pn, PTn = Ppd[it % 2], PTpd[it % 2]
                    pnk, ptnk = ("Pp", it % 2, d_), ("PTp", it % 2, d_)
                    for hh in range(HG):
                        MM(bank(pb_)[:, hh * 128:(hh + 1) * 128], PTc[:, hh, :], Pc[:, hh, :], True, True, [pck, ptk_], [bkey(pb_)])
                    CP("act", Pn, v4(bank(pb_)), [bkey(pb_)], [pnk])
                    if it < 4:
                        for hh in range(HG):
                            MM(bank(pa)[:, hh * 128:(hh + 1) * 128], Pc[:, hh, :], PTc[:, hh, :], True, True, [pck, ptk_], [bkey(pa)])
                        CP("dve", PTn, v4(bank(pa)), [bkey(pa)], [ptnk])
                    for hh in range(HG):
                        MM(xbank[:, hh * 128:(hh + 1) * 128], Pn[:, hh, :], XT[:, hh, :], True, True, [pnk, dk("XT")], [XK])
                    TT("dve", XT, XT, v4(xbank), ALU.add, [dk("XT"), XK], [dk("XT")])
                    Pc, PTc, pck, ptk_ = Pn, PTn, pnk, ptnk
                for hh in range(HG):
                    MM(bank(pa)[:, hh * 128:(hh + 1) * 128], XT[:, hh, :], vbeta[:, hh, :], True, True, [dk("XT"), dk("vbeta")], [bkey(pa)])
                CP("act", B["u_sb"], v4(bank(pa)), [bkey(pa)], [dk("u_sb")])
                for hh in range(HG):
                    MM(bank(pb_)[:, hh * 128:(hh + 1) * 128], kbg[:, hh, :], XT[:, hh, :], True, True, [dk("kbg"), dk("XT")], [bkey(pb_)])
                CP("dve", B["wT_sb"], v4(bank(pb_)), [bkey(pb_)], [dk("wT_sb")])
                sb0 = 4
                Sg, Sg_bf, vnew = B["Sg"], B["Sg_bf"], B["vnew"]
                chunks = (0, 1) if d_ == 0 else (1, 0)
                for c in chunks:
                    sl = slice(c * 64, c * 64 + 64)
                    for hh in range(HG):
                        MM(bank(sb0)[:, hh * 128:(hh + 1) * 128], B["wT_sb"][:, hh, :], Sg_bf[:, hh, :], True, True,
                           [dk("wT_sb"), dk("Sg_bf")], [bkey(sb0)])
                    TT("dve", vnew[sl], B["u_sb"][sl], v4(bank(sb0))[sl], ALU.subtract, [dk("u_sb"), bkey(sb0)], [dk("vnew")])
                    for hh in range(HG):
                        MM(bank(sb0 + 1)[:, hh * 128:(hh + 1) * 128], B["qdTg"][:, hh, :], Sg_bf[:, hh, :], True, False,
                           [dk("qdTg"), dk("Sg_bf")], [bkey(sb0 + 1)])
                        MM(bank(sb0 + 1)[:, hh * 128:(hh + 1) * 128], B["attnT"][:, hh, :], vnew[:, hh, :], False, True,
                           [dk("attnT"), dk("vnew")], [bkey(sb0 + 1)])
                    for hh in range(HG):
                        MM(bank(sb0)[:, hh * 128:(hh + 1) * 128], B["ktl%d" % c][:, hh, :], vnew[:, hh, :], True, True,
                           [dk("ktl%d" % c), dk("vnew")], [bkey(sb0)])
                    TT("pool", Sg, Sg, bc_h(EA[:, 2 + c, n, :]), ALU.mult, [dk("Sg"), ("esc_all", d_)], [dk("Sg")])
                    TT("dve", Sg, Sg, v4(bank(sb0)), ALU.add, [dk("Sg"), bkey(sb0)], [dk("Sg")])
                    CP("act", Sg_bf, Sg, [dk("Sg")], [dk("Sg_bf")])
                    if not second:
                        CP("act", B["ostage"][sl], v4(bank(sb0 + 1))[sl], [bkey(sb0 + 1)], [dk("ostage")])
                    else:
                        TT("dve", osum[sl], v4(bank(sb0 + 1))[sl], oland[n % 2][sl], ALU.add,
                           [bkey(sb0 + 1), ("oland", n % 2)], ["osum"])
                if not second:
                    stored.add(n)
                    DMA("sp", gdn_o[n], B["ostage"].rearrange("p h d -> p (h d)"), ("ost", d_), [dk("ostage")], [("o_dram", n)])
                    return
                osq = fsig.rearrange("p (h d) -> p h d", h=HG)
                TT("pool", osq, osum, osum, ALU.mult, ["osum"], ["fsig"])
                P.op("dve", lambda e: e.tensor_reduce(out=frs[:, 0:HG], in_=osq, axis=AX.X, op=ALU.add), ["fsig"], ["frs"], cost=600.0)
                rstd_inplace(frs[:, 0:HG], 128, "frs")
                for half, ws in enumerate(wsl):
                    for kt in range(KT):
                        MM(bank(half), hT[:, kt, tsl], ws[:, kt, :], kt == 0, kt == KT - 1,
                           [("wsl", half), ("hT", n)], [bkey(half)])
                fGf = fG.rearrange("p h d -> p (h d)")
                for half in range(2):
                    ACTF(fsig, bank(half), AF.Exp, [bkey(half)], ["fsig"], scale=-1.0)
                    ACTF(fsig, fsig, AF.Ln, ["fsig"], ["fsig"], bias=1.0)
                    ACTF(fsig, fsig, AF.Exp, ["fsig"], ["fsig"], scale=-1.0)
                    if half == 0:
                        TT("dve", fGf, fsig, bank(0), ALU.mult, ["fsig", bkey(0)], ["fG"])
                    else:
                        TT("pool", fGf, fGf, fsig, ALU.mult, ["fsig", "fG"], ["fG"])
                TT("pool", fG, fG, bc_m(gdnw_bc), ALU.mult, ["fG", "gdnw_bc"], ["fG"])
                TT("pool", osum, osum, bc_h(frs[:, 0:HG]), ALU.mult, ["osum", "frs"], ["osum"])
                TT("pool", osum, osum, fG, ALU.mult, ["osum", "fG"], ["osum"])
                mslice = mixed[:, n, hs0 * 128:(hs0 + HG) * 128].rearrange("p (h d) -> p h d", h=HG)
                TT("dve", mslice, mslice, osum, ALU.add, ["osum", ("mixed", n)], [("mixed", n)])

            for d_ in range(2):
                MEMSET("pool", DB[d_]["vnew"], 0.0, [("vnew", d_)])
                MEMSET("dve", DB[d_]["Sg"], 0.0, [("Sg", d_)])
                CP("act", DB[d_]["Sg_bf"], DB[d_]["Sg"], [("Sg", d_)], [("Sg_bf", d_)])
            for i in range(NT):
                gdn_tile(0, i)
                gdn_tile(1, NT - 1 - i)
            MARK("d2")
            P.barrier()

        P.barrier()
        wout_d = dram("w_out", [D, D])
        off = 0
        x1, off = carve(off, [NT, D], F32)
        X1_END = off
        mT, off = carve(off, [KT, T], BF16)
        wout, off = carve(off, [KT, D], BF16)
        hn2 = [None, None]
        hn2[0], off = carve(off, [D], BF16)
        hn2[1], off = carve(off, [D], BF16)
        junk, off = carve(off, [D], BF16)
        n2_bc, off = carve(off, [D], F32)
        DMA("sp", n2_bc, n2_d.partition_broadcast(128), "c_n2", [], ["n2_bc"])
        DMA("pool", wout, wout_d.rearrange("(k p) c -> p k c", p=128), "w_wout", [], ["wout"])
        for n in range(NT):
            b = n % 2
            for kt in range(KT):
                TR(bbank(b)[:, kt * 128:(kt + 1) * 128], mixed[:, n, kt * 128:(kt + 1) * 128], ident_b[:],
                   [("mixed", n), "ident_b"], [("pbb", b)])
            CP("act", mT[:, :, n * 128:(n + 1) * 128], bbank(b).rearrange("p (k t) -> p k t", k=KT), [("pbb", b)], [("mT", n)])
        for n in range(NT):
            tsl = slice(n * 128, (n + 1) * 128)
            DMA("sp", x1[:, n, :], x_d[tsl, :], ("x1ld", n % 4), [], [("x1", n)])
            pp = pt[n % 2]
            for half in range(2):
                for kt in range(KT):
                    MM(pp[:, half * 512:(half + 1) * 512], mT[:, kt, tsl], wout[:, kt, half * 512:(half + 1) * 512],
                       kt == 0, kt == KT - 1, [("mT", n), "wout"], [bkey((n % 2) * 2 + half)])
            TT("dve", x1[:, n, :], x1[:, n, :], pp[:, :], ALU.add, [("x1", n), bkey((n % 2) * 2), bkey((n % 2) * 2 + 1)], [("x1", n)])
            b = n % 2
            ssap = small[:, 8 + b:9 + b]
            ssk = ("ss2", b)
            ACTF(junk, x1[:, n, :], AF.Square, [("x1", n)], ["junk", ssk], accum_out=ssap)
            rstd_inplace(ssap, D, ssk)
            STT(mixed[:, n, :], x1[:, n, :], ssap, n2_bc, ALU.mult, ALU.mult, [("x1", n), ssk, "n2_bc"], [("mixed", n)])
            for kt in range(KT):
                TR(bbank(b)[:, kt * 128:(kt + 1) * 128], mixed[:, n, kt * 128:(kt + 1) * 128], ident_b[:],
                   [("mixed", n), "ident_b"], [("pbb", b)])
            CP("act", hT[:, :, tsl], bbank(b).rearrange("p (k t) -> p k t", k=KT), [("pbb", b)], [("hT", n)])
        MARK("e0")

        P.barrier()
        NB = 64
        wr_d = dram("moe_wr", [D, 36])
        wgu0_d = dram("moe_wgu0", [4096, 2048])
        wgu1_d = dram("moe_wgu1", [4096, 2048])
        wdr_d = dram("moe_wdr", [4096, 2048])
        xb_d = nc.dram_tensor("moe_xb", [NB * 128, D], BF16, kind="Internal").ap()
        yb_d = nc.dram_tensor("moe_yb", [NB * 128, D], F32, kind="Internal").ap()
        off = X1_END
        stg = []
        for i in range(3):
            a, off = carve(off, [2048], F32)
            stg.append(a)
        wgu_bf = []
        wd_bf = []
        for i in range(2):
            a, off = carve(off, [KT, 512], BF16)
            wgu_bf.append(a)
            a, off = carve(off, [2, D], BF16)
            wd_bf.append(a)
        wr, off = carve(off, [KT, 36], BF16)
        lg, off = carve(off, [NT, 36], F32)
        oh1, off = carve(off, [NT, 32], F32)
        oh2, off = carve(off, [NT, 32], F32)
        msk, off = carve(off, [NT, 32], F32)
        rank, off = carve(off, [NT, 32], F32)
        tmp3, off = carve(off, [NT, 32], F32)
        gtmp, off = carve(off, [NT, 4], F32)
        ohg, off = carve(off, [NT, 4], F32)
        rv, off = carve(off, [8, NT], F32)
        mcum, off = carve(off, [32], F32)
        cnt, off = carve(off, [32], F32)
        padded, off = carve(off, [32], F32)
        ends, off = carve(off, [32], F32)
        pstart, off = carve(off, [32], F32)
        ebf, off = carve(off, [NB], F32)
        widx_f, off = carve(off, [NB], F32)
        widx, off = carve(off, [NB], I32)
        dest_f, off = carve(off, [2, NT], F32)
        dest_i, off = carve(off, [2, NT], I32)
        MOE_END = off
        cmpb = stg[0].rearrange("p (b e) -> p b e", b=NB)
        cmpj = stg[1][:, 0:512].rearrange("p (e j) -> p e j", e=32)
        ht32 = hT[:].rearrange("p k t -> p (k t)").bitcast(F32)
        HTCAP = 32 * 1024
        hoff = 0
        xg, xgT, sil, hid_bf, hidT, ysb = [], [], [], [], [], []
        for i in range(2):
            a, hoff = carve(hoff, [D], BF16, ht32, HTCAP); xg.append(a)
            a, hoff = carve(hoff, [KT, 128], BF16, ht32, HTCAP); xgT.append(a)
            a, hoff = carve(hoff, [256], F32, ht32, HTCAP); sil.append(a)
            a, hoff = carve(hoff, [256], BF16, ht32, HTCAP); hid_bf.append(a)
            a, hoff = carve(hoff, [2, 128], BF16, ht32, HTCAP); hidT.append(a)
            a, hoff = carve(hoff, [D], F32, ht32, HTCAP); ysb.append(a)
        mix32 = mixed[:].rearrange("p n d -> p (n d)").bitcast(F32)
        MIXCAP = 32 * 1024
        moff = 0
        yg = []
        for i in range(2):
            a, moff = carve(moff, [D], F32, mix32, MIXCAP); yg.append(a)
        stgB = []
        for i in range(3):
            a, moff = carve(moff, [2048], F32, mix32, MIXCAP); stgB.append(a)

        DMA("pool", wr, wr_d.rearrange("(k p) c -> p k c", p=128), "w_wr", [], ["wr"])
        for n in range(NT):
            bi = n % 2
            for kt in range(KT):
                MM(bank(bi)[:, 0:36], hT[:, kt, n * 128:(n + 1) * 128], wr[:, kt, :], kt == 0, kt == KT - 1,
                   ["wr", ("hT", n)], [bkey(bi)])
            CP("act", lg[:, n, :], bank(bi)[:, 0:36], [bkey(bi)], ["lg"])
        BIG = 10000.0
        glv = lg[:, :, 0:4]
        elv = lg[:, :, 4:36]

        def RED(out, in_, op, R, W):
            P.op("dve", lambda e: e.tensor_reduce(out=out, in_=in_, axis=AX.X, op=op), R, W, cost=100.0 + _fsz(in_) * 1.0)

        def bcn(ap2, k):
            return ap2.unsqueeze(2).to_broadcast([128, NT, k])

        gmax, gsum, m1, m2, w1, w2 = (rv[:, i, :] for i in range(6))
        RED(gmax, glv, ALU.max, ["lg"], ["rv"])
        TT("dve", ohg, glv, bcn(gmax, 4), ALU.is_equal, ["lg", "rv"], ["ohg"])
        TT("dve", gtmp, glv, bcn(gmax, 4), ALU.subtract, ["lg", "rv"], ["gtmp"])
        ACTF(gtmp, gtmp, AF.Exp, ["gtmp"], ["gtmp"])
        RED(gsum, gtmp, ALU.add, ["gtmp"], ["rv"])
        RECIP(gsum, gsum, ["rv"], ["rv"])
        TS("dve", ohg, ohg, BIG, -BIG, ALU.mult, ALU.add, ["ohg"], ["ohg"])
        TT("dve", msk.rearrange("p n (g e) -> p n g e", g=4), elv.rearrange("p n (g e) -> p n g e", g=4),
           ohg.unsqueeze(3).to_broadcast([128, NT, 4, 8]), ALU.add, ["lg", "ohg"], ["msk"])
        RED(m1, msk, ALU.max, ["msk"], ["rv"])
        TT("dve", oh1, msk, bcn(m1, 32), ALU.is_equal, ["msk", "rv"], ["oh1"])
        STT(msk, oh1, -BIG, msk, ALU.mult, ALU.add, ["oh1", "msk"], ["msk"])
        RED(m2, msk, ALU.max, ["msk"], ["rv"])
        TT("dve", oh2, msk, bcn(m2, 32), ALU.is_equal, ["msk", "rv"], ["oh2"])
        TT("dve", w2, m2, m1, ALU.subtract, ["rv"], ["rv"])
        ACTF(w2, w2, AF.Exp, ["rv"], ["rv"])
        TS("dve", w1, w2, 1.0, None, ALU.add, ALU.bypass, ["rv"], ["rv"])
        RECIP(w1, w1, ["rv"], ["rv"])
        TT("dve", w1, w1, gsum, ALU.mult, ["rv"], ["rv"])
        TT("dve", w2, w2, w1, ALU.mult, ["rv"], ["rv"])
        TT("dve", msk, oh1, oh2, ALU.add, ["oh1", "oh2", "msk"], ["msk"])
        MEMSET("dve", mcum, 0.0, ["mcum"])
        for n in range(NT):
            bi = n % 2
            MM(bank(bi)[:, 0:32], cst("m_lt"), msk[:, n, :], True, False, ["consts", "msk"], [bkey(bi)])
            MM(bank(bi)[:, 0:32], cst("ones"), mcum, False, True, ["consts", "mcum"], [bkey(bi)])
            CP("act", rank[:, n, :], bank(bi)[:, 0:32], [bkey(bi)], ["rank"])
            TT("dve", mcum, mcum, msk[:, n, :], ALU.add, ["mcum", "msk"], ["mcum"])
        MM(bank(0)[:, 0:32], cst("ones"), mcum, True, True, ["consts", "mcum"], [bkey(0)])
        CP("act", cnt, bank(0)[:, 0:32], [bkey(0)], ["cnt"])
        TT("dve", cmpj, cnt.unsqueeze(2).to_broadcast([128, 32, 16]),
           cst("bvals")[:, 0:16].unsqueeze(1).to_broadcast([128, 32, 16]), ALU.is_gt, ["cnt", "consts"], [("stg", 1)])
        RED(padded, cmpj, ALU.add, [("stg", 1)], ["padded"])
        TS("dve", padded, padded, 128.0, None, ALU.mult, ALU.bypass, ["padded"], ["padded"])
        P.op("dve", lambda e: e.tensor_tensor_scan(out=ends, data0=cst("ones")[:, 0:32], data1=padded, initial=0.0,
                                                  op0=ALU.mult, op1=ALU.add), ["consts", "padded"], ["ends"], cost=300.0)
        TT("dve", pstart, ends, padded, ALU.subtract, ["ends", "padded"], ["pstart"])
        TT("dve", rank, rank, pstart.unsqueeze(1).to_broadcast([128, NT, 32]), ALU.add, ["rank", "pstart"], ["rank"])
        for k, ohk in ((0, oh1), (1, oh2)):
            TT("dve", tmp3, ohk, rank, ALU.mult, ["oh1", "oh2", "rank"], ["tmp3"])
            RED(dest_f[:, k, :], tmp3, ALU.add, ["tmp3"], ["dest_f"])
        CP("dve", dest_i, dest_f, ["dest_f"], ["dest_i"])
        TT("dve", cmpb, ends.unsqueeze(1).to_broadcast([128, NB, 32]),
           cst("bvals")[:, 0:NB].unsqueeze(2).to_broadcast([128, NB, 32]), ALU.is_le, ["ends", "consts"], [("stg", 0)])
        RED(ebf, cmpb, ALU.add, [("stg", 0)], ["ebf"])
        STT(widx_f, ebf, 128.0, cst("pidx")[:, 0:NB], ALU.mult, ALU.add, ["ebf", "consts"], ["widx_f"])
        CP("dve", widx, widx_f, ["widx_f"], ["widx"])
        MARK("e1")

        IOA = bass.IndirectOffsetOnAxis
        regs = {}

        def _pool_init(e):
            regs["bc"] = e.alloc_register("moe_bc")
            e.reg_mov(regs["bc"], 4095)
        P.pool_init = _pool_init
        XB_KEYS = []
        zt, off = carve(off, [D], BF16)
        MEMSET("pool", zt, 0.0, ["zt"])
        DMA("sp", xb_d.rearrange("(p r) d -> p r d", p=128), zt.unsqueeze(1).to_broadcast([128, NB, D]), "xbz", ["zt"], ["xb0"])
        for n in range(NT):
            for k in range(2):
                idx_ap = dest_i[:, k, n:n + 1]
                src_ap = mixed[:, n, :]
                P.dma("pool", lambda e, idx_ap=idx_ap, src_ap=src_ap: e.indirect_dma_start(
                    out=xb_d[:, :], out_offset=IOA(ap=idx_ap, axis=0), in_=src_ap, in_offset=None),
                    ("sc", (2 * n + k) % 4), [("mixed", n), "dest_i", "xb0"], [("xb", n, k)], nbytes=256 * 1024)
                XB_KEYS.append(("xb", n, k))

        def gather_w(dst, src_d, b, skey, extra):
            idx_ap = widx[:, b:b + 1]
            P.dma("pool", lambda e: e.indirect_dma_start(
                out=dst, out_offset=None, in_=src_d[:, :], in_offset=IOA(ap=idx_ap, axis=0),
                bounds_check=regs["bc"], oob_is_err=False),
                skey, ["widx"] + extra, [skey], nbytes=1 << 20)

        YB_KEYS = []
        for b in range(NB):
            s = b % 2
            sset = stg if b % 2 == 0 else stgB
            so = 0 if b % 2 == 0 else 3
            extra = [] if b % 2 == 0 else XB_KEYS
            gather_w(sset[0], wgu0_d, b, ("stg", so + 0), extra)
            gather_w(sset[1], wgu1_d, b, ("stg", so + 1), extra)
            gather_w(sset[2], wdr_d, b, ("stg", so + 2), extra)
            CP("act", wgu_bf[s][:, 0:4, :], sset[0].rearrange("p (k c) -> p k c", k=4), [("stg", so + 0)], [("wgu_bf", s, 0)])
            CP("dve", wgu_bf[s][:, 4:8, :], sset[1].rearrange("p (k c) -> p k c", k=4), [("stg", so + 1)], [("wgu_bf", s, 1)])
            CP("act" if b % 4 < 2 else "dve", wd_bf[s], sset[2].rearrange("p (k c) -> p k c", k=2), [("stg", so + 2)], [("wd_bf", s)])
            DMA("sp", xg[s], xb_d[b * 128:(b + 1) * 128, :], ("xg", s), XB_KEYS, [("xg", s)])
            for kt in range(KT):
                TR(bbank(s)[:, kt * 128:(kt + 1) * 128], xg[s][:, kt * 128:(kt + 1) * 128], ident_b[:],
                   [("xg", s), "ident_b"], [("pbb", s)])
            CP("act", xgT[s], bbank(s).rearrange("p (k t) -> p k t", k=KT), [("pbb", s)], [("xgT", s)])
            hb = bank(s)
            for kt in range(KT):
                MM(hb, xgT[s][:, kt, :], wgu_bf[s][:, kt, :], kt == 0, kt == KT - 1,
                   [("xgT", s), ("wgu_bf", s, 0), ("wgu_bf", s, 1)], [bkey(s)])
            ACTF(sil[s], hb[:, 0:256], AF.Silu, [bkey(s)], [("sil", s)])
            TT("dve", hid_bf[s], sil[s], hb[:, 256:512], ALU.mult, [("sil", s), bkey(s)], [("hid_bf", s)])
            for ft in range(2):
                TR(bbank(s)[:, ft * 128:(ft + 1) * 128], hid_bf[s][:, ft * 128:(ft + 1) * 128], ident_b[:],
                   [("hid_bf", s), "ident_b"], [("pbb", s)])
            CP("act", hidT[s], bbank(s)[:, 0:256].rearrange("p (k t) -> p k t", k=2), [("pbb", s)], [("hidT", s)])
            yp = pt[1 + s]
            for half in range(2):
                for ft in range(2):
                    MM(yp[:, half * 512:(half + 1) * 512], hidT[s][:, ft, :], wd_bf[s][:, ft, half * 512:(half + 1) * 512],
                       ft == 0, ft == 1, [("hidT", s), ("wd_bf", s)], [bkey(2 + 2 * s + half)])
            CP("act" if b % 2 else "dve", ysb[s], yp[:, :], [bkey(2 + 2 * s), bkey(3 + 2 * s)], [("ysb", s)])
            DMA("sp", yb_d[b * 128:(b + 1) * 128, :], ysb[s], ("yst", s), [("ysb", s)], [("yb", b)])
            YB_KEYS.append(("yb", b))
        MARK("e2")
        ygs = list(yg)
        for sb_ in stgB:
            ygs.append(sb_[:, 0:1024])
            ygs.append(sb_[:, 1024:2048])
        for n in range(NT):
            for k in range(2):
                s = (2 * n + k) % len(ygs)
                idx_ap = dest_i[:, k, n:n + 1]
                dst = ygs[s]
                P.dma("pool", lambda e, idx_ap=idx_ap, dst=dst: e.indirect_dma_start(
                    out=dst, out_offset=None, in_=yb_d[:, :], in_offset=IOA(ap=idx_ap, axis=0)),
                    ("yg", s), YB_KEYS + ["dest_i"], [("yg", s)], nbytes=512 * 1024)
                wk = rv[:, 4 + k, n:n + 1]
                STT(x1[:, n, :], ygs[s], wk, x1[:, n, :], ALU.mult, ALU.add, [("yg", s), "rv", ("x1", n)], [("x1", n)])

        P.barrier()
        off = X1_END
        nf_bc, off = carve(off, [D], F32)
        ob = [None, None]
        ob[0], off = carve(off, [D], F32)
        ob[1], off = carve(off, [D], F32)
        junk2, off = carve(off, [D], BF16)
        DMA("sp", nf_bc, nf_d.partition_broadcast(128), "c_nf", [], ["nf_bc"])
        for n in range(NT):
            b = n % 2
            ssap = small[:, 12 + b:13 + b]
            ssk = ("ss3", b)
            ACTF(junk2, x1[:, n, :], AF.Square, [("x1", n)], ["junk2", ssk], accum_out=ssap)
            rstd_inplace(ssap, D, ssk)
            STT(ob[b], x1[:, n, :], ssap, nf_bc, ALU.mult, ALU.mult, [("x1", n), ssk, "nf_bc"], [("ob", b)])
            DMA("sp", out_d[n * 128:(n + 1) * 128, :], ob[b], ("out_st", b), [("ob", b)], [("out", n)])
        if not dbg:
            P.wait_all("sp", [("out", n) for n in range(NT)])
        if dbg:
            P.enabled = True
            P.barrier()
            for n in range(NT):
                DMA("sp", dbg_d[n * 128:(n + 1) * 128, :], x1[:, n, :], ("dbg_out", n % 2), [("x1", n)], [("dbg", n)])
            P.wait_all("sp", [("dbg", n) for n in range(NT)] + [("out", n) for n in range(NT)])
        P.emit()
    return nc


def make_in_maps(inputs, n_cores=8):
    f = lambda k: np.asarray(inputs[k], np.float32)
    x = f("x")
    _gu = np.concatenate([f("moe_w_gate")[0], f("moe_w_up")[0]], axis=2).reshape(32, 8, 128, 512).transpose(0, 2, 1, 3)
    shared = {
        "norm1_w": f("norm1_w").reshape(1, D),
        "norm2_w": f("norm2_w").reshape(1, D),
        "norm_f_w": f("norm_f_w").reshape(1, D),
        "consts": CONST_ARR,
        "w_in": np.ascontiguousarray(f("w_in")[0]),
        "gla_w2b_f": np.ascontiguousarray(np.concatenate([f("gla_gate_w2_fwd")[0], f("gla_gate_b_fwd")], axis=0)),
        "gla_w2b_b": np.ascontiguousarray(np.concatenate([f("gla_gate_w2_bwd")[0], f("gla_gate_b_bwd")], axis=0)),
        "gla_norm_w": f("gla_norm_w").reshape(1, 256),
        "w_out": np.ascontiguousarray(f("w_out")[0]),
        "moe_wr": np.ascontiguousarray(np.concatenate([f("moe_w_group")[0], f("moe_w_router")[0]], axis=1)),
        "moe_wgu0": _gu[:, :, 0:4, :].reshape(4096, 2048).copy(),
        "moe_wgu1": _gu[:, :, 4:8, :].reshape(4096, 2048).copy(),
        "moe_wdr": np.ascontiguousarray(f("moe_w_down")[0].reshape(32, 2, 128, 1024).transpose(0, 2, 1, 3)).reshape(4096, 2048),
        "gdn_norm_w": f("gdn_norm_w").reshape(1, 128),
        "gdn_vec": np.ascontiguousarray(np.concatenate([f("gdn_dt_bias_fwd")[0], f("gdn_dt_bias_bwd")[0],
                                                        f("gdn_a_log_fwd")[0], f("gdn_a_log_bwd")[0]]).reshape(1, 32)),
        "gdn_conv_wT": np.ascontiguousarray(f("gdn_conv_w")[0].T.reshape(24, 128, 5).transpose(1, 0, 2)),
    }
    maps = []
    for c in range(n_cores):
        m = dict(shared)
        m["x"] = np.ascontiguousarray(x[c])
        maps.append(m)
    return maps


def kernel(**inputs):
    nc = build()
    in_maps = make_in_maps(inputs)
    res = run_bass_kernel_spmd(nc, in_maps, core_ids=list(range(8)))
    out = np.stack([np.asarray(r["out"]) for r in res.results], axis=0)
    return out.astype(np.float32)
```

```python
import contextlib
import heapq
import numpy as np
import concourse.bass as bass
import concourse.mybir as mybir
from concourse.bass_utils import run_bass_kernel_spmd

F32 = mybir.dt.float32
BF16 = mybir.dt.bfloat16
I32 = mybir.dt.int32
AF = mybir.ActivationFunctionType
ALU = mybir.AluOpType
AX = mybir.AxisListType

T = 2048
D = 1024
NT = T // 128
KT = D // 128
EPS = 1e-6
SAME_ENGINE_SYNC = True
EPOCH = 20000
SYNC_NS = 250.0
DMA_LAT_NS = 2200.0
PRIO = True


class Prog:
    ENGS = ("pe", "act", "dve", "pool", "sp")

    def __init__(self, nc, stack):
        self.nc = nc
        self.stack = stack
        self.streams = {e: [] for e in self.ENGS}
        self.count = {e: 0 for e in self.ENGS}
        self.esems = {e: [] for e in self.ENGS}
        self.known = {e: {} for e in self.ENGS}
        self.last_write = {}
        self.readers = {}
        self.dma_sems = {}
        self.dma_vals = {}
        self.dma_last = {}
        self.enabled = True
        self.seg = []
        self.ticks = {}
        self.nops = 0
        self.seg_base = 0
        self.pool_init = None

    def _new_sem(self, name):
        return self.stack.enter_context(self.nc.semaphore(name))

    @staticmethod
    def _psum_fix(reads, writes):
        r2, w2 = [], list(writes)
        for k in reads:
            if isinstance(k, tuple) and k[0] in ("pb", "pbb"):
                if k not in w2:
                    w2.append(k)
            else:
                r2.append(k)
        return r2, w2

    def _record(self, eng, fn, reads, writes, cost, kind, semkey=None):
        reads, writes = self._psum_fix(list(reads), list(writes))
        oid = self.nops
        self.nops += 1
        preds = set()
        for r in reads:
            t = self.last_write.get(r)
            if t is not None:
                preds.add(t)
        for w in writes:
            t = self.last_write.get(w)
            if t is not None:
                preds.add(t)
            preds.update(self.readers.get(w, ()))
        if kind == "dma":
            prev = self.dma_last.get(semkey)
            if prev is not None:
                preds.add(prev)
            self.dma_last[semkey] = oid
        preds = {p for p in preds if p >= self.seg_base}
        self.seg.append(dict(id=oid, eng=eng, fn=fn, preds=preds, cost=float(cost), kind=kind, semkey=semkey))
        for w in writes:
            self.last_write[w] = oid
            self.readers[w] = []
        for r in reads:
            self.readers.setdefault(r, []).append(oid)
        return oid

    def op(self, eng, fn, reads=(), writes=(), cost=300.0):
        if not self.enabled:
            return
        self._record(eng, fn, reads, writes, cost, "op")

    def dma(self, eng, fn, semkey, reads=(), writes=(), nbytes=1 << 20):
        if not self.enabled:
            return
        self._record(eng, fn, reads, writes, DMA_LAT_NS + nbytes / 160.0, "dma", semkey)

    def wait_all(self, eng, keys):
        self._record(eng, None, list(keys), [], 0.0, "op")

    def _schedule_segment(self):
        ops = self.seg
        if not ops:
            return
        byid = {o["id"]: o for o in ops}
        succ = {o["id"]: [] for o in ops}
        indeg = {}
        for o in ops:
            indeg[o["id"]] = len(o["preds"])
            for p in o["preds"]:
                succ[p].append(o["id"])
        ready_t = {o["id"]: 0.0 for o in ops}
        finish = {}
        bl = {}
        for o in reversed(ops):
            m = 0.0
            for s_ in succ[o["id"]]:
                if bl[s_] > m:
                    m = bl[s_]
            bl[o["id"]] = o["cost"] + m
        future = {e: [] for e in self.ENGS}
        avail = {e: [] for e in self.ENGS}
        for o in ops:
            if indeg[o["id"]] == 0:
                heapq.heappush(future[o["eng"]], (0.0, o["id"]))
        etime = {e: 0.0 for e in self.ENGS}
        order = {e: [] for e in self.ENGS}
        remaining = len(ops)
        while remaining:
            best = None
            for e in self.ENGS:
                fu, av = future[e], avail[e]
                while fu and fu[0][0] <= etime[e]:
                    rt, oid = heapq.heappop(fu)
                    heapq.heappush(av, (-bl[oid] if PRIO else rt, oid))
                if av:
                    cand = (etime[e], av[0][0], av[0][1], e, True)
                elif fu:
                    cand = (fu[0][0], 0.0, fu[0][1], e, False)
                else:
                    continue
                if best is None or cand[:3] < best[:3]:
                    best = cand
            st, _, oid, e, from_av = best
            if from_av:
                heapq.heappop(avail[e])
            else:
                heapq.heappop(future[e])
            o = byid[oid]
            if o["kind"] == "dma":
                etime[e] = st + 150.0
                fin = st + o["cost"]
            else:
                etime[e] = st + o["cost"]
                fin = etime[e]
            finish[oid] = fin
            order[e].append(o)
            remaining -= 1
            for s in succ[oid]:
                so = byid[s]
                lat = SYNC_NS if (so["eng"] != e or o["kind"] == "dma") else (60.0 if e != "pe" else 0.0)
                ready_t[s] = max(ready_t[s], fin + lat)
                indeg[s] -= 1
                if indeg[s] == 0:
                    heapq.heappush(future[so["eng"]], (ready_t[s], s))
        self.est_ns = getattr(self, "est_ns", 0.0) + max(list(finish.values()) + [0.0])
        for o in ops:
            if o["kind"] == "dma":
                k = o["semkey"]
                if k not in self.dma_sems:
                    self.dma_sems[k] = self._new_sem(f"d{len(self.dma_sems)}")
                    self.dma_vals[k] = 0
                self.dma_vals[k] += 16
                self.ticks[o["id"]] = (self.dma_sems[k], self.dma_vals[k], "dma")
        def needs_sem(o):
            for s_ in succ[o["id"]]:
                se = byid[s_]["eng"]
                if se != o["eng"] or (SAME_ENGINE_SYNC and se != "pe"):
                    return True
            return False
        for e in self.ENGS:
            real = [o for o in order[e] if o["kind"] == "op" and o["fn"] is not None]
            for i_, o in enumerate(real):
                o["sig"] = needs_sem(o) or i_ == len(real) - 1
        for e in self.ENGS:
            for o in order[e]:
                if o["kind"] == "op" and o["fn"] is not None and o["sig"]:
                    c = self.count[e]
                    ep, v = divmod(c, EPOCH)
                    while len(self.esems[e]) <= ep:
                        self.esems[e].append(self._new_sem(f"s_{e}_{len(self.esems[e])}"))
                    self.count[e] = c + 1
                    self.ticks[o["id"]] = (self.esems[e][ep], v + 1, e)
        for e in self.ENGS:
            for o in order[e]:
                waits = {}
                for p in o["preds"]:
                    if byid[p]["eng"] == e and byid[p]["kind"] == "op" and (not SAME_ENGINE_SYNC or e == "pe"):
                        continue
                    sem, val, src = self.ticks[p]
                    sid = id(sem)
                    if self.known[e].get(sid, 0) >= val:
                        continue
                    if sid not in waits or waits[sid][1] < val:
                        waits[sid] = (sem, val)
                for sid, (sem, val) in waits.items():
                    self.known[e][sid] = val
                inc = None
                if o["fn"] is not None and o["id"] in self.ticks:
                    sem, val, src = self.ticks[o["id"]]
                    inc = (sem, 16 if o["kind"] == "dma" else 1)
                self.streams[e].append((o["fn"], list(waits.values()), inc))
        self.seg = []
        self.seg_base = self.nops

    def barrier(self):
        if not self.enabled and not self.seg:
            return
        self._schedule_segment()
        ticks = []
        for e2 in self.ENGS:
            c = self.count[e2]
            if c > 0:
                ep, v = divmod(c - 1, EPOCH)
                ticks.append((self.esems[e2][ep], v + 1))
        for k, sem in self.dma_sems.items():
            ticks.append((sem, self.dma_vals[k]))
        for eng in self.ENGS:
            waits = []
            for (sem, val) in ticks:
                if self.known[eng].get(id(sem), 0) >= val:
                    continue
                self.known[eng][id(sem)] = val
                waits.append((sem, val))
            if waits:
                self.streams[eng].append((None, waits, None))

    def emit(self):
        self._schedule_segment()
        nc = self.nc
        with nc.Block() as block:
            def run(e, stream):
                for fn, waits, inc in stream:
                    for sem, val in waits:
                        e.wait_ge(sem, val)
                    if fn is None:
                        continue
                    ins = fn(e)
                    if inc is not None:
                        ins.then_inc(inc[0], inc[1])

            @block.tensor
            def _(e):
                run(e, self.streams["pe"])

            @block.scalar
            def _(e):
                run(e, self.streams["act"])

            @block.vector
            def _(e):
                run(e, self.streams["dve"])

            @block.gpsimd
            def _(e):
                if self.pool_init is not None:
                    self.pool_init(e)
                run(e, self.streams["pool"])

            @block.sync
            def _(e):
                run(e, self.streams["sp"])


def _fsz(ap):
    s = ap.shape
    n = 1
    for v in s[1:]:
        n *= int(v)
    return n


C_GQ, C_GK, C_GV, C_GR = 0, 512, 1024, 2048
C_GLF, C_GLB = 3072, 3088
C_DQ, C_DK, C_DV, C_DZ = 3104, 4128, 5152, 6176
C_DAB = 7200
C_MA, C_MB = 7232, 8256
D_IN = 9280


def host_consts():
    r = np.arange(128)[:, None]
    t = np.arange(128)[None, :]
    same = (r // 64) == (t // 64)
    c = {}
    c["ident"] = np.eye(128, dtype=np.float32)
    c["a_le"] = np.where(r <= t, -1.0 / 16, 0.0)
    c["a_ge"] = np.where(r >= t, -1.0 / 16, 0.0)
    c["a_gt"] = np.where(r > t, -1.0 / 16, 0.0)
    c["a_lt"] = np.where(r < t, -1.0 / 16, 0.0)
    c["m_le"] = np.where(r <= t, 1.0, 0.0)
    c["m_ge"] = np.where(r >= t, 1.0, 0.0)
    c["b_le"] = np.where((r <= t) & same, 1.0, 0.0)
    c["b_ge"] = np.where((r >= t) & same, 1.0, 0.0)
    c["b_gt"] = np.where((r > t) & same, 1.0, 0.0)
    c["b_lt"] = np.where((r < t) & same, 1.0, 0.0)
    c["csel0"] = np.where(r < 64, 1.0, 0.0) + 0.0 * t
    c["csel1"] = np.where(r >= 64, 1.0, 0.0) + 0.0 * t
    c["ones"] = np.ones((128, 128))
    c["m_lt"] = np.where(r < t, 1.0, 0.0)
    c["bvals"] = 128.0 * t + 0.0 * r
    c["pidx"] = 1.0 * r + 0.0 * t
    names = list(c.keys())
    arr = np.stack([np.asarray(c[n], np.float32) for n in names], axis=1)
    return names, np.ascontiguousarray(arr)


CONST_NAMES, CONST_ARR = host_consts()
NCONST = len(CONST_NAMES)


COST = dict(pe_a=55.0, pe_b=0.45, pe_f32=3.0, tr=110.0, act_a=200.0, act_b=1.2, dve_a=100.0, dve_b=0.8,
            pool_a=150.0, pool_b=2.4)


def build(stage="all", dbg=False):
    nc = bass.Bass("TRN2", target_bir_lowering=False)
    stack = contextlib.ExitStack()
    with stack:
        P = Prog(nc, stack)

        def dram(name, shape, dt=F32, kind="ExternalInput"):
            return nc.dram_tensor(name, list(shape), dt, kind=kind).ap()

        def sb(name, shape, dt=F32):
            return stack.enter_context(nc.sbuf_tensor(name, list(shape), dt))

        def ps(name, shape, dt=F32):
            return stack.enter_context(nc.psum_tensor(name, list(shape), dt))

        def MM(out, lhsT, rhs, start, stop, R, W):
            n = _fsz(rhs)
            c = COST["pe_a"] + n * COST["pe_b"]
            if rhs.dtype == F32:
                c *= COST["pe_f32"]
            P.op("pe", lambda e: e.matmul(out, lhsT, rhs, start=start, stop=stop), R, W, cost=c)

        def TR(out, in_, ident, R, W):
            P.op("pe", lambda e: e.transpose(out=out, in_=in_, identity=ident), R, W, cost=COST["tr"])

        def ACTF(out, in_, func, R, W, **kw):
            c = COST["act_a"] + _fsz(in_) * COST["act_b"] + (90.0 if "accum_out" in kw else 0.0)
            P.op("act", lambda e: e.activation(out=out, in_=in_, func=func, **kw), R, W, cost=c)

        def _vc(eng, n, k=1.5):
            return (COST["dve_a"] + n * k * COST["dve_b"]) if eng == "dve" else (COST["pool_a"] + n * COST["pool_b"])

        def TT(eng, out, in0, in1, op, R, W):
            P.op(eng, lambda e: e.tensor_tensor(out=out, in0=in0, in1=in1, op=op), R, W, cost=_vc(eng, _fsz(out)))

        def TS(eng, out, in0, s1, s2, op0, op1, R, W):
            P.op(eng, lambda e: e.tensor_scalar(out=out, in0=in0, scalar1=s1, scalar2=s2, op0=op0, op1=op1), R, W,
                 cost=_vc(eng, _fsz(out), 1.05))

        def STT(out, in0, scalar, in1, op0, op1, R, W):
            P.op("dve", lambda e: e.scalar_tensor_tensor(out=out, in0=in0, scalar=scalar, in1=in1, op0=op0, op1=op1), R, W,
                 cost=_vc("dve", _fsz(out)))

        def CP(eng, out, in_, R, W):
            if eng == "act":
                P.op("act", lambda e: e.activation(out=out, in_=in_, func=AF.Copy), R, W, cost=COST["act_a"] + _fsz(in_) * COST["act_b"])
            else:
                P.op(eng, lambda e: e.tensor_copy(out=out, in_=in_), R, W, cost=_vc(eng, _fsz(out), 1.05))

        def MEMSET(eng, ap, val, W):
            P.op(eng, lambda e: e.memset(ap, val), [], W, cost=_vc(eng, _fsz(ap), 0.6))

        def DMA(eng, out, in_, semkey, R, W):
            P.dma(eng, lambda e: e.dma_start(out=out, in_=in_), semkey, R, W, nbytes=_fsz(out) * int(out.shape[0]) * 4)

        def RECIP(out, in_, R, W):
            P.op("dve", lambda e: e.reciprocal(out=out, in_=in_), R, W, cost=_vc("dve", _fsz(out), 1.05))

        def MARK(name):
            if stage == name:
                P.enabled = False

        def rstd_inplace(ap, n, key):
            TS("dve", ap, ap, 1.0 / n, EPS, ALU.mult, ALU.add, [key], [key])
            ACTF(ap, ap, AF.Ln, [key], [key])
            ACTF(ap, ap, AF.Exp, [key], [key], scale=-0.5)

        x_d = dram("x", [T, D])
        n1_d = dram("norm1_w", [1, D])
        n2_d = dram("norm2_w", [1, D])
        nf_d = dram("norm_f_w", [1, D])
        consts_d = dram("consts", [128, NCONST, 128])
        w_in_d = dram("w_in", [D, D_IN])
        w2b_d = [dram("gla_w2b_f", [17, 512]), dram("gla_w2b_b", [17, 512])]
        gnw_d = dram("gla_norm_w", [1, 256])
        out_d = dram("out", [T, D], kind="ExternalOutput")
        dbg_d = dram("dbg", [T, D], kind="ExternalOutput") if dbg else None

        consts = sb("consts_sb", [128, NCONST, 128])
        CI = {n: i for i, n in enumerate(CONST_NAMES)}

        def cst(name):
            return consts[:, CI[name], :]

        ident_b = sb("ident_b", [128, 128], BF16)
        ones_b = sb("ones_b", [128, 128], BF16)
        hT = sb("hT", [128, KT, T], BF16)
        mixed = sb("mixed", [128, NT, D], BF16)
        small = sb("small", [128, 64])
        ARENA_BYTES = 134 * 1024
        arena = sb("arena", [128, ARENA_BYTES // 4])

        def carve(off, shape, dt, base=None, cap=None):
            base = arena if base is None else base
            cap = ARENA_BYTES if cap is None else cap
            nb = int(np.prod(shape)) * (2 if dt == BF16 else 4)
            nb = (nb + 3) // 4 * 4
            assert off % 4 == 0 and off + nb <= cap, (off, nb)
            v = base[:, off // 4:(off + nb) // 4]
            if dt != F32:
                v = v.bitcast(dt)
            if len(shape) == 2:
                pat = "p (a b) -> p a b"
                v = v.rearrange(pat, a=shape[0])
            elif len(shape) == 3:
                v = v.rearrange("p (a b c) -> p a b c", a=shape[0], b=shape[1])
            return v, off + nb

        pt = [ps(f"pt{i}", [128, 1024]) for i in range(3)]
        ptb = ps("ptb", [128, 2048], BF16)

        def bank(i):
            return pt[i // 2][:, (i % 2) * 512:(i % 2 + 1) * 512]

        def bkey(i):
            return ("pb", i)

        def bbank(i):
            return ptb[:, i * 1024:(i + 1) * 1024]

        DMA("sp", consts[:], consts_d[:, :, :], "c_consts", [], ["consts"])
        CP("dve", ident_b[:], cst("ident"), ["consts"], ["ident_b"])
        MEMSET("pool", ones_b[:], 1.0, ["ones_b"])

        off = 116 * 1024
        xt0, off = carve(off, [D], F32)
        xt1, off = carve(off, [D], F32)
        hn0, off = carve(off, [D], BF16)
        hn1, off = carve(off, [D], BF16)
        sq, off = carve(off, [D], BF16)
        n1_bc, off = carve(off, [D], F32)
        DMA("sp", n1_bc, n1_d.partition_broadcast(128), "c_n1", [], ["n1_bc"])
        xts = [xt0, xt1]
        hns = [hn0, hn1]
        for tt in range(NT):
            b = tt % 2
            xb, hb = xts[b], hns[b]
            DMA("sp", xb, x_d[tt * 128:(tt + 1) * 128, :], ("xt", b), [], [("xt", b)])
            ACTF(sq, xb, AF.Square, [("xt", b)], ["sq", "ss0"], accum_out=small[:, 0:1])
            rstd_inplace(small[:, 0:1], D, "ss0")
            STT(hb, xb, small[:, 0:1], n1_bc, ALU.mult, ALU.mult, [("xt", b), "ss0", "n1_bc"], [("hn", b)])
            for kt in range(KT):
                TR(bbank(b)[:, kt * 128:(kt + 1) * 128], hb[:, kt * 128:(kt + 1) * 128], ident_b[:],
                   [("hn", b), "ident_b"], [("pbb", b)])
            CP("act", hT[:, :, tt * 128:(tt + 1) * 128], bbank(b).rearrange("p (k t) -> p k t", k=KT),
               [("pbb", b)], [("hT", tt)])
        HT_ALL = [("hT", tt) for tt in range(NT)]
        MARK("p1")

        off = 0
        qT, off = carve(off, [T], F32)
        kT, off = carve(off, [T], F32)
        k_tok, off = carve(off, [NT, 128], F32)
        v_tok, off = carve(off, [NT, 256], BF16)
        qdT = [None, None]
        kiT = [None, None]
        ktail = [None, None]
        for d_ in range(2):
            qdT[d_], off = carve(off, [T], BF16)
            kiT[d_], off = carve(off, [T], BF16)
            ktail[d_], off = carve(off, [NT, 128], BF16)
        sb_store, off = carve(off, [NT, 256], BF16)
        dec, off = carve(off, [2, NT], F32)
        S, off = carve(off, [256], F32)
        S_bf, off = carve(off, [256], BF16)
        NTMP = 3
        tmp = []
        for i in range(NTMP):
            d = {}
            for nm in ("e", "lg", "E", "Ei", "Et"):
                d[nm], off = carve(off, [128], F32)
            d["Pf"], off = carve(off, [128], BF16)
            d["Pb"], off = carve(off, [128], BF16)
            d["sig"], off = carve(off, [512], F32)
            d["G"], off = carve(off, [256], F32)
            tmp.append(d)
        gl, off = carve(off, [2, T], BF16)
        w2b, off = carve(off, [2, 512], BF16)
        wqk, off = carve(off, [KT, 256], BF16)
        wkv, off = carve(off, [KT, 384], BF16)
        wgm, off = carve(off, [KT, 512], BF16)
        wgl, off = carve(off, [KT, 32], BF16)
        gnw_bc, off = carve(off, [256], F32)
        GLA_END = off
        assert GLA_END <= 116 * 1024, GLA_END

        DMA("sp", gnw_bc, gnw_d.partition_broadcast(128), "c_gnw", [], ["gnw_bc"])
        MEMSET("pool", gl[:, :, :], 1.0, ["gl"])
        MEMSET("pool", w2b[:, :, :], 0.0, ["w2b"])
        for d_ in range(2):
            DMA("pool", w2b[0:17, d_, :], w2b_d[d_][:, :], "c_w2b", [], ["w2b"])
        DMA("pool", wgl, w_in_d[:, C_GLF:C_GLF + 32].rearrange("(k p) c -> p k c", p=128), "w_wgl", [], ["wgl"])
        for d_ in range(2):
            for tg in range(4):
                bi = tg % 2
                for kt in range(KT):
                    MM(bank(bi)[0:16, :], wgl[:, kt, d_ * 16:(d_ + 1) * 16], hT[:, kt, tg * 512:(tg + 1) * 512],
                       kt == 0, kt == KT - 1, ["wgl"] + HT_ALL[tg * 4:tg * 4 + 4], [bkey(bi)])
                CP("act", gl[0:16, d_, tg * 512:(tg + 1) * 512], bank(bi)[0:16, :], [bkey(bi)], ["gl"])

        MARK("g0")
        QSCALE = 128.0 ** -0.5
        for h in range(4):
            def wcols(dst, c0, n):
                return (dst, w_in_d[:, c0:c0 + n].rearrange("(k p) c -> p k c", p=128))
            for (dst, src) in (wcols(wqk[:, :, 0:128], C_GQ + h * 128, 128), wcols(wqk[:, :, 128:256], C_GK + h * 128, 128)):
                DMA("pool", dst, src, "w_wqk", [], ["wqk"])
            for (dst, src) in (wcols(wkv[:, :, 0:128], C_GK + h * 128, 128), wcols(wkv[:, :, 128:384], C_GV + h * 256, 256)):
                DMA("pool", dst, src, "w_wkv", [], ["wkv"])
            for (dst, src) in (wcols(wgm[:, :, 0:256], C_GR + h * 256, 256), wcols(wgm[:, :, 256:512], C_MA + h * 256, 256)):
                DMA("pool", dst, src, "w_wgm", [], ["wgm"])
            MARK("g1a")
            for which, dstT in ((0, qT), (1, kT)):
                for tg in range(4):
                    bi = (which * 4 + tg) % 4
                    for kt in range(KT):
                        MM(bank(bi), wqk[:, kt, which * 128:(which + 1) * 128], hT[:, kt, tg * 512:(tg + 1) * 512],
                           kt == 0, kt == KT - 1, ["wqk"] + HT_ALL[tg * 4:tg * 4 + 4], [bkey(bi)])
                    CP("act" if tg % 2 else "dve", dstT[:, tg * 512:(tg + 1) * 512], bank(bi), [bkey(bi)],
                       [("qkT", which, tg)])
            MARK("g1b")
            for n in range(NT):
                bi = 4 + n % 2
                for kt in range(KT):
                    MM(bank(bi)[:, 0:384], hT[:, kt, n * 128:(n + 1) * 128], wkv[:, kt, :],
                       kt == 0, kt == KT - 1, ["wkv", ("hT", n)], [bkey(bi)])
                CP("dve", k_tok[:, n, :], bank(bi)[:, 0:128], [bkey(bi)], [("k_tok", n)])
                CP("act", v_tok[:, n, :], bank(bi)[:, 128:384], [bkey(bi)], [("v_tok", n)])
            MARK("g1")
            for n in range(NT):
                tsl = slice(n * 128, (n + 1) * 128)
                tg = n // 4
                for d_ in range(2):
                    tm = tmp[(n * 2 + d_) % NTMP]
                    tk = ("gtmp", (n * 2 + d_) % NTMP)
                    a_c = cst("a_le") if d_ == 0 else cst("a_ge")
                    a_s = cst("a_gt") if d_ == 0 else cst("a_lt")
                    b0 = (n * 2 + d_) % 2 * 2
                    zb, cb = bank(b0), bank(b0 + 1)
                    MM(zb[:, 0:128], gl[:, d_, tsl], w2b[:, d_, h * 128:(h + 1) * 128], True, True,
                       ["gl", "w2b"], [bkey(b0)])
                    ACTF(tm["e"], zb[:, 0:128], AF.Exp, [bkey(b0)], [tk], scale=-1.0)
                    ACTF(tm["lg"], tm["e"], AF.Ln, [tk], [tk], bias=1.0)
                    MM(cb[:, 0:128], tm["lg"], a_c, True, True, [tk, "consts"], [bkey(b0 + 1)])
                    MM(cb[:, 128:256], a_s, tm["lg"], True, True, [tk, "consts"], [bkey(b0 + 1)])
                    ACTF(tm["E"], cb[:, 0:128], AF.Exp, [bkey(b0 + 1)], [tk])
                    ACTF(tm["Ei"], cb[:, 0:128], AF.Exp, [bkey(b0 + 1)], [tk], scale=-1.0)
                    ACTF(tm["Et"], cb[:, 128:256], AF.Exp, [bkey(b0 + 1)], [tk])
                    STT(qdT[d_][:, tsl], qT[:, tsl], QSCALE, tm["E"], ALU.mult, ALU.mult,
                        [("qkT", 0, tg), tk], [("qdT", d_, n)])
                    TT("dve", kiT[d_][:, tsl], kT[:, tsl], tm["Ei"], ALU.mult, [("qkT", 1, tg), tk], [("kiT", d_, n)])
                    TT("dve", ktail[d_][:, n, :], k_tok[:, n, :], tm["Et"], ALU.mult, [("k_tok", n), tk], [("ktail", d_, n)])
                    col = 127 if d_ == 0 else 0
                    CP("dve", dec[:, d_, n:n + 1], tm["E"][:, col:col + 1], [tk], [("dec", d_, n)])
            MARK("g2")
            MEMSET("dve", S, 0.0, ["S"])
            for n in range(NT - 1, -1, -1):
                CP("act", sb_store[:, n, :], S, ["S"], [("sb_store", n)])
                bi = 4 + n % 2
                MM(bank(bi)[:, 0:256], ktail[1][:, n, :], v_tok[:, n, :], True, True,
                   [("ktail", 1, n), ("v_tok", n)], [bkey(bi)])
                STT(S, S, dec[:, 1, n:n + 1], bank(bi)[:, 0:256], ALU.mult, ALU.add,
                    ["S", ("dec", 1, n), bkey(bi)], ["S"])
            MARK("g3")
            MEMSET("dve", S, 0.0, ["S"])
            for n in range(NT):
                tsl = slice(n * 128, (n + 1) * 128)
                tm = tmp[n % NTMP]
                tk = ("ftmp", n % NTMP)
                CP("act", S_bf, S, ["S"], ["S_bf"])
                b0 = (n % 2) * 2
                sc = bank(b0)
                MM(sc[:, 0:128], kiT[0][:, tsl], qdT[0][:, tsl], True, True, [("kiT", 0, n), ("qdT", 0, n)], [bkey(b0)])
                MM(sc[:, 128:256], kiT[1][:, tsl], qdT[1][:, tsl], True, True, [("kiT", 1, n), ("qdT", 1, n)], [bkey(b0)])
                TT("dve", tm["Pf"], sc[:, 0:128], cst("m_le"), ALU.mult, [bkey(b0), "consts"], [tk])
                TT("dve", tm["Pb"], sc[:, 128:256], cst("m_ge"), ALU.mult, [bkey(b0), "consts"], [tk])
                ob = bank(b0 + 1)
                ok = bkey(b0 + 1)
                MM(ob[:, 0:256], qdT[0][:, tsl], S_bf, True, False, [("qdT", 0, n), "S_bf"], [ok])
                MM(ob[:, 0:256], qdT[1][:, tsl], sb_store[:, n, :], False, False, [("qdT", 1, n), ("sb_store", n)], [ok])
                MM(ob[:, 0:256], tm["Pf"], v_tok[:, n, :], False, False, [tk, ("v_tok", n)], [ok])
                MM(ob[:, 0:256], tm["Pb"], v_tok[:, n, :], False, True, [tk, ("v_tok", n)], [ok])
                kb = 4 + n % 2
                MM(bank(kb)[:, 0:256], ktail[0][:, n, :], v_tok[:, n, :], True, True,
                   [("ktail", 0, n), ("v_tok", n)], [bkey(kb)])
                STT(S, S, dec[:, 0, n:n + 1], bank(kb)[:, 0:256], ALU.mult, ALU.add,
                    ["S", ("dec", 0, n), bkey(kb)], ["S"])
                gb = 4 + n % 2
                for kt in range(KT):
                    MM(bank(gb), hT[:, kt, tsl], wgm[:, kt, :], kt == 0, kt == KT - 1, ["wgm", ("hT", n)], [bkey(gb)])
                ACTF(tm["sig"], bank(gb), AF.Exp, [bkey(gb)], [("sig", n % NTMP)], scale=-1.0)
                ACTF(tm["sig"], tm["sig"], AF.Ln, [("sig", n % NTMP)], [("sig", n % NTMP)], bias=1.0)
                ACTF(tm["sig"], tm["sig"], AF.Exp, [("sig", n % NTMP)], [("sig", n % NTMP)], scale=-1.0)
                TT("pool", tm["G"], tm["sig"][:, 0:256], tm["sig"][:, 256:512], ALU.mult, [("sig", n % NTMP)], [("G", n % NTMP)])
                TT("dve", tm["G"], tm["G"], bank(gb)[:, 0:256], ALU.mult, [("G", n % NTMP), bkey(gb)], [("G", n % NTMP)])
                TT("pool", tm["G"], tm["G"], gnw_bc, ALU.mult, [("G", n % NTMP), "gnw_bc"], [("G", n % NTMP)])
                ssk = ("ssq", n % 2)
                ssap = small[:, 2 + n % 2:3 + n % 2]
                ACTF(tm["sig"][:, 0:256], ob[:, 0:256], AF.Square, [ok, ("G", n % NTMP)], [("sig", n % NTMP), ssk],
                     accum_out=ssap)
                rstd_inplace(ssap, 256, ssk)
                STT(mixed[:, n, h * 256:(h + 1) * 256], ob[:, 0:256], ssap, tm["G"], ALU.mult, ALU.mult,
                    [ok, ssk, ("G", n % NTMP)], [("mixed", n)])

        P.barrier()
        HG = 4
        off = 0
        gqT, off = carve(off, [HG, T], BF16)
        gkT, off = carve(off, [HG, T], BF16)
        gvT, off = carve(off, [HG, T], BF16)
        dabs, off = carve(off, [NT, 32], F32)
        g_raw, off = carve(off, [NT, 2, 8], F32)
        beta, off = carve(off, [NT, 2, 8], F32)
        gvec, off = carve(off, [64], F32)
        wsl0, off = carve(off, [KT, 512], BF16)
        wsl1, off = carve(off, [KT, 512], BF16)
        wsl = [wsl0, wsl1]
        cwT, off = carve(off, [24, 5], F32)
        gdnw_bc, off = carve(off, [128], F32)
        wdab, off = carve(off, [KT, 32], BF16)
        TMP0 = off
        xc = [None, None]
        xc[0], off = carve(off, [T + 4], BF16)
        xc[1], off = carve(off, [T + 4], BF16)
        diag, off = carve(off, [5, 128], BF16)
        ce = [None, None]
        cy = [None, None]
        for i in range(2):
            ce[i], off = carve(off, [512], F32)
            cy[i], off = carve(off, [512], F32)
        cysq, off = carve(off, [512], BF16)
        crs, off = carve(off, [512], F32)
        CONV_END = off
        off = TMP0
        DB = []
        for d_ in range(2):
            dd = {}
            for nm in ("GMB", "Wd", "decT", "u_sb", "Sg"):
                dd[nm], off = carve(off, [HG, 128], F32)
            for nm in ("Lm", "LTm", "XT", "Pp0", "Pp1", "PTp0", "PTp1", "kbg", "vbeta", "qd_tok",
                       "attnT", "ktl0", "ktl1", "qdTg", "wT_sb", "vnew", "Sg_bf", "ostage"):
                dd[nm], off = carve(off, [HG, 128], BF16)
            dd["bg"], off = carve(off, [HG], F32)
            dd["et2"], off = carve(off, [2, HG], F32)
            DB.append(dd)
        oland = []
        for i in range(2):
            a, off = carve(off, [HG, 128], BF16)
            oland.append(a)
        osum, off = carve(off, [HG, 128], F32)
        fsig, off = carve(off, [512], F32)
        fG, off = carve(off, [HG, 128], F32)
        frs, off = carve(off, [8], F32)
        SWEEP_END = off
        gdn_o = nc.dram_tensor("gdn_o_spill", [NT, 128, HG * 128], BF16, kind="Internal").ap()

        gdnw_d = dram("gdn_norm_w", [1, 128])
        gvec_d = dram("gdn_vec", [1, 32])
        cw_d = dram("gdn_conv_wT", [128, 24, 5])
        DMA("sp", gdnw_bc, gdnw_d.partition_broadcast(128), "c_gdnw", [], ["gdnw_bc"])
        DMA("sp", gvec[:, 0:32], gvec_d.partition_broadcast(128), "c_gvec", [], ["gvec"])
        DMA("sp", cwT, cw_d[:, :, :], "c_cw", [], ["cwT"])
        DMA("pool", wdab, w_in_d[:, C_DAB:C_DAB + 32].rearrange("(k p) c -> p k c", p=128), "w_wdab", [], ["wdab"])
        ACTF(gvec[:, 16:32], gvec[:, 16:32], AF.Exp, ["gvec"], ["gvec"])
        TS("dve", gvec[:, 16:32], gvec[:, 16:32], -1.0, None, ALU.mult, ALU.bypass, ["gvec"], ["gvec"])
        for n in range(NT):
            bi = n % 2
            for kt in range(KT):
                MM(bank(bi)[:, 0:32], hT[:, kt, n * 128:(n + 1) * 128], wdab[:, kt, :], kt == 0, kt == KT - 1,
                   ["wdab", ("hT", n)], [bkey(bi)])
            CP("act", dabs[:, n, :], bank(bi)[:, 0:32], [bkey(bi)], ["dabs"])
        a_view = dabs[:, :, 0:16]
        b_view = dabs[:, :, 16:32]
        g_flat = g_raw.rearrange("p n d h -> p n (d h)")
        be_flat = beta.rearrange("p n d h -> p n (d h)")
        TT("dve", g_flat, a_view, gvec[:, 0:16].unsqueeze(1).to_broadcast([128, NT, 16]), ALU.add, ["dabs", "gvec"], ["g_raw"])
        ACTF(g_flat, g_flat, AF.Exp, ["g_raw"], ["g_raw"])
        ACTF(g_flat, g_flat, AF.Ln, ["g_raw"], ["g_raw"], bias=1.0)
        TT("dve", g_flat, g_flat, gvec[:, 16:32].unsqueeze(1).to_broadcast([128, NT, 16]), ALU.mult, ["g_raw", "gvec"], ["g_raw"])
        ACTF(be_flat, b_view, AF.Exp, ["dabs"], ["beta"], scale=-1.0)
        TS("dve", be_flat, be_flat, 1.0, None, ALU.add, ALU.bypass, ["beta"], ["beta"])
        RECIP(be_flat, be_flat, ["beta"], ["beta"])
        MARK("d0")

        GSCALE = 128.0 ** -0.5
        ident_bc4 = ident_b[:].unsqueeze(1).to_broadcast([128, HG, 128])

        def bc_h(ap2):
            return ap2.unsqueeze(2).to_broadcast([128, HG, 128])

        def bc_m(ap2):
            return ap2.unsqueeze(1).to_broadcast([128, HG, 128])

        def v4(ap2):
            return ap2.rearrange("p (h d) -> p h d", h=HG)

        for grp in range(2):
            hs0 = grp * HG
            for which, c_base, dstT in ((0, C_DQ, gqT), (1, C_DK, gkT), (2, C_DV, gvT)):
                ws = wsl[which % 2]
                wk = ("wsl", which % 2)
                DMA("pool", ws, w_in_d[:, c_base + hs0 * 128:c_base + (hs0 + HG) * 128].rearrange("(k p) c -> p k c", p=128),
                    ("w_wsl", which % 2), [], [wk])
                for hh in range(HG):
                    ci = which * 8 + hs0 + hh
                    xi = (which * HG + hh) % 2
                    xcb = xc[xi]
                    xk = ("xc", xi)
                    MEMSET("pool", xcb[:, 0:2], 0.0, [xk])
                    MEMSET("pool", xcb[:, T + 2:T + 4], 0.0, [xk])
                    for k in range(5):
                        TS("dve", diag[:, k, :], cst("ident"), cwT[:, ci, k:k + 1], None, ALU.mult, ALU.bypass,
                           ["consts", "cwT"], ["diag"])
                    for tg in range(4):
                        bi = tg % 2
                        for kt in range(KT):
                            MM(bank(bi), ws[:, kt, hh * 128:(hh + 1) * 128], hT[:, kt, tg * 512:(tg + 1) * 512],
                               kt == 0, kt == KT - 1, [wk] + HT_ALL[tg * 4:tg * 4 + 4], [bkey(bi)])
                        CP("act" if tg % 2 else "dve", xcb[:, 2 + tg * 512:2 + (tg + 1) * 512], bank(bi), [bkey(bi)], [xk])
                    for tg in range(4):
                        bi = 2 + tg % 2
                        i2 = tg % 2
                        for k in range(5):
                            MM(bank(bi), diag[:, k, :], xcb[:, tg * 512 + k:tg * 512 + k + 512], k == 0, k == 4,
                               ["diag", xk], [bkey(bi)])
                        ck = ("ctmp", i2)
                        ACTF(ce[i2], bank(bi), AF.Exp, [bkey(bi)], [ck], scale=-1.0)
                        ACTF(ce[i2], ce[i2], AF.Ln, [ck], [ck], bias=1.0)
                        ACTF(ce[i2], ce[i2], AF.Exp, [ck], [ck], scale=-1.0)
                        dst = dstT[:, hh, tg * 512:(tg + 1) * 512]
                        dk = ("gT", which, hh, tg)
                        if which == 2:
                            TT("dve", dst, ce[i2], bank(bi), ALU.mult, [ck, bkey(bi)], [dk])
                        else:
                            TT("dve", cy[i2], ce[i2], bank(bi), ALU.mult, [ck, bkey(bi)], [("cy", i2)])
                            TT("pool", cysq, cy[i2], cy[i2], ALU.mult, [("cy", i2)], ["cysq"])
                            MM(bank(4), ones_b[:], cysq, True, True, ["ones_b", "cysq"], [bkey(4)])
                            ACTF(crs, bank(4), AF.Ln, [bkey(4)], ["crs"], bias=EPS)
                            ACTF(crs, crs, AF.Exp, ["crs"], ["crs"], scale=-0.5)
                            if which == 0:
                                STT(dst, cy[i2], GSCALE, crs, ALU.mult, ALU.mult, [("cy", i2), "crs"], [dk])
                            else:
                                TT("dve", dst, cy[i2], crs, ALU.mult, [("cy", i2), "crs"], [dk])
            MARK("d1")
            P.barrier()
            DMA("pool", wsl[0], w_in_d[:, C_DZ + hs0 * 128:C_DZ + (hs0 + HG) * 128].rearrange("(k p) c -> p k c", p=128),
                ("w_wsl", 0), [], [("wsl", 0)])
            DMA("pool", wsl[1], w_in_d[:, C_MB + hs0 * 128:C_MB + (hs0 + HG) * 128].rearrange("(k p) c -> p k c", p=128),
                ("w_wsl", 1), [], [("wsl", 1)])

            def gT_keys(which, n):
                return [("gT", which, hh, n // 4) for hh in range(HG)]

            stored = set()
            esc_all = [dabs[:, 0:8, :].rearrange("p a b -> p (a b)").rearrange("p (k n h) -> p k n h", k=4, n=NT),
                       dabs[:, 8:16, :].rearrange("p a b -> p (a b)").rearrange("p (k n h) -> p k n h", k=4, n=NT)]
            for d_ in range(2):
                Mc_ = cst("b_le") if d_ == 0 else cst("b_ge")
                Ms_ = cst("b_gt") if d_ == 0 else cst("b_lt")
                for ki, mk in enumerate((Mc_, Ms_, cst("csel0"), cst("csel1"))):
                    MM(bank(d_)[:, ki * 64:(ki + 1) * 64].rearrange("p (n h) -> p n h", n=NT), mk,
                       g_raw[:, :, d_, hs0:hs0 + HG], True, True, ["consts", "g_raw"], [bkey(d_)])
                ACTF(esc_all[d_].rearrange("p k n h -> p (k n h)"), bank(d_)[:, 0:256], AF.Exp, [bkey(d_)], [("esc_all", d_), "dabs"])

            gb0 = ptb[:, 0:512]
            gb1 = ptb[:, 512:1024]
            xbank = ptb[:, 1024:2048].bitcast(F32)
            XK = ("pb", 7)

            def gdn_tile(d_, n):
                B = DB[d_]
                dk = lambda nm: (nm, d_)
                Mc = cst("b_le") if d_ == 0 else cst("b_ge")
                Ms = cst("b_gt") if d_ == 0 else cst("b_lt")
                bg = B["bg"]
                GMB, Wd, decT, Lm, LTm, XT = B["GMB"], B["Wd"], B["decT"], B["Lm"], B["LTm"], B["XT"]
                kbg, vbeta, qd_tok = B["kbg"], B["vbeta"], B["qd_tok"]
                Ppd = [B["Pp0"], B["Pp1"]]
                PTpd = [B["PTp0"], B["PTp1"]]
                pa, pb_ = (0, 1) if d_ == 0 else (2, 3)
                tsl = slice(n * 128, (n + 1) * 128)
                gv = g_raw[:, n, d_, hs0:hs0 + HG]
                bv = beta[:, n, d_, hs0:hs0 + HG]
                EA = esc_all[d_]
                e_cum, e_tail = EA[:, 0, n, :], EA[:, 1, n, :]
                second = n in stored
                if second:
                    par = n % 2
                    DMA("sp", oland[par].rearrange("p h d -> p (h d)"), gdn_o[n], ("oland", par), [("o_dram", n)], [("oland", par)])
                TT("dve", bg, bv, e_cum, ALU.mult, ["beta", ("esc_all", d_)], [dk("bg")])
                for hh in range(HG):
                    TR(gb0[:, hh * 128:(hh + 1) * 128], gkT[:, hh, tsl], ident_b[:], gT_keys(1, n) + ["ident_b"], [("pbb", 0)])
                for hh in range(HG):
                    TR(gb1[:, hh * 128:(hh + 1) * 128], gvT[:, hh, tsl], ident_b[:], gT_keys(2, n) + ["ident_b"], [("pbb", 0)])
                TT("dve", kbg, v4(gb0), bc_h(bg), ALU.mult, [("pbb", 0), dk("bg")], [dk("kbg")])
                for c_ in range(2):
                    TT("dve", B["et2"][:, c_, :], e_tail, cst("csel%d" % c_)[:, 0:HG], ALU.mult, [("esc_all", d_), "consts"], [dk("et2")])
                    TT("dve", B["ktl%d" % c_], v4(gb0), bc_h(B["et2"][:, c_, :]), ALU.mult, [("pbb", 0), dk("et2")], [dk("ktl%d" % c_)])
                TT("dve", vbeta, v4(gb1), bc_h(bv), ALU.mult, [("pbb", 0), "beta"], [dk("vbeta")])
                for hh in range(HG):
                    TR(gb0[:, hh * 128:(hh + 1) * 128], gqT[:, hh, tsl], ident_b[:], gT_keys(0, n) + ["ident_b"], [("pbb", 0)])
                TT("dve", qd_tok, v4(gb0), bc_h(e_cum), ALU.mult, [("pbb", 0), ("esc_all", d_)], [dk("qd_tok")])
                for hh in range(HG):
                    TR(gb1[:, hh * 128:(hh + 1) * 128], qd_tok[:, hh, :], ident_b[:], [dk("qd_tok"), "ident_b"], [("pbb", 0)])
                CP("act", B["qdTg"], v4(gb1), [("pbb", 0)], [dk("qdTg")])
                TT("pool", GMB, bc_h(gv), bc_m(Ms), ALU.mult, ["g_raw", "consts"], [dk("GMB")])
                MM(bank(pb_), Mc, GMB.rearrange("p h s -> p (h s)"), True, True, ["consts", dk("GMB")], [bkey(pb_)])
                ACTF(Wd.rearrange("p h s -> p (h s)"), bank(pb_), AF.Exp, [bkey(pb_)], [dk("Wd")])
                TT("pool", GMB, bc_h(bv), bc_m(Ms), ALU.mult, ["beta", "consts"], [dk("GMB")])
                TT("pool", Wd, Wd, GMB, ALU.mult, [dk("Wd"), dk("GMB")], [dk("Wd")])
                TT("pool", GMB, bc_h(gv), bc_m(Mc), ALU.mult, ["g_raw", "consts"], [dk("GMB")])
                MM(bank(pa), Ms, GMB.rearrange("p h s -> p (h s)"), True, True, ["consts", dk("GMB")], [bkey(pa)])
                ACTF(decT.rearrange("p h s -> p (h s)"), bank(pa), AF.Exp, [bkey(pa)], [dk("decT")])
                TT("pool", decT, decT, bc_m(Mc), ALU.mult, [dk("decT"), "consts"], [dk("decT")])
                for hh in range(HG):
                    MM(bank(pb_)[:, hh * 128:(hh + 1) * 128], gkT[:, hh, tsl], gkT[:, hh, tsl], True, True,
                       gT_keys(1, n), [bkey(pb_)])
                TT("dve", Lm, v4(bank(pb_)), Wd, ALU.mult, [bkey(pb_), dk("Wd")], [dk("Lm")])
                for hh in range(HG):
                    MM(bank(pa)[:, hh * 128:(hh + 1) * 128], gkT[:, hh, tsl], gqT[:, hh, tsl], True, True,
                       gT_keys(1, n) + gT_keys(0, n), [bkey(pa)])
                TT("dve", B["attnT"], v4(bank(pa)), decT, ALU.mult, [bkey(pa), dk("decT")], [dk("attnT")])
                for hh in range(HG):
                    TR(gb0[:, hh * 128:(hh + 1) * 128], Lm[:, hh, :], ident_b[:], [dk("Lm"), "ident_b"], [("pbb", 0)])
                CP("act", LTm, v4(gb0), [("pbb", 0)], [dk("LTm")])
                TT("dve", XT, ident_bc4, v4(gb0), ALU.subtract, ["ident_b", ("pbb", 0)], [dk("XT")])
                Pc, PTc = Lm, LTm
                pck, ptk_ = dk("Lm"), dk("LTm")
                for it in range(5):
                    Pn, PTn = Ppd[it % 2], PTpd[it % 2]
                    pnk, ptnk = ("Pp", it % 2, d_), ("PTp", it % 2, d_)
                    for hh in range(HG):
                        MM(bank(pb_)[:, hh * 128:(hh + 1) * 128], PTc[:, hh, :], Pc[:, hh, :], True, True, [pck, ptk_], [bkey(pb_)])
                    CP("act", Pn, v4(bank(pb_)), [bkey(pb_)], [pnk])
                    if it < 4:
                        for hh in range(HG):
                            MM(bank(pa)[:, hh * 128:(hh + 1) * 128], Pc[:, hh, :], PTc[:, hh, :], True, True, [pck, ptk_], [bkey(pa)])
                        CP("dve", PTn, v4(bank(pa)), [bkey(pa)], [ptnk])
                    for hh in range(HG):
                        MM(xbank[:, hh * 128:(hh + 1) * 128], Pn[:, hh, :], XT[:, hh, :], True, True, [pnk, dk("XT")], [XK])
                    TT("dve", XT, XT, v4(xbank), ALU.add, [dk("XT"), XK], [dk("XT")])
                    Pc, PTc, pck, ptk_ = Pn, PTn, pnk, ptnk
                for hh in range(HG):
                    MM(bank(pa)[:, hh * 128:(hh + 1) * 128], XT[:, hh, :], vbeta[:, hh, :], True, True, [dk("XT"), dk("vbeta")], [bkey(pa)])
                CP("act", B["u_sb"], v4(bank(pa)), [bkey(pa)], [dk("u_sb")])
                for hh in range(HG):
                    MM(bank(pb_)[:, hh * 128:(hh + 1) * 128], kbg[:, hh, :], XT[:, hh, :], True, True, [dk("kbg"), dk("XT")], [bkey(pb_)])
                CP("dve", B["wT_sb"], v4(bank(pb_)), [bkey(pb_)], [dk("wT_sb")])
                sb0 = 4
                Sg, Sg_bf, vnew = B["Sg"], B["Sg_bf"], B["vnew"]
                chunks = (0, 1) if d_ == 0 else (1, 0)
                for c in chunks:
                    sl = slice(c * 64, c * 64 + 64)
                    for hh in range(HG):
                        MM(bank(sb0)[:, hh * 128:(hh + 1) * 128], B["wT_sb"][:, hh, :], Sg_bf[:, hh, :], True, True,
                           [dk("wT_sb"), dk("Sg_bf")], [bkey(sb0)])
                    TT("dve", vnew[sl], B["u_sb"][sl], v4(bank(sb0))[sl], ALU.subtract, [dk("u_sb"), bkey(sb0)], [dk("vnew")])
                    for hh in range(HG):
                        MM(bank(sb0 + 1)[:, hh * 128:(hh + 1) * 128], B["qdTg"][:, hh, :], Sg_bf[:, hh, :], True, False,
                           [dk("qdTg"), dk("Sg_bf")], [bkey(sb0 + 1)])
                        MM(bank(sb0 + 1)[:, hh * 128:(hh + 1) * 128], B["attnT"][:, hh, :], vnew[:, hh, :], False, True,
                           [dk("attnT"), dk("vnew")], [bkey(sb0 + 1)])
                    for hh in range(HG):
                        MM(bank(sb0)[:, hh * 128:(hh + 1) * 128], B["ktl%d" % c][:, hh, :], vnew[:, hh, :], True, True,
                           [dk("ktl%d" % c), dk("vnew")], [bkey(sb0)])
                    TT("pool", Sg, Sg, bc_h(EA[:, 2 + c, n, :]), ALU.mult, [dk("Sg"), ("esc_all", d_)], [dk("Sg")])
                    TT("dve", Sg, Sg, v4(bank(sb0)), ALU.add, [dk("Sg"), bkey(sb0)], [dk("Sg")])
                    CP("act", Sg_bf, Sg, [dk("Sg")], [dk("Sg_bf")])
                    if not second:
                        CP("act", B["ostage"][sl], v4(bank(sb0 + 1))[sl], [bkey(sb0 + 1)], [dk("ostage")])
                    else:
                        TT("dve", osum[sl], v4(bank(sb0 + 1))[sl], oland[n % 2][sl], ALU.add,
                           [bkey(sb0 + 1), ("oland", n % 2)], ["osum"])
                if not second:
                    stored.add(n)
                    DMA("sp", gdn_o[n], B["ostage"].rearrange("p h d -> p (h d)"), ("ost", d_), [dk("ostage")], [("o_dram", n)])
                    return
                osq = fsig.rearrange("p (h d) -> p h d", h=HG)
                TT("pool", osq, osum, osum, ALU.mult, ["osum"], ["fsig"])
                P.op("dve", lambda e: e.tensor_reduce(out=frs[:, 0:HG], in_=osq, axis=AX.X, op=ALU.add), ["fsig"], ["frs"], cost=600.0)
                rstd_inplace(frs[:, 0:HG], 128, "frs")
                for half, ws in enumerate(wsl):
                    for kt in range(KT):
                        MM(bank(half), hT[:, kt, tsl], ws[:, kt, :], kt == 0, kt == KT - 1,
                           [("wsl", half), ("hT", n)], [bkey(half)])
                fGf = fG.rearrange("p h d -> p (h d)")
                for half in range(2):
                    ACTF(fsig, bank(half), AF.Exp, [bkey(half)], ["fsig"], scale=-1.0)
                    ACTF(fsig, fsig, AF.Ln, ["fsig"], ["fsig"], bias=1.0)
                    ACTF(fsig, fsig, AF.Exp, ["fsig"], ["fsig"], scale=-1.0)
                    if half == 0:
                        TT("dve", fGf, fsig, bank(0), ALU.mult, ["fsig", bkey(0)], ["fG"])
                    else:
                        TT("pool", fGf, fGf, fsig, ALU.mult, ["fsig", "fG"], ["fG"])
                TT("pool", fG, fG, bc_m(gdnw_bc), ALU.mult, ["fG", "gdnw_bc"], ["fG"])
                TT("pool", osum, osum, bc_h(frs[:, 0:HG]), ALU.mult, ["osum", "frs"], ["osum"])
                TT("pool", osum, osum, fG, ALU.mult, ["osum", "fG"], ["osum"])
                mslice = mixed[:, n, hs0 * 128:(hs0 + HG) * 128].rearrange("p (h d) -> p h d", h=HG)
                TT("dve", mslice, mslice, osum, ALU.add, ["osum", ("mixed", n)], [("mixed", n)])

            for d_ in range(2):
                MEMSET("pool", DB[d_]["vnew"], 0.0, [("vnew", d_)])
                MEMSET("dve", DB[d_]["Sg"], 0.0, [("Sg", d_)])
                CP("act", DB[d_]["Sg_bf"], DB[d_]["Sg"], [("Sg", d_)], [("Sg_bf", d_)])
            for i in range(NT):
                gdn_tile(0, i)
                gdn_tile(1, NT - 1 - i)
            MARK("d2")
            P.barrier()

        P.barrier()
        wout_d = dram("w_out", [D, D])
        off = 0
        x1, off = carve(off, [NT, D], F32)
        X1_END = off
        mT, off = carve(off, [KT, T], BF16)
        wout, off = carve(off, [KT, D], BF16)
        hn2 = [None, None]
        hn2[0], off = carve(off, [D], BF16)
        hn2[1], off = carve(off, [D], BF16)
        junk, off = carve(off, [D], BF16)
        n2_bc, off = carve(off, [D], F32)
        DMA("sp", n2_bc, n2_d.partition_broadcast(128), "c_n2", [], ["n2_bc"])
        DMA("pool", wout, wout_d.rearrange("(k p) c -> p k c", p=128), "w_wout", [], ["wout"])
        for n in range(NT):
            b = n % 2
            for kt in range(KT):
                TR(bbank(b)[:, kt * 128:(kt + 1) * 128], mixed[:, n, kt * 128:(kt + 1) * 128], ident_b[:],
                   [("mixed", n), "ident_b"], [("pbb", b)])
            CP("act", mT[:, :, n * 128:(n + 1) * 128], bbank(b).rearrange("p (k t) -> p k t", k=KT), [("pbb", b)], [("mT", n)])
        for n in range(NT):
            tsl = slice(n * 128, (n + 1) * 128)
            DMA("sp", x1[:, n, :], x_d[tsl, :], ("x1ld", n % 4), [], [("x1", n)])
            pp = pt[n % 2]
            for half in range(2):
                for kt in range(KT):
                    MM(pp[:, half * 512:(half + 1) * 512], mT[:, kt, tsl], wout[:, kt, half * 512:(half + 1) * 512],
                       kt == 0, kt == KT - 1, [("mT", n), "wout"], [bkey((n % 2) * 2 + half)])
            TT("dve", x1[:, n, :], x1[:, n, :], pp[:, :], ALU.add, [("x1", n), bkey((n % 2) * 2), bkey((n % 2) * 2 + 1)], [("x1", n)])
            b = n % 2
            ssap = small[:, 8 + b:9 + b]
            ssk = ("ss2", b)
            ACTF(junk, x1[:, n, :], AF.Square, [("x1", n)], ["junk", ssk], accum_out=ssap)
            rstd_inplace(ssap, D, ssk)
            STT(mixed[:, n, :], x1[:, n, :], ssap, n2_bc, ALU.mult, ALU.mult, [("x1", n), ssk, "n2_bc"], [("mixed", n)])
            for kt in range(KT):
                TR(bbank(b)[:, kt * 128:(kt + 1) * 128], mixed[:, n, kt * 128:(kt + 1) * 128], ident_b[:],
                   [("mixed", n), "ident_b"], [("pbb", b)])
            CP("act", hT[:, :, tsl], bbank(b).rearrange("p (k t) -> p k t", k=KT), [("pbb", b)], [("hT", n)])
        MARK("e0")

        P.barrier()
        NB = 64
        wr_d = dram("moe_wr", [D, 36])
        wgu0_d = dram("moe_wgu0", [4096, 2048])
        wgu1_d = dram("moe_wgu1", [4096, 2048])
        wdr_d = dram("moe_wdr", [4096, 2048])
        xb_d = nc.dram_tensor("moe_xb", [NB * 128, D], BF16, kind="Internal").ap()
        yb_d = nc.dram_tensor("moe_yb", [NB * 128, D], F32, kind="Internal").ap()
        off = X1_END
        stg = []
        for i in range(3):
            a, off = carve(off, [2048], F32)
            stg.append(a)
        wgu_bf = []
        wd_bf = []
        for i in range(2):
            a, off = carve(off, [KT, 512], BF16)
            wgu_bf.append(a)
            a, off = carve(off, [2, D], BF16)
            wd_bf.append(a)
        wr, off = carve(off, [KT, 36], BF16)
        lg, off = carve(off, [NT, 36], F32)
        oh1, off = carve(off, [NT, 32], F32)
        oh2, off = carve(off, [NT, 32], F32)
        msk, off = carve(off, [NT, 32], F32)
        rank, off = carve(off, [NT, 32], F32)
        tmp3, off = carve(off, [NT, 32], F32)
        gtmp, off = carve(off, [NT, 4], F32)
        ohg, off = carve(off, [NT, 4], F32)
        rv, off = carve(off, [8, NT], F32)
        mcum, off = carve(off, [32], F32)
        cnt, off = carve(off, [32], F32)
        padded, off = carve(off, [32], F32)
        ends, off = carve(off, [32], F32)
        pstart, off = carve(off, [32], F32)
        ebf, off = carve(off, [NB], F32)
        widx_f, off = carve(off, [NB], F32)
        widx, off = carve(off, [NB], I32)
        dest_f, off = carve(off, [2, NT], F32)
        dest_i, off = carve(off, [2, NT], I32)
        MOE_END = off
        cmpb = stg[0].rearrange("p (b e) -> p b e", b=NB)
        cmpj = stg[1][:, 0:512].rearrange("p (e j) -> p e j", e=32)
        ht32 = hT[:].rearrange("p k t -> p (k t)").bitcast(F32)
        HTCAP = 32 * 1024
        hoff = 0
        xg, xgT, sil, hid_bf, hidT, ysb = [], [], [], [], [], []
        for i in range(2):
            a, hoff = carve(hoff, [D], BF16, ht32, HTCAP); xg.append(a)
            a, hoff = carve(hoff, [KT, 128], BF16, ht32, HTCAP); xgT.append(a)
            a, hoff = carve(hoff, [256], F32, ht32, HTCAP); sil.append(a)
            a, hoff = carve(hoff, [256], BF16, ht32, HTCAP); hid_bf.append(a)
            a, hoff = carve(hoff, [2, 128], BF16, ht32, HTCAP); hidT.append(a)
            a, hoff = carve(hoff, [D], F32, ht32, HTCAP); ysb.append(a)
        mix32 = mixed[:].rearrange("p n d -> p (n d)").bitcast(F32)
        MIXCAP = 32 * 1024
        moff = 0
        yg = []
        for i in range(2):
            a, moff = carve(moff, [D], F32, mix32, MIXCAP); yg.append(a)
        stgB = []
        for i in range(3):
            a, moff = carve(moff, [2048], F32, mix32, MIXCAP); stgB.append(a)

        DMA("pool", wr, wr_d.rearrange("(k p) c -> p k c", p=128), "w_wr", [], ["wr"])
        for n in range(NT):
            bi = n % 2
            for kt in range(KT):
                MM(bank(bi)[:, 0:36], hT[:, kt, n * 128:(n + 1) * 128], wr[:, kt, :], kt == 0, kt == KT - 1,
                   ["wr", ("hT", n)], [bkey(bi)])
            CP("act", lg[:, n, :], bank(bi)[:, 0:36], [bkey(bi)], ["lg"])
        BIG = 10000.0
        glv = lg[:, :, 0:4]
        elv = lg[:, :, 4:36]

        def RED(out, in_, op, R, W):
            P.op("dve", lambda e: e.tensor_reduce(out=out, in_=in_, axis=AX.X, op=op), R, W, cost=100.0 + _fsz(in_) * 1.0)

        def bcn(ap2, k):
            return ap2.unsqueeze(2).to_broadcast([128, NT, k])

        gmax, gsum, m1, m2, w1, w2 = (rv[:, i, :] for i in range(6))
        RED(gmax, glv, ALU.max, ["lg"], ["rv"])
        TT("dve", ohg, glv, bcn(gmax, 4), ALU.is_equal, ["lg", "rv"], ["ohg"])
        TT("dve", gtmp, glv, bcn(gmax, 4), ALU.subtract, ["lg", "rv"], ["gtmp"])
        ACTF(gtmp, gtmp, AF.Exp, ["gtmp"], ["gtmp"])
        RED(gsum, gtmp, ALU.add, ["gtmp"], ["rv"])
        RECIP(gsum, gsum, ["rv"], ["rv"])
        TS("dve", ohg, ohg, BIG, -BIG, ALU.mult, ALU.add, ["ohg"], ["ohg"])
        TT("dve", msk.rearrange("p n (g e) -> p n g e", g=4), elv.rearrange("p n (g e) -> p n g e", g=4),
           ohg.unsqueeze(3).to_broadcast([128, NT, 4, 8]), ALU.add, ["lg", "ohg"], ["msk"])
        RED(m1, msk, ALU.max, ["msk"], ["rv"])
        TT("dve", oh1, msk, bcn(m1, 32), ALU.is_equal, ["msk", "rv"], ["oh1"])
        STT(msk, oh1, -BIG, msk, ALU.mult, ALU.add, ["oh1", "msk"], ["msk"])
        RED(m2, msk, ALU.max, ["msk"], ["rv"])
        TT("dve", oh2, msk, bcn(m2, 32), ALU.is_equal, ["msk", "rv"], ["oh2"])
        TT("dve", w2, m2, m1, ALU.subtract, ["rv"], ["rv"])
        ACTF(w2, w2, AF.Exp, ["rv"], ["rv"])
        TS("dve", w1, w2, 1.0, None, ALU.add, ALU.bypass, ["rv"], ["rv"])
        RECIP(w1, w1, ["rv"], ["rv"])
        TT("dve", w1, w1, gsum, ALU.mult, ["rv"], ["rv"])
        TT("dve", w2, w2, w1, ALU.mult, ["rv"], ["rv"])
        TT("dve", msk, oh1, oh2, ALU.add, ["oh1", "oh2", "msk"], ["msk"])
        MEMSET("dve", mcum, 0.0, ["mcum"])
        for n in range(NT):
            bi = n % 2
            MM(bank(bi)[:, 0:32], cst("m_lt"), msk[:, n, :], True, False, ["consts", "msk"], [bkey(bi)])
            MM(bank(bi)[:, 0:32], cst("ones"), mcum, False, True, ["consts", "mcum"], [bkey(bi)])
            CP("act", rank[:, n, :], bank(bi)[:, 0:32], [bkey(bi)], ["rank"])
            TT("dve", mcum, mcum, msk[:, n, :], ALU.add, ["mcum", "msk"], ["mcum"])
        MM(bank(0)[:, 0:32], cst("ones"), mcum, True, True, ["consts", "mcum"], [bkey(0)])
        CP("act", cnt, bank(0)[:, 0:32], [bkey(0)], ["cnt"])
        TT("dve", cmpj, cnt.unsqueeze(2).to_broadcast([128, 32, 16]),
           cst("bvals")[:, 0:16].unsqueeze(1).to_broadcast([128, 32, 16]), ALU.is_gt, ["cnt", "consts"], [("stg", 1)])
        RED(padded, cmpj, ALU.add, [("stg", 1)], ["padded"])
        TS("dve", padded, padded, 128.0, None, ALU.mult, ALU.bypass, ["padded"], ["padded"])
        P.op("dve", lambda e: e.tensor_tensor_scan(out=ends, data0=cst("ones")[:, 0:32], data1=padded, initial=0.0,
                                                  op0=ALU.mult, op1=ALU.add), ["consts", "padded"], ["ends"], cost=300.0)
        TT("dve", pstart, ends, padded, ALU.subtract, ["ends", "padded"], ["pstart"])
        TT("dve", rank, rank, pstart.unsqueeze(1).to_broadcast([128, NT, 32]), ALU.add, ["rank", "pstart"], ["rank"])
        for k, ohk in ((0, oh1), (1, oh2)):
            TT("dve", tmp3, ohk, rank, ALU.mult, ["oh1", "oh2", "rank"], ["tmp3"])
            RED(dest_f[:, k, :], tmp3, ALU.add, ["tmp3"], ["dest_f"])
        CP("dve", dest_i, dest_f, ["dest_f"], ["dest_i"])
        TT("dve", cmpb, ends.unsqueeze(1).to_broadcast([128, NB, 32]),
           cst("bvals")[:, 0:NB].unsqueeze(2).to_broadcast([128, NB, 32]), ALU.is_le, ["ends", "consts"], [("stg", 0)])
        RED(ebf, cmpb, ALU.add, [("stg", 0)], ["ebf"])
        STT(widx_f, ebf, 128.0, cst("pidx")[:, 0:NB], ALU.mult, ALU.add, ["ebf", "consts"], ["widx_f"])
        CP("dve", widx, widx_f, ["widx_f"], ["widx"])
        MARK("e1")

        IOA = bass.IndirectOffsetOnAxis
        regs = {}

        def _pool_init(e):
            regs["bc"] = e.alloc_register("moe_bc")
            e.reg_mov(regs["bc"], 4095)
        P.pool_init = _pool_init
        XB_KEYS = []
        zt, off = carve(off, [D], BF16)
        MEMSET("pool", zt, 0.0, ["zt"])
        DMA("sp", xb_d.rearrange("(p r) d -> p r d", p=128), zt.unsqueeze(1).to_broadcast([128, NB, D]), "xbz", ["zt"], ["xb0"])
        for n in range(NT):
            for k in range(2):
                idx_ap = dest_i[:, k, n:n + 1]
                src_ap = mixed[:, n, :]
                P.dma("pool", lambda e, idx_ap=idx_ap, src_ap=src_ap: e.indirect_dma_start(
                    out=xb_d[:, :], out_offset=IOA(ap=idx_ap, axis=0), in_=src_ap, in_offset=None),
                    ("sc", (2 * n + k) % 4), [("mixed", n), "dest_i", "xb0"], [("xb", n, k)], nbytes=256 * 1024)
                XB_KEYS.append(("xb", n, k))

        def gather_w(dst, src_d, b, skey, extra):
            idx_ap = widx[:, b:b + 1]
            P.dma("pool", lambda e: e.indirect_dma_start(
                out=dst, out_offset=None, in_=src_d[:, :], in_offset=IOA(ap=idx_ap, axis=0),
                bounds_check=regs["bc"], oob_is_err=False),
                skey, ["widx"] + extra, [skey], nbytes=1 << 20)

        YB_KEYS = []
        for b in range(NB):
            s = b % 2
            sset = stg if b % 2 == 0 else stgB
            so = 0 if b % 2 == 0 else 3
            extra = [] if b % 2 == 0 else XB_KEYS
            gather_w(sset[0], wgu0_d, b, ("stg", so + 0), extra)
            gather_w(sset[1], wgu1_d, b, ("stg", so + 1), extra)
            gather_w(sset[2], wdr_d, b, ("stg", so + 2), extra)
            CP("act", wgu_bf[s][:, 0:4, :], sset[0].rearrange("p (k c) -> p k c", k=4), [("stg", so + 0)], [("wgu_bf", s, 0)])
            CP("dve", wgu_bf[s][:, 4:8, :], sset[1].rearrange("p (k c) -> p k c", k=4), [("stg", so + 1)], [("wgu_bf", s, 1)])
            CP("act" if b % 4 < 2 else "dve", wd_bf[s], sset[2].rearrange("p (k c) -> p k c", k=2), [("stg", so + 2)], [("wd_bf", s)])
            DMA("sp", xg[s], xb_d[b * 128:(b + 1) * 128, :], ("xg", s), XB_KEYS, [("xg", s)])
            for kt in range(KT):
                TR(bbank(s)[:, kt * 128:(kt + 1) * 128], xg[s][:, kt * 128:(kt + 1) * 128], ident_b[:],
                   [("xg", s), "ident_b"], [("pbb", s)])
            CP("act", xgT[s], bbank(s).rearrange("p (k t) -> p k t", k=KT), [("pbb", s)], [("xgT", s)])
            hb = bank(s)
            for kt in range(KT):
                MM(hb, xgT[s][:, kt, :], wgu_bf[s][:, kt, :], kt == 0, kt == KT - 1,
                   [("xgT", s), ("wgu_bf", s, 0), ("wgu_bf", s, 1)], [bkey(s)])
            ACTF(sil[s], hb[:, 0:256], AF.Silu, [bkey(s)], [("sil", s)])
            TT("dve", hid_bf[s], sil[s], hb[:, 256:512], ALU.mult, [("sil", s), bkey(s)], [("hid_bf", s)])
            for ft in range(2):
                TR(bbank(s)[:, ft * 128:(ft + 1) * 128], hid_bf[s][:, ft * 128:(ft + 1) * 128], ident_b[:],
                   [("hid_bf", s), "ident_b"], [("pbb", s)])
            CP("act", hidT[s], bbank(s)[:, 0:256].rearrange("p (k t) -> p k t", k=2), [("pbb", s)], [("hidT", s)])
            yp = pt[1 + s]
            for half in range(2):
                for ft in range(2):
                    MM(yp[:, half * 512:(half + 1) * 512], hidT[s][:, ft, :], wd_bf[s][:, ft, half * 512:(half + 1) * 512],
                       ft == 0, ft == 1, [("hidT", s), ("wd_bf", s)], [bkey(2 + 2 * s + half)])
            CP("act" if b % 2 else "dve", ysb[s], yp[:, :], [bkey(2 + 2 * s), bkey(3 + 2 * s)], [("ysb", s)])
            DMA("sp", yb_d[b * 128:(b + 1) * 128, :], ysb[s], ("yst", s), [("ysb", s)], [("yb", b)])
            YB_KEYS.append(("yb", b))
        MARK("e2")
        ygs = list(yg)
        for sb_ in stgB:
            ygs.append(sb_[:, 0:1024])
            ygs.append(sb_[:, 1024:2048])
        for n in range(NT):
            for k in range(2):
                s = (2 * n + k) % len(ygs)
                idx_ap = dest_i[:, k, n:n + 1]
                dst = ygs[s]
                P.dma("pool", lambda e, idx_ap=idx_ap, dst=dst: e.indirect_dma_start(
                    out=dst, out_offset=None, in_=yb_d[:, :], in_offset=IOA(ap=idx_ap, axis=0)),
                    ("yg", s), YB_KEYS + ["dest_i"], [("yg", s)], nbytes=512 * 1024)
                wk = rv[:, 4 + k, n:n + 1]
                STT(x1[:, n, :], ygs[s], wk, x1[:, n, :], ALU.mult, ALU.add, [("yg", s), "rv", ("x1", n)], [("x1", n)])

        P.barrier()
        off = X1_END
        nf_bc, off = carve(off, [D], F32)
        ob = [None, None]
        ob[0], off = carve(off, [D], F32)
        ob[1], off = carve(off, [D], F32)
        junk2, off = carve(off, [D], BF16)
        DMA("sp", nf_bc, nf_d.partition_broadcast(128), "c_nf", [], ["nf_bc"])
        for n in range(NT):
            b = n % 2
            ssap = small[:, 12 + b:13 + b]
            ssk = ("ss3", b)
            ACTF(junk2, x1[:, n, :], AF.Square, [("x1", n)], ["junk2", ssk], accum_out=ssap)
            rstd_inplace(ssap, D, ssk)
            STT(ob[b], x1[:, n, :], ssap, nf_bc, ALU.mult, ALU.mult, [("x1", n), ssk, "nf_bc"], [("ob", b)])
            DMA("sp", out_d[n * 128:(n + 1) * 128, :], ob[b], ("out_st", b), [("ob", b)], [("out", n)])
        if not dbg:
            P.wait_all("sp", [("out", n) for n in range(NT)])
        if dbg:
            P.enabled = True
            P.barrier()
            for n in range(NT):
                DMA("sp", dbg_d[n * 128:(n + 1) * 128, :], x1[:, n, :], ("dbg_out", n % 2), [("x1", n)], [("dbg", n)])
            P.wait_all("sp", [("dbg", n) for n in range(NT)] + [("out", n) for n in range(NT)])
        P.emit()
    return nc


def make_in_maps(inputs, n_cores=8):
    f = lambda k: np.asarray(inputs[k], np.float32)
    x = f("x")
    _gu = np.concatenate([f("moe_w_gate")[0], f("moe_w_up")[0]], axis=2).reshape(32, 8, 128, 512).transpose(0, 2, 1, 3)
    shared = {
        "norm1_w": f("norm1_w").reshape(1, D),
        "norm2_w": f("norm2_w").reshape(1, D),
        "norm_f_w": f("norm_f_w").reshape(1, D),
        "consts": CONST_ARR,
        "w_in": np.ascontiguousarray(f("w_in")[0]),
        "gla_w2b_f": np.ascontiguousarray(np.concatenate([f("gla_gate_w2_fwd")[0], f("gla_gate_b_fwd")], axis=0)),
        "gla_w2b_b": np.ascontiguousarray(np.concatenate([f("gla_gate_w2_bwd")[0], f("gla_gate_b_bwd")], axis=0)),
        "gla_norm_w": f("gla_norm_w").reshape(1, 256),
        "w_out": np.ascontiguousarray(f("w_out")[0]),
        "moe_wr": np.ascontiguousarray(np.concatenate([f("moe_w_group")[0], f("moe_w_router")[0]], axis=1)),
        "moe_wgu0": _gu[:, :, 0:4, :].reshape(4096, 2048).copy(),
        "moe_wgu1": _gu[:, :, 4:8, :].reshape(4096, 2048).copy(),
        "moe_wdr": np.ascontiguousarray(f("moe_w_down")[0].reshape(32, 2, 128, 1024).transpose(0, 2, 1, 3)).reshape(4096, 2048),
        "gdn_norm_w": f("gdn_norm_w").reshape(1, 128),
        "gdn_vec": np.ascontiguousarray(np.concatenate([f("gdn_dt_bias_fwd")[0], f("gdn_dt_bias_bwd")[0],
                                                        f("gdn_a_log_fwd")[0], f("gdn_a_log_bwd")[0]]).reshape(1, 32)),
        "gdn_conv_wT": np.ascontiguousarray(f("gdn_conv_w")[0].T.reshape(24, 128, 5).transpose(1, 0, 2)),
    }
    maps = []
    for c in range(n_cores):
        m = dict(shared)
        m["x"] = np.ascontiguousarray(x[c])
        maps.append(m)
    return maps


def kernel(**inputs):
    nc = build()
    in_maps = make_in_maps(inputs)
    res = run_bass_kernel_spmd(nc, in_maps, core_ids=list(range(8)))
    out = np.stack([np.asarray(r["out"]) for r in res.results], axis=0)
    return out.astype(np.float32)
```

```python
import contextlib
import heapq
import numpy as np
import concourse.bass as bass
import concourse.mybir as mybir
from concourse.bass_utils import run_bass_kernel_spmd

F32 = mybir.dt.float32
BF16 = mybir.dt.bfloat16
I32 = mybir.dt.int32
AF = mybir.ActivationFunctionType
ALU = mybir.AluOpType
AX = mybir.AxisListType

T = 2048
D = 1024
NT = T // 128
KT = D // 128
EPS = 1e-6
SAME_ENGINE_SYNC = True
EPOCH = 20000
SYNC_NS = 250.0
DMA_LAT_NS = 2200.0
PRIO = True


class Prog:
    ENGS = ("pe", "act", "dve", "pool", "sp")

    def __init__(self, nc, stack):
        self.nc = nc
        self.stack = stack
        self.streams = {e: [] for e in self.ENGS}
        self.count = {e: 0 for e in self.ENGS}
        self.esems = {e: [] for e in self.ENGS}
        self.known = {e: {} for e in self.ENGS}
        self.last_write = {}
        self.readers = {}
        self.dma_sems = {}
        self.dma_vals = {}
        self.dma_last = {}
        self.enabled = True
        self.seg = []
        self.ticks = {}
        self.nops = 0
        self.seg_base = 0
        self.pool_init = None

    def _new_sem(self, name):
        return self.stack.enter_context(self.nc.semaphore(name))

    @staticmethod
    def _psum_fix(reads, writes):
        r2, w2 = [], list(writes)
        for k in reads:
            if isinstance(k, tuple) and k[0] in ("pb", "pbb"):
                if k not in w2:
                    w2.append(k)
            else:
                r2.append(k)
        return r2, w2

    def _record(self, eng, fn, reads, writes, cost, kind, semkey=None):
        reads, writes = self._psum_fix(list(reads), list(writes))
        oid = self.nops
        self.nops += 1
        preds = set()
        for r in reads:
            t = self.last_write.get(r)
            if t is not None:
                preds.add(t)
        for w in writes:
            t = self.last_write.get(w)
            if t is not None:
                preds.add(t)
            preds.update(self.readers.get(w, ()))
        if kind == "dma":
            prev = self.dma_last.get(semkey)
            if prev is not None:
                preds.add(prev)
            self.dma_last[semkey] = oid
        preds = {p for p in preds if p >= self.seg_base}
        self.seg.append(dict(id=oid, eng=eng, fn=fn, preds=preds, cost=float(cost), kind=kind, semkey=semkey))
        for w in writes:
            self.last_write[w] = oid
            self.readers[w] = []
        for r in reads:
            self.readers.setdefault(r, []).append(oid)
        return oid

    def op(self, eng, fn, reads=(), writes=(), cost=300.0):
        if not self.enabled:
            return
        self._record(eng, fn, reads, writes, cost, "op")

    def dma(self, eng, fn, semkey, reads=(), writes=(), nbytes=1 << 20):
        if not self.enabled:
            return
        self._record(eng, fn, reads, writes, DMA_LAT_NS + nbytes / 160.0, "dma", semkey)

    def wait_all(self, eng, keys):
        self._record(eng, None, list(keys), [], 0.0, "op")

    def _schedule_segment(self):
        ops = self.seg
        if not ops:
            return
        byid = {o["id"]: o for o in ops}
        succ = {o["id"]: [] for o in ops}
        indeg = {}
        for o in ops:
            indeg[o["id"]] = len(o["preds"])
            for p in o["preds"]:
                succ[p].append(o["id"])
        ready_t = {o["id"]: 0.0 for o in ops}
        finish = {}
        bl = {}
        for o in reversed(ops):
            m = 0.0
            for s_ in succ[o["id"]]:
                if bl[s_] > m:
                    m = bl[s_]
            bl[o["id"]] = o["cost"] + m
        future = {e: [] for e in self.ENGS}
        avail = {e: [] for e in self.ENGS}
        for o in ops:
            if indeg[o["id"]] == 0:
                heapq.heappush(future[o["eng"]], (0.0, o["id"]))
        etime = {e: 0.0 for e in self.ENGS}
        order = {e: [] for e in self.ENGS}
        remaining = len(ops)
        while remaining:
            best = None
            for e in self.ENGS:
                fu, av = future[e], avail[e]
                while fu and fu[0][0] <= etime[e]:
                    rt, oid = heapq.heappop(fu)
                    heapq.heappush(av, (-bl[oid] if PRIO else rt, oid))
                if av:
                    cand = (etime[e], av[0][0], av[0][1], e, True)
                elif fu:
                    cand = (fu[0][0], 0.0, fu[0][1], e, False)
                else:
                    continue
                if best is None or cand[:3] < best[:3]:
                    best = cand
            st, _, oid, e, from_av = best
            if from_av:
                heapq.heappop(avail[e])
            else:
                heapq.heappop(future[e])
            o = byid[oid]
            if o["kind"] == "dma":
                etime[e] = st + 150.0
                fin = st + o["cost"]
            else:
                etime[e] = st + o["cost"]
                fin = etime[e]
            finish[oid] = fin
            order[e].append(o)
            remaining -= 1
            for s in succ[oid]:
                so = byid[s]
                lat = SYNC_NS if (so["eng"] != e or o["kind"] == "dma") else (60.0 if e != "pe" else 0.0)
                ready_t[s] = max(ready_t[s], fin + lat)
                indeg[s] -= 1
                if indeg[s] == 0:
                    heapq.heappush(future[so["eng"]], (ready_t[s], s))
        self.est_ns = getattr(self, "est_ns", 0.0) + max(list(finish.values()) + [0.0])
        for o in ops:
            if o["kind"] == "dma":
                k = o["semkey"]
                if k not in self.dma_sems:
                    self.dma_sems[k] = self._new_sem(f"d{len(self.dma_sems)}")
                    self.dma_vals[k] = 0
                self.dma_vals[k] += 16
                self.ticks[o["id"]] = (self.dma_sems[k], self.dma_vals[k], "dma")
        def needs_sem(o):
            for s_ in succ[o["id"]]:
                se = byid[s_]["eng"]
                if se != o["eng"] or (SAME_ENGINE_SYNC and se != "pe"):
                    return True
            return False
        for e in self.ENGS:
            real = [o for o in order[e] if o["kind"] == "op" and o["fn"] is not None]
            for i_, o in enumerate(real):
                o["sig"] = needs_sem(o) or i_ == len(real) - 1
        for e in self.ENGS:
            for o in order[e]:
                if o["kind"] == "op" and o["fn"] is not None and o["sig"]:
                    c = self.count[e]
                    ep, v = divmod(c, EPOCH)
                    while len(self.esems[e]) <= ep:
                        self.esems[e].append(self._new_sem(f"s_{e}_{len(self.esems[e])}"))
                    self.count[e] = c + 1
                    self.ticks[o["id"]] = (self.esems[e][ep], v + 1, e)
        for e in self.ENGS:
            for o in order[e]:
                waits = {}
                for p in o["preds"]:
                    if byid[p]["eng"] == e and byid[p]["kind"] == "op" and (not SAME_ENGINE_SYNC or e == "pe"):
                        continue
                    sem, val, src = self.ticks[p]
                    sid = id(sem)
                    if self.known[e].get(sid, 0) >= val:
                        continue
                    if sid not in waits or waits[sid][1] < val:
                        waits[sid] = (sem, val)
                for sid, (sem, val) in waits.items():
                    self.known[e][sid] = val
                inc = None
                if o["fn"] is not None and o["id"] in self.ticks:
                    sem, val, src = self.ticks[o["id"]]
                    inc = (sem, 16 if o["kind"] == "dma" else 1)
                self.streams[e].append((o["fn"], list(waits.values()), inc))
        self.seg = []
        self.seg_base = self.nops

    def barrier(self):
        if not self.enabled and not self.seg:
            return
        self._schedule_segment()
        ticks = []
        for e2 in self.ENGS:
            c = self.count[e2]
            if c > 0:
                ep, v = divmod(c - 1, EPOCH)
                ticks.append((self.esems[e2][ep], v + 1))
        for k, sem in self.dma_sems.items():
            ticks.append((sem, self.dma_vals[k]))
        for eng in self.ENGS:
            waits = []
            for (sem, val) in ticks:
                if self.known[eng].get(id(sem), 0) >= val:
                    continue
                self.known[eng][id(sem)] = val
                waits.append((sem, val))
            if waits:
                self.streams[eng].append((None, waits, None))

    def emit(self):
        self._schedule_segment()
        nc = self.nc
        with nc.Block() as block:
            def run(e, stream):
                for fn, waits, inc in stream:
                    for sem, val in waits:
                        e.wait_ge(sem, val)
                    if fn is None:
                        continue
                    ins = fn(e)
                    if inc is not None:
                        ins.then_inc(inc[0], inc[1])

            @block.tensor
            def _(e):
                run(e, self.streams["pe"])

            @block.scalar
            def _(e):
                run(e, self.streams["act"])

            @block.vector
            def _(e):
                run(e, self.streams["dve"])

            @block.gpsimd
            def _(e):
                if self.pool_init is not None:
                    self.pool_init(e)
                run(e, self.streams["pool"])

            @block.sync
            def _(e):
                run(e, self.streams["sp"])


def _fsz(ap):
    s = ap.shape
    n = 1
    for v in s[1:]:
        n *= int(v)
    return n


C_GQ, C_GK, C_GV, C_GR = 0, 512, 1024, 2048
C_GLF, C_GLB = 3072, 3088
C_DQ, C_DK, C_DV, C_DZ = 3104, 4128, 5152, 6176
C_DAB = 7200
C_MA, C_MB = 7232, 8256
D_IN = 9280


def host_consts():
    r = np.arange(128)[:, None]
    t = np.arange(128)[None, :]
    same = (r // 64) == (t // 64)
    c = {}
    c["ident"] = np.eye(128, dtype=np.float32)
    c["a_le"] = np.where(r <= t, -1.0 / 16, 0.0)
    c["a_ge"] = np.where(r >= t, -1.0 / 16, 0.0)
    c["a_gt"] = np.where(r > t, -1.0 / 16, 0.0)
    c["a_lt"] = np.where(r < t, -1.0 / 16, 0.0)
    c["m_le"] = np.where(r <= t, 1.0, 0.0)
    c["m_ge"] = np.where(r >= t, 1.0, 0.0)
    c["b_le"] = np.where((r <= t) & same, 1.0, 0.0)
    c["b_ge"] = np.where((r >= t) & same, 1.0, 0.0)
    c["b_gt"] = np.where((r > t) & same, 1.0, 0.0)
    c["b_lt"] = np.where((r < t) & same, 1.0, 0.0)
    c["csel0"] = np.where(r < 64, 1.0, 0.0) + 0.0 * t
    c["csel1"] = np.where(r >= 64, 1.0, 0.0) + 0.0 * t
    c["ones"] = np.ones((128, 128))
    c["m_lt"] = np.where(r < t, 1.0, 0.0)
    c["bvals"] = 128.0 * t + 0.0 * r
    c["pidx"] = 1.0 * r + 0.0 * t
    names = list(c.keys())
    arr = np.stack([np.asarray(c[n], np.float32) for n in names], axis=1)
    return names, np.ascontiguousarray(arr)


CONST_NAMES, CONST_ARR = host_consts()
NCONST = len(CONST_NAMES)


COST = dict(pe_a=55.0, pe_b=0.45, pe_f32=3.0, tr=110.0, act_a=150.0, act_b=1.0, dve_a=100.0, dve_b=0.6,
            pool_a=150.0, pool_b=1.9)


def build(stage="all", dbg=False):
    nc = bass.Bass("TRN2", target_bir_lowering=False)
    stack = contextlib.ExitStack()
    with stack:
        P = Prog(nc, stack)

        def dram(name, shape, dt=F32, kind="ExternalInput"):
            return nc.dram_tensor(name, list(shape), dt, kind=kind).ap()

        def sb(name, shape, dt=F32):
            return stack.enter_context(nc.sbuf_tensor(name, list(shape), dt))

        def ps(name, shape, dt=F32):
            return stack.enter_context(nc.psum_tensor(name, list(shape), dt))

        def MM(out, lhsT, rhs, start, stop, R, W):
            n = _fsz(rhs)
            c = COST["pe_a"] + n * COST["pe_b"]
            if rhs.dtype == F32:
                c *= COST["pe_f32"]
            P.op("pe", lambda e: e.matmul(out, lhsT, rhs, start=start, stop=stop), R, W, cost=c)

        def TR(out, in_, ident, R, W):
            P.op("pe", lambda e: e.transpose(out=out, in_=in_, identity=ident), R, W, cost=COST["tr"])

        def ACTF(out, in_, func, R, W, **kw):
            c = COST["act_a"] + _fsz(in_) * COST["act_b"] + (90.0 if "accum_out" in kw else 0.0)
            P.op("act", lambda e: e.activation(out=out, in_=in_, func=func, **kw), R, W, cost=c)

        def _vc(eng, n, k=1.5):
            return (COST["dve_a"] + n * k * COST["dve_b"]) if eng == "dve" else (COST["pool_a"] + n * COST["pool_b"])

        def TT(eng, out, in0, in1, op, R, W):
            P.op(eng, lambda e: e.tensor_tensor(out=out, in0=in0, in1=in1, op=op), R, W, cost=_vc(eng, _fsz(out)))

        def TS(eng, out, in0, s1, s2, op0, op1, R, W):
            P.op(eng, lambda e: e.tensor_scalar(out=out, in0=in0, scalar1=s1, scalar2=s2, op0=op0, op1=op1), R, W,
                 cost=_vc(eng, _fsz(out), 1.05))

        def STT(out, in0, scalar, in1, op0, op1, R, W):
            P.op("dve", lambda e: e.scalar_tensor_tensor(out=out, in0=in0, scalar=scalar, in1=in1, op0=op0, op1=op1), R, W,
                 cost=_vc("dve", _fsz(out)))

        def CP(eng, out, in_, R, W):
            if eng == "act":
                P.op("act", lambda e: e.activation(out=out, in_=in_, func=AF.Copy), R, W, cost=COST["act_a"] + _fsz(in_) * COST["act_b"])
            else:
                P.op(eng, lambda e: e.tensor_copy(out=out, in_=in_), R, W, cost=_vc(eng, _fsz(out), 1.05))

        def MEMSET(eng, ap, val, W):
            P.op(eng, lambda e: e.memset(ap, val), [], W, cost=_vc(eng, _fsz(ap), 0.6))

        def DMA(eng, out, in_, semkey, R, W):
            P.dma(eng, lambda e: e.dma_start(out=out, in_=in_), semkey, R, W, nbytes=_fsz(out) * int(out.shape[0]) * 4)

        def RECIP(out, in_, R, W):
            P.op("dve", lambda e: e.reciprocal(out=out, in_=in_), R, W, cost=_vc("dve", _fsz(out), 1.05))

        def MARK(name):
            if stage == name:
                P.enabled = False

        def rstd_inplace(ap, n, key):
            TS("dve", ap, ap, 1.0 / n, EPS, ALU.mult, ALU.add, [key], [key])
            ACTF(ap, ap, AF.Ln, [key], [key])
            ACTF(ap, ap, AF.Exp, [key], [key], scale=-0.5)

        x_d = dram("x", [T, D])
        n1_d = dram("norm1_w", [1, D])
        n2_d = dram("norm2_w", [1, D])
        nf_d = dram("norm_f_w", [1, D])
        consts_d = dram("consts", [128, NCONST, 128])
        w_in_d = dram("w_in", [D, D_IN])
        w2b_d = [dram("gla_w2b_f", [17, 512]), dram("gla_w2b_b", [17, 512])]
        gnw_d = dram("gla_norm_w", [1, 256])
        out_d = dram("out", [T, D], kind="ExternalOutput")
        dbg_d = dram("dbg", [T, D], kind="ExternalOutput") if dbg else None

        consts = sb("consts_sb", [128, NCONST, 128])
        CI = {n: i for i, n in enumerate(CONST_NAMES)}

        def cst(name):
            return consts[:, CI[name], :]

        ident_b = sb("ident_b", [128, 128], BF16)
        ones_b = sb("ones_b", [128, 128], BF16)
        hT = sb("hT", [128, KT, T], BF16)
        mixed = sb("mixed", [128, NT, D], BF16)
        small = sb("small", [128, 64])
        ARENA_BYTES = 134 * 1024
        arena = sb("arena", [128, ARENA_BYTES // 4])

        def carve(off, shape, dt, base=None, cap=None):
            base = arena if base is None else base
            cap = ARENA_BYTES if cap is None else cap
            nb = int(np.prod(shape)) * (2 if dt == BF16 else 4)
            nb = (nb + 3) // 4 * 4
            assert off % 4 == 0 and off + nb <= cap, (off, nb)
            v = base[:, off // 4:(off + nb) // 4]
            if dt != F32:
                v = v.bitcast(dt)
            if len(shape) == 2:
                pat = "p (a b) -> p a b"
                v = v.rearrange(pat, a=shape[0])
            elif len(shape) == 3:
                v = v.rearrange("p (a b c) -> p a b c", a=shape[0], b=shape[1])
            return v, off + nb

        pt = [ps(f"pt{i}", [128, 1024]) for i in range(3)]
        ptb = ps("ptb", [128, 2048], BF16)

        def bank(i):
            return pt[i // 2][:, (i % 2) * 512:(i % 2 + 1) * 512]

        def bkey(i):
            return ("pb", i)

        def bbank(i):
            return ptb[:, i * 1024:(i + 1) * 1024]

        DMA("sp", consts[:], consts_d[:, :, :], "c_consts", [], ["consts"])
        CP("dve", ident_b[:], cst("ident"), ["consts"], ["ident_b"])
        MEMSET("pool", ones_b[:], 1.0, ["ones_b"])

        off = 116 * 1024
        xt0, off = carve(off, [D], F32)
        xt1, off = carve(off, [D], F32)
        hn0, off = carve(off, [D], BF16)
        hn1, off = carve(off, [D], BF16)
        sq, off = carve(off, [D], BF16)
        n1_bc, off = carve(off, [D], F32)
        DMA("sp", n1_bc, n1_d.partition_broadcast(128), "c_n1", [], ["n1_bc"])
        xts = [xt0, xt1]
        hns = [hn0, hn1]
        for tt in range(NT):
            b = tt % 2
            xb, hb = xts[b], hns[b]
            DMA("sp", xb, x_d[tt * 128:(tt + 1) * 128, :], ("xt", b), [], [("xt", b)])
            ACTF(sq, xb, AF.Square, [("xt", b)], ["sq", "ss0"], accum_out=small[:, 0:1])
            rstd_inplace(small[:, 0:1], D, "ss0")
            STT(hb, xb, small[:, 0:1], n1_bc, ALU.mult, ALU.mult, [("xt", b), "ss0", "n1_bc"], [("hn", b)])
            for kt in range(KT):
                TR(bbank(b)[:, kt * 128:(kt + 1) * 128], hb[:, kt * 128:(kt + 1) * 128], ident_b[:],
                   [("hn", b), "ident_b"], [("pbb", b)])
            CP("act", hT[:, :, tt * 128:(tt + 1) * 128], bbank(b).rearrange("p (k t) -> p k t", k=KT),
               [("pbb", b)], [("hT", tt)])
        HT_ALL = [("hT", tt) for tt in range(NT)]
        MARK("p1")

        off = 0
        qT, off = carve(off, [T], F32)
        kT, off = carve(off, [T], F32)
        k_tok, off = carve(off, [NT, 128], F32)
        v_tok, off = carve(off, [NT, 256], BF16)
        qdT = [None, None]
        kiT = [None, None]
        ktail = [None, None]
        for d_ in range(2):
            qdT[d_], off = carve(off, [T], BF16)
            kiT[d_], off = carve(off, [T], BF16)
            ktail[d_], off = carve(off, [NT, 128], BF16)
        sb_store, off = carve(off, [NT, 256], BF16)
        dec, off = carve(off, [2, NT], F32)
        S, off = carve(off, [256], F32)
        S_bf, off = carve(off, [256], BF16)
        NTMP = 3
        tmp = []
        for i in range(NTMP):
            d = {}
            for nm in ("e", "lg", "E", "Ei", "Et"):
                d[nm], off = carve(off, [128], F32)
            d["Pf"], off = carve(off, [128], BF16)
            d["Pb"], off = carve(off, [128], BF16)
            d["sig"], off = carve(off, [512], F32)
            d["G"], off = carve(off, [256], F32)
            tmp.append(d)
        gl, off = carve(off, [2, T], BF16)
        w2b, off = carve(off, [2, 512], BF16)
        wqk, off = carve(off, [KT, 256], BF16)
        wkv, off = carve(off, [KT, 384], BF16)
        wgm, off = carve(off, [KT, 512], BF16)
        wgl, off = carve(off, [KT, 32], BF16)
        gnw_bc, off = carve(off, [256], F32)
        GLA_END = off
        assert GLA_END <= 116 * 1024, GLA_END

        DMA("sp", gnw_bc, gnw_d.partition_broadcast(128), "c_gnw", [], ["gnw_bc"])
        MEMSET("pool", gl[:, :, :], 1.0, ["gl"])
        MEMSET("pool", w2b[:, :, :], 0.0, ["w2b"])
        for d_ in range(2):
            DMA("pool", w2b[0:17, d_, :], w2b_d[d_][:, :], "c_w2b", [], ["w2b"])
        DMA("pool", wgl, w_in_d[:, C_GLF:C_GLF + 32].rearrange("(k p) c -> p k c", p=128), "w_wgl", [], ["wgl"])
        for d_ in range(2):
            for tg in range(4):
                bi = tg % 2
                for kt in range(KT):
                    MM(bank(bi)[0:16, :], wgl[:, kt, d_ * 16:(d_ + 1) * 16], hT[:, kt, tg * 512:(tg + 1) * 512],
                       kt == 0, kt == KT - 1, ["wgl"] + HT_ALL[tg * 4:tg * 4 + 4], [bkey(bi)])
                CP("act", gl[0:16, d_, tg * 512:(tg + 1) * 512], bank(bi)[0:16, :], [bkey(bi)], ["gl"])

        MARK("g0")
        QSCALE = 128.0 ** -0.5
        for h in range(4):
            def wcols(dst, c0, n):
                return (dst, w_in_d[:, c0:c0 + n].rearrange("(k p) c -> p k c", p=128))
            for (dst, src) in (wcols(wqk[:, :, 0:128], C_GQ + h * 128, 128), wcols(wqk[:, :, 128:256], C_GK + h * 128, 128)):
                DMA("pool", dst, src, "w_wqk", [], ["wqk"])
            for (dst, src) in (wcols(wkv[:, :, 0:128], C_GK + h * 128, 128), wcols(wkv[:, :, 128:384], C_GV + h * 256, 256)):
                DMA("pool", dst, src, "w_wkv", [], ["wkv"])
            for (dst, src) in (wcols(wgm[:, :, 0:256], C_GR + h * 256, 256), wcols(wgm[:, :, 256:512], C_MA + h * 256, 256)):
                DMA("pool", dst, src, "w_wgm", [], ["wgm"])
            MARK("g1a")
            for which, dstT in ((0, qT), (1, kT)):
                for tg in range(4):
                    bi = (which * 4 + tg) % 4
                    for kt in range(KT):
                        MM(bank(bi), wqk[:, kt, which * 128:(which + 1) * 128], hT[:, kt, tg * 512:(tg + 1) * 512],
                           kt == 0, kt == KT - 1, ["wqk"] + HT_ALL[tg * 4:tg * 4 + 4], [bkey(bi)])
                    CP("act" if tg % 2 else "dve", dstT[:, tg * 512:(tg + 1) * 512], bank(bi), [bkey(bi)],
                       [("qkT", which, tg)])
            MARK("g1b")
            for n in range(NT):
                bi = 4 + n % 2
                for kt in range(KT):
                    MM(bank(bi)[:, 0:384], hT[:, kt, n * 128:(n + 1) * 128], wkv[:, kt, :],
                       kt == 0, kt == KT - 1, ["wkv", ("hT", n)], [bkey(bi)])
                CP("dve", k_tok[:, n, :], bank(bi)[:, 0:128], [bkey(bi)], [("k_tok", n)])
                CP("act", v_tok[:, n, :], bank(bi)[:, 128:384], [bkey(bi)], [("v_tok", n)])
            MARK("g1")
            for n in range(NT):
                tsl = slice(n * 128, (n + 1) * 128)
                tg = n // 4
                for d_ in range(2):
                    tm = tmp[(n * 2 + d_) % NTMP]
                    tk = ("gtmp", (n * 2 + d_) % NTMP)
                    a_c = cst("a_le") if d_ == 0 else cst("a_ge")
                    a_s = cst("a_gt") if d_ == 0 else cst("a_lt")
                    b0 = (n * 2 + d_) % 2 * 2
                    zb, cb = bank(b0), bank(b0 + 1)
                    MM(zb[:, 0:128], gl[:, d_, tsl], w2b[:, d_, h * 128:(h + 1) * 128], True, True,
                       ["gl", "w2b"], [bkey(b0)])
                    ACTF(tm["e"], zb[:, 0:128], AF.Exp, [bkey(b0)], [tk], scale=-1.0)
                    ACTF(tm["lg"], tm["e"], AF.Ln, [tk], [tk], bias=1.0)
                    MM(cb[:, 0:128], tm["lg"], a_c, True, True, [tk, "consts"], [bkey(b0 + 1)])
                    MM(cb[:, 128:256], a_s, tm["lg"], True, True, [tk, "consts"], [bkey(b0 + 1)])
                    ACTF(tm["E"], cb[:, 0:128], AF.Exp, [bkey(b0 + 1)], [tk])
                    ACTF(tm["Ei"], cb[:, 0:128], AF.Exp, [bkey(b0 + 1)], [tk], scale=-1.0)
                    ACTF(tm["Et"], cb[:, 128:256], AF.Exp, [bkey(b0 + 1)], [tk])
                    STT(qdT[d_][:, tsl], qT[:, tsl], QSCALE, tm["E"], ALU.mult, ALU.mult,
                        [("qkT", 0, tg), tk], [("qdT", d_, n)])
                    TT("dve", kiT[d_][:, tsl], kT[:, tsl], tm["Ei"], ALU.mult, [("qkT", 1, tg), tk], [("kiT", d_, n)])
                    TT("dve", ktail[d_][:, n, :], k_tok[:, n, :], tm["Et"], ALU.mult, [("k_tok", n), tk], [("ktail", d_, n)])
                    col = 127 if d_ == 0 else 0
                    CP("dve", dec[:, d_, n:n + 1], tm["E"][:, col:col + 1], [tk], [("dec", d_, n)])
            MARK("g2")
            MEMSET("dve", S, 0.0, ["S"])
            for n in range(NT - 1, -1, -1):
                CP("act", sb_store[:, n, :], S, ["S"], [("sb_store", n)])
                bi = 4 + n % 2
                MM(bank(bi)[:, 0:256], ktail[1][:, n, :], v_tok[:, n, :], True, True,
                   [("ktail", 1, n), ("v_tok", n)], [bkey(bi)])
                STT(S, S, dec[:, 1, n:n + 1], bank(bi)[:, 0:256], ALU.mult, ALU.add,
                    ["S", ("dec", 1, n), bkey(bi)], ["S"])
            MARK("g3")
            MEMSET("dve", S, 0.0, ["S"])
            for n in range(NT):
                tsl = slice(n * 128, (n + 1) * 128)
                tm = tmp[n % NTMP]
                tk = ("ftmp", n % NTMP)
                CP("act", S_bf, S, ["S"], ["S_bf"])
                b0 = (n % 2) * 2
                sc = bank(b0)
                MM(sc[:, 0:128], kiT[0][:, tsl], qdT[0][:, tsl], True, True, [("kiT", 0, n), ("qdT", 0, n)], [bkey(b0)])
                MM(sc[:, 128:256], kiT[1][:, tsl], qdT[1][:, tsl], True, True, [("kiT", 1, n), ("qdT", 1, n)], [bkey(b0)])
                TT("dve", tm["Pf"], sc[:, 0:128], cst("m_le"), ALU.mult, [bkey(b0), "consts"], [tk])
                TT("dve", tm["Pb"], sc[:, 128:256], cst("m_ge"), ALU.mult, [bkey(b0), "consts"], [tk])
                ob = bank(b0 + 1)
                ok = bkey(b0 + 1)
                MM(ob[:, 0:256], qdT[0][:, tsl], S_bf, True, False, [("qdT", 0, n), "S_bf"], [ok])
                MM(ob[:, 0:256], qdT[1][:, tsl], sb_store[:, n, :], False, False, [("qdT", 1, n), ("sb_store", n)], [ok])
                MM(ob[:, 0:256], tm["Pf"], v_tok[:, n, :], False, False, [tk, ("v_tok", n)], [ok])
                MM(ob[:, 0:256], tm["Pb"], v_tok[:, n, :], False, True, [tk, ("v_tok", n)], [ok])
                kb = 4 + n % 2
                MM(bank(kb)[:, 0:256], ktail[0][:, n, :], v_tok[:, n, :], True, True,
                   [("ktail", 0, n), ("v_tok", n)], [bkey(kb)])
                STT(S, S, dec[:, 0, n:n + 1], bank(kb)[:, 0:256], ALU.mult, ALU.add,
                    ["S", ("dec", 0, n), bkey(kb)], ["S"])
                gb = 4 + n % 2
                for kt in range(KT):
                    MM(bank(gb), hT[:, kt, tsl], wgm[:, kt, :], kt == 0, kt == KT - 1, ["wgm", ("hT", n)], [bkey(gb)])
                ACTF(tm["sig"], bank(gb), AF.Exp, [bkey(gb)], [("sig", n % NTMP)], scale=-1.0)
                ACTF(tm["sig"], tm["sig"], AF.Ln, [("sig", n % NTMP)], [("sig", n % NTMP)], bias=1.0)
                ACTF(tm["sig"], tm["sig"], AF.Exp, [("sig", n % NTMP)], [("sig", n % NTMP)], scale=-1.0)
                TT("pool", tm["G"], tm["sig"][:, 0:256], tm["sig"][:, 256:512], ALU.mult, [("sig", n % NTMP)], [("G", n % NTMP)])
                TT("dve", tm["G"], tm["G"], bank(gb)[:, 0:256], ALU.mult, [("G", n % NTMP), bkey(gb)], [("G", n % NTMP)])
                TT("pool", tm["G"], tm["G"], gnw_bc, ALU.mult, [("G", n % NTMP), "gnw_bc"], [("G", n % NTMP)])
                ssk = ("ssq", n % 2)
                ssap = small[:, 2 + n % 2:3 + n % 2]
                ACTF(tm["sig"][:, 0:256], ob[:, 0:256], AF.Square, [ok, ("G", n % NTMP)], [("sig", n % NTMP), ssk],
                     accum_out=ssap)
                rstd_inplace(ssap, 256, ssk)
                STT(mixed[:, n, h * 256:(h + 1) * 256], ob[:, 0:256], ssap, tm["G"], ALU.mult, ALU.mult,
                    [ok, ssk, ("G", n % NTMP)], [("mixed", n)])

        P.barrier()
        HG = 4
        off = 0
        gqT, off = carve(off, [HG, T], BF16)
        gkT, off = carve(off, [HG, T], BF16)
        gvT, off = carve(off, [HG, T], BF16)
        dabs, off = carve(off, [NT, 32], F32)
        g_raw, off = carve(off, [NT, 2, 8], F32)
        beta, off = carve(off, [NT, 2, 8], F32)
        gvec, off = carve(off, [64], F32)
        wsl0, off = carve(off, [KT, 512], BF16)
        wsl1, off = carve(off, [KT, 512], BF16)
        wsl = [wsl0, wsl1]
        cwT, off = carve(off, [24, 5], F32)
        gdnw_bc, off = carve(off, [128], F32)
        wdab, off = carve(off, [KT, 32], BF16)
        TMP0 = off
        xc = [None, None]
        xc[0], off = carve(off, [T + 4], BF16)
        xc[1], off = carve(off, [T + 4], BF16)
        diag, off = carve(off, [5, 128], BF16)
        ce = [None, None]
        cy = [None, None]
        for i in range(2):
            ce[i], off = carve(off, [512], F32)
            cy[i], off = carve(off, [512], F32)
        cysq, off = carve(off, [512], BF16)
        crs, off = carve(off, [512], F32)
        CONV_END = off
        off = TMP0
        DB = []
        for d_ in range(2):
            dd = {}
            for nm in ("GMB", "Wd", "decT", "u_sb", "Sg"):
                dd[nm], off = carve(off, [HG, 128], F32)
            for nm in ("Lm", "LTm", "XT", "Pp0", "Pp1", "PTp0", "PTp1", "kbg", "vbeta", "qd_tok",
                       "attnT", "ktl0", "ktl1", "qdTg", "wT_sb", "vnew", "Sg_bf", "ostage"):
                dd[nm], off = carve(off, [HG, 128], BF16)
            dd["bg"], off = carve(off, [HG], F32)
            dd["et2"], off = carve(off, [2, HG], F32)
            DB.append(dd)
        oland = []
        for i in range(2):
            a, off = carve(off, [HG, 128], BF16)
            oland.append(a)
        osum, off = carve(off, [HG, 128], F32)
        fsig, off = carve(off, [512], F32)
        fG, off = carve(off, [HG, 128], F32)
        frs, off = carve(off, [8], F32)
        SWEEP_END = off
        gdn_o = nc.dram_tensor("gdn_o_spill", [NT, 128, HG * 128], BF16, kind="Internal").ap()

        gdnw_d = dram("gdn_norm_w", [1, 128])
        gvec_d = dram("gdn_vec", [1, 32])
        cw_d = dram("gdn_conv_wT", [128, 24, 5])
        DMA("sp", gdnw_bc, gdnw_d.partition_broadcast(128), "c_gdnw", [], ["gdnw_bc"])
        DMA("sp", gvec[:, 0:32], gvec_d.partition_broadcast(128), "c_gvec", [], ["gvec"])
        DMA("sp", cwT, cw_d[:, :, :], "c_cw", [], ["cwT"])
        DMA("pool", wdab, w_in_d[:, C_DAB:C_DAB + 32].rearrange("(k p) c -> p k c", p=128), "w_wdab", [], ["wdab"])
        ACTF(gvec[:, 16:32], gvec[:, 16:32], AF.Exp, ["gvec"], ["gvec"])
        TS("dve", gvec[:, 16:32], gvec[:, 16:32], -1.0, None, ALU.mult, ALU.bypass, ["gvec"], ["gvec"])
        for n in range(NT):
            bi = n % 2
            for kt in range(KT):
                MM(bank(bi)[:, 0:32], hT[:, kt, n * 128:(n + 1) * 128], wdab[:, kt, :], kt == 0, kt == KT - 1,
                   ["wdab", ("hT", n)], [bkey(bi)])
            CP("act", dabs[:, n, :], bank(bi)[:, 0:32], [bkey(bi)], ["dabs"])
        a_view = dabs[:, :, 0:16]
        b_view = dabs[:, :, 16:32]
        g_flat = g_raw.rearrange("p n d h -> p n (d h)")
        be_flat = beta.rearrange("p n d h -> p n (d h)")
        TT("dve", g_flat, a_view, gvec[:, 0:16].unsqueeze(1).to_broadcast([128, NT, 16]), ALU.add, ["dabs", "gvec"], ["g_raw"])
        ACTF(g_flat, g_flat, AF.Exp, ["g_raw"], ["g_raw"])
        ACTF(g_flat, g_flat, AF.Ln, ["g_raw"], ["g_raw"], bias=1.0)
        TT("dve", g_flat, g_flat, gvec[:, 16:32].unsqueeze(1).to_broadcast([128, NT, 16]), ALU.mult, ["g_raw", "gvec"], ["g_raw"])
        ACTF(be_flat, b_view, AF.Exp, ["dabs"], ["beta"], scale=-1.0)
        TS("dve", be_flat, be_flat, 1.0, None, ALU.add, ALU.bypass, ["beta"], ["beta"])
        RECIP(be_flat, be_flat, ["beta"], ["beta"])
        MARK("d0")

        GSCALE = 128.0 ** -0.5
        ident_bc4 = ident_b[:].unsqueeze(1).to_broadcast([128, HG, 128])

        def bc_h(ap2):
            return ap2.unsqueeze(2).to_broadcast([128, HG, 128])

        def bc_m(ap2):
            return ap2.unsqueeze(1).to_broadcast([128, HG, 128])

        def v4(ap2):
            return ap2.rearrange("p (h d) -> p h d", h=HG)

        for grp in range(2):
            hs0 = grp * HG
            for which, c_base, dstT in ((0, C_DQ, gqT), (1, C_DK, gkT), (2, C_DV, gvT)):
                ws = wsl[which % 2]
                wk = ("wsl", which % 2)
                DMA("pool", ws, w_in_d[:, c_base + hs0 * 128:c_base + (hs0 + HG) * 128].rearrange("(k p) c -> p k c", p=128),
                    ("w_wsl", which % 2), [], [wk])
                for hh in range(HG):
                    ci = which * 8 + hs0 + hh
                    xi = (which * HG + hh) % 2
                    xcb = xc[xi]
                    xk = ("xc", xi)
                    MEMSET("pool", xcb[:, 0:2], 0.0, [xk])
                    MEMSET("pool", xcb[:, T + 2:T + 4], 0.0, [xk])
                    for k in range(5):
                        TS("dve", diag[:, k, :], cst("ident"), cwT[:, ci, k:k + 1], None, ALU.mult, ALU.bypass,
                           ["consts", "cwT"], ["diag"])
                    for tg in range(4):
                        bi = tg % 2
                        for kt in range(KT):
                            MM(bank(bi), ws[:, kt, hh * 128:(hh + 1) * 128], hT[:, kt, tg * 512:(tg + 1) * 512],
                               kt == 0, kt == KT - 1, [wk] + HT_ALL[tg * 4:tg * 4 + 4], [bkey(bi)])
                        CP("act" if tg % 2 else "dve", xcb[:, 2 + tg * 512:2 + (tg + 1) * 512], bank(bi), [bkey(bi)], [xk])
                    for tg in range(4):
                        bi = 2 + tg % 2
                        i2 = tg % 2
                        for k in range(5):
                            MM(bank(bi), diag[:, k, :], xcb[:, tg * 512 + k:tg * 512 + k + 512], k == 0, k == 4,
                               ["diag", xk], [bkey(bi)])
                        ck = ("ctmp", i2)
                        ACTF(ce[i2], bank(bi), AF.Exp, [bkey(bi)], [ck], scale=-1.0)
                        ACTF(ce[i2], ce[i2], AF.Ln, [ck], [ck], bias=1.0)
                        ACTF(ce[i2], ce[i2], AF.Exp, [ck], [ck], scale=-1.0)
                        dst = dstT[:, hh, tg * 512:(tg + 1) * 512]
                        dk = ("gT", which, hh, tg)
                        if which == 2:
                            TT("dve", dst, ce[i2], bank(bi), ALU.mult, [ck, bkey(bi)], [dk])
                        else:
                            TT("dve", cy[i2], ce[i2], bank(bi), ALU.mult, [ck, bkey(bi)], [("cy", i2)])
                            TT("pool", cysq, cy[i2], cy[i2], ALU.mult, [("cy", i2)], ["cysq"])
                            MM(bank(4), ones_b[:], cysq, True, True, ["ones_b", "cysq"], [bkey(4)])
                            ACTF(crs, bank(4), AF.Ln, [bkey(4)], ["crs"], bias=EPS)
                            ACTF(crs, crs, AF.Exp, ["crs"], ["crs"], scale=-0.5)
                            if which == 0:
                                STT(dst, cy[i2], GSCALE, crs, ALU.mult, ALU.mult, [("cy", i2), "crs"], [dk])
                            else:
                                TT("dve", dst, cy[i2], crs, ALU.mult, [("cy", i2), "crs"], [dk])
            MARK("d1")
            P.barrier()
            DMA("pool", wsl[0], w_in_d[:, C_DZ + hs0 * 128:C_DZ + (hs0 + HG) * 128].rearrange("(k p) c -> p k c", p=128),
                ("w_wsl", 0), [], [("wsl", 0)])
            DMA("pool", wsl[1], w_in_d[:, C_MB + hs0 * 128:C_MB + (hs0 + HG) * 128].rearrange("(k p) c -> p k c", p=128),
                ("w_wsl", 1), [], [("wsl", 1)])

            def gT_keys(which, n):
                return [("gT", which, hh, n // 4) for hh in range(HG)]

            stored = set()
            esc_all = [dabs[:, 0:8, :].rearrange("p a b -> p (a b)").rearrange("p (k n h) -> p k n h", k=4, n=NT),
                       dabs[:, 8:16, :].rearrange("p a b -> p (a b)").rearrange("p (k n h) -> p k n h", k=4, n=NT)]
            for d_ in range(2):
                Mc_ = cst("b_le") if d_ == 0 else cst("b_ge")
                Ms_ = cst("b_gt") if d_ == 0 else cst("b_lt")
                for ki, mk in enumerate((Mc_, Ms_, cst("csel0"), cst("csel1"))):
                    MM(bank(d_)[:, ki * 64:(ki + 1) * 64].rearrange("p (n h) -> p n h", n=NT), mk,
                       g_raw[:, :, d_, hs0:hs0 + HG], True, True, ["consts", "g_raw"], [bkey(d_)])
                ACTF(esc_all[d_].rearrange("p k n h -> p (k n h)"), bank(d_)[:, 0:256], AF.Exp, [bkey(d_)], [("esc_all", d_), "dabs"])

            gb0 = ptb[:, 0:512]
            gb1 = ptb[:, 512:1024]
            xbank = ptb[:, 1024:2048].bitcast(F32)
            XK = ("pb", 7)

            def gdn_tile(d_, n):
                B = DB[d_]
                dk = lambda nm: (nm, d_)
                Mc = cst("b_le") if d_ == 0 else cst("b_ge")
                Ms = cst("b_gt") if d_ == 0 else cst("b_lt")
                bg = B["bg"]
                GMB, Wd, decT, Lm, LTm, XT = B["GMB"], B["Wd"], B["decT"], B["Lm"], B["LTm"], B["XT"]
                kbg, vbeta, qd_tok = B["kbg"], B["vbeta"], B["qd_tok"]
                Ppd = [B["Pp0"], B["Pp1"]]
                PTpd = [B["PTp0"], B["PTp1"]]
                pa, pb_ = (0, 1) if d_ == 0 else (2, 3)
                tsl = slice(n * 128, (n + 1) * 128)
                gv = g_raw[:, n, d_, hs0:hs0 + HG]
                bv = beta[:, n, d_, hs0:hs0 + HG]
                EA = esc_all[d_]
                e_cum, e_tail = EA[:, 0, n, :], EA[:, 1, n, :]
                second = n in stored
                if second:
                    par = n % 2
                    DMA("sp", oland[par].rearrange("p h d -> p (h d)"), gdn_o[n], ("oland", par), [("o_dram", n)], [("oland", par)])
                TT("dve", bg, bv, e_cum, ALU.mult, ["beta", ("esc_all", d_)], [dk("bg")])
                for hh in range(HG):
                    TR(gb0[:, hh * 128:(hh + 1) * 128], gkT[:, hh, tsl], ident_b[:], gT_keys(1, n) + ["ident_b"], [("pbb", 0)])
                for hh in range(HG):
                    TR(gb1[:, hh * 128:(hh + 1) * 128], gvT[:, hh, tsl], ident_b[:], gT_keys(2, n) + ["ident_b"], [("pbb", 0)])
                TT("dve", kbg, v4(gb0), bc_h(bg), ALU.mult, [("pbb", 0), dk("bg")], [dk("kbg")])
                for c_ in range(2):
                    TT("dve", B["et2"][:, c_, :], e_tail, cst("csel%d" % c_)[:, 0:HG], ALU.mult, [("esc_all", d_), "consts"], [dk("et2")])
                    TT("dve", B["ktl%d" % c_], v4(gb0), bc_h(B["et2"][:, c_, :]), ALU.mult, [("pbb", 0), dk("et2")], [dk("ktl%d" % c_)])
                TT("dve", vbeta, v4(gb1), bc_h(bv), ALU.mult, [("pbb", 0), "beta"], [dk("vbeta")])
                for hh in range(HG):
                    TR(gb0[:, hh * 128:(hh + 1) * 128], gqT[:, hh, tsl], ident_b[:], gT_keys(0, n) + ["ident_b"], [("pbb", 0)])
                TT("dve", qd_tok, v4(gb0), bc_h(e_cum), ALU.mult, [("pbb", 0), ("esc_all", d_)], [dk("qd_tok")])
                for hh in range(HG):
                    TR(gb1[:, hh * 128:(hh + 1) * 128], qd_tok[:, hh, :], ident_b[:], [dk("qd_tok"), "ident_b"], [("pbb", 0)])
                CP("act", B["qdTg"], v4(gb1), [("pbb", 0)], [dk("qdTg")])
                TT("pool", GMB, bc_h(gv), bc_m(Ms), ALU.mult, ["g_raw", "consts"], [dk("GMB")])
                MM(bank(pb_), Mc, GMB.rearrange("p h s -> p (h s)"), True, True, ["consts", dk("GMB")], [bkey(pb_)])
                ACTF(Wd.rearrange("p h s -> p (h s)"), bank(pb_), AF.Exp, [bkey(pb_)], [dk("Wd")])
                TT("pool", GMB, bc_h(bv), bc_m(Ms), ALU.mult, ["beta", "consts"], [dk("GMB")])
                TT("pool", Wd, Wd, GMB, ALU.mult, [dk("Wd"), dk("GMB")], [dk("Wd")])
                TT("pool", GMB, bc_h(gv), bc_m(Mc), ALU.mult, ["g_raw", "consts"], [dk("GMB")])
                MM(bank(pa), Ms, GMB.rearrange("p h s -> p (h s)"), True, True, ["consts", dk("GMB")], [bkey(pa)])
                ACTF(decT.rearrange("p h s -> p (h s)"), bank(pa), AF.Exp, [bkey(pa)], [dk("decT")])
                TT("pool", decT, decT, bc_m(Mc), ALU.mult, [dk("decT"), "consts"], [dk("decT")])
                for hh in range(HG):
                    MM(bank(pb_)[:, hh * 128:(hh + 1) * 128], gkT[:, hh, tsl], gkT[:, hh, tsl], True, True,
                       gT_keys(1, n), [bkey(pb_)])
                TT("dve", Lm, v4(bank(pb_)), Wd, ALU.mult, [bkey(pb_), dk("Wd")], [dk("Lm")])
                for hh in range(HG):
                    MM(bank(pa)[:, hh * 128:(hh + 1) * 128], gkT[:, hh, tsl], gqT[:, hh, tsl], True, True,
                       gT_keys(1, n) + gT_keys(0, n), [bkey(pa)])
                TT("dve", B["attnT"], v4(bank(pa)), decT, ALU.mult, [bkey(pa), dk("decT")], [dk("attnT")])
                for hh in range(HG):
                    TR(gb0[:, hh * 128:(hh + 1) * 128], Lm[:, hh, :], ident_b[:], [dk("Lm"), "ident_b"], [("pbb", 0)])
                CP("act", LTm, v4(gb0), [("pbb", 0)], [dk("LTm")])
                TT("dve", XT, ident_bc4, v4(gb0), ALU.subtract, ["ident_b", ("pbb", 0)], [dk("XT")])
                Pc, PTc = Lm, LTm
                pck, ptk_ = dk("Lm"), dk("LTm")
                for it in range(5):
                    Pn, PTn = Ppd[it % 2], PTpd[it % 2]
                    pnk, ptnk = ("Pp", it % 2, d_), ("PTp", it % 2, d_)
                    for hh in range(HG):
                        MM(bank(pb_)[:, hh * 128:(hh + 1) * 128], PTc[:, hh, :], Pc[:, hh, :], True, True, [pck, ptk_], [bkey(pb_)])
                    CP("act", Pn, v4(bank(pb_)), [bkey(pb_)], [pnk])
                    if it < 4:
                        for hh in range(HG):
                            MM(bank(pa)[:, hh * 128:(hh + 1) * 128], Pc[:, hh, :], PTc[:, hh, :], True, True, [pck, ptk_], [bkey(pa)])
                        CP("dve", PTn, v4(bank(pa)), [bkey(pa)], [ptnk])
                    for hh in range(HG):
                        MM(xbank[:, hh * 128:(hh + 1) * 128], Pn[:, hh, :], XT[:, hh, :], True, True, [pnk, dk("XT")], [XK])
                    TT("dve", XT, XT, v4(xbank), ALU.add, [dk("XT"), XK], [dk("XT")])
                    Pc, PTc, pck, ptk_ = Pn, PTn, pnk, ptnk
                for hh in range(HG):
                    MM(bank(pa)[:, hh * 128:(hh + 1) * 128], XT[:, hh, :], vbeta[:, hh, :], True, True, [dk("XT"), dk("vbeta")], [bkey(pa)])
                CP("act", B["u_sb"], v4(bank(pa)), [bkey(pa)], [dk("u_sb")])
                for hh in range(HG):
                    MM(bank(pb_)[:, hh * 128:(hh + 1) * 128], kbg[:, hh, :], XT[:, hh, :], True, True, [dk("kbg"), dk("XT")], [bkey(pb_)])
                CP("dve", B["wT_sb"], v4(bank(pb_)), [bkey(pb_)], [dk("wT_sb")])
                sb0 = 4
                Sg, Sg_bf, vnew = B["Sg"], B["Sg_bf"], B["vnew"]
                chunks = (0, 1) if d_ == 0 else (1, 0)
                for c in chunks:
                    sl = slice(c * 64, c * 64 + 64)
                    for hh in range(HG):
                        MM(bank(sb0)[:, hh * 128:(hh + 1) * 128], B["wT_sb"][:, hh, :], Sg_bf[:, hh, :], True, True,
                           [dk("wT_sb"), dk("Sg_bf")], [bkey(sb0)])
                    TT("dve", vnew[sl], B["u_sb"][sl], v4(bank(sb0))[sl], ALU.subtract, [dk("u_sb"), bkey(sb0)], [dk("vnew")])
                    for hh in range(HG):
                        MM(bank(sb0 + 1)[:, hh * 128:(hh + 1) * 128], B["qdTg"][:, hh, :], Sg_bf[:, hh, :], True, False,
                           [dk("qdTg"), dk("Sg_bf")], [bkey(sb0 + 1)])
                        MM(bank(sb0 + 1)[:, hh * 128:(hh + 1) * 128], B["attnT"][:, hh, :], vnew[:, hh, :], False, True,
                           [dk("attnT"), dk("vnew")], [bkey(sb0 + 1)])
                    for hh in range(HG):
                        MM(bank(sb0)[:, hh * 128:(hh + 1) * 128], B["ktl%d" % c][:, hh, :], vnew[:, hh, :], True, True,
                           [dk("ktl%d" % c), dk("vnew")], [bkey(sb0)])
                    TT("pool", Sg, Sg, bc_h(EA[:, 2 + c, n, :]), ALU.mult, [dk("Sg"), ("esc_all", d_)], [dk("Sg")])
                    TT("dve", Sg, Sg, v4(bank(sb0)), ALU.add, [dk("Sg"), bkey(sb0)], [dk("Sg")])
                    CP("act", Sg_bf, Sg, [dk("Sg")], [dk("Sg_bf")])
                    if not second:
                        CP("act", B["ostage"][sl], v4(bank(sb0 + 1))[sl], [bkey(sb0 + 1)], [dk("ostage")])
                    else:
                        TT("dve", osum[sl], v4(bank(sb0 + 1))[sl], oland[n % 2][sl], ALU.add,
                           [bkey(sb0 + 1), ("oland", n % 2)], ["osum"])
                if not second:
                    stored.add(n)
                    DMA("sp", gdn_o[n], B["ostage"].rearrange("p h d -> p (h d)"), ("ost", d_), [dk("ostage")], [("o_dram", n)])
                    return
                osq = fsig.rearrange("p (h d) -> p h d", h=HG)
                TT("pool", osq, osum, osum, ALU.mult, ["osum"], ["fsig"])
                P.op("dve", lambda e: e.tensor_reduce(out=frs[:, 0:HG], in_=osq, axis=AX.X, op=ALU.add), ["fsig"], ["frs"], cost=600.0)
                rstd_inplace(frs[:, 0:HG], 128, "frs")
                for half, ws in enumerate(wsl):
                    for kt in range(KT):
                        MM(bank(half), hT[:, kt, tsl], ws[:, kt, :], kt == 0, kt == KT - 1,
                           [("wsl", half), ("hT", n)], [bkey(half)])
                fGf = fG.rearrange("p h d -> p (h d)")
                for half in range(2):
                    ACTF(fsig, bank(half), AF.Exp, [bkey(half)], ["fsig"], scale=-1.0)
                    ACTF(fsig, fsig, AF.Ln, ["fsig"], ["fsig"], bias=1.0)
                    ACTF(fsig, fsig, AF.Exp, ["fsig"], ["fsig"], scale=-1.0)
                    if half == 0:
                        TT("dve", fGf, fsig, bank(0), ALU.mult, ["fsig", bkey(0)], ["fG"])
                    else:
                        TT("pool", fGf, fGf, fsig, ALU.mult, ["fsig", "fG"], ["fG"])
                TT("pool", fG, fG, bc_m(gdnw_bc), ALU.mult, ["fG", "gdnw_bc"], ["fG"])
                TT("pool", osum, osum, bc_h(frs[:, 0:HG]), ALU.mult, ["osum", "frs"], ["osum"])
                TT("pool", osum, osum, fG, ALU.mult, ["osum", "fG"], ["osum"])
                mslice = mixed[:, n, hs0 * 128:(hs0 + HG) * 128].rearrange("p (h d) -> p h d", h=HG)
                TT("dve", mslice, mslice, osum, ALU.add, ["osum", ("mixed", n)], [("mixed", n)])

            for d_ in range(2):
                MEMSET("pool", DB[d_]["vnew"], 0.0, [("vnew", d_)])
                MEMSET("dve", DB[d_]["Sg"], 0.0, [("Sg", d_)])
                CP("act", DB[d_]["Sg_bf"], DB[d_]["Sg"], [("Sg", d_)], [("Sg_bf", d_)])
            for i in range(NT):
                gdn_tile(0, i)
                gdn_tile(1, NT - 1 - i)
            MARK("d2")
            P.barrier()

        P.barrier()
        wout_d = dram("w_out", [D, D])
        off = 0
        x1, off = carve(off, [NT, D], F32)
        X1_END = off
        mT, off = carve(off, [KT, T], BF16)
        wout, off = carve(off, [KT, D], BF16)
        hn2 = [None, None]
        hn2[0], off = carve(off, [D], BF16)
        hn2[1], off = carve(off, [D], BF16)
        junk, off = carve(off, [D], BF16)
        n2_bc, off = carve(off, [D], F32)
        DMA("sp", n2_bc, n2_d.partition_broadcast(128), "c_n2", [], ["n2_bc"])
        DMA("pool", wout, wout_d.rearrange("(k p) c -> p k c", p=128), "w_wout", [], ["wout"])
        for n in range(NT):
            b = n % 2
            for kt in range(KT):
                TR(bbank(b)[:, kt * 128:(kt + 1) * 128], mixed[:, n, kt * 128:(kt + 1) * 128], ident_b[:],
                   [("mixed", n), "ident_b"], [("pbb", b)])
            CP("act", mT[:, :, n * 128:(n + 1) * 128], bbank(b).rearrange("p (k t) -> p k t", k=KT), [("pbb", b)], [("mT", n)])
        for n in range(NT):
            tsl = slice(n * 128, (n + 1) * 128)
            DMA("sp", x1[:, n, :], x_d[tsl, :], ("x1ld", n % 4), [], [("x1", n)])
            pp = pt[n % 2]
            for half in range(2):
                for kt in range(KT):
                    MM(pp[:, half * 512:(half + 1) * 512], mT[:, kt, tsl], wout[:, kt, half * 512:(half + 1) * 512],
                       kt == 0, kt == KT - 1, [("mT", n), "wout"], [bkey((n % 2) * 2 + half)])
            TT("dve", x1[:, n, :], x1[:, n, :], pp[:, :], ALU.add, [("x1", n), bkey((n % 2) * 2), bkey((n % 2) * 2 + 1)], [("x1", n)])
            b = n % 2
            ssap = small[:, 8 + b:9 + b]
            ssk = ("ss2", b)
            ACTF(junk, x1[:, n, :], AF.Square, [("x1", n)], ["junk", ssk], accum_out=ssap)
            rstd_inplace(ssap, D, ssk)
            STT(mixed[:, n, :], x1[:, n, :], ssap, n2_bc, ALU.mult, ALU.mult, [("x1", n), ssk, "n2_bc"], [("mixed", n)])
            for kt in range(KT):
                TR(bbank(b)[:, kt * 128:(kt + 1) * 128], mixed[:, n, kt * 128:(kt + 1) * 128], ident_b[:],
                   [("mixed", n), "ident_b"], [("pbb", b)])
            CP("act", hT[:, :, tsl], bbank(b).rearrange("p (k t) -> p k t", k=KT), [("pbb", b)], [("hT", n)])
        MARK("e0")

        P.barrier()
        NB = 64
        wr_d = dram("moe_wr", [D, 36])
        wgu0_d = dram("moe_wgu0", [4096, 2048])
        wgu1_d = dram("moe_wgu1", [4096, 2048])
        wdr_d = dram("moe_wdr", [4096, 2048])
        xb_d = nc.dram_tensor("moe_xb", [NB * 128, D], BF16, kind="Internal").ap()
        yb_d = nc.dram_tensor("moe_yb", [NB * 128, D], F32, kind="Internal").ap()
        off = X1_END
        stg = []
        for i in range(3):
            a, off = carve(off, [2048], F32)
            stg.append(a)
        wgu_bf = []
        wd_bf = []
        for i in range(2):
            a, off = carve(off, [KT, 512], BF16)
            wgu_bf.append(a)
            a, off = carve(off, [2, D], BF16)
            wd_bf.append(a)
        wr, off = carve(off, [KT, 36], BF16)
        lg, off = carve(off, [NT, 36], F32)
        oh1, off = carve(off, [NT, 32], F32)
        oh2, off = carve(off, [NT, 32], F32)
        msk, off = carve(off, [NT, 32], F32)
        rank, off = carve(off, [NT, 32], F32)
        tmp3, off = carve(off, [NT, 32], F32)
        gtmp, off = carve(off, [NT, 4], F32)
        ohg, off = carve(off, [NT, 4], F32)
        rv, off = carve(off, [8, NT], F32)
        mcum, off = carve(off, [32], F32)
        cnt, off = carve(off, [32], F32)
        padded, off = carve(off, [32], F32)
        ends, off = carve(off, [32], F32)
        pstart, off = carve(off, [32], F32)
        ebf, off = carve(off, [NB], F32)
        widx_f, off = carve(off, [NB], F32)
        widx, off = carve(off, [NB], I32)
        dest_f, off = carve(off, [2, NT], F32)
        dest_i, off = carve(off, [2, NT], I32)
        MOE_END = off
        cmpb = stg[0].rearrange("p (b e) -> p b e", b=NB)
        cmpj = stg[1][:, 0:512].rearrange("p (e j) -> p e j", e=32)
        ht32 = hT[:].rearrange("p k t -> p (k t)").bitcast(F32)
        HTCAP = 32 * 1024
        hoff = 0
        xg, xgT, sil, hid_bf, hidT, ysb = [], [], [], [], [], []
        for i in range(2):
            a, hoff = carve(hoff, [D], BF16, ht32, HTCAP); xg.append(a)
            a, hoff = carve(hoff, [KT, 128], BF16, ht32, HTCAP); xgT.append(a)
            a, hoff = carve(hoff, [256], F32, ht32, HTCAP); sil.append(a)
            a, hoff = carve(hoff, [256], BF16, ht32, HTCAP); hid_bf.append(a)
            a, hoff = carve(hoff, [2, 128], BF16, ht32, HTCAP); hidT.append(a)
            a, hoff = carve(hoff, [D], F32, ht32, HTCAP); ysb.append(a)
        mix32 = mixed[:].rearrange("p n d -> p (n d)").bitcast(F32)
        MIXCAP = 32 * 1024
        moff = 0
        yg = []
        for i in range(2):
            a, moff = carve(moff, [D], F32, mix32, MIXCAP); yg.append(a)
        stgB = []
        for i in range(3):
            a, moff = carve(moff, [2048], F32, mix32, MIXCAP); stgB.append(a)

        DMA("pool", wr, wr_d.rearrange("(k p) c -> p k c", p=128), "w_wr", [], ["wr"])
        for n in range(NT):
            bi = n % 2
            for kt in range(KT):
                MM(bank(bi)[:, 0:36], hT[:, kt, n * 128:(n + 1) * 128], wr[:, kt, :], kt == 0, kt == KT - 1,
                   ["wr", ("hT", n)], [bkey(bi)])
            CP("act", lg[:, n, :], bank(bi)[:, 0:36], [bkey(bi)], ["lg"])
        BIG = 10000.0
        glv = lg[:, :, 0:4]
        elv = lg[:, :, 4:36]

        def RED(out, in_, op, R, W):
            P.op("dve", lambda e: e.tensor_reduce(out=out, in_=in_, axis=AX.X, op=op), R, W, cost=100.0 + _fsz(in_) * 1.0)

        def bcn(ap2, k):
            return ap2.unsqueeze(2).to_broadcast([128, NT, k])

        gmax, gsum, m1, m2, w1, w2 = (rv[:, i, :] for i in range(6))
        RED(gmax, glv, ALU.max, ["lg"], ["rv"])
        TT("dve", ohg, glv, bcn(gmax, 4), ALU.is_equal, ["lg", "rv"], ["ohg"])
        TT("dve", gtmp, glv, bcn(gmax, 4), ALU.subtract, ["lg", "rv"], ["gtmp"])
        ACTF(gtmp, gtmp, AF.Exp, ["gtmp"], ["gtmp"])
        RED(gsum, gtmp, ALU.add, ["gtmp"], ["rv"])
        RECIP(gsum, gsum, ["rv"], ["rv"])
        TS("dve", ohg, ohg, BIG, -BIG, ALU.mult, ALU.add, ["ohg"], ["ohg"])
        TT("dve", msk.rearrange("p n (g e) -> p n g e", g=4), elv.rearrange("p n (g e) -> p n g e", g=4),
           ohg.unsqueeze(3).to_broadcast([128, NT, 4, 8]), ALU.add, ["lg", "ohg"], ["msk"])
        RED(m1, msk, ALU.max, ["msk"], ["rv"])
        TT("dve", oh1, msk, bcn(m1, 32), ALU.is_equal, ["msk", "rv"], ["oh1"])
        STT(msk, oh1, -BIG, msk, ALU.mult, ALU.add, ["oh1", "msk"], ["msk"])
        RED(m2, msk, ALU.max, ["msk"], ["rv"])
        TT("dve", oh2, msk, bcn(m2, 32), ALU.is_equal, ["msk", "rv"], ["oh2"])
        TT("dve", w2, m2, m1, ALU.subtract, ["rv"], ["rv"])
        ACTF(w2, w2, AF.Exp, ["rv"], ["rv"])
        TS("dve", w1, w2, 1.0, None, ALU.add, ALU.bypass, ["rv"], ["rv"])
        RECIP(w1, w1, ["rv"], ["rv"])
        TT("dve", w1, w1, gsum, ALU.mult, ["rv"], ["rv"])
        TT("dve", w2, w2, w1, ALU.mult, ["rv"], ["rv"])
        TT("dve", msk, oh1, oh2, ALU.add, ["oh1", "oh2", "msk"], ["msk"])
        MEMSET("dve", mcum, 0.0, ["mcum"])
        for n in range(NT):
            bi = n % 2
            MM(bank(bi)[:, 0:32], cst("m_lt"), msk[:, n, :], True, False, ["consts", "msk"], [bkey(bi)])
            MM(bank(bi)[:, 0:32], cst("ones"), mcum, False, True, ["consts", "mcum"], [bkey(bi)])
            CP("act", rank[:, n, :], bank(bi)[:, 0:32], [bkey(bi)], ["rank"])
            TT("dve", mcum, mcum, msk[:, n, :], ALU.add, ["mcum", "msk"], ["mcum"])
        MM(bank(0)[:, 0:32], cst("ones"), mcum, True, True, ["consts", "mcum"], [bkey(0)])
        CP("act", cnt, bank(0)[:, 0:32], [bkey(0)], ["cnt"])
        TT("dve", cmpj, cnt.unsqueeze(2).to_broadcast([128, 32, 16]),
           cst("bvals")[:, 0:16].unsqueeze(1).to_broadcast([128, 32, 16]), ALU.is_gt, ["cnt", "consts"], [("stg", 1)])
        RED(padded, cmpj, ALU.add, [("stg", 1)], ["padded"])
        TS("dve", padded, padded, 128.0, None, ALU.mult, ALU.bypass, ["padded"], ["padded"])
        P.op("dve", lambda e: e.tensor_tensor_scan(out=ends, data0=cst("ones")[:, 0:32], data1=padded, initial=0.0,
                                                  op0=ALU.mult, op1=ALU.add), ["consts", "padded"], ["ends"], cost=300.0)
        TT("dve", pstart, ends, padded, ALU.subtract, ["ends", "padded"], ["pstart"])
        TT("dve", rank, rank, pstart.unsqueeze(1).to_broadcast([128, NT, 32]), ALU.add, ["rank", "pstart"], ["rank"])
        for k, ohk in ((0, oh1), (1, oh2)):
            TT("dve", tmp3, ohk, rank, ALU.mult, ["oh1", "oh2", "rank"], ["tmp3"])
            RED(dest_f[:, k, :], tmp3, ALU.add, ["tmp3"], ["dest_f"])
        CP("dve", dest_i, dest_f, ["dest_f"], ["dest_i"])
        TT("dve", cmpb, ends.unsqueeze(1).to_broadcast([128, NB, 32]),
           cst("bvals")[:, 0:NB].unsqueeze(2).to_broadcast([128, NB, 32]), ALU.is_le, ["ends", "consts"], [("stg", 0)])
        RED(ebf, cmpb, ALU.add, [("stg", 0)], ["ebf"])
        STT(widx_f, ebf, 128.0, cst("pidx")[:, 0:NB], ALU.mult, ALU.add, ["ebf", "consts"], ["widx_f"])
        CP("dve", widx, widx_f, ["widx_f"], ["widx"])
        MARK("e1")

        IOA = bass.IndirectOffsetOnAxis
        regs = {}

        def _pool_init(e):
            regs["bc"] = e.alloc_register("moe_bc")
            e.reg_mov(regs["bc"], 4095)
        P.pool_init = _pool_init
        XB_KEYS = []
        zt, off = carve(off, [D], BF16)
        MEMSET("pool", zt, 0.0, ["zt"])
        DMA("sp", xb_d.rearrange("(p r) d -> p r d", p=128), zt.unsqueeze(1).to_broadcast([128, NB, D]), "xbz", ["zt"], ["xb0"])
        for n in range(NT):
            for k in range(2):
                idx_ap = dest_i[:, k, n:n + 1]
                src_ap = mixed[:, n, :]
                P.dma("pool", lambda e, idx_ap=idx_ap, src_ap=src_ap: e.indirect_dma_start(
                    out=xb_d[:, :], out_offset=IOA(ap=idx_ap, axis=0), in_=src_ap, in_offset=None),
                    ("sc", (2 * n + k) % 4), [("mixed", n), "dest_i", "xb0"], [("xb", n, k)], nbytes=256 * 1024)
                XB_KEYS.append(("xb", n, k))

        def gather_w(dst, src_d, b, skey, extra):
            idx_ap = widx[:, b:b + 1]
            P.dma("pool", lambda e: e.indirect_dma_start(
                out=dst, out_offset=None, in_=src_d[:, :], in_offset=IOA(ap=idx_ap, axis=0),
                bounds_check=regs["bc"], oob_is_err=False),
                skey, ["widx"] + extra, [skey], nbytes=1 << 20)

        YB_KEYS = []
        for b in range(NB):
            s = b % 2
            sset = stg if b % 2 == 0 else stgB
            so = 0 if b % 2 == 0 else 3
            extra = [] if b % 2 == 0 else XB_KEYS
            gather_w(sset[0], wgu0_d, b, ("stg", so + 0), extra)
            gather_w(sset[1], wgu1_d, b, ("stg", so + 1), extra)
            gather_w(sset[2], wdr_d, b, ("stg", so + 2), extra)
            CP("act", wgu_bf[s][:, 0:4, :], sset[0].rearrange("p (k c) -> p k c", k=4), [("stg", so + 0)], [("wgu_bf", s, 0)])
            CP("dve", wgu_bf[s][:, 4:8, :], sset[1].rearrange("p (k c) -> p k c", k=4), [("stg", so + 1)], [("wgu_bf", s, 1)])
            CP("act" if b % 4 < 2 else "dve", wd_bf[s], sset[2].rearrange("p (k c) -> p k c", k=2), [("stg", so + 2)], [("wd_bf", s)])
            DMA("sp", xg[s], xb_d[b * 128:(b + 1) * 128, :], ("xg", s), XB_KEYS, [("xg", s)])
            for kt in range(KT):
                TR(bbank(s)[:, kt * 128:(kt + 1) * 128], xg[s][:, kt * 128:(kt + 1) * 128], ident_b[:],
                   [("xg", s), "ident_b"], [("pbb", s)])
            CP("act", xgT[s], bbank(s).rearrange("p (k t) -> p k t", k=KT), [("pbb", s)], [("xgT", s)])
            hb = bank(s)
            for kt in range(KT):
                MM(hb, xgT[s][:, kt, :], wgu_bf[s][:, kt, :], kt == 0, kt == KT - 1,
                   [("xgT", s), ("wgu_bf", s, 0), ("wgu_bf", s, 1)], [bkey(s)])
            ACTF(sil[s], hb[:, 0:256], AF.Silu, [bkey(s)], [("sil", s)])
            TT("dve", hid_bf[s], sil[s], hb[:, 256:512], ALU.mult, [("sil", s), bkey(s)], [("hid_bf", s)])
            for ft in range(2):
                TR(bbank(s)[:, ft * 128:(ft + 1) * 128], hid_bf[s][:, ft * 128:(ft + 1) * 128], ident_b[:],
                   [("hid_bf", s), "ident_b"], [("pbb", s)])
            CP("act", hidT[s], bbank(s)[:, 0:256].rearrange("p (k t) -> p k t", k=2), [("pbb", s)], [("hidT", s)])
            yp = pt[1 + s]
            for half in range(2):
                for ft in range(2):
                    MM(yp[:, half * 512:(half + 1) * 512], hidT[s][:, ft, :], wd_bf[s][:, ft, half * 512:(half + 1) * 512],
                       ft == 0, ft == 1, [("hidT", s), ("wd_bf", s)], [bkey(2 + 2 * s + half)])
            CP("act" if b % 2 else "dve", ysb[s], yp[:, :], [bkey(2 + 2 * s), bkey(3 + 2 * s)], [("ysb", s)])
            DMA("sp", yb_d[b * 128:(b + 1) * 128, :], ysb[s], ("yst", s), [("ysb", s)], [("yb", b)])
            YB_KEYS.append(("yb", b))
        MARK("e2")
        ygs = list(yg)
        for sb_ in stgB:
            ygs.append(sb_[:, 0:1024])
            ygs.append(sb_[:, 1024:2048])
        for n in range(NT):
            for k in range(2):
                s = (2 * n + k) % len(ygs)
                idx_ap = dest_i[:, k, n:n + 1]
                dst = ygs[s]
                P.dma("pool", lambda e, idx_ap=idx_ap, dst=dst: e.indirect_dma_start(
                    out=dst, out_offset=None, in_=yb_d[:, :], in_offset=IOA(ap=idx_ap, axis=0)),
                    ("yg", s), YB_KEYS + ["dest_i"], [("yg", s)], nbytes=512 * 1024)
                wk = rv[:, 4 + k, n:n + 1]
                STT(x1[:, n, :], ygs[s], wk, x1[:, n, :], ALU.mult, ALU.add, [("yg", s), "rv", ("x1", n)], [("x1", n)])

        P.barrier()
        off = X1_END
        nf_bc, off = carve(off, [D], F32)
        ob = [None, None]
        ob[0], off = carve(off, [D], F32)
        ob[1], off = carve(off, [D], F32)
        junk2, off = carve(off, [D], BF16)
        DMA("sp", nf_bc, nf_d.partition_broadcast(128), "c_nf", [], ["nf_bc"])
        for n in range(NT):
            b = n % 2
            ssap = small[:, 12 + b:13 + b]
            ssk = ("ss3", b)
            ACTF(junk2, x1[:, n, :], AF.Square, [("x1", n)], ["junk2", ssk], accum_out=ssap)
            rstd_inplace(ssap, D, ssk)
            STT(ob[b], x1[:, n, :], ssap, nf_bc, ALU.mult, ALU.mult, [("x1", n), ssk, "nf_bc"], [("ob", b)])
            DMA("sp", out_d[n * 128:(n + 1) * 128, :], ob[b], ("out_st", b), [("ob", b)], [("out", n)])
        if not dbg:
            P.wait_all("sp", [("out", n) for n in range(NT)])
        if dbg:
            P.enabled = True
            P.barrier()
            for n in range(NT):
                DMA("sp", dbg_d[n * 128:(n + 1) * 128, :], x1[:, n, :], ("dbg_out", n % 2), [("x1", n)], [("dbg", n)])
            P.wait_all("sp", [("dbg", n) for n in range(NT)] + [("out", n) for n in range(NT)])
        P.emit()
    return nc


def make_in_maps(inputs, n_cores=8):
    f = lambda k: np.asarray(inputs[k], np.float32)
    x = f("x")
    _gu = np.concatenate([f("moe_w_gate")[0], f("moe_w_up")[0]], axis=2).reshape(32, 8, 128, 512).transpose(0, 2, 1, 3)
    shared = {
        "norm1_w": f("norm1_w").reshape(1, D),
        "norm2_w": f("norm2_w").reshape(1, D),
        "norm_f_w": f("norm_f_w").reshape(1, D),
        "consts": CONST_ARR,
        "w_in": np.ascontiguousarray(f("w_in")[0]),
        "gla_w2b_f": np.ascontiguousarray(np.concatenate([f("gla_gate_w2_fwd")[0], f("gla_gate_b_fwd")], axis=0)),
        "gla_w2b_b": np.ascontiguousarray(np.concatenate([f("gla_gate_w2_bwd")[0], f("gla_gate_b_bwd")], axis=0)),
        "gla_norm_w": f("gla_norm_w").reshape(1, 256),
        "w_out": np.ascontiguousarray(f("w_out")[0]),
        "moe_wr": np.ascontiguousarray(np.concatenate([f("moe_w_group")[0], f("moe_w_router")[0]], axis=1)),
        "moe_wgu0": _gu[:, :, 0:4, :].reshape(4096, 2048).copy(),
        "moe_wgu1": _gu[:, :, 4:8, :].reshape(4096, 2048).copy(),
        "moe_wdr": np.ascontiguousarray(f("moe_w_down")[0].reshape(32, 2, 128, 1024).transpose(0, 2, 1, 3)).reshape(4096, 2048),
        "gdn_norm_w": f("gdn_norm_w").reshape(1, 128),
        "gdn_vec": np.ascontiguousarray(np.concatenate([f("gdn_dt_bias_fwd")[0], f("gdn_dt_bias_bwd")[0],
                                                        f("gdn_a_log_fwd")[0], f("gdn_a_log_bwd")[0]]).reshape(1, 32)),
        "gdn_conv_wT": np.ascontiguousarray(f("gdn_conv_w")[0].T.reshape(24, 128, 5).transpose(1, 0, 2)),
    }
    maps = []
    for c in range(n_cores):
        m = dict(shared)
        m["x"] = np.ascontiguousarray(x[c])
        maps.append(m)
    return maps


def kernel(**inputs):
    nc = build()
    in_maps = make_in_maps(inputs)
    res = run_bass_kernel_spmd(nc, in_maps, core_ids=list(range(8)))
    out = np.stack([np.asarray(r["out"]) for r in res.results], axis=0)
    return out.astype(np.float32)
```
